# Optimizing a Trainium2 kernel written in Bass

```python
import math
import jax, jax.numpy as jnp
from jax import lax
import numpy as np

D_MODEL = 1024
BATCH = 8
SEQ = 4096
DEPTH = 2

CTX_LEN = 256
GRID_W = 64
EPS = 1e-6
GN_EPS = 64e-5
ROPE_BASE = 10000.0
NEG_INF = -1e30

MIX_W = 256
N_BRANCH = 4
HEAD_DIM = 64

RWKV_HEADS = MIX_W // HEAD_DIM
RWKV_DECAY_RANK = 32
RWKV_ICLR_RANK = 32
RWKV_GATE_RANK = 64

GLA_HEADS = 4
GLA_DK = 32
GLA_DV = 64
GLA_RANK = 16
GLA_TAU = 16.0
GLA_CHUNK = 64

ATTN_HEADS = 4
ATTN_KV_HEADS = 2
WINDOW = 128
ATTN_BLOCK = 128

S5_GROUPS = 16
S5_GROUP_CH = MIX_W // S5_GROUPS
S5_STATE = 64

N_GROUPS = 4
EXPERTS_PER_GROUP = 8
N_EXPERTS = N_GROUPS * EXPERTS_PER_GROUP
EXPERT_HIDDEN = 512
TOP_K = 2
MOE_BLOCK = 128

RWKV_IN = 4 * MIX_W
GLA_IN = 2 * GLA_HEADS * GLA_DK + 2 * GLA_HEADS * GLA_DV + 2 * GLA_RANK
ATTN_IN = (ATTN_HEADS + 2 * ATTN_KV_HEADS) * HEAD_DIM
S5_IN = MIX_W
GATE_IN = N_BRANCH * D_MODEL
P_IN = RWKV_IN + GLA_IN + ATTN_IN + S5_IN + GATE_IN

kernel_name = "hybrid_rwkv_gla_swa_s5_hmoe_dit"


def rmsnorm(x, g):
    xf = x.astype(jnp.float32)
    y = xf * lax.rsqrt(jnp.mean(xf * xf, axis=-1, keepdims=True) + EPS)
    return (y * g.astype(jnp.float32)).astype(x.dtype)


def modulate(h, shift, scale):
    return h * (1.0 + scale) + shift


def to_heads(z, h):
    return z.reshape(z.shape[:-1] + (h, z.shape[-1] // h))


def centered_shift(z):
    zp = jnp.pad(z, ((0, 0), (1, 1), (0, 0)))
    return 0.5 * (zp[:, :-2] + zp[:, 2:])


def split_sections(u):
    o1 = RWKV_IN
    o2 = o1 + GLA_IN
    o3 = o2 + ATTN_IN
    o4 = o3 + S5_IN
    return jnp.split(u, [o1, o2, o3, o4], axis=-1)


def axial_rope_tables(rows):
    row = jnp.repeat(jnp.arange(rows, dtype=jnp.float32), GRID_W)
    col = jnp.tile(jnp.arange(GRID_W, dtype=jnp.float32), rows)
    n_freq = HEAD_DIM // 4
    inv_freq = ROPE_BASE ** (-jnp.arange(n_freq, dtype=jnp.float32) / n_freq)
    ang = jnp.concatenate([row[:, None] * inv_freq, col[:, None] * inv_freq], axis=-1)
    return jnp.cos(ang), jnp.sin(ang)


def apply_rope(z, cos, sin):
    half = HEAD_DIM // 2
    z1, z2 = z[..., :half], z[..., half:]
    cos, sin = cos[:, None, :], sin[:, None, :]
    return jnp.concatenate([z1 * cos - z2 * sin, z1 * sin + z2 * cos], axis=-1)


def rwkv7_scan(r, w, k, v, a, b, s0, reverse):
    def step(s, inp):
        r_t, w_t, k_t, v_t, a_t, b_t = inp
        sa = jnp.einsum('bhvk,bhk->bhv', s, a_t)
        s = s * w_t[:, :, None, :] + sa[..., :, None] * b_t[..., None, :] + v_t[..., :, None] * k_t[..., None, :]
        return s, jnp.einsum('bhvk,bhk->bhv', s, r_t)
    xs = tuple(jnp.moveaxis(z, 1, 0) for z in (r, w, k, v, a, b))
    s_last, ys = lax.scan(step, s0, xs, reverse=reverse)
    return jnp.moveaxis(ys, 0, 1), s_last


def rwkv7_mixer(u_lat, u_ctx, p, ctx_out):
    H = RWKV_HEADS

    def prep(u):
        u = u.astype(jnp.float32)
        mixed = u + (centered_shift(u) - u) * p['rwkv_mu'].reshape(-1)
        r, k, v, xa = jnp.split(mixed, 4, axis=-1)
        kk = to_heads(k * p['rwkv_kk'], H)
        kk = kk * lax.rsqrt(jnp.sum(kk * kk, axis=-1, keepdims=True) + EPS)
        return r, k, v, xa, kk

    def direction(k, xa, kk, d):
        log_w = -jnp.exp(-jax.nn.softplus(-(p['rwkv_w0'][d] + jnp.tanh(xa @ p['rwkv_w1'][d]) @ p['rwkv_w2'][d])) - 0.5)
        iclr = jax.nn.sigmoid(p['rwkv_a0'][d] + (xa @ p['rwkv_a1'][d]) @ p['rwkv_a2'][d])
        k_eff = k * (1.0 + (iclr - 1.0) * p['rwkv_ka'])
        return jnp.exp(to_heads(log_w, H)), to_heads(k_eff, H), -kk, kk * to_heads(iclr, H)

    rl, kl, vl, xal, kkl = prep(u_lat)
    rc, kc, vc, xac, kkc = prep(u_ctx)
    rlh, vlh, rch, vch = to_heads(rl, H), to_heads(vl, H), to_heads(rc, H), to_heads(vc, H)
    s0 = jnp.zeros((u_lat.shape[0], H, HEAD_DIM, HEAD_DIM), jnp.float32)
    y_lat, y_ctx = 0.0, 0.0
    for d in range(2):
        rev = d == 1
        wc, kec, ac, bc = direction(kc, xac, kkc, d)
        yc_d, s_ctx = rwkv7_scan(rch, wc, kec, vch, ac, bc, s0, rev)
        wl, kel, al, bl = direction(kl, xal, kkl, d)
        yl_d, _ = rwkv7_scan(rlh, wl, kel, vlh, al, bl, s_ctx, rev)
        y_lat = y_lat + yl_d
        y_ctx = y_ctx + yc_d

    def finish(y, r, k, v, xa):
        mu = jnp.mean(y, axis=-1, keepdims=True)
        var = jnp.mean(jnp.square(y - mu), axis=-1, keepdims=True)
        y = ((y - mu) * lax.rsqrt(var + GN_EPS)).reshape(r.shape) * p['rwkv_ln_g']
        rh, kh, vh = to_heads(r, H), to_heads(k, H), to_heads(v, H)
        bonus = (jnp.sum(rh * kh * p['rwkv_rk'], axis=-1, keepdims=True) * vh).reshape(r.shape)
        g = jax.nn.sigmoid(xa @ p['rwkv_g1']) @ p['rwkv_g2']
        return (y + bonus) * g

    out_lat = finish(y_lat, rl, kl, vl, xal)
    out_ctx = finish(y_ctx, rc, kc, vc, xac) if ctx_out else None
    return out_lat, out_ctx


def gla_chunked(q, k, v, log_a, s0):
    b, h, t, _ = q.shape
    dv = v.shape[-1]
    n = t // GLA_CHUNK

    def chunks(z):
        return z.reshape(b, h, n, GLA_CHUNK, z.shape[-1])
    q, k, v, log_a = chunks(q), chunks(k), chunks(v), chunks(log_a)
    cum = jnp.cumsum(log_a, axis=3)
    total = cum[:, :, :, -1:, :]
    q_dec = q * jnp.exp(cum)
    k_inv = k * jnp.exp(-cum)
    k_end = k * jnp.exp(total - cum)
    prefix = jnp.tril(jnp.ones((GLA_CHUNK, GLA_CHUNK), jnp.float32))
    scores = jnp.einsum('bhnik,bhnjk->bhnij', q_dec, k_inv) * prefix
    o_intra = jnp.einsum('bhnij,bhnjv->bhniv', scores, v)
    delta = jnp.einsum('bhnjk,bhnjv->bhnkv', k_end, v)
    chunk_decay = jnp.exp(total[:, :, :, 0, :])

    def step(s, inp):
        dcy, ds = inp
        return s * dcy[..., None] + ds, s
    s_last, s_enter = lax.scan(step, s0, (jnp.moveaxis(chunk_decay, 2, 0), jnp.moveaxis(delta, 2, 0)))
    o_inter = jnp.einsum('bhnik,bhnkv->bhniv', q_dec, jnp.moveaxis(s_enter, 0, 2))
    return (o_intra + o_inter).reshape(b, h, t, dv), s_last


def gla_mixer(u_lat, u_ctx, p, ctx_out):
    qk, vw = GLA_HEADS * GLA_DK, GLA_HEADS * GLA_DV

    def bhtd(z):
        return jnp.moveaxis(to_heads(z, GLA_HEADS), 2, 1)

    def prep(u):
        u = u.astype(jnp.float32)
        q, k, v, r, alow = jnp.split(u, [qk, 2 * qk, 2 * qk + vw, 2 * qk + 2 * vw], axis=-1)
        log_a = [bhtd(jax.nn.log_sigmoid(alow[..., d * GLA_RANK:(d + 1) * GLA_RANK] @ p['gla_a2'][d] + p['gla_ab'][d]) / GLA_TAU)
                 for d in range(2)]
        return bhtd(q) * GLA_DK ** -0.5, bhtd(k), bhtd(v), r, log_a

    ql, kl, vl, rl, al = prep(u_lat)
    qc, kc, vc, rc, ac = prep(u_ctx)
    s0 = jnp.zeros((u_lat.shape[0], GLA_HEADS, GLA_DK, GLA_DV), jnp.float32)
    o_lat, o_ctx = 0.0, 0.0
    for d in range(2):
        if d == 1:
            flip = lambda z: jnp.flip(z, axis=2)
        else:
            flip = lambda z: z
        oc_d, s_ctx = gla_chunked(flip(qc), flip(kc), flip(vc), flip(ac[d]), s0)
        ol_d, _ = gla_chunked(flip(ql), flip(kl), flip(vl), flip(al[d]), s_ctx)
        o_lat = o_lat + flip(ol_d)
        o_ctx = o_ctx + flip(oc_d)

    def finish(o, r):
        o = jnp.moveaxis(o, 1, 2)
        o = o * lax.rsqrt(jnp.mean(o * o, axis=-1, keepdims=True) + EPS)
        return o.reshape(r.shape) * p['gla_ln_g'] * jax.nn.silu(r)

    return finish(o_lat, rl), (finish(o_ctx, rc) if ctx_out else None)


def window_attention(u_lat, u_ctx, p, cos, sin, ctx_out):
    H, KVH, hd, BLK = ATTN_HEADS, ATTN_KV_HEADS, HEAD_DIM, ATTN_BLOCK
    G = H // KVH
    scale = hd ** -0.5

    def split_qkv(u):
        q, k, v = jnp.split(u.astype(jnp.float32), [H * hd, (H + KVH) * hd], axis=-1)
        return to_heads(q, H), to_heads(k, KVH), to_heads(v, KVH)

    q, k, v = split_qkv(u_lat)
    qc, kc, vc = split_qkv(u_ctx)
    q = apply_rope(q, cos, sin) * scale
    k = apply_rope(k, cos, sin)
    b, t = q.shape[0], q.shape[1]
    nb = t // BLK
    lc = kc.shape[1]
    sink = p['attn_sink'].astype(jnp.float32).reshape(KVH, G)

    qb = q.reshape(b, nb, BLK, KVH, G, hd)

    def band(z):
        zp = jnp.pad(z, ((0, 0), (BLK, BLK), (0, 0), (0, 0))).reshape(b, nb + 2, BLK, KVH, hd)
        return jnp.concatenate([zp[:, :-2], zp[:, 1:-1], zp[:, 2:]], axis=2)
    kw, vw = band(k), band(v)
    s_w = jnp.einsum('bnqhgd,bnkhd->bnhgqk', qb, kw)
    s_c = jnp.einsum('bnqhgd,bkhd->bnhgqk', qb, kc)
    qpos = jnp.arange(nb)[:, None, None] * BLK + jnp.arange(BLK)[None, :, None]
    kpos = jnp.arange(nb)[:, None, None] * BLK - BLK + jnp.arange(3 * BLK)[None, None, :]
    valid = (jnp.abs(qpos - kpos) <= WINDOW) & (kpos >= 0) & (kpos < t)
    s_w = jnp.where(valid[None, :, None, None], s_w, NEG_INF)
    s_sink = jnp.broadcast_to(sink[None, None, :, :, None, None], s_w.shape[:-1] + (1,))
    probs = jax.nn.softmax(jnp.concatenate([s_w, s_c, s_sink], axis=-1), axis=-1)
    o = (jnp.einsum('bnhgqk,bnkhd->bnqhgd', probs[..., :3 * BLK], vw)
         + jnp.einsum('bnhgqk,bkhd->bnqhgd', probs[..., 3 * BLK:3 * BLK + lc], vc))
    out_lat = o.reshape(b, t, H * hd)

    out_ctx = None
    if ctx_out:
        qcs = qc.reshape(b, lc, KVH, G, hd) * scale
        sc = jnp.einsum('bqhgd,bkhd->bhgqk', qcs, kc)
        sc_sink = jnp.broadcast_to(sink[None, :, :, None, None], sc.shape[:-1] + (1,))
        pc = jax.nn.softmax(jnp.concatenate([sc, sc_sink], axis=-1), axis=-1)
        out_ctx = jnp.einsum('bhgqk,bkhd->bqhgd', pc[..., :lc], vc).reshape(b, lc, H * hd)
    return out_lat, out_ctx


def lti_scan(a_bar, bu, x0):
    bu = bu.at[:, 0].add(a_bar * x0)
    a = jnp.broadcast_to(a_bar, bu.shape)

    def combine(e1, e2):
        a1, b1 = e1
        a2, b2 = e2
        return a1 * a2, a2 * b1 + b2
    return lax.associative_scan(combine, (a, bu), axis=1)[1]


def s5_mixer(u_lat, u_ctx, p, ctx_out):
    f32 = jnp.float32

    def groups(u):
        return u.astype(f32).reshape(u.shape[0], u.shape[1], S5_GROUPS, S5_GROUP_CH)
    ul, uc = groups(u_lat), groups(u_ctx)
    b_mat = lax.complex(p['s5_b_re'].astype(f32), p['s5_b_im'].astype(f32))
    d_skip = p['s5_d'].astype(f32)
    y_lat, y_ctx = d_skip * ul, d_skip * uc
    x0 = jnp.zeros((u_lat.shape[0], S5_GROUPS, S5_STATE), jnp.complex64)
    for d in range(2):
        if d == 1:
            flip = lambda z: jnp.flip(z, axis=1)
        else:
            flip = lambda z: z
        lam = lax.complex(p['s5_lam_re'][d].astype(f32), p['s5_lam_im'][d].astype(f32))
        dt = jnp.exp(p['s5_log_dt'][d].astype(f32))[:, None]
        a_bar = jnp.exp(lam * dt)
        b_bar = ((a_bar - 1.0) / lam)[..., None] * b_mat
        c_mat = lax.complex(p['s5_c_re'][d].astype(f32), p['s5_c_im'][d].astype(f32))
        bu_c = jnp.einsum('gpc,btgc->btgp', b_bar, flip(uc).astype(jnp.complex64))
        bu_l = jnp.einsum('gpc,btgc->btgp', b_bar, flip(ul).astype(jnp.complex64))
        xs_c = lti_scan(a_bar, bu_c, x0)
        xs_l = lti_scan(a_bar, bu_l, xs_c[:, -1])
        y_lat = y_lat + jnp.real(jnp.einsum('gcp,btgp->btgc', c_mat, flip(xs_l)))
        if ctx_out:
            y_ctx = y_ctx + jnp.real(jnp.einsum('gcp,btgp->btgc', c_mat, flip(xs_c)))

    def finish(y):
        y = jax.nn.gelu(y.reshape(y.shape[0], y.shape[1], MIX_W))
        return y * jax.nn.sigmoid(y @ p['s5_w_glu'] + p['s5_b_glu'])
    return finish(y_lat), (finish(y_ctx) if ctx_out else None)


def merge_branches(ys, gate_logits, p):
    gates = jnp.split(gate_logits.astype(jnp.float32), N_BRANCH, axis=-1)
    merged = 0.0
    for i in range(N_BRANCH):
        merged = merged + jax.nn.sigmoid(gates[i]) * (ys[i] @ p['w_branch'][i])
    return merged @ p['w_out']


def hier_moe(h, p):
    n, d = h.shape
    hf = h.astype(jnp.float32)
    rows = jnp.arange(n)
    g_logits = hf @ p['w_router_g'] + p['b_router_g']
    g_sel = jnp.argmax(g_logits, axis=-1).astype(jnp.int32)
    g_w = jax.nn.softmax(g_logits, axis=-1)[rows, g_sel]
    e_logits = (hf @ p['w_router_e'] + p['b_router_e']).reshape(n, N_GROUPS, EXPERTS_PER_GROUP)[rows, g_sel]
    top_v, top_i = lax.top_k(e_logits, TOP_K)
    weights = jax.nn.softmax(top_v, axis=-1) * g_w[:, None]
    expert = g_sel[:, None] * EXPERTS_PER_GROUP + top_i.astype(jnp.int32)

    flat_e = expert.reshape(-1)
    flat_w = weights.reshape(-1)
    flat_tok = jnp.arange(n * TOP_K, dtype=jnp.int32) // TOP_K
    order = jnp.argsort(flat_e)
    se, stok, sw = flat_e[order], flat_tok[order], flat_w[order]
    counts = jnp.zeros((N_EXPERTS,), jnp.int32).at[flat_e].add(1)
    padded = (counts + MOE_BLOCK - 1) // MOE_BLOCK * MOE_BLOCK
    pend = jnp.cumsum(padded)
    pstart = pend - padded
    start = jnp.cumsum(counts) - counts
    dest = pstart[se] + (jnp.arange(n * TOP_K, dtype=jnp.int32) - start[se])
    cap = -(-(n * TOP_K) // MOE_BLOCK) * MOE_BLOCK + N_EXPERTS * MOE_BLOCK
    n_blk = cap // MOE_BLOCK
    buf_tok = jnp.full((cap,), n, jnp.int32).at[dest].set(stok)
    buf_w = jnp.zeros((cap,), jnp.float32).at[dest].set(sw)
    blk_e = jnp.minimum(jnp.searchsorted(pend, jnp.arange(n_blk, dtype=jnp.int32) * MOE_BLOCK, side='right'),
                        N_EXPERTS - 1)
    h_pad = jnp.concatenate([h, jnp.zeros((1, d), h.dtype)], axis=0)
    xb = h_pad[buf_tok].reshape(n_blk, MOE_BLOCK, d)

    def expert_block(args):
        x_blk, e = args
        hid = jax.nn.silu(x_blk @ p['w_exp_gate'][e]) * (x_blk @ p['w_exp_up'][e])
        return hid @ p['w_exp_down'][e]
    y_buf = lax.map(expert_block, (xb, blk_e)).reshape(cap, d) * buf_w[:, None]
    return jax.ops.segment_sum(y_buf, buf_tok, num_segments=n + 1)[:n].astype(h.dtype)


def adaln(cvec, p):
    return jnp.split(jax.nn.silu(cvec) @ p['w_ada'] + p['b_ada'], 6, axis=-1)


def hybrid_layer(x, xc, c, c_ctx, p, cos, sin, ctx_out):
    mods = [m[:, None, :] for m in adaln(c, p)]
    cmods = adaln(c_ctx, p)
    h = modulate(rmsnorm(x, p['norm1_g']), mods[0], mods[1])
    hc = modulate(rmsnorm(xc, p['norm1_g']), cmods[0], cmods[1])
    ua, ub, uatt, us5, ug = split_sections(h @ p['w_in'])
    ca, cb, catt, cs5, cg = split_sections(hc @ p['w_in'])
    ya, yac = rwkv7_mixer(ua, ca, p, ctx_out)
    yb, ybc = gla_mixer(ub, cb, p, ctx_out)
    yc, ycc = window_attention(uatt, catt, p, cos, sin, ctx_out)
    yd, ydc = s5_mixer(us5, cs5, p, ctx_out)
    x = x + (mods[2] * merge_branches((ya, yb, yc, yd), ug, p)).astype(x.dtype)
    if ctx_out:
        xc = xc + (cmods[2] * merge_branches((yac, ybc, ycc, ydc), cg, p)).astype(xc.dtype)

    b, t, d = x.shape
    tokens = modulate(rmsnorm(x, p['norm2_g']), mods[3], mods[4]).reshape(-1, d)
    if ctx_out:
        h2c = modulate(rmsnorm(xc, p['norm2_g']), cmods[3], cmods[4])
        tokens = jnp.concatenate([tokens, h2c.reshape(-1, d)], axis=0)
    y = hier_moe(tokens, p)
    x = x + (mods[5] * y[:b * t].reshape(b, t, d)).astype(x.dtype)
    if ctx_out:
        xc = xc + (cmods[5] * y[b * t:].reshape(xc.shape)).astype(xc.dtype)
    return x, xc


def setup_inputs(seed: int = 0) -> dict:
    key = jax.random.key(seed)
    ks = iter(jax.random.split(key, 64))
    f32 = jnp.float32

    def nrm(shape, scale):
        return scale * jax.random.normal(next(ks), shape, f32)

    def uni(shape, lo, hi):
        return jax.random.uniform(next(ks), shape, f32, lo, hi)

    L, D = DEPTH, D_MODEL
    G, P, CH = S5_GROUPS, S5_STATE, S5_GROUP_CH
    return {
        'x': nrm((BATCH, SEQ, D), 1.0),
        'c': nrm((BATCH, D), 1.0),
        'ctx': nrm((BATCH, CTX_LEN, D), 1.0),
        'c_ctx': nrm((D,), 1.0),
        'norm1_g': 1.0 + nrm((L, D), 0.02),
        'norm2_g': 1.0 + nrm((L, D), 0.02),
        'final_norm_g': 1.0 + nrm((D,), 0.02),
        'w_ada': nrm((L, D, 6 * D), 0.5 * D ** -0.5),
        'b_ada': nrm((L, 6 * D), 0.02),
        'w_in': nrm((L, D, P_IN), D ** -0.5),
        'rwkv_mu': uni((L, 4, MIX_W), 0.0, 1.0),
        'rwkv_w0': uni((L, 2, MIX_W), -5.0, 0.5),
        'rwkv_w1': nrm((L, 2, MIX_W, RWKV_DECAY_RANK), MIX_W ** -0.5),
        'rwkv_w2': nrm((L, 2, RWKV_DECAY_RANK, MIX_W), 0.5 * RWKV_DECAY_RANK ** -0.5),
        'rwkv_a0': nrm((L, 2, MIX_W), 0.1),
        'rwkv_a1': nrm((L, 2, MIX_W, RWKV_ICLR_RANK), MIX_W ** -0.5),
        'rwkv_a2': nrm((L, 2, RWKV_ICLR_RANK, MIX_W), 0.5 * RWKV_ICLR_RANK ** -0.5),
        'rwkv_kk': 0.85 + nrm((L, MIX_W), 0.02),
        'rwkv_ka': 1.0 + nrm((L, MIX_W), 0.02),
        'rwkv_rk': nrm((L, RWKV_HEADS, HEAD_DIM), 0.1),
        'rwkv_g1': nrm((L, MIX_W, RWKV_GATE_RANK), MIX_W ** -0.5),
        'rwkv_g2': nrm((L, RWKV_GATE_RANK, MIX_W), RWKV_GATE_RANK ** -0.5),
        'rwkv_ln_g': 1.0 + nrm((L, MIX_W), 0.02),
        'gla_a2': nrm((L, 2, GLA_RANK, GLA_HEADS * GLA_DK), GLA_RANK ** -0.5),
        'gla_ab': nrm((L, 2, GLA_HEADS * GLA_DK), 0.1),
        'gla_ln_g': 1.0 + nrm((L, MIX_W), 0.02),
        'attn_sink': nrm((L, ATTN_HEADS), 0.5),
        's5_lam_re': -0.5 + nrm((L, 2, G, P), 0.01),
        's5_lam_im': jnp.pi * jnp.arange(P, dtype=f32) + nrm((L, 2, G, P), 0.01),
        's5_log_dt': uni((L, 2, G), math.log(1e-3), math.log(1e-1)),
        's5_b_re': nrm((L, G, P, CH), (2 * CH) ** -0.5),
        's5_b_im': nrm((L, G, P, CH), (2 * CH) ** -0.5),
        's5_c_re': nrm((L, 2, G, CH, P), P ** -0.5),
        's5_c_im': nrm((L, 2, G, CH, P), P ** -0.5),
        's5_d': nrm((L, G, CH), 1.0),
        's5_w_glu': nrm((L, MIX_W, MIX_W), MIX_W ** -0.5),
        's5_b_glu': nrm((L, MIX_W), 0.02),
        'w_branch': nrm((L, N_BRANCH, MIX_W, D), MIX_W ** -0.5),
        'w_out': nrm((L, D, D), D ** -0.5),
        'w_router_g': nrm((L, D, N_GROUPS), D ** -0.5),
        'b_router_g': nrm((L, N_GROUPS), 0.01),
        'w_router_e': nrm((L, D, N_EXPERTS), D ** -0.5),
        'b_router_e': nrm((L, N_EXPERTS), 0.01),
        'w_exp_gate': nrm((L, N_EXPERTS, D, EXPERT_HIDDEN), D ** -0.5),
        'w_exp_up': nrm((L, N_EXPERTS, D, EXPERT_HIDDEN), D ** -0.5),
        'w_exp_down': nrm((L, N_EXPERTS, EXPERT_HIDDEN, D), EXPERT_HIDDEN ** -0.5),
    }


def reference(x, c, ctx, c_ctx, norm1_g, norm2_g, final_norm_g, w_ada, b_ada, w_in,
              rwkv_mu, rwkv_w0, rwkv_w1, rwkv_w2, rwkv_a0, rwkv_a1, rwkv_a2, rwkv_kk, rwkv_ka,
              rwkv_rk, rwkv_g1, rwkv_g2, rwkv_ln_g, gla_a2, gla_ab, gla_ln_g, attn_sink,
              s5_lam_re, s5_lam_im, s5_log_dt, s5_b_re, s5_b_im, s5_c_re, s5_c_im, s5_d,
              s5_w_glu, s5_b_glu, w_branch, w_out, w_router_g, b_router_g, w_router_e, b_router_e,
              w_exp_gate, w_exp_up, w_exp_down):
    rows = x.shape[1] // GRID_W
    cos, sin = axial_rope_tables(rows)
    xc = ctx
    for l in range(DEPTH):
        p = {
            'norm1_g': norm1_g[l], 'norm2_g': norm2_g[l], 'w_ada': w_ada[l], 'b_ada': b_ada[l],
            'w_in': w_in[l],
            'rwkv_mu': rwkv_mu[l], 'rwkv_w0': rwkv_w0[l], 'rwkv_w1': rwkv_w1[l], 'rwkv_w2': rwkv_w2[l],
            'rwkv_a0': rwkv_a0[l], 'rwkv_a1': rwkv_a1[l], 'rwkv_a2': rwkv_a2[l], 'rwkv_kk': rwkv_kk[l],
            'rwkv_ka': rwkv_ka[l], 'rwkv_rk': rwkv_rk[l], 'rwkv_g1': rwkv_g1[l], 'rwkv_g2': rwkv_g2[l],
            'rwkv_ln_g': rwkv_ln_g[l],
            'gla_a2': gla_a2[l], 'gla_ab': gla_ab[l], 'gla_ln_g': gla_ln_g[l],
            'attn_sink': attn_sink[l],
            's5_lam_re': s5_lam_re[l], 's5_lam_im': s5_lam_im[l], 's5_log_dt': s5_log_dt[l],
            's5_b_re': s5_b_re[l], 's5_b_im': s5_b_im[l], 's5_c_re': s5_c_re[l], 's5_c_im': s5_c_im[l],
            's5_d': s5_d[l], 's5_w_glu': s5_w_glu[l], 's5_b_glu': s5_b_glu[l],
            'w_branch': w_branch[l], 'w_out': w_out[l],
            'w_router_g': w_router_g[l], 'b_router_g': b_router_g[l],
            'w_router_e': w_router_e[l], 'b_router_e': b_router_e[l],
            'w_exp_gate': w_exp_gate[l], 'w_exp_up': w_exp_up[l], 'w_exp_down': w_exp_down[l],
        }
        x, xc = hybrid_layer(x, xc, c, c_ctx, p, cos, sin, l < DEPTH - 1)
    return rmsnorm(x, final_norm_g)
```

```python
import math
from contextlib import ExitStack

import numpy as np
import concourse.bass as bass
import concourse.mybir as mybir
from concourse.bass_utils import run_bass_kernel_spmd

F32 = mybir.dt.float32
BF16 = mybir.dt.bfloat16
ALU = mybir.AluOpType
AF = mybir.ActivationFunctionType
AX = mybir.AxisListType

D = 1024
SEQ = 4096
CTX = 256
NT = (SEQ + CTX) // 128
NTOK = SEQ + CTX
DEPTH = 2
EPS = 1e-6
GN_EPS = 64e-5
O1, O2, O3, O4, PIN = 1024, 1824, 2336, 2592, 6688
PV_MU, PV_KK, PV_KA, PV_RK, PV_W0, PV_A0, PV_LNG, PV_GAB, PV_GLNG = 0, 1024, 1280, 1536, 1792, 2304, 2816, 3072, 3328
NPV = 3584

DEBUG = False
PENDING = "PENDING"
WKEYS = ("out", "accum_out", "ap")
SKEYS = ("scalar1", "scalar2", "scale", "bias", "scalar")


class Buf:
    __slots__ = ("name", "w", "rd", "ws")

    def __init__(self, name=""):
        self.name = name
        self.w = None
        self.rd = {}
        self.ws = False


class V:
    __slots__ = ("ap", "bufs")

    def __init__(self, ap, bufs):
        self.ap = ap
        self.bufs = bufs if isinstance(bufs, tuple) else (bufs,)

    def __getitem__(self, k):
        return V(self.ap[k], self.bufs)

    def rr(self, pat, **kw):
        return V(self.ap.rearrange(pat, **kw), self.bufs)

    def bc(self, shape):
        return V(self.ap.to_broadcast(list(shape)), self.bufs)

    def bitcast(self, dt):
        return V(self.ap.bitcast(dt), self.bufs)

    def wb(self, *bufs):
        return V(self.ap, tuple(bufs))

    @property
    def shape(self):
        return tuple(self.ap.shape)


class Prog:
    ENG = ("pe", "act", "dve", "pool", "sp")
    K = 6
    STRICT_ALL = False

    def __init__(self, nc):
        self.nc = nc
        self.eng = {"pe": nc.tensor, "act": nc.scalar, "dve": nc.vector, "pool": nc.gpsimd, "sp": nc.sync}
        self.sem = {e: nc.alloc_semaphore("s_" + e) for e in self.ENG}
        self.cnt = {e: 0 for e in self.ENG}
        self.dsem = {q: [nc.alloc_semaphore("d_%s%d" % (q, i)) for i in range(self.K)] for q in ("sp", "act", "pool")}
        self.dcnt = {q: 0 for q in self.dsem}
        self.known = {e: {} for e in self.ENG}
        self.pend_r = []
        self.pend_w = []
        self.uid = 0
        self.nins = 0

    def _need(self, e, tok, is_dma, strict=False):
        if tok is None:
            return
        if tok is PENDING:
            assert e == "pe" and not is_dma, "dependency on an unmarked PE op"
            return
        sem, val, owner = tok
        if owner == e and not is_dma and not (strict and e != "pe") and not self.STRICT_ALL:
            return
        k = self.known[e]
        if k.get(sem.num, 0) >= val:
            return
        self.eng[e].wait_ge(sem, val)
        self.nins += 1
        k[sem.num] = val

    def op(self, e, meth, mark=True, lax=False, **kw):
        reads, writes, args, sreads, awrites = [], [], {}, [], []
        for k, v in kw.items():
            if isinstance(v, V):
                (writes if k in WKEYS else reads).extend(v.bufs)
                if k in SKEYS:
                    sreads.extend(v.bufs)
                if k == "accum_out":
                    awrites.extend(v.bufs)
                args[k] = v.ap
            else:
                args[k] = v
        is_dma = meth == "dma_start"
        for b in reads:
            self._need(e, b.w, is_dma, strict=(not lax) or b.ws or e == "act" or b in sreads)
        for b in writes:
            self._need(e, b.w, is_dma)
            for t in b.rd.values():
                self._need(e, t, is_dma)
        if is_dma:
            n = self.dcnt[e]
            sem = self.dsem[e][n % self.K]
            r = n // self.K
            if r > 0:
                self._need(e, (sem, 16 * r, None), True)
            ins = getattr(self.eng[e], meth)(**args)
            ins.then_inc(sem, 16)
            self.dcnt[e] = n + 1
            tok = (sem, 16 * (r + 1), None)
            key = ("d", sem.num)
        else:
            ins = getattr(self.eng[e], meth)(**args)
            key = e
            if mark:
                self.cnt[e] += 1
                ins.then_inc(self.sem[e], 1)
                tok = (self.sem[e], self.cnt[e], e)
                if e == "pe" and (self.pend_r or self.pend_w):
                    for b in self.pend_r:
                        if b.rd.get("pe") is PENDING:
                            b.rd["pe"] = tok
                    for b in self.pend_w:
                        if b.w is PENDING:
                            b.w = tok
                    self.pend_r = []
                    self.pend_w = []
            else:
                assert e == "pe"
                tok = PENDING
                self.pend_r.extend(reads)
                self.pend_w.extend(writes)
        self.nins += 1
        for b in reads:
            b.rd[key] = tok
        for b in writes:
            b.w = tok
            b.rd = {}
            b.ws = (b in awrites) or e == "act"
        return ins

    def barrier(self):
        assert not self.pend_r and not self.pend_w
        toks = [(self.sem[e], self.cnt[e], e) for e in self.ENG if self.cnt[e] > 0]
        for q in self.dsem:
            n = self.dcnt[q]
            for i in range(self.K):
                c = (n - i + self.K - 1) // self.K if n > i else 0
                if c > 0:
                    toks.append((self.dsem[q][i], 16 * c, None))
        for e in self.ENG:
            for t in toks:
                self._need(e, t, False)

    def name(self, s):
        self.uid += 1
        return "%s_%d" % (s, self.uid)

    def dram(self, name, shape, dt, kind="Internal"):
        return self.nc.dram_tensor(name, list(shape), dt, kind=kind).ap()


class Arena:
    def __init__(self, P):
        self.P = P
        self.stack = ExitStack()

    def sb(self, name, shape, dt=F32):
        h = self.stack.enter_context(self.P.nc.sbuf_tensor(self.P.name(name), list(shape), dt))
        return V(h.ap(), Buf(name))

    def close(self):
        self.P.barrier()
        self.stack.close()


def dma(P, q, out, in_):
    return P.op(q, "dma_start", out=out, in_=in_)


def mm(P, out, lhsT, rhs, start, stop, mark=None):
    return P.op("pe", "matmul", mark=(stop if mark is None else mark), out=out, lhsT=lhsT, rhs=rhs,
                start=start, stop=stop)


def tr(P, out, in_, ident, mark=True):
    return P.op("pe", "transpose", mark=mark, out=out, in_=in_, identity=ident)


def tt(P, e, out, in0, in1, op):
    return P.op(e, "tensor_tensor", out=out, in0=in0, in1=in1, op=op)


def ts(P, e, out, in0, s1, op0, s2=None, op1=None, **kw):
    if op1 is None:
        return P.op(e, "tensor_scalar", out=out, in0=in0, scalar1=s1, scalar2=None, op0=op0, **kw)
    return P.op(e, "tensor_scalar", out=out, in0=in0, scalar1=s1, scalar2=s2, op0=op0, op1=op1, **kw)


def act(P, out, in_, func, **kw):
    return P.op("act", "activation", out=out, in_=in_, func=func, **kw)


def cp(P, e, out, in_):
    if e == "act":
        return act(P, out, in_, AF.Copy)
    return P.op(e, "tensor_copy", out=out, in_=in_)


class Ctx:
    pass


def build_program(dbg=None):
    dbg = dbg or {}
    nc = bass.Bass("TRN2", target_bir_lowering=False)
    P = Prog(nc)
    C = Ctx()
    C.P, C.nc, C.dbg = P, nc, dbg
    skind = "ExternalOutput" if dbg.get("expose") else "Internal"

    def din(name, shape, dt=F32):
        return V(nc.dram_tensor(name, list(shape), dt, kind="ExternalInput").ap(), Buf(name))

    I = {}
    I["xb"] = din("xb", [SEQ, D])
    I["ctxb"] = din("ctxb", [CTX, D])
    I["cc"] = din("cc", [128, 8, 2])
    I["w_ada"] = din("w_ada", [DEPTH, D, 6 * D])
    I["b_ada"] = din("b_ada", [DEPTH, 6 * D])
    I["b_ada_fm"] = din("b_ada_fm", [DEPTH, 128, 48])
    I["g1_fm"] = din("g1_fm", [DEPTH, 128, 8])
    I["g2_fm"] = din("g2_fm", [DEPTH, 128, 8])
    I["w_in"] = din("w_in", [DEPTH, D, PIN])
    I["ident"] = din("ident", [128, 128])
    I["sel"] = din("sel", [128, 64, 128])
    I["maskw"] = din("maskw", [128, 384])
    I["ropec"] = din("ropec", [SEQ, 32])
    I["ropes"] = din("ropes", [SEQ, 32])
    I["attn_sink"] = din("attn_sink", [DEPTH, 16])
    I["s5_sm"] = din("s5_sm", [DEPTH, 128, 2, 3, 8])
    I["s5_rows"] = din("s5_rows", [DEPTH, 2, 3, 1024])
    I["s5_bt"] = din("s5_bt", [DEPTH, 2, 8, 128, 128])
    I["s5_ct"] = din("s5_ct", [DEPTH, 2, 2, 8, 128, 128])
    I["pw2"] = din("pw2", [1, 16])
    I["cmasks"] = din("cmasks", [128, 7, 128])
    I["w_branch"] = din("w_branch", [DEPTH, 4, 256, D])
    I["w_out"] = din("w_out", [DEPTH, D, D])
    I["w_router"] = din("w_router", [DEPTH, D, 36])
    I["b_router"] = din("b_router", [DEPTH, 36])
    I["w_exp_gate"] = din("w_exp_gate", [DEPTH, 32, D, 512])
    I["w_exp_up"] = din("w_exp_up", [DEPTH, 32, D, 512])
    I["w_exp_down"] = din("w_exp_down", [DEPTH, 32, 512, D])
    I["s5_d_fm"] = din("s5_d_fm", [DEPTH, 128, 16])
    I["s5_bglu_fm"] = din("s5_bglu_fm", [DEPTH, 128, 16])
    I["s5_w_glu"] = din("s5_w_glu", [DEPTH, 256, 256])
    I["pv"] = din("pv", [DEPTH, NPV])
    I["w1cat"] = din("w1cat", [DEPTH, 256, 128])
    I["w2blk"] = din("w2blk", [DEPTH, 128, 1024])
    I["g1"] = din("g1", [DEPTH, 256, 64])
    I["g2"] = din("g2", [DEPTH, 64, 256])
    I["a2blk"] = din("a2blk", [DEPTH, 32, 256])
    I["final_g"] = din("final_g", [1, D])
    C.I = I

    out = V(nc.dram_tensor("out", [SEQ, D], F32, kind="ExternalOutput").ap(), Buf("out"))
    C.out = out

    def dout(name, shape, dt=F32):
        return V(nc.dram_tensor(name, list(shape), dt, kind="ExternalOutput").ap(), Buf(name))
    C.dout = dout

    xres_ap = P.dram("xres", [NTOK, D], F32, kind=skind)
    C.xres = [V(xres_ap[t * 128:(t + 1) * 128, :], Buf("xres%d" % t)) for t in range(NT)]
    C.U_ap = P.dram("U", [NTOK + 3, O4], F32, kind=skind)
    C.U_bufs = [Buf("U%d" % t) for t in range(NT)]
    C.U_pad = Buf("Upad")
    C.STR_ap = [P.dram("STR%d" % d, [NTOK, 2, 1024], BF16, kind=skind) for d in range(2)]
    C.STR_bufs = [[Buf("STR%d_%d" % (d, t)) for t in range(NT)] for d in range(2)]
    C.Vs_ap = P.dram("Vs", [128, NTOK, 6], F32, kind=skind)
    C.Vs_bufs = [Buf("Vs%d" % t) for t in range(NT)]
    C.Y_ap = [P.dram("Y%d" % d, [128, NTOK, 6], F32, kind=skind) for d in range(2)]
    C.Y_bufs = [[Buf("Y%d_%d" % (d, c)) for c in range(NTOK // 64)] for d in range(2)]
    C.FIN_ap = P.dram("FIN", [NTOK, 768], F32, kind=skind)
    C.FIN_bufs = [Buf("FIN%d" % t) for t in range(NT)]
    C.YB_ap = P.dram("YB", [4, 256, NTOK], BF16, kind=skind)
    C.YB_bufs = [[Buf("YB%d_%d" % (i, t)) for t in range(NT)] for i in range(4)]
    C.CH_ap = P.dram("CH", [NTOK, 3072], F32, kind=skind)
    C.CH_bufs = [Buf("CH%d" % t) for t in range(NT)]
    C.YT_ap = [P.dram("YT%d" % d, [NTOK, 256], F32, kind=skind) for d in range(2)]
    C.YT_bufs = [[Buf("YT%d_%d" % (d, t)) for t in range(NT)] for d in range(2)]
    C.YTG_ap = [P.dram("YTG%d" % d, [NTOK, 256], F32, kind=skind) for d in range(2)]
    C.YTG_bufs = [[Buf("YTG%d_%d" % (d, t)) for t in range(NT)] for d in range(2)]
    C.Gt_ap = P.dram("Gt", [4096, NTOK], BF16, kind=skind)
    C.Gt_bufs = [Buf("Gt%d" % b) for b in range(9)]
    C.H2_ap = P.dram("H2", [128, 8, NTOK], BF16, kind=skind)
    C.H2_buf = Buf("H2")

    G = Arena(P)
    C.G = G
    C.psall = nc.alloc_psum_tensor("psall", [128, 4096], F32).ap()
    C.psb = [Buf("ps%d" % i) for i in range(8)]
    C.ps = [V(C.psall[:, i * 512:(i + 1) * 512], C.psb[i]) for i in range(8)]
    C.ident = G.sb("ident", [128, 128])
    dma(P, "sp", C.ident, I["ident"])

    for l in range(DEPTH):
        layer(C, l)
        if dbg.get("stop_layer") == l:
            break
    if not dbg.get("stop"):
        final_norm(C)
    P.barrier()
    return nc


def urow(t):
    return 1 + t * 128 if t < 2 else 258 + (t - 2) * 128


def xsrc(C, l, t):
    if l == 0 and not C.__dict__.get("x_in_scratch"):
        if t < 2:
            return C.I["ctxb"][t * 128:(t + 1) * 128, :]
        return C.I["xb"][(t - 2) * 128:(t - 1) * 128, :]
    return C.xres[t]


def phase_ada(C, l, L):
    P, I = C.P, C.I
    A = Arena(P)
    cc = A.sb("cc", [128, 8, 2])
    sc = A.sb("sc", [128, 8, 2])
    screp = A.sb("screp", [128, 8, 2, 128])
    bfm = A.sb("bfm", [128, 48])
    g1 = A.sb("g1", [128, 8])
    g2 = A.sb("g2", [128, 8])
    wst = [A.sb("wst%d" % i, [128, 8, 512]) for i in range(2)]
    brow = [A.sb("brow%d" % i, [128, 1024]) for i in range(2)]
    dma(P, "sp", cc, I["cc"])
    dma(P, "sp", bfm, I["b_ada_fm"][l])
    dma(P, "sp", g1, I["g1_fm"][l])
    dma(P, "sp", g2, I["g2_fm"][l])
    for ii, i in enumerate((2, 5)):
        dma(P, "pool", brow[ii], I["b_ada"][l:l + 1, i * 1024:(i + 1) * 1024].bc([128, 1024]))
    act(P, sc, cc, AF.Silu)
    for k in range(8):
        for j in range(2):
            cp(P, "dve", screp[:, k, j, :], sc[:, k, j:j + 1].bc([128, 128]))
    wv = I["w_ada"][l].rr("(k p) n -> p k n", p=128)
    psA = C.ps[0]
    for c in range(12):
        w = wst[c % 2]
        dma(P, "sp" if c % 2 == 0 else "pool", w, wv[:, :, c * 512:(c + 1) * 512])
        for mi in range(4):
            m = c * 4 + mi
            for k in range(8):
                mm(P, psA[:, m * 2:(m + 1) * 2], w[:, k, mi * 128:(mi + 1) * 128], sc[:, k, :], k == 0, k == 7)
        if c in (4, 5, 10, 11):
            ii = 0 if c < 6 else 1
            half = c % 2
            for j in range(2):
                pr = C.ps[1 + j]
                for k in range(8):
                    mm(P, pr, screp[:, k, j, :], w[:, k, :], k == 0, k == 7)
                tt(P, "dve", L.grow[ii][j][:, half * 512:(half + 1) * 512], pr,
                   brow[ii][:, half * 512:(half + 1) * 512], ALU.add)
    tt(P, "dve", L.mod, psA[:, 0:96].rr("p (m j) -> p m j", j=2), bfm[:, :, None].bc([128, 48, 2]), ALU.add)
    ts(P, "dve", L.sc1, L.mod[:, 8:16, :], 1.0, ALU.add)
    tt(P, "dve", L.sc1, L.sc1, g1[:, :, None].bc([128, 8, 2]), ALU.mult)
    ts(P, "dve", L.sc2, L.mod[:, 32:40, :], 1.0, ALU.add)
    tt(P, "dve", L.sc2, L.sc2, g2[:, :, None].bc([128, 8, 2]), ALU.mult)
    A.close()


def phase_norm(C, l, L, which, hfm, per_tile=None):
    P = C.P
    A = Arena(P)
    sc = L.sc1 if which == 1 else L.sc2
    shb = 0 if which == 1 else 24
    xt = [A.sb("xt%d" % i, [128, D]) for i in range(2)]
    junk = A.sb("junk", [128, D])
    st = [A.sb("st%d" % i, [128, 2]) for i in range(2)]
    hf = [A.sb("hf%d" % i, [128, 8, 128]) for i in range(2)] if per_tile else None
    for t in range(NT):
        j = 1 if t < 2 else 0
        x = xt[t % 2]
        s = st[t % 2]
        dma(P, "sp" if t % 2 == 0 else "pool", x, xsrc(C, l, t))
        act(P, junk, x, AF.Square, accum_out=s[:, 0:1])
        ts(P, "dve", s[:, 1:2], s[:, 0:1], 1.0 / D, ALU.mult, EPS, ALU.add)
        act(P, s[:, 1:2], s[:, 1:2], AF.Sqrt)
        P.op("dve", "reciprocal", out=s[:, 1:2], in_=s[:, 1:2])
        ts(P, "dve", x, x, s[:, 1:2], ALU.mult)
        pa, pb = C.ps[2 + 2 * (t % 2)], C.ps[3 + 2 * (t % 2)]
        if C.dbg.get("dump_norm") and t == 2 and which == 1:
            dma(P, "sp", C.dout("d_xn", [128, D]), x)
            dma(P, "sp", C.dout("d_st", [128, 2]), s)
        for k in range(8):
            pp = pa if k < 4 else pb
            tr(P, pp[:, (k % 4) * 128:(k % 4 + 1) * 128], x[:, k * 128:(k + 1) * 128], C.ident)
        if C.dbg.get("dump_norm") and t == 2 and which == 1:
            cp(P, "dve", junk[:, 0:512], pa)
            dma(P, "sp", C.dout("d_pa", [128, 512]), junk[:, 0:512])
        for k in range(8):
            pp = pa if k < 4 else pb
            src = pp[:, (k % 4) * 128:(k % 4 + 1) * 128]
            if per_tile:
                dst = hf[t % 2][:, k, :]
            else:
                dst = hfm[:, k, t * 128:(t + 1) * 128]
            if k % 2 == 0:
                act(P, dst, src, AF.Identity, scale=sc[:, k, j:j + 1], bias=L.mod[:, shb + k, j:j + 1])
            else:
                ts(P, "dve", dst, src, sc[:, k, j:j + 1], ALU.mult, L.mod[:, shb + k, j:j + 1], ALU.add)
        if per_tile:
            cp(P, "pool", hfm[:, :, t * 128:(t + 1) * 128], hf[t % 2])
            per_tile(t, hf[t % 2])
    A.close()


def phase_win_tm(C, l, L, hfm):
    P, I = C.P, C.I
    A = Arena(P)
    wA = A.sb("wA", [128, 8, O4], BF16)
    wst = [A.sb("wst%d" % i, [128, 8, 512]) for i in range(2)]
    ust = [A.sb("ust%d" % i, [128, O4]) for i in range(2)]
    zer = A.sb("zer", [1, O4])
    P.op("dve", "memset", ap=zer, constant=0.0)
    for r in (0, 257, NTOK + 2):
        dma(P, "sp", V(C.U_ap[r:r + 1, :], C.U_pad), zer)
    wv = I["w_in"][l].rr("(k p) n -> p k n", p=128)
    blocks = [(0, 512), (512, 1024), (1024, 1536), (1536, 1824), (1824, 2336), (2336, 2592)]
    for bi, (c0, c1) in enumerate(blocks):
        w = wst[bi % 2]
        dma(P, "sp" if bi % 2 == 0 else "pool", w[:, :, 0:c1 - c0], wv[:, :, c0:c1])
        cp(P, "pool", wA[:, :, c0:c1], w[:, :, 0:c1 - c0])
    n = 0
    for t in range(NT):
        u = ust[t % 2]
        for bi, (c0, c1) in enumerate(blocks):
            ps = C.ps[n % 4]
            n += 1
            for k in range(8):
                mm(P, ps[:, 0:c1 - c0], hfm[:, k, t * 128:(t + 1) * 128], wA[:, k, c0:c1], k == 0, k == 7)
            cp(P, "act" if bi % 2 == 0 else "dve", u[:, c0:c1], ps[:, 0:c1 - c0])
        r0 = urow(t)
        dma(P, "sp" if t % 2 == 0 else "pool", V(C.U_ap[r0:r0 + 128, :], C.U_bufs[t]), u)
    A.close()


def stt(P, e, out, in0, scalar, in1, op0, op1, **kw):
    return P.op(e, "scalar_tensor_tensor", out=out, in0=in0, scalar=scalar, in1=in1, op0=op0, op1=op1, **kw)


def red(P, e, out, in_, **kw):
    return P.op(e, "tensor_reduce", out=out, in_=in_, axis=AX.X, op=ALU.add, **kw)


def load_bf16(P, A, name, shape, src, q="sp", ce="pool"):
    st = A.sb(name + "_f", shape)
    wb = A.sb(name, shape, BF16)
    dma(P, q, st, src)
    cp(P, ce, wb, st)
    return wb


def cust(v, offset_elems, dims):
    ap = v.ap
    base = ap.ap[0]
    new = type(ap)(ap.tensor, ap.offset + offset_elems, [tuple(base)] + [tuple(d) for d in dims])
    return V(new, v.bufs)


def phase_prep(C, l, L):
    P, I = C.P, C.I
    A = Arena(P)
    pv = A.sb("pv", [128, NPV])
    dma(P, "sp", pv, I["pv"][l:l + 1, :].bc([128, NPV]))
    w1cat = load_bf16(P, A, "w1cat", [128, 2, 128], I["w1cat"][l].rr("(k p) n -> p k n", p=128))
    w2blk = load_bf16(P, A, "w2blk", [128, 1024], I["w2blk"][l])
    g1 = load_bf16(P, A, "g1w", [128, 2, 64], I["g1"][l].rr("(k p) n -> p k n", p=128))
    g2 = load_bf16(P, A, "g2w", [64, 256], I["g2"][l])
    a2blk = load_bf16(P, A, "a2blk", [32, 256], I["a2blk"][l])
    mu = pv[:, PV_MU:PV_MU + 1024]
    kkp = pv[:, PV_KK:PV_KK + 256]
    ka = pv[:, PV_KA:PV_KA + 256]
    rkp = pv[:, PV_RK:PV_RK + 256]
    w0 = pv[:, PV_W0:PV_W0 + 512]
    a0 = pv[:, PV_A0:PV_A0 + 512]
    gab = pv[:, PV_GAB:PV_GAB + 256]
    glng = pv[:, PV_GLNG:PV_GLNG + 256]

    uc = [A.sb("uc%d" % i, [128, O2]) for i in range(2)]
    up = [A.sb("up%d" % i, [128, 1024]) for i in range(2)]
    un = [A.sb("un%d" % i, [128, 1024]) for i in range(2)]
    rows = [[A.sb("rows%d%d" % (d, i), [128, 2, 1024], BF16) for i in range(2)] for d in range(2)]
    vt = [A.sb("vt%d" % i, [128, 128, 6]) for i in range(2)]
    fin = [A.sb("fin%d" % i, [128, 768]) for i in range(2)]
    cht = [A.sb("cht%d" % i, [128, 3072]) for i in range(2)]
    t0 = A.sb("t0", [128, 1024])
    mx = A.sb("mx", [128, 1024])
    xaT = A.sb("xaT", [128, 2, 128], BF16)
    z = A.sb("z", [128, 128], BF16)
    sg = A.sb("sg", [64, 128], BF16)
    wl = A.sb("wl", [128, 512])
    wdec = A.sb("wdec", [128, 512])
    il = A.sb("il", [128, 512])
    iclr = A.sb("iclr", [128, 512])
    kk0 = A.sb("kk0", [128, 256])
    sq = A.sb("sq", [128, 256])
    ss = A.sb("ss", [128, 8])
    kk = A.sb("kk", [128, 256])
    t1 = A.sb("t1", [128, 512])
    keff = A.sb("keff", [128, 512])
    bb = A.sb("bb", [128, 512])
    rkt = A.sb("rkt", [128, 256])
    alT = A.sb("alT", [32, 128], BF16)
    gl = A.sb("gl", [128, 256])
    gdec = A.sb("gdec", [128, 256])
    sr = A.sb("sr", [128, 256])

    def rv(R, c0, n):
        return R[:, :, c0:c0 + n]

    for t in range(NT):
        i = t % 2
        r0 = urow(t)
        nb = [C.U_bufs[t]]
        if t > 0:
            nb.append(C.U_bufs[t - 1])
        if t < NT - 1:
            nb.append(C.U_bufs[t + 1])
        nb.append(C.U_pad)
        dma(P, "sp", uc[i], V(C.U_ap[r0:r0 + 128, 0:O2], C.U_bufs[t]))
        dma(P, "pool", up[i], V(C.U_ap[r0 - 1:r0 + 127, 0:1024], tuple(nb)))
        dma(P, "sp", un[i], V(C.U_ap[r0 + 1:r0 + 129, 0:1024], tuple(nb)))
        u = uc[i]
        R0, R1 = rows[0][i], rows[1][i]
        F = fin[i]
        tt(P, "pool", t0, up[i], un[i], ALU.add)
        stt(P, "dve", t0, t0, 0.5, u[:, 0:1024], ALU.mult, ALU.subtract)
        tt(P, "pool", t0, t0, mu, ALU.mult)
        tt(P, "dve", mx, t0, u[:, 0:1024], ALU.add)
        r_, k_, v_, xa_ = mx[:, 0:256], mx[:, 256:512], mx[:, 512:768], mx[:, 768:1024]
        pT = C.ps[0]
        for kt in range(2):
            tr(P, pT[:, kt * 128:(kt + 1) * 128], xa_[:, kt * 128:(kt + 1) * 128], C.ident)
        cp(P, "act", xaT, pT[:, 0:256].rr("p (k n) -> p k n", k=2))
        pz = C.ps[1]
        for kt in range(2):
            mm(P, pz[:, 0:128], w1cat[:, kt, :], xaT[:, kt, :], kt == 0, kt == 1)
        for kt in range(2):
            mm(P, pz[0:64, 128:256], g1[:, kt, :], xaT[:, kt, :], kt == 0, kt == 1)
        act(P, z[0:64, :], pz[0:64, 0:128], AF.Tanh)
        cp(P, "dve", z[64:128, :], pz[64:128, 0:128])
        act(P, sg, pz[0:64, 128:256], AF.Sigmoid)
        pw, pa_, pg = C.ps[2], C.ps[3], C.ps[4]
        mm(P, pw, z, w2blk[:, 0:512], True, True)
        mm(P, pa_, z, w2blk[:, 512:1024], True, True)
        mm(P, pg[:, 0:256], sg, g2, True, True)
        tt(P, "dve", wl, pw, w0, ALU.add)
        act(P, wl, wl, AF.Sigmoid)
        act(P, wdec, wl, AF.Exp, scale=-0.6065306597126334)
        tt(P, "dve", il, pa_, a0, ALU.add)
        act(P, iclr, il, AF.Sigmoid)
        cp(P, "act", F[:, 0:256], pg[:, 0:256])
        tt(P, "pool", kk0, k_, kkp, ALU.mult)
        tt(P, "pool", sq, kk0, kk0, ALU.mult)
        red(P, "dve", ss[:, 0:4], sq.rr("p (h k) -> p h k", h=4))
        ts(P, "dve", ss[:, 0:4], ss[:, 0:4], EPS, ALU.add)
        act(P, ss[:, 0:4], ss[:, 0:4], AF.Sqrt)
        P.op("dve", "reciprocal", out=ss[:, 0:4], in_=ss[:, 0:4])
        tt(P, "dve", kk.rr("p (h k) -> p h k", h=4), kk0.rr("p (h k) -> p h k", h=4),
           ss[:, 0:4][:, :, None].bc([128, 4, 64]), ALU.mult)
        ic3 = iclr.rr("p (d c) -> p d c", d=2)
        stt(P, "dve", t1.rr("p (d c) -> p d c", d=2), ic3, -1.0, ka[:, None, :].bc([128, 2, 256]), ALU.add, ALU.mult)
        stt(P, "dve", keff.rr("p (d c) -> p d c", d=2), t1.rr("p (d c) -> p d c", d=2), 1.0,
            k_[:, None, :].bc([128, 2, 256]), ALU.add, ALU.mult)
        tt(P, "pool", bb.rr("p (d c) -> p d c", d=2), ic3, kk[:, None, :].bc([128, 2, 256]), ALU.mult)
        tt(P, "pool", rkt, r_, k_, ALU.mult)
        tt(P, "pool", rkt, rkt, rkp, ALU.mult)
        red(P, "dve", ss[:, 4:8], rkt.rr("p (h k) -> p h k", h=4))
        tt(P, "dve", F[:, 256:512].rr("p (h k) -> p h k", h=4), v_.rr("p (h k) -> p h k", h=4),
           ss[:, 4:8][:, :, None].bc([128, 4, 64]), ALU.mult)
        pT2 = C.ps[5]
        tr(P, pT2[0:32, 0:128], u[:, O1 + 768:O1 + 800], C.ident)
        cp(P, "act", alT, pT2[0:32, 0:128])
        mm(P, pT2[:, 128:384], alT, a2blk, True, True)
        tt(P, "dve", gl, pT2[:, 128:384], gab, ALU.add)
        act(P, gl, gl, AF.Sigmoid)
        act(P, gl, gl, AF.Ln)
        if C.dbg.get("old_gla"):
            act(P, gdec, gl, AF.Exp, scale=1.0 / 16.0)
        act(P, sr, u[:, O1 + 512:O1 + 768], AF.Silu)
        tt(P, "pool", F[:, 512:768], sr, glng, ALU.mult)
        CHt = cht[i]
        ts(P, "pool", CHt[:, 0:512], wl, -0.6065306597126334, ALU.mult)
        cp(P, "act", CHt[:, 512:1024], keff)
        cp(P, "pool", CHt[:, 1024:1536], bb)
        ts(P, "dve", CHt[:, 1536:1792], kk, -1.0, ALU.mult)
        cp(P, "act", CHt[:, 1792:2048], r_)
        cp(P, "pool", CHt[:, 2048:2304], v_)
        ts(P, "dve", CHt[:, 2304:2560], gl, 1.0 / 16.0, ALU.mult)
        ts(P, "pool", CHt[:, 2560:2688], u[:, O1:O1 + 128], 32.0 ** -0.5, ALU.mult)
        cp(P, "act", CHt[:, 2688:2816], u[:, O1 + 128:O1 + 256])
        cp(P, "pool", CHt[:, 2816:3072], u[:, O1 + 256:O1 + 512])
        dma(P, "sp", V(C.CH_ap[t * 128:(t + 1) * 128, :], C.CH_bufs[t]), CHt)
        if not C.dbg.get("old_gla"):
            dma(P, "pool", V(C.FIN_ap[t * 128:(t + 1) * 128, :], C.FIN_bufs[t]), F)
            continue
        for d, R in ((0, R0), (1, R1)):
            e1 = "dve" if d == 0 else "pool"
            e2 = "pool" if d == 0 else "dve"
            src = wdec[:, d * 256:(d + 1) * 256].rr("p (a h k) -> p a h k", a=2, h=2)
            hi = rv(R, 0, 128).rr("p h (a k) -> p a h k", a=2)
            lo = rv(R, 192, 128).rr("p h (a k) -> p a h k", a=2)
            cp(P, e1, hi, src)
            tt(P, e1, lo, src, hi, ALU.subtract)
            gsrc = gdec[:, d * 128:(d + 1) * 128].rr("p (a h k) -> p a h k", a=2, h=2)
            ghi = rv(R, 128, 64).rr("p h (a k) -> p a h k", a=2)
            glo = rv(R, 320, 64).rr("p h (a k) -> p a h k", a=2)
            cp(P, e2, ghi, gsrc)
            tt(P, e2, glo, gsrc, ghi, ALU.subtract)
            cp(P, e1, rv(R, 384, 128).rr("p h (a k) -> p a h k", a=2),
               keff[:, d * 256:(d + 1) * 256].rr("p (a h k) -> p a h k", a=2, h=2))
            cp(P, e2, rv(R, 512, 64).rr("p h (a k) -> p a h k", a=2),
               u[:, O1 + 128:O1 + 256].rr("p (a h k) -> p a h k", a=2, h=2))
            cp(P, e1, rv(R, 576, 128).rr("p h (a k) -> p a h k", a=2), r_.rr("p (a h k) -> p a h k", a=2, h=2))
            ts(P, e2, rv(R, 704, 64).rr("p h (a k) -> p a h k", a=2),
               u[:, O1:O1 + 128].rr("p (a h k) -> p a h k", a=2, h=2), 32.0 ** -0.5, ALU.mult)
            ts(P, e1, rv(R, 768, 128).rr("p h (a k) -> p a h k", a=2), kk.rr("p (a h k) -> p a h k", a=2, h=2),
               -1.0, ALU.mult)
            cp(P, e2, rv(R, 896, 128).rr("p h (a k) -> p a h k", a=2),
               bb[:, d * 256:(d + 1) * 256].rr("p (a h k) -> p a h k", a=2, h=2))
            dma(P, "sp" if d == 0 else "pool",
                V(C.STR_ap[d][t * 128:(t + 1) * 128].rearrange("t h n -> t (h n)"), C.STR_bufs[d][t]),
                R.rr("p h n -> p (h n)"))
        pv4 = C.ps[6]
        for a in range(2):
            tr(P, pv4[:, a * 128:(a + 1) * 128], v_[:, a * 128:(a + 1) * 128], C.ident)
        for a in range(2):
            tr(P, pv4[:, (2 + a) * 128:(3 + a) * 128], u[:, O1 + 256 + a * 128:O1 + 384 + a * 128], C.ident)
        VT = vt[i]
        cp(P, "act", VT[:, :, 0], pv4[:, 0:128])
        cp(P, "dve", VT[:, :, 1], pv4[:, 0:128])
        cp(P, "act", VT[:, :, 2], pv4[:, 128:256])
        cp(P, "dve", VT[:, :, 3], pv4[:, 128:256])
        cp(P, "act", VT[:, :, 4], pv4[:, 256:384])
        cp(P, "dve", VT[:, :, 5], pv4[:, 384:512])
        dma(P, "sp", V(C.Vs_ap[:, t * 128:(t + 1) * 128, :], C.Vs_bufs[t]), VT)
        dma(P, "pool", V(C.FIN_ap[t * 128:(t + 1) * 128, :], C.FIN_bufs[t]), F)
    A.close()


def phase_scan(C, l, nchunks=None):
    P, I = C.P, C.I
    A = Arena(P)
    S = A.sb("S", [128, 2, 64])
    T3 = A.sb("T3", [128, 2, 64])
    T4 = A.sb("T4", [128, 2, 64])
    self_f = A.sb("sel_f", [128, 64, 128])
    sel = A.sb("sel", [128, 64, 128], BF16)
    dma(P, "sp", self_f, I["sel"])
    cp(P, "pool", sel, self_f)
    rows = [[A.sb("srow%d%d" % (d, i), [128, 1024], BF16) for i in range(2)] for d in range(2)]
    vb = [A.sb("vb%d" % i, [128, 2, 64, 6]) for i in range(2)]
    yb = [A.sb("yb%d" % i, [128, 2, 64, 6]) for i in range(2)]
    P.op("dve", "memset", ap=S, constant=0.0)
    for i in range(2):
        P.op("pool", "memset", ap=yb[i], constant=0.0)
    NCH = NTOK // 64
    for c in range(NCH if nchunks is None else nchunks):
        zf = c * 64
        zb = (192 - 64 * c) if c < 4 else (4544 - 64 * c)
        i = c % 2
        dma(P, "sp", rows[0][i], V(C.STR_ap[0][zf:zf + 64].rearrange("t h n -> (t h) n"), C.STR_bufs[0][zf // 128]))
        dma(P, "pool", rows[1][i], V(C.STR_ap[1][zb:zb + 64].rearrange("t h n -> (t h) n"), C.STR_bufs[1][zb // 128]))
        dma(P, "sp", vb[i][:, 0], V(C.Vs_ap[:, zf:zf + 64, :], C.Vs_bufs[zf // 128]))
        dma(P, "pool", vb[i][:, 1], V(C.Vs_ap[:, zb:zb + 64, :], C.Vs_bufs[zb // 128]))
        YB = yb[i]
        for j in range(64):
            s = c * 64 + j
            pb = (s % 2) * 2
            for d in range(2):
                jj = j if d == 0 else 63 - j
                lt = sel[:, jj, :]
                R = rows[d][i]
                bx = C.ps[pb + d]
                mm(P, bx[:, 0:64], lt, R[:, 128:192], True, False, mark=False)
                mm(P, bx[:, 0:64], lt, R[:, 320:384], False, True, mark=False)
                mm(P, bx[:, 64:128], lt, R[:, 512:576], True, True, mark=False)
                mm(P, bx[:, 128:192], lt, R[:, 704:768], True, True, mark=(d == 1))
            R4 = V(C.psall[:, pb * 512:pb * 512 + 1024].rearrange("p (d x) -> p d x", d=2), tuple(C.psb[pb:pb + 2]))
            Dv, KKv, RQv = R4[:, :, 0:64], R4[:, :, 64:128], R4[:, :, 128:192]
            P.op("dve", "tensor_tensor", lax=True, out=S, in0=S, in1=Dv, op=ALU.mult)
            vv = cust(vb[i], j * 6 + 4, [((127 - 2 * j) * 6, 2), (1, 2), (0, 32)])
            P.op("dve", "tensor_tensor", lax=True, out=T3.rr("p d (g k) -> p d g k", g=2),
                 in0=KKv.rr("p d (g k) -> p d g k", g=2), in1=vv, op=ALU.mult)
            P.op("dve", "tensor_tensor", lax=True, out=S, in0=S, in1=T3, op=ALU.add)
            P.op("dve", "tensor_tensor", lax=True, out=T4, in0=S, in1=RQv, op=ALU.mult)
            yv = cust(YB, j * 6 + 4, [((127 - 2 * j) * 6, 2), (1, 2)])
            P.op("dve", "tensor_reduce", lax=True, out=yv, in_=T4.rr("p d (g k) -> p d g k", g=2), axis=AX.X, op=ALU.add)
        dma(P, "sp", V(C.Y_ap[0][:, zf:zf + 64, :], C.Y_bufs[0][zf // 64]), YB[:, 0])
        dma(P, "pool", V(C.Y_ap[1][:, zb:zb + 64, :], C.Y_bufs[1][zb // 64]), YB[:, 1])
    A.close()


def phase_fin_ab(C, l, L):
    P, I = C.P, C.I
    A = Arena(P)
    pv = A.sb("pv", [128, NPV])
    dma(P, "sp", pv, I["pv"][l:l + 1, :].bc([128, NPV]))
    lng = pv[:, PV_LNG:PV_LNG + 256]
    yt = [A.sb("yt%d" % i, [128, 2, 128, 6]) for i in range(2)]
    fin = [A.sb("finf%d" % i, [128, 768]) for i in range(2)]
    ya = A.sb("ya", [128, 256])
    ytm = [A.sb("ytm%d" % i, [128, 2, 256]) for i in range(2)]
    ytg = [A.sb("ytg%d" % i, [128, 2, 256]) for i in range(2)]
    yg = A.sb("yg", [128, 256])
    sq = A.sb("sqf", [128, 256])
    st = A.sb("stf", [128, 16])
    ob = [A.sb("ob%d" % i, [128, 4, 128], BF16) for i in range(2)]
    for t in range(NT):
        i = t % 2
        Y = yt[i]
        F = fin[i]
        if C.dbg.get("old_gla"):
            for d in range(2):
                dma(P, "sp" if d == 0 else "pool", Y[:, d],
                    V(C.Y_ap[d][:, t * 128:(t + 1) * 128, :], (C.Y_bufs[d][2 * t], C.Y_bufs[d][2 * t + 1])))
        dma(P, "sp", F, V(C.FIN_ap[t * 128:(t + 1) * 128, :], C.FIN_bufs[t]))
        pr, pg = C.ps[0], C.ps[1]
        if C.dbg.get("old_rwkv"):
            for a in range(2):
                n = 0
                for d in range(2):
                    for g in (2 * a, 2 * a + 1):
                        mm(P, pr[:, a * 128:(a + 1) * 128], Y[:, d, :, g], C.ident, n == 0, n == 3)
                        n += 1
        if C.dbg.get("old_gla"):
            for a in range(2):
                for d in range(2):
                    mm(P, pg[:, a * 128:(a + 1) * 128], Y[:, d, :, 4 + a], C.ident, d == 0, d == 1)
        if C.dbg.get("old_rwkv"):
            cp(P, "act", ya, pr[:, 0:256])
        else:
            for d in range(2):
                dma(P, "sp" if d == 0 else "pool", ytm[i][:, d, :], V(C.YT_ap[d][t * 128:(t + 1) * 128, :], C.YT_bufs[d][t]))
            tt(P, "pool", ya, ytm[i][:, 0, :], ytm[i][:, 1, :], ALU.add)
        ya4 = ya.rr("p (h k) -> p h k", h=4)
        red(P, "dve", st[:, 0:4], ya4)
        ts(P, "dve", st[:, 0:4], st[:, 0:4], 1.0 / 64.0, ALU.mult)
        tt(P, "dve", ya4, ya4, st[:, 0:4][:, :, None].bc([128, 4, 64]), ALU.subtract)
        tt(P, "pool", sq, ya, ya, ALU.mult)
        red(P, "dve", st[:, 4:8], sq.rr("p (h k) -> p h k", h=4))
        ts(P, "dve", st[:, 4:8], st[:, 4:8], 1.0 / 64.0, ALU.mult, GN_EPS, ALU.add)
        act(P, st[:, 4:8], st[:, 4:8], AF.Sqrt)
        P.op("dve", "reciprocal", out=st[:, 4:8], in_=st[:, 4:8])
        tt(P, "dve", ya4, ya4, st[:, 4:8][:, :, None].bc([128, 4, 64]), ALU.mult)
        tt(P, "pool", ya, ya, lng, ALU.mult)
        tt(P, "pool", ya, ya, F[:, 256:512], ALU.add)
        tt(P, "pool", ya, ya, F[:, 0:256], ALU.mult)
        if C.dbg.get("old_gla"):
            cp(P, "act", yg, pg[:, 0:256])
        else:
            for d in range(2):
                dma(P, "sp" if d == 0 else "pool", ytg[i][:, d, :], V(C.YTG_ap[d][t * 128:(t + 1) * 128, :], C.YTG_bufs[d][t]))
            tt(P, "pool", yg, ytg[i][:, 0, :], ytg[i][:, 1, :], ALU.add)
        yg4 = yg.rr("p (h k) -> p h k", h=4)
        tt(P, "pool", sq, yg, yg, ALU.mult)
        red(P, "dve", st[:, 8:12], sq.rr("p (h k) -> p h k", h=4))
        ts(P, "dve", st[:, 8:12], st[:, 8:12], 1.0 / 64.0, ALU.mult, EPS, ALU.add)
        act(P, st[:, 8:12], st[:, 8:12], AF.Sqrt)
        P.op("dve", "reciprocal", out=st[:, 8:12], in_=st[:, 8:12])
        tt(P, "dve", yg4, yg4, st[:, 8:12][:, :, None].bc([128, 4, 64]), ALU.mult)
        tt(P, "pool", yg, yg, F[:, 512:768], ALU.mult)
        po = C.ps[2]
        for a in range(2):
            tr(P, po[:, a * 128:(a + 1) * 128], ya[:, a * 128:(a + 1) * 128], C.ident)
            tr(P, po[:, (2 + a) * 128:(3 + a) * 128], yg[:, a * 128:(a + 1) * 128], C.ident)
        OB = ob[i]
        cp(P, "act", OB, po.rr("p (a n) -> p a n", a=4))
        for br in range(2):
            dma(P, "sp" if br == 0 else "pool",
                V(C.YB_ap[br][:, t * 128:(t + 1) * 128].rearrange("(a p) n -> p a n", p=128), C.YB_bufs[br][t]),
                OB[:, 2 * br:2 * br + 2, :])
    A.close()


def phase_attn(C, l, L):
    P, I = C.P, C.I
    A = Arena(P)
    qT = A.sb("qT", [128, 2, NTOK], BF16)
    kT = A.sb("kT", [128, 2, NTOK], BF16)
    vtm = A.sb("vtm", [128, NT, 128], BF16)
    maskw = A.sb("maskw", [128, 384])
    sink = A.sb("sink", [128, 16])
    identb = A.sb("identb", [128, 128], BF16)
    dma(P, "sp", maskw, I["maskw"])
    dma(P, "sp", sink, I["attn_sink"][l:l + 1, :].bc([128, 16]))
    cp(P, "pool", identb, C.ident)
    ua = [A.sb("ua%d" % i, [128, 512]) for i in range(2)]
    rc = [A.sb("rc%d" % i, [128, 32]) for i in range(2)]
    rs = [A.sb("rs%d" % i, [128, 32]) for i in range(2)]
    qk = A.sb("qk", [128, 6, 64])
    tmp = A.sb("tmpr", [128, 6, 32])
    kd = A.sb("kd", [128, 2, 2, 64])
    cut = C.dbg.get("attn_cut", 9)
    for t in range(NT if cut > 1 else 0):
        i = t % 2
        r0 = urow(t)
        u = ua[i]
        dma(P, "sp", u, V(C.U_ap[r0:r0 + 128, O2:O3], C.U_bufs[t]))
        u6 = u[:, 0:384].rr("p (h d) -> p h d", h=6)
        if t >= 2 and not C.dbg.get("norope"):
            dma(P, "pool", rc[i], I["ropec"][(t - 2) * 128:(t - 1) * 128, :])
            dma(P, "pool", rs[i], I["ropes"][(t - 2) * 128:(t - 1) * 128, :])
            cb = rc[i][:, None, :].bc([128, 6, 32])
            sb_ = rs[i][:, None, :].bc([128, 6, 32])
            z1, z2 = u6[:, :, 0:32], u6[:, :, 32:64]
            tt(P, "dve", qk[:, :, 0:32], z1, cb, ALU.mult)
            tt(P, "pool", tmp, z2, sb_, ALU.mult)
            tt(P, "dve", qk[:, :, 0:32], qk[:, :, 0:32], tmp, ALU.subtract)
            tt(P, "dve", qk[:, :, 32:64], z1, sb_, ALU.mult)
            tt(P, "pool", tmp, z2, cb, ALU.mult)
            tt(P, "dve", qk[:, :, 32:64], qk[:, :, 32:64], tmp, ALU.add)
        else:
            cp(P, "dve", qk, u6)
        if cut < 3:
            continue
        cp(P, "pool", kd[:, :, 0, :], qk[:, 4:6, :])
        cp(P, "pool", kd[:, :, 1, :], qk[:, 4:6, :])
        cp(P, "pool", vtm[:, t, :], u[:, 384:512])
        if cut < 4:
            continue
        pq = C.ps[t % 2]
        qf = qk.rr("p h d -> p (h d)")
        kf = kd.rr("p k r d -> p (k r d)")
        for a in range(2):
            tr(P, pq[:, a * 128:(a + 1) * 128], qf[:, a * 128:(a + 1) * 128], C.ident)
            tr(P, pq[:, (2 + a) * 128:(3 + a) * 128], kf[:, a * 128:(a + 1) * 128], C.ident)
        ts(P, "dve", qT[:, :, t * 128:(t + 1) * 128], pq[:, 0:256].rr("p (a n) -> p a n", a=2), 0.125, ALU.mult)
        cp(P, "dve", kT[:, :, t * 128:(t + 1) * 128], pq[:, 256:512].rr("p (a n) -> p a n", a=2))
    if C.dbg.get("attn_p1"):
        A.close()
        return
    sc = [A.sb("sc%d" % i, [128, 640]) for i in range(2)]
    pb = [A.sb("pb%d" % i, [128, 640], BF16) for i in range(2)]
    pTs = [A.sb("pTs%d" % i, [128, 5, 128], BF16) for i in range(2)]
    st = [A.sb("sta%d" % i, [128, 8]) for i in range(2)]
    yo = [A.sb("yo%d" % i, [128, 256]) for i in range(2)]
    oc = [A.sb("oc%d" % i, [128, 2, 128], BF16) for i in range(2)]
    psT = [V(C.psall[:, b * 512:(b + 1) * 512].bitcast(BF16), C.psb[b]) for b in (4, 5)]
    n = 0
    for t in range(NT):
        YO = yo[t % 2]
        if t >= 2:
            lo, hi = max(t - 1, 2), min(t + 1, NT - 1)
            nw = hi - lo + 1
            m0 = (lo - (t - 1)) * 128
        else:
            nw = 0
        nk = nw * 128 + 256
        nblk = nw + 2
        kblocks = ([lo + b for b in range(nw)] if nw else []) + [0, 1]
        po = C.ps[6 + (t % 2)]
        for h in range(4):
            kv, hl = h // 2, h % 2
            i = n % 2
            n += 1
            S_, Pb, PT, ST = sc[i], pb[i], pTs[i], st[i]
            qv = qT[hl * 64:(hl + 1) * 64, kv, t * 128:(t + 1) * 128]
            pa_, pc_ = C.ps[2 * i], C.ps[2 * i + 1]
            if nw:
                mm(P, pa_[:, 0:nw * 128], qv, kT[hl * 64:(hl + 1) * 64, kv, lo * 128:(hi + 1) * 128], True, True)
            mm(P, pc_[:, 0:256], qv, kT[hl * 64:(hl + 1) * 64, kv, 0:256], True, True)
            if nw:
                tt(P, "dve", S_[:, 0:nw * 128], pa_[:, 0:nw * 128], maskw[:, m0:m0 + nw * 128], ALU.add)
            cp(P, "act", S_[:, nw * 128:nk], pc_[:, 0:256])
            P.op("dve", "tensor_reduce", out=ST[:, 0:1], in_=S_[:, 0:nk], axis=AX.X, op=ALU.max)
            tt(P, "dve", ST[:, 0:1], ST[:, 0:1], sink[:, h:h + 1], ALU.max)
            ts(P, "dve", ST[:, 1:2], ST[:, 0:1], -1.0, ALU.mult)
            act(P, Pb[:, 0:nk], S_[:, 0:nk], AF.Exp, bias=ST[:, 1:2], accum_out=ST[:, 2:3])
            act(P, ST[:, 3:4], sink[:, h:h + 1], AF.Exp, bias=ST[:, 1:2])
            tt(P, "dve", ST[:, 4:5], ST[:, 2:3], ST[:, 3:4], ALU.add)
            P.op("dve", "reciprocal", out=ST[:, 5:6], in_=ST[:, 4:5])
            pt = psT[i]
            for b in range(nblk):
                tr(P, pt[:, b * 128:(b + 1) * 128], Pb[:, b * 128:(b + 1) * 128], identb)
            cp(P, "act" if h % 2 == 0 else "dve", PT[:, 0:nblk, :], pt[:, 0:nblk * 128].rr("p (b n) -> p b n", b=nblk))
            for b in range(nblk):
                mm(P, po[:, h * 64:(h + 1) * 64], PT[:, b, :], vtm[:, kblocks[b], kv * 64:(kv + 1) * 64],
                   b == 0, b == nblk - 1)
            ts(P, "dve", YO[:, h * 64:(h + 1) * 64], po[:, h * 64:(h + 1) * 64], ST[:, 5:6], ALU.mult)
        pf = C.ps[t % 2]
        for a in range(2):
            tr(P, pf[:, a * 128:(a + 1) * 128], YO[:, a * 128:(a + 1) * 128], C.ident)
        OC = oc[t % 2]
        cp(P, "act", OC, pf[:, 0:256].rr("p (a n) -> p a n", a=2))
        dma(P, "sp", V(C.YB_ap[2][:, t * 128:(t + 1) * 128].rearrange("(a p) n -> p a n", p=128), C.YB_bufs[2][t]), OC)
    A.close()


PI = math.pi
S5_BLOCKS = [(0, 256)] + [(256 + 512 * i, 512) for i in range(8)]


I32 = mybir.dt.int32
TWO_PI_HI = 6.28125
TWO_PI_LO = 2.0 * math.pi - 6.28125


def sincos(P, A, s_out, c_out, x, shape, tag):
    qi = A.sb("qi" + tag, shape, I32)
    kf = A.sb("kf" + tag, shape)
    r = A.sb("rr" + tag, shape)
    m = A.sb("mm" + tag, shape)
    for extra, out in ((0.0, s_out), (0.5 * PI, c_out)):
        ts(P, "dve", r, x, 16.0 * PI + extra, ALU.add)
        ts(P, "dve", qi, r, 1.0 / (2.0 * PI), ALU.mult)
        cp(P, "dve", kf, qi)
        stt(P, "dve", r, kf, -TWO_PI_HI, r, ALU.mult, ALU.add)
        stt(P, "dve", r, kf, -TWO_PI_LO, r, ALU.mult, ALU.add)
        ts(P, "dve", m, r, PI, ALU.is_gt)
        stt(P, "dve", r, m, -2.0 * PI, r, ALU.mult, ALU.add)
        ts(P, "dve", m, r, -PI, ALU.is_lt)
        stt(P, "dve", r, m, 2.0 * PI, r, ALU.mult, ALU.add)
        ts(P, "dve", r, r, PI, ALU.min, -PI, ALU.max)
        act(P, out, r, AF.Sin)


def phase_s5(C, l, L):
    P, I = C.P, C.I
    A = Arena(P)
    th_s = A.sb("th_s", [128, 2, 8])
    rho_s = A.sb("rho_s", [128, 2, 8])
    Ck = A.sb("Ck", [128, 2, 8, 13])
    Sk = A.sb("Sk", [128, 2, 8, 13])
    BbT = A.sb("BbT", [128, 2, 2, 8, 128], BF16)
    CT = A.sb("CT", [128, 2, 2, 8, 128], BF16)
    A0 = A
    A = Arena(P)
    tmpA = [A.sb("s5r%d" % i, [128, 1024]) for i in range(8)]
    lre, lim, ldt, t_s, t_c, t_a, t_b, t_d = tmpA
    pw2f = A.sb("pw2", [128, 16])
    dma(P, "sp", pw2f, I["pw2"][0:1, :].bc([128, 16]))
    pw2 = pw2f[:, 0:13]
    fR = [A.sb("fR%d" % d, [128, 1024]) for d in range(2)]
    fI = [A.sb("fI%d" % d, [128, 1024]) for d in range(2)]
    sm = A.sb("sm", [128, 2, 3, 8])
    dma(P, "sp", sm, I["s5_sm"][l])
    act(P, sm[:, :, 2, :], sm[:, :, 2, :], AF.Exp)
    tt(P, "dve", th_s, sm[:, :, 1, :], sm[:, :, 2, :], ALU.mult)
    tt(P, "dve", rho_s, sm[:, :, 0, :], sm[:, :, 2, :], ALU.mult)
    act(P, rho_s, rho_s, AF.Exp)
    ang13 = A.sb("ang13", [128, 16, 13])
    tt(P, "dve", ang13, th_s.rr("p d j -> p (d j)")[:, :, None].bc([128, 16, 13]), pw2[:, None, :].bc([128, 16, 13]), ALU.mult)
    sincos(P, A, Sk.rr("p d j k -> p (d j) k"), Ck.rr("p d j k -> p (d j) k"), ang13, [128, 16, 13], "k")
    for d in range(2):
        dma(P, "sp", lre, I["s5_rows"][l, d, 0:1, :].bc([128, 1024]))
        dma(P, "pool", lim, I["s5_rows"][l, d, 1:2, :].bc([128, 1024]))
        dma(P, "sp", ldt, I["s5_rows"][l, d, 2:3, :].bc([128, 1024]))
        act(P, ldt, ldt, AF.Exp)
        tt(P, "dve", t_a, lim, ldt, ALU.mult)
        sincos(P, A, t_s, t_c, t_a, [128, 1024], "r%d" % d)
        tt(P, "dve", t_a, lre, ldt, ALU.mult)
        act(P, t_a, t_a, AF.Exp)
        tt(P, "dve", t_c, t_c, t_a, ALU.mult)
        tt(P, "dve", t_s, t_s, t_a, ALU.mult)
        ts(P, "dve", t_c, t_c, -1.0, ALU.add)
        tt(P, "dve", t_a, lre, lre, ALU.mult)
        tt(P, "pool", t_b, lim, lim, ALU.mult)
        tt(P, "dve", t_a, t_a, t_b, ALU.add)
        P.op("dve", "reciprocal", out=t_a, in_=t_a)
        tt(P, "dve", t_b, t_c, lre, ALU.mult)
        tt(P, "pool", t_d, t_s, lim, ALU.mult)
        tt(P, "dve", t_b, t_b, t_d, ALU.add)
        tt(P, "dve", fR[d], t_b, t_a, ALU.mult)
        tt(P, "dve", t_b, t_s, lre, ALU.mult)
        tt(P, "pool", t_d, t_c, lim, ALU.mult)
        tt(P, "dve", t_b, t_b, t_d, ALU.subtract)
        tt(P, "dve", fI[d], t_b, t_a, ALU.mult)
    bt_f = A.sb("bt_f", [128, 2, 8, 128])
    dma(P, "sp", bt_f[:, 0], I["s5_bt"][l, 0].rr("j c s -> c j s"))
    dma(P, "pool", bt_f[:, 1], I["s5_bt"][l, 1].rr("j c s -> c j s"))
    for d in range(2):
        fr = fR[d].rr("p (j s) -> p j s", j=8)
        fi = fI[d].rr("p (j s) -> p j s", j=8)
        ta = t_a.rr("p (j s) -> p j s", j=8)
        tb = t_b.rr("p (j s) -> p j s", j=8)
        tt(P, "dve", ta, bt_f[:, 0], fr, ALU.mult)
        tt(P, "pool", tb, bt_f[:, 1], fi, ALU.mult)
        tt(P, "dve", BbT[:, d, 0], ta, tb, ALU.subtract)
        tt(P, "dve", ta, bt_f[:, 0], fi, ALU.mult)
        tt(P, "pool", tb, bt_f[:, 1], fr, ALU.mult)
        tt(P, "dve", BbT[:, d, 1], ta, tb, ALU.add)
        for ri in range(2):
            ctf = t_c if ri == 0 else t_d
            dma(P, "sp" if ri == 0 else "pool", ctf.rr("p (j c) -> p j c", j=8), I["s5_ct"][l, d, ri].rr("j s c -> s j c"))
            if ri == 0:
                cp(P, "pool", CT[:, d, 0], ctf.rr("p (j c) -> p j c", j=8))
            else:
                ts(P, "pool", CT[:, d, 1], ctf.rr("p (j c) -> p j c", j=8), -1.0, ALU.mult)
    A.close()
    A = A0
    cut = C.dbg.get("s5_cut", 99)
    if cut <= 1:
        A.close(); return
    if not C.dbg.get("s5_small"):
        ct, sn = A.sb("ct", [128, NTOK]), A.sb("sn", [128, NTOK])
        w_re, w_im = A.sb("w_re", [128, NTOK]), A.sb("w_im", [128, NTOK])
    uB = A.sb("uB", [128, 2, NTOK], BF16)
    yacc = A.sb("yacc", [128, 2, NTOK])
    x_re, x_im = A.sb("x_re", [128, NTOK], BF16), A.sb("x_im", [128, NTOK], BF16)
    dsk = A.sb("dsk", [128, 16])
    bgl = A.sb("bgl", [128, 16])
    dma(P, "sp", dsk, I["s5_d_fm"][l])
    dma(P, "sp", bgl, I["s5_bglu_fm"][l])
    wglu = load_bf16(P, A, "wglu", [128, 2, 256], I["s5_w_glu"][l].rr("(k p) n -> p k n", p=128))
    ut = [A.sb("ut%d" % i, [128, 256]) for i in range(2)]
    var = C.dbg.get("s5_var", 9)
    for t in range(NT if var > 0 else 0):
        i = t % 2
        r0 = urow(t)
        dma(P, "sp" if i == 0 else "pool", ut[i], V(C.U_ap[r0:r0 + 128, O3:O4], C.U_bufs[t]))
        pp = C.ps[i]
        for a in range(2):
            tr(P, pp[:, a * 128:(a + 1) * 128], ut[i][:, a * 128:(a + 1) * 128], C.ident)
        if var < 2:
            continue
        for a in range(2):
            cp(P, "dve", uB[:, a, t * 128:(t + 1) * 128], pp[:, a * 128:(a + 1) * 128])
        for a in range(2):
            ts(P, "dve", yacc[:, a, t * 128:(t + 1) * 128], pp[:, a * 128:(a + 1) * 128], dsk[:, a:a + 1], ALU.mult)
    tm = [A.sb("tm%d" % i, [128, 512]) for i in range(4)]
    if cut <= 2:
        A.close(); return
    nb = 0
    for d in range(2):
        for j in range(8):
            jt = j // 4
            th = th_s[:, d, j:j + 1]
            P.op("dve", "memset", ap=ct[:, 0:1], constant=1.0)
            P.op("dve", "memset", ap=sn[:, 0:1], constant=0.0)
            k = 0
            n = 1
            while n < NTOK:
                m = min(n, NTOK - n)
                ck, sk = Ck[:, d, j, k:k + 1], Sk[:, d, j, k:k + 1]
                e1, e2 = ("dve", "pool") if m >= 256 else ("dve", "dve")
                ts(P, e1, ct[:, n:n + m], ct[:, 0:m], ck, ALU.mult)
                ts(P, e2, sn[:, n:n + m], sn[:, 0:m], ck, ALU.mult)
                ts(P, e2, tm[0][:, 0:min(m, 512)] if m <= 512 else w_re[:, 0:m], sn[:, 0:m], sk, ALU.mult)
                ts(P, e1, tm[1][:, 0:min(m, 512)] if m <= 512 else w_im[:, 0:m], ct[:, 0:m], sk, ALU.mult)
                ta_ = tm[0][:, 0:m] if m <= 512 else w_re[:, 0:m]
                tb_ = tm[1][:, 0:m] if m <= 512 else w_im[:, 0:m]
                tt(P, e1, ct[:, n:n + m], ct[:, n:n + m], ta_, ALU.subtract)
                tt(P, e2, sn[:, n:n + m], sn[:, n:n + m], tb_, ALU.add)
                n += m
                k += 1
            if cut <= 3:
                A.close(); return
            for bi, (t0, n) in enumerate(S5_BLOCKS):
                if d == 0:
                    rhs = uB[:, jt, t0:t0 + n]
                else:
                    last = (255 - t0) if t0 < 256 else (4607 - t0)
                    rhs = cust(uB, jt * NTOK + last, [(-1, n)])
                pr, pi_ = C.ps[(nb % 2) * 2], C.ps[(nb % 2) * 2 + 1]
                nb += 1
                mm(P, pr[:, 0:n], BbT[:, d, 0, j, :], rhs, True, True)
                mm(P, pi_[:, 0:n], BbT[:, d, 1, j, :], rhs, True, True)
                c_, s_ = ct[:, t0:t0 + n], sn[:, t0:t0 + n]
                tt(P, "dve", tm[0][:, 0:n], pr[:, 0:n], c_, ALU.mult)
                tt(P, "dve", tm[1][:, 0:n], pi_[:, 0:n], s_, ALU.mult)
                tt(P, "pool", w_re[:, t0:t0 + n], tm[0][:, 0:n], tm[1][:, 0:n], ALU.add)
                tt(P, "dve", tm[2][:, 0:n], pi_[:, 0:n], c_, ALU.mult)
                tt(P, "dve", tm[3][:, 0:n], pr[:, 0:n], s_, ALU.mult)
                tt(P, "pool", w_im[:, t0:t0 + n], tm[2][:, 0:n], tm[3][:, 0:n], ALU.subtract)
            if cut <= 4:
                A.close(); return
            rb = rho_s[:, d, j:j + 1].bc([128, NTOK])
            P.op("dve", "tensor_tensor_scan", out=w_re, data0=rb, data1=w_re, initial=0.0, op0=ALU.mult, op1=ALU.add)
            P.op("dve", "tensor_tensor_scan", out=w_im, data0=rb, data1=w_im, initial=0.0, op0=ALU.mult, op1=ALU.add)
            if cut <= 5:
                A.close(); return
            for bi, (t0, n) in enumerate(S5_BLOCKS):
                c_, s_ = ct[:, t0:t0 + n], sn[:, t0:t0 + n]
                tt(P, "pool", tm[0][:, 0:n], w_re[:, t0:t0 + n], c_, ALU.mult)
                tt(P, "pool", tm[1][:, 0:n], w_im[:, t0:t0 + n], s_, ALU.mult)
                tt(P, "dve", x_re[:, t0:t0 + n], tm[0][:, 0:n], tm[1][:, 0:n], ALU.subtract)
                tt(P, "pool", tm[2][:, 0:n], w_re[:, t0:t0 + n], s_, ALU.mult)
                tt(P, "pool", tm[3][:, 0:n], w_im[:, t0:t0 + n], c_, ALU.mult)
                tt(P, "dve", x_im[:, t0:t0 + n], tm[2][:, 0:n], tm[3][:, 0:n], ALU.add)
                py = C.ps[4 + (bi % 2)]
                if d == 0:
                    xr, xi = x_re[:, t0:t0 + n], x_im[:, t0:t0 + n]
                    k0 = t0
                else:
                    k0 = (256 - t0 - n) if t0 < 256 else (4608 - t0 - n)
                    s_last = t0 + n - 1
                    xr, xi = cust(x_re, s_last, [(-1, n)]), cust(x_im, s_last, [(-1, n)])
                mm(P, py[:, 0:n], CT[:, d, 0, j, :], xr, True, False)
                mm(P, py[:, 0:n], CT[:, d, 1, j, :], xi, False, True)
                tt(P, "dve", yacc[:, jt, k0:k0 + n], yacc[:, jt, k0:k0 + n], py[:, 0:n], ALU.add)
    if cut <= 7:
        A.close(); return
    glb = uB
    ob = [A.sb("obs%d" % i, [128, 2, 512], BF16) for i in range(2)]
    for bi, (t0, n) in enumerate(S5_BLOCKS):
        for a in range(2):
            y = yacc[:, a, t0:t0 + n]
            tt(P, "pool", tm[0][:, 0:n], y, y, ALU.mult)
            ts(P, "dve", tm[0][:, 0:n], tm[0][:, 0:n], 0.044715, ALU.mult, 1.0, ALU.add)
            tt(P, "pool", tm[0][:, 0:n], tm[0][:, 0:n], y, ALU.mult)
            act(P, tm[0][:, 0:n], tm[0][:, 0:n], AF.Sigmoid, scale=1.5957691216057308)
            tt(P, "dve", y, y, tm[0][:, 0:n], ALU.mult)
            cp(P, "pool", glb[:, a, t0:t0 + n], y)
        OB = ob[bi % 2]
        for a in range(2):
            pz = C.ps[6 + a]
            for kt in range(2):
                mm(P, pz[:, 0:n], wglu[:, kt, a * 128:(a + 1) * 128], glb[:, kt, t0:t0 + n], kt == 0, kt == 1)
            act(P, tm[1 + a][:, 0:n], pz[:, 0:n], AF.Sigmoid, bias=bgl[:, a:a + 1])
            tt(P, "dve", OB[:, a, 0:n], yacc[:, a, t0:t0 + n], tm[1 + a][:, 0:n], ALU.mult)
        tl = [t for t in range(NT) if t * 128 >= t0 and t * 128 < t0 + n]
        dma(P, "sp", V(C.YB_ap[3][:, t0:t0 + n].rearrange("(a p) n -> p a n", p=128), tuple(C.YB_bufs[3][t] for t in tl)),
            OB[:, :, 0:n])
    A.close()


TOKBLK = [(0, 256)] + [(256 + 512 * i, 512) for i in range(8)]


def phase_win_gates(C, l, L, hfm):
    P, I = C.P, C.I
    A = Arena(P)
    wst = [A.sb("wgs%d" % i, [128, 8, 512]) for i in range(2)]
    wb = [A.sb("wgb%d" % i, [128, 8, 512], BF16) for i in range(2)]
    gst = [A.sb("gst%d" % i, [128, 512], BF16) for i in range(4)]
    wv = I["w_in"][l].rr("(k p) n -> p k n", p=128)
    n = 0
    for cb in range(8):
        c0 = O4 + cb * 512
        dma(P, "sp" if cb % 2 == 0 else "pool", wst[cb % 2], wv[:, :, c0:c0 + 512])
        cp(P, "pool", wb[cb % 2], wst[cb % 2])
        w = wb[cb % 2]
        for mi in range(4):
            row0 = cb * 512 + mi * 128
            for bi, (t0, nn) in enumerate(TOKBLK):
                ps = C.ps[n % 4]
                g = gst[n % 4]
                for k in range(8):
                    mm(P, ps[:, 0:nn], w[:, k, mi * 128:(mi + 1) * 128], hfm[:, k, t0:t0 + nn], k == 0, k == 7)
                cp(P, "act" if n % 2 == 0 else "dve", g[:, 0:nn], ps[:, 0:nn])
                dma(P, "sp" if n % 2 == 0 else "pool", V(C.Gt_ap[row0:row0 + 128, t0:t0 + nn], C.Gt_bufs[bi]), g[:, 0:nn])
                n += 1
    A.close()


def phase_merge(C, l, L):
    P, I = C.P, C.I
    A = Arena(P)
    wbr = A.sb("wbr", [128, 4, 2, 1024], BF16)
    wout = A.sb("wout", [128, 8, 1024], BF16)
    wst = A.sb("wmst", [128, 8, 1024])
    for i in range(4):
        dma(P, "sp", wst[:, 0:2, :], I["w_branch"][l, i].rr("(k p) n -> p k n", p=128))
        cp(P, "pool", wbr[:, i], wst[:, 0:2, :])
    dma(P, "sp", wst, I["w_out"][l].rr("(k p) n -> p k n", p=128))
    cp(P, "pool", wout, wst)
    yb = [A.sb("myb%d" % i, [128, 4, 2, 512], BF16) for i in range(2)]
    gt = [A.sb("mgt%d" % i, [128, 512], BF16) for i in range(4)]
    sg = [A.sb("msg%d" % i, [128, 512]) for i in range(2)]
    tmp = A.sb("mtmp", [128, 512])
    acc = A.sb("macc", [128, 512])
    mg = [A.sb("mmg%d" % i, [128, 8, 512], BF16) for i in range(2)]
    xt = [A.sb("mxt%d" % i, [128, 1024]) for i in range(2)]
    tm2 = A.sb("mtm2", [128, 1024])
    n = 0
    nx = 0
    for bi, (t0, nn) in enumerate(TOKBLK):
        YB = yb[bi % 2]
        tl = [t for t in range(NT) if t0 <= t * 128 < t0 + nn]
        for i in range(4):
            dma(P, "sp" if i % 2 == 0 else "pool", YB[:, i, :, 0:nn],
                V(C.YB_ap[i][:, t0:t0 + nn].rearrange("(a p) n -> p a n", p=128), tuple(C.YB_bufs[i][t] for t in tl)))
        MG = mg[bi % 2]
        for m in range(8):
            for i in range(4):
                g = gt[n % 4]
                dma(P, "sp" if n % 2 == 0 else "pool", g[:, 0:nn],
                    V(C.Gt_ap[i * 1024 + m * 128:i * 1024 + (m + 1) * 128, t0:t0 + nn], C.Gt_bufs[bi]))
                s_ = sg[n % 2]
                act(P, s_[:, 0:nn], g[:, 0:nn], AF.Sigmoid)
                ps = C.ps[n % 4]
                n += 1
                for kt in range(2):
                    mm(P, ps[:, 0:nn], wbr[:, i, kt, m * 128:(m + 1) * 128], YB[:, i, kt, 0:nn], kt == 0, kt == 1)
                if i == 0:
                    tt(P, "dve", acc[:, 0:nn], ps[:, 0:nn], s_[:, 0:nn], ALU.mult)
                elif i < 3:
                    tt(P, "dve", tmp[:, 0:nn], ps[:, 0:nn], s_[:, 0:nn], ALU.mult)
                    tt(P, "pool", acc[:, 0:nn], acc[:, 0:nn], tmp[:, 0:nn], ALU.add)
                else:
                    tt(P, "dve", tmp[:, 0:nn], ps[:, 0:nn], s_[:, 0:nn], ALU.mult)
                    tt(P, "dve", MG[:, m, 0:nn], acc[:, 0:nn], tmp[:, 0:nn], ALU.add)
        for ti, t in enumerate(tl):
            j = 1 if t < 2 else 0
            x = xt[nx % 2]
            nx += 1
            dma(P, "sp", x, xsrc(C, l, t))
            for half in range(2):
                po = C.ps[4 + half + 2 * (nx % 2)]
                for k in range(8):
                    mm(P, po, MG[:, k, ti * 128:(ti + 1) * 128], wout[:, k, half * 512:(half + 1) * 512], k == 0, k == 7)
                tt(P, "dve", tm2[:, half * 512:(half + 1) * 512], po, L.grow[0][j][:, half * 512:(half + 1) * 512], ALU.mult)
            tt(P, "pool", x, x, tm2, ALU.add)
            dma(P, "pool", C.xres[t], x)
    A.close()
    C.x_in_scratch = True


def make_router(C, l, L, RA):
    P, I = C.P, C.I
    wr = RA.sb("wr", [128, 8, 36])
    brow = RA.sb("brow", [128, 36])
    dma(P, "sp", wr, I["w_router"][l].rr("(k p) n -> p k n", p=128))
    dma(P, "sp", brow, I["b_router"][l:l + 1, :].bc([128, 36]))
    lg = RA.sb("lg", [128, 36])
    st = RA.sb("rst", [128, 16])
    oh = RA.sb("roh", [128, 4])
    em = RA.sb("rem", [128, 32])
    em2 = RA.sb("rem2", [128, 32])
    oh1 = RA.sb("roh1", [128, 32])
    oh2 = RA.sb("roh2", [128, 32])
    wg = RA.sb("rwg", [128, 32])
    junk = RA.sb("rjunk", [128, 4])

    def per_tile(t, hf):
        pl = C.ps[6]
        for k in range(8):
            mm(P, pl[:, 0:36], hf[:, k, :], wr[:, k, :], k == 0, k == 7)
        tt(P, "dve", lg, pl[:, 0:36], brow, ALU.add)
        g, e = lg[:, 0:4], lg[:, 4:36]
        P.op("dve", "tensor_reduce", out=st[:, 0:1], in_=g, axis=AX.X, op=ALU.max)
        ts(P, "dve", oh, g, st[:, 0:1], ALU.is_equal)
        ts(P, "dve", st[:, 1:2], st[:, 0:1], -1.0, ALU.mult)
        act(P, junk, g, AF.Exp, bias=st[:, 1:2], accum_out=st[:, 2:3])
        P.op("dve", "reciprocal", out=st[:, 3:4], in_=st[:, 2:3])
        ts(P, "dve", oh, oh, 1e30, ALU.mult, -1e30, ALU.add)
        tt(P, "dve", em.rr("p (g k) -> p g k", g=4), e.rr("p (g k) -> p g k", g=4),
           oh[:, :, None].bc([128, 4, 8]), ALU.add)
        P.op("dve", "tensor_reduce", out=st[:, 4:5], in_=em, axis=AX.X, op=ALU.max)
        ts(P, "dve", oh1, em, st[:, 4:5], ALU.is_equal)
        stt(P, "dve", em2, oh1, -1e30, em, ALU.mult, ALU.add)
        P.op("dve", "tensor_reduce", out=st[:, 5:6], in_=em2, axis=AX.X, op=ALU.max)
        ts(P, "dve", oh2, em2, st[:, 5:6], ALU.is_equal)
        tt(P, "dve", st[:, 6:7], st[:, 5:6], st[:, 4:5], ALU.subtract)
        act(P, st[:, 7:8], st[:, 6:7], AF.Exp)
        ts(P, "dve", st[:, 8:9], st[:, 7:8], 1.0, ALU.add)
        P.op("dve", "reciprocal", out=st[:, 9:10], in_=st[:, 8:9])
        tt(P, "dve", st[:, 10:11], st[:, 7:8], st[:, 9:10], ALU.mult)
        ts(P, "dve", wg, oh1, st[:, 9:10], ALU.mult)
        stt(P, "dve", wg, oh2, st[:, 10:11], wg, ALU.mult, ALU.add)
        ts(P, "dve", wg, wg, st[:, 3:4], ALU.mult)
        pt = C.ps[7]
        tr(P, pt[0:32, 0:128], wg, C.ident)
        cp(P, "dve", L.WT[:, t * 128:(t + 1) * 128], pt[0:32, 0:128])
    return per_tile


MOE_GROUPS = [list(range(0, 12)), list(range(12, 24)), list(range(24, 34))]


def phase_moe(C, l, L, H2, last):
    P, I = C.P, C.I
    A = Arena(P)
    hg = A.sb("hg", [128, 8, 12 * 128], BF16)
    wst = [A.sb("ews%d" % i, [128, 4, 512]) for i in range(2)]
    wgu = [A.sb("wgu%d" % i, [128, 2, 8, 512], BF16) for i in range(2)]
    wd = [A.sb("wd%d" % i, [128, 4, 1024], BF16) for i in range(2)]
    yacc = A.sb("eyacc", [128, 12, 1024])
    hid = [A.sb("hid%d" % i, [128, 4, 512], BF16) for i in range(2)]
    sil = [A.sb("sil%d" % i, [128, 512], BF16) for i in range(2)]
    tu = [A.sb("etu%d" % i, [128, 512]) for i in range(2)]
    xt = [A.sb("ext%d" % i, [128, 1024]) for i in range(2)]
    tm2 = A.sb("etm2", [128, 1024])
    ne = 0
    nst = 0
    nb = 0
    for G in MOE_GROUPS:
        tiles = [t for t in G if not (last and t < 2)]
        if not tiles:
            continue
        blocks = [tiles[i:i + 4] for i in range(0, len(tiles), 4)]
        g0 = tiles[0] * 128
        gn = len(tiles) * 128
        dma(P, "sp", hg[:, :, 0:gn], H2[:, :, g0:g0 + gn])
        for e in range(32):
            WGU, WD = wgu[ne % 2], wd[ne % 2]
            ne += 1
            for gi, nm in enumerate(("w_exp_gate", "w_exp_up")):
                src = I[nm][l, e].rr("(k p) n -> p k n", p=128)
                for hf_ in range(2):
                    s_ = wst[nst % 2]
                    dma(P, "sp" if nst % 2 == 0 else "pool", s_, src[:, hf_ * 4:(hf_ + 1) * 4, :])
                    cp(P, "pool", WGU[:, gi, hf_ * 4:(hf_ + 1) * 4, :], s_)
                    nst += 1
            srcd = I["w_exp_down"][l, e].rr("(k p) n -> p k n", p=128)
            for hf_ in range(2):
                s_ = wst[nst % 2]
                dma(P, "sp" if nst % 2 == 0 else "pool", s_.rr("p k n -> p (k n)").rr("p (k n) -> p k n", k=2), srcd[:, hf_ * 2:(hf_ + 1) * 2, :])
                cp(P, "pool", WD[:, hf_ * 2:(hf_ + 1) * 2, :], s_.rr("p k n -> p (k n)").rr("p (k n) -> p k n", k=2))
                nst += 1
            for blk in blocks:
                t0 = blk[0] * 128
                nn = len(blk) * 128
                HID = hid[nb % 2]
                psW = C.ps[0]
                mm(P, psW[:, 0:nn], C.ident[0:32, e:e + 1].bc([32, 128]), L.WT[:, t0:t0 + nn], True, True)
                for f in range(4):
                    i2 = (nb * 4 + f) % 2
                    psG, psU = C.ps[1 + 2 * i2], C.ps[2 + 2 * i2]
                    for k in range(8):
                        mm(P, psG[:, 0:nn], WGU[:, 0, k, f * 128:(f + 1) * 128], hg[:, k, t0 - g0:t0 - g0 + nn], k == 0, k == 7)
                    for k in range(8):
                        mm(P, psU[:, 0:nn], WGU[:, 1, k, f * 128:(f + 1) * 128], hg[:, k, t0 - g0:t0 - g0 + nn], k == 0, k == 7)
                    act(P, sil[i2][:, 0:nn], psG[:, 0:nn], AF.Silu)
                    tt(P, "dve", tu[i2][:, 0:nn], psU[:, 0:nn], sil[i2][:, 0:nn], ALU.mult)
                    tt(P, "dve", HID[:, f, 0:nn], tu[i2][:, 0:nn], psW[:, 0:nn], ALU.mult)
                for ti, t in enumerate(blk):
                    ya = yacc[:, t - tiles[0], :]
                    for half in range(2):
                        po = C.ps[5 + (nb * 8 + ti * 2 + half) % 3]
                        for f in range(4):
                            mm(P, po, HID[:, f, ti * 128:(ti + 1) * 128], WD[:, f, half * 512:(half + 1) * 512], f == 0, f == 3)
                        if e == 0:
                            cp(P, "dve", ya[:, half * 512:(half + 1) * 512], po)
                        else:
                            tt(P, "dve", ya[:, half * 512:(half + 1) * 512], ya[:, half * 512:(half + 1) * 512], po, ALU.add)
                nb += 1
        for t in tiles:
            j = 1 if t < 2 else 0
            x = xt[t % 2]
            dma(P, "sp", x, C.xres[t])
            tt(P, "dve", tm2, yacc[:, t - tiles[0], :], L.grow[1][j], ALU.mult)
            tt(P, "pool", x, x, tm2, ALU.add)
            dma(P, "pool", C.xres[t], x)
    A.close()


def final_norm(C):
    P, I = C.P, C.I
    A = Arena(P)
    g = A.sb("fg", [128, D])
    dma(P, "sp", g, I["final_g"][0:1, :].bc([128, D]))
    xt = [A.sb("fxt%d" % i, [128, D]) for i in range(2)]
    junk = A.sb("fjunk", [128, D])
    st = [A.sb("fst%d" % i, [128, 2]) for i in range(2)]
    for t in range(2, NT):
        x, s = xt[t % 2], st[t % 2]
        dma(P, "sp" if t % 2 == 0 else "pool", x, C.xres[t])
        act(P, junk, x, AF.Square, accum_out=s[:, 0:1])
        ts(P, "dve", s[:, 1:2], s[:, 0:1], 1.0 / D, ALU.mult, EPS, ALU.add)
        act(P, s[:, 1:2], s[:, 1:2], AF.Sqrt)
        P.op("dve", "reciprocal", out=s[:, 1:2], in_=s[:, 1:2])
        stt(P, "dve", x, x, s[:, 1:2], g, ALU.mult, ALU.mult)
        dma(P, "sp" if t % 2 == 1 else "pool", V(C.out.ap[(t - 2) * 128:(t - 1) * 128, :], Buf("o%d" % t)), x)
    A.close()


CH_COLS = 3072


def phase_chunk(C, l, L):
    P, I = C.P, C.I
    A = Arena(P)
    mk = A.sb("cmasks", [128, 7, 128])
    dma(P, "sp", mk, I["cmasks"])
    idn = C.ident
    chs = [A.sb("chs%d" % i, [128, CH_COLS]) for i in range(2)]
    ST = [[A.sb("cST%d%d" % (d, h), [64, 64]) for h in range(4)] for d in range(2)]
    for d in range(2):
        for h in range(4):
            P.op("dve", "memset", ap=ST[d][h], constant=0.0)

    def mk2(name, shape):
        return [A.sb("%s%d" % (name, i), shape) for i in range(2)]
    TOT, incS, Ein, Enin, Eex, Eend, Etot, tmpx, tmpy = [mk2("cE%d" % i, [128, 256]) for i in range(9)]
    at, rt, bt, kt, bh, kh = [mk2("cq%d" % i, [128, 256]) for i in range(6)]
    aT, rT, bT, kT = [mk2("cT%d" % i, [128, 2, 128]) for i in range(4)]
    bhc = [mk2("cbhc%d" % c, [128, 256]) for c in range(2)]
    khc = [mk2("ckhc%d" % c, [128, 256]) for c in range(2)]
    IM = A.sb("cIM", [128, 64])
    tt(P, "pool", IM, idn[:, 0:64], idn[:, 64:128], ALU.add)

    def mkh(name, shape):
        return [A.sb("%s%d" % (name, h), shape) for h in range(4)]
    X0, XT0, X1, XT1, AakT, ArbT, ArkT, TT = [mkh("cM%d" % i, [128, 128]) for i in range(8)]
    Ap, M1, U0 = [mkh("cP%d" % i, [128, 64]) for i in range(3)]
    RpT = mkh("cRpT", [64, 128])
    DPC = mkh("cDPC", [128, 64])
    Y0c = [mkh("cY0c%d" % c, [64, 64]) for c in range(2)]
    GT = [mkh("cGT%d" % c, [64, 64]) for c in range(2)]
    Hc = [mkh("cH%d" % c, [64, 64]) for c in range(2)]
    yo = [A.sb("cyo%d" % i, [64, 2, 256]) for i in range(2)]
    STg = [[A.sb("gST%d%d" % (d, h), [32, 64]) for h in range(4)] for d in range(2)]
    for d in range(2):
        for h in range(4):
            P.op("dve", "memset", ap=STg[d][h], constant=0.0)
    gTOT, gincS, gEin, gEnin, gEend, gEtot, gtmp = [mk2("gE%d" % i, [128, 128]) for i in range(7)]
    gq, gk, gkh = [mk2("gq%d" % i, [128, 128]) for i in range(3)]
    gkhc = [mk2("gkhc%d" % c, [128, 128]) for c in range(2)]
    gqT, gkT, gPT = [[mk2("gT%d_%d" % (i, h), [32, 128]) for h in range(4)] for i in range(3)]
    gA = mkh("gA", [128, 128])
    gY0 = [mkh("gY0%d" % c, [64, 64]) for c in range(2)]
    gH = [mkh("gH%d" % c, [32, 64]) for c in range(2)]
    gyo = [A.sb("gyo%d" % i, [64, 2, 256]) for i in range(2)]
    ps = C.ps
    border = [1, 0] + list(range(NT - 1, 1, -1))
    H4 = range(4)
    it = 0
    cut = C.dbg.get("chunk_cut", 99)
    for n in range(NT if cut > 50 else 1):
        for d in range(2):
            t = n if d == 0 else border[n]
            q = it % 2
            ch = chs[q]
            YO = yo[q]
            it += 1
            dma(P, "sp" if d == 0 else "pool", ch, V(C.CH_ap[t * 128:(t + 1) * 128, :], C.CH_bufs[t]))
            lw = ch[:, d * 256:(d + 1) * 256]
            ke = ch[:, 512 + d * 256:768 + d * 256]
            b_ = ch[:, 1024 + d * 256:1280 + d * 256]
            a_, r_, v_ = ch[:, 1536:1792], ch[:, 1792:2048], ch[:, 2048:2304]
            m_s, m_st, m_it = (0, 1, 2) if d == 0 else (3, 4, 5)
            pc = ps[4 + q]
            mm(P, pc[:, 0:256], mk[:, m_it, :], lw, True, True)
            mm(P, pc[:, 256:512], mk[:, 6, :], lw, True, True)
            cp(P, "dve", TOT[q], pc[:, 256:512])
            cp(P, "dve", incS[q], pc[:, 0:256])
            act(P, Ein[q], incS[q], AF.Exp)
            act(P, Enin[q], incS[q], AF.Exp, scale=-1.0)
            tt(P, "pool", tmpx[q], incS[q], lw, ALU.subtract)
            act(P, Eex[q], tmpx[q], AF.Exp)
            tt(P, "pool", tmpy[q], TOT[q], incS[q], ALU.subtract)
            act(P, Eend[q], tmpy[q], AF.Exp)
            act(P, Etot[q], TOT[q], AF.Exp)
            tt(P, "pool", at[q], a_, Eex[q], ALU.mult)
            tt(P, "pool", rt[q], r_, Ein[q], ALU.mult)
            tt(P, "pool", bt[q], b_, Enin[q], ALU.mult)
            tt(P, "pool", kt[q], ke, Enin[q], ALU.mult)
            tt(P, "pool", bh[q], b_, Eend[q], ALU.mult)
            tt(P, "pool", kh[q], ke, Eend[q], ALU.mult)
            for c in range(2):
                ts(P, "pool", bhc[c][q], bh[q], mk[:, 6, c * 64:c * 64 + 1], ALU.mult)
                ts(P, "pool", khc[c][q], kh[q], mk[:, 6, c * 64:c * 64 + 1], ALU.mult)
            for qi, (src, dst) in enumerate(((at, aT), (rt, rT), (bt, bT), (kt, kT))):
                pb_ = ps[6 + qi % 2]
                for a2 in range(2):
                    tr(P, pb_[:, a2 * 128:(a2 + 1) * 128], src[q][:, a2 * 128:(a2 + 1) * 128], idn)
                cp(P, "dve", dst[q], pb_[:, 0:256].rr("p (a n) -> p a n", a=2))

            def hv(h):
                pair, hl = h // 2, h % 2
                return pair, slice(hl * 64, (hl + 1) * 64), slice(h * 64, (h + 1) * 64)
            if cut <= 2:
                continue
            for h in H4:
                pair, hs, hc = hv(h)
                pA = ps[h]
                mm(P, pA[:, 0:128], aT[q][hs, pair, :], bT[q][hs, pair, :], True, True)
                mm(P, pA[:, 128:256], bT[q][hs, pair, :], aT[q][hs, pair, :], True, True)
                mm(P, pA[:, 256:384], kT[q][hs, pair, :], aT[q][hs, pair, :], True, True)
                mm(P, pA[:, 384:512], bT[q][hs, pair, :], rT[q][hs, pair, :], True, True)
            for h in H4:
                pA = ps[h]
                tt(P, "dve", X0[h], pA[:, 0:128], mk[:, m_s, :], ALU.mult)
                tt(P, "dve", XT0[h], pA[:, 128:256], mk[:, m_st, :], ALU.mult)
                tt(P, "dve", AakT[h], pA[:, 256:384], mk[:, m_st, :], ALU.mult)
                tt(P, "dve", ArbT[h], pA[:, 384:512], mk[:, m_it, :], ALU.mult)
                tt(P, "pool", TT[h], XT0[h], idn, ALU.add)
            for h in H4:
                pair, hs, hc = hv(h)
                mm(P, ps[h][:, 0:128], kT[q][hs, pair, :], rT[q][hs, pair, :], True, True)
            for h in H4:
                tt(P, "dve", ArkT[h], ps[h][:, 0:128], mk[:, m_it, :], ALU.mult)
            if cut <= 3:
                continue
            for s in range(5):
                Xc, XTc = (X0, XT0) if s % 2 == 0 else (X1, XT1)
                Xn, XTn = (X1, XT1) if s % 2 == 0 else (X0, XT0)
                for h in H4:
                    pX = ps[h]
                    mm(P, pX[:, 128:256], XTc[h], Xc[h], True, True)
                    if s < 4:
                        mm(P, pX[:, 256:384], Xc[h], XTc[h], True, True)
                for h in H4:
                    pX = ps[h]
                    cp(P, "dve", Xn[h], pX[:, 128:256])
                    if s < 4:
                        cp(P, "dve", XTn[h], pX[:, 256:384])
                for h in H4:
                    mm(P, ps[h][:, 384:512], Xn[h], TT[h], True, True)
                for h in H4:
                    tt(P, "dve", TT[h], TT[h], ps[h][:, 384:512], ALU.add)
            if cut <= 4:
                continue
            for h in H4:
                pair, hs, hc = hv(h)
                mm(P, ps[h][:, 0:64], TT[h], at[q][:, hc], True, True)
                mm(P, ps[h][:, 64:128], AakT[h], v_[:, hc], True, True)
            for h in H4:
                cp(P, "dve", Ap[h], ps[h][:, 0:64])
                cp(P, "dve", M1[h], ps[h][:, 64:128])
            for h in H4:
                mm(P, ps[h][:, 128:192], TT[h], M1[h], True, True)
            for h in H4:
                cp(P, "dve", U0[h], ps[h][:, 128:192])
            for h in H4:
                pair, hs, hc = hv(h)
                for c in range(2):
                    cs = slice(c * 64, (c + 1) * 64)
                    mm(P, ps[h][0:64, 192 + c * 64:256 + c * 64], ArbT[h][:, cs], U0[h], True, False)
                    mm(P, ps[h][0:64, 192 + c * 64:256 + c * 64], ArkT[h][:, cs], v_[:, hc], False, True)
                mm(P, ps[h][0:64, 320:448], Ap[h], ArbT[h], True, True)
                tt(P, "pool", DPC[h], IM, Etot[q][:, hc], ALU.mult)
            for h in H4:
                pair, hs, hc = hv(h)
                for c in range(2):
                    cp(P, "dve", Y0c[c][h], ps[h][0:64, 192 + c * 64:256 + c * 64])
                tt(P, "dve", RpT[h], ps[h][0:64, 320:448], rT[q][hs, pair, :], ALU.add)
            if cut <= 5:
                continue
            for h in H4:
                pair, hs, hc = hv(h)
                pG = ps[h]
                for c in range(2):
                    cs = slice(c * 64, (c + 1) * 64)
                    o = c * 128
                    mm(P, pG[0:64, o:o + 64], Ap[h], bhc[c][q][:, hc], True, False)
                    mm(P, pG[0:64, o:o + 64], idn[:, cs], DPC[h], False, True)
                    mm(P, pG[0:64, o + 64:o + 128], bhc[c][q][:, hc], U0[h], True, False)
                    mm(P, pG[0:64, o + 64:o + 128], khc[c][q][:, hc], v_[:, hc], False, True)
            for h in H4:
                pG = ps[h]
                for c in range(2):
                    o = c * 128
                    cp(P, "dve", GT[c][h], pG[0:64, o:o + 64])
                    cp(P, "dve", Hc[c][h], pG[0:64, o + 64:o + 128])
            if cut <= 6:
                continue
            for c in ((0, 1) if d == 0 else (1, 0)):
                cs = slice(c * 64, (c + 1) * 64)
                for h in H4:
                    pG = ps[h]
                    S_ = ST[d][h]
                    mm(P, pG[0:64, 256:320], RpT[h][:, cs], S_, True, False)
                    mm(P, pG[0:64, 256:320], idn[0:64, 0:64], Y0c[c][h], False, True)
                    mm(P, pG[0:64, 320:384], GT[c][h], S_, True, False)
                    mm(P, pG[0:64, 320:384], idn[0:64, 0:64], Hc[c][h], False, True)
                for h in H4:
                    pair, hs, hc = hv(h)
                    pG = ps[h]
                    cp(P, "dve", YO[:, c, hc], pG[0:64, 256:320])
                    cp(P, "dve", ST[d][h], pG[0:64, 320:384])
            dma(P, "sp" if d == 0 else "pool",
                V(C.YT_ap[d][t * 128:(t + 1) * 128, :].rearrange("(c p) n -> p c n", p=64), C.YT_bufs[d][t]), YO)
            GYO = gyo[q]
            glw = ch[:, 2304 + d * 128:2432 + d * 128]
            gq_, gk_, gv_ = ch[:, 2560:2688], ch[:, 2688:2816], ch[:, 2816:3072]
            pcg = ps[4 + q]
            mm(P, pcg[:, 0:128], mk[:, m_it, :], glw, True, True)
            mm(P, pcg[:, 128:256], mk[:, 6, :], glw, True, True)
            cp(P, "dve", gincS[q], pcg[:, 0:128])
            cp(P, "dve", gTOT[q], pcg[:, 128:256])
            act(P, gEin[q], gincS[q], AF.Exp)
            act(P, gEnin[q], gincS[q], AF.Exp, scale=-1.0)
            tt(P, "pool", gtmp[q], gTOT[q], gincS[q], ALU.subtract)
            act(P, gEend[q], gtmp[q], AF.Exp)
            act(P, gEtot[q], gTOT[q], AF.Exp)
            tt(P, "pool", gq[q], gq_, gEin[q], ALU.mult)
            tt(P, "pool", gk[q], gk_, gEnin[q], ALU.mult)
            tt(P, "pool", gkh[q], gk_, gEend[q], ALU.mult)
            for c in range(2):
                ts(P, "pool", gkhc[c][q], gkh[q], mk[:, 6, c * 64:c * 64 + 1], ALU.mult)
            for h in H4:
                pT_ = ps[6 + h % 2]
                g32 = slice(h * 32, (h + 1) * 32)
                tr(P, pT_[0:32, 0:128], gq[q][:, g32], idn)
                tr(P, pT_[0:32, 128:256], gk[q][:, g32], idn)
                tr(P, pT_[0:32, 256:384], gEtot[q][:, g32], idn)
                cp(P, "dve", gqT[h][q], pT_[0:32, 0:128])
                cp(P, "dve", gkT[h][q], pT_[0:32, 128:256])
                cp(P, "dve", gPT[h][q], pT_[0:32, 256:384])
            for h in H4:
                mm(P, ps[h][:, 0:128], gkT[h][q], gqT[h][q], True, True)
            for h in H4:
                tt(P, "dve", gA[h], ps[h][:, 0:128], mk[:, m_it, :], ALU.mult)
            for h in H4:
                hc = slice(h * 64, (h + 1) * 64)
                g32 = slice(h * 32, (h + 1) * 32)
                for c in range(2):
                    cs = slice(c * 64, (c + 1) * 64)
                    mm(P, ps[h][0:64, 128 + c * 64:192 + c * 64], gA[h][:, cs], gv_[:, hc], True, True)
                    mm(P, ps[h][0:32, 256 + c * 64:320 + c * 64], gkhc[c][q][:, g32], gv_[:, hc], True, True)
            for h in H4:
                for c in range(2):
                    cp(P, "dve", gY0[c][h], ps[h][0:64, 128 + c * 64:192 + c * 64])
                    cp(P, "dve", gH[c][h], ps[h][0:32, 256 + c * 64:320 + c * 64])
            for c in ((0, 1) if d == 0 else (1, 0)):
                cs = slice(c * 64, (c + 1) * 64)
                for h in H4:
                    S_ = STg[d][h]
                    mm(P, ps[h][0:64, 384:448], gqT[h][q][:, cs], S_, True, False)
                    mm(P, ps[h][0:64, 384:448], idn[0:64, 0:64], gY0[c][h], False, True)
                for h in H4:
                    hc = slice(h * 64, (h + 1) * 64)
                    S_ = STg[d][h]
                    cp(P, "dve", GYO[:, c, hc], ps[h][0:64, 384:448])
                    stt(P, "dve", S_, S_, gPT[h][q][:, c * 64:c * 64 + 1], gH[c][h], ALU.mult, ALU.add)
            dma(P, "sp" if d == 1 else "pool",
                V(C.YTG_ap[d][t * 128:(t + 1) * 128, :].rearrange("(c p) n -> p c n", p=64), C.YTG_bufs[d][t]), GYO)
    A.close()


class LayerState:
    pass


def layer(C, l):
    P = C.P
    LA = Arena(P)
    L = LayerState()
    L.mod = LA.sb("mod", [128, 48, 2])
    L.sc1 = LA.sb("sc1", [128, 8, 2])
    L.sc2 = LA.sb("sc2", [128, 8, 2])
    L.grow = [[LA.sb("grow%d%d" % (ii, j), [128, 1024]) for j in range(2)] for ii in range(2)]
    L.WT = LA.sb("WT", [32, NTOK])
    phase_ada(C, l, L)
    if C.dbg.get("dump") and l == C.dbg.get("layer", 0):
        dma(P, "sp", C.dout("d_mod", [128, 48, 2]), L.mod)
        for ii in range(2):
            for j in range(2):
                dma(P, "sp", C.dout("d_grow%d%d" % (ii, j), [128, 1024]), L.grow[ii][j])
    HA = Arena(P)
    hfm = HA.sb("hfm", [128, 8, NTOK], BF16)
    phase_norm(C, l, L, 1, hfm)
    if C.dbg.get("dump") and l == C.dbg.get("layer", 0):
        dma(P, "sp", C.dout("d_hfm", [128, 8, NTOK], BF16), hfm)
    phase_win_tm(C, l, L, hfm)
    if not C.dbg.get("skip_gates"):
        phase_win_gates(C, l, L, hfm)
    HA.close()
    if C.dbg.get("stop_after") == "win":
        LA.close(); return
    if not C.dbg.get("skip_ab"):
        phase_prep(C, l, L)
        if C.dbg.get("stop_after") == "prep":
            LA.close(); return
        if C.dbg.get("old_gla") and not C.dbg.get("skip_scan"):
            phase_scan(C, l, C.dbg.get("nchunks"))
        if not C.dbg.get("old_rwkv"):
            phase_chunk(C, l, L)
        if C.dbg.get("stop_after") == "chunk":
            LA.close(); return
        if C.dbg.get("stop_after") == "scan":
            LA.close(); return
        phase_fin_ab(C, l, L)
    if C.dbg.get("stop_after") == "fin":
        LA.close(); return
    if not C.dbg.get("skip_attn"):
        phase_attn(C, l, L)
    if C.dbg.get("stop_after") == "attn":
        LA.close(); return
    if not C.dbg.get("skip_s5"):
        phase_s5(C, l, L)
    if C.dbg.get("stop_after") == "s5":
        LA.close(); return
    phase_merge(C, l, L)
    if C.dbg.get("stop_after") == "merge":
        LA.close(); return
    HA = Arena(P)
    hfm2 = HA.sb("hfm2", [128, 8, NTOK], BF16)
    RA = Arena(P)
    phase_norm(C, l, L, 2, hfm2, per_tile=make_router(C, l, L, RA))
    RA.close()
    if C.dbg.get("dump") and l == C.dbg.get("layer", 0):
        dma(P, "sp", C.dout("d_hfm2", [128, 8, NTOK], BF16), hfm2)
        dma(P, "sp", C.dout("d_WT", [32, NTOK]), L.WT)
    H2 = V(C.H2_ap, C.H2_buf)
    dma(P, "sp", H2, hfm2)
    HA.close()
    if C.dbg.get("stop_after") == "norm2":
        LA.close(); return
    phase_moe(C, l, L, H2, l == DEPTH - 1)
    LA.close()


def make_maskw():
    m = np.zeros((128, 384), np.float32)
    i = np.arange(128)[:, None]
    j = np.arange(128)[None, :]
    m[:, 0:128] = np.where(j >= i, 0.0, -1e30)
    m[:, 256:384] = np.where(j <= i, 0.0, -1e30)
    return m


def make_rope():
    rows = SEQ // 64
    row = np.repeat(np.arange(rows, dtype=np.float32), 64)
    col = np.tile(np.arange(64, dtype=np.float32), rows)
    inv = (10000.0 ** (-np.arange(16, dtype=np.float32) / 16)).astype(np.float32)
    ang = np.concatenate([row[:, None] * inv, col[:, None] * inv], axis=-1).astype(np.float32)
    return np.cos(ang).astype(np.float32), np.sin(ang).astype(np.float32)


ROPE = make_rope()


def s5_host(A):
    f = np.float32
    lre, lim, ldt = A("s5_lam_re"), A("s5_lam_im"), A("s5_log_dt")
    ldt_e = np.repeat(ldt[..., None], 64, axis=-1)
    rows = np.stack([lre.reshape(DEPTH, 2, 1024), lim.reshape(DEPTH, 2, 1024), ldt_e.reshape(DEPTH, 2, 1024)], axis=2)
    sm = rows.reshape(DEPTH, 2, 3, 8, 128).transpose(0, 4, 1, 2, 3)
    bre, bim = A("s5_b_re"), A("s5_b_im")
    bt = np.zeros((DEPTH, 2, 8, 128, 128), f)
    cre, cim = A("s5_c_re"), A("s5_c_im")
    ct = np.zeros((DEPTH, 2, 2, 8, 128, 128), f)
    for g in range(16):
        j, hh = g // 2, g % 2
        c0 = (g % 8) * 16
        for ri, b in enumerate((bre, bim)):
            bt[:, ri, j, c0:c0 + 16, hh * 64:(hh + 1) * 64] = b[:, g].transpose(0, 2, 1)
        for ri, c in enumerate((cre, cim)):
            ct[:, :, ri, j, hh * 64:(hh + 1) * 64, c0:c0 + 16] = c[:, :, g].transpose(0, 1, 3, 2)
    return {
        "s5_sm": np.ascontiguousarray(sm, f), "s5_rows": np.ascontiguousarray(rows, f),
        "s5_bt": bt, "s5_ct": ct,
        "pw2": (2.0 ** np.arange(16)).astype(f).reshape(1, 16),
        "s5_d_fm": np.ascontiguousarray(np.pad(A("s5_d").reshape(DEPTH, 2, 128).transpose(0, 2, 1), ((0, 0), (0, 0), (0, 14)))),
        "s5_bglu_fm": np.ascontiguousarray(np.pad(A("s5_b_glu").reshape(DEPTH, 2, 128).transpose(0, 2, 1), ((0, 0), (0, 0), (0, 14)))),
        "s5_w_glu": A("s5_w_glu"),
    }


def make_sele():
    s = np.zeros((32, 32, 128), np.float32)
    for e in range(32):
        s[e, e, :] = 1.0
    return s


def make_cmasks():
    r = np.arange(128)[:, None]
    c = np.arange(128)[None, :]
    same = (r // 64) == (c // 64)
    m = np.zeros((128, 7, 128), np.float32)
    m[:, 0] = same & (c < r)
    m[:, 1] = same & (r < c)
    m[:, 2] = same & (r <= c)
    m[:, 3] = same & (c > r)
    m[:, 4] = same & (r > c)
    m[:, 5] = same & (r >= c)
    m[:, 6] = same
    return m


def make_sel():
    s = np.zeros((128, 64, 128), np.float32)
    for j in range(64):
        for hh in range(2):
            s[2 * j + hh, j, hh * 64:(hh + 1) * 64] = 1.0
    return s


def blkdiag(mats):
    n = len(mats)
    L, r, c = mats[0].shape
    o = np.zeros((L, n * r, n * c), np.float32)
    for i, m in enumerate(mats):
        o[:, i * r:(i + 1) * r, i * c:(i + 1) * c] = m
    return o


def host_inputs(inputs, b):
    f = np.float32

    def A(k):
        return np.asarray(inputs[k], f)
    c = np.asarray(inputs["c"], f)[b]
    cctx = np.asarray(inputs["c_ctx"], f)
    cc = np.stack([c.reshape(8, 128).T, cctx.reshape(8, 128).T], axis=-1)
    m = {
        "xb": np.ascontiguousarray(np.asarray(inputs["x"], f)[b]),
        "ctxb": np.ascontiguousarray(np.asarray(inputs["ctx"], f)[b]),
        "cc": np.ascontiguousarray(cc),
        "w_ada": np.asarray(inputs["w_ada"], f),
        "b_ada": np.asarray(inputs["b_ada"], f),
        "b_ada_fm": np.ascontiguousarray(np.asarray(inputs["b_ada"], f).reshape(DEPTH, 48, 128).transpose(0, 2, 1)),
        "g1_fm": np.ascontiguousarray(np.asarray(inputs["norm1_g"], f).reshape(DEPTH, 8, 128).transpose(0, 2, 1)),
        "g2_fm": np.ascontiguousarray(np.asarray(inputs["norm2_g"], f).reshape(DEPTH, 8, 128).transpose(0, 2, 1)),
        "w_in": np.asarray(inputs["w_in"], f),
        "ident": np.eye(128, dtype=f),
        "sel": make_sel(),
        "maskw": make_maskw(),
        "ropec": ROPE[0],
        "ropes": ROPE[1],
        "attn_sink": np.ascontiguousarray(np.pad(A("attn_sink"), ((0, 0), (0, 12)))),
        **s5_host(A),
        "cmasks": make_cmasks(),
        "w_branch": A("w_branch"),
        "w_out": A("w_out"),
        "w_router": np.ascontiguousarray(np.concatenate([A("w_router_g"), A("w_router_e")], axis=2)),
        "b_router": np.ascontiguousarray(np.concatenate([A("b_router_g"), A("b_router_e")], axis=1)),
        "w_exp_gate": A("w_exp_gate"),
        "w_exp_up": A("w_exp_up"),
        "w_exp_down": A("w_exp_down"),
        "pv": np.ascontiguousarray(np.concatenate([
            A("rwkv_mu").reshape(DEPTH, -1), A("rwkv_kk"), A("rwkv_ka"), A("rwkv_rk").reshape(DEPTH, -1),
            A("rwkv_w0").reshape(DEPTH, -1), A("rwkv_a0").reshape(DEPTH, -1), A("rwkv_ln_g"),
            A("gla_ab").reshape(DEPTH, -1), A("gla_ln_g")], axis=1)),
        "w1cat": np.ascontiguousarray(np.concatenate([A("rwkv_w1")[:, 0], A("rwkv_w1")[:, 1],
                                                      A("rwkv_a1")[:, 0], A("rwkv_a1")[:, 1]], axis=2)),
        "w2blk": blkdiag([A("rwkv_w2")[:, 0], A("rwkv_w2")[:, 1], A("rwkv_a2")[:, 0], A("rwkv_a2")[:, 1]]),
        "g1": A("rwkv_g1"),
        "g2": A("rwkv_g2"),
        "a2blk": blkdiag([A("gla_a2")[:, 0], A("gla_a2")[:, 1]]),
        "final_g": np.asarray(inputs["final_norm_g"], f).reshape(1, D),
    }
    return m


def kernel(**inputs):
    nc = build_program()
    in_maps = [host_inputs(inputs, b) for b in range(8)]
    res = run_bass_kernel_spmd(nc, in_maps, core_ids=list(range(8)))
    return np.stack([r["out"] for r in res.results], axis=0)
```

```python
import math
from contextlib import ExitStack

import numpy as np
import concourse.bass as bass
import concourse.mybir as mybir
from concourse.bass_utils import run_bass_kernel_spmd

F32 = mybir.dt.float32
BF16 = mybir.dt.bfloat16
ALU = mybir.AluOpType
AF = mybir.ActivationFunctionType
AX = mybir.AxisListType

D = 1024
SEQ = 4096
CTX = 256
NT = (SEQ + CTX) // 128
NTOK = SEQ + CTX
DEPTH = 2
EPS = 1e-6
GN_EPS = 64e-5
O1, O2, O3, O4, PIN = 1024, 1824, 2336, 2592, 6688
PV_MU, PV_KK, PV_KA, PV_RK, PV_W0, PV_A0, PV_LNG, PV_GAB, PV_GLNG = 0, 1024, 1280, 1536, 1792, 2304, 2816, 3072, 3328
NPV = 3584

DEBUG = False
PENDING = "PENDING"
WKEYS = ("out", "accum_out", "ap")
SKEYS = ("scalar1", "scalar2", "scale", "bias", "scalar")


class Buf:
    __slots__ = ("name", "w", "rd", "ws")

    def __init__(self, name=""):
        self.name = name
        self.w = None
        self.rd = {}
        self.ws = False


class V:
    __slots__ = ("ap", "bufs")

    def __init__(self, ap, bufs):
        self.ap = ap
        self.bufs = bufs if isinstance(bufs, tuple) else (bufs,)

    def __getitem__(self, k):
        return V(self.ap[k], self.bufs)

    def rr(self, pat, **kw):
        return V(self.ap.rearrange(pat, **kw), self.bufs)

    def bc(self, shape):
        return V(self.ap.to_broadcast(list(shape)), self.bufs)

    def bitcast(self, dt):
        return V(self.ap.bitcast(dt), self.bufs)

    def wb(self, *bufs):
        return V(self.ap, tuple(bufs))

    @property
    def shape(self):
        return tuple(self.ap.shape)


class Prog:
    ENG = ("pe", "act", "dve", "pool", "sp")
    K = 6
    STRICT_ALL = False

    def __init__(self, nc):
        self.nc = nc
        self.eng = {"pe": nc.tensor, "act": nc.scalar, "dve": nc.vector, "pool": nc.gpsimd, "sp": nc.sync}
        self.sem = {e: nc.alloc_semaphore("s_" + e) for e in self.ENG}
        self.cnt = {e: 0 for e in self.ENG}
        self.dsem = {q: [nc.alloc_semaphore("d_%s%d" % (q, i)) for i in range(self.K)] for q in ("sp", "act", "pool")}
        self.dcnt = {q: 0 for q in self.dsem}
        self.known = {e: {} for e in self.ENG}
        self.pend_r = []
        self.pend_w = []
        self.uid = 0
        self.nins = 0

    def _need(self, e, tok, is_dma, strict=False):
        if tok is None:
            return
        if tok is PENDING:
            assert e == "pe" and not is_dma, "dependency on an unmarked PE op"
            return
        sem, val, owner = tok
        if owner == e and not is_dma and not (strict and e != "pe") and not self.STRICT_ALL:
            return
        k = self.known[e]
        if k.get(sem.num, 0) >= val:
            return
        self.eng[e].wait_ge(sem, val)
        self.nins += 1
        k[sem.num] = val

    def op(self, e, meth, mark=True, lax=False, **kw):
        reads, writes, args, sreads, awrites = [], [], {}, [], []
        for k, v in kw.items():
            if isinstance(v, V):
                (writes if k in WKEYS else reads).extend(v.bufs)
                if k in SKEYS:
                    sreads.extend(v.bufs)
                if k == "accum_out":
                    awrites.extend(v.bufs)
                args[k] = v.ap
            else:
                args[k] = v
        is_dma = meth == "dma_start"
        for b in reads:
            self._need(e, b.w, is_dma, strict=(not lax) or b.ws or e == "act" or b in sreads)
        for b in writes:
            self._need(e, b.w, is_dma)
            for t in b.rd.values():
                self._need(e, t, is_dma)
        if is_dma:
            n = self.dcnt[e]
            sem = self.dsem[e][n % self.K]
            r = n // self.K
            if r > 0:
                self._need(e, (sem, 16 * r, None), True)
            ins = getattr(self.eng[e], meth)(**args)
            ins.then_inc(sem, 16)
            self.dcnt[e] = n + 1
            tok = (sem, 16 * (r + 1), None)
            key = ("d", sem.num)
        else:
            ins = getattr(self.eng[e], meth)(**args)
            key = e
            if mark:
                self.cnt[e] += 1
                ins.then_inc(self.sem[e], 1)
                tok = (self.sem[e], self.cnt[e], e)
                if e == "pe" and (self.pend_r or self.pend_w):
                    for b in self.pend_r:
                        if b.rd.get("pe") is PENDING:
                            b.rd["pe"] = tok
                    for b in self.pend_w:
                        if b.w is PENDING:
                            b.w = tok
                    self.pend_r = []
                    self.pend_w = []
            else:
                assert e == "pe"
                tok = PENDING
                self.pend_r.extend(reads)
                self.pend_w.extend(writes)
        self.nins += 1
        for b in reads:
            b.rd[key] = tok
        for b in writes:
            b.w = tok
            b.rd = {}
            b.ws = (b in awrites) or e == "act"
        return ins

    def barrier(self):
        assert not self.pend_r and not self.pend_w
        toks = [(self.sem[e], self.cnt[e], e) for e in self.ENG if self.cnt[e] > 0]
        for q in self.dsem:
            n = self.dcnt[q]
            for i in range(self.K):
                c = (n - i + self.K - 1) // self.K if n > i else 0
                if c > 0:
                    toks.append((self.dsem[q][i], 16 * c, None))
        for e in self.ENG:
            for t in toks:
                self._need(e, t, False)

    def name(self, s):
        self.uid += 1
        return "%s_%d" % (s, self.uid)

    def dram(self, name, shape, dt, kind="Internal"):
        return self.nc.dram_tensor(name, list(shape), dt, kind=kind).ap()


class Arena:
    def __init__(self, P):
        self.P = P
        self.stack = ExitStack()

    def sb(self, name, shape, dt=F32):
        h = self.stack.enter_context(self.P.nc.sbuf_tensor(self.P.name(name), list(shape), dt))
        return V(h.ap(), Buf(name))

    def close(self):
        self.P.barrier()
        self.stack.close()


def dma(P, q, out, in_):
    return P.op(q, "dma_start", out=out, in_=in_)


def mm(P, out, lhsT, rhs, start, stop, mark=None):
    return P.op("pe", "matmul", mark=(stop if mark is None else mark), out=out, lhsT=lhsT, rhs=rhs,
                start=start, stop=stop)


def tr(P, out, in_, ident, mark=True):
    return P.op("pe", "transpose", mark=mark, out=out, in_=in_, identity=ident)


def tt(P, e, out, in0, in1, op):
    return P.op(e, "tensor_tensor", out=out, in0=in0, in1=in1, op=op)


def ts(P, e, out, in0, s1, op0, s2=None, op1=None, **kw):
    if op1 is None:
        return P.op(e, "tensor_scalar", out=out, in0=in0, scalar1=s1, scalar2=None, op0=op0, **kw)
    return P.op(e, "tensor_scalar", out=out, in0=in0, scalar1=s1, scalar2=s2, op0=op0, op1=op1, **kw)


def act(P, out, in_, func, **kw):
    return P.op("act", "activation", out=out, in_=in_, func=func, **kw)


def cp(P, e, out, in_):
    if e == "act":
        return act(P, out, in_, AF.Copy)
    return P.op(e, "tensor_copy", out=out, in_=in_)


class Ctx:
    pass


def build_program(dbg=None):
    dbg = dbg or {}
    nc = bass.Bass("TRN2", target_bir_lowering=False)
    P = Prog(nc)
    C = Ctx()
    C.P, C.nc, C.dbg = P, nc, dbg
    skind = "ExternalOutput" if dbg.get("expose") else "Internal"

    def din(name, shape, dt=F32):
        return V(nc.dram_tensor(name, list(shape), dt, kind="ExternalInput").ap(), Buf(name))

    I = {}
    I["xb"] = din("xb", [SEQ, D])
    I["ctxb"] = din("ctxb", [CTX, D])
    I["cc"] = din("cc", [128, 8, 2])
    I["w_ada"] = din("w_ada", [DEPTH, D, 6 * D])
    I["b_ada"] = din("b_ada", [DEPTH, 6 * D])
    I["b_ada_fm"] = din("b_ada_fm", [DEPTH, 128, 48])
    I["g1_fm"] = din("g1_fm", [DEPTH, 128, 8])
    I["g2_fm"] = din("g2_fm", [DEPTH, 128, 8])
    I["w_in"] = din("w_in", [DEPTH, D, PIN])
    I["ident"] = din("ident", [128, 128])
    I["sel"] = din("sel", [128, 64, 128])
    I["maskw"] = din("maskw", [128, 384])
    I["ropec"] = din("ropec", [SEQ, 32])
    I["ropes"] = din("ropes", [SEQ, 32])
    I["attn_sink"] = din("attn_sink", [DEPTH, 16])
    I["s5_sm"] = din("s5_sm", [DEPTH, 128, 2, 3, 8])
    I["s5_rows"] = din("s5_rows", [DEPTH, 2, 3, 1024])
    I["s5_bt"] = din("s5_bt", [DEPTH, 2, 8, 128, 128])
    I["s5_ct"] = din("s5_ct", [DEPTH, 2, 2, 8, 128, 128])
    I["pw2"] = din("pw2", [1, 16])
    I["cmasks"] = din("cmasks", [128, 7, 128])
    I["w_branch"] = din("w_branch", [DEPTH, 4, 256, D])
    I["w_out"] = din("w_out", [DEPTH, D, D])
    I["w_router"] = din("w_router", [DEPTH, D, 36])
    I["b_router"] = din("b_router", [DEPTH, 36])
    I["w_exp_gate"] = din("w_exp_gate", [DEPTH, 32, D, 512])
    I["w_exp_up"] = din("w_exp_up", [DEPTH, 32, D, 512])
    I["w_exp_down"] = din("w_exp_down", [DEPTH, 32, 512, D])
    I["s5_d_fm"] = din("s5_d_fm", [DEPTH, 128, 16])
    I["s5_bglu_fm"] = din("s5_bglu_fm", [DEPTH, 128, 16])
    I["s5_w_glu"] = din("s5_w_glu", [DEPTH, 256, 256])
    I["pv"] = din("pv", [DEPTH, NPV])
    I["w1cat"] = din("w1cat", [DEPTH, 256, 128])
    I["w2blk"] = din("w2blk", [DEPTH, 128, 1024])
    I["g1"] = din("g1", [DEPTH, 256, 64])
    I["g2"] = din("g2", [DEPTH, 64, 256])
    I["a2blk"] = din("a2blk", [DEPTH, 32, 256])
    I["final_g"] = din("final_g", [1, D])
    C.I = I

    out = V(nc.dram_tensor("out", [SEQ, D], F32, kind="ExternalOutput").ap(), Buf("out"))
    C.out = out

    def dout(name, shape, dt=F32):
        return V(nc.dram_tensor(name, list(shape), dt, kind="ExternalOutput").ap(), Buf(name))
    C.dout = dout

    xres_ap = P.dram("xres", [NTOK, D], F32, kind=skind)
    C.xres = [V(xres_ap[t * 128:(t + 1) * 128, :], Buf("xres%d" % t)) for t in range(NT)]
    C.U_ap = P.dram("U", [NTOK + 3, O4], F32, kind=skind)
    C.U_bufs = [Buf("U%d" % t) for t in range(NT)]
    C.U_pad = Buf("Upad")
    C.STR_ap = [P.dram("STR%d" % d, [NTOK, 2, 1024], BF16, kind=skind) for d in range(2)]
    C.STR_bufs = [[Buf("STR%d_%d" % (d, t)) for t in range(NT)] for d in range(2)]
    C.Vs_ap = P.dram("Vs", [128, NTOK, 6], F32, kind=skind)
    C.Vs_bufs = [Buf("Vs%d" % t) for t in range(NT)]
    C.Y_ap = [P.dram("Y%d" % d, [128, NTOK, 6], F32, kind=skind) for d in range(2)]
    C.Y_bufs = [[Buf("Y%d_%d" % (d, c)) for c in range(NTOK // 64)] for d in range(2)]
    C.FIN_ap = P.dram("FIN", [NTOK, 768], F32, kind=skind)
    C.FIN_bufs = [Buf("FIN%d" % t) for t in range(NT)]
    C.YB_ap = P.dram("YB", [4, 256, NTOK], BF16, kind=skind)
    C.YB_bufs = [[Buf("YB%d_%d" % (i, t)) for t in range(NT)] for i in range(4)]
    C.CH_ap = P.dram("CH", [NTOK, 3072], F32, kind=skind)
    C.CH_bufs = [Buf("CH%d" % t) for t in range(NT)]
    C.YT_ap = [P.dram("YT%d" % d, [NTOK, 256], F32, kind=skind) for d in range(2)]
    C.YT_bufs = [[Buf("YT%d_%d" % (d, t)) for t in range(NT)] for d in range(2)]
    C.YTG_ap = [P.dram("YTG%d" % d, [NTOK, 256], F32, kind=skind) for d in range(2)]
    C.YTG_bufs = [[Buf("YTG%d_%d" % (d, t)) for t in range(NT)] for d in range(2)]
    C.Gt_ap = P.dram("Gt", [4096, NTOK], BF16, kind=skind)
    C.Gt_bufs = [Buf("Gt%d" % b) for b in range(9)]
    C.H2_ap = P.dram("H2", [128, 8, NTOK], BF16, kind=skind)
    C.H2_buf = Buf("H2")

    G = Arena(P)
    C.G = G
    C.psall = nc.alloc_psum_tensor("psall", [128, 4096], F32).ap()
    C.psb = [Buf("ps%d" % i) for i in range(8)]
    C.ps = [V(C.psall[:, i * 512:(i + 1) * 512], C.psb[i]) for i in range(8)]
    C.ident = G.sb("ident", [128, 128])
    dma(P, "sp", C.ident, I["ident"])

    for l in range(DEPTH):
        layer(C, l)
        if dbg.get("stop_layer") == l:
            break
    if not dbg.get("stop"):
        final_norm(C)
    P.barrier()
    return nc


def urow(t):
    return 1 + t * 128 if t < 2 else 258 + (t - 2) * 128


def xsrc(C, l, t):
    if l == 0 and not C.__dict__.get("x_in_scratch"):
        if t < 2:
            return C.I["ctxb"][t * 128:(t + 1) * 128, :]
        return C.I["xb"][(t - 2) * 128:(t - 1) * 128, :]
    return C.xres[t]


def phase_ada(C, l, L):
    P, I = C.P, C.I
    A = Arena(P)
    cc = A.sb("cc", [128, 8, 2])
    sc = A.sb("sc", [128, 8, 2])
    screp = A.sb("screp", [128, 8, 2, 128])
    bfm = A.sb("bfm", [128, 48])
    g1 = A.sb("g1", [128, 8])
    g2 = A.sb("g2", [128, 8])
    wst = [A.sb("wst%d" % i, [128, 8, 512]) for i in range(2)]
    brow = [A.sb("brow%d" % i, [128, 1024]) for i in range(2)]
    dma(P, "sp", cc, I["cc"])
    dma(P, "sp", bfm, I["b_ada_fm"][l])
    dma(P, "sp", g1, I["g1_fm"][l])
    dma(P, "sp", g2, I["g2_fm"][l])
    for ii, i in enumerate((2, 5)):
        dma(P, "pool", brow[ii], I["b_ada"][l:l + 1, i * 1024:(i + 1) * 1024].bc([128, 1024]))
    act(P, sc, cc, AF.Silu)
    for k in range(8):
        for j in range(2):
            cp(P, "dve", screp[:, k, j, :], sc[:, k, j:j + 1].bc([128, 128]))
    wv = I["w_ada"][l].rr("(k p) n -> p k n", p=128)
    psA = C.ps[0]
    for c in range(12):
        w = wst[c % 2]
        dma(P, "sp" if c % 2 == 0 else "pool", w, wv[:, :, c * 512:(c + 1) * 512])
        for mi in range(4):
            m = c * 4 + mi
            for k in range(8):
                mm(P, psA[:, m * 2:(m + 1) * 2], w[:, k, mi * 128:(mi + 1) * 128], sc[:, k, :], k == 0, k == 7)
        if c in (4, 5, 10, 11):
            ii = 0 if c < 6 else 1
            half = c % 2
            for j in range(2):
                pr = C.ps[1 + j]
                for k in range(8):
                    mm(P, pr, screp[:, k, j, :], w[:, k, :], k == 0, k == 7)
                tt(P, "dve", L.grow[ii][j][:, half * 512:(half + 1) * 512], pr,
                   brow[ii][:, half * 512:(half + 1) * 512], ALU.add)
    tt(P, "dve", L.mod, psA[:, 0:96].rr("p (m j) -> p m j", j=2), bfm[:, :, None].bc([128, 48, 2]), ALU.add)
    ts(P, "dve", L.sc1, L.mod[:, 8:16, :], 1.0, ALU.add)
    tt(P, "dve", L.sc1, L.sc1, g1[:, :, None].bc([128, 8, 2]), ALU.mult)
    ts(P, "dve", L.sc2, L.mod[:, 32:40, :], 1.0, ALU.add)
    tt(P, "dve", L.sc2, L.sc2, g2[:, :, None].bc([128, 8, 2]), ALU.mult)
    A.close()


def phase_norm(C, l, L, which, hfm, per_tile=None):
    P = C.P
    A = Arena(P)
    sc = L.sc1 if which == 1 else L.sc2
    shb = 0 if which == 1 else 24
    xt = [A.sb("xt%d" % i, [128, D]) for i in range(2)]
    junk = A.sb("junk", [128, D])
    st = [A.sb("st%d" % i, [128, 2]) for i in range(2)]
    hf = [A.sb("hf%d" % i, [128, 8, 128]) for i in range(2)] if per_tile else None
    for t in range(NT):
        j = 1 if t < 2 else 0
        x = xt[t % 2]
        s = st[t % 2]
        dma(P, "sp" if t % 2 == 0 else "pool", x, xsrc(C, l, t))
        act(P, junk, x, AF.Square, accum_out=s[:, 0:1])
        ts(P, "dve", s[:, 1:2], s[:, 0:1], 1.0 / D, ALU.mult, EPS, ALU.add)
        act(P, s[:, 1:2], s[:, 1:2], AF.Sqrt)
        P.op("dve", "reciprocal", out=s[:, 1:2], in_=s[:, 1:2])
        ts(P, "dve", x, x, s[:, 1:2], ALU.mult)
        pa, pb = C.ps[2 + 2 * (t % 2)], C.ps[3 + 2 * (t % 2)]
        if C.dbg.get("dump_norm") and t == 2 and which == 1:
            dma(P, "sp", C.dout("d_xn", [128, D]), x)
            dma(P, "sp", C.dout("d_st", [128, 2]), s)
        for k in range(8):
            pp = pa if k < 4 else pb
            tr(P, pp[:, (k % 4) * 128:(k % 4 + 1) * 128], x[:, k * 128:(k + 1) * 128], C.ident)
        if C.dbg.get("dump_norm") and t == 2 and which == 1:
            cp(P, "dve", junk[:, 0:512], pa)
            dma(P, "sp", C.dout("d_pa", [128, 512]), junk[:, 0:512])
        for k in range(8):
            pp = pa if k < 4 else pb
            src = pp[:, (k % 4) * 128:(k % 4 + 1) * 128]
            if per_tile:
                dst = hf[t % 2][:, k, :]
            else:
                dst = hfm[:, k, t * 128:(t + 1) * 128]
            if k % 2 == 0:
                act(P, dst, src, AF.Identity, scale=sc[:, k, j:j + 1], bias=L.mod[:, shb + k, j:j + 1])
            else:
                ts(P, "dve", dst, src, sc[:, k, j:j + 1], ALU.mult, L.mod[:, shb + k, j:j + 1], ALU.add)
        if per_tile:
            for k in range(8):
                cp(P, "act" if k % 2 == 0 else "dve", hfm[:, k, t * 128:(t + 1) * 128], hf[t % 2][:, k, :])
            per_tile(t, hf[t % 2])
    A.close()


def phase_win_tm(C, l, L, hfm):
    P, I = C.P, C.I
    A = Arena(P)
    wA = A.sb("wA", [128, 8, O4], BF16)
    wst = [A.sb("wst%d" % i, [128, 8, 512]) for i in range(2)]
    ust = [A.sb("ust%d" % i, [128, O4]) for i in range(2)]
    zer = A.sb("zer", [1, O4])
    P.op("dve", "memset", ap=zer, constant=0.0)
    for r in (0, 257, NTOK + 2):
        dma(P, "sp", V(C.U_ap[r:r + 1, :], C.U_pad), zer)
    wv = I["w_in"][l].rr("(k p) n -> p k n", p=128)
    blocks = [(0, 512), (512, 1024), (1024, 1536), (1536, 1824), (1824, 2336), (2336, 2592)]
    for bi, (c0, c1) in enumerate(blocks):
        w = wst[bi % 2]
        dma(P, "sp" if bi % 2 == 0 else "pool", w[:, :, 0:c1 - c0], wv[:, :, c0:c1])
        cp(P, "act", wA[:, :, c0:c1], w[:, :, 0:c1 - c0])
    n = 0
    for t in range(NT):
        u = ust[t % 2]
        for bi, (c0, c1) in enumerate(blocks):
            ps = C.ps[n % 4]
            n += 1
            for k in range(8):
                mm(P, ps[:, 0:c1 - c0], hfm[:, k, t * 128:(t + 1) * 128], wA[:, k, c0:c1], k == 0, k == 7)
            cp(P, "act" if bi % 2 == 0 else "dve", u[:, c0:c1], ps[:, 0:c1 - c0])
        r0 = urow(t)
        dma(P, "sp" if t % 2 == 0 else "pool", V(C.U_ap[r0:r0 + 128, :], C.U_bufs[t]), u)
    A.close()


def stt(P, e, out, in0, scalar, in1, op0, op1, **kw):
    return P.op(e, "scalar_tensor_tensor", out=out, in0=in0, scalar=scalar, in1=in1, op0=op0, op1=op1, **kw)


def red(P, e, out, in_, **kw):
    return P.op(e, "tensor_reduce", out=out, in_=in_, axis=AX.X, op=ALU.add, **kw)


def load_bf16(P, A, name, shape, src, q="sp", ce="act"):
    st = A.sb(name + "_f", shape)
    wb = A.sb(name, shape, BF16)
    dma(P, q, st, src)
    cp(P, ce, wb, st)
    return wb


def cust(v, offset_elems, dims):
    ap = v.ap
    base = ap.ap[0]
    new = type(ap)(ap.tensor, ap.offset + offset_elems, [tuple(base)] + [tuple(d) for d in dims])
    return V(new, v.bufs)


def phase_prep(C, l, L):
    P, I = C.P, C.I
    A = Arena(P)
    pv = A.sb("pv", [128, NPV])
    dma(P, "sp", pv, I["pv"][l:l + 1, :].bc([128, NPV]))
    w1cat = load_bf16(P, A, "w1cat", [128, 2, 128], I["w1cat"][l].rr("(k p) n -> p k n", p=128))
    w2blk = load_bf16(P, A, "w2blk", [128, 1024], I["w2blk"][l])
    g1 = load_bf16(P, A, "g1w", [128, 2, 64], I["g1"][l].rr("(k p) n -> p k n", p=128))
    g2 = load_bf16(P, A, "g2w", [64, 256], I["g2"][l])
    a2blk = load_bf16(P, A, "a2blk", [32, 256], I["a2blk"][l])
    mu = pv[:, PV_MU:PV_MU + 1024]
    kkp = pv[:, PV_KK:PV_KK + 256]
    ka = pv[:, PV_KA:PV_KA + 256]
    rkp = pv[:, PV_RK:PV_RK + 256]
    w0 = pv[:, PV_W0:PV_W0 + 512]
    a0 = pv[:, PV_A0:PV_A0 + 512]
    gab = pv[:, PV_GAB:PV_GAB + 256]
    glng = pv[:, PV_GLNG:PV_GLNG + 256]

    uc = [A.sb("uc%d" % i, [128, O2]) for i in range(2)]
    up = [A.sb("up%d" % i, [128, 1024]) for i in range(2)]
    un = [A.sb("un%d" % i, [128, 1024]) for i in range(2)]
    rows = [[A.sb("rows%d%d" % (d, i), [128, 2, 1024], BF16) for i in range(2)] for d in range(2)]
    vt = [A.sb("vt%d" % i, [128, 128, 6]) for i in range(2)]
    fin = [A.sb("fin%d" % i, [128, 768]) for i in range(2)]
    cht = [A.sb("cht%d" % i, [128, 3072]) for i in range(2)]
    t0 = A.sb("t0", [128, 1024])
    mx = A.sb("mx", [128, 1024])
    xaT = A.sb("xaT", [128, 2, 128], BF16)
    z = A.sb("z", [128, 128], BF16)
    sg = A.sb("sg", [64, 128], BF16)
    wl = A.sb("wl", [128, 512])
    wdec = A.sb("wdec", [128, 512])
    il = A.sb("il", [128, 512])
    iclr = A.sb("iclr", [128, 512])
    kk0 = A.sb("kk0", [128, 256])
    sq = A.sb("sq", [128, 256])
    ss = A.sb("ss", [128, 8])
    kk = A.sb("kk", [128, 256])
    t1 = A.sb("t1", [128, 512])
    keff = A.sb("keff", [128, 512])
    bb = A.sb("bb", [128, 512])
    rkt = A.sb("rkt", [128, 256])
    alT = A.sb("alT", [32, 128], BF16)
    gl = A.sb("gl", [128, 256])
    gdec = A.sb("gdec", [128, 256])
    sr = A.sb("sr", [128, 256])

    def rv(R, c0, n):
        return R[:, :, c0:c0 + n]

    for t in range(NT):
        i = t % 2
        r0 = urow(t)
        nb = [C.U_bufs[t]]
        if t > 0:
            nb.append(C.U_bufs[t - 1])
        if t < NT - 1:
            nb.append(C.U_bufs[t + 1])
        nb.append(C.U_pad)
        dma(P, "sp", uc[i], V(C.U_ap[r0:r0 + 128, 0:O2], C.U_bufs[t]))
        dma(P, "pool", up[i], V(C.U_ap[r0 - 1:r0 + 127, 0:1024], tuple(nb)))
        dma(P, "sp", un[i], V(C.U_ap[r0 + 1:r0 + 129, 0:1024], tuple(nb)))
        u = uc[i]
        R0, R1 = rows[0][i], rows[1][i]
        F = fin[i]
        tt(P, "pool", t0, up[i], un[i], ALU.add)
        stt(P, "dve", t0, t0, 0.5, u[:, 0:1024], ALU.mult, ALU.subtract)
        tt(P, "pool", t0, t0, mu, ALU.mult)
        tt(P, "dve", mx, t0, u[:, 0:1024], ALU.add)
        r_, k_, v_, xa_ = mx[:, 0:256], mx[:, 256:512], mx[:, 512:768], mx[:, 768:1024]
        pT = C.ps[0]
        for kt in range(2):
            tr(P, pT[:, kt * 128:(kt + 1) * 128], xa_[:, kt * 128:(kt + 1) * 128], C.ident)
        cp(P, "act", xaT, pT[:, 0:256].rr("p (k n) -> p k n", k=2))
        pz = C.ps[1]
        for kt in range(2):
            mm(P, pz[:, 0:128], w1cat[:, kt, :], xaT[:, kt, :], kt == 0, kt == 1)
        for kt in range(2):
            mm(P, pz[0:64, 128:256], g1[:, kt, :], xaT[:, kt, :], kt == 0, kt == 1)
        act(P, z[0:64, :], pz[0:64, 0:128], AF.Tanh)
        cp(P, "dve", z[64:128, :], pz[64:128, 0:128])
        act(P, sg, pz[0:64, 128:256], AF.Sigmoid)
        pw, pa_, pg = C.ps[2], C.ps[3], C.ps[4]
        mm(P, pw, z, w2blk[:, 0:512], True, True)
        mm(P, pa_, z, w2blk[:, 512:1024], True, True)
        mm(P, pg[:, 0:256], sg, g2, True, True)
        tt(P, "dve", wl, pw, w0, ALU.add)
        act(P, wl, wl, AF.Sigmoid)
        act(P, wdec, wl, AF.Exp, scale=-0.6065306597126334)
        tt(P, "dve", il, pa_, a0, ALU.add)
        act(P, iclr, il, AF.Sigmoid)
        cp(P, "act", F[:, 0:256], pg[:, 0:256])
        tt(P, "pool", kk0, k_, kkp, ALU.mult)
        tt(P, "pool", sq, kk0, kk0, ALU.mult)
        red(P, "dve", ss[:, 0:4], sq.rr("p (h k) -> p h k", h=4))
        ts(P, "dve", ss[:, 0:4], ss[:, 0:4], EPS, ALU.add)
        act(P, ss[:, 0:4], ss[:, 0:4], AF.Sqrt)
        P.op("dve", "reciprocal", out=ss[:, 0:4], in_=ss[:, 0:4])
        tt(P, "dve", kk.rr("p (h k) -> p h k", h=4), kk0.rr("p (h k) -> p h k", h=4),
           ss[:, 0:4][:, :, None].bc([128, 4, 64]), ALU.mult)
        ic3 = iclr.rr("p (d c) -> p d c", d=2)
        stt(P, "dve", t1.rr("p (d c) -> p d c", d=2), ic3, -1.0, ka[:, None, :].bc([128, 2, 256]), ALU.add, ALU.mult)
        stt(P, "dve", keff.rr("p (d c) -> p d c", d=2), t1.rr("p (d c) -> p d c", d=2), 1.0,
            k_[:, None, :].bc([128, 2, 256]), ALU.add, ALU.mult)
        tt(P, "pool", bb.rr("p (d c) -> p d c", d=2), ic3, kk[:, None, :].bc([128, 2, 256]), ALU.mult)
        tt(P, "pool", rkt, r_, k_, ALU.mult)
        tt(P, "pool", rkt, rkt, rkp, ALU.mult)
        red(P, "dve", ss[:, 4:8], rkt.rr("p (h k) -> p h k", h=4))
        tt(P, "dve", F[:, 256:512].rr("p (h k) -> p h k", h=4), v_.rr("p (h k) -> p h k", h=4),
           ss[:, 4:8][:, :, None].bc([128, 4, 64]), ALU.mult)
        pT2 = C.ps[5]
        tr(P, pT2[0:32, 0:128], u[:, O1 + 768:O1 + 800], C.ident)
        cp(P, "act", alT, pT2[0:32, 0:128])
        mm(P, pT2[:, 128:384], alT, a2blk, True, True)
        tt(P, "dve", gl, pT2[:, 128:384], gab, ALU.add)
        act(P, gl, gl, AF.Sigmoid)
        act(P, gl, gl, AF.Ln)
        if C.dbg.get("old_gla"):
            act(P, gdec, gl, AF.Exp, scale=1.0 / 16.0)
        act(P, sr, u[:, O1 + 512:O1 + 768], AF.Silu)
        tt(P, "pool", F[:, 512:768], sr, glng, ALU.mult)
        CHt = cht[i]
        ts(P, "pool", CHt[:, 0:512], wl, -0.6065306597126334, ALU.mult)
        cp(P, "act", CHt[:, 512:1024], keff)
        cp(P, "pool", CHt[:, 1024:1536], bb)
        ts(P, "dve", CHt[:, 1536:1792], kk, -1.0, ALU.mult)
        cp(P, "act", CHt[:, 1792:2048], r_)
        cp(P, "pool", CHt[:, 2048:2304], v_)
        ts(P, "dve", CHt[:, 2304:2560], gl, 1.0 / 16.0, ALU.mult)
        ts(P, "pool", CHt[:, 2560:2688], u[:, O1:O1 + 128], 32.0 ** -0.5, ALU.mult)
        cp(P, "act", CHt[:, 2688:2816], u[:, O1 + 128:O1 + 256])
        cp(P, "pool", CHt[:, 2816:3072], u[:, O1 + 256:O1 + 512])
        dma(P, "sp", V(C.CH_ap[t * 128:(t + 1) * 128, :], C.CH_bufs[t]), CHt)
        if not C.dbg.get("old_gla"):
            dma(P, "pool", V(C.FIN_ap[t * 128:(t + 1) * 128, :], C.FIN_bufs[t]), F)
            continue
        for d, R in ((0, R0), (1, R1)):
            e1 = "dve" if d == 0 else "pool"
            e2 = "pool" if d == 0 else "dve"
            src = wdec[:, d * 256:(d + 1) * 256].rr("p (a h k) -> p a h k", a=2, h=2)
            hi = rv(R, 0, 128).rr("p h (a k) -> p a h k", a=2)
            lo = rv(R, 192, 128).rr("p h (a k) -> p a h k", a=2)
            cp(P, e1, hi, src)
            tt(P, e1, lo, src, hi, ALU.subtract)
            gsrc = gdec[:, d * 128:(d + 1) * 128].rr("p (a h k) -> p a h k", a=2, h=2)
            ghi = rv(R, 128, 64).rr("p h (a k) -> p a h k", a=2)
            glo = rv(R, 320, 64).rr("p h (a k) -> p a h k", a=2)
            cp(P, e2, ghi, gsrc)
            tt(P, e2, glo, gsrc, ghi, ALU.subtract)
            cp(P, e1, rv(R, 384, 128).rr("p h (a k) -> p a h k", a=2),
               keff[:, d * 256:(d + 1) * 256].rr("p (a h k) -> p a h k", a=2, h=2))
            cp(P, e2, rv(R, 512, 64).rr("p h (a k) -> p a h k", a=2),
               u[:, O1 + 128:O1 + 256].rr("p (a h k) -> p a h k", a=2, h=2))
            cp(P, e1, rv(R, 576, 128).rr("p h (a k) -> p a h k", a=2), r_.rr("p (a h k) -> p a h k", a=2, h=2))
            ts(P, e2, rv(R, 704, 64).rr("p h (a k) -> p a h k", a=2),
               u[:, O1:O1 + 128].rr("p (a h k) -> p a h k", a=2, h=2), 32.0 ** -0.5, ALU.mult)
            ts(P, e1, rv(R, 768, 128).rr("p h (a k) -> p a h k", a=2), kk.rr("p (a h k) -> p a h k", a=2, h=2),
               -1.0, ALU.mult)
            cp(P, e2, rv(R, 896, 128).rr("p h (a k) -> p a h k", a=2),
               bb[:, d * 256:(d + 1) * 256].rr("p (a h k) -> p a h k", a=2, h=2))
            dma(P, "sp" if d == 0 else "pool",
                V(C.STR_ap[d][t * 128:(t + 1) * 128].rearrange("t h n -> t (h n)"), C.STR_bufs[d][t]),
                R.rr("p h n -> p (h n)"))
        pv4 = C.ps[6]
        for a in range(2):
            tr(P, pv4[:, a * 128:(a + 1) * 128], v_[:, a * 128:(a + 1) * 128], C.ident)
        for a in range(2):
            tr(P, pv4[:, (2 + a) * 128:(3 + a) * 128], u[:, O1 + 256 + a * 128:O1 + 384 + a * 128], C.ident)
        VT = vt[i]
        cp(P, "act", VT[:, :, 0], pv4[:, 0:128])
        cp(P, "dve", VT[:, :, 1], pv4[:, 0:128])
        cp(P, "act", VT[:, :, 2], pv4[:, 128:256])
        cp(P, "dve", VT[:, :, 3], pv4[:, 128:256])
        cp(P, "act", VT[:, :, 4], pv4[:, 256:384])
        cp(P, "dve", VT[:, :, 5], pv4[:, 384:512])
        dma(P, "sp", V(C.Vs_ap[:, t * 128:(t + 1) * 128, :], C.Vs_bufs[t]), VT)
        dma(P, "pool", V(C.FIN_ap[t * 128:(t + 1) * 128, :], C.FIN_bufs[t]), F)
    A.close()


def phase_scan(C, l, nchunks=None):
    P, I = C.P, C.I
    A = Arena(P)
    S = A.sb("S", [128, 2, 64])
    T3 = A.sb("T3", [128, 2, 64])
    T4 = A.sb("T4", [128, 2, 64])
    self_f = A.sb("sel_f", [128, 64, 128])
    sel = A.sb("sel", [128, 64, 128], BF16)
    dma(P, "sp", self_f, I["sel"])
    cp(P, "pool", sel, self_f)
    rows = [[A.sb("srow%d%d" % (d, i), [128, 1024], BF16) for i in range(2)] for d in range(2)]
    vb = [A.sb("vb%d" % i, [128, 2, 64, 6]) for i in range(2)]
    yb = [A.sb("yb%d" % i, [128, 2, 64, 6]) for i in range(2)]
    P.op("dve", "memset", ap=S, constant=0.0)
    for i in range(2):
        P.op("pool", "memset", ap=yb[i], constant=0.0)
    NCH = NTOK // 64
    for c in range(NCH if nchunks is None else nchunks):
        zf = c * 64
        zb = (192 - 64 * c) if c < 4 else (4544 - 64 * c)
        i = c % 2
        dma(P, "sp", rows[0][i], V(C.STR_ap[0][zf:zf + 64].rearrange("t h n -> (t h) n"), C.STR_bufs[0][zf // 128]))
        dma(P, "pool", rows[1][i], V(C.STR_ap[1][zb:zb + 64].rearrange("t h n -> (t h) n"), C.STR_bufs[1][zb // 128]))
        dma(P, "sp", vb[i][:, 0], V(C.Vs_ap[:, zf:zf + 64, :], C.Vs_bufs[zf // 128]))
        dma(P, "pool", vb[i][:, 1], V(C.Vs_ap[:, zb:zb + 64, :], C.Vs_bufs[zb // 128]))
        YB = yb[i]
        for j in range(64):
            s = c * 64 + j
            pb = (s % 2) * 2
            for d in range(2):
                jj = j if d == 0 else 63 - j
                lt = sel[:, jj, :]
                R = rows[d][i]
                bx = C.ps[pb + d]
                mm(P, bx[:, 0:64], lt, R[:, 128:192], True, False, mark=False)
                mm(P, bx[:, 0:64], lt, R[:, 320:384], False, True, mark=False)
                mm(P, bx[:, 64:128], lt, R[:, 512:576], True, True, mark=False)
                mm(P, bx[:, 128:192], lt, R[:, 704:768], True, True, mark=(d == 1))
            R4 = V(C.psall[:, pb * 512:pb * 512 + 1024].rearrange("p (d x) -> p d x", d=2), tuple(C.psb[pb:pb + 2]))
            Dv, KKv, RQv = R4[:, :, 0:64], R4[:, :, 64:128], R4[:, :, 128:192]
            P.op("dve", "tensor_tensor", lax=True, out=S, in0=S, in1=Dv, op=ALU.mult)
            vv = cust(vb[i], j * 6 + 4, [((127 - 2 * j) * 6, 2), (1, 2), (0, 32)])
            P.op("dve", "tensor_tensor", lax=True, out=T3.rr("p d (g k) -> p d g k", g=2),
                 in0=KKv.rr("p d (g k) -> p d g k", g=2), in1=vv, op=ALU.mult)
            P.op("dve", "tensor_tensor", lax=True, out=S, in0=S, in1=T3, op=ALU.add)
            P.op("dve", "tensor_tensor", lax=True, out=T4, in0=S, in1=RQv, op=ALU.mult)
            yv = cust(YB, j * 6 + 4, [((127 - 2 * j) * 6, 2), (1, 2)])
            P.op("dve", "tensor_reduce", lax=True, out=yv, in_=T4.rr("p d (g k) -> p d g k", g=2), axis=AX.X, op=ALU.add)
        dma(P, "sp", V(C.Y_ap[0][:, zf:zf + 64, :], C.Y_bufs[0][zf // 64]), YB[:, 0])
        dma(P, "pool", V(C.Y_ap[1][:, zb:zb + 64, :], C.Y_bufs[1][zb // 64]), YB[:, 1])
    A.close()


def phase_fin_ab(C, l, L):
    P, I = C.P, C.I
    A = Arena(P)
    pv = A.sb("pv", [128, NPV])
    dma(P, "sp", pv, I["pv"][l:l + 1, :].bc([128, NPV]))
    lng = pv[:, PV_LNG:PV_LNG + 256]
    yt = [A.sb("yt%d" % i, [128, 2, 128, 6]) for i in range(2)]
    fin = [A.sb("finf%d" % i, [128, 768]) for i in range(2)]
    ya = A.sb("ya", [128, 256])
    ytm = [A.sb("ytm%d" % i, [128, 2, 256]) for i in range(2)]
    ytg = [A.sb("ytg%d" % i, [128, 2, 256]) for i in range(2)]
    yg = A.sb("yg", [128, 256])
    sq = A.sb("sqf", [128, 256])
    st = A.sb("stf", [128, 16])
    ob = [A.sb("ob%d" % i, [128, 4, 128], BF16) for i in range(2)]
    for t in range(NT):
        i = t % 2
        Y = yt[i]
        F = fin[i]
        if C.dbg.get("old_gla"):
            for d in range(2):
                dma(P, "sp" if d == 0 else "pool", Y[:, d],
                    V(C.Y_ap[d][:, t * 128:(t + 1) * 128, :], (C.Y_bufs[d][2 * t], C.Y_bufs[d][2 * t + 1])))
        dma(P, "sp", F, V(C.FIN_ap[t * 128:(t + 1) * 128, :], C.FIN_bufs[t]))
        pr, pg = C.ps[0], C.ps[1]
        if C.dbg.get("old_rwkv"):
            for a in range(2):
                n = 0
                for d in range(2):
                    for g in (2 * a, 2 * a + 1):
                        mm(P, pr[:, a * 128:(a + 1) * 128], Y[:, d, :, g], C.ident, n == 0, n == 3)
                        n += 1
        if C.dbg.get("old_gla"):
            for a in range(2):
                for d in range(2):
                    mm(P, pg[:, a * 128:(a + 1) * 128], Y[:, d, :, 4 + a], C.ident, d == 0, d == 1)
        if C.dbg.get("old_rwkv"):
            cp(P, "act", ya, pr[:, 0:256])
        else:
            for d in range(2):
                dma(P, "sp" if d == 0 else "pool", ytm[i][:, d, :], V(C.YT_ap[d][t * 128:(t + 1) * 128, :], C.YT_bufs[d][t]))
            tt(P, "pool", ya, ytm[i][:, 0, :], ytm[i][:, 1, :], ALU.add)
        ya4 = ya.rr("p (h k) -> p h k", h=4)
        red(P, "dve", st[:, 0:4], ya4)
        ts(P, "dve", st[:, 0:4], st[:, 0:4], 1.0 / 64.0, ALU.mult)
        tt(P, "dve", ya4, ya4, st[:, 0:4][:, :, None].bc([128, 4, 64]), ALU.subtract)
        tt(P, "pool", sq, ya, ya, ALU.mult)
        red(P, "dve", st[:, 4:8], sq.rr("p (h k) -> p h k", h=4))
        ts(P, "dve", st[:, 4:8], st[:, 4:8], 1.0 / 64.0, ALU.mult, GN_EPS, ALU.add)
        act(P, st[:, 4:8], st[:, 4:8], AF.Sqrt)
        P.op("dve", "reciprocal", out=st[:, 4:8], in_=st[:, 4:8])
        tt(P, "dve", ya4, ya4, st[:, 4:8][:, :, None].bc([128, 4, 64]), ALU.mult)
        tt(P, "pool", ya, ya, lng, ALU.mult)
        tt(P, "pool", ya, ya, F[:, 256:512], ALU.add)
        tt(P, "pool", ya, ya, F[:, 0:256], ALU.mult)
        if C.dbg.get("old_gla"):
            cp(P, "act", yg, pg[:, 0:256])
        else:
            for d in range(2):
                dma(P, "sp" if d == 0 else "pool", ytg[i][:, d, :], V(C.YTG_ap[d][t * 128:(t + 1) * 128, :], C.YTG_bufs[d][t]))
            tt(P, "pool", yg, ytg[i][:, 0, :], ytg[i][:, 1, :], ALU.add)
        yg4 = yg.rr("p (h k) -> p h k", h=4)
        tt(P, "pool", sq, yg, yg, ALU.mult)
        red(P, "dve", st[:, 8:12], sq.rr("p (h k) -> p h k", h=4))
        ts(P, "dve", st[:, 8:12], st[:, 8:12], 1.0 / 64.0, ALU.mult, EPS, ALU.add)
        act(P, st[:, 8:12], st[:, 8:12], AF.Sqrt)
        P.op("dve", "reciprocal", out=st[:, 8:12], in_=st[:, 8:12])
        tt(P, "dve", yg4, yg4, st[:, 8:12][:, :, None].bc([128, 4, 64]), ALU.mult)
        tt(P, "pool", yg, yg, F[:, 512:768], ALU.mult)
        po = C.ps[2]
        for a in range(2):
            tr(P, po[:, a * 128:(a + 1) * 128], ya[:, a * 128:(a + 1) * 128], C.ident)
            tr(P, po[:, (2 + a) * 128:(3 + a) * 128], yg[:, a * 128:(a + 1) * 128], C.ident)
        OB = ob[i]
        cp(P, "act", OB, po.rr("p (a n) -> p a n", a=4))
        for br in range(2):
            dma(P, "sp" if br == 0 else "pool",
                V(C.YB_ap[br][:, t * 128:(t + 1) * 128].rearrange("(a p) n -> p a n", p=128), C.YB_bufs[br][t]),
                OB[:, 2 * br:2 * br + 2, :])
    A.close()


def phase_attn(C, l, L):
    P, I = C.P, C.I
    A = Arena(P)
    qT = A.sb("qT", [128, 2, NTOK], BF16)
    kT = A.sb("kT", [128, 2, NTOK], BF16)
    vtm = A.sb("vtm", [128, NT, 128], BF16)
    maskw = A.sb("maskw", [128, 384])
    sink = A.sb("sink", [128, 16])
    identb = A.sb("identb", [128, 128], BF16)
    dma(P, "sp", maskw, I["maskw"])
    dma(P, "sp", sink, I["attn_sink"][l:l + 1, :].bc([128, 16]))
    cp(P, "pool", identb, C.ident)
    ua = [A.sb("ua%d" % i, [128, 512]) for i in range(2)]
    rc = [A.sb("rc%d" % i, [128, 32]) for i in range(2)]
    rs = [A.sb("rs%d" % i, [128, 32]) for i in range(2)]
    qk = A.sb("qk", [128, 6, 64])
    tmp = A.sb("tmpr", [128, 6, 32])
    kd = A.sb("kd", [128, 2, 2, 64])
    cut = C.dbg.get("attn_cut", 9)
    for t in range(NT if cut > 1 else 0):
        i = t % 2
        r0 = urow(t)
        u = ua[i]
        dma(P, "sp", u, V(C.U_ap[r0:r0 + 128, O2:O3], C.U_bufs[t]))
        u6 = u[:, 0:384].rr("p (h d) -> p h d", h=6)
        if t >= 2 and not C.dbg.get("norope"):
            dma(P, "pool", rc[i], I["ropec"][(t - 2) * 128:(t - 1) * 128, :])
            dma(P, "pool", rs[i], I["ropes"][(t - 2) * 128:(t - 1) * 128, :])
            cb = rc[i][:, None, :].bc([128, 6, 32])
            sb_ = rs[i][:, None, :].bc([128, 6, 32])
            z1, z2 = u6[:, :, 0:32], u6[:, :, 32:64]
            tt(P, "dve", qk[:, :, 0:32], z1, cb, ALU.mult)
            tt(P, "pool", tmp, z2, sb_, ALU.mult)
            tt(P, "dve", qk[:, :, 0:32], qk[:, :, 0:32], tmp, ALU.subtract)
            tt(P, "dve", qk[:, :, 32:64], z1, sb_, ALU.mult)
            tt(P, "pool", tmp, z2, cb, ALU.mult)
            tt(P, "dve", qk[:, :, 32:64], qk[:, :, 32:64], tmp, ALU.add)
        else:
            cp(P, "dve", qk, u6)
        if cut < 3:
            continue
        cp(P, "pool", kd[:, :, 0, :], qk[:, 4:6, :])
        cp(P, "pool", kd[:, :, 1, :], qk[:, 4:6, :])
        cp(P, "pool", vtm[:, t, :], u[:, 384:512])
        if cut < 4:
            continue
        pq = C.ps[t % 2]
        qf = qk.rr("p h d -> p (h d)")
        kf = kd.rr("p k r d -> p (k r d)")
        for a in range(2):
            tr(P, pq[:, a * 128:(a + 1) * 128], qf[:, a * 128:(a + 1) * 128], C.ident)
            tr(P, pq[:, (2 + a) * 128:(3 + a) * 128], kf[:, a * 128:(a + 1) * 128], C.ident)
        ts(P, "dve", qT[:, :, t * 128:(t + 1) * 128], pq[:, 0:256].rr("p (a n) -> p a n", a=2), 0.125, ALU.mult)
        cp(P, "dve", kT[:, :, t * 128:(t + 1) * 128], pq[:, 256:512].rr("p (a n) -> p a n", a=2))
    if C.dbg.get("attn_p1"):
        A.close()
        return
    sc = [A.sb("sc%d" % i, [128, 640]) for i in range(2)]
    pb = [A.sb("pb%d" % i, [128, 640], BF16) for i in range(2)]
    pTs = [A.sb("pTs%d" % i, [128, 5, 128], BF16) for i in range(2)]
    st = [A.sb("sta%d" % i, [128, 8]) for i in range(2)]
    yo = [A.sb("yo%d" % i, [128, 256]) for i in range(2)]
    oc = [A.sb("oc%d" % i, [128, 2, 128], BF16) for i in range(2)]
    psT = [V(C.psall[:, b * 512:(b + 1) * 512].bitcast(BF16), C.psb[b]) for b in (4, 5)]
    n = 0
    for t in range(NT):
        YO = yo[t % 2]
        if t >= 2:
            lo, hi = max(t - 1, 2), min(t + 1, NT - 1)
            nw = hi - lo + 1
            m0 = (lo - (t - 1)) * 128
        else:
            nw = 0
        nk = nw * 128 + 256
        nblk = nw + 2
        kblocks = ([lo + b for b in range(nw)] if nw else []) + [0, 1]
        po = C.ps[6 + (t % 2)]
        for h in range(4):
            kv, hl = h // 2, h % 2
            i = n % 2
            n += 1
            S_, Pb, PT, ST = sc[i], pb[i], pTs[i], st[i]
            qv = qT[hl * 64:(hl + 1) * 64, kv, t * 128:(t + 1) * 128]
            pa_, pc_ = C.ps[2 * i], C.ps[2 * i + 1]
            if nw:
                mm(P, pa_[:, 0:nw * 128], qv, kT[hl * 64:(hl + 1) * 64, kv, lo * 128:(hi + 1) * 128], True, True)
            mm(P, pc_[:, 0:256], qv, kT[hl * 64:(hl + 1) * 64, kv, 0:256], True, True)
            if nw:
                tt(P, "dve", S_[:, 0:nw * 128], pa_[:, 0:nw * 128], maskw[:, m0:m0 + nw * 128], ALU.add)
            cp(P, "act", S_[:, nw * 128:nk], pc_[:, 0:256])
            P.op("dve", "tensor_reduce", out=ST[:, 0:1], in_=S_[:, 0:nk], axis=AX.X, op=ALU.max)
            tt(P, "dve", ST[:, 0:1], ST[:, 0:1], sink[:, h:h + 1], ALU.max)
            ts(P, "dve", ST[:, 1:2], ST[:, 0:1], -1.0, ALU.mult)
            act(P, Pb[:, 0:nk], S_[:, 0:nk], AF.Exp, bias=ST[:, 1:2], accum_out=ST[:, 2:3])
            act(P, ST[:, 3:4], sink[:, h:h + 1], AF.Exp, bias=ST[:, 1:2])
            tt(P, "dve", ST[:, 4:5], ST[:, 2:3], ST[:, 3:4], ALU.add)
            P.op("dve", "reciprocal", out=ST[:, 5:6], in_=ST[:, 4:5])
            pt = psT[i]
            for b in range(nblk):
                tr(P, pt[:, b * 128:(b + 1) * 128], Pb[:, b * 128:(b + 1) * 128], identb)
            cp(P, "act" if h % 2 == 0 else "dve", PT[:, 0:nblk, :], pt[:, 0:nblk * 128].rr("p (b n) -> p b n", b=nblk))
            for b in range(nblk):
                mm(P, po[:, h * 64:(h + 1) * 64], PT[:, b, :], vtm[:, kblocks[b], kv * 64:(kv + 1) * 64],
                   b == 0, b == nblk - 1)
            ts(P, "dve", YO[:, h * 64:(h + 1) * 64], po[:, h * 64:(h + 1) * 64], ST[:, 5:6], ALU.mult)
        pf = C.ps[t % 2]
        for a in range(2):
            tr(P, pf[:, a * 128:(a + 1) * 128], YO[:, a * 128:(a + 1) * 128], C.ident)
        OC = oc[t % 2]
        cp(P, "act", OC, pf[:, 0:256].rr("p (a n) -> p a n", a=2))
        dma(P, "sp", V(C.YB_ap[2][:, t * 128:(t + 1) * 128].rearrange("(a p) n -> p a n", p=128), C.YB_bufs[2][t]), OC)
    A.close()


PI = math.pi
S5_BLOCKS = [(0, 256)] + [(256 + 512 * i, 512) for i in range(8)]


I32 = mybir.dt.int32
TWO_PI_HI = 6.28125
TWO_PI_LO = 2.0 * math.pi - 6.28125


def sincos(P, A, s_out, c_out, x, shape, tag):
    qi = A.sb("qi" + tag, shape, I32)
    kf = A.sb("kf" + tag, shape)
    r = A.sb("rr" + tag, shape)
    m = A.sb("mm" + tag, shape)
    for extra, out in ((0.0, s_out), (0.5 * PI, c_out)):
        ts(P, "dve", r, x, 16.0 * PI + extra, ALU.add)
        ts(P, "dve", qi, r, 1.0 / (2.0 * PI), ALU.mult)
        cp(P, "dve", kf, qi)
        stt(P, "dve", r, kf, -TWO_PI_HI, r, ALU.mult, ALU.add)
        stt(P, "dve", r, kf, -TWO_PI_LO, r, ALU.mult, ALU.add)
        ts(P, "dve", m, r, PI, ALU.is_gt)
        stt(P, "dve", r, m, -2.0 * PI, r, ALU.mult, ALU.add)
        ts(P, "dve", m, r, -PI, ALU.is_lt)
        stt(P, "dve", r, m, 2.0 * PI, r, ALU.mult, ALU.add)
        ts(P, "dve", r, r, PI, ALU.min, -PI, ALU.max)
        act(P, out, r, AF.Sin)


def phase_s5(C, l, L):
    P, I = C.P, C.I
    A = Arena(P)
    th_s = A.sb("th_s", [128, 2, 8])
    rho_s = A.sb("rho_s", [128, 2, 8])
    Ck = A.sb("Ck", [128, 2, 8, 13])
    Sk = A.sb("Sk", [128, 2, 8, 13])
    BbT = A.sb("BbT", [128, 2, 2, 8, 128], BF16)
    CT = A.sb("CT", [128, 2, 2, 8, 128], BF16)
    A0 = A
    A = Arena(P)
    tmpA = [A.sb("s5r%d" % i, [128, 1024]) for i in range(8)]
    lre, lim, ldt, t_s, t_c, t_a, t_b, t_d = tmpA
    pw2f = A.sb("pw2", [128, 16])
    dma(P, "sp", pw2f, I["pw2"][0:1, :].bc([128, 16]))
    pw2 = pw2f[:, 0:13]
    fR = [A.sb("fR%d" % d, [128, 1024]) for d in range(2)]
    fI = [A.sb("fI%d" % d, [128, 1024]) for d in range(2)]
    sm = A.sb("sm", [128, 2, 3, 8])
    dma(P, "sp", sm, I["s5_sm"][l])
    act(P, sm[:, :, 2, :], sm[:, :, 2, :], AF.Exp)
    tt(P, "dve", th_s, sm[:, :, 1, :], sm[:, :, 2, :], ALU.mult)
    tt(P, "dve", rho_s, sm[:, :, 0, :], sm[:, :, 2, :], ALU.mult)
    act(P, rho_s, rho_s, AF.Exp)
    ang13 = A.sb("ang13", [128, 16, 13])
    tt(P, "dve", ang13, th_s.rr("p d j -> p (d j)")[:, :, None].bc([128, 16, 13]), pw2[:, None, :].bc([128, 16, 13]), ALU.mult)
    sincos(P, A, Sk.rr("p d j k -> p (d j) k"), Ck.rr("p d j k -> p (d j) k"), ang13, [128, 16, 13], "k")
    for d in range(2):
        dma(P, "sp", lre, I["s5_rows"][l, d, 0:1, :].bc([128, 1024]))
        dma(P, "pool", lim, I["s5_rows"][l, d, 1:2, :].bc([128, 1024]))
        dma(P, "sp", ldt, I["s5_rows"][l, d, 2:3, :].bc([128, 1024]))
        act(P, ldt, ldt, AF.Exp)
        tt(P, "dve", t_a, lim, ldt, ALU.mult)
        sincos(P, A, t_s, t_c, t_a, [128, 1024], "r%d" % d)
        tt(P, "dve", t_a, lre, ldt, ALU.mult)
        act(P, t_a, t_a, AF.Exp)
        tt(P, "dve", t_c, t_c, t_a, ALU.mult)
        tt(P, "dve", t_s, t_s, t_a, ALU.mult)
        ts(P, "dve", t_c, t_c, -1.0, ALU.add)
        tt(P, "dve", t_a, lre, lre, ALU.mult)
        tt(P, "pool", t_b, lim, lim, ALU.mult)
        tt(P, "dve", t_a, t_a, t_b, ALU.add)
        P.op("dve", "reciprocal", out=t_a, in_=t_a)
        tt(P, "dve", t_b, t_c, lre, ALU.mult)
        tt(P, "pool", t_d, t_s, lim, ALU.mult)
        tt(P, "dve", t_b, t_b, t_d, ALU.add)
        tt(P, "dve", fR[d], t_b, t_a, ALU.mult)
        tt(P, "dve", t_b, t_s, lre, ALU.mult)
        tt(P, "pool", t_d, t_c, lim, ALU.mult)
        tt(P, "dve", t_b, t_b, t_d, ALU.subtract)
        tt(P, "dve", fI[d], t_b, t_a, ALU.mult)
    bt_f = A.sb("bt_f", [128, 2, 8, 128])
    dma(P, "sp", bt_f[:, 0], I["s5_bt"][l, 0].rr("j c s -> c j s"))
    dma(P, "pool", bt_f[:, 1], I["s5_bt"][l, 1].rr("j c s -> c j s"))
    for d in range(2):
        fr = fR[d].rr("p (j s) -> p j s", j=8)
        fi = fI[d].rr("p (j s) -> p j s", j=8)
        ta = t_a.rr("p (j s) -> p j s", j=8)
        tb = t_b.rr("p (j s) -> p j s", j=8)
        tt(P, "dve", ta, bt_f[:, 0], fr, ALU.mult)
        tt(P, "pool", tb, bt_f[:, 1], fi, ALU.mult)
        tt(P, "dve", BbT[:, d, 0], ta, tb, ALU.subtract)
        tt(P, "dve", ta, bt_f[:, 0], fi, ALU.mult)
        tt(P, "pool", tb, bt_f[:, 1], fr, ALU.mult)
        tt(P, "dve", BbT[:, d, 1], ta, tb, ALU.add)
        for ri in range(2):
            ctf = t_c if ri == 0 else t_d
            dma(P, "sp" if ri == 0 else "pool", ctf.rr("p (j c) -> p j c", j=8), I["s5_ct"][l, d, ri].rr("j s c -> s j c"))
            if ri == 0:
                cp(P, "pool", CT[:, d, 0], ctf.rr("p (j c) -> p j c", j=8))
            else:
                ts(P, "pool", CT[:, d, 1], ctf.rr("p (j c) -> p j c", j=8), -1.0, ALU.mult)
    A.close()
    A = A0
    cut = C.dbg.get("s5_cut", 99)
    if cut <= 1:
        A.close(); return
    if not C.dbg.get("s5_small"):
        ct, sn = A.sb("ct", [128, NTOK]), A.sb("sn", [128, NTOK])
        w_re, w_im = A.sb("w_re", [128, NTOK]), A.sb("w_im", [128, NTOK])
    uB = A.sb("uB", [128, 2, NTOK], BF16)
    yacc = A.sb("yacc", [128, 2, NTOK])
    x_re, x_im = A.sb("x_re", [128, NTOK], BF16), A.sb("x_im", [128, NTOK], BF16)
    dsk = A.sb("dsk", [128, 16])
    bgl = A.sb("bgl", [128, 16])
    dma(P, "sp", dsk, I["s5_d_fm"][l])
    dma(P, "sp", bgl, I["s5_bglu_fm"][l])
    wglu = load_bf16(P, A, "wglu", [128, 2, 256], I["s5_w_glu"][l].rr("(k p) n -> p k n", p=128))
    tmA = [[A.sb("tm%d_%d" % (b, i), [128, 512]) for i in range(4)] for b in range(2)]
    ut = [tmA[1][i][:, 0:256] for i in range(2)]
    var = C.dbg.get("s5_var", 9)
    for t in range(NT if var > 0 else 0):
        i = t % 2
        r0 = urow(t)
        dma(P, "sp" if i == 0 else "pool", ut[i], V(C.U_ap[r0:r0 + 128, O3:O4], C.U_bufs[t]))
        pp = C.ps[i]
        for a in range(2):
            tr(P, pp[:, a * 128:(a + 1) * 128], ut[i][:, a * 128:(a + 1) * 128], C.ident)
        if var < 2:
            continue
        for a in range(2):
            cp(P, "dve", uB[:, a, t * 128:(t + 1) * 128], pp[:, a * 128:(a + 1) * 128])
        for a in range(2):
            ts(P, "dve", yacc[:, a, t * 128:(t + 1) * 128], pp[:, a * 128:(a + 1) * 128], dsk[:, a:a + 1], ALU.mult)
    tm = tmA[0]
    if cut <= 2:
        A.close(); return
    nb = 0
    for d in range(2):
        for j in range(8):
            jt = j // 4
            th = th_s[:, d, j:j + 1]
            P.op("dve", "memset", ap=ct[:, 0:1], constant=1.0)
            P.op("dve", "memset", ap=sn[:, 0:1], constant=0.0)
            k = 0
            n = 1
            while n < NTOK:
                m = min(n, NTOK - n)
                ck, sk = Ck[:, d, j, k:k + 1], Sk[:, d, j, k:k + 1]
                e1, e2 = ("dve", "pool") if m >= 256 else ("dve", "dve")
                ts(P, e1, ct[:, n:n + m], ct[:, 0:m], ck, ALU.mult)
                ts(P, e2, sn[:, n:n + m], sn[:, 0:m], ck, ALU.mult)
                ts(P, e2, tm[0][:, 0:min(m, 512)] if m <= 512 else w_re[:, 0:m], sn[:, 0:m], sk, ALU.mult)
                ts(P, e1, tm[1][:, 0:min(m, 512)] if m <= 512 else w_im[:, 0:m], ct[:, 0:m], sk, ALU.mult)
                ta_ = tm[0][:, 0:m] if m <= 512 else w_re[:, 0:m]
                tb_ = tm[1][:, 0:m] if m <= 512 else w_im[:, 0:m]
                tt(P, e1, ct[:, n:n + m], ct[:, n:n + m], ta_, ALU.subtract)
                tt(P, e2, sn[:, n:n + m], sn[:, n:n + m], tb_, ALU.add)
                n += m
                k += 1
            if cut <= 3:
                A.close(); return
            for bi, (t0, n) in enumerate(S5_BLOCKS):
                if d == 0:
                    rhs = uB[:, jt, t0:t0 + n]
                else:
                    last = (255 - t0) if t0 < 256 else (4607 - t0)
                    rhs = cust(uB, jt * NTOK + last, [(-1, n)])
                pr, pi_ = C.ps[(nb % 2) * 2], C.ps[(nb % 2) * 2 + 1]
                nb += 1
                mm(P, pr[:, 0:n], BbT[:, d, 0, j, :], rhs, True, True)
                mm(P, pi_[:, 0:n], BbT[:, d, 1, j, :], rhs, True, True)
                c_, s_ = ct[:, t0:t0 + n], sn[:, t0:t0 + n]
                tm = tmA[bi % 2]
                tt(P, "dve", tm[0][:, 0:n], pr[:, 0:n], c_, ALU.mult)
                tt(P, "dve", tm[1][:, 0:n], pi_[:, 0:n], s_, ALU.mult)
                tt(P, "pool", w_re[:, t0:t0 + n], tm[0][:, 0:n], tm[1][:, 0:n], ALU.add)
                tt(P, "dve", tm[2][:, 0:n], pi_[:, 0:n], c_, ALU.mult)
                tt(P, "dve", tm[3][:, 0:n], pr[:, 0:n], s_, ALU.mult)
                tt(P, "pool", w_im[:, t0:t0 + n], tm[2][:, 0:n], tm[3][:, 0:n], ALU.subtract)
            if cut <= 4:
                A.close(); return
            rb = rho_s[:, d, j:j + 1].bc([128, NTOK])
            P.op("dve", "tensor_tensor_scan", out=w_re, data0=rb, data1=w_re, initial=0.0, op0=ALU.mult, op1=ALU.add)
            P.op("dve", "tensor_tensor_scan", out=w_im, data0=rb, data1=w_im, initial=0.0, op0=ALU.mult, op1=ALU.add)
            if cut <= 5:
                A.close(); return
            for bi, (t0, n) in enumerate(S5_BLOCKS):
                c_, s_ = ct[:, t0:t0 + n], sn[:, t0:t0 + n]
                tm = tmA[bi % 2]
                tt(P, "pool", tm[0][:, 0:n], w_re[:, t0:t0 + n], c_, ALU.mult)
                tt(P, "pool", tm[1][:, 0:n], w_im[:, t0:t0 + n], s_, ALU.mult)
                tt(P, "dve", x_re[:, t0:t0 + n], tm[0][:, 0:n], tm[1][:, 0:n], ALU.subtract)
                tt(P, "dve", tm[2][:, 0:n], w_re[:, t0:t0 + n], s_, ALU.mult)
                tt(P, "dve", tm[3][:, 0:n], w_im[:, t0:t0 + n], c_, ALU.mult)
                tt(P, "pool", x_im[:, t0:t0 + n], tm[2][:, 0:n], tm[3][:, 0:n], ALU.add)
                py = C.ps[4 + (bi % 2)]
                if d == 0:
                    xr, xi = x_re[:, t0:t0 + n], x_im[:, t0:t0 + n]
                    k0 = t0
                else:
                    k0 = (256 - t0 - n) if t0 < 256 else (4608 - t0 - n)
                    s_last = t0 + n - 1
                    xr, xi = cust(x_re, s_last, [(-1, n)]), cust(x_im, s_last, [(-1, n)])
                mm(P, py[:, 0:n], CT[:, d, 0, j, :], xr, True, False)
                mm(P, py[:, 0:n], CT[:, d, 1, j, :], xi, False, True)
                tt(P, "dve", yacc[:, jt, k0:k0 + n], yacc[:, jt, k0:k0 + n], py[:, 0:n], ALU.add)
    if cut <= 7:
        A.close(); return
    glb = uB
    tm = tmA[0]
    ob = [x_re[:, 0:1024].rr("p (a n) -> p a n", a=2), x_im[:, 0:1024].rr("p (a n) -> p a n", a=2)]
    for bi, (t0, n) in enumerate(S5_BLOCKS):
        for a in range(2):
            y = yacc[:, a, t0:t0 + n]
            tt(P, "pool", tm[0][:, 0:n], y, y, ALU.mult)
            ts(P, "dve", tm[0][:, 0:n], tm[0][:, 0:n], 0.044715, ALU.mult, 1.0, ALU.add)
            tt(P, "pool", tm[0][:, 0:n], tm[0][:, 0:n], y, ALU.mult)
            act(P, tm[0][:, 0:n], tm[0][:, 0:n], AF.Sigmoid, scale=1.5957691216057308)
            tt(P, "dve", y, y, tm[0][:, 0:n], ALU.mult)
            cp(P, "pool", glb[:, a, t0:t0 + n], y)
        OB = ob[bi % 2]
        for a in range(2):
            pz = C.ps[6 + a]
            for kt in range(2):
                mm(P, pz[:, 0:n], wglu[:, kt, a * 128:(a + 1) * 128], glb[:, kt, t0:t0 + n], kt == 0, kt == 1)
            act(P, tm[1 + a][:, 0:n], pz[:, 0:n], AF.Sigmoid, bias=bgl[:, a:a + 1])
            tt(P, "dve", OB[:, a, 0:n], yacc[:, a, t0:t0 + n], tm[1 + a][:, 0:n], ALU.mult)
        tl = [t for t in range(NT) if t * 128 >= t0 and t * 128 < t0 + n]
        dma(P, "sp", V(C.YB_ap[3][:, t0:t0 + n].rearrange("(a p) n -> p a n", p=128), tuple(C.YB_bufs[3][t] for t in tl)),
            OB[:, :, 0:n])
    A.close()


TOKBLK = [(0, 256)] + [(256 + 512 * i, 512) for i in range(8)]


def phase_win_gates(C, l, L, hfm):
    P, I = C.P, C.I
    A = Arena(P)
    wst = [A.sb("wgs%d" % i, [128, 8, 512]) for i in range(2)]
    wb = [A.sb("wgb%d" % i, [128, 8, 512], BF16) for i in range(2)]
    gst = [A.sb("gst%d" % i, [128, 512], BF16) for i in range(4)]
    wv = I["w_in"][l].rr("(k p) n -> p k n", p=128)
    n = 0
    for cb in range(8):
        c0 = O4 + cb * 512
        dma(P, "sp" if cb % 2 == 0 else "pool", wst[cb % 2], wv[:, :, c0:c0 + 512])
        cp(P, "act" if cb % 2 == 0 else "dve", wb[cb % 2], wst[cb % 2])
        w = wb[cb % 2]
        for mi in range(4):
            row0 = cb * 512 + mi * 128
            for bi, (t0, nn) in enumerate(TOKBLK):
                ps = C.ps[n % 4]
                g = gst[n % 4]
                for k in range(8):
                    mm(P, ps[:, 0:nn], w[:, k, mi * 128:(mi + 1) * 128], hfm[:, k, t0:t0 + nn], k == 0, k == 7)
                cp(P, "act" if n % 2 == 0 else "dve", g[:, 0:nn], ps[:, 0:nn])
                dma(P, "sp" if n % 2 == 0 else "pool", V(C.Gt_ap[row0:row0 + 128, t0:t0 + nn], C.Gt_bufs[bi]), g[:, 0:nn])
                n += 1
    A.close()


def phase_merge(C, l, L):
    P, I = C.P, C.I
    A = Arena(P)
    wbr = A.sb("wbr", [128, 4, 2, 1024], BF16)
    wout = A.sb("wout", [128, 8, 1024], BF16)
    wst = A.sb("wmst", [128, 8, 1024])
    for i in range(4):
        dma(P, "sp", wst[:, 0:2, :], I["w_branch"][l, i].rr("(k p) n -> p k n", p=128))
        cp(P, "act", wbr[:, i], wst[:, 0:2, :])
    dma(P, "sp", wst, I["w_out"][l].rr("(k p) n -> p k n", p=128))
    cp(P, "act", wout, wst)
    yb = [A.sb("myb%d" % i, [128, 4, 2, 512], BF16) for i in range(2)]
    gt = [A.sb("mgt%d" % i, [128, 512], BF16) for i in range(4)]
    sg = [A.sb("msg%d" % i, [128, 512]) for i in range(2)]
    tmp = A.sb("mtmp", [128, 512])
    acc = A.sb("macc", [128, 512])
    mg = [A.sb("mmg%d" % i, [128, 8, 512], BF16) for i in range(2)]
    xt = [A.sb("mxt%d" % i, [128, 1024]) for i in range(2)]
    tm2 = A.sb("mtm2", [128, 1024])
    n = 0
    nx = 0
    for bi, (t0, nn) in enumerate(TOKBLK):
        YB = yb[bi % 2]
        tl = [t for t in range(NT) if t0 <= t * 128 < t0 + nn]
        for i in range(4):
            dma(P, "sp" if i % 2 == 0 else "pool", YB[:, i, :, 0:nn],
                V(C.YB_ap[i][:, t0:t0 + nn].rearrange("(a p) n -> p a n", p=128), tuple(C.YB_bufs[i][t] for t in tl)))
        MG = mg[bi % 2]
        for m in range(8):
            for i in range(4):
                g = gt[n % 4]
                dma(P, "sp" if n % 2 == 0 else "pool", g[:, 0:nn],
                    V(C.Gt_ap[i * 1024 + m * 128:i * 1024 + (m + 1) * 128, t0:t0 + nn], C.Gt_bufs[bi]))
                s_ = sg[n % 2]
                act(P, s_[:, 0:nn], g[:, 0:nn], AF.Sigmoid)
                ps = C.ps[n % 4]
                n += 1
                for kt in range(2):
                    mm(P, ps[:, 0:nn], wbr[:, i, kt, m * 128:(m + 1) * 128], YB[:, i, kt, 0:nn], kt == 0, kt == 1)
                if i == 0:
                    tt(P, "dve", acc[:, 0:nn], ps[:, 0:nn], s_[:, 0:nn], ALU.mult)
                elif i < 3:
                    tt(P, "dve", tmp[:, 0:nn], ps[:, 0:nn], s_[:, 0:nn], ALU.mult)
                    tt(P, "pool", acc[:, 0:nn], acc[:, 0:nn], tmp[:, 0:nn], ALU.add)
                else:
                    tt(P, "dve", tmp[:, 0:nn], ps[:, 0:nn], s_[:, 0:nn], ALU.mult)
                    tt(P, "dve", MG[:, m, 0:nn], acc[:, 0:nn], tmp[:, 0:nn], ALU.add)
        for ti, t in enumerate(tl):
            j = 1 if t < 2 else 0
            x = xt[nx % 2]
            nx += 1
            dma(P, "sp", x, xsrc(C, l, t))
            for half in range(2):
                po = C.ps[4 + half + 2 * (nx % 2)]
                for k in range(8):
                    mm(P, po, MG[:, k, ti * 128:(ti + 1) * 128], wout[:, k, half * 512:(half + 1) * 512], k == 0, k == 7)
                tt(P, "dve", tm2[:, half * 512:(half + 1) * 512], po, L.grow[0][j][:, half * 512:(half + 1) * 512], ALU.mult)
            tt(P, "pool", x, x, tm2, ALU.add)
            dma(P, "pool", C.xres[t], x)
    A.close()
    C.x_in_scratch = True


def make_router(C, l, L, RA):
    P, I = C.P, C.I
    wr = RA.sb("wr", [128, 8, 36])
    brow = RA.sb("brow", [128, 36])
    dma(P, "sp", wr, I["w_router"][l].rr("(k p) n -> p k n", p=128))
    dma(P, "sp", brow, I["b_router"][l:l + 1, :].bc([128, 36]))
    lg = RA.sb("lg", [128, 36])
    st = RA.sb("rst", [128, 16])
    oh = RA.sb("roh", [128, 4])
    em = RA.sb("rem", [128, 32])
    em2 = RA.sb("rem2", [128, 32])
    oh1 = RA.sb("roh1", [128, 32])
    oh2 = RA.sb("roh2", [128, 32])
    wg = RA.sb("rwg", [128, 32])
    junk = RA.sb("rjunk", [128, 4])

    def per_tile(t, hf):
        pl = C.ps[6]
        for k in range(8):
            mm(P, pl[:, 0:36], hf[:, k, :], wr[:, k, :], k == 0, k == 7)
        tt(P, "dve", lg, pl[:, 0:36], brow, ALU.add)
        g, e = lg[:, 0:4], lg[:, 4:36]
        P.op("dve", "tensor_reduce", out=st[:, 0:1], in_=g, axis=AX.X, op=ALU.max)
        ts(P, "dve", oh, g, st[:, 0:1], ALU.is_equal)
        ts(P, "dve", st[:, 1:2], st[:, 0:1], -1.0, ALU.mult)
        act(P, junk, g, AF.Exp, bias=st[:, 1:2], accum_out=st[:, 2:3])
        P.op("dve", "reciprocal", out=st[:, 3:4], in_=st[:, 2:3])
        ts(P, "dve", oh, oh, 1e30, ALU.mult, -1e30, ALU.add)
        tt(P, "dve", em.rr("p (g k) -> p g k", g=4), e.rr("p (g k) -> p g k", g=4),
           oh[:, :, None].bc([128, 4, 8]), ALU.add)
        P.op("dve", "tensor_reduce", out=st[:, 4:5], in_=em, axis=AX.X, op=ALU.max)
        ts(P, "dve", oh1, em, st[:, 4:5], ALU.is_equal)
        stt(P, "dve", em2, oh1, -1e30, em, ALU.mult, ALU.add)
        P.op("dve", "tensor_reduce", out=st[:, 5:6], in_=em2, axis=AX.X, op=ALU.max)
        ts(P, "dve", oh2, em2, st[:, 5:6], ALU.is_equal)
        tt(P, "dve", st[:, 6:7], st[:, 5:6], st[:, 4:5], ALU.subtract)
        act(P, st[:, 7:8], st[:, 6:7], AF.Exp)
        ts(P, "dve", st[:, 8:9], st[:, 7:8], 1.0, ALU.add)
        P.op("dve", "reciprocal", out=st[:, 9:10], in_=st[:, 8:9])
        tt(P, "dve", st[:, 10:11], st[:, 7:8], st[:, 9:10], ALU.mult)
        ts(P, "dve", wg, oh1, st[:, 9:10], ALU.mult)
        stt(P, "dve", wg, oh2, st[:, 10:11], wg, ALU.mult, ALU.add)
        ts(P, "dve", wg, wg, st[:, 3:4], ALU.mult)
        pt = C.ps[7]
        tr(P, pt[0:32, 0:128], wg, C.ident)
        cp(P, "dve", L.WT[:, t * 128:(t + 1) * 128], pt[0:32, 0:128])
    return per_tile


MOE_GROUPS = [list(range(0, 12)), list(range(12, 24)), list(range(24, 34))]


def phase_moe(C, l, L, H2, last):
    P, I = C.P, C.I
    A = Arena(P)
    hg = A.sb("hg", [128, 8, 12 * 128], BF16)
    wst = [A.sb("ews%d" % i, [128, 4, 512]) for i in range(2)]
    wgu = [A.sb("wgu%d" % i, [128, 2, 8, 512], BF16) for i in range(2)]
    wd = [A.sb("wd%d" % i, [128, 4, 1024], BF16) for i in range(2)]
    yacc = A.sb("eyacc", [128, 12, 1024])
    hid = [A.sb("hid%d" % i, [128, 4, 512], BF16) for i in range(2)]
    sil = [A.sb("sil%d" % i, [128, 512], BF16) for i in range(2)]
    tu = [A.sb("etu%d" % i, [128, 512]) for i in range(2)]
    xt = [A.sb("ext%d" % i, [128, 1024]) for i in range(2)]
    tm2 = A.sb("etm2", [128, 1024])
    ne = 0
    nst = 0
    nb = 0
    for G in MOE_GROUPS:
        tiles = [t for t in G if not (last and t < 2)]
        if not tiles:
            continue
        blocks = [tiles[i:i + 4] for i in range(0, len(tiles), 4)]
        g0 = tiles[0] * 128
        gn = len(tiles) * 128
        dma(P, "sp", hg[:, :, 0:gn], H2[:, :, g0:g0 + gn])
        for e in range(32):
            WGU, WD = wgu[ne % 2], wd[ne % 2]
            ne += 1
            for gi, nm in enumerate(("w_exp_gate", "w_exp_up")):
                src = I[nm][l, e].rr("(k p) n -> p k n", p=128)
                for hf_ in range(2):
                    s_ = wst[nst % 2]
                    dma(P, "sp" if nst % 2 == 0 else "pool", s_, src[:, hf_ * 4:(hf_ + 1) * 4, :])
                    cp(P, "act", WGU[:, gi, hf_ * 4:(hf_ + 1) * 4, :], s_)
                    nst += 1
            srcd = I["w_exp_down"][l, e].rr("(k p) n -> p k n", p=128)
            for hf_ in range(2):
                s_ = wst[nst % 2]
                dma(P, "sp" if nst % 2 == 0 else "pool", s_.rr("p k n -> p (k n)").rr("p (k n) -> p k n", k=2), srcd[:, hf_ * 2:(hf_ + 1) * 2, :])
                cp(P, "dve", WD[:, hf_ * 2:(hf_ + 1) * 2, :], s_.rr("p k n -> p (k n)").rr("p (k n) -> p k n", k=2))
                nst += 1
            for blk in blocks:
                t0 = blk[0] * 128
                nn = len(blk) * 128
                HID = hid[nb % 2]
                psW = C.ps[0]
                mm(P, psW[:, 0:nn], C.ident[0:32, e:e + 1].bc([32, 128]), L.WT[:, t0:t0 + nn], True, True)
                for f in range(4):
                    i2 = (nb * 4 + f) % 2
                    psG, psU = C.ps[1 + 2 * i2], C.ps[2 + 2 * i2]
                    for k in range(8):
                        mm(P, psG[:, 0:nn], WGU[:, 0, k, f * 128:(f + 1) * 128], hg[:, k, t0 - g0:t0 - g0 + nn], k == 0, k == 7)
                    for k in range(8):
                        mm(P, psU[:, 0:nn], WGU[:, 1, k, f * 128:(f + 1) * 128], hg[:, k, t0 - g0:t0 - g0 + nn], k == 0, k == 7)
                    act(P, sil[i2][:, 0:nn], psG[:, 0:nn], AF.Silu)
                    tt(P, "dve", tu[i2][:, 0:nn], psU[:, 0:nn], sil[i2][:, 0:nn], ALU.mult)
                    tt(P, "dve", HID[:, f, 0:nn], tu[i2][:, 0:nn], psW[:, 0:nn], ALU.mult)
                for ti, t in enumerate(blk):
                    ya = yacc[:, t - tiles[0], :]
                    for half in range(2):
                        po = C.ps[5 + (nb * 8 + ti * 2 + half) % 3]
                        for f in range(4):
                            mm(P, po, HID[:, f, ti * 128:(ti + 1) * 128], WD[:, f, half * 512:(half + 1) * 512], f == 0, f == 3)
                        if e == 0:
                            cp(P, "dve", ya[:, half * 512:(half + 1) * 512], po)
                        else:
                            tt(P, "dve", ya[:, half * 512:(half + 1) * 512], ya[:, half * 512:(half + 1) * 512], po, ALU.add)
                nb += 1
        for t in tiles:
            j = 1 if t < 2 else 0
            x = xt[t % 2]
            dma(P, "sp", x, C.xres[t])
            tt(P, "dve", tm2, yacc[:, t - tiles[0], :], L.grow[1][j], ALU.mult)
            tt(P, "pool", x, x, tm2, ALU.add)
            dma(P, "pool", C.xres[t], x)
    A.close()


def final_norm(C):
    P, I = C.P, C.I
    A = Arena(P)
    g = A.sb("fg", [128, D])
    dma(P, "sp", g, I["final_g"][0:1, :].bc([128, D]))
    xt = [A.sb("fxt%d" % i, [128, D]) for i in range(2)]
    junk = A.sb("fjunk", [128, D])
    st = [A.sb("fst%d" % i, [128, 2]) for i in range(2)]
    for t in range(2, NT):
        x, s = xt[t % 2], st[t % 2]
        dma(P, "sp" if t % 2 == 0 else "pool", x, C.xres[t])
        act(P, junk, x, AF.Square, accum_out=s[:, 0:1])
        ts(P, "dve", s[:, 1:2], s[:, 0:1], 1.0 / D, ALU.mult, EPS, ALU.add)
        act(P, s[:, 1:2], s[:, 1:2], AF.Sqrt)
        P.op("dve", "reciprocal", out=s[:, 1:2], in_=s[:, 1:2])
        stt(P, "dve", x, x, s[:, 1:2], g, ALU.mult, ALU.mult)
        dma(P, "sp" if t % 2 == 1 else "pool", V(C.out.ap[(t - 2) * 128:(t - 1) * 128, :], Buf("o%d" % t)), x)
    A.close()


CH_COLS = 3072


def phase_chunk(C, l, L):
    P, I = C.P, C.I
    A = Arena(P)
    mk = A.sb("cmasks", [128, 7, 128])
    dma(P, "sp", mk, I["cmasks"])
    idn = C.ident
    chs = [A.sb("chs%d" % i, [128, CH_COLS]) for i in range(2)]
    ST = [[A.sb("cST%d%d" % (d, h), [64, 64]) for h in range(4)] for d in range(2)]
    for d in range(2):
        for h in range(4):
            P.op("dve", "memset", ap=ST[d][h], constant=0.0)

    def mk2(name, shape):
        return [A.sb("%s%d" % (name, i), shape) for i in range(2)]
    TOT, incS, Ein, Enin, Eex, Eend, Etot, tmpx, tmpy = [mk2("cE%d" % i, [128, 256]) for i in range(9)]
    at, rt, bt, kt, bh, kh = [mk2("cq%d" % i, [128, 256]) for i in range(6)]
    aT, rT, bT, kT = [mk2("cT%d" % i, [128, 2, 128]) for i in range(4)]
    bhc = [mk2("cbhc%d" % c, [128, 256]) for c in range(2)]
    khc = [mk2("ckhc%d" % c, [128, 256]) for c in range(2)]
    IM = A.sb("cIM", [128, 64])
    tt(P, "pool", IM, idn[:, 0:64], idn[:, 64:128], ALU.add)

    def mkh(name, shape):
        return [A.sb("%s%d" % (name, h), shape) for h in range(4)]
    X0, XT0, X1, XT1, AakT, ArbT, ArkT, TT = [mkh("cM%d" % i, [128, 128]) for i in range(8)]
    Ap, M1, U0 = [mkh("cP%d" % i, [128, 64]) for i in range(3)]
    RpT = mkh("cRpT", [64, 128])
    DPC = mkh("cDPC", [128, 64])
    Y0c = [mkh("cY0c%d" % c, [64, 64]) for c in range(2)]
    GT = [mkh("cGT%d" % c, [64, 64]) for c in range(2)]
    Hc = [mkh("cH%d" % c, [64, 64]) for c in range(2)]
    yo = [A.sb("cyo%d" % i, [64, 2, 256]) for i in range(2)]
    STg = [[A.sb("gST%d%d" % (d, h), [32, 64]) for h in range(4)] for d in range(2)]
    for d in range(2):
        for h in range(4):
            P.op("dve", "memset", ap=STg[d][h], constant=0.0)
    gTOT, gincS, gEin, gEnin, gEend, gEtot, gtmp = [mk2("gE%d" % i, [128, 128]) for i in range(7)]
    gq, gk, gkh = [mk2("gq%d" % i, [128, 128]) for i in range(3)]
    gkhc = [mk2("gkhc%d" % c, [128, 128]) for c in range(2)]
    gqT, gkT, gPT = [[mk2("gT%d_%d" % (i, h), [32, 128]) for h in range(4)] for i in range(3)]
    gA = mkh("gA", [128, 128])
    gY0 = [mkh("gY0%d" % c, [64, 64]) for c in range(2)]
    gH = [mkh("gH%d" % c, [32, 64]) for c in range(2)]
    gyo = [A.sb("gyo%d" % i, [64, 2, 256]) for i in range(2)]
    ps = C.ps
    border = [1, 0] + list(range(NT - 1, 1, -1))
    H4 = range(4)
    it = 0
    cut = C.dbg.get("chunk_cut", 99)
    for n in range(NT if cut > 50 else 1):
        for d in range(2):
            t = n if d == 0 else border[n]
            q = it % 2
            ch = chs[q]
            YO = yo[q]
            it += 1
            dma(P, "sp" if d == 0 else "pool", ch, V(C.CH_ap[t * 128:(t + 1) * 128, :], C.CH_bufs[t]))
            lw = ch[:, d * 256:(d + 1) * 256]
            ke = ch[:, 512 + d * 256:768 + d * 256]
            b_ = ch[:, 1024 + d * 256:1280 + d * 256]
            a_, r_, v_ = ch[:, 1536:1792], ch[:, 1792:2048], ch[:, 2048:2304]
            m_s, m_st, m_it = (0, 1, 2) if d == 0 else (3, 4, 5)
            pc = ps[4 + q]
            mm(P, pc[:, 0:256], mk[:, m_it, :], lw, True, True)
            mm(P, pc[:, 256:512], mk[:, 6, :], lw, True, True)
            cp(P, "dve", TOT[q], pc[:, 256:512])
            cp(P, "dve", incS[q], pc[:, 0:256])
            act(P, Ein[q], incS[q], AF.Exp)
            act(P, Enin[q], incS[q], AF.Exp, scale=-1.0)
            tt(P, "pool", tmpx[q], incS[q], lw, ALU.subtract)
            act(P, Eex[q], tmpx[q], AF.Exp)
            tt(P, "pool", tmpy[q], TOT[q], incS[q], ALU.subtract)
            act(P, Eend[q], tmpy[q], AF.Exp)
            act(P, Etot[q], TOT[q], AF.Exp)
            tt(P, "pool", at[q], a_, Eex[q], ALU.mult)
            tt(P, "pool", rt[q], r_, Ein[q], ALU.mult)
            tt(P, "pool", bt[q], b_, Enin[q], ALU.mult)
            tt(P, "pool", kt[q], ke, Enin[q], ALU.mult)
            tt(P, "pool", bh[q], b_, Eend[q], ALU.mult)
            tt(P, "pool", kh[q], ke, Eend[q], ALU.mult)
            for c in range(2):
                ts(P, "pool", bhc[c][q], bh[q], mk[:, 6, c * 64:c * 64 + 1], ALU.mult)
                ts(P, "pool", khc[c][q], kh[q], mk[:, 6, c * 64:c * 64 + 1], ALU.mult)
            for qi, (src, dst) in enumerate(((at, aT), (rt, rT), (bt, bT), (kt, kT))):
                pb_ = ps[6 + qi % 2]
                for a2 in range(2):
                    tr(P, pb_[:, a2 * 128:(a2 + 1) * 128], src[q][:, a2 * 128:(a2 + 1) * 128], idn)
                cp(P, "dve", dst[q], pb_[:, 0:256].rr("p (a n) -> p a n", a=2))

            def hv(h):
                pair, hl = h // 2, h % 2
                return pair, slice(hl * 64, (hl + 1) * 64), slice(h * 64, (h + 1) * 64)
            if cut <= 2:
                continue
            for h in H4:
                pair, hs, hc = hv(h)
                pA = ps[h]
                mm(P, pA[:, 0:128], aT[q][hs, pair, :], bT[q][hs, pair, :], True, True)
                mm(P, pA[:, 128:256], bT[q][hs, pair, :], aT[q][hs, pair, :], True, True)
                mm(P, pA[:, 256:384], kT[q][hs, pair, :], aT[q][hs, pair, :], True, True)
                mm(P, pA[:, 384:512], bT[q][hs, pair, :], rT[q][hs, pair, :], True, True)
            for h in H4:
                pA = ps[h]
                tt(P, "dve", X0[h], pA[:, 0:128], mk[:, m_s, :], ALU.mult)
                tt(P, "dve", XT0[h], pA[:, 128:256], mk[:, m_st, :], ALU.mult)
                tt(P, "dve", AakT[h], pA[:, 256:384], mk[:, m_st, :], ALU.mult)
                tt(P, "dve", ArbT[h], pA[:, 384:512], mk[:, m_it, :], ALU.mult)
                tt(P, "pool", TT[h], XT0[h], idn, ALU.add)
            for h in H4:
                pair, hs, hc = hv(h)
                mm(P, ps[h][:, 0:128], kT[q][hs, pair, :], rT[q][hs, pair, :], True, True)
            for h in H4:
                tt(P, "dve", ArkT[h], ps[h][:, 0:128], mk[:, m_it, :], ALU.mult)
            if cut <= 3:
                continue
            for s in range(5):
                Xc, XTc = (X0, XT0) if s % 2 == 0 else (X1, XT1)
                Xn, XTn = (X1, XT1) if s % 2 == 0 else (X0, XT0)
                for h in H4:
                    pX = ps[h]
                    mm(P, pX[:, 128:256], XTc[h], Xc[h], True, True)
                    if s < 4:
                        mm(P, pX[:, 256:384], Xc[h], XTc[h], True, True)
                for h in H4:
                    pX = ps[h]
                    cp(P, "dve", Xn[h], pX[:, 128:256])
                    if s < 4:
                        cp(P, "dve", XTn[h], pX[:, 256:384])
                for h in H4:
                    mm(P, ps[h][:, 384:512], Xn[h], TT[h], True, True)
                for h in H4:
                    tt(P, "dve", TT[h], TT[h], ps[h][:, 384:512], ALU.add)
            if cut <= 4:
                continue
            for h in H4:
                pair, hs, hc = hv(h)
                mm(P, ps[h][:, 0:64], TT[h], at[q][:, hc], True, True)
                mm(P, ps[h][:, 64:128], AakT[h], v_[:, hc], True, True)
            for h in H4:
                cp(P, "dve", Ap[h], ps[h][:, 0:64])
                cp(P, "dve", M1[h], ps[h][:, 64:128])
            for h in H4:
                mm(P, ps[h][:, 128:192], TT[h], M1[h], True, True)
            for h in H4:
                cp(P, "dve", U0[h], ps[h][:, 128:192])
            for h in H4:
                pair, hs, hc = hv(h)
                for c in range(2):
                    cs = slice(c * 64, (c + 1) * 64)
                    mm(P, ps[h][0:64, 192 + c * 64:256 + c * 64], ArbT[h][:, cs], U0[h], True, False)
                    mm(P, ps[h][0:64, 192 + c * 64:256 + c * 64], ArkT[h][:, cs], v_[:, hc], False, True)
                mm(P, ps[h][0:64, 320:448], Ap[h], ArbT[h], True, True)
                tt(P, "pool", DPC[h], IM, Etot[q][:, hc], ALU.mult)
            for h in H4:
                pair, hs, hc = hv(h)
                for c in range(2):
                    cp(P, "dve", Y0c[c][h], ps[h][0:64, 192 + c * 64:256 + c * 64])
                tt(P, "dve", RpT[h], ps[h][0:64, 320:448], rT[q][hs, pair, :], ALU.add)
            if cut <= 5:
                continue
            for h in H4:
                pair, hs, hc = hv(h)
                pG = ps[h]
                for c in range(2):
                    cs = slice(c * 64, (c + 1) * 64)
                    o = c * 128
                    mm(P, pG[0:64, o:o + 64], Ap[h], bhc[c][q][:, hc], True, False)
                    mm(P, pG[0:64, o:o + 64], idn[:, cs], DPC[h], False, True)
                    mm(P, pG[0:64, o + 64:o + 128], bhc[c][q][:, hc], U0[h], True, False)
                    mm(P, pG[0:64, o + 64:o + 128], khc[c][q][:, hc], v_[:, hc], False, True)
            for h in H4:
                pG = ps[h]
                for c in range(2):
                    o = c * 128
                    cp(P, "dve", GT[c][h], pG[0:64, o:o + 64])
                    cp(P, "dve", Hc[c][h], pG[0:64, o + 64:o + 128])
            if cut <= 6:
                continue
            for c in ((0, 1) if d == 0 else (1, 0)):
                cs = slice(c * 64, (c + 1) * 64)
                for h in H4:
                    pG = ps[h]
                    S_ = ST[d][h]
                    mm(P, pG[0:64, 256:320], RpT[h][:, cs], S_, True, False)
                    mm(P, pG[0:64, 256:320], idn[0:64, 0:64], Y0c[c][h], False, True)
                    mm(P, pG[0:64, 320:384], GT[c][h], S_, True, False)
                    mm(P, pG[0:64, 320:384], idn[0:64, 0:64], Hc[c][h], False, True)
                for h in H4:
                    pair, hs, hc = hv(h)
                    pG = ps[h]
                    cp(P, "dve", YO[:, c, hc], pG[0:64, 256:320])
                    cp(P, "dve", ST[d][h], pG[0:64, 320:384])
            dma(P, "sp" if d == 0 else "pool",
                V(C.YT_ap[d][t * 128:(t + 1) * 128, :].rearrange("(c p) n -> p c n", p=64), C.YT_bufs[d][t]), YO)
            GYO = gyo[q]
            glw = ch[:, 2304 + d * 128:2432 + d * 128]
            gq_, gk_, gv_ = ch[:, 2560:2688], ch[:, 2688:2816], ch[:, 2816:3072]
            pcg = ps[4 + q]
            mm(P, pcg[:, 0:128], mk[:, m_it, :], glw, True, True)
            mm(P, pcg[:, 128:256], mk[:, 6, :], glw, True, True)
            cp(P, "dve", gincS[q], pcg[:, 0:128])
            cp(P, "dve", gTOT[q], pcg[:, 128:256])
            act(P, gEin[q], gincS[q], AF.Exp)
            act(P, gEnin[q], gincS[q], AF.Exp, scale=-1.0)
            tt(P, "pool", gtmp[q], gTOT[q], gincS[q], ALU.subtract)
            act(P, gEend[q], gtmp[q], AF.Exp)
            act(P, gEtot[q], gTOT[q], AF.Exp)
            tt(P, "pool", gq[q], gq_, gEin[q], ALU.mult)
            tt(P, "pool", gk[q], gk_, gEnin[q], ALU.mult)
            tt(P, "pool", gkh[q], gk_, gEend[q], ALU.mult)
            for c in range(2):
                ts(P, "pool", gkhc[c][q], gkh[q], mk[:, 6, c * 64:c * 64 + 1], ALU.mult)
            for h in H4:
                pT_ = ps[6 + h % 2]
                g32 = slice(h * 32, (h + 1) * 32)
                tr(P, pT_[0:32, 0:128], gq[q][:, g32], idn)
                tr(P, pT_[0:32, 128:256], gk[q][:, g32], idn)
                tr(P, pT_[0:32, 256:384], gEtot[q][:, g32], idn)
                cp(P, "dve", gqT[h][q], pT_[0:32, 0:128])
                cp(P, "dve", gkT[h][q], pT_[0:32, 128:256])
                cp(P, "dve", gPT[h][q], pT_[0:32, 256:384])
            for h in H4:
                mm(P, ps[h][:, 0:128], gkT[h][q], gqT[h][q], True, True)
            for h in H4:
                tt(P, "dve", gA[h], ps[h][:, 0:128], mk[:, m_it, :], ALU.mult)
            for h in H4:
                hc = slice(h * 64, (h + 1) * 64)
                g32 = slice(h * 32, (h + 1) * 32)
                for c in range(2):
                    cs = slice(c * 64, (c + 1) * 64)
                    mm(P, ps[h][0:64, 128 + c * 64:192 + c * 64], gA[h][:, cs], gv_[:, hc], True, True)
                    mm(P, ps[h][0:32, 256 + c * 64:320 + c * 64], gkhc[c][q][:, g32], gv_[:, hc], True, True)
            for h in H4:
                for c in range(2):
                    cp(P, "dve", gY0[c][h], ps[h][0:64, 128 + c * 64:192 + c * 64])
                    cp(P, "dve", gH[c][h], ps[h][0:32, 256 + c * 64:320 + c * 64])
            for c in ((0, 1) if d == 0 else (1, 0)):
                cs = slice(c * 64, (c + 1) * 64)
                for h in H4:
                    S_ = STg[d][h]
                    mm(P, ps[h][0:64, 384:448], gqT[h][q][:, cs], S_, True, False)
                    mm(P, ps[h][0:64, 384:448], idn[0:64, 0:64], gY0[c][h], False, True)
                for h in H4:
                    hc = slice(h * 64, (h + 1) * 64)
                    S_ = STg[d][h]
                    cp(P, "dve", GYO[:, c, hc], ps[h][0:64, 384:448])
                    stt(P, "dve", S_, S_, gPT[h][q][:, c * 64:c * 64 + 1], gH[c][h], ALU.mult, ALU.add)
            dma(P, "sp" if d == 1 else "pool",
                V(C.YTG_ap[d][t * 128:(t + 1) * 128, :].rearrange("(c p) n -> p c n", p=64), C.YTG_bufs[d][t]), GYO)
    A.close()


class LayerState:
    pass


def layer(C, l):
    P = C.P
    LA = Arena(P)
    L = LayerState()
    L.mod = LA.sb("mod", [128, 48, 2])
    L.sc1 = LA.sb("sc1", [128, 8, 2])
    L.sc2 = LA.sb("sc2", [128, 8, 2])
    L.grow = [[LA.sb("grow%d%d" % (ii, j), [128, 1024]) for j in range(2)] for ii in range(2)]
    L.WT = LA.sb("WT", [32, NTOK])
    phase_ada(C, l, L)
    if C.dbg.get("dump") and l == C.dbg.get("layer", 0):
        dma(P, "sp", C.dout("d_mod", [128, 48, 2]), L.mod)
        for ii in range(2):
            for j in range(2):
                dma(P, "sp", C.dout("d_grow%d%d" % (ii, j), [128, 1024]), L.grow[ii][j])
    HA = Arena(P)
    hfm = HA.sb("hfm", [128, 8, NTOK], BF16)
    phase_norm(C, l, L, 1, hfm)
    if C.dbg.get("dump") and l == C.dbg.get("layer", 0):
        dma(P, "sp", C.dout("d_hfm", [128, 8, NTOK], BF16), hfm)
    phase_win_tm(C, l, L, hfm)
    if not C.dbg.get("skip_gates"):
        phase_win_gates(C, l, L, hfm)
    HA.close()
    if C.dbg.get("stop_after") == "win":
        LA.close(); return
    if not C.dbg.get("skip_ab"):
        phase_prep(C, l, L)
        if C.dbg.get("stop_after") == "prep":
            LA.close(); return
        if C.dbg.get("old_gla") and not C.dbg.get("skip_scan"):
            phase_scan(C, l, C.dbg.get("nchunks"))
        if not C.dbg.get("old_rwkv"):
            phase_chunk(C, l, L)
        if C.dbg.get("stop_after") == "chunk":
            LA.close(); return
        if C.dbg.get("stop_after") == "scan":
            LA.close(); return
        phase_fin_ab(C, l, L)
    if C.dbg.get("stop_after") == "fin":
        LA.close(); return
    if not C.dbg.get("skip_attn"):
        phase_attn(C, l, L)
    if C.dbg.get("stop_after") == "attn":
        LA.close(); return
    if not C.dbg.get("skip_s5"):
        phase_s5(C, l, L)
    if C.dbg.get("stop_after") == "s5":
        LA.close(); return
    phase_merge(C, l, L)
    if C.dbg.get("stop_after") == "merge":
        LA.close(); return
    HA = Arena(P)
    hfm2 = HA.sb("hfm2", [128, 8, NTOK], BF16)
    RA = Arena(P)
    phase_norm(C, l, L, 2, hfm2, per_tile=make_router(C, l, L, RA))
    RA.close()
    if C.dbg.get("dump") and l == C.dbg.get("layer", 0):
        dma(P, "sp", C.dout("d_hfm2", [128, 8, NTOK], BF16), hfm2)
        dma(P, "sp", C.dout("d_WT", [32, NTOK]), L.WT)
    H2 = V(C.H2_ap, C.H2_buf)
    dma(P, "sp", H2, hfm2)
    HA.close()
    if C.dbg.get("stop_after") == "norm2":
        LA.close(); return
    phase_moe(C, l, L, H2, l == DEPTH - 1)
    LA.close()


def make_maskw():
    m = np.zeros((128, 384), np.float32)
    i = np.arange(128)[:, None]
    j = np.arange(128)[None, :]
    m[:, 0:128] = np.where(j >= i, 0.0, -1e30)
    m[:, 256:384] = np.where(j <= i, 0.0, -1e30)
    return m


def make_rope():
    rows = SEQ // 64
    row = np.repeat(np.arange(rows, dtype=np.float32), 64)
    col = np.tile(np.arange(64, dtype=np.float32), rows)
    inv = (10000.0 ** (-np.arange(16, dtype=np.float32) / 16)).astype(np.float32)
    ang = np.concatenate([row[:, None] * inv, col[:, None] * inv], axis=-1).astype(np.float32)
    return np.cos(ang).astype(np.float32), np.sin(ang).astype(np.float32)


ROPE = make_rope()


def s5_host(A):
    f = np.float32
    lre, lim, ldt = A("s5_lam_re"), A("s5_lam_im"), A("s5_log_dt")
    ldt_e = np.repeat(ldt[..., None], 64, axis=-1)
    rows = np.stack([lre.reshape(DEPTH, 2, 1024), lim.reshape(DEPTH, 2, 1024), ldt_e.reshape(DEPTH, 2, 1024)], axis=2)
    sm = rows.reshape(DEPTH, 2, 3, 8, 128).transpose(0, 4, 1, 2, 3)
    bre, bim = A("s5_b_re"), A("s5_b_im")
    bt = np.zeros((DEPTH, 2, 8, 128, 128), f)
    cre, cim = A("s5_c_re"), A("s5_c_im")
    ct = np.zeros((DEPTH, 2, 2, 8, 128, 128), f)
    for g in range(16):
        j, hh = g // 2, g % 2
        c0 = (g % 8) * 16
        for ri, b in enumerate((bre, bim)):
            bt[:, ri, j, c0:c0 + 16, hh * 64:(hh + 1) * 64] = b[:, g].transpose(0, 2, 1)
        for ri, c in enumerate((cre, cim)):
            ct[:, :, ri, j, hh * 64:(hh + 1) * 64, c0:c0 + 16] = c[:, :, g].transpose(0, 1, 3, 2)
    return {
        "s5_sm": np.ascontiguousarray(sm, f), "s5_rows": np.ascontiguousarray(rows, f),
        "s5_bt": bt, "s5_ct": ct,
        "pw2": (2.0 ** np.arange(16)).astype(f).reshape(1, 16),
        "s5_d_fm": np.ascontiguousarray(np.pad(A("s5_d").reshape(DEPTH, 2, 128).transpose(0, 2, 1), ((0, 0), (0, 0), (0, 14)))),
        "s5_bglu_fm": np.ascontiguousarray(np.pad(A("s5_b_glu").reshape(DEPTH, 2, 128).transpose(0, 2, 1), ((0, 0), (0, 0), (0, 14)))),
        "s5_w_glu": A("s5_w_glu"),
    }


def make_sele():
    s = np.zeros((32, 32, 128), np.float32)
    for e in range(32):
        s[e, e, :] = 1.0
    return s


def make_cmasks():
    r = np.arange(128)[:, None]
    c = np.arange(128)[None, :]
    same = (r // 64) == (c // 64)
    m = np.zeros((128, 7, 128), np.float32)
    m[:, 0] = same & (c < r)
    m[:, 1] = same & (r < c)
    m[:, 2] = same & (r <= c)
    m[:, 3] = same & (c > r)
    m[:, 4] = same & (r > c)
    m[:, 5] = same & (r >= c)
    m[:, 6] = same
    return m


def make_sel():
    s = np.zeros((128, 64, 128), np.float32)
    for j in range(64):
        for hh in range(2):
            s[2 * j + hh, j, hh * 64:(hh + 1) * 64] = 1.0
    return s


def blkdiag(mats):
    n = len(mats)
    L, r, c = mats[0].shape
    o = np.zeros((L, n * r, n * c), np.float32)
    for i, m in enumerate(mats):
        o[:, i * r:(i + 1) * r, i * c:(i + 1) * c] = m
    return o


def host_inputs(inputs, b):
    f = np.float32

    def A(k):
        return np.asarray(inputs[k], f)
    c = np.asarray(inputs["c"], f)[b]
    cctx = np.asarray(inputs["c_ctx"], f)
    cc = np.stack([c.reshape(8, 128).T, cctx.reshape(8, 128).T], axis=-1)
    m = {
        "xb": np.ascontiguousarray(np.asarray(inputs["x"], f)[b]),
        "ctxb": np.ascontiguousarray(np.asarray(inputs["ctx"], f)[b]),
        "cc": np.ascontiguousarray(cc),
        "w_ada": np.asarray(inputs["w_ada"], f),
        "b_ada": np.asarray(inputs["b_ada"], f),
        "b_ada_fm": np.ascontiguousarray(np.asarray(inputs["b_ada"], f).reshape(DEPTH, 48, 128).transpose(0, 2, 1)),
        "g1_fm": np.ascontiguousarray(np.asarray(inputs["norm1_g"], f).reshape(DEPTH, 8, 128).transpose(0, 2, 1)),
        "g2_fm": np.ascontiguousarray(np.asarray(inputs["norm2_g"], f).reshape(DEPTH, 8, 128).transpose(0, 2, 1)),
        "w_in": np.asarray(inputs["w_in"], f),
        "ident": np.eye(128, dtype=f),
        "sel": make_sel(),
        "maskw": make_maskw(),
        "ropec": ROPE[0],
        "ropes": ROPE[1],
        "attn_sink": np.ascontiguousarray(np.pad(A("attn_sink"), ((0, 0), (0, 12)))),
        **s5_host(A),
        "cmasks": make_cmasks(),
        "w_branch": A("w_branch"),
        "w_out": A("w_out"),
        "w_router": np.ascontiguousarray(np.concatenate([A("w_router_g"), A("w_router_e")], axis=2)),
        "b_router": np.ascontiguousarray(np.concatenate([A("b_router_g"), A("b_router_e")], axis=1)),
        "w_exp_gate": A("w_exp_gate"),
        "w_exp_up": A("w_exp_up"),
        "w_exp_down": A("w_exp_down"),
        "pv": np.ascontiguousarray(np.concatenate([
            A("rwkv_mu").reshape(DEPTH, -1), A("rwkv_kk"), A("rwkv_ka"), A("rwkv_rk").reshape(DEPTH, -1),
            A("rwkv_w0").reshape(DEPTH, -1), A("rwkv_a0").reshape(DEPTH, -1), A("rwkv_ln_g"),
            A("gla_ab").reshape(DEPTH, -1), A("gla_ln_g")], axis=1)),
        "w1cat": np.ascontiguousarray(np.concatenate([A("rwkv_w1")[:, 0], A("rwkv_w1")[:, 1],
                                                      A("rwkv_a1")[:, 0], A("rwkv_a1")[:, 1]], axis=2)),
        "w2blk": blkdiag([A("rwkv_w2")[:, 0], A("rwkv_w2")[:, 1], A("rwkv_a2")[:, 0], A("rwkv_a2")[:, 1]]),
        "g1": A("rwkv_g1"),
        "g2": A("rwkv_g2"),
        "a2blk": blkdiag([A("gla_a2")[:, 0], A("gla_a2")[:, 1]]),
        "final_g": np.asarray(inputs["final_norm_g"], f).reshape(1, D),
    }
    return m


def kernel(**inputs):
    nc = build_program()
    in_maps = [host_inputs(inputs, b) for b in range(8)]
    res = run_bass_kernel_spmd(nc, in_maps, core_ids=list(range(8)))
    return np.stack([r["out"] for r in res.results], axis=0)
```

```python
import math
from contextlib import ExitStack

import numpy as np
import concourse.bass as bass
import concourse.mybir as mybir
from concourse.bass_utils import run_bass_kernel_spmd

F32 = mybir.dt.float32
BF16 = mybir.dt.bfloat16
ALU = mybir.AluOpType
AF = mybir.ActivationFunctionType
AX = mybir.AxisListType

D = 1024
SEQ = 4096
CTX = 256
NT = (SEQ + CTX) // 128
NTOK = SEQ + CTX
DEPTH = 2
EPS = 1e-6
GN_EPS = 64e-5
O1, O2, O3, O4, PIN = 1024, 1824, 2336, 2592, 6688
PV_MU, PV_KK, PV_KA, PV_RK, PV_W0, PV_A0, PV_LNG, PV_GAB, PV_GLNG = 0, 1024, 1280, 1536, 1792, 2304, 2816, 3072, 3328
NPV = 3584

DEBUG = False
PENDING = "PENDING"
WKEYS = ("out", "accum_out", "ap")
SKEYS = ("scalar1", "scalar2", "scale", "bias", "scalar")


class Buf:
    __slots__ = ("name", "w", "rd", "ws")

    def __init__(self, name=""):
        self.name = name
        self.w = None
        self.rd = {}
        self.ws = False


class V:
    __slots__ = ("ap", "bufs")

    def __init__(self, ap, bufs):
        self.ap = ap
        self.bufs = bufs if isinstance(bufs, tuple) else (bufs,)

    def __getitem__(self, k):
        return V(self.ap[k], self.bufs)

    def rr(self, pat, **kw):
        return V(self.ap.rearrange(pat, **kw), self.bufs)

    def bc(self, shape):
        return V(self.ap.to_broadcast(list(shape)), self.bufs)

    def bitcast(self, dt):
        return V(self.ap.bitcast(dt), self.bufs)

    def wb(self, *bufs):
        return V(self.ap, tuple(bufs))

    @property
    def shape(self):
        return tuple(self.ap.shape)


class Prog:
    ENG = ("pe", "act", "dve", "pool", "sp")
    K = 6
    STRICT_ALL = False

    def __init__(self, nc):
        self.nc = nc
        self.eng = {"pe": nc.tensor, "act": nc.scalar, "dve": nc.vector, "pool": nc.gpsimd, "sp": nc.sync}
        self.sem = {e: nc.alloc_semaphore("s_" + e) for e in self.ENG}
        self.cnt = {e: 0 for e in self.ENG}
        self.dsem = {q: [nc.alloc_semaphore("d_%s%d" % (q, i)) for i in range(self.K)] for q in ("sp", "act", "pool")}
        self.dcnt = {q: 0 for q in self.dsem}
        self.known = {e: {} for e in self.ENG}
        self.pend_r = []
        self.pend_w = []
        self.uid = 0
        self.nins = 0

    def _need(self, e, tok, is_dma, strict=False):
        if tok is None:
            return
        if tok is PENDING:
            assert e == "pe" and not is_dma, "dependency on an unmarked PE op"
            return
        sem, val, owner = tok
        if owner == e and not is_dma and not (strict and e != "pe") and not self.STRICT_ALL:
            return
        k = self.known[e]
        if k.get(sem.num, 0) >= val:
            return
        self.eng[e].wait_ge(sem, val)
        self.nins += 1
        k[sem.num] = val

    def op(self, e, meth, mark=True, lax=False, **kw):
        reads, writes, args, sreads, awrites = [], [], {}, [], []
        for k, v in kw.items():
            if isinstance(v, V):
                (writes if k in WKEYS else reads).extend(v.bufs)
                if k in SKEYS:
                    sreads.extend(v.bufs)
                if k == "accum_out":
                    awrites.extend(v.bufs)
                args[k] = v.ap
            else:
                args[k] = v
        is_dma = meth == "dma_start"
        for b in reads:
            self._need(e, b.w, is_dma, strict=(not lax) or b.ws or e == "act" or b in sreads)
        for b in writes:
            self._need(e, b.w, is_dma)
            for t in b.rd.values():
                self._need(e, t, is_dma)
        if is_dma:
            n = self.dcnt[e]
            sem = self.dsem[e][n % self.K]
            r = n // self.K
            if r > 0:
                self._need(e, (sem, 16 * r, None), True)
            ins = getattr(self.eng[e], meth)(**args)
            ins.then_inc(sem, 16)
            self.dcnt[e] = n + 1
            tok = (sem, 16 * (r + 1), None)
            key = ("d", sem.num)
        else:
            ins = getattr(self.eng[e], meth)(**args)
            key = e
            if mark:
                self.cnt[e] += 1
                ins.then_inc(self.sem[e], 1)
                tok = (self.sem[e], self.cnt[e], e)
                if e == "pe" and (self.pend_r or self.pend_w):
                    for b in self.pend_r:
                        if b.rd.get("pe") is PENDING:
                            b.rd["pe"] = tok
                    for b in self.pend_w:
                        if b.w is PENDING:
                            b.w = tok
                    self.pend_r = []
                    self.pend_w = []
            else:
                assert e == "pe"
                tok = PENDING
                self.pend_r.extend(reads)
                self.pend_w.extend(writes)
        self.nins += 1
        for b in reads:
            b.rd[key] = tok
        for b in writes:
            b.w = tok
            b.rd = {}
            b.ws = (b in awrites) or e == "act"
        return ins

    def barrier(self):
        assert not self.pend_r and not self.pend_w
        toks = [(self.sem[e], self.cnt[e], e) for e in self.ENG if self.cnt[e] > 0]
        for q in self.dsem:
            n = self.dcnt[q]
            for i in range(self.K):
                c = (n - i + self.K - 1) // self.K if n > i else 0
                if c > 0:
                    toks.append((self.dsem[q][i], 16 * c, None))
        for e in self.ENG:
            for t in toks:
                self._need(e, t, False)

    def name(self, s):
        self.uid += 1
        return "%s_%d" % (s, self.uid)

    def dram(self, name, shape, dt, kind="Internal"):
        return self.nc.dram_tensor(name, list(shape), dt, kind=kind).ap()


class Arena:
    def __init__(self, P):
        self.P = P
        self.stack = ExitStack()

    def sb(self, name, shape, dt=F32):
        h = self.stack.enter_context(self.P.nc.sbuf_tensor(self.P.name(name), list(shape), dt))
        return V(h.ap(), Buf(name))

    def close(self):
        self.P.barrier()
        self.stack.close()


def dma(P, q, out, in_):
    return P.op(q, "dma_start", out=out, in_=in_)


def mm(P, out, lhsT, rhs, start, stop, mark=None):
    return P.op("pe", "matmul", mark=(stop if mark is None else mark), out=out, lhsT=lhsT, rhs=rhs,
                start=start, stop=stop)


def tr(P, out, in_, ident, mark=True):
    return P.op("pe", "transpose", mark=mark, out=out, in_=in_, identity=ident)


def tt(P, e, out, in0, in1, op):
    return P.op(e, "tensor_tensor", out=out, in0=in0, in1=in1, op=op)


def ts(P, e, out, in0, s1, op0, s2=None, op1=None, **kw):
    if op1 is None:
        return P.op(e, "tensor_scalar", out=out, in0=in0, scalar1=s1, scalar2=None, op0=op0, **kw)
    return P.op(e, "tensor_scalar", out=out, in0=in0, scalar1=s1, scalar2=s2, op0=op0, op1=op1, **kw)


def act(P, out, in_, func, **kw):
    return P.op("act", "activation", out=out, in_=in_, func=func, **kw)


def cp(P, e, out, in_):
    if e == "act":
        return act(P, out, in_, AF.Copy)
    return P.op(e, "tensor_copy", out=out, in_=in_)


class Ctx:
    pass


def build_program(dbg=None):
    dbg = dbg or {}
    nc = bass.Bass("TRN2", target_bir_lowering=False)
    P = Prog(nc)
    C = Ctx()
    C.P, C.nc, C.dbg = P, nc, dbg
    skind = "ExternalOutput" if dbg.get("expose") else "Internal"

    def din(name, shape, dt=F32):
        return V(nc.dram_tensor(name, list(shape), dt, kind="ExternalInput").ap(), Buf(name))

    I = {}
    I["xb"] = din("xb", [SEQ, D])
    I["ctxb"] = din("ctxb", [CTX, D])
    I["cc"] = din("cc", [128, 8, 2])
    I["w_ada"] = din("w_ada", [DEPTH, D, 6 * D])
    I["b_ada"] = din("b_ada", [DEPTH, 6 * D])
    I["b_ada_fm"] = din("b_ada_fm", [DEPTH, 128, 48])
    I["g1_fm"] = din("g1_fm", [DEPTH, 128, 8])
    I["g2_fm"] = din("g2_fm", [DEPTH, 128, 8])
    I["w_in"] = din("w_in", [DEPTH, D, PIN])
    I["ident"] = din("ident", [128, 128])
    I["sel"] = din("sel", [128, 64, 128])
    I["maskw"] = din("maskw", [128, 384])
    I["ropec"] = din("ropec", [SEQ, 32])
    I["ropes"] = din("ropes", [SEQ, 32])
    I["attn_sink"] = din("attn_sink", [DEPTH, 16])
    I["s5_sm"] = din("s5_sm", [DEPTH, 128, 2, 3, 8])
    I["s5_rows"] = din("s5_rows", [DEPTH, 2, 3, 1024])
    I["s5_bt"] = din("s5_bt", [DEPTH, 2, 8, 128, 128])
    I["s5_ct"] = din("s5_ct", [DEPTH, 2, 2, 8, 128, 128])
    I["pw2"] = din("pw2", [1, 16])
    I["cmasks"] = din("cmasks", [128, 7, 128])
    I["w_branch"] = din("w_branch", [DEPTH, 4, 256, D])
    I["w_out"] = din("w_out", [DEPTH, D, D])
    I["w_router"] = din("w_router", [DEPTH, D, 36])
    I["b_router"] = din("b_router", [DEPTH, 36])
    I["w_exp_gate"] = din("w_exp_gate", [DEPTH, 32, D, 512])
    I["w_exp_up"] = din("w_exp_up", [DEPTH, 32, D, 512])
    I["w_exp_down"] = din("w_exp_down", [DEPTH, 32, 512, D])
    I["s5_d_fm"] = din("s5_d_fm", [DEPTH, 128, 16])
    I["s5_bglu_fm"] = din("s5_bglu_fm", [DEPTH, 128, 16])
    I["s5_w_glu"] = din("s5_w_glu", [DEPTH, 256, 256])
    I["pv"] = din("pv", [DEPTH, NPV])
    I["w1cat"] = din("w1cat", [DEPTH, 256, 128])
    I["w2blk"] = din("w2blk", [DEPTH, 128, 1024])
    I["g1"] = din("g1", [DEPTH, 256, 64])
    I["g2"] = din("g2", [DEPTH, 64, 256])
    I["a2blk"] = din("a2blk", [DEPTH, 32, 256])
    I["final_g"] = din("final_g", [1, D])
    C.I = I

    out = V(nc.dram_tensor("out", [SEQ, D], F32, kind="ExternalOutput").ap(), Buf("out"))
    C.out = out

    def dout(name, shape, dt=F32):
        return V(nc.dram_tensor(name, list(shape), dt, kind="ExternalOutput").ap(), Buf(name))
    C.dout = dout

    xres_ap = P.dram("xres", [NTOK, D], F32, kind=skind)
    C.xres = [V(xres_ap[t * 128:(t + 1) * 128, :], Buf("xres%d" % t)) for t in range(NT)]
    C.U_ap = P.dram("U", [NTOK + 3, O4], F32, kind=skind)
    C.U_bufs = [Buf("U%d" % t) for t in range(NT)]
    C.U_pad = Buf("Upad")
    C.STR_ap = [P.dram("STR%d" % d, [NTOK, 2, 1024], BF16, kind=skind) for d in range(2)]
    C.STR_bufs = [[Buf("STR%d_%d" % (d, t)) for t in range(NT)] for d in range(2)]
    C.Vs_ap = P.dram("Vs", [128, NTOK, 6], F32, kind=skind)
    C.Vs_bufs = [Buf("Vs%d" % t) for t in range(NT)]
    C.Y_ap = [P.dram("Y%d" % d, [128, NTOK, 6], F32, kind=skind) for d in range(2)]
    C.Y_bufs = [[Buf("Y%d_%d" % (d, c)) for c in range(NTOK // 64)] for d in range(2)]
    C.FIN_ap = P.dram("FIN", [NTOK, 768], F32, kind=skind)
    C.FIN_bufs = [Buf("FIN%d" % t) for t in range(NT)]
    C.YB_ap = P.dram("YB", [4, 256, NTOK], BF16, kind=skind)
    C.YB_bufs = [[Buf("YB%d_%d" % (i, t)) for t in range(NT)] for i in range(4)]
    C.CH_ap = P.dram("CH", [NTOK, 3072], F32, kind=skind)
    C.CH_bufs = [Buf("CH%d" % t) for t in range(NT)]
    C.YT_ap = [P.dram("YT%d" % d, [NTOK, 256], F32, kind=skind) for d in range(2)]
    C.YT_bufs = [[Buf("YT%d_%d" % (d, t)) for t in range(NT)] for d in range(2)]
    C.YTG_ap = [P.dram("YTG%d" % d, [NTOK, 256], F32, kind=skind) for d in range(2)]
    C.YTG_bufs = [[Buf("YTG%d_%d" % (d, t)) for t in range(NT)] for d in range(2)]
    C.Gt_ap = P.dram("Gt", [4096, NTOK], BF16, kind=skind)
    C.Gt_bufs = [Buf("Gt%d" % b) for b in range(9)]
    C.H2_ap = P.dram("H2", [128, 8, NTOK], BF16, kind=skind)
    C.H2_buf = Buf("H2")

    G = Arena(P)
    C.G = G
    C.psall = nc.alloc_psum_tensor("psall", [128, 4096], F32).ap()
    C.psb = [Buf("ps%d" % i) for i in range(8)]
    C.ps = [V(C.psall[:, i * 512:(i + 1) * 512], C.psb[i]) for i in range(8)]
    C.ident = G.sb("ident", [128, 128])
    dma(P, "sp", C.ident, I["ident"])

    for l in range(DEPTH):
        layer(C, l)
        if dbg.get("stop_layer") == l:
            break
    if not dbg.get("stop"):
        final_norm(C)
    P.barrier()
    return nc


def urow(t):
    return 1 + t * 128 if t < 2 else 258 + (t - 2) * 128


def xsrc(C, l, t):
    if l == 0 and not C.__dict__.get("x_in_scratch"):
        if t < 2:
            return C.I["ctxb"][t * 128:(t + 1) * 128, :]
        return C.I["xb"][(t - 2) * 128:(t - 1) * 128, :]
    return C.xres[t]


def phase_ada(C, l, L):
    P, I = C.P, C.I
    A = Arena(P)
    cc = A.sb("cc", [128, 8, 2])
    sc = A.sb("sc", [128, 8, 2])
    screp = A.sb("screp", [128, 8, 2, 128])
    bfm = A.sb("bfm", [128, 48])
    g1 = A.sb("g1", [128, 8])
    g2 = A.sb("g2", [128, 8])
    wst = [A.sb("wst%d" % i, [128, 8, 512]) for i in range(2)]
    brow = [A.sb("brow%d" % i, [128, 1024]) for i in range(2)]
    dma(P, "sp", cc, I["cc"])
    dma(P, "sp", bfm, I["b_ada_fm"][l])
    dma(P, "sp", g1, I["g1_fm"][l])
    dma(P, "sp", g2, I["g2_fm"][l])
    for ii, i in enumerate((2, 5)):
        dma(P, "pool", brow[ii], I["b_ada"][l:l + 1, i * 1024:(i + 1) * 1024].bc([128, 1024]))
    act(P, sc, cc, AF.Silu)
    for k in range(8):
        for j in range(2):
            cp(P, "dve", screp[:, k, j, :], sc[:, k, j:j + 1].bc([128, 128]))
    wv = I["w_ada"][l].rr("(k p) n -> p k n", p=128)
    psA = C.ps[0]
    for c in range(12):
        w = wst[c % 2]
        dma(P, "sp" if c % 2 == 0 else "pool", w, wv[:, :, c * 512:(c + 1) * 512])
        for mi in range(4):
            m = c * 4 + mi
            for k in range(8):
                mm(P, psA[:, m * 2:(m + 1) * 2], w[:, k, mi * 128:(mi + 1) * 128], sc[:, k, :], k == 0, k == 7)
        if c in (4, 5, 10, 11):
            ii = 0 if c < 6 else 1
            half = c % 2
            for j in range(2):
                pr = C.ps[1 + j]
                for k in range(8):
                    mm(P, pr, screp[:, k, j, :], w[:, k, :], k == 0, k == 7)
                tt(P, "dve", L.grow[ii][j][:, half * 512:(half + 1) * 512], pr,
                   brow[ii][:, half * 512:(half + 1) * 512], ALU.add)
    tt(P, "dve", L.mod, psA[:, 0:96].rr("p (m j) -> p m j", j=2), bfm[:, :, None].bc([128, 48, 2]), ALU.add)
    ts(P, "dve", L.sc1, L.mod[:, 8:16, :], 1.0, ALU.add)
    tt(P, "dve", L.sc1, L.sc1, g1[:, :, None].bc([128, 8, 2]), ALU.mult)
    ts(P, "dve", L.sc2, L.mod[:, 32:40, :], 1.0, ALU.add)
    tt(P, "dve", L.sc2, L.sc2, g2[:, :, None].bc([128, 8, 2]), ALU.mult)
    A.close()


def phase_norm(C, l, L, which, hfm, per_tile=None):
    P = C.P
    A = Arena(P)
    sc = L.sc1 if which == 1 else L.sc2
    shb = 0 if which == 1 else 24
    xt = [A.sb("xt%d" % i, [128, D]) for i in range(2)]
    junk = A.sb("junk", [128, D])
    st = [A.sb("st%d" % i, [128, 2]) for i in range(2)]
    hf = [A.sb("hf%d" % i, [128, 8, 128]) for i in range(2)] if per_tile else None
    for t in range(NT):
        j = 1 if t < 2 else 0
        x = xt[t % 2]
        s = st[t % 2]
        dma(P, "sp" if t % 2 == 0 else "pool", x, xsrc(C, l, t))
        act(P, junk, x, AF.Square, accum_out=s[:, 0:1])
        ts(P, "dve", s[:, 1:2], s[:, 0:1], 1.0 / D, ALU.mult, EPS, ALU.add)
        act(P, s[:, 1:2], s[:, 1:2], AF.Sqrt)
        P.op("dve", "reciprocal", out=s[:, 1:2], in_=s[:, 1:2])
        ts(P, "dve", x, x, s[:, 1:2], ALU.mult)
        pa, pb = C.ps[2 + 2 * (t % 2)], C.ps[3 + 2 * (t % 2)]
        if C.dbg.get("dump_norm") and t == 2 and which == 1:
            dma(P, "sp", C.dout("d_xn", [128, D]), x)
            dma(P, "sp", C.dout("d_st", [128, 2]), s)
        for k in range(8):
            pp = pa if k < 4 else pb
            tr(P, pp[:, (k % 4) * 128:(k % 4 + 1) * 128], x[:, k * 128:(k + 1) * 128], C.ident)
        if C.dbg.get("dump_norm") and t == 2 and which == 1:
            cp(P, "dve", junk[:, 0:512], pa)
            dma(P, "sp", C.dout("d_pa", [128, 512]), junk[:, 0:512])
        for k in range(8):
            pp = pa if k < 4 else pb
            src = pp[:, (k % 4) * 128:(k % 4 + 1) * 128]
            if per_tile:
                dst = hf[t % 2][:, k, :]
            else:
                dst = hfm[:, k, t * 128:(t + 1) * 128]
            if k % 2 == 0:
                act(P, dst, src, AF.Identity, scale=sc[:, k, j:j + 1], bias=L.mod[:, shb + k, j:j + 1])
            else:
                ts(P, "dve", dst, src, sc[:, k, j:j + 1], ALU.mult, L.mod[:, shb + k, j:j + 1], ALU.add)
        if per_tile:
            for k in range(8):
                cp(P, "act" if k % 2 == 0 else "dve", hfm[:, k, t * 128:(t + 1) * 128], hf[t % 2][:, k, :])
            per_tile(t, hf[t % 2])
    A.close()


def phase_win_tm(C, l, L, hfm):
    P, I = C.P, C.I
    A = Arena(P)
    wA = A.sb("wA", [128, 8, O4], BF16)
    wst = [A.sb("wst%d" % i, [128, 8, 512]) for i in range(2)]
    ust = [A.sb("ust%d" % i, [128, O4]) for i in range(2)]
    zer = A.sb("zer", [1, O4])
    P.op("dve", "memset", ap=zer, constant=0.0)
    for r in (0, 257, NTOK + 2):
        dma(P, "sp", V(C.U_ap[r:r + 1, :], C.U_pad), zer)
    wv = I["w_in"][l].rr("(k p) n -> p k n", p=128)
    blocks = [(0, 512), (512, 1024), (1024, 1536), (1536, 1824), (1824, 2336), (2336, 2592)]
    for bi, (c0, c1) in enumerate(blocks):
        w = wst[bi % 2]
        dma(P, "sp" if bi % 2 == 0 else "pool", w[:, :, 0:c1 - c0], wv[:, :, c0:c1])
        cp(P, "act", wA[:, :, c0:c1], w[:, :, 0:c1 - c0])
    n = 0
    for t in range(NT):
        u = ust[t % 2]
        for bi, (c0, c1) in enumerate(blocks):
            ps = C.ps[n % 4]
            n += 1
            for k in range(8):
                mm(P, ps[:, 0:c1 - c0], hfm[:, k, t * 128:(t + 1) * 128], wA[:, k, c0:c1], k == 0, k == 7)
            cp(P, "act" if bi % 2 == 0 else "dve", u[:, c0:c1], ps[:, 0:c1 - c0])
        r0 = urow(t)
        dma(P, "sp" if t % 2 == 0 else "pool", V(C.U_ap[r0:r0 + 128, :], C.U_bufs[t]), u)
    A.close()


def stt(P, e, out, in0, scalar, in1, op0, op1, **kw):
    return P.op(e, "scalar_tensor_tensor", out=out, in0=in0, scalar=scalar, in1=in1, op0=op0, op1=op1, **kw)


def red(P, e, out, in_, **kw):
    return P.op(e, "tensor_reduce", out=out, in_=in_, axis=AX.X, op=ALU.add, **kw)


def load_bf16(P, A, name, shape, src, q="sp", ce="act"):
    st = A.sb(name + "_f", shape)
    wb = A.sb(name, shape, BF16)
    dma(P, q, st, src)
    cp(P, ce, wb, st)
    return wb


def cust(v, offset_elems, dims):
    ap = v.ap
    base = ap.ap[0]
    new = type(ap)(ap.tensor, ap.offset + offset_elems, [tuple(base)] + [tuple(d) for d in dims])
    return V(new, v.bufs)


def phase_prep(C, l, L):
    P, I = C.P, C.I
    A = Arena(P)
    pv = A.sb("pv", [128, NPV])
    dma(P, "sp", pv, I["pv"][l:l + 1, :].bc([128, NPV]))
    w1cat = load_bf16(P, A, "w1cat", [128, 2, 128], I["w1cat"][l].rr("(k p) n -> p k n", p=128))
    w2blk = load_bf16(P, A, "w2blk", [128, 1024], I["w2blk"][l])
    g1 = load_bf16(P, A, "g1w", [128, 2, 64], I["g1"][l].rr("(k p) n -> p k n", p=128))
    g2 = load_bf16(P, A, "g2w", [64, 256], I["g2"][l])
    a2blk = load_bf16(P, A, "a2blk", [32, 256], I["a2blk"][l])
    mu = pv[:, PV_MU:PV_MU + 1024]
    kkp = pv[:, PV_KK:PV_KK + 256]
    ka = pv[:, PV_KA:PV_KA + 256]
    rkp = pv[:, PV_RK:PV_RK + 256]
    w0 = pv[:, PV_W0:PV_W0 + 512]
    a0 = pv[:, PV_A0:PV_A0 + 512]
    gab = pv[:, PV_GAB:PV_GAB + 256]
    glng = pv[:, PV_GLNG:PV_GLNG + 256]

    uc = [A.sb("uc%d" % i, [128, O2]) for i in range(2)]
    up = [A.sb("up%d" % i, [128, 1024]) for i in range(2)]
    un = [A.sb("un%d" % i, [128, 1024]) for i in range(2)]
    rows = [[A.sb("rows%d%d" % (d, i), [128, 2, 1024], BF16) for i in range(2)] for d in range(2)]
    vt = [A.sb("vt%d" % i, [128, 128, 6]) for i in range(2)]
    fin = [A.sb("fin%d" % i, [128, 768]) for i in range(2)]
    cht = [A.sb("cht%d" % i, [128, 3072]) for i in range(2)]
    t0 = A.sb("t0", [128, 1024])
    mx = A.sb("mx", [128, 1024])
    xaT = A.sb("xaT", [128, 2, 128], BF16)
    z = A.sb("z", [128, 128], BF16)
    sg = A.sb("sg", [64, 128], BF16)
    wl = A.sb("wl", [128, 512])
    wdec = A.sb("wdec", [128, 512])
    il = A.sb("il", [128, 512])
    iclr = A.sb("iclr", [128, 512])
    kk0 = A.sb("kk0", [128, 256])
    sq = A.sb("sq", [128, 256])
    ss = A.sb("ss", [128, 8])
    kk = A.sb("kk", [128, 256])
    t1 = A.sb("t1", [128, 512])
    keff = A.sb("keff", [128, 512])
    bb = A.sb("bb", [128, 512])
    rkt = A.sb("rkt", [128, 256])
    alT = A.sb("alT", [32, 128], BF16)
    gl = A.sb("gl", [128, 256])
    gdec = A.sb("gdec", [128, 256])
    sr = A.sb("sr", [128, 256])

    def rv(R, c0, n):
        return R[:, :, c0:c0 + n]

    for t in range(NT):
        i = t % 2
        r0 = urow(t)
        nb = [C.U_bufs[t]]
        if t > 0:
            nb.append(C.U_bufs[t - 1])
        if t < NT - 1:
            nb.append(C.U_bufs[t + 1])
        nb.append(C.U_pad)
        dma(P, "sp", uc[i], V(C.U_ap[r0:r0 + 128, 0:O2], C.U_bufs[t]))
        dma(P, "pool", up[i], V(C.U_ap[r0 - 1:r0 + 127, 0:1024], tuple(nb)))
        dma(P, "sp", un[i], V(C.U_ap[r0 + 1:r0 + 129, 0:1024], tuple(nb)))
        u = uc[i]
        R0, R1 = rows[0][i], rows[1][i]
        F = fin[i]
        tt(P, "pool", t0, up[i], un[i], ALU.add)
        stt(P, "dve", t0, t0, 0.5, u[:, 0:1024], ALU.mult, ALU.subtract)
        tt(P, "pool", t0, t0, mu, ALU.mult)
        tt(P, "dve", mx, t0, u[:, 0:1024], ALU.add)
        r_, k_, v_, xa_ = mx[:, 0:256], mx[:, 256:512], mx[:, 512:768], mx[:, 768:1024]
        pT = C.ps[0]
        for kt in range(2):
            tr(P, pT[:, kt * 128:(kt + 1) * 128], xa_[:, kt * 128:(kt + 1) * 128], C.ident)
        cp(P, "act", xaT, pT[:, 0:256].rr("p (k n) -> p k n", k=2))
        pz = C.ps[1]
        for kt in range(2):
            mm(P, pz[:, 0:128], w1cat[:, kt, :], xaT[:, kt, :], kt == 0, kt == 1)
        for kt in range(2):
            mm(P, pz[0:64, 128:256], g1[:, kt, :], xaT[:, kt, :], kt == 0, kt == 1)
        act(P, z[0:64, :], pz[0:64, 0:128], AF.Tanh)
        cp(P, "dve", z[64:128, :], pz[64:128, 0:128])
        act(P, sg, pz[0:64, 128:256], AF.Sigmoid)
        pw, pa_, pg = C.ps[2], C.ps[3], C.ps[4]
        mm(P, pw, z, w2blk[:, 0:512], True, True)
        mm(P, pa_, z, w2blk[:, 512:1024], True, True)
        mm(P, pg[:, 0:256], sg, g2, True, True)
        tt(P, "dve", wl, pw, w0, ALU.add)
        act(P, wl, wl, AF.Sigmoid)
        act(P, wdec, wl, AF.Exp, scale=-0.6065306597126334)
        tt(P, "dve", il, pa_, a0, ALU.add)
        act(P, iclr, il, AF.Sigmoid)
        cp(P, "act", F[:, 0:256], pg[:, 0:256])
        tt(P, "pool", kk0, k_, kkp, ALU.mult)
        tt(P, "pool", sq, kk0, kk0, ALU.mult)
        red(P, "dve", ss[:, 0:4], sq.rr("p (h k) -> p h k", h=4))
        ts(P, "dve", ss[:, 0:4], ss[:, 0:4], EPS, ALU.add)
        act(P, ss[:, 0:4], ss[:, 0:4], AF.Sqrt)
        P.op("dve", "reciprocal", out=ss[:, 0:4], in_=ss[:, 0:4])
        tt(P, "dve", kk.rr("p (h k) -> p h k", h=4), kk0.rr("p (h k) -> p h k", h=4),
           ss[:, 0:4][:, :, None].bc([128, 4, 64]), ALU.mult)
        ic3 = iclr.rr("p (d c) -> p d c", d=2)
        stt(P, "dve", t1.rr("p (d c) -> p d c", d=2), ic3, -1.0, ka[:, None, :].bc([128, 2, 256]), ALU.add, ALU.mult)
        stt(P, "dve", keff.rr("p (d c) -> p d c", d=2), t1.rr("p (d c) -> p d c", d=2), 1.0,
            k_[:, None, :].bc([128, 2, 256]), ALU.add, ALU.mult)
        tt(P, "pool", bb.rr("p (d c) -> p d c", d=2), ic3, kk[:, None, :].bc([128, 2, 256]), ALU.mult)
        tt(P, "pool", rkt, r_, k_, ALU.mult)
        tt(P, "pool", rkt, rkt, rkp, ALU.mult)
        red(P, "dve", ss[:, 4:8], rkt.rr("p (h k) -> p h k", h=4))
        tt(P, "dve", F[:, 256:512].rr("p (h k) -> p h k", h=4), v_.rr("p (h k) -> p h k", h=4),
           ss[:, 4:8][:, :, None].bc([128, 4, 64]), ALU.mult)
        pT2 = C.ps[5]
        tr(P, pT2[0:32, 0:128], u[:, O1 + 768:O1 + 800], C.ident)
        cp(P, "act", alT, pT2[0:32, 0:128])
        mm(P, pT2[:, 128:384], alT, a2blk, True, True)
        tt(P, "dve", gl, pT2[:, 128:384], gab, ALU.add)
        act(P, gl, gl, AF.Sigmoid)
        act(P, gl, gl, AF.Ln)
        if C.dbg.get("old_gla"):
            act(P, gdec, gl, AF.Exp, scale=1.0 / 16.0)
        act(P, sr, u[:, O1 + 512:O1 + 768], AF.Silu)
        tt(P, "pool", F[:, 512:768], sr, glng, ALU.mult)
        CHt = cht[i]
        ts(P, "pool", CHt[:, 0:512], wl, -0.6065306597126334, ALU.mult)
        cp(P, "act", CHt[:, 512:1024], keff)
        cp(P, "pool", CHt[:, 1024:1536], bb)
        ts(P, "dve", CHt[:, 1536:1792], kk, -1.0, ALU.mult)
        cp(P, "act", CHt[:, 1792:2048], r_)
        cp(P, "pool", CHt[:, 2048:2304], v_)
        ts(P, "dve", CHt[:, 2304:2560], gl, 1.0 / 16.0, ALU.mult)
        ts(P, "pool", CHt[:, 2560:2688], u[:, O1:O1 + 128], 32.0 ** -0.5, ALU.mult)
        cp(P, "act", CHt[:, 2688:2816], u[:, O1 + 128:O1 + 256])
        cp(P, "pool", CHt[:, 2816:3072], u[:, O1 + 256:O1 + 512])
        dma(P, "sp", V(C.CH_ap[t * 128:(t + 1) * 128, :], C.CH_bufs[t]), CHt)
        if not C.dbg.get("old_gla"):
            dma(P, "pool", V(C.FIN_ap[t * 128:(t + 1) * 128, :], C.FIN_bufs[t]), F)
            continue
        for d, R in ((0, R0), (1, R1)):
            e1 = "dve" if d == 0 else "pool"
            e2 = "pool" if d == 0 else "dve"
            src = wdec[:, d * 256:(d + 1) * 256].rr("p (a h k) -> p a h k", a=2, h=2)
            hi = rv(R, 0, 128).rr("p h (a k) -> p a h k", a=2)
            lo = rv(R, 192, 128).rr("p h (a k) -> p a h k", a=2)
            cp(P, e1, hi, src)
            tt(P, e1, lo, src, hi, ALU.subtract)
            gsrc = gdec[:, d * 128:(d + 1) * 128].rr("p (a h k) -> p a h k", a=2, h=2)
            ghi = rv(R, 128, 64).rr("p h (a k) -> p a h k", a=2)
            glo = rv(R, 320, 64).rr("p h (a k) -> p a h k", a=2)
            cp(P, e2, ghi, gsrc)
            tt(P, e2, glo, gsrc, ghi, ALU.subtract)
            cp(P, e1, rv(R, 384, 128).rr("p h (a k) -> p a h k", a=2),
               keff[:, d * 256:(d + 1) * 256].rr("p (a h k) -> p a h k", a=2, h=2))
            cp(P, e2, rv(R, 512, 64).rr("p h (a k) -> p a h k", a=2),
               u[:, O1 + 128:O1 + 256].rr("p (a h k) -> p a h k", a=2, h=2))
            cp(P, e1, rv(R, 576, 128).rr("p h (a k) -> p a h k", a=2), r_.rr("p (a h k) -> p a h k", a=2, h=2))
            ts(P, e2, rv(R, 704, 64).rr("p h (a k) -> p a h k", a=2),
               u[:, O1:O1 + 128].rr("p (a h k) -> p a h k", a=2, h=2), 32.0 ** -0.5, ALU.mult)
            ts(P, e1, rv(R, 768, 128).rr("p h (a k) -> p a h k", a=2), kk.rr("p (a h k) -> p a h k", a=2, h=2),
               -1.0, ALU.mult)
            cp(P, e2, rv(R, 896, 128).rr("p h (a k) -> p a h k", a=2),
               bb[:, d * 256:(d + 1) * 256].rr("p (a h k) -> p a h k", a=2, h=2))
            dma(P, "sp" if d == 0 else "pool",
                V(C.STR_ap[d][t * 128:(t + 1) * 128].rearrange("t h n -> t (h n)"), C.STR_bufs[d][t]),
                R.rr("p h n -> p (h n)"))
        pv4 = C.ps[6]
        for a in range(2):
            tr(P, pv4[:, a * 128:(a + 1) * 128], v_[:, a * 128:(a + 1) * 128], C.ident)
        for a in range(2):
            tr(P, pv4[:, (2 + a) * 128:(3 + a) * 128], u[:, O1 + 256 + a * 128:O1 + 384 + a * 128], C.ident)
        VT = vt[i]
        cp(P, "act", VT[:, :, 0], pv4[:, 0:128])
        cp(P, "dve", VT[:, :, 1], pv4[:, 0:128])
        cp(P, "act", VT[:, :, 2], pv4[:, 128:256])
        cp(P, "dve", VT[:, :, 3], pv4[:, 128:256])
        cp(P, "act", VT[:, :, 4], pv4[:, 256:384])
        cp(P, "dve", VT[:, :, 5], pv4[:, 384:512])
        dma(P, "sp", V(C.Vs_ap[:, t * 128:(t + 1) * 128, :], C.Vs_bufs[t]), VT)
        dma(P, "pool", V(C.FIN_ap[t * 128:(t + 1) * 128, :], C.FIN_bufs[t]), F)
    A.close()


def phase_scan(C, l, nchunks=None):
    P, I = C.P, C.I
    A = Arena(P)
    S = A.sb("S", [128, 2, 64])
    T3 = A.sb("T3", [128, 2, 64])
    T4 = A.sb("T4", [128, 2, 64])
    self_f = A.sb("sel_f", [128, 64, 128])
    sel = A.sb("sel", [128, 64, 128], BF16)
    dma(P, "sp", self_f, I["sel"])
    cp(P, "pool", sel, self_f)
    rows = [[A.sb("srow%d%d" % (d, i), [128, 1024], BF16) for i in range(2)] for d in range(2)]
    vb = [A.sb("vb%d" % i, [128, 2, 64, 6]) for i in range(2)]
    yb = [A.sb("yb%d" % i, [128, 2, 64, 6]) for i in range(2)]
    P.op("dve", "memset", ap=S, constant=0.0)
    for i in range(2):
        P.op("pool", "memset", ap=yb[i], constant=0.0)
    NCH = NTOK // 64
    for c in range(NCH if nchunks is None else nchunks):
        zf = c * 64
        zb = (192 - 64 * c) if c < 4 else (4544 - 64 * c)
        i = c % 2
        dma(P, "sp", rows[0][i], V(C.STR_ap[0][zf:zf + 64].rearrange("t h n -> (t h) n"), C.STR_bufs[0][zf // 128]))
        dma(P, "pool", rows[1][i], V(C.STR_ap[1][zb:zb + 64].rearrange("t h n -> (t h) n"), C.STR_bufs[1][zb // 128]))
        dma(P, "sp", vb[i][:, 0], V(C.Vs_ap[:, zf:zf + 64, :], C.Vs_bufs[zf // 128]))
        dma(P, "pool", vb[i][:, 1], V(C.Vs_ap[:, zb:zb + 64, :], C.Vs_bufs[zb // 128]))
        YB = yb[i]
        for j in range(64):
            s = c * 64 + j
            pb = (s % 2) * 2
            for d in range(2):
                jj = j if d == 0 else 63 - j
                lt = sel[:, jj, :]
                R = rows[d][i]
                bx = C.ps[pb + d]
                mm(P, bx[:, 0:64], lt, R[:, 128:192], True, False, mark=False)
                mm(P, bx[:, 0:64], lt, R[:, 320:384], False, True, mark=False)
                mm(P, bx[:, 64:128], lt, R[:, 512:576], True, True, mark=False)
                mm(P, bx[:, 128:192], lt, R[:, 704:768], True, True, mark=(d == 1))
            R4 = V(C.psall[:, pb * 512:pb * 512 + 1024].rearrange("p (d x) -> p d x", d=2), tuple(C.psb[pb:pb + 2]))
            Dv, KKv, RQv = R4[:, :, 0:64], R4[:, :, 64:128], R4[:, :, 128:192]
            P.op("dve", "tensor_tensor", lax=True, out=S, in0=S, in1=Dv, op=ALU.mult)
            vv = cust(vb[i], j * 6 + 4, [((127 - 2 * j) * 6, 2), (1, 2), (0, 32)])
            P.op("dve", "tensor_tensor", lax=True, out=T3.rr("p d (g k) -> p d g k", g=2),
                 in0=KKv.rr("p d (g k) -> p d g k", g=2), in1=vv, op=ALU.mult)
            P.op("dve", "tensor_tensor", lax=True, out=S, in0=S, in1=T3, op=ALU.add)
            P.op("dve", "tensor_tensor", lax=True, out=T4, in0=S, in1=RQv, op=ALU.mult)
            yv = cust(YB, j * 6 + 4, [((127 - 2 * j) * 6, 2), (1, 2)])
            P.op("dve", "tensor_reduce", lax=True, out=yv, in_=T4.rr("p d (g k) -> p d g k", g=2), axis=AX.X, op=ALU.add)
        dma(P, "sp", V(C.Y_ap[0][:, zf:zf + 64, :], C.Y_bufs[0][zf // 64]), YB[:, 0])
        dma(P, "pool", V(C.Y_ap[1][:, zb:zb + 64, :], C.Y_bufs[1][zb // 64]), YB[:, 1])
    A.close()


def phase_fin_ab(C, l, L):
    P, I = C.P, C.I
    A = Arena(P)
    pv = A.sb("pv", [128, NPV])
    dma(P, "sp", pv, I["pv"][l:l + 1, :].bc([128, NPV]))
    lng = pv[:, PV_LNG:PV_LNG + 256]
    yt = [A.sb("yt%d" % i, [128, 2, 128, 6]) for i in range(2)]
    fin = [A.sb("finf%d" % i, [128, 768]) for i in range(2)]
    ya = A.sb("ya", [128, 256])
    ytm = [A.sb("ytm%d" % i, [128, 2, 256]) for i in range(2)]
    ytg = [A.sb("ytg%d" % i, [128, 2, 256]) for i in range(2)]
    yg = A.sb("yg", [128, 256])
    sq = A.sb("sqf", [128, 256])
    st = A.sb("stf", [128, 16])
    ob = [A.sb("ob%d" % i, [128, 4, 128], BF16) for i in range(2)]
    for t in range(NT):
        i = t % 2
        Y = yt[i]
        F = fin[i]
        if C.dbg.get("old_gla"):
            for d in range(2):
                dma(P, "sp" if d == 0 else "pool", Y[:, d],
                    V(C.Y_ap[d][:, t * 128:(t + 1) * 128, :], (C.Y_bufs[d][2 * t], C.Y_bufs[d][2 * t + 1])))
        dma(P, "sp", F, V(C.FIN_ap[t * 128:(t + 1) * 128, :], C.FIN_bufs[t]))
        pr, pg = C.ps[0], C.ps[1]
        if C.dbg.get("old_rwkv"):
            for a in range(2):
                n = 0
                for d in range(2):
                    for g in (2 * a, 2 * a + 1):
                        mm(P, pr[:, a * 128:(a + 1) * 128], Y[:, d, :, g], C.ident, n == 0, n == 3)
                        n += 1
        if C.dbg.get("old_gla"):
            for a in range(2):
                for d in range(2):
                    mm(P, pg[:, a * 128:(a + 1) * 128], Y[:, d, :, 4 + a], C.ident, d == 0, d == 1)
        if C.dbg.get("old_rwkv"):
            cp(P, "act", ya, pr[:, 0:256])
        else:
            for d in range(2):
                dma(P, "sp" if d == 0 else "pool", ytm[i][:, d, :], V(C.YT_ap[d][t * 128:(t + 1) * 128, :], C.YT_bufs[d][t]))
            tt(P, "pool", ya, ytm[i][:, 0, :], ytm[i][:, 1, :], ALU.add)
        ya4 = ya.rr("p (h k) -> p h k", h=4)
        red(P, "dve", st[:, 0:4], ya4)
        ts(P, "dve", st[:, 0:4], st[:, 0:4], 1.0 / 64.0, ALU.mult)
        tt(P, "dve", ya4, ya4, st[:, 0:4][:, :, None].bc([128, 4, 64]), ALU.subtract)
        tt(P, "pool", sq, ya, ya, ALU.mult)
        red(P, "dve", st[:, 4:8], sq.rr("p (h k) -> p h k", h=4))
        ts(P, "dve", st[:, 4:8], st[:, 4:8], 1.0 / 64.0, ALU.mult, GN_EPS, ALU.add)
        act(P, st[:, 4:8], st[:, 4:8], AF.Sqrt)
        P.op("dve", "reciprocal", out=st[:, 4:8], in_=st[:, 4:8])
        tt(P, "dve", ya4, ya4, st[:, 4:8][:, :, None].bc([128, 4, 64]), ALU.mult)
        tt(P, "pool", ya, ya, lng, ALU.mult)
        tt(P, "pool", ya, ya, F[:, 256:512], ALU.add)
        tt(P, "pool", ya, ya, F[:, 0:256], ALU.mult)
        if C.dbg.get("old_gla"):
            cp(P, "act", yg, pg[:, 0:256])
        else:
            for d in range(2):
                dma(P, "sp" if d == 0 else "pool", ytg[i][:, d, :], V(C.YTG_ap[d][t * 128:(t + 1) * 128, :], C.YTG_bufs[d][t]))
            tt(P, "pool", yg, ytg[i][:, 0, :], ytg[i][:, 1, :], ALU.add)
        yg4 = yg.rr("p (h k) -> p h k", h=4)
        tt(P, "pool", sq, yg, yg, ALU.mult)
        red(P, "dve", st[:, 8:12], sq.rr("p (h k) -> p h k", h=4))
        ts(P, "dve", st[:, 8:12], st[:, 8:12], 1.0 / 64.0, ALU.mult, EPS, ALU.add)
        act(P, st[:, 8:12], st[:, 8:12], AF.Sqrt)
        P.op("dve", "reciprocal", out=st[:, 8:12], in_=st[:, 8:12])
        tt(P, "dve", yg4, yg4, st[:, 8:12][:, :, None].bc([128, 4, 64]), ALU.mult)
        tt(P, "pool", yg, yg, F[:, 512:768], ALU.mult)
        po = C.ps[2]
        for a in range(2):
            tr(P, po[:, a * 128:(a + 1) * 128], ya[:, a * 128:(a + 1) * 128], C.ident)
            tr(P, po[:, (2 + a) * 128:(3 + a) * 128], yg[:, a * 128:(a + 1) * 128], C.ident)
        OB = ob[i]
        cp(P, "act", OB, po.rr("p (a n) -> p a n", a=4))
        for br in range(2):
            dma(P, "sp" if br == 0 else "pool",
                V(C.YB_ap[br][:, t * 128:(t + 1) * 128].rearrange("(a p) n -> p a n", p=128), C.YB_bufs[br][t]),
                OB[:, 2 * br:2 * br + 2, :])
    A.close()


def phase_attn(C, l, L):
    P, I = C.P, C.I
    A = Arena(P)
    qT = A.sb("qT", [128, 2, NTOK], BF16)
    kT = A.sb("kT", [128, 2, NTOK], BF16)
    vtm = A.sb("vtm", [128, NT, 128], BF16)
    maskw = A.sb("maskw", [128, 384])
    sink = A.sb("sink", [128, 16])
    identb = A.sb("identb", [128, 128], BF16)
    dma(P, "sp", maskw, I["maskw"])
    dma(P, "sp", sink, I["attn_sink"][l:l + 1, :].bc([128, 16]))
    cp(P, "pool", identb, C.ident)
    ua = [A.sb("ua%d" % i, [128, 512]) for i in range(2)]
    rc = [A.sb("rc%d" % i, [128, 32]) for i in range(2)]
    rs = [A.sb("rs%d" % i, [128, 32]) for i in range(2)]
    qk = A.sb("qk", [128, 6, 64])
    tmp = A.sb("tmpr", [128, 6, 32])
    kd = A.sb("kd", [128, 2, 2, 64])
    cut = C.dbg.get("attn_cut", 9)
    for t in range(NT if cut > 1 else 0):
        i = t % 2
        r0 = urow(t)
        u = ua[i]
        dma(P, "sp", u, V(C.U_ap[r0:r0 + 128, O2:O3], C.U_bufs[t]))
        u6 = u[:, 0:384].rr("p (h d) -> p h d", h=6)
        if t >= 2 and not C.dbg.get("norope"):
            dma(P, "pool", rc[i], I["ropec"][(t - 2) * 128:(t - 1) * 128, :])
            dma(P, "pool", rs[i], I["ropes"][(t - 2) * 128:(t - 1) * 128, :])
            cb = rc[i][:, None, :].bc([128, 6, 32])
            sb_ = rs[i][:, None, :].bc([128, 6, 32])
            z1, z2 = u6[:, :, 0:32], u6[:, :, 32:64]
            tt(P, "dve", qk[:, :, 0:32], z1, cb, ALU.mult)
            tt(P, "pool", tmp, z2, sb_, ALU.mult)
            tt(P, "dve", qk[:, :, 0:32], qk[:, :, 0:32], tmp, ALU.subtract)
            tt(P, "dve", qk[:, :, 32:64], z1, sb_, ALU.mult)
            tt(P, "pool", tmp, z2, cb, ALU.mult)
            tt(P, "dve", qk[:, :, 32:64], qk[:, :, 32:64], tmp, ALU.add)
        else:
            cp(P, "dve", qk, u6)
        if cut < 3:
            continue
        cp(P, "pool", kd[:, :, 0, :], qk[:, 4:6, :])
        cp(P, "pool", kd[:, :, 1, :], qk[:, 4:6, :])
        cp(P, "pool", vtm[:, t, :], u[:, 384:512])
        if cut < 4:
            continue
        pq = C.ps[t % 2]
        qf = qk.rr("p h d -> p (h d)")
        kf = kd.rr("p k r d -> p (k r d)")
        for a in range(2):
            tr(P, pq[:, a * 128:(a + 1) * 128], qf[:, a * 128:(a + 1) * 128], C.ident)
            tr(P, pq[:, (2 + a) * 128:(3 + a) * 128], kf[:, a * 128:(a + 1) * 128], C.ident)
        ts(P, "dve", qT[:, :, t * 128:(t + 1) * 128], pq[:, 0:256].rr("p (a n) -> p a n", a=2), 0.125, ALU.mult)
        cp(P, "dve", kT[:, :, t * 128:(t + 1) * 128], pq[:, 256:512].rr("p (a n) -> p a n", a=2))
    if C.dbg.get("attn_p1"):
        A.close()
        return
    sc = [A.sb("sc%d" % i, [128, 640]) for i in range(2)]
    pb = [A.sb("pb%d" % i, [128, 640], BF16) for i in range(2)]
    pTs = [A.sb("pTs%d" % i, [128, 5, 128], BF16) for i in range(2)]
    st = [A.sb("sta%d" % i, [128, 8]) for i in range(2)]
    yo = [A.sb("yo%d" % i, [128, 256]) for i in range(2)]
    oc = [A.sb("oc%d" % i, [128, 2, 128], BF16) for i in range(2)]
    psT = [V(C.psall[:, b * 512:(b + 1) * 512].bitcast(BF16), C.psb[b]) for b in (4, 5)]
    n = 0
    for t in range(NT):
        YO = yo[t % 2]
        if t >= 2:
            lo, hi = max(t - 1, 2), min(t + 1, NT - 1)
            nw = hi - lo + 1
            m0 = (lo - (t - 1)) * 128
        else:
            nw = 0
        nk = nw * 128 + 256
        nblk = nw + 2
        kblocks = ([lo + b for b in range(nw)] if nw else []) + [0, 1]
        po = C.ps[6 + (t % 2)]
        for h in range(4):
            kv, hl = h // 2, h % 2
            i = n % 2
            n += 1
            S_, Pb, PT, ST = sc[i], pb[i], pTs[i], st[i]
            qv = qT[hl * 64:(hl + 1) * 64, kv, t * 128:(t + 1) * 128]
            pa_, pc_ = C.ps[2 * i], C.ps[2 * i + 1]
            if nw:
                mm(P, pa_[:, 0:nw * 128], qv, kT[hl * 64:(hl + 1) * 64, kv, lo * 128:(hi + 1) * 128], True, True)
            mm(P, pc_[:, 0:256], qv, kT[hl * 64:(hl + 1) * 64, kv, 0:256], True, True)
            if nw:
                tt(P, "dve", S_[:, 0:nw * 128], pa_[:, 0:nw * 128], maskw[:, m0:m0 + nw * 128], ALU.add)
            cp(P, "act", S_[:, nw * 128:nk], pc_[:, 0:256])
            P.op("dve", "tensor_reduce", out=ST[:, 0:1], in_=S_[:, 0:nk], axis=AX.X, op=ALU.max)
            tt(P, "dve", ST[:, 0:1], ST[:, 0:1], sink[:, h:h + 1], ALU.max)
            ts(P, "dve", ST[:, 1:2], ST[:, 0:1], -1.0, ALU.mult)
            act(P, Pb[:, 0:nk], S_[:, 0:nk], AF.Exp, bias=ST[:, 1:2], accum_out=ST[:, 2:3])
            act(P, ST[:, 3:4], sink[:, h:h + 1], AF.Exp, bias=ST[:, 1:2])
            tt(P, "dve", ST[:, 4:5], ST[:, 2:3], ST[:, 3:4], ALU.add)
            P.op("dve", "reciprocal", out=ST[:, 5:6], in_=ST[:, 4:5])
            pt = psT[i]
            for b in range(nblk):
                tr(P, pt[:, b * 128:(b + 1) * 128], Pb[:, b * 128:(b + 1) * 128], identb)
            cp(P, "act" if h % 2 == 0 else "dve", PT[:, 0:nblk, :], pt[:, 0:nblk * 128].rr("p (b n) -> p b n", b=nblk))
            for b in range(nblk):
                mm(P, po[:, h * 64:(h + 1) * 64], PT[:, b, :], vtm[:, kblocks[b], kv * 64:(kv + 1) * 64],
                   b == 0, b == nblk - 1)
            ts(P, "dve", YO[:, h * 64:(h + 1) * 64], po[:, h * 64:(h + 1) * 64], ST[:, 5:6], ALU.mult)
        pf = C.ps[t % 2]
        for a in range(2):
            tr(P, pf[:, a * 128:(a + 1) * 128], YO[:, a * 128:(a + 1) * 128], C.ident)
        OC = oc[t % 2]
        cp(P, "act", OC, pf[:, 0:256].rr("p (a n) -> p a n", a=2))
        dma(P, "sp", V(C.YB_ap[2][:, t * 128:(t + 1) * 128].rearrange("(a p) n -> p a n", p=128), C.YB_bufs[2][t]), OC)
    A.close()


PI = math.pi
S5_BLOCKS = [(0, 256)] + [(256 + 512 * i, 512) for i in range(8)]


I32 = mybir.dt.int32
TWO_PI_HI = 6.28125
TWO_PI_LO = 2.0 * math.pi - 6.28125


def sincos(P, A, s_out, c_out, x, shape, tag):
    qi = A.sb("qi" + tag, shape, I32)
    kf = A.sb("kf" + tag, shape)
    r = A.sb("rr" + tag, shape)
    m = A.sb("mm" + tag, shape)
    for extra, out in ((0.0, s_out), (0.5 * PI, c_out)):
        ts(P, "dve", r, x, 16.0 * PI + extra, ALU.add)
        ts(P, "dve", qi, r, 1.0 / (2.0 * PI), ALU.mult)
        cp(P, "dve", kf, qi)
        stt(P, "dve", r, kf, -TWO_PI_HI, r, ALU.mult, ALU.add)
        stt(P, "dve", r, kf, -TWO_PI_LO, r, ALU.mult, ALU.add)
        ts(P, "dve", m, r, PI, ALU.is_gt)
        stt(P, "dve", r, m, -2.0 * PI, r, ALU.mult, ALU.add)
        ts(P, "dve", m, r, -PI, ALU.is_lt)
        stt(P, "dve", r, m, 2.0 * PI, r, ALU.mult, ALU.add)
        ts(P, "dve", r, r, PI, ALU.min, -PI, ALU.max)
        act(P, out, r, AF.Sin)


def phase_s5(C, l, L):
    P, I = C.P, C.I
    A = Arena(P)
    th_s = A.sb("th_s", [128, 2, 8])
    rho_s = A.sb("rho_s", [128, 2, 8])
    Ck = A.sb("Ck", [128, 2, 8, 13])
    Sk = A.sb("Sk", [128, 2, 8, 13])
    BbT = A.sb("BbT", [128, 2, 2, 8, 128], BF16)
    CT = A.sb("CT", [128, 2, 2, 8, 128], BF16)
    A0 = A
    A = Arena(P)
    tmpA = [A.sb("s5r%d" % i, [128, 1024]) for i in range(8)]
    lre, lim, ldt, t_s, t_c, t_a, t_b, t_d = tmpA
    pw2f = A.sb("pw2", [128, 16])
    dma(P, "sp", pw2f, I["pw2"][0:1, :].bc([128, 16]))
    pw2 = pw2f[:, 0:13]
    fR = [A.sb("fR%d" % d, [128, 1024]) for d in range(2)]
    fI = [A.sb("fI%d" % d, [128, 1024]) for d in range(2)]
    sm = A.sb("sm", [128, 2, 3, 8])
    dma(P, "sp", sm, I["s5_sm"][l])
    act(P, sm[:, :, 2, :], sm[:, :, 2, :], AF.Exp)
    tt(P, "dve", th_s, sm[:, :, 1, :], sm[:, :, 2, :], ALU.mult)
    tt(P, "dve", rho_s, sm[:, :, 0, :], sm[:, :, 2, :], ALU.mult)
    act(P, rho_s, rho_s, AF.Exp)
    ang13 = A.sb("ang13", [128, 16, 13])
    tt(P, "dve", ang13, th_s.rr("p d j -> p (d j)")[:, :, None].bc([128, 16, 13]), pw2[:, None, :].bc([128, 16, 13]), ALU.mult)
    sincos(P, A, Sk.rr("p d j k -> p (d j) k"), Ck.rr("p d j k -> p (d j) k"), ang13, [128, 16, 13], "k")
    for d in range(2):
        dma(P, "sp", lre, I["s5_rows"][l, d, 0:1, :].bc([128, 1024]))
        dma(P, "pool", lim, I["s5_rows"][l, d, 1:2, :].bc([128, 1024]))
        dma(P, "sp", ldt, I["s5_rows"][l, d, 2:3, :].bc([128, 1024]))
        act(P, ldt, ldt, AF.Exp)
        tt(P, "dve", t_a, lim, ldt, ALU.mult)
        sincos(P, A, t_s, t_c, t_a, [128, 1024], "r%d" % d)
        tt(P, "dve", t_a, lre, ldt, ALU.mult)
        act(P, t_a, t_a, AF.Exp)
        tt(P, "dve", t_c, t_c, t_a, ALU.mult)
        tt(P, "dve", t_s, t_s, t_a, ALU.mult)
        ts(P, "dve", t_c, t_c, -1.0, ALU.add)
        tt(P, "dve", t_a, lre, lre, ALU.mult)
        tt(P, "pool", t_b, lim, lim, ALU.mult)
        tt(P, "dve", t_a, t_a, t_b, ALU.add)
        P.op("dve", "reciprocal", out=t_a, in_=t_a)
        tt(P, "dve", t_b, t_c, lre, ALU.mult)
        tt(P, "pool", t_d, t_s, lim, ALU.mult)
        tt(P, "dve", t_b, t_b, t_d, ALU.add)
        tt(P, "dve", fR[d], t_b, t_a, ALU.mult)
        tt(P, "dve", t_b, t_s, lre, ALU.mult)
        tt(P, "pool", t_d, t_c, lim, ALU.mult)
        tt(P, "dve", t_b, t_b, t_d, ALU.subtract)
        tt(P, "dve", fI[d], t_b, t_a, ALU.mult)
    bt_f = A.sb("bt_f", [128, 2, 8, 128])
    dma(P, "sp", bt_f[:, 0], I["s5_bt"][l, 0].rr("j c s -> c j s"))
    dma(P, "pool", bt_f[:, 1], I["s5_bt"][l, 1].rr("j c s -> c j s"))
    for d in range(2):
        fr = fR[d].rr("p (j s) -> p j s", j=8)
        fi = fI[d].rr("p (j s) -> p j s", j=8)
        ta = t_a.rr("p (j s) -> p j s", j=8)
        tb = t_b.rr("p (j s) -> p j s", j=8)
        tt(P, "dve", ta, bt_f[:, 0], fr, ALU.mult)
        tt(P, "pool", tb, bt_f[:, 1], fi, ALU.mult)
        tt(P, "dve", BbT[:, d, 0], ta, tb, ALU.subtract)
        tt(P, "dve", ta, bt_f[:, 0], fi, ALU.mult)
        tt(P, "pool", tb, bt_f[:, 1], fr, ALU.mult)
        tt(P, "dve", BbT[:, d, 1], ta, tb, ALU.add)
        for ri in range(2):
            ctf = t_c if ri == 0 else t_d
            dma(P, "sp" if ri == 0 else "pool", ctf.rr("p (j c) -> p j c", j=8), I["s5_ct"][l, d, ri].rr("j s c -> s j c"))
            if ri == 0:
                cp(P, "pool", CT[:, d, 0], ctf.rr("p (j c) -> p j c", j=8))
            else:
                ts(P, "pool", CT[:, d, 1], ctf.rr("p (j c) -> p j c", j=8), -1.0, ALU.mult)
    A.close()
    A = A0
    cut = C.dbg.get("s5_cut", 99)
    if cut <= 1:
        A.close(); return
    if not C.dbg.get("s5_small"):
        ct, sn = A.sb("ct", [128, NTOK]), A.sb("sn", [128, NTOK])
        w_re, w_im = A.sb("w_re", [128, NTOK]), A.sb("w_im", [128, NTOK])
    uB = A.sb("uB", [128, 2, NTOK], BF16)
    yacc = A.sb("yacc", [128, 2, NTOK])
    x_re, x_im = A.sb("x_re", [128, NTOK], BF16), A.sb("x_im", [128, NTOK], BF16)
    dsk = A.sb("dsk", [128, 16])
    bgl = A.sb("bgl", [128, 16])
    dma(P, "sp", dsk, I["s5_d_fm"][l])
    dma(P, "sp", bgl, I["s5_bglu_fm"][l])
    wglu = load_bf16(P, A, "wglu", [128, 2, 256], I["s5_w_glu"][l].rr("(k p) n -> p k n", p=128))
    tmA = [[A.sb("tm%d_%d" % (b, i), [128, 512]) for i in range(4)] for b in range(2)]
    ut = [tmA[1][i][:, 0:256] for i in range(2)]
    var = C.dbg.get("s5_var", 9)
    for t in range(NT if var > 0 else 0):
        i = t % 2
        r0 = urow(t)
        dma(P, "sp" if i == 0 else "pool", ut[i], V(C.U_ap[r0:r0 + 128, O3:O4], C.U_bufs[t]))
        pp = C.ps[i]
        for a in range(2):
            tr(P, pp[:, a * 128:(a + 1) * 128], ut[i][:, a * 128:(a + 1) * 128], C.ident)
        if var < 2:
            continue
        for a in range(2):
            cp(P, "dve", uB[:, a, t * 128:(t + 1) * 128], pp[:, a * 128:(a + 1) * 128])
        for a in range(2):
            ts(P, "dve", yacc[:, a, t * 128:(t + 1) * 128], pp[:, a * 128:(a + 1) * 128], dsk[:, a:a + 1], ALU.mult)
    tm = tmA[0]
    if cut <= 2:
        A.close(); return
    nb = 0
    for d in range(2):
        for j in range(8):
            jt = j // 4
            th = th_s[:, d, j:j + 1]
            P.op("dve", "memset", ap=ct[:, 0:1], constant=1.0)
            P.op("dve", "memset", ap=sn[:, 0:1], constant=0.0)
            k = 0
            n = 1
            while n < NTOK:
                m = min(n, NTOK - n)
                ck, sk = Ck[:, d, j, k:k + 1], Sk[:, d, j, k:k + 1]
                e1, e2 = ("dve", "pool") if m >= 256 else ("dve", "dve")
                ts(P, e1, ct[:, n:n + m], ct[:, 0:m], ck, ALU.mult)
                ts(P, e2, sn[:, n:n + m], sn[:, 0:m], ck, ALU.mult)
                ts(P, e2, tm[0][:, 0:min(m, 512)] if m <= 512 else w_re[:, 0:m], sn[:, 0:m], sk, ALU.mult)
                ts(P, e1, tm[1][:, 0:min(m, 512)] if m <= 512 else w_im[:, 0:m], ct[:, 0:m], sk, ALU.mult)
                ta_ = tm[0][:, 0:m] if m <= 512 else w_re[:, 0:m]
                tb_ = tm[1][:, 0:m] if m <= 512 else w_im[:, 0:m]
                tt(P, e1, ct[:, n:n + m], ct[:, n:n + m], ta_, ALU.subtract)
                tt(P, e2, sn[:, n:n + m], sn[:, n:n + m], tb_, ALU.add)
                n += m
                k += 1
            if cut <= 3:
                A.close(); return
            for bi, (t0, n) in enumerate(S5_BLOCKS):
                if d == 0:
                    rhs = uB[:, jt, t0:t0 + n]
                else:
                    last = (255 - t0) if t0 < 256 else (4607 - t0)
                    rhs = cust(uB, jt * NTOK + last, [(-1, n)])
                pr, pi_ = C.ps[(nb % 2) * 2], C.ps[(nb % 2) * 2 + 1]
                nb += 1
                mm(P, pr[:, 0:n], BbT[:, d, 0, j, :], rhs, True, True)
                mm(P, pi_[:, 0:n], BbT[:, d, 1, j, :], rhs, True, True)
                c_, s_ = ct[:, t0:t0 + n], sn[:, t0:t0 + n]
                tm = tmA[bi % 2]
                tt(P, "dve", tm[0][:, 0:n], pr[:, 0:n], c_, ALU.mult)
                tt(P, "dve", tm[1][:, 0:n], pi_[:, 0:n], s_, ALU.mult)
                tt(P, "pool", w_re[:, t0:t0 + n], tm[0][:, 0:n], tm[1][:, 0:n], ALU.add)
                tt(P, "dve", tm[2][:, 0:n], pi_[:, 0:n], c_, ALU.mult)
                tt(P, "dve", tm[3][:, 0:n], pr[:, 0:n], s_, ALU.mult)
                tt(P, "pool", w_im[:, t0:t0 + n], tm[2][:, 0:n], tm[3][:, 0:n], ALU.subtract)
            if cut <= 4:
                A.close(); return
            rb = rho_s[:, d, j:j + 1].bc([128, NTOK])
            P.op("dve", "tensor_tensor_scan", out=w_re, data0=rb, data1=w_re, initial=0.0, op0=ALU.mult, op1=ALU.add)
            P.op("dve", "tensor_tensor_scan", out=w_im, data0=rb, data1=w_im, initial=0.0, op0=ALU.mult, op1=ALU.add)
            if cut <= 5:
                A.close(); return
            for bi, (t0, n) in enumerate(S5_BLOCKS):
                c_, s_ = ct[:, t0:t0 + n], sn[:, t0:t0 + n]
                tm = tmA[bi % 2]
                tt(P, "pool", tm[0][:, 0:n], w_re[:, t0:t0 + n], c_, ALU.mult)
                tt(P, "pool", tm[1][:, 0:n], w_im[:, t0:t0 + n], s_, ALU.mult)
                tt(P, "dve", x_re[:, t0:t0 + n], tm[0][:, 0:n], tm[1][:, 0:n], ALU.subtract)
                tt(P, "dve", tm[2][:, 0:n], w_re[:, t0:t0 + n], s_, ALU.mult)
                tt(P, "dve", tm[3][:, 0:n], w_im[:, t0:t0 + n], c_, ALU.mult)
                tt(P, "pool", x_im[:, t0:t0 + n], tm[2][:, 0:n], tm[3][:, 0:n], ALU.add)
                py = C.ps[4 + (bi % 2)]
                if d == 0:
                    xr, xi = x_re[:, t0:t0 + n], x_im[:, t0:t0 + n]
                    k0 = t0
                else:
                    k0 = (256 - t0 - n) if t0 < 256 else (4608 - t0 - n)
                    s_last = t0 + n - 1
                    xr, xi = cust(x_re, s_last, [(-1, n)]), cust(x_im, s_last, [(-1, n)])
                mm(P, py[:, 0:n], CT[:, d, 0, j, :], xr, True, False)
                mm(P, py[:, 0:n], CT[:, d, 1, j, :], xi, False, True)
                tt(P, "dve", yacc[:, jt, k0:k0 + n], yacc[:, jt, k0:k0 + n], py[:, 0:n], ALU.add)
    if cut <= 7:
        A.close(); return
    glb = uB
    tm = tmA[0]
    ob = [x_re[:, 0:1024].rr("p (a n) -> p a n", a=2), x_im[:, 0:1024].rr("p (a n) -> p a n", a=2)]
    for bi, (t0, n) in enumerate(S5_BLOCKS):
        for a in range(2):
            y = yacc[:, a, t0:t0 + n]
            tt(P, "pool", tm[0][:, 0:n], y, y, ALU.mult)
            ts(P, "dve", tm[0][:, 0:n], tm[0][:, 0:n], 0.044715, ALU.mult, 1.0, ALU.add)
            tt(P, "pool", tm[0][:, 0:n], tm[0][:, 0:n], y, ALU.mult)
            act(P, tm[0][:, 0:n], tm[0][:, 0:n], AF.Sigmoid, scale=1.5957691216057308)
            tt(P, "dve", y, y, tm[0][:, 0:n], ALU.mult)
            cp(P, "pool", glb[:, a, t0:t0 + n], y)
        OB = ob[bi % 2]
        for a in range(2):
            pz = C.ps[6 + a]
            for kt in range(2):
                mm(P, pz[:, 0:n], wglu[:, kt, a * 128:(a + 1) * 128], glb[:, kt, t0:t0 + n], kt == 0, kt == 1)
            act(P, tm[1 + a][:, 0:n], pz[:, 0:n], AF.Sigmoid, bias=bgl[:, a:a + 1])
            tt(P, "dve", OB[:, a, 0:n], yacc[:, a, t0:t0 + n], tm[1 + a][:, 0:n], ALU.mult)
        tl = [t for t in range(NT) if t * 128 >= t0 and t * 128 < t0 + n]
        dma(P, "sp", V(C.YB_ap[3][:, t0:t0 + n].rearrange("(a p) n -> p a n", p=128), tuple(C.YB_bufs[3][t] for t in tl)),
            OB[:, :, 0:n])
    A.close()


TOKBLK = [(0, 256)] + [(256 + 512 * i, 512) for i in range(8)]


def phase_win_gates(C, l, L, hfm):
    P, I = C.P, C.I
    A = Arena(P)
    wst = [A.sb("wgs%d" % i, [128, 8, 512]) for i in range(2)]
    wb = [A.sb("wgb%d" % i, [128, 8, 512], BF16) for i in range(2)]
    gst = [A.sb("gst%d" % i, [128, 512], BF16) for i in range(4)]
    wv = I["w_in"][l].rr("(k p) n -> p k n", p=128)
    n = 0
    for cb in range(8):
        c0 = O4 + cb * 512
        dma(P, "sp" if cb % 2 == 0 else "pool", wst[cb % 2], wv[:, :, c0:c0 + 512])
        cp(P, "act" if cb % 2 == 0 else "dve", wb[cb % 2], wst[cb % 2])
        w = wb[cb % 2]
        for mi in range(4):
            row0 = cb * 512 + mi * 128
            for bi, (t0, nn) in enumerate(TOKBLK):
                ps = C.ps[n % 4]
                g = gst[n % 4]
                for k in range(8):
                    mm(P, ps[:, 0:nn], w[:, k, mi * 128:(mi + 1) * 128], hfm[:, k, t0:t0 + nn], k == 0, k == 7)
                cp(P, "act" if n % 2 == 0 else "dve", g[:, 0:nn], ps[:, 0:nn])
                dma(P, "sp" if n % 2 == 0 else "pool", V(C.Gt_ap[row0:row0 + 128, t0:t0 + nn], C.Gt_bufs[bi]), g[:, 0:nn])
                n += 1
    A.close()


def phase_merge(C, l, L):
    P, I = C.P, C.I
    A = Arena(P)
    wbr = A.sb("wbr", [128, 4, 2, 1024], BF16)
    wout = A.sb("wout", [128, 8, 1024], BF16)
    wst = A.sb("wmst", [128, 8, 1024])
    for i in range(4):
        dma(P, "sp", wst[:, 0:2, :], I["w_branch"][l, i].rr("(k p) n -> p k n", p=128))
        cp(P, "act", wbr[:, i], wst[:, 0:2, :])
    dma(P, "sp", wst, I["w_out"][l].rr("(k p) n -> p k n", p=128))
    cp(P, "act", wout, wst)
    yb = [A.sb("myb%d" % i, [128, 4, 2, 512], BF16) for i in range(2)]
    gt = [A.sb("mgt%d" % i, [128, 512], BF16) for i in range(4)]
    sg = [A.sb("msg%d" % i, [128, 512]) for i in range(2)]
    tmp = A.sb("mtmp", [128, 512])
    acc = A.sb("macc", [128, 512])
    mg = [A.sb("mmg%d" % i, [128, 8, 512], BF16) for i in range(2)]
    xt = [A.sb("mxt%d" % i, [128, 1024]) for i in range(2)]
    tm2 = A.sb("mtm2", [128, 1024])
    n = 0
    nx = 0
    for bi, (t0, nn) in enumerate(TOKBLK):
        YB = yb[bi % 2]
        tl = [t for t in range(NT) if t0 <= t * 128 < t0 + nn]
        for i in range(4):
            dma(P, "sp" if i % 2 == 0 else "pool", YB[:, i, :, 0:nn],
                V(C.YB_ap[i][:, t0:t0 + nn].rearrange("(a p) n -> p a n", p=128), tuple(C.YB_bufs[i][t] for t in tl)))
        MG = mg[bi % 2]
        for m in range(8):
            for i in range(4):
                g = gt[n % 4]
                dma(P, "sp" if n % 2 == 0 else "pool", g[:, 0:nn],
                    V(C.Gt_ap[i * 1024 + m * 128:i * 1024 + (m + 1) * 128, t0:t0 + nn], C.Gt_bufs[bi]))
                s_ = sg[n % 2]
                act(P, s_[:, 0:nn], g[:, 0:nn], AF.Sigmoid)
                ps = C.ps[n % 4]
                n += 1
                for kt in range(2):
                    mm(P, ps[:, 0:nn], wbr[:, i, kt, m * 128:(m + 1) * 128], YB[:, i, kt, 0:nn], kt == 0, kt == 1)
                if i == 0:
                    tt(P, "dve", acc[:, 0:nn], ps[:, 0:nn], s_[:, 0:nn], ALU.mult)
                elif i < 3:
                    tt(P, "dve", tmp[:, 0:nn], ps[:, 0:nn], s_[:, 0:nn], ALU.mult)
                    tt(P, "pool", acc[:, 0:nn], acc[:, 0:nn], tmp[:, 0:nn], ALU.add)
                else:
                    tt(P, "dve", tmp[:, 0:nn], ps[:, 0:nn], s_[:, 0:nn], ALU.mult)
                    tt(P, "dve", MG[:, m, 0:nn], acc[:, 0:nn], tmp[:, 0:nn], ALU.add)
        for ti, t in enumerate(tl):
            j = 1 if t < 2 else 0
            x = xt[nx % 2]
            nx += 1
            dma(P, "sp", x, xsrc(C, l, t))
            for half in range(2):
                po = C.ps[4 + half + 2 * (nx % 2)]
                for k in range(8):
                    mm(P, po, MG[:, k, ti * 128:(ti + 1) * 128], wout[:, k, half * 512:(half + 1) * 512], k == 0, k == 7)
                tt(P, "dve", tm2[:, half * 512:(half + 1) * 512], po, L.grow[0][j][:, half * 512:(half + 1) * 512], ALU.mult)
            tt(P, "pool", x, x, tm2, ALU.add)
            dma(P, "pool", C.xres[t], x)
    A.close()
    C.x_in_scratch = True


def make_router(C, l, L, RA):
    P, I = C.P, C.I
    wr = RA.sb("wr", [128, 8, 36])
    brow = RA.sb("brow", [128, 36])
    dma(P, "sp", wr, I["w_router"][l].rr("(k p) n -> p k n", p=128))
    dma(P, "sp", brow, I["b_router"][l:l + 1, :].bc([128, 36]))
    lg = RA.sb("lg", [128, 36])
    st = RA.sb("rst", [128, 16])
    oh = RA.sb("roh", [128, 4])
    em = RA.sb("rem", [128, 32])
    em2 = RA.sb("rem2", [128, 32])
    oh1 = RA.sb("roh1", [128, 32])
    oh2 = RA.sb("roh2", [128, 32])
    wg = RA.sb("rwg", [128, 32])
    junk = RA.sb("rjunk", [128, 4])

    def per_tile(t, hf):
        pl = C.ps[6]
        for k in range(8):
            mm(P, pl[:, 0:36], hf[:, k, :], wr[:, k, :], k == 0, k == 7)
        tt(P, "dve", lg, pl[:, 0:36], brow, ALU.add)
        g, e = lg[:, 0:4], lg[:, 4:36]
        P.op("dve", "tensor_reduce", out=st[:, 0:1], in_=g, axis=AX.X, op=ALU.max)
        ts(P, "dve", oh, g, st[:, 0:1], ALU.is_equal)
        ts(P, "dve", st[:, 1:2], st[:, 0:1], -1.0, ALU.mult)
        act(P, junk, g, AF.Exp, bias=st[:, 1:2], accum_out=st[:, 2:3])
        P.op("dve", "reciprocal", out=st[:, 3:4], in_=st[:, 2:3])
        ts(P, "dve", oh, oh, 1e30, ALU.mult, -1e30, ALU.add)
        tt(P, "dve", em.rr("p (g k) -> p g k", g=4), e.rr("p (g k) -> p g k", g=4),
           oh[:, :, None].bc([128, 4, 8]), ALU.add)
        P.op("dve", "tensor_reduce", out=st[:, 4:5], in_=em, axis=AX.X, op=ALU.max)
        ts(P, "dve", oh1, em, st[:, 4:5], ALU.is_equal)
        stt(P, "dve", em2, oh1, -1e30, em, ALU.mult, ALU.add)
        P.op("dve", "tensor_reduce", out=st[:, 5:6], in_=em2, axis=AX.X, op=ALU.max)
        ts(P, "dve", oh2, em2, st[:, 5:6], ALU.is_equal)
        tt(P, "dve", st[:, 6:7], st[:, 5:6], st[:, 4:5], ALU.subtract)
        act(P, st[:, 7:8], st[:, 6:7], AF.Exp)
        ts(P, "dve", st[:, 8:9], st[:, 7:8], 1.0, ALU.add)
        P.op("dve", "reciprocal", out=st[:, 9:10], in_=st[:, 8:9])
        tt(P, "dve", st[:, 10:11], st[:, 7:8], st[:, 9:10], ALU.mult)
        ts(P, "dve", wg, oh1, st[:, 9:10], ALU.mult)
        stt(P, "dve", wg, oh2, st[:, 10:11], wg, ALU.mult, ALU.add)
        ts(P, "dve", wg, wg, st[:, 3:4], ALU.mult)
        pt = C.ps[7]
        tr(P, pt[0:32, 0:128], wg, C.ident)
        cp(P, "dve", L.WT[:, t * 128:(t + 1) * 128], pt[0:32, 0:128])
    return per_tile


MOE_GROUPS = [list(range(0, 12)), list(range(12, 24)), list(range(24, 34))]


def phase_moe(C, l, L, H2, last):
    P, I = C.P, C.I
    A = Arena(P)
    hg = A.sb("hg", [128, 8, 12 * 128], BF16)
    wst = [A.sb("ews%d" % i, [128, 4, 512]) for i in range(2)]
    wgu = [A.sb("wgu%d" % i, [128, 2, 8, 512], BF16) for i in range(2)]
    wd = [A.sb("wd%d" % i, [128, 4, 1024], BF16) for i in range(2)]
    yacc = A.sb("eyacc", [128, 12, 1024])
    hid = [A.sb("hid%d" % i, [128, 4, 512], BF16) for i in range(2)]
    sil = [A.sb("sil%d" % i, [128, 512], BF16) for i in range(2)]
    tu = [A.sb("etu%d" % i, [128, 512]) for i in range(2)]
    xt = [A.sb("ext%d" % i, [128, 1024]) for i in range(2)]
    tm2 = A.sb("etm2", [128, 1024])
    ne = 0
    nst = 0
    nb = 0
    for G in MOE_GROUPS:
        tiles = [t for t in G if not (last and t < 2)]
        if not tiles:
            continue
        blocks = [tiles[i:i + 4] for i in range(0, len(tiles), 4)]
        g0 = tiles[0] * 128
        gn = len(tiles) * 128
        dma(P, "sp", hg[:, :, 0:gn], H2[:, :, g0:g0 + gn])
        for e in range(32):
            WGU, WD = wgu[ne % 2], wd[ne % 2]
            ne += 1
            for gi, nm in enumerate(("w_exp_gate", "w_exp_up")):
                src = I[nm][l, e].rr("(k p) n -> p k n", p=128)
                for hf_ in range(2):
                    s_ = wst[nst % 2]
                    dma(P, "sp" if nst % 2 == 0 else "pool", s_, src[:, hf_ * 4:(hf_ + 1) * 4, :])
                    cp(P, "act", WGU[:, gi, hf_ * 4:(hf_ + 1) * 4, :], s_)
                    nst += 1
            srcd = I["w_exp_down"][l, e].rr("(k p) n -> p k n", p=128)
            for hf_ in range(2):
                s_ = wst[nst % 2]
                dma(P, "sp" if nst % 2 == 0 else "pool", s_.rr("p k n -> p (k n)").rr("p (k n) -> p k n", k=2), srcd[:, hf_ * 2:(hf_ + 1) * 2, :])
                cp(P, "dve", WD[:, hf_ * 2:(hf_ + 1) * 2, :], s_.rr("p k n -> p (k n)").rr("p (k n) -> p k n", k=2))
                nst += 1
            for blk in blocks:
                t0 = blk[0] * 128
                nn = len(blk) * 128
                HID = hid[nb % 2]
                psW = C.ps[0]
                mm(P, psW[:, 0:nn], C.ident[0:32, e:e + 1].bc([32, 128]), L.WT[:, t0:t0 + nn], True, True)
                for f in range(4):
                    i2 = (nb * 4 + f) % 2
                    psG, psU = C.ps[1 + 2 * i2], C.ps[2 + 2 * i2]
                    for k in range(8):
                        mm(P, psG[:, 0:nn], WGU[:, 0, k, f * 128:(f + 1) * 128], hg[:, k, t0 - g0:t0 - g0 + nn], k == 0, k == 7)
                    for k in range(8):
                        mm(P, psU[:, 0:nn], WGU[:, 1, k, f * 128:(f + 1) * 128], hg[:, k, t0 - g0:t0 - g0 + nn], k == 0, k == 7)
                    act(P, sil[i2][:, 0:nn], psG[:, 0:nn], AF.Silu)
                    tt(P, "dve", tu[i2][:, 0:nn], psU[:, 0:nn], sil[i2][:, 0:nn], ALU.mult)
                    tt(P, "dve", HID[:, f, 0:nn], tu[i2][:, 0:nn], psW[:, 0:nn], ALU.mult)
                for ti, t in enumerate(blk):
                    ya = yacc[:, t - tiles[0], :]
                    for half in range(2):
                        po = C.ps[5 + (nb * 8 + ti * 2 + half) % 3]
                        for f in range(4):
                            mm(P, po, HID[:, f, ti * 128:(ti + 1) * 128], WD[:, f, half * 512:(half + 1) * 512], f == 0, f == 3)
                        if e == 0:
                            cp(P, "dve", ya[:, half * 512:(half + 1) * 512], po)
                        else:
                            tt(P, "dve", ya[:, half * 512:(half + 1) * 512], ya[:, half * 512:(half + 1) * 512], po, ALU.add)
                nb += 1
        for t in tiles:
            j = 1 if t < 2 else 0
            x = xt[t % 2]
            dma(P, "sp", x, C.xres[t])
            tt(P, "dve", tm2, yacc[:, t - tiles[0], :], L.grow[1][j], ALU.mult)
            tt(P, "pool", x, x, tm2, ALU.add)
            dma(P, "pool", C.xres[t], x)
    A.close()


def final_norm(C):
    P, I = C.P, C.I
    A = Arena(P)
    g = A.sb("fg", [128, D])
    dma(P, "sp", g, I["final_g"][0:1, :].bc([128, D]))
    xt = [A.sb("fxt%d" % i, [128, D]) for i in range(2)]
    junk = A.sb("fjunk", [128, D])
    st = [A.sb("fst%d" % i, [128, 2]) for i in range(2)]
    for t in range(2, NT):
        x, s = xt[t % 2], st[t % 2]
        dma(P, "sp" if t % 2 == 0 else "pool", x, C.xres[t])
        act(P, junk, x, AF.Square, accum_out=s[:, 0:1])
        ts(P, "dve", s[:, 1:2], s[:, 0:1], 1.0 / D, ALU.mult, EPS, ALU.add)
        act(P, s[:, 1:2], s[:, 1:2], AF.Sqrt)
        P.op("dve", "reciprocal", out=s[:, 1:2], in_=s[:, 1:2])
        stt(P, "dve", x, x, s[:, 1:2], g, ALU.mult, ALU.mult)
        dma(P, "sp" if t % 2 == 1 else "pool", V(C.out.ap[(t - 2) * 128:(t - 1) * 128, :], Buf("o%d" % t)), x)
    A.close()


CH_COLS = 3072


def alloc_chunk_heads(A, dd):
    def mkh(name, shape, dt=F32):
        return [A.sb("%s_%d_%d" % (name, dd, h), shape, dt) for h in range(4)]
    AM = mkh("cAM", [128, 4, 128], BF16)
    XB = mkh("cXB", [128, 2, 128], BF16)
    XX = [[AM[h][:, 0:2, :] for h in range(4)], [XB[h] for h in range(4)]]
    AakT = [AM[h][:, 2, :] for h in range(4)]
    ArbT = [AM[h][:, 3, :] for h in range(4)]
    ArkT, TT = [mkh("cM%d" % i, [128, 128], BF16) for i in range(2)]
    PM = mkh("cPM", [128, 2, 64], BF16)
    Ap = [PM[h][:, 0, :] for h in range(4)]
    M1 = [PM[h][:, 1, :] for h in range(4)]
    U0 = mkh("cU0", [128, 64], BF16)
    RpT = mkh("cRpT", [64, 128])
    DPC = mkh("cDPC", [128, 64])
    Y0cc = mkh("cY0cc", [64, 2, 64])
    Y0c = [[Y0cc[h][:, c, :] for h in range(4)] for c in range(2)]
    GH = mkh("cGH", [64, 4, 64])
    GT = [[GH[h][:, 2 * c, :] for h in range(4)] for c in range(2)]
    Hc = [[GH[h][:, 2 * c + 1, :] for h in range(4)] for c in range(2)]
    gA = mkh("gA", [128, 128])
    gY0 = [mkh("gY0%d" % c, [64, 64]) for c in range(2)]
    gH = [mkh("gH%d" % c, [32, 64]) for c in range(2)]
    return (AM, XB, XX, AakT, ArbT, ArkT, TT, PM, Ap, M1, U0, RpT, DPC, Y0cc, Y0c, GH, GT, Hc, gA, gY0, gH)


def phase_chunk(C, l, L):
    P, I = C.P, C.I
    A = Arena(P)
    mk = A.sb("cmasks", [128, 7, 128])
    dma(P, "sp", mk, I["cmasks"])
    idn = C.ident
    chs = [A.sb("chs%d" % i, [128, CH_COLS]) for i in range(2)]
    ST = [[A.sb("cST%d%d" % (d, h), [64, 64]) for h in range(4)] for d in range(2)]
    for d in range(2):
        for h in range(4):
            P.op("dve", "memset", ap=ST[d][h], constant=0.0)

    def mk2(name, shape, dt=F32):
        return [A.sb("%s%d" % (name, i), shape, dt) for i in range(2)]
    TOT, incS, Ein, Enin, Eex, Eend, Etot, tmpx, tmpy = [mk2("cE%d" % i, [128, 256]) for i in range(9)]
    at, rt, bt, kt, bh, kh = [mk2("cq%d" % i, [128, 256]) for i in range(6)]
    aT, rTb, bT, kT = [mk2("cT%d" % i, [128, 2, 128], BF16) for i in range(4)]
    rT = mk2("cTr", [128, 2, 128])
    bhc = [mk2("cbhc%d" % c, [128, 256], BF16) for c in range(2)]
    khc = [mk2("ckhc%d" % c, [128, 256], BF16) for c in range(2)]
    at_b = mk2("cat_b", [128, 256], BF16)
    v_b = mk2("cv_b", [128, 256], BF16)
    IM = A.sb("cIM", [128, 64])
    tt(P, "pool", IM, idn[:, 0:64], idn[:, 64:128], ALU.add)

    HB = []
    for dd in range(2):
        HB.append(alloc_chunk_heads(A, dd))
    MK4 = [A.sb("cMK4%d" % d, [128, 4, 128]) for d in range(2)]
    for d in range(2):
        ms_, mst_, mit_ = (0, 1, 2) if d == 0 else (3, 4, 5)
        for i_, mi_ in enumerate((ms_, mst_, mst_, mit_)):
            cp(P, "pool", MK4[d][:, i_, :], mk[:, mi_, :])
    yo = [A.sb("cyo%d" % i, [64, 2, 256]) for i in range(2)]
    STg = [[A.sb("gST%d%d" % (d, h), [32, 64]) for h in range(4)] for d in range(2)]
    for d in range(2):
        for h in range(4):
            P.op("dve", "memset", ap=STg[d][h], constant=0.0)
    gTOT, gincS, gEin, gEnin, gEend, gEtot, gtmp = [mk2("gE%d" % i, [128, 128]) for i in range(7)]
    gq, gk, gkh = [mk2("gq%d" % i, [128, 128]) for i in range(3)]
    gkhc = [mk2("gkhc%d" % c, [128, 128]) for c in range(2)]
    gqT, gkT, gPT = [[mk2("gT%d_%d" % (i, h), [32, 128]) for h in range(4)] for i in range(3)]
    gyo = [A.sb("gyo%d" % i, [64, 2, 256]) for i in range(2)]
    ps = C.ps
    border = [1, 0] + list(range(NT - 1, 1, -1))
    H4 = range(4)
    it = 0
    cut = C.dbg.get("chunk_cut", 99)

    def body(n, d):
        if True:
            (AM, XB, XX, AakT, ArbT, ArkT, TT, PM, Ap, M1, U0, RpT, DPC, Y0cc, Y0c, GH, GT, Hc, gA, gY0, gH) = HB[d]
            ps = C.ps[4 * d:4 * d + 4] + C.ps[4 - 4 * d:8 - 4 * d]
            t = n if d == 0 else border[n]
            q = d
            ch = chs[q]
            YO = yo[q]
            dma(P, "sp" if d == 0 else "pool", ch, V(C.CH_ap[t * 128:(t + 1) * 128, :], C.CH_bufs[t]))
            lw = ch[:, d * 256:(d + 1) * 256]
            ke = ch[:, 512 + d * 256:768 + d * 256]
            b_ = ch[:, 1024 + d * 256:1280 + d * 256]
            a_, r_, v_ = ch[:, 1536:1792], ch[:, 1792:2048], ch[:, 2048:2304]
            m_s, m_st, m_it = (0, 1, 2) if d == 0 else (3, 4, 5)
            pc = ps[4 + q]
            mm(P, pc[:, 0:256], mk[:, m_it, :], lw, True, True)
            mm(P, pc[:, 256:512], mk[:, 6, :], lw, True, True)
            cp(P, "dve", TOT[q], pc[:, 256:512])
            cp(P, "dve", incS[q], pc[:, 0:256])
            act(P, Ein[q], incS[q], AF.Exp)
            act(P, Enin[q], incS[q], AF.Exp, scale=-1.0)
            tt(P, "pool", tmpx[q], incS[q], lw, ALU.subtract)
            act(P, Eex[q], tmpx[q], AF.Exp)
            tt(P, "pool", tmpy[q], TOT[q], incS[q], ALU.subtract)
            act(P, Eend[q], tmpy[q], AF.Exp)
            act(P, Etot[q], TOT[q], AF.Exp)
            tt(P, "pool", at[q], a_, Eex[q], ALU.mult)
            cp(P, "act", at_b[q], at[q])
            cp(P, "act", v_b[q], v_)
            tt(P, "pool", rt[q], r_, Ein[q], ALU.mult)
            tt(P, "pool", bt[q], b_, Enin[q], ALU.mult)
            tt(P, "pool", kt[q], ke, Enin[q], ALU.mult)
            tt(P, "pool", bh[q], b_, Eend[q], ALU.mult)
            tt(P, "pool", kh[q], ke, Eend[q], ALU.mult)
            for c in range(2):
                ts(P, "pool", bhc[c][q], bh[q], mk[:, 6, c * 64:c * 64 + 1], ALU.mult)
                ts(P, "pool", khc[c][q], kh[q], mk[:, 6, c * 64:c * 64 + 1], ALU.mult)
            yield
            for qi, (src, dst) in enumerate(((at, aT), (rt, rT), (bt, bT), (kt, kT))):
                pb_ = ps[6 + qi % 2]
                for a2 in range(2):
                    tr(P, pb_[:, a2 * 128:(a2 + 1) * 128], src[q][:, a2 * 128:(a2 + 1) * 128], idn)
                cp(P, "dve", dst[q], pb_[:, 0:256].rr("p (a n) -> p a n", a=2))
                if qi == 1:
                    cp(P, "dve", rTb[q], pb_[:, 0:256].rr("p (a n) -> p a n", a=2))

            def hv(h):
                pair, hl = h // 2, h % 2
                return pair, slice(hl * 64, (hl + 1) * 64), slice(h * 64, (h + 1) * 64)
            if cut <= 2:
                return
            yield
            for h in H4:
                pair, hs, hc = hv(h)
                pA = ps[h]
                mm(P, pA[:, 0:128], aT[q][hs, pair, :], bT[q][hs, pair, :], True, True)
                mm(P, pA[:, 128:256], bT[q][hs, pair, :], aT[q][hs, pair, :], True, True)
                mm(P, pA[:, 256:384], kT[q][hs, pair, :], aT[q][hs, pair, :], True, True)
                mm(P, pA[:, 384:512], bT[q][hs, pair, :], rTb[q][hs, pair, :], True, True)
            for h in H4:
                pA = ps[h]
                tt(P, "dve", AM[h].rr("p a n -> p (a n)"), pA, MK4[d].rr("p a n -> p (a n)"), ALU.mult)
                tt(P, "pool", TT[h], AM[h][:, 1, :], idn, ALU.add)
            for h in H4:
                pair, hs, hc = hv(h)
                mm(P, ps[h][:, 0:128], kT[q][hs, pair, :], rTb[q][hs, pair, :], True, True)
            for h in H4:
                tt(P, "dve", ArkT[h], ps[h][:, 0:128], mk[:, m_it, :], ALU.mult)
            if cut <= 3:
                return
            yield
            for s in range(5):
                yield
                XXc, XXn = XX[s % 2], XX[(s + 1) % 2]
                for h in H4:
                    pX = ps[h]
                    mm(P, pX[:, 128:256], XXc[h][:, 1, :], XXc[h][:, 0, :], True, True)
                    if s < 4:
                        mm(P, pX[:, 256:384], XXc[h][:, 0, :], XXc[h][:, 1, :], True, True)
                for h in H4:
                    pX = ps[h]
                    if s < 4:
                        cp(P, "dve", XXn[h], pX[:, 128:384].rr("p (a n) -> p a n", a=2))
                    else:
                        cp(P, "dve", XXn[h][:, 0, :], pX[:, 128:256])
                for h in H4:
                    mm(P, ps[h][:, 384:512], XXn[h][:, 0, :], TT[h], True, True)
                for h in H4:
                    tt(P, "dve", TT[h], TT[h], ps[h][:, 384:512], ALU.add)
            if cut <= 4:
                return
            yield
            for h in H4:
                pair, hs, hc = hv(h)
                mm(P, ps[h][:, 0:64], TT[h], at_b[q][:, hc], True, True)
                mm(P, ps[h][:, 64:128], AakT[h], v_b[q][:, hc], True, True)
            for h in H4:
                cp(P, "dve", PM[h], ps[h][:, 0:128].rr("p (a n) -> p a n", a=2))
            for h in H4:
                mm(P, ps[h][:, 128:192], TT[h], M1[h], True, True)
            for h in H4:
                cp(P, "dve", U0[h], ps[h][:, 128:192])
            for h in H4:
                pair, hs, hc = hv(h)
                for c in range(2):
                    cs = slice(c * 64, (c + 1) * 64)
                    mm(P, ps[h][0:64, 192 + c * 64:256 + c * 64], ArbT[h][:, cs], U0[h], True, False)
                    mm(P, ps[h][0:64, 192 + c * 64:256 + c * 64], ArkT[h][:, cs], v_b[q][:, hc], False, True)
                mm(P, ps[h][0:64, 320:448], Ap[h], ArbT[h], True, True)
                tt(P, "pool", DPC[h], IM, Etot[q][:, hc], ALU.mult)
            for h in H4:
                pair, hs, hc = hv(h)
                cp(P, "dve", Y0cc[h], ps[h][0:64, 192:320].rr("p (a n) -> p a n", a=2))
                tt(P, "dve", RpT[h], ps[h][0:64, 320:448], rT[q][hs, pair, :], ALU.add)
            if cut <= 5:
                return
            for h in H4:
                pair, hs, hc = hv(h)
                pG = ps[h]
                for c in range(2):
                    cs = slice(c * 64, (c + 1) * 64)
                    o = c * 128
                    mm(P, pG[0:64, o:o + 64], Ap[h], bhc[c][q][:, hc], True, False)
                    mm(P, pG[0:64, o:o + 64], idn[:, cs], DPC[h], False, True)
                    mm(P, pG[0:64, o + 64:o + 128], bhc[c][q][:, hc], U0[h], True, False)
                    mm(P, pG[0:64, o + 64:o + 128], khc[c][q][:, hc], v_b[q][:, hc], False, True)
            for h in H4:
                pG = ps[h]
                cp(P, "dve", GH[h], pG[0:64, 0:256].rr("p (a n) -> p a n", a=4))
            if cut <= 6:
                return
            yield
            for c in ((0, 1) if d == 0 else (1, 0)):
                cs = slice(c * 64, (c + 1) * 64)
                for h in H4:
                    pG = ps[h]
                    S_ = ST[d][h]
                    mm(P, pG[0:64, 256:320], RpT[h][:, cs], S_, True, False)
                    mm(P, pG[0:64, 256:320], idn[0:64, 0:64], Y0c[c][h], False, True)
                    mm(P, pG[0:64, 320:384], GT[c][h], S_, True, False)
                    mm(P, pG[0:64, 320:384], idn[0:64, 0:64], Hc[c][h], False, True)
                for h in H4:
                    pair, hs, hc = hv(h)
                    pG = ps[h]
                    cp(P, "dve", YO[:, c, hc], pG[0:64, 256:320])
                    cp(P, "dve", ST[d][h], pG[0:64, 320:384])
            dma(P, "sp" if d == 0 else "pool",
                V(C.YT_ap[d][t * 128:(t + 1) * 128, :].rearrange("(c p) n -> p c n", p=64), C.YT_bufs[d][t]), YO)
            yield
            GYO = gyo[q]
            glw = ch[:, 2304 + d * 128:2432 + d * 128]
            gq_, gk_, gv_ = ch[:, 2560:2688], ch[:, 2688:2816], ch[:, 2816:3072]
            pcg = ps[4 + q]
            mm(P, pcg[:, 0:128], mk[:, m_it, :], glw, True, True)
            mm(P, pcg[:, 128:256], mk[:, 6, :], glw, True, True)
            cp(P, "dve", gincS[q], pcg[:, 0:128])
            cp(P, "dve", gTOT[q], pcg[:, 128:256])
            act(P, gEin[q], gincS[q], AF.Exp)
            act(P, gEnin[q], gincS[q], AF.Exp, scale=-1.0)
            tt(P, "pool", gtmp[q], gTOT[q], gincS[q], ALU.subtract)
            act(P, gEend[q], gtmp[q], AF.Exp)
            act(P, gEtot[q], gTOT[q], AF.Exp)
            tt(P, "pool", gq[q], gq_, gEin[q], ALU.mult)
            tt(P, "pool", gk[q], gk_, gEnin[q], ALU.mult)
            tt(P, "pool", gkh[q], gk_, gEend[q], ALU.mult)
            for c in range(2):
                ts(P, "pool", gkhc[c][q], gkh[q], mk[:, 6, c * 64:c * 64 + 1], ALU.mult)
            for h in H4:
                pT_ = ps[6 + h % 2]
                g32 = slice(h * 32, (h + 1) * 32)
                tr(P, pT_[0:32, 0:128], gq[q][:, g32], idn)
                tr(P, pT_[0:32, 128:256], gk[q][:, g32], idn)
                tr(P, pT_[0:32, 256:384], gEtot[q][:, g32], idn)
                cp(P, "dve", gqT[h][q], pT_[0:32, 0:128])
                cp(P, "dve", gkT[h][q], pT_[0:32, 128:256])
                cp(P, "dve", gPT[h][q], pT_[0:32, 256:384])
            for h in H4:
                mm(P, ps[h][:, 0:128], gkT[h][q], gqT[h][q], True, True)
            for h in H4:
                tt(P, "dve", gA[h], ps[h][:, 0:128], mk[:, m_it, :], ALU.mult)
            for h in H4:
                hc = slice(h * 64, (h + 1) * 64)
                g32 = slice(h * 32, (h + 1) * 32)
                for c in range(2):
                    cs = slice(c * 64, (c + 1) * 64)
                    mm(P, ps[h][0:64, 128 + c * 64:192 + c * 64], gA[h][:, cs], gv_[:, hc], True, True)
                    mm(P, ps[h][0:32, 256 + c * 64:320 + c * 64], gkhc[c][q][:, g32], gv_[:, hc], True, True)
            for h in H4:
                for c in range(2):
                    cp(P, "dve", gY0[c][h], ps[h][0:64, 128 + c * 64:192 + c * 64])
                    cp(P, "dve", gH[c][h], ps[h][0:32, 256 + c * 64:320 + c * 64])
            for c in ((0, 1) if d == 0 else (1, 0)):
                cs = slice(c * 64, (c + 1) * 64)
                for h in H4:
                    S_ = STg[d][h]
                    mm(P, ps[h][0:64, 384:448], gqT[h][q][:, cs], S_, True, False)
                    mm(P, ps[h][0:64, 384:448], idn[0:64, 0:64], gY0[c][h], False, True)
                for h in H4:
                    hc = slice(h * 64, (h + 1) * 64)
                    S_ = STg[d][h]
                    cp(P, "dve", GYO[:, c, hc], ps[h][0:64, 384:448])
                    stt(P, "dve", S_, S_, gPT[h][q][:, c * 64:c * 64 + 1], gH[c][h], ALU.mult, ALU.add)
            dma(P, "sp" if d == 1 else "pool",
                V(C.YTG_ap[d][t * 128:(t + 1) * 128, :].rearrange("(c p) n -> p c n", p=64), C.YTG_bufs[d][t]), GYO)
    for n in range(NT if cut > 50 else 1):
        gens = [body(n, 0), body(n, 1)]
        while gens:
            nxt = []
            for g in gens:
                try:
                    next(g)
                    nxt.append(g)
                except StopIteration:
                    pass
            gens = nxt
    A.close()


class LayerState:
    pass


def layer(C, l):
    P = C.P
    LA = Arena(P)
    L = LayerState()
    L.mod = LA.sb("mod", [128, 48, 2])
    L.sc1 = LA.sb("sc1", [128, 8, 2])
    L.sc2 = LA.sb("sc2", [128, 8, 2])
    L.grow = [[LA.sb("grow%d%d" % (ii, j), [128, 1024]) for j in range(2)] for ii in range(2)]
    phase_ada(C, l, L)
    if C.dbg.get("dump") and l == C.dbg.get("layer", 0):
        dma(P, "sp", C.dout("d_mod", [128, 48, 2]), L.mod)
        for ii in range(2):
            for j in range(2):
                dma(P, "sp", C.dout("d_grow%d%d" % (ii, j), [128, 1024]), L.grow[ii][j])
    HA = Arena(P)
    hfm = HA.sb("hfm", [128, 8, NTOK], BF16)
    phase_norm(C, l, L, 1, hfm)
    if C.dbg.get("dump") and l == C.dbg.get("layer", 0):
        dma(P, "sp", C.dout("d_hfm", [128, 8, NTOK], BF16), hfm)
    phase_win_tm(C, l, L, hfm)
    if not C.dbg.get("skip_gates"):
        phase_win_gates(C, l, L, hfm)
    HA.close()
    if C.dbg.get("stop_after") == "win":
        LA.close(); return
    if not C.dbg.get("skip_ab"):
        phase_prep(C, l, L)
        if C.dbg.get("stop_after") == "prep":
            LA.close(); return
        if C.dbg.get("old_gla") and not C.dbg.get("skip_scan"):
            phase_scan(C, l, C.dbg.get("nchunks"))
        if not C.dbg.get("old_rwkv"):
            phase_chunk(C, l, L)
        if C.dbg.get("stop_after") == "chunk":
            LA.close(); return
        if C.dbg.get("stop_after") == "scan":
            LA.close(); return
        phase_fin_ab(C, l, L)
    if C.dbg.get("stop_after") == "fin":
        LA.close(); return
    if not C.dbg.get("skip_attn"):
        phase_attn(C, l, L)
    if C.dbg.get("stop_after") == "attn":
        LA.close(); return
    if not C.dbg.get("skip_s5"):
        phase_s5(C, l, L)
    if C.dbg.get("stop_after") == "s5":
        LA.close(); return
    phase_merge(C, l, L)
    if C.dbg.get("stop_after") == "merge":
        LA.close(); return
    WA = Arena(P)
    L.WT = WA.sb("WT", [32, NTOK])
    HA = Arena(P)
    hfm2 = HA.sb("hfm2", [128, 8, NTOK], BF16)
    RA = Arena(P)
    phase_norm(C, l, L, 2, hfm2, per_tile=make_router(C, l, L, RA))
    RA.close()
    if C.dbg.get("dump") and l == C.dbg.get("layer", 0):
        dma(P, "sp", C.dout("d_hfm2", [128, 8, NTOK], BF16), hfm2)
        dma(P, "sp", C.dout("d_WT", [32, NTOK]), L.WT)
    H2 = V(C.H2_ap, C.H2_buf)
    dma(P, "sp", H2, hfm2)
    HA.close()
    if C.dbg.get("stop_after") == "norm2":
        WA.close(); LA.close(); return
    phase_moe(C, l, L, H2, l == DEPTH - 1)
    WA.close()
    LA.close()


def make_maskw():
    m = np.zeros((128, 384), np.float32)
    i = np.arange(128)[:, None]
    j = np.arange(128)[None, :]
    m[:, 0:128] = np.where(j >= i, 0.0, -1e30)
    m[:, 256:384] = np.where(j <= i, 0.0, -1e30)
    return m


def make_rope():
    rows = SEQ // 64
    row = np.repeat(np.arange(rows, dtype=np.float32), 64)
    col = np.tile(np.arange(64, dtype=np.float32), rows)
    inv = (10000.0 ** (-np.arange(16, dtype=np.float32) / 16)).astype(np.float32)
    ang = np.concatenate([row[:, None] * inv, col[:, None] * inv], axis=-1).astype(np.float32)
    return np.cos(ang).astype(np.float32), np.sin(ang).astype(np.float32)


ROPE = make_rope()


def s5_host(A):
    f = np.float32
    lre, lim, ldt = A("s5_lam_re"), A("s5_lam_im"), A("s5_log_dt")
    ldt_e = np.repeat(ldt[..., None], 64, axis=-1)
    rows = np.stack([lre.reshape(DEPTH, 2, 1024), lim.reshape(DEPTH, 2, 1024), ldt_e.reshape(DEPTH, 2, 1024)], axis=2)
    sm = rows.reshape(DEPTH, 2, 3, 8, 128).transpose(0, 4, 1, 2, 3)
    bre, bim = A("s5_b_re"), A("s5_b_im")
    bt = np.zeros((DEPTH, 2, 8, 128, 128), f)
    cre, cim = A("s5_c_re"), A("s5_c_im")
    ct = np.zeros((DEPTH, 2, 2, 8, 128, 128), f)
    for g in range(16):
        j, hh = g // 2, g % 2
        c0 = (g % 8) * 16
        for ri, b in enumerate((bre, bim)):
            bt[:, ri, j, c0:c0 + 16, hh * 64:(hh + 1) * 64] = b[:, g].transpose(0, 2, 1)
        for ri, c in enumerate((cre, cim)):
            ct[:, :, ri, j, hh * 64:(hh + 1) * 64, c0:c0 + 16] = c[:, :, g].transpose(0, 1, 3, 2)
    return {
        "s5_sm": np.ascontiguousarray(sm, f), "s5_rows": np.ascontiguousarray(rows, f),
        "s5_bt": bt, "s5_ct": ct,
        "pw2": (2.0 ** np.arange(16)).astype(f).reshape(1, 16),
        "s5_d_fm": np.ascontiguousarray(np.pad(A("s5_d").reshape(DEPTH, 2, 128).transpose(0, 2, 1), ((0, 0), (0, 0), (0, 14)))),
        "s5_bglu_fm": np.ascontiguousarray(np.pad(A("s5_b_glu").reshape(DEPTH, 2, 128).transpose(0, 2, 1), ((0, 0), (0, 0), (0, 14)))),
        "s5_w_glu": A("s5_w_glu"),
    }


def make_sele():
    s = np.zeros((32, 32, 128), np.float32)
    for e in range(32):
        s[e, e, :] = 1.0
    return s


def make_cmasks():
    r = np.arange(128)[:, None]
    c = np.arange(128)[None, :]
    same = (r // 64) == (c // 64)
    m = np.zeros((128, 7, 128), np.float32)
    m[:, 0] = same & (c < r)
    m[:, 1] = same & (r < c)
    m[:, 2] = same & (r <= c)
    m[:, 3] = same & (c > r)
    m[:, 4] = same & (r > c)
    m[:, 5] = same & (r >= c)
    m[:, 6] = same
    return m


def make_sel():
    s = np.zeros((128, 64, 128), np.float32)
    for j in range(64):
        for hh in range(2):
            s[2 * j + hh, j, hh * 64:(hh + 1) * 64] = 1.0
    return s


def blkdiag(mats):
    n = len(mats)
    L, r, c = mats[0].shape
    o = np.zeros((L, n * r, n * c), np.float32)
    for i, m in enumerate(mats):
        o[:, i * r:(i + 1) * r, i * c:(i + 1) * c] = m
    return o


def host_inputs(inputs, b):
    f = np.float32

    def A(k):
        return np.asarray(inputs[k], f)
    c = np.asarray(inputs["c"], f)[b]
    cctx = np.asarray(inputs["c_ctx"], f)
    cc = np.stack([c.reshape(8, 128).T, cctx.reshape(8, 128).T], axis=-1)
    m = {
        "xb": np.ascontiguousarray(np.asarray(inputs["x"], f)[b]),
        "ctxb": np.ascontiguousarray(np.asarray(inputs["ctx"], f)[b]),
        "cc": np.ascontiguousarray(cc),
        "w_ada": np.asarray(inputs["w_ada"], f),
        "b_ada": np.asarray(inputs["b_ada"], f),
        "b_ada_fm": np.ascontiguousarray(np.asarray(inputs["b_ada"], f).reshape(DEPTH, 48, 128).transpose(0, 2, 1)),
        "g1_fm": np.ascontiguousarray(np.asarray(inputs["norm1_g"], f).reshape(DEPTH, 8, 128).transpose(0, 2, 1)),
        "g2_fm": np.ascontiguousarray(np.asarray(inputs["norm2_g"], f).reshape(DEPTH, 8, 128).transpose(0, 2, 1)),
        "w_in": np.asarray(inputs["w_in"], f),
        "ident": np.eye(128, dtype=f),
        "sel": make_sel(),
        "maskw": make_maskw(),
        "ropec": ROPE[0],
        "ropes": ROPE[1],
        "attn_sink": np.ascontiguousarray(np.pad(A("attn_sink"), ((0, 0), (0, 12)))),
        **s5_host(A),
        "cmasks": make_cmasks(),
        "w_branch": A("w_branch"),
        "w_out": A("w_out"),
        "w_router": np.ascontiguousarray(np.concatenate([A("w_router_g"), A("w_router_e")], axis=2)),
        "b_router": np.ascontiguousarray(np.concatenate([A("b_router_g"), A("b_router_e")], axis=1)),
        "w_exp_gate": A("w_exp_gate"),
        "w_exp_up": A("w_exp_up"),
        "w_exp_down": A("w_exp_down"),
        "pv": np.ascontiguousarray(np.concatenate([
            A("rwkv_mu").reshape(DEPTH, -1), A("rwkv_kk"), A("rwkv_ka"), A("rwkv_rk").reshape(DEPTH, -1),
            A("rwkv_w0").reshape(DEPTH, -1), A("rwkv_a0").reshape(DEPTH, -1), A("rwkv_ln_g"),
            A("gla_ab").reshape(DEPTH, -1), A("gla_ln_g")], axis=1)),
        "w1cat": np.ascontiguousarray(np.concatenate([A("rwkv_w1")[:, 0], A("rwkv_w1")[:, 1],
                                                      A("rwkv_a1")[:, 0], A("rwkv_a1")[:, 1]], axis=2)),
        "w2blk": blkdiag([A("rwkv_w2")[:, 0], A("rwkv_w2")[:, 1], A("rwkv_a2")[:, 0], A("rwkv_a2")[:, 1]]),
        "g1": A("rwkv_g1"),
        "g2": A("rwkv_g2"),
        "a2blk": blkdiag([A("gla_a2")[:, 0], A("gla_a2")[:, 1]]),
        "final_g": np.asarray(inputs["final_norm_g"], f).reshape(1, D),
    }
    return m


def kernel(**inputs):
    nc = build_program()
    in_maps = [host_inputs(inputs, b) for b in range(8)]
    res = run_bass_kernel_spmd(nc, in_maps, core_ids=list(range(8)))
    return np.stack([r["out"] for r in res.results], axis=0)
```

```python
import math
from contextlib import ExitStack

import numpy as np
import concourse.bass as bass
import concourse.mybir as mybir
from concourse.bass_utils import run_bass_kernel_spmd

F32 = mybir.dt.float32
BF16 = mybir.dt.bfloat16
ALU = mybir.AluOpType
AF = mybir.ActivationFunctionType
AX = mybir.AxisListType

D = 1024
SEQ = 4096
CTX = 256
NT = (SEQ + CTX) // 128
NTOK = SEQ + CTX
DEPTH = 2
EPS = 1e-6
GN_EPS = 64e-5
O1, O2, O3, O4, PIN = 1024, 1824, 2336, 2592, 6688
PV_MU, PV_KK, PV_KA, PV_RK, PV_W0, PV_A0, PV_LNG, PV_GAB, PV_GLNG = 0, 1024, 1280, 1536, 1792, 2304, 2816, 3072, 3328
NPV = 3584

DEBUG = False
PENDING = "PENDING"
WKEYS = ("out", "accum_out", "ap")
SKEYS = ("scalar1", "scalar2", "scale", "bias", "scalar")


class Buf:
    __slots__ = ("name", "w", "rd", "ws")

    def __init__(self, name=""):
        self.name = name
        self.w = None
        self.rd = {}
        self.ws = False


class V:
    __slots__ = ("ap", "bufs")

    def __init__(self, ap, bufs):
        self.ap = ap
        self.bufs = bufs if isinstance(bufs, tuple) else (bufs,)

    def __getitem__(self, k):
        return V(self.ap[k], self.bufs)

    def rr(self, pat, **kw):
        return V(self.ap.rearrange(pat, **kw), self.bufs)

    def bc(self, shape):
        return V(self.ap.to_broadcast(list(shape)), self.bufs)

    def bitcast(self, dt):
        return V(self.ap.bitcast(dt), self.bufs)

    def wb(self, *bufs):
        return V(self.ap, tuple(bufs))

    @property
    def shape(self):
        return tuple(self.ap.shape)


class Prog:
    ENG = ("pe", "act", "dve", "pool", "sp")
    K = 6
    STRICT_ALL = False

    def __init__(self, nc):
        self.nc = nc
        self.eng = {"pe": nc.tensor, "act": nc.scalar, "dve": nc.vector, "pool": nc.gpsimd, "sp": nc.sync}
        self.sem = {e: nc.alloc_semaphore("s_" + e) for e in self.ENG}
        self.cnt = {e: 0 for e in self.ENG}
        self.dsem = {q: [nc.alloc_semaphore("d_%s%d" % (q, i)) for i in range(self.K)] for q in ("sp", "act", "pool")}
        self.dcnt = {q: 0 for q in self.dsem}
        self.known = {e: {} for e in self.ENG}
        self.pend_r = []
        self.pend_w = []
        self.uid = 0
        self.nins = 0

    def _need(self, e, tok, is_dma, strict=False):
        if tok is None:
            return
        if tok is PENDING:
            assert e == "pe" and not is_dma, "dependency on an unmarked PE op"
            return
        sem, val, owner = tok
        if owner == e and not is_dma and not (strict and e != "pe") and not self.STRICT_ALL:
            return
        k = self.known[e]
        if k.get(sem.num, 0) >= val:
            return
        self.eng[e].wait_ge(sem, val)
        self.nins += 1
        k[sem.num] = val

    def op(self, e, meth, mark=True, lax=False, **kw):
        reads, writes, args, sreads, awrites = [], [], {}, [], []
        for k, v in kw.items():
            if isinstance(v, V):
                (writes if k in WKEYS else reads).extend(v.bufs)
                if k in SKEYS:
                    sreads.extend(v.bufs)
                if k == "accum_out":
                    awrites.extend(v.bufs)
                args[k] = v.ap
            else:
                args[k] = v
        is_dma = meth == "dma_start"
        for b in reads:
            self._need(e, b.w, is_dma, strict=(not lax) or b.ws or e == "act" or b in sreads)
        for b in writes:
            self._need(e, b.w, is_dma)
            for t in b.rd.values():
                self._need(e, t, is_dma)
        if is_dma:
            n = self.dcnt[e]
            sem = self.dsem[e][n % self.K]
            r = n // self.K
            if r > 0:
                self._need(e, (sem, 16 * r, None), True)
            ins = getattr(self.eng[e], meth)(**args)
            ins.then_inc(sem, 16)
            self.dcnt[e] = n + 1
            tok = (sem, 16 * (r + 1), None)
            key = ("d", sem.num)
        else:
            ins = getattr(self.eng[e], meth)(**args)
            key = e
            if mark:
                self.cnt[e] += 1
                ins.then_inc(self.sem[e], 1)
                tok = (self.sem[e], self.cnt[e], e)
                if e == "pe" and (self.pend_r or self.pend_w):
                    for b in self.pend_r:
                        if b.rd.get("pe") is PENDING:
                            b.rd["pe"] = tok
                    for b in self.pend_w:
                        if b.w is PENDING:
                            b.w = tok
                    self.pend_r = []
                    self.pend_w = []
            else:
                assert e == "pe"
                tok = PENDING
                self.pend_r.extend(reads)
                self.pend_w.extend(writes)
        self.nins += 1
        for b in reads:
            b.rd[key] = tok
        for b in writes:
            b.w = tok
            b.rd = {}
            b.ws = (b in awrites) or e == "act"
        return ins

    def barrier(self):
        assert not self.pend_r and not self.pend_w
        toks = [(self.sem[e], self.cnt[e], e) for e in self.ENG if self.cnt[e] > 0]
        for q in self.dsem:
            n = self.dcnt[q]
            for i in range(self.K):
                c = (n - i + self.K - 1) // self.K if n > i else 0
                if c > 0:
                    toks.append((self.dsem[q][i], 16 * c, None))
        for e in self.ENG:
            for t in toks:
                self._need(e, t, False)

    def name(self, s):
        self.uid += 1
        return "%s_%d" % (s, self.uid)

    def dram(self, name, shape, dt, kind="Internal"):
        return self.nc.dram_tensor(name, list(shape), dt, kind=kind).ap()


class Arena:
    def __init__(self, P):
        self.P = P
        self.stack = ExitStack()

    def sb(self, name, shape, dt=F32):
        h = self.stack.enter_context(self.P.nc.sbuf_tensor(self.P.name(name), list(shape), dt))
        return V(h.ap(), Buf(name))

    def close(self):
        self.P.barrier()
        self.stack.close()


def dma(P, q, out, in_):
    return P.op(q, "dma_start", out=out, in_=in_)


def mm(P, out, lhsT, rhs, start, stop, mark=None):
    return P.op("pe", "matmul", mark=(stop if mark is None else mark), out=out, lhsT=lhsT, rhs=rhs,
                start=start, stop=stop)


def tr(P, out, in_, ident, mark=True):
    return P.op("pe", "transpose", mark=mark, out=out, in_=in_, identity=ident)


def tt(P, e, out, in0, in1, op):
    return P.op(e, "tensor_tensor", out=out, in0=in0, in1=in1, op=op)


def ts(P, e, out, in0, s1, op0, s2=None, op1=None, **kw):
    if op1 is None:
        return P.op(e, "tensor_scalar", out=out, in0=in0, scalar1=s1, scalar2=None, op0=op0, **kw)
    return P.op(e, "tensor_scalar", out=out, in0=in0, scalar1=s1, scalar2=s2, op0=op0, op1=op1, **kw)


def act(P, out, in_, func, **kw):
    return P.op("act", "activation", out=out, in_=in_, func=func, **kw)


def cp(P, e, out, in_):
    if e == "act":
        return act(P, out, in_, AF.Copy)
    return P.op(e, "tensor_copy", out=out, in_=in_)


class Ctx:
    pass


def build_program(dbg=None):
    dbg = dbg or {}
    nc = bass.Bass("TRN2", target_bir_lowering=False)
    P = Prog(nc)
    C = Ctx()
    C.P, C.nc, C.dbg = P, nc, dbg
    skind = "ExternalOutput" if dbg.get("expose") else "Internal"

    def din(name, shape, dt=F32):
        return V(nc.dram_tensor(name, list(shape), dt, kind="ExternalInput").ap(), Buf(name))

    I = {}
    I["xb"] = din("xb", [SEQ, D])
    I["ctxb"] = din("ctxb", [CTX, D])
    I["cc"] = din("cc", [128, 8, 2])
    I["w_ada"] = din("w_ada", [DEPTH, D, 6 * D])
    I["b_ada"] = din("b_ada", [DEPTH, 6 * D])
    I["b_ada_fm"] = din("b_ada_fm", [DEPTH, 128, 48])
    I["g1_fm"] = din("g1_fm", [DEPTH, 128, 8])
    I["g2_fm"] = din("g2_fm", [DEPTH, 128, 8])
    I["w_in"] = din("w_in", [DEPTH, D, PIN])
    I["ident"] = din("ident", [128, 128])
    I["sel"] = din("sel", [128, 64, 128])
    I["maskw"] = din("maskw", [128, 384])
    I["ropec"] = din("ropec", [SEQ, 32])
    I["ropes"] = din("ropes", [SEQ, 32])
    I["attn_sink"] = din("attn_sink", [DEPTH, 16])
    I["s5_sm"] = din("s5_sm", [DEPTH, 128, 2, 3, 8])
    I["s5_rows"] = din("s5_rows", [DEPTH, 2, 3, 1024])
    I["s5_bt"] = din("s5_bt", [DEPTH, 2, 8, 128, 128])
    I["s5_ct"] = din("s5_ct", [DEPTH, 2, 2, 8, 128, 128])
    I["pw2"] = din("pw2", [1, 16])
    I["cmasks"] = din("cmasks", [128, 7, 128])
    I["w_branch"] = din("w_branch", [DEPTH, 4, 256, D])
    I["w_out"] = din("w_out", [DEPTH, D, D])
    I["w_router"] = din("w_router", [DEPTH, D, 36])
    I["b_router"] = din("b_router", [DEPTH, 36])
    I["w_exp_gate"] = din("w_exp_gate", [DEPTH, 32, D, 512])
    I["w_exp_up"] = din("w_exp_up", [DEPTH, 32, D, 512])
    I["w_exp_down"] = din("w_exp_down", [DEPTH, 32, 512, D])
    I["s5_d_fm"] = din("s5_d_fm", [DEPTH, 128, 16])
    I["s5_bglu_fm"] = din("s5_bglu_fm", [DEPTH, 128, 16])
    I["s5_w_glu"] = din("s5_w_glu", [DEPTH, 256, 256])
    I["pv"] = din("pv", [DEPTH, NPV])
    I["w1cat"] = din("w1cat", [DEPTH, 256, 128])
    I["w2blk"] = din("w2blk", [DEPTH, 128, 1024])
    I["g1"] = din("g1", [DEPTH, 256, 64])
    I["g2"] = din("g2", [DEPTH, 64, 256])
    I["a2blk"] = din("a2blk", [DEPTH, 32, 256])
    I["final_g"] = din("final_g", [1, D])
    C.I = I

    out = V(nc.dram_tensor("out", [SEQ, D], F32, kind="ExternalOutput").ap(), Buf("out"))
    C.out = out

    def dout(name, shape, dt=F32):
        return V(nc.dram_tensor(name, list(shape), dt, kind="ExternalOutput").ap(), Buf(name))
    C.dout = dout

    xres_ap = P.dram("xres", [NTOK, D], F32, kind=skind)
    C.xres = [V(xres_ap[t * 128:(t + 1) * 128, :], Buf("xres%d" % t)) for t in range(NT)]
    C.U_ap = P.dram("U", [NTOK + 3, O4], F32, kind=skind)
    C.U_bufs = [Buf("U%d" % t) for t in range(NT)]
    C.U_pad = Buf("Upad")
    C.STR_ap = [P.dram("STR%d" % d, [NTOK, 2, 1024], BF16, kind=skind) for d in range(2)]
    C.STR_bufs = [[Buf("STR%d_%d" % (d, t)) for t in range(NT)] for d in range(2)]
    C.Vs_ap = P.dram("Vs", [128, NTOK, 6], F32, kind=skind)
    C.Vs_bufs = [Buf("Vs%d" % t) for t in range(NT)]
    C.Y_ap = [P.dram("Y%d" % d, [128, NTOK, 6], F32, kind=skind) for d in range(2)]
    C.Y_bufs = [[Buf("Y%d_%d" % (d, c)) for c in range(NTOK // 64)] for d in range(2)]
    C.FIN_ap = P.dram("FIN", [NTOK, 768], F32, kind=skind)
    C.FIN_bufs = [Buf("FIN%d" % t) for t in range(NT)]
    C.YB_ap = P.dram("YB", [4, 256, NTOK], BF16, kind=skind)
    C.YB_bufs = [[Buf("YB%d_%d" % (i, t)) for t in range(NT)] for i in range(4)]
    C.CH_ap = P.dram("CH", [NTOK, 3072], F32, kind=skind)
    C.CH_bufs = [Buf("CH%d" % t) for t in range(NT)]
    C.YT_ap = [P.dram("YT%d" % d, [NTOK, 256], F32, kind=skind) for d in range(2)]
    C.YT_bufs = [[Buf("YT%d_%d" % (d, t)) for t in range(NT)] for d in range(2)]
    C.YTG_ap = [P.dram("YTG%d" % d, [NTOK, 256], F32, kind=skind) for d in range(2)]
    C.YTG_bufs = [[Buf("YTG%d_%d" % (d, t)) for t in range(NT)] for d in range(2)]
    C.Gt_ap = P.dram("Gt", [4096, NTOK], BF16, kind=skind)
    C.Gt_bufs = [Buf("Gt%d" % b) for b in range(9)]
    C.H2_ap = P.dram("H2", [128, 8, NTOK], BF16, kind=skind)
    C.H2_buf = Buf("H2")

    G = Arena(P)
    C.G = G
    C.psall = nc.alloc_psum_tensor("psall", [128, 4096], F32).ap()
    C.psb = [Buf("ps%d" % i) for i in range(8)]
    C.ps = [V(C.psall[:, i * 512:(i + 1) * 512], C.psb[i]) for i in range(8)]
    C.ident = G.sb("ident", [128, 128])
    dma(P, "sp", C.ident, I["ident"])

    for l in range(DEPTH):
        layer(C, l)
        if dbg.get("stop_layer") == l:
            break
    if not dbg.get("stop"):
        final_norm(C)
    P.barrier()
    return nc


def urow(t):
    return 1 + t * 128 if t < 2 else 258 + (t - 2) * 128


def xsrc(C, l, t):
    if l == 0 and not C.__dict__.get("x_in_scratch"):
        if t < 2:
            return C.I["ctxb"][t * 128:(t + 1) * 128, :]
        return C.I["xb"][(t - 2) * 128:(t - 1) * 128, :]
    return C.xres[t]


def phase_ada(C, l, L):
    P, I = C.P, C.I
    A = Arena(P)
    cc = A.sb("cc", [128, 8, 2])
    sc = A.sb("sc", [128, 8, 2])
    screp = A.sb("screp", [128, 8, 2, 128])
    bfm = A.sb("bfm", [128, 48])
    g1 = A.sb("g1", [128, 8])
    g2 = A.sb("g2", [128, 8])
    wst = [A.sb("wst%d" % i, [128, 8, 512]) for i in range(2)]
    brow = [A.sb("brow%d" % i, [128, 1024]) for i in range(2)]
    dma(P, "sp", cc, I["cc"])
    dma(P, "sp", bfm, I["b_ada_fm"][l])
    dma(P, "sp", g1, I["g1_fm"][l])
    dma(P, "sp", g2, I["g2_fm"][l])
    for ii, i in enumerate((2, 5)):
        dma(P, "pool", brow[ii], I["b_ada"][l:l + 1, i * 1024:(i + 1) * 1024].bc([128, 1024]))
    act(P, sc, cc, AF.Silu)
    for k in range(8):
        for j in range(2):
            cp(P, "dve", screp[:, k, j, :], sc[:, k, j:j + 1].bc([128, 128]))
    wv = I["w_ada"][l].rr("(k p) n -> p k n", p=128)
    psA = C.ps[0]
    for c in range(12):
        w = wst[c % 2]
        dma(P, "sp" if c % 2 == 0 else "pool", w, wv[:, :, c * 512:(c + 1) * 512])
        for mi in range(4):
            m = c * 4 + mi
            for k in range(8):
                mm(P, psA[:, m * 2:(m + 1) * 2], w[:, k, mi * 128:(mi + 1) * 128], sc[:, k, :], k == 0, k == 7)
        if c in (4, 5, 10, 11):
            ii = 0 if c < 6 else 1
            half = c % 2
            for j in range(2):
                pr = C.ps[1 + j]
                for k in range(8):
                    mm(P, pr, screp[:, k, j, :], w[:, k, :], k == 0, k == 7)
                tt(P, "dve", L.grow[ii][j][:, half * 512:(half + 1) * 512], pr,
                   brow[ii][:, half * 512:(half + 1) * 512], ALU.add)
    tt(P, "dve", L.mod, psA[:, 0:96].rr("p (m j) -> p m j", j=2), bfm[:, :, None].bc([128, 48, 2]), ALU.add)
    ts(P, "dve", L.sc1, L.mod[:, 8:16, :], 1.0, ALU.add)
    tt(P, "dve", L.sc1, L.sc1, g1[:, :, None].bc([128, 8, 2]), ALU.mult)
    ts(P, "dve", L.sc2, L.mod[:, 32:40, :], 1.0, ALU.add)
    tt(P, "dve", L.sc2, L.sc2, g2[:, :, None].bc([128, 8, 2]), ALU.mult)
    A.close()


def phase_norm(C, l, L, which, hfm, per_tile=None):
    P = C.P
    A = Arena(P)
    sc = L.sc1 if which == 1 else L.sc2
    shb = 0 if which == 1 else 24
    xt = [A.sb("xt%d" % i, [128, D]) for i in range(2)]
    junk = A.sb("junk", [128, D])
    st = [A.sb("st%d" % i, [128, 2]) for i in range(2)]
    hf = [A.sb("hf%d" % i, [128, 8, 128]) for i in range(2)] if per_tile else None
    for t in range(NT):
        j = 1 if t < 2 else 0
        x = xt[t % 2]
        s = st[t % 2]
        dma(P, "sp" if t % 2 == 0 else "pool", x, xsrc(C, l, t))
        act(P, junk, x, AF.Square, accum_out=s[:, 0:1])
        ts(P, "dve", s[:, 1:2], s[:, 0:1], 1.0 / D, ALU.mult, EPS, ALU.add)
        act(P, s[:, 1:2], s[:, 1:2], AF.Sqrt)
        P.op("dve", "reciprocal", out=s[:, 1:2], in_=s[:, 1:2])
        ts(P, "dve", x, x, s[:, 1:2], ALU.mult)
        pa, pb = C.ps[2 + 2 * (t % 2)], C.ps[3 + 2 * (t % 2)]
        if C.dbg.get("dump_norm") and t == 2 and which == 1:
            dma(P, "sp", C.dout("d_xn", [128, D]), x)
            dma(P, "sp", C.dout("d_st", [128, 2]), s)
        for k in range(8):
            pp = pa if k < 4 else pb
            tr(P, pp[:, (k % 4) * 128:(k % 4 + 1) * 128], x[:, k * 128:(k + 1) * 128], C.ident)
        if C.dbg.get("dump_norm") and t == 2 and which == 1:
            cp(P, "dve", junk[:, 0:512], pa)
            dma(P, "sp", C.dout("d_pa", [128, 512]), junk[:, 0:512])
        for k in range(8):
            pp = pa if k < 4 else pb
            src = pp[:, (k % 4) * 128:(k % 4 + 1) * 128]
            if per_tile:
                dst = hf[t % 2][:, k, :]
            else:
                dst = hfm[:, k, t * 128:(t + 1) * 128]
            if k % 2 == 0:
                act(P, dst, src, AF.Identity, scale=sc[:, k, j:j + 1], bias=L.mod[:, shb + k, j:j + 1])
            else:
                ts(P, "dve", dst, src, sc[:, k, j:j + 1], ALU.mult, L.mod[:, shb + k, j:j + 1], ALU.add)
        if per_tile:
            for k in range(8):
                cp(P, "act" if k % 2 == 0 else "dve", hfm[:, k, t * 128:(t + 1) * 128], hf[t % 2][:, k, :])
            per_tile(t, hf[t % 2])
    A.close()


def phase_win_tm(C, l, L, hfm):
    P, I = C.P, C.I
    A = Arena(P)
    wA = A.sb("wA", [128, 8, O4], BF16)
    wst = [A.sb("wst%d" % i, [128, 8, 512]) for i in range(2)]
    ust = [A.sb("ust%d" % i, [128, O4]) for i in range(2)]
    zer = A.sb("zer", [1, O4])
    P.op("dve", "memset", ap=zer, constant=0.0)
    for r in (0, 257, NTOK + 2):
        dma(P, "sp", V(C.U_ap[r:r + 1, :], C.U_pad), zer)
    wv = I["w_in"][l].rr("(k p) n -> p k n", p=128)
    blocks = [(0, 512), (512, 1024), (1024, 1536), (1536, 1824), (1824, 2336), (2336, 2592)]
    for bi, (c0, c1) in enumerate(blocks):
        w = wst[bi % 2]
        dma(P, "sp" if bi % 2 == 0 else "pool", w[:, :, 0:c1 - c0], wv[:, :, c0:c1])
        cp(P, "act", wA[:, :, c0:c1], w[:, :, 0:c1 - c0])
    n = 0
    for t in range(NT):
        u = ust[t % 2]
        for bi, (c0, c1) in enumerate(blocks):
            ps = C.ps[n % 4]
            n += 1
            for k in range(8):
                mm(P, ps[:, 0:c1 - c0], hfm[:, k, t * 128:(t + 1) * 128], wA[:, k, c0:c1], k == 0, k == 7)
            cp(P, "act" if bi % 2 == 0 else "dve", u[:, c0:c1], ps[:, 0:c1 - c0])
        r0 = urow(t)
        dma(P, "sp" if t % 2 == 0 else "pool", V(C.U_ap[r0:r0 + 128, :], C.U_bufs[t]), u)
    A.close()


def stt(P, e, out, in0, scalar, in1, op0, op1, **kw):
    return P.op(e, "scalar_tensor_tensor", out=out, in0=in0, scalar=scalar, in1=in1, op0=op0, op1=op1, **kw)


def red(P, e, out, in_, **kw):
    return P.op(e, "tensor_reduce", out=out, in_=in_, axis=AX.X, op=ALU.add, **kw)


def load_bf16(P, A, name, shape, src, q="sp", ce="act"):
    st = A.sb(name + "_f", shape)
    wb = A.sb(name, shape, BF16)
    dma(P, q, st, src)
    cp(P, ce, wb, st)
    return wb


def cust(v, offset_elems, dims):
    ap = v.ap
    base = ap.ap[0]
    new = type(ap)(ap.tensor, ap.offset + offset_elems, [tuple(base)] + [tuple(d) for d in dims])
    return V(new, v.bufs)


def phase_prep(C, l, L):
    P, I = C.P, C.I
    A = Arena(P)
    pv = A.sb("pv", [128, NPV])
    dma(P, "sp", pv, I["pv"][l:l + 1, :].bc([128, NPV]))
    w1cat = load_bf16(P, A, "w1cat", [128, 2, 128], I["w1cat"][l].rr("(k p) n -> p k n", p=128))
    w2blk = load_bf16(P, A, "w2blk", [128, 1024], I["w2blk"][l])
    g1 = load_bf16(P, A, "g1w", [128, 2, 64], I["g1"][l].rr("(k p) n -> p k n", p=128))
    g2 = load_bf16(P, A, "g2w", [64, 256], I["g2"][l])
    a2blk = load_bf16(P, A, "a2blk", [32, 256], I["a2blk"][l])
    mu = pv[:, PV_MU:PV_MU + 1024]
    kkp = pv[:, PV_KK:PV_KK + 256]
    ka = pv[:, PV_KA:PV_KA + 256]
    rkp = pv[:, PV_RK:PV_RK + 256]
    w0 = pv[:, PV_W0:PV_W0 + 512]
    a0 = pv[:, PV_A0:PV_A0 + 512]
    gab = pv[:, PV_GAB:PV_GAB + 256]
    glng = pv[:, PV_GLNG:PV_GLNG + 256]

    uc = [A.sb("uc%d" % i, [128, O2]) for i in range(2)]
    up = [A.sb("up%d" % i, [128, 1024]) for i in range(2)]
    un = [A.sb("un%d" % i, [128, 1024]) for i in range(2)]
    rows = [[A.sb("rows%d%d" % (d, i), [128, 2, 1024], BF16) for i in range(2)] for d in range(2)]
    vt = [A.sb("vt%d" % i, [128, 128, 6]) for i in range(2)]
    fin = [A.sb("fin%d" % i, [128, 768]) for i in range(2)]
    cht = [A.sb("cht%d" % i, [128, 3072]) for i in range(2)]
    t0 = A.sb("t0", [128, 1024])
    mx = A.sb("mx", [128, 1024])
    xaT = A.sb("xaT", [128, 2, 128], BF16)
    z = A.sb("z", [128, 128], BF16)
    sg = A.sb("sg", [64, 128], BF16)
    wl = A.sb("wl", [128, 512])
    wdec = A.sb("wdec", [128, 512])
    il = A.sb("il", [128, 512])
    iclr = A.sb("iclr", [128, 512])
    kk0 = A.sb("kk0", [128, 256])
    sq = A.sb("sq", [128, 256])
    ss = A.sb("ss", [128, 8])
    kk = A.sb("kk", [128, 256])
    t1 = A.sb("t1", [128, 512])
    keff = A.sb("keff", [128, 512])
    bb = A.sb("bb", [128, 512])
    rkt = A.sb("rkt", [128, 256])
    alT = A.sb("alT", [32, 128], BF16)
    gl = A.sb("gl", [128, 256])
    gdec = A.sb("gdec", [128, 256])
    sr = A.sb("sr", [128, 256])

    def rv(R, c0, n):
        return R[:, :, c0:c0 + n]

    for t in range(NT):
        i = t % 2
        r0 = urow(t)
        nb = [C.U_bufs[t]]
        if t > 0:
            nb.append(C.U_bufs[t - 1])
        if t < NT - 1:
            nb.append(C.U_bufs[t + 1])
        nb.append(C.U_pad)
        dma(P, "sp", uc[i], V(C.U_ap[r0:r0 + 128, 0:O2], C.U_bufs[t]))
        dma(P, "pool", up[i], V(C.U_ap[r0 - 1:r0 + 127, 0:1024], tuple(nb)))
        dma(P, "sp", un[i], V(C.U_ap[r0 + 1:r0 + 129, 0:1024], tuple(nb)))
        u = uc[i]
        R0, R1 = rows[0][i], rows[1][i]
        F = fin[i]
        tt(P, "pool", t0, up[i], un[i], ALU.add)
        stt(P, "dve", t0, t0, 0.5, u[:, 0:1024], ALU.mult, ALU.subtract)
        tt(P, "pool", t0, t0, mu, ALU.mult)
        tt(P, "dve", mx, t0, u[:, 0:1024], ALU.add)
        r_, k_, v_, xa_ = mx[:, 0:256], mx[:, 256:512], mx[:, 512:768], mx[:, 768:1024]
        pT = C.ps[0]
        for kt in range(2):
            tr(P, pT[:, kt * 128:(kt + 1) * 128], xa_[:, kt * 128:(kt + 1) * 128], C.ident)
        cp(P, "act", xaT, pT[:, 0:256].rr("p (k n) -> p k n", k=2))
        pz = C.ps[1]
        for kt in range(2):
            mm(P, pz[:, 0:128], w1cat[:, kt, :], xaT[:, kt, :], kt == 0, kt == 1)
        for kt in range(2):
            mm(P, pz[0:64, 128:256], g1[:, kt, :], xaT[:, kt, :], kt == 0, kt == 1)
        act(P, z[0:64, :], pz[0:64, 0:128], AF.Tanh)
        cp(P, "dve", z[64:128, :], pz[64:128, 0:128])
        act(P, sg, pz[0:64, 128:256], AF.Sigmoid)
        pw, pa_, pg = C.ps[2], C.ps[3], C.ps[4]
        mm(P, pw, z, w2blk[:, 0:512], True, True)
        mm(P, pa_, z, w2blk[:, 512:1024], True, True)
        mm(P, pg[:, 0:256], sg, g2, True, True)
        tt(P, "dve", wl, pw, w0, ALU.add)
        act(P, wl, wl, AF.Sigmoid)
        act(P, wdec, wl, AF.Exp, scale=-0.6065306597126334)
        tt(P, "dve", il, pa_, a0, ALU.add)
        act(P, iclr, il, AF.Sigmoid)
        cp(P, "act", F[:, 0:256], pg[:, 0:256])
        tt(P, "pool", kk0, k_, kkp, ALU.mult)
        tt(P, "pool", sq, kk0, kk0, ALU.mult)
        red(P, "dve", ss[:, 0:4], sq.rr("p (h k) -> p h k", h=4))
        ts(P, "dve", ss[:, 0:4], ss[:, 0:4], EPS, ALU.add)
        act(P, ss[:, 0:4], ss[:, 0:4], AF.Sqrt)
        P.op("dve", "reciprocal", out=ss[:, 0:4], in_=ss[:, 0:4])
        tt(P, "dve", kk.rr("p (h k) -> p h k", h=4), kk0.rr("p (h k) -> p h k", h=4),
           ss[:, 0:4][:, :, None].bc([128, 4, 64]), ALU.mult)
        ic3 = iclr.rr("p (d c) -> p d c", d=2)
        stt(P, "dve", t1.rr("p (d c) -> p d c", d=2), ic3, -1.0, ka[:, None, :].bc([128, 2, 256]), ALU.add, ALU.mult)
        stt(P, "dve", keff.rr("p (d c) -> p d c", d=2), t1.rr("p (d c) -> p d c", d=2), 1.0,
            k_[:, None, :].bc([128, 2, 256]), ALU.add, ALU.mult)
        tt(P, "pool", bb.rr("p (d c) -> p d c", d=2), ic3, kk[:, None, :].bc([128, 2, 256]), ALU.mult)
        tt(P, "pool", rkt, r_, k_, ALU.mult)
        tt(P, "pool", rkt, rkt, rkp, ALU.mult)
        red(P, "dve", ss[:, 4:8], rkt.rr("p (h k) -> p h k", h=4))
        tt(P, "dve", F[:, 256:512].rr("p (h k) -> p h k", h=4), v_.rr("p (h k) -> p h k", h=4),
           ss[:, 4:8][:, :, None].bc([128, 4, 64]), ALU.mult)
        pT2 = C.ps[5]
        tr(P, pT2[0:32, 0:128], u[:, O1 + 768:O1 + 800], C.ident)
        cp(P, "act", alT, pT2[0:32, 0:128])
        mm(P, pT2[:, 128:384], alT, a2blk, True, True)
        tt(P, "dve", gl, pT2[:, 128:384], gab, ALU.add)
        act(P, gl, gl, AF.Sigmoid)
        act(P, gl, gl, AF.Ln)
        if C.dbg.get("old_gla"):
            act(P, gdec, gl, AF.Exp, scale=1.0 / 16.0)
        act(P, sr, u[:, O1 + 512:O1 + 768], AF.Silu)
        tt(P, "pool", F[:, 512:768], sr, glng, ALU.mult)
        CHt = cht[i]
        ts(P, "pool", CHt[:, 0:512], wl, -0.6065306597126334, ALU.mult)
        cp(P, "act", CHt[:, 512:1024], keff)
        cp(P, "pool", CHt[:, 1024:1536], bb)
        ts(P, "dve", CHt[:, 1536:1792], kk, -1.0, ALU.mult)
        cp(P, "act", CHt[:, 1792:2048], r_)
        cp(P, "pool", CHt[:, 2048:2304], v_)
        ts(P, "dve", CHt[:, 2304:2560], gl, 1.0 / 16.0, ALU.mult)
        ts(P, "pool", CHt[:, 2560:2688], u[:, O1:O1 + 128], 32.0 ** -0.5, ALU.mult)
        cp(P, "act", CHt[:, 2688:2816], u[:, O1 + 128:O1 + 256])
        cp(P, "pool", CHt[:, 2816:3072], u[:, O1 + 256:O1 + 512])
        dma(P, "sp", V(C.CH_ap[t * 128:(t + 1) * 128, :], C.CH_bufs[t]), CHt)
        if not C.dbg.get("old_gla"):
            dma(P, "pool", V(C.FIN_ap[t * 128:(t + 1) * 128, :], C.FIN_bufs[t]), F)
            continue
        for d, R in ((0, R0), (1, R1)):
            e1 = "dve" if d == 0 else "pool"
            e2 = "pool" if d == 0 else "dve"
            src = wdec[:, d * 256:(d + 1) * 256].rr("p (a h k) -> p a h k", a=2, h=2)
            hi = rv(R, 0, 128).rr("p h (a k) -> p a h k", a=2)
            lo = rv(R, 192, 128).rr("p h (a k) -> p a h k", a=2)
            cp(P, e1, hi, src)
            tt(P, e1, lo, src, hi, ALU.subtract)
            gsrc = gdec[:, d * 128:(d + 1) * 128].rr("p (a h k) -> p a h k", a=2, h=2)
            ghi = rv(R, 128, 64).rr("p h (a k) -> p a h k", a=2)
            glo = rv(R, 320, 64).rr("p h (a k) -> p a h k", a=2)
            cp(P, e2, ghi, gsrc)
            tt(P, e2, glo, gsrc, ghi, ALU.subtract)
            cp(P, e1, rv(R, 384, 128).rr("p h (a k) -> p a h k", a=2),
               keff[:, d * 256:(d + 1) * 256].rr("p (a h k) -> p a h k", a=2, h=2))
            cp(P, e2, rv(R, 512, 64).rr("p h (a k) -> p a h k", a=2),
               u[:, O1 + 128:O1 + 256].rr("p (a h k) -> p a h k", a=2, h=2))
            cp(P, e1, rv(R, 576, 128).rr("p h (a k) -> p a h k", a=2), r_.rr("p (a h k) -> p a h k", a=2, h=2))
            ts(P, e2, rv(R, 704, 64).rr("p h (a k) -> p a h k", a=2),
               u[:, O1:O1 + 128].rr("p (a h k) -> p a h k", a=2, h=2), 32.0 ** -0.5, ALU.mult)
            ts(P, e1, rv(R, 768, 128).rr("p h (a k) -> p a h k", a=2), kk.rr("p (a h k) -> p a h k", a=2, h=2),
               -1.0, ALU.mult)
            cp(P, e2, rv(R, 896, 128).rr("p h (a k) -> p a h k", a=2),
               bb[:, d * 256:(d + 1) * 256].rr("p (a h k) -> p a h k", a=2, h=2))
            dma(P, "sp" if d == 0 else "pool",
                V(C.STR_ap[d][t * 128:(t + 1) * 128].rearrange("t h n -> t (h n)"), C.STR_bufs[d][t]),
                R.rr("p h n -> p (h n)"))
        pv4 = C.ps[6]
        for a in range(2):
            tr(P, pv4[:, a * 128:(a + 1) * 128], v_[:, a * 128:(a + 1) * 128], C.ident)
        for a in range(2):
            tr(P, pv4[:, (2 + a) * 128:(3 + a) * 128], u[:, O1 + 256 + a * 128:O1 + 384 + a * 128], C.ident)
        VT = vt[i]
        cp(P, "act", VT[:, :, 0], pv4[:, 0:128])
        cp(P, "dve", VT[:, :, 1], pv4[:, 0:128])
        cp(P, "act", VT[:, :, 2], pv4[:, 128:256])
        cp(P, "dve", VT[:, :, 3], pv4[:, 128:256])
        cp(P, "act", VT[:, :, 4], pv4[:, 256:384])
        cp(P, "dve", VT[:, :, 5], pv4[:, 384:512])
        dma(P, "sp", V(C.Vs_ap[:, t * 128:(t + 1) * 128, :], C.Vs_bufs[t]), VT)
        dma(P, "pool", V(C.FIN_ap[t * 128:(t + 1) * 128, :], C.FIN_bufs[t]), F)
    A.close()


def phase_scan(C, l, nchunks=None):
    P, I = C.P, C.I
    A = Arena(P)
    S = A.sb("S", [128, 2, 64])
    T3 = A.sb("T3", [128, 2, 64])
    T4 = A.sb("T4", [128, 2, 64])
    self_f = A.sb("sel_f", [128, 64, 128])
    sel = A.sb("sel", [128, 64, 128], BF16)
    dma(P, "sp", self_f, I["sel"])
    cp(P, "pool", sel, self_f)
    rows = [[A.sb("srow%d%d" % (d, i), [128, 1024], BF16) for i in range(2)] for d in range(2)]
    vb = [A.sb("vb%d" % i, [128, 2, 64, 6]) for i in range(2)]
    yb = [A.sb("yb%d" % i, [128, 2, 64, 6]) for i in range(2)]
    P.op("dve", "memset", ap=S, constant=0.0)
    for i in range(2):
        P.op("pool", "memset", ap=yb[i], constant=0.0)
    NCH = NTOK // 64
    for c in range(NCH if nchunks is None else nchunks):
        zf = c * 64
        zb = (192 - 64 * c) if c < 4 else (4544 - 64 * c)
        i = c % 2
        dma(P, "sp", rows[0][i], V(C.STR_ap[0][zf:zf + 64].rearrange("t h n -> (t h) n"), C.STR_bufs[0][zf // 128]))
        dma(P, "pool", rows[1][i], V(C.STR_ap[1][zb:zb + 64].rearrange("t h n -> (t h) n"), C.STR_bufs[1][zb // 128]))
        dma(P, "sp", vb[i][:, 0], V(C.Vs_ap[:, zf:zf + 64, :], C.Vs_bufs[zf // 128]))
        dma(P, "pool", vb[i][:, 1], V(C.Vs_ap[:, zb:zb + 64, :], C.Vs_bufs[zb // 128]))
        YB = yb[i]
        for j in range(64):
            s = c * 64 + j
            pb = (s % 2) * 2
            for d in range(2):
                jj = j if d == 0 else 63 - j
                lt = sel[:, jj, :]
                R = rows[d][i]
                bx = C.ps[pb + d]
                mm(P, bx[:, 0:64], lt, R[:, 128:192], True, False, mark=False)
                mm(P, bx[:, 0:64], lt, R[:, 320:384], False, True, mark=False)
                mm(P, bx[:, 64:128], lt, R[:, 512:576], True, True, mark=False)
                mm(P, bx[:, 128:192], lt, R[:, 704:768], True, True, mark=(d == 1))
            R4 = V(C.psall[:, pb * 512:pb * 512 + 1024].rearrange("p (d x) -> p d x", d=2), tuple(C.psb[pb:pb + 2]))
            Dv, KKv, RQv = R4[:, :, 0:64], R4[:, :, 64:128], R4[:, :, 128:192]
            P.op("dve", "tensor_tensor", lax=True, out=S, in0=S, in1=Dv, op=ALU.mult)
            vv = cust(vb[i], j * 6 + 4, [((127 - 2 * j) * 6, 2), (1, 2), (0, 32)])
            P.op("dve", "tensor_tensor", lax=True, out=T3.rr("p d (g k) -> p d g k", g=2),
                 in0=KKv.rr("p d (g k) -> p d g k", g=2), in1=vv, op=ALU.mult)
            P.op("dve", "tensor_tensor", lax=True, out=S, in0=S, in1=T3, op=ALU.add)
            P.op("dve", "tensor_tensor", lax=True, out=T4, in0=S, in1=RQv, op=ALU.mult)
            yv = cust(YB, j * 6 + 4, [((127 - 2 * j) * 6, 2), (1, 2)])
            P.op("dve", "tensor_reduce", lax=True, out=yv, in_=T4.rr("p d (g k) -> p d g k", g=2), axis=AX.X, op=ALU.add)
        dma(P, "sp", V(C.Y_ap[0][:, zf:zf + 64, :], C.Y_bufs[0][zf // 64]), YB[:, 0])
        dma(P, "pool", V(C.Y_ap[1][:, zb:zb + 64, :], C.Y_bufs[1][zb // 64]), YB[:, 1])
    A.close()


def phase_fin_ab(C, l, L):
    P, I = C.P, C.I
    A = Arena(P)
    pv = A.sb("pv", [128, NPV])
    dma(P, "sp", pv, I["pv"][l:l + 1, :].bc([128, NPV]))
    lng = pv[:, PV_LNG:PV_LNG + 256]
    yt = [A.sb("yt%d" % i, [128, 2, 128, 6]) for i in range(2)]
    fin = [A.sb("finf%d" % i, [128, 768]) for i in range(2)]
    ya = A.sb("ya", [128, 256])
    ytm = [A.sb("ytm%d" % i, [128, 2, 256]) for i in range(2)]
    ytg = [A.sb("ytg%d" % i, [128, 2, 256]) for i in range(2)]
    yg = A.sb("yg", [128, 256])
    sq = A.sb("sqf", [128, 256])
    st = A.sb("stf", [128, 16])
    ob = [A.sb("ob%d" % i, [128, 4, 128], BF16) for i in range(2)]
    for t in range(NT):
        i = t % 2
        Y = yt[i]
        F = fin[i]
        if C.dbg.get("old_gla"):
            for d in range(2):
                dma(P, "sp" if d == 0 else "pool", Y[:, d],
                    V(C.Y_ap[d][:, t * 128:(t + 1) * 128, :], (C.Y_bufs[d][2 * t], C.Y_bufs[d][2 * t + 1])))
        dma(P, "sp", F, V(C.FIN_ap[t * 128:(t + 1) * 128, :], C.FIN_bufs[t]))
        pr, pg = C.ps[0], C.ps[1]
        if C.dbg.get("old_rwkv"):
            for a in range(2):
                n = 0
                for d in range(2):
                    for g in (2 * a, 2 * a + 1):
                        mm(P, pr[:, a * 128:(a + 1) * 128], Y[:, d, :, g], C.ident, n == 0, n == 3)
                        n += 1
        if C.dbg.get("old_gla"):
            for a in range(2):
                for d in range(2):
                    mm(P, pg[:, a * 128:(a + 1) * 128], Y[:, d, :, 4 + a], C.ident, d == 0, d == 1)
        if C.dbg.get("old_rwkv"):
            cp(P, "act", ya, pr[:, 0:256])
        else:
            for d in range(2):
                dma(P, "sp" if d == 0 else "pool", ytm[i][:, d, :], V(C.YT_ap[d][t * 128:(t + 1) * 128, :], C.YT_bufs[d][t]))
            tt(P, "pool", ya, ytm[i][:, 0, :], ytm[i][:, 1, :], ALU.add)
        ya4 = ya.rr("p (h k) -> p h k", h=4)
        red(P, "dve", st[:, 0:4], ya4)
        ts(P, "dve", st[:, 0:4], st[:, 0:4], 1.0 / 64.0, ALU.mult)
        tt(P, "dve", ya4, ya4, st[:, 0:4][:, :, None].bc([128, 4, 64]), ALU.subtract)
        tt(P, "pool", sq, ya, ya, ALU.mult)
        red(P, "dve", st[:, 4:8], sq.rr("p (h k) -> p h k", h=4))
        ts(P, "dve", st[:, 4:8], st[:, 4:8], 1.0 / 64.0, ALU.mult, GN_EPS, ALU.add)
        act(P, st[:, 4:8], st[:, 4:8], AF.Sqrt)
        P.op("dve", "reciprocal", out=st[:, 4:8], in_=st[:, 4:8])
        tt(P, "dve", ya4, ya4, st[:, 4:8][:, :, None].bc([128, 4, 64]), ALU.mult)
        tt(P, "pool", ya, ya, lng, ALU.mult)
        tt(P, "pool", ya, ya, F[:, 256:512], ALU.add)
        tt(P, "pool", ya, ya, F[:, 0:256], ALU.mult)
        if C.dbg.get("old_gla"):
            cp(P, "act", yg, pg[:, 0:256])
        else:
            for d in range(2):
                dma(P, "sp" if d == 0 else "pool", ytg[i][:, d, :], V(C.YTG_ap[d][t * 128:(t + 1) * 128, :], C.YTG_bufs[d][t]))
            tt(P, "pool", yg, ytg[i][:, 0, :], ytg[i][:, 1, :], ALU.add)
        yg4 = yg.rr("p (h k) -> p h k", h=4)
        tt(P, "pool", sq, yg, yg, ALU.mult)
        red(P, "dve", st[:, 8:12], sq.rr("p (h k) -> p h k", h=4))
        ts(P, "dve", st[:, 8:12], st[:, 8:12], 1.0 / 64.0, ALU.mult, EPS, ALU.add)
        act(P, st[:, 8:12], st[:, 8:12], AF.Sqrt)
        P.op("dve", "reciprocal", out=st[:, 8:12], in_=st[:, 8:12])
        tt(P, "dve", yg4, yg4, st[:, 8:12][:, :, None].bc([128, 4, 64]), ALU.mult)
        tt(P, "pool", yg, yg, F[:, 512:768], ALU.mult)
        po = C.ps[2]
        for a in range(2):
            tr(P, po[:, a * 128:(a + 1) * 128], ya[:, a * 128:(a + 1) * 128], C.ident)
            tr(P, po[:, (2 + a) * 128:(3 + a) * 128], yg[:, a * 128:(a + 1) * 128], C.ident)
        OB = ob[i]
        cp(P, "act", OB, po.rr("p (a n) -> p a n", a=4))
        for br in range(2):
            dma(P, "sp" if br == 0 else "pool",
                V(C.YB_ap[br][:, t * 128:(t + 1) * 128].rearrange("(a p) n -> p a n", p=128), C.YB_bufs[br][t]),
                OB[:, 2 * br:2 * br + 2, :])
    A.close()


def phase_attn(C, l, L):
    P, I = C.P, C.I
    A = Arena(P)
    qT = A.sb("qT", [128, 2, NTOK], BF16)
    kT = A.sb("kT", [128, 2, NTOK], BF16)
    vtm = A.sb("vtm", [128, NT, 128], BF16)
    maskw = A.sb("maskw", [128, 384])
    sink = A.sb("sink", [128, 16])
    identb = A.sb("identb", [128, 128], BF16)
    dma(P, "sp", maskw, I["maskw"])
    dma(P, "sp", sink, I["attn_sink"][l:l + 1, :].bc([128, 16]))
    cp(P, "pool", identb, C.ident)
    ua = [A.sb("ua%d" % i, [128, 512]) for i in range(2)]
    rc = [A.sb("rc%d" % i, [128, 32]) for i in range(2)]
    rs = [A.sb("rs%d" % i, [128, 32]) for i in range(2)]
    qk = A.sb("qk", [128, 6, 64])
    tmp = A.sb("tmpr", [128, 6, 32])
    kd = A.sb("kd", [128, 2, 2, 64])
    cut = C.dbg.get("attn_cut", 9)
    for t in range(NT if cut > 1 else 0):
        i = t % 2
        r0 = urow(t)
        u = ua[i]
        dma(P, "sp", u, V(C.U_ap[r0:r0 + 128, O2:O3], C.U_bufs[t]))
        u6 = u[:, 0:384].rr("p (h d) -> p h d", h=6)
        if t >= 2 and not C.dbg.get("norope"):
            dma(P, "pool", rc[i], I["ropec"][(t - 2) * 128:(t - 1) * 128, :])
            dma(P, "pool", rs[i], I["ropes"][(t - 2) * 128:(t - 1) * 128, :])
            cb = rc[i][:, None, :].bc([128, 6, 32])
            sb_ = rs[i][:, None, :].bc([128, 6, 32])
            z1, z2 = u6[:, :, 0:32], u6[:, :, 32:64]
            tt(P, "dve", qk[:, :, 0:32], z1, cb, ALU.mult)
            tt(P, "pool", tmp, z2, sb_, ALU.mult)
            tt(P, "dve", qk[:, :, 0:32], qk[:, :, 0:32], tmp, ALU.subtract)
            tt(P, "dve", qk[:, :, 32:64], z1, sb_, ALU.mult)
            tt(P, "pool", tmp, z2, cb, ALU.mult)
            tt(P, "dve", qk[:, :, 32:64], qk[:, :, 32:64], tmp, ALU.add)
        else:
            cp(P, "dve", qk, u6)
        if cut < 3:
            continue
        cp(P, "pool", kd[:, :, 0, :], qk[:, 4:6, :])
        cp(P, "pool", kd[:, :, 1, :], qk[:, 4:6, :])
        cp(P, "pool", vtm[:, t, :], u[:, 384:512])
        if cut < 4:
            continue
        pq = C.ps[t % 2]
        qf = qk.rr("p h d -> p (h d)")
        kf = kd.rr("p k r d -> p (k r d)")
        for a in range(2):
            tr(P, pq[:, a * 128:(a + 1) * 128], qf[:, a * 128:(a + 1) * 128], C.ident)
            tr(P, pq[:, (2 + a) * 128:(3 + a) * 128], kf[:, a * 128:(a + 1) * 128], C.ident)
        ts(P, "dve", qT[:, :, t * 128:(t + 1) * 128], pq[:, 0:256].rr("p (a n) -> p a n", a=2), 0.125, ALU.mult)
        cp(P, "dve", kT[:, :, t * 128:(t + 1) * 128], pq[:, 256:512].rr("p (a n) -> p a n", a=2))
    if C.dbg.get("attn_p1"):
        A.close()
        return
    sc = [A.sb("sc%d" % i, [128, 640]) for i in range(2)]
    pb = [A.sb("pb%d" % i, [128, 640], BF16) for i in range(2)]
    pTs = [A.sb("pTs%d" % i, [128, 5, 128], BF16) for i in range(2)]
    st = [A.sb("sta%d" % i, [128, 8]) for i in range(2)]
    yo = [A.sb("yo%d" % i, [128, 256]) for i in range(2)]
    oc = [A.sb("oc%d" % i, [128, 2, 128], BF16) for i in range(2)]
    psT = [V(C.psall[:, b * 512:(b + 1) * 512].bitcast(BF16), C.psb[b]) for b in (4, 5)]
    n = 0
    for t in range(NT):
        YO = yo[t % 2]
        if t >= 2:
            lo, hi = max(t - 1, 2), min(t + 1, NT - 1)
            nw = hi - lo + 1
            m0 = (lo - (t - 1)) * 128
        else:
            nw = 0
        nk = nw * 128 + 256
        nblk = nw + 2
        kblocks = ([lo + b for b in range(nw)] if nw else []) + [0, 1]
        po = C.ps[6 + (t % 2)]
        for h in range(4):
            kv, hl = h // 2, h % 2
            i = n % 2
            n += 1
            S_, Pb, PT, ST = sc[i], pb[i], pTs[i], st[i]
            qv = qT[hl * 64:(hl + 1) * 64, kv, t * 128:(t + 1) * 128]
            pa_, pc_ = C.ps[2 * i], C.ps[2 * i + 1]
            if nw:
                mm(P, pa_[:, 0:nw * 128], qv, kT[hl * 64:(hl + 1) * 64, kv, lo * 128:(hi + 1) * 128], True, True)
            mm(P, pc_[:, 0:256], qv, kT[hl * 64:(hl + 1) * 64, kv, 0:256], True, True)
            if nw:
                tt(P, "dve", S_[:, 0:nw * 128], pa_[:, 0:nw * 128], maskw[:, m0:m0 + nw * 128], ALU.add)
            cp(P, "act", S_[:, nw * 128:nk], pc_[:, 0:256])
            P.op("dve", "tensor_reduce", out=ST[:, 0:1], in_=S_[:, 0:nk], axis=AX.X, op=ALU.max)
            tt(P, "dve", ST[:, 0:1], ST[:, 0:1], sink[:, h:h + 1], ALU.max)
            ts(P, "dve", ST[:, 1:2], ST[:, 0:1], -1.0, ALU.mult)
            act(P, Pb[:, 0:nk], S_[:, 0:nk], AF.Exp, bias=ST[:, 1:2], accum_out=ST[:, 2:3])
            act(P, ST[:, 3:4], sink[:, h:h + 1], AF.Exp, bias=ST[:, 1:2])
            tt(P, "dve", ST[:, 4:5], ST[:, 2:3], ST[:, 3:4], ALU.add)
            P.op("dve", "reciprocal", out=ST[:, 5:6], in_=ST[:, 4:5])
            pt = psT[i]
            for b in range(nblk):
                tr(P, pt[:, b * 128:(b + 1) * 128], Pb[:, b * 128:(b + 1) * 128], identb)
            cp(P, "act" if h % 2 == 0 else "dve", PT[:, 0:nblk, :], pt[:, 0:nblk * 128].rr("p (b n) -> p b n", b=nblk))
            for b in range(nblk):
                mm(P, po[:, h * 64:(h + 1) * 64], PT[:, b, :], vtm[:, kblocks[b], kv * 64:(kv + 1) * 64],
                   b == 0, b == nblk - 1)
            ts(P, "dve", YO[:, h * 64:(h + 1) * 64], po[:, h * 64:(h + 1) * 64], ST[:, 5:6], ALU.mult)
        pf = C.ps[t % 2]
        for a in range(2):
            tr(P, pf[:, a * 128:(a + 1) * 128], YO[:, a * 128:(a + 1) * 128], C.ident)
        OC = oc[t % 2]
        cp(P, "act", OC, pf[:, 0:256].rr("p (a n) -> p a n", a=2))
        dma(P, "sp", V(C.YB_ap[2][:, t * 128:(t + 1) * 128].rearrange("(a p) n -> p a n", p=128), C.YB_bufs[2][t]), OC)
    A.close()


PI = math.pi
S5_BLOCKS = [(0, 256)] + [(256 + 512 * i, 512) for i in range(8)]


I32 = mybir.dt.int32
TWO_PI_HI = 6.28125
TWO_PI_LO = 2.0 * math.pi - 6.28125


def sincos(P, A, s_out, c_out, x, shape, tag):
    qi = A.sb("qi" + tag, shape, I32)
    kf = A.sb("kf" + tag, shape)
    r = A.sb("rr" + tag, shape)
    m = A.sb("mm" + tag, shape)
    for extra, out in ((0.0, s_out), (0.5 * PI, c_out)):
        ts(P, "dve", r, x, 16.0 * PI + extra, ALU.add)
        ts(P, "dve", qi, r, 1.0 / (2.0 * PI), ALU.mult)
        cp(P, "dve", kf, qi)
        stt(P, "dve", r, kf, -TWO_PI_HI, r, ALU.mult, ALU.add)
        stt(P, "dve", r, kf, -TWO_PI_LO, r, ALU.mult, ALU.add)
        ts(P, "dve", m, r, PI, ALU.is_gt)
        stt(P, "dve", r, m, -2.0 * PI, r, ALU.mult, ALU.add)
        ts(P, "dve", m, r, -PI, ALU.is_lt)
        stt(P, "dve", r, m, 2.0 * PI, r, ALU.mult, ALU.add)
        ts(P, "dve", r, r, PI, ALU.min, -PI, ALU.max)
        act(P, out, r, AF.Sin)


def phase_s5(C, l, L):
    P, I = C.P, C.I
    A = Arena(P)
    th_s = A.sb("th_s", [128, 2, 8])
    rho_s = A.sb("rho_s", [128, 2, 8])
    Ck = A.sb("Ck", [128, 2, 8, 13])
    Sk = A.sb("Sk", [128, 2, 8, 13])
    BbT = A.sb("BbT", [128, 2, 2, 8, 128], BF16)
    CT = A.sb("CT", [128, 2, 2, 8, 128], BF16)
    A0 = A
    A = Arena(P)
    tmpA = [A.sb("s5r%d" % i, [128, 1024]) for i in range(8)]
    lre, lim, ldt, t_s, t_c, t_a, t_b, t_d = tmpA
    pw2f = A.sb("pw2", [128, 16])
    dma(P, "sp", pw2f, I["pw2"][0:1, :].bc([128, 16]))
    pw2 = pw2f[:, 0:13]
    fR = [A.sb("fR%d" % d, [128, 1024]) for d in range(2)]
    fI = [A.sb("fI%d" % d, [128, 1024]) for d in range(2)]
    sm = A.sb("sm", [128, 2, 3, 8])
    dma(P, "sp", sm, I["s5_sm"][l])
    act(P, sm[:, :, 2, :], sm[:, :, 2, :], AF.Exp)
    tt(P, "dve", th_s, sm[:, :, 1, :], sm[:, :, 2, :], ALU.mult)
    tt(P, "dve", rho_s, sm[:, :, 0, :], sm[:, :, 2, :], ALU.mult)
    act(P, rho_s, rho_s, AF.Exp)
    ang13 = A.sb("ang13", [128, 16, 13])
    tt(P, "dve", ang13, th_s.rr("p d j -> p (d j)")[:, :, None].bc([128, 16, 13]), pw2[:, None, :].bc([128, 16, 13]), ALU.mult)
    sincos(P, A, Sk.rr("p d j k -> p (d j) k"), Ck.rr("p d j k -> p (d j) k"), ang13, [128, 16, 13], "k")
    for d in range(2):
        dma(P, "sp", lre, I["s5_rows"][l, d, 0:1, :].bc([128, 1024]))
        dma(P, "pool", lim, I["s5_rows"][l, d, 1:2, :].bc([128, 1024]))
        dma(P, "sp", ldt, I["s5_rows"][l, d, 2:3, :].bc([128, 1024]))
        act(P, ldt, ldt, AF.Exp)
        tt(P, "dve", t_a, lim, ldt, ALU.mult)
        sincos(P, A, t_s, t_c, t_a, [128, 1024], "r%d" % d)
        tt(P, "dve", t_a, lre, ldt, ALU.mult)
        act(P, t_a, t_a, AF.Exp)
        tt(P, "dve", t_c, t_c, t_a, ALU.mult)
        tt(P, "dve", t_s, t_s, t_a, ALU.mult)
        ts(P, "dve", t_c, t_c, -1.0, ALU.add)
        tt(P, "dve", t_a, lre, lre, ALU.mult)
        tt(P, "pool", t_b, lim, lim, ALU.mult)
        tt(P, "dve", t_a, t_a, t_b, ALU.add)
        P.op("dve", "reciprocal", out=t_a, in_=t_a)
        tt(P, "dve", t_b, t_c, lre, ALU.mult)
        tt(P, "pool", t_d, t_s, lim, ALU.mult)
        tt(P, "dve", t_b, t_b, t_d, ALU.add)
        tt(P, "dve", fR[d], t_b, t_a, ALU.mult)
        tt(P, "dve", t_b, t_s, lre, ALU.mult)
        tt(P, "pool", t_d, t_c, lim, ALU.mult)
        tt(P, "dve", t_b, t_b, t_d, ALU.subtract)
        tt(P, "dve", fI[d], t_b, t_a, ALU.mult)
    bt_f = A.sb("bt_f", [128, 2, 8, 128])
    dma(P, "sp", bt_f[:, 0], I["s5_bt"][l, 0].rr("j c s -> c j s"))
    dma(P, "pool", bt_f[:, 1], I["s5_bt"][l, 1].rr("j c s -> c j s"))
    for d in range(2):
        fr = fR[d].rr("p (j s) -> p j s", j=8)
        fi = fI[d].rr("p (j s) -> p j s", j=8)
        ta = t_a.rr("p (j s) -> p j s", j=8)
        tb = t_b.rr("p (j s) -> p j s", j=8)
        tt(P, "dve", ta, bt_f[:, 0], fr, ALU.mult)
        tt(P, "pool", tb, bt_f[:, 1], fi, ALU.mult)
        tt(P, "dve", BbT[:, d, 0], ta, tb, ALU.subtract)
        tt(P, "dve", ta, bt_f[:, 0], fi, ALU.mult)
        tt(P, "pool", tb, bt_f[:, 1], fr, ALU.mult)
        tt(P, "dve", BbT[:, d, 1], ta, tb, ALU.add)
        for ri in range(2):
            ctf = t_c if ri == 0 else t_d
            dma(P, "sp" if ri == 0 else "pool", ctf.rr("p (j c) -> p j c", j=8), I["s5_ct"][l, d, ri].rr("j s c -> s j c"))
            if ri == 0:
                cp(P, "pool", CT[:, d, 0], ctf.rr("p (j c) -> p j c", j=8))
            else:
                ts(P, "pool", CT[:, d, 1], ctf.rr("p (j c) -> p j c", j=8), -1.0, ALU.mult)
    A.close()
    A = A0
    cut = C.dbg.get("s5_cut", 99)
    if cut <= 1:
        A.close(); return
    if not C.dbg.get("s5_small"):
        ct, sn = A.sb("ct", [128, NTOK]), A.sb("sn", [128, NTOK])
        w_re, w_im = A.sb("w_re", [128, NTOK]), A.sb("w_im", [128, NTOK])
    uB = A.sb("uB", [128, 2, NTOK], BF16)
    yacc = A.sb("yacc", [128, 2, NTOK])
    x_re, x_im = A.sb("x_re", [128, NTOK], BF16), A.sb("x_im", [128, NTOK], BF16)
    dsk = A.sb("dsk", [128, 16])
    bgl = A.sb("bgl", [128, 16])
    dma(P, "sp", dsk, I["s5_d_fm"][l])
    dma(P, "sp", bgl, I["s5_bglu_fm"][l])
    wglu = load_bf16(P, A, "wglu", [128, 2, 256], I["s5_w_glu"][l].rr("(k p) n -> p k n", p=128))
    tmA = [[A.sb("tm%d_%d" % (b, i), [128, 512]) for i in range(4)] for b in range(2)]
    ut = [tmA[1][i][:, 0:256] for i in range(2)]
    var = C.dbg.get("s5_var", 9)
    for t in range(NT if var > 0 else 0):
        i = t % 2
        r0 = urow(t)
        dma(P, "sp" if i == 0 else "pool", ut[i], V(C.U_ap[r0:r0 + 128, O3:O4], C.U_bufs[t]))
        pp = C.ps[i]
        for a in range(2):
            tr(P, pp[:, a * 128:(a + 1) * 128], ut[i][:, a * 128:(a + 1) * 128], C.ident)
        if var < 2:
            continue
        for a in range(2):
            cp(P, "dve", uB[:, a, t * 128:(t + 1) * 128], pp[:, a * 128:(a + 1) * 128])
        for a in range(2):
            ts(P, "dve", yacc[:, a, t * 128:(t + 1) * 128], pp[:, a * 128:(a + 1) * 128], dsk[:, a:a + 1], ALU.mult)
    tm = tmA[0]
    if cut <= 2:
        A.close(); return
    nb = 0
    for d in range(2):
        for j in range(8):
            jt = j // 4
            th = th_s[:, d, j:j + 1]
            P.op("dve", "memset", ap=ct[:, 0:1], constant=1.0)
            P.op("dve", "memset", ap=sn[:, 0:1], constant=0.0)
            k = 0
            n = 1
            while n < NTOK:
                m = min(n, NTOK - n)
                ck, sk = Ck[:, d, j, k:k + 1], Sk[:, d, j, k:k + 1]
                e1, e2 = ("dve", "pool") if m >= 256 else ("dve", "dve")
                ts(P, e1, ct[:, n:n + m], ct[:, 0:m], ck, ALU.mult)
                ts(P, e2, sn[:, n:n + m], sn[:, 0:m], ck, ALU.mult)
                ts(P, e2, tm[0][:, 0:min(m, 512)] if m <= 512 else w_re[:, 0:m], sn[:, 0:m], sk, ALU.mult)
                ts(P, e1, tm[1][:, 0:min(m, 512)] if m <= 512 else w_im[:, 0:m], ct[:, 0:m], sk, ALU.mult)
                ta_ = tm[0][:, 0:m] if m <= 512 else w_re[:, 0:m]
                tb_ = tm[1][:, 0:m] if m <= 512 else w_im[:, 0:m]
                tt(P, e1, ct[:, n:n + m], ct[:, n:n + m], ta_, ALU.subtract)
                tt(P, e2, sn[:, n:n + m], sn[:, n:n + m], tb_, ALU.add)
                n += m
                k += 1
            if cut <= 3:
                A.close(); return
            for bi, (t0, n) in enumerate(S5_BLOCKS):
                if d == 0:
                    rhs = uB[:, jt, t0:t0 + n]
                else:
                    last = (255 - t0) if t0 < 256 else (4607 - t0)
                    rhs = cust(uB, jt * NTOK + last, [(-1, n)])
                pr, pi_ = C.ps[(nb % 2) * 2], C.ps[(nb % 2) * 2 + 1]
                nb += 1
                mm(P, pr[:, 0:n], BbT[:, d, 0, j, :], rhs, True, True)
                mm(P, pi_[:, 0:n], BbT[:, d, 1, j, :], rhs, True, True)
                c_, s_ = ct[:, t0:t0 + n], sn[:, t0:t0 + n]
                tm = tmA[bi % 2]
                tt(P, "dve", tm[0][:, 0:n], pr[:, 0:n], c_, ALU.mult)
                tt(P, "dve", tm[1][:, 0:n], pi_[:, 0:n], s_, ALU.mult)
                tt(P, "pool", w_re[:, t0:t0 + n], tm[0][:, 0:n], tm[1][:, 0:n], ALU.add)
                tt(P, "dve", tm[2][:, 0:n], pi_[:, 0:n], c_, ALU.mult)
                tt(P, "dve", tm[3][:, 0:n], pr[:, 0:n], s_, ALU.mult)
                tt(P, "pool", w_im[:, t0:t0 + n], tm[2][:, 0:n], tm[3][:, 0:n], ALU.subtract)
            if cut <= 4:
                A.close(); return
            rb = rho_s[:, d, j:j + 1].bc([128, NTOK])
            P.op("dve", "tensor_tensor_scan", out=w_re, data0=rb, data1=w_re, initial=0.0, op0=ALU.mult, op1=ALU.add)
            P.op("dve", "tensor_tensor_scan", out=w_im, data0=rb, data1=w_im, initial=0.0, op0=ALU.mult, op1=ALU.add)
            if cut <= 5:
                A.close(); return
            for bi, (t0, n) in enumerate(S5_BLOCKS):
                c_, s_ = ct[:, t0:t0 + n], sn[:, t0:t0 + n]
                tm = tmA[bi % 2]
                tt(P, "pool", tm[0][:, 0:n], w_re[:, t0:t0 + n], c_, ALU.mult)
                tt(P, "pool", tm[1][:, 0:n], w_im[:, t0:t0 + n], s_, ALU.mult)
                tt(P, "dve", x_re[:, t0:t0 + n], tm[0][:, 0:n], tm[1][:, 0:n], ALU.subtract)
                tt(P, "dve", tm[2][:, 0:n], w_re[:, t0:t0 + n], s_, ALU.mult)
                tt(P, "dve", tm[3][:, 0:n], w_im[:, t0:t0 + n], c_, ALU.mult)
                tt(P, "pool", x_im[:, t0:t0 + n], tm[2][:, 0:n], tm[3][:, 0:n], ALU.add)
                py = C.ps[4 + (bi % 2)]
                if d == 0:
                    xr, xi = x_re[:, t0:t0 + n], x_im[:, t0:t0 + n]
                    k0 = t0
                else:
                    k0 = (256 - t0 - n) if t0 < 256 else (4608 - t0 - n)
                    s_last = t0 + n - 1
                    xr, xi = cust(x_re, s_last, [(-1, n)]), cust(x_im, s_last, [(-1, n)])
                mm(P, py[:, 0:n], CT[:, d, 0, j, :], xr, True, False)
                mm(P, py[:, 0:n], CT[:, d, 1, j, :], xi, False, True)
                tt(P, "dve", yacc[:, jt, k0:k0 + n], yacc[:, jt, k0:k0 + n], py[:, 0:n], ALU.add)
    if cut <= 7:
        A.close(); return
    glb = uB
    tm = tmA[0]
    ob = [x_re[:, 0:1024].rr("p (a n) -> p a n", a=2), x_im[:, 0:1024].rr("p (a n) -> p a n", a=2)]
    for bi, (t0, n) in enumerate(S5_BLOCKS):
        for a in range(2):
            y = yacc[:, a, t0:t0 + n]
            tt(P, "pool", tm[0][:, 0:n], y, y, ALU.mult)
            ts(P, "dve", tm[0][:, 0:n], tm[0][:, 0:n], 0.044715, ALU.mult, 1.0, ALU.add)
            tt(P, "pool", tm[0][:, 0:n], tm[0][:, 0:n], y, ALU.mult)
            act(P, tm[0][:, 0:n], tm[0][:, 0:n], AF.Sigmoid, scale=1.5957691216057308)
            tt(P, "dve", y, y, tm[0][:, 0:n], ALU.mult)
            cp(P, "pool", glb[:, a, t0:t0 + n], y)
        OB = ob[bi % 2]
        for a in range(2):
            pz = C.ps[6 + a]
            for kt in range(2):
                mm(P, pz[:, 0:n], wglu[:, kt, a * 128:(a + 1) * 128], glb[:, kt, t0:t0 + n], kt == 0, kt == 1)
            act(P, tm[1 + a][:, 0:n], pz[:, 0:n], AF.Sigmoid, bias=bgl[:, a:a + 1])
            tt(P, "dve", OB[:, a, 0:n], yacc[:, a, t0:t0 + n], tm[1 + a][:, 0:n], ALU.mult)
        tl = [t for t in range(NT) if t * 128 >= t0 and t * 128 < t0 + n]
        dma(P, "sp", V(C.YB_ap[3][:, t0:t0 + n].rearrange("(a p) n -> p a n", p=128), tuple(C.YB_bufs[3][t] for t in tl)),
            OB[:, :, 0:n])
    A.close()


TOKBLK = [(0, 256)] + [(256 + 512 * i, 512) for i in range(8)]


def phase_win_gates(C, l, L, hfm):
    P, I = C.P, C.I
    A = Arena(P)
    wst = [A.sb("wgs%d" % i, [128, 8, 512]) for i in range(2)]
    wb = [A.sb("wgb%d" % i, [128, 8, 512], BF16) for i in range(2)]
    gst = [A.sb("gst%d" % i, [128, 512], BF16) for i in range(4)]
    wv = I["w_in"][l].rr("(k p) n -> p k n", p=128)
    n = 0
    for cb in range(8):
        c0 = O4 + cb * 512
        dma(P, "sp" if cb % 2 == 0 else "pool", wst[cb % 2], wv[:, :, c0:c0 + 512])
        cp(P, "act" if cb % 2 == 0 else "dve", wb[cb % 2], wst[cb % 2])
        w = wb[cb % 2]
        for mi in range(4):
            row0 = cb * 512 + mi * 128
            for bi, (t0, nn) in enumerate(TOKBLK):
                ps = C.ps[n % 4]
                g = gst[n % 4]
                for k in range(8):
                    mm(P, ps[:, 0:nn], w[:, k, mi * 128:(mi + 1) * 128], hfm[:, k, t0:t0 + nn], k == 0, k == 7)
                cp(P, "act" if n % 2 == 0 else "dve", g[:, 0:nn], ps[:, 0:nn])
                dma(P, "sp" if n % 2 == 0 else "pool", V(C.Gt_ap[row0:row0 + 128, t0:t0 + nn], C.Gt_bufs[bi]), g[:, 0:nn])
                n += 1
    A.close()


def phase_merge(C, l, L):
    P, I = C.P, C.I
    A = Arena(P)
    wbr = A.sb("wbr", [128, 4, 2, 1024], BF16)
    wout = A.sb("wout", [128, 8, 1024], BF16)
    wst = A.sb("wmst", [128, 8, 1024])
    for i in range(4):
        dma(P, "sp", wst[:, 0:2, :], I["w_branch"][l, i].rr("(k p) n -> p k n", p=128))
        cp(P, "act", wbr[:, i], wst[:, 0:2, :])
    dma(P, "sp", wst, I["w_out"][l].rr("(k p) n -> p k n", p=128))
    cp(P, "act", wout, wst)
    yb = [A.sb("myb%d" % i, [128, 4, 2, 512], BF16) for i in range(2)]
    gt4 = [A.sb("mgt%d" % i, [128, 4, 512], BF16) for i in range(2)]
    sg4 = [A.sb("msg%d" % i, [128, 4, 512]) for i in range(2)]
    tmp = A.sb("mtmp", [128, 512])
    acc = A.sb("macc", [128, 512])
    mg = [A.sb("mmg%d" % i, [128, 8, 512], BF16) for i in range(2)]
    xt = [A.sb("mxt%d" % i, [128, 1024]) for i in range(2)]
    tm2 = A.sb("mtm2", [128, 1024])
    n = 0
    nx = 0
    for bi, (t0, nn) in enumerate(TOKBLK):
        YB = yb[bi % 2]
        tl = [t for t in range(NT) if t0 <= t * 128 < t0 + nn]
        for i in range(4):
            dma(P, "sp" if i % 2 == 0 else "pool", YB[:, i, :, 0:nn],
                V(C.YB_ap[i][:, t0:t0 + nn].rearrange("(a p) n -> p a n", p=128), tuple(C.YB_bufs[i][t] for t in tl)))
        MG = mg[bi % 2]
        for m in range(8):
            g4 = gt4[m % 2]
            s4 = sg4[m % 2]
            dma(P, "sp" if m % 2 == 0 else "pool", g4[:, :, 0:nn],
                V(C.Gt_ap.rearrange("(i r) n -> r i n", i=4)[m * 128:(m + 1) * 128, :, t0:t0 + nn], C.Gt_bufs[bi]))
            act(P, s4[:, :, 0:nn], g4[:, :, 0:nn], AF.Sigmoid)
            for i in range(4):
                s_ = s4[:, i, :]
                ps = C.ps[n % 4]
                n += 1
                for kt in range(2):
                    mm(P, ps[:, 0:nn], wbr[:, i, kt, m * 128:(m + 1) * 128], YB[:, i, kt, 0:nn], kt == 0, kt == 1)
                if i == 0:
                    tt(P, "dve", acc[:, 0:nn], ps[:, 0:nn], s_[:, 0:nn], ALU.mult)
                elif i < 3:
                    tt(P, "dve", tmp[:, 0:nn], ps[:, 0:nn], s_[:, 0:nn], ALU.mult)
                    tt(P, "pool", acc[:, 0:nn], acc[:, 0:nn], tmp[:, 0:nn], ALU.add)
                else:
                    tt(P, "dve", tmp[:, 0:nn], ps[:, 0:nn], s_[:, 0:nn], ALU.mult)
                    tt(P, "dve", MG[:, m, 0:nn], acc[:, 0:nn], tmp[:, 0:nn], ALU.add)
        for ti, t in enumerate(tl):
            j = 1 if t < 2 else 0
            x = xt[nx % 2]
            nx += 1
            dma(P, "sp", x, xsrc(C, l, t))
            for half in range(2):
                po = C.ps[4 + half + 2 * (nx % 2)]
                for k in range(8):
                    mm(P, po, MG[:, k, ti * 128:(ti + 1) * 128], wout[:, k, half * 512:(half + 1) * 512], k == 0, k == 7)
                tt(P, "dve", tm2[:, half * 512:(half + 1) * 512], po, L.grow[0][j][:, half * 512:(half + 1) * 512], ALU.mult)
            tt(P, "pool", x, x, tm2, ALU.add)
            dma(P, "pool", C.xres[t], x)
    A.close()
    C.x_in_scratch = True


def make_router(C, l, L, RA):
    P, I = C.P, C.I
    wr = RA.sb("wr", [128, 8, 36])
    brow = RA.sb("brow", [128, 36])
    dma(P, "sp", wr, I["w_router"][l].rr("(k p) n -> p k n", p=128))
    dma(P, "sp", brow, I["b_router"][l:l + 1, :].bc([128, 36]))
    lg = RA.sb("lg", [128, 36])
    st = RA.sb("rst", [128, 16])
    oh = RA.sb("roh", [128, 4])
    em = RA.sb("rem", [128, 32])
    em2 = RA.sb("rem2", [128, 32])
    oh1 = RA.sb("roh1", [128, 32])
    oh2 = RA.sb("roh2", [128, 32])
    wg = RA.sb("rwg", [128, 32])
    junk = RA.sb("rjunk", [128, 4])

    def per_tile(t, hf):
        pl = C.ps[6]
        for k in range(8):
            mm(P, pl[:, 0:36], hf[:, k, :], wr[:, k, :], k == 0, k == 7)
        tt(P, "dve", lg, pl[:, 0:36], brow, ALU.add)
        g, e = lg[:, 0:4], lg[:, 4:36]
        P.op("dve", "tensor_reduce", out=st[:, 0:1], in_=g, axis=AX.X, op=ALU.max)
        ts(P, "dve", oh, g, st[:, 0:1], ALU.is_equal)
        ts(P, "dve", st[:, 1:2], st[:, 0:1], -1.0, ALU.mult)
        act(P, junk, g, AF.Exp, bias=st[:, 1:2], accum_out=st[:, 2:3])
        P.op("dve", "reciprocal", out=st[:, 3:4], in_=st[:, 2:3])
        ts(P, "dve", oh, oh, 1e30, ALU.mult, -1e30, ALU.add)
        tt(P, "dve", em.rr("p (g k) -> p g k", g=4), e.rr("p (g k) -> p g k", g=4),
           oh[:, :, None].bc([128, 4, 8]), ALU.add)
        P.op("dve", "tensor_reduce", out=st[:, 4:5], in_=em, axis=AX.X, op=ALU.max)
        ts(P, "dve", oh1, em, st[:, 4:5], ALU.is_equal)
        stt(P, "dve", em2, oh1, -1e30, em, ALU.mult, ALU.add)
        P.op("dve", "tensor_reduce", out=st[:, 5:6], in_=em2, axis=AX.X, op=ALU.max)
        ts(P, "dve", oh2, em2, st[:, 5:6], ALU.is_equal)
        tt(P, "dve", st[:, 6:7], st[:, 5:6], st[:, 4:5], ALU.subtract)
        act(P, st[:, 7:8], st[:, 6:7], AF.Exp)
        ts(P, "dve", st[:, 8:9], st[:, 7:8], 1.0, ALU.add)
        P.op("dve", "reciprocal", out=st[:, 9:10], in_=st[:, 8:9])
        tt(P, "dve", st[:, 10:11], st[:, 7:8], st[:, 9:10], ALU.mult)
        ts(P, "dve", wg, oh1, st[:, 9:10], ALU.mult)
        stt(P, "dve", wg, oh2, st[:, 10:11], wg, ALU.mult, ALU.add)
        ts(P, "dve", wg, wg, st[:, 3:4], ALU.mult)
        pt = C.ps[7]
        tr(P, pt[0:32, 0:128], wg, C.ident)
        cp(P, "dve", L.WT[:, t * 128:(t + 1) * 128], pt[0:32, 0:128])
    return per_tile


MOE_GROUPS = [list(range(0, 12)), list(range(12, 24)), list(range(24, 34))]


def phase_moe(C, l, L, H2, last):
    P, I = C.P, C.I
    A = Arena(P)
    hg = A.sb("hg", [128, 8, 12 * 128], BF16)
    wst = [A.sb("ews%d" % i, [128, 4, 512]) for i in range(2)]
    wgu = [A.sb("wgu%d" % i, [128, 2, 8, 512], BF16) for i in range(2)]
    wd = [A.sb("wd%d" % i, [128, 4, 1024], BF16) for i in range(2)]
    yacc = A.sb("eyacc", [128, 12, 1024])
    hid = [A.sb("hid%d" % i, [128, 4, 512], BF16) for i in range(2)]
    sil = [A.sb("sil%d" % i, [128, 512], BF16) for i in range(2)]
    tu = [A.sb("etu%d" % i, [128, 512]) for i in range(2)]
    xt = [A.sb("ext%d" % i, [128, 1024]) for i in range(2)]
    tm2 = A.sb("etm2", [128, 1024])
    ne = 0
    nst = 0
    nb = 0
    for G in MOE_GROUPS:
        tiles = [t for t in G if not (last and t < 2)]
        if not tiles:
            continue
        blocks = [tiles[i:i + 4] for i in range(0, len(tiles), 4)]
        g0 = tiles[0] * 128
        gn = len(tiles) * 128
        dma(P, "sp", hg[:, :, 0:gn], H2[:, :, g0:g0 + gn])
        for e in range(32):
            WGU, WD = wgu[ne % 2], wd[ne % 2]
            ne += 1
            for gi, nm in enumerate(("w_exp_gate", "w_exp_up")):
                src = I[nm][l, e].rr("(k p) n -> p k n", p=128)
                for hf_ in range(2):
                    s_ = wst[nst % 2]
                    dma(P, "sp" if nst % 2 == 0 else "pool", s_, src[:, hf_ * 4:(hf_ + 1) * 4, :])
                    cp(P, "act", WGU[:, gi, hf_ * 4:(hf_ + 1) * 4, :], s_)
                    nst += 1
            srcd = I["w_exp_down"][l, e].rr("(k p) n -> p k n", p=128)
            for hf_ in range(2):
                s_ = wst[nst % 2]
                dma(P, "sp" if nst % 2 == 0 else "pool", s_.rr("p k n -> p (k n)").rr("p (k n) -> p k n", k=2), srcd[:, hf_ * 2:(hf_ + 1) * 2, :])
                cp(P, "dve", WD[:, hf_ * 2:(hf_ + 1) * 2, :], s_.rr("p k n -> p (k n)").rr("p (k n) -> p k n", k=2))
                nst += 1
            for blk in blocks:
                t0 = blk[0] * 128
                nn = len(blk) * 128
                HID = hid[nb % 2]
                psW = C.ps[0]
                mm(P, psW[:, 0:nn], C.ident[0:32, e:e + 1].bc([32, 128]), L.WT[:, t0:t0 + nn], True, True)
                for f in range(4):
                    i2 = (nb * 4 + f) % 2
                    psG, psU = C.ps[1 + 2 * i2], C.ps[2 + 2 * i2]
                    for k in range(8):
                        mm(P, psG[:, 0:nn], WGU[:, 0, k, f * 128:(f + 1) * 128], hg[:, k, t0 - g0:t0 - g0 + nn], k == 0, k == 7)
                    for k in range(8):
                        mm(P, psU[:, 0:nn], WGU[:, 1, k, f * 128:(f + 1) * 128], hg[:, k, t0 - g0:t0 - g0 + nn], k == 0, k == 7)
                    act(P, sil[i2][:, 0:nn], psG[:, 0:nn], AF.Silu)
                    tt(P, "dve", tu[i2][:, 0:nn], psU[:, 0:nn], sil[i2][:, 0:nn], ALU.mult)
                    tt(P, "dve", HID[:, f, 0:nn], tu[i2][:, 0:nn], psW[:, 0:nn], ALU.mult)
                for ti, t in enumerate(blk):
                    ya = yacc[:, t - tiles[0], :]
                    for half in range(2):
                        po = C.ps[5 + (nb * 8 + ti * 2 + half) % 3]
                        for f in range(4):
                            mm(P, po, HID[:, f, ti * 128:(ti + 1) * 128], WD[:, f, half * 512:(half + 1) * 512], f == 0, f == 3)
                        if e == 0:
                            cp(P, "dve", ya[:, half * 512:(half + 1) * 512], po)
                        else:
                            tt(P, "dve", ya[:, half * 512:(half + 1) * 512], ya[:, half * 512:(half + 1) * 512], po, ALU.add)
                nb += 1
        for t in tiles:
            j = 1 if t < 2 else 0
            x = xt[t % 2]
            dma(P, "sp", x, C.xres[t])
            tt(P, "dve", tm2, yacc[:, t - tiles[0], :], L.grow[1][j], ALU.mult)
            tt(P, "pool", x, x, tm2, ALU.add)
            dma(P, "pool", C.xres[t], x)
    A.close()


def final_norm(C):
    P, I = C.P, C.I
    A = Arena(P)
    g = A.sb("fg", [128, D])
    dma(P, "sp", g, I["final_g"][0:1, :].bc([128, D]))
    xt = [A.sb("fxt%d" % i, [128, D]) for i in range(2)]
    junk = A.sb("fjunk", [128, D])
    st = [A.sb("fst%d" % i, [128, 2]) for i in range(2)]
    for t in range(2, NT):
        x, s = xt[t % 2], st[t % 2]
        dma(P, "sp" if t % 2 == 0 else "pool", x, C.xres[t])
        act(P, junk, x, AF.Square, accum_out=s[:, 0:1])
        ts(P, "dve", s[:, 1:2], s[:, 0:1], 1.0 / D, ALU.mult, EPS, ALU.add)
        act(P, s[:, 1:2], s[:, 1:2], AF.Sqrt)
        P.op("dve", "reciprocal", out=s[:, 1:2], in_=s[:, 1:2])
        stt(P, "dve", x, x, s[:, 1:2], g, ALU.mult, ALU.mult)
        dma(P, "sp" if t % 2 == 1 else "pool", V(C.out.ap[(t - 2) * 128:(t - 1) * 128, :], Buf("o%d" % t)), x)
    A.close()


CH_COLS = 3072


def alloc_chunk_heads(A, dd):
    def mkh(name, shape, dt=F32):
        return [A.sb("%s_%d_%d" % (name, dd, h), shape, dt) for h in range(4)]
    AM = mkh("cAM", [128, 4, 128], BF16)
    XB = mkh("cXB", [128, 2, 128], BF16)
    XX = [[AM[h][:, 0:2, :] for h in range(4)], [XB[h] for h in range(4)]]
    AakT = [AM[h][:, 2, :] for h in range(4)]
    ArbT = [AM[h][:, 3, :] for h in range(4)]
    ArkT, TT = [mkh("cM%d" % i, [128, 128], BF16) for i in range(2)]
    PM = mkh("cPM", [128, 2, 64], BF16)
    Ap = [PM[h][:, 0, :] for h in range(4)]
    M1 = [PM[h][:, 1, :] for h in range(4)]
    U0 = mkh("cU0", [128, 64], BF16)
    RpT = mkh("cRpT", [64, 128])
    DPC = mkh("cDPC", [128, 64])
    Y0cc = mkh("cY0cc", [64, 2, 64])
    Y0c = [[Y0cc[h][:, c, :] for h in range(4)] for c in range(2)]
    GH = mkh("cGH", [64, 4, 64])
    GT = [[GH[h][:, 2 * c, :] for h in range(4)] for c in range(2)]
    Hc = [[GH[h][:, 2 * c + 1, :] for h in range(4)] for c in range(2)]
    gA = mkh("gA", [128, 128])
    gY0 = [mkh("gY0%d" % c, [64, 64]) for c in range(2)]
    gH = [mkh("gH%d" % c, [32, 64]) for c in range(2)]
    return (AM, XB, XX, AakT, ArbT, ArkT, TT, PM, Ap, M1, U0, RpT, DPC, Y0cc, Y0c, GH, GT, Hc, gA, gY0, gH)


def phase_chunk(C, l, L):
    P, I = C.P, C.I
    A = Arena(P)
    mk = A.sb("cmasks", [128, 7, 128])
    dma(P, "sp", mk, I["cmasks"])
    idn = C.ident
    chs = [A.sb("chs%d" % i, [128, CH_COLS]) for i in range(2)]
    ST = [[A.sb("cST%d%d" % (d, h), [64, 64]) for h in range(4)] for d in range(2)]
    for d in range(2):
        for h in range(4):
            P.op("dve", "memset", ap=ST[d][h], constant=0.0)

    def mk2(name, shape, dt=F32):
        return [A.sb("%s%d" % (name, i), shape, dt) for i in range(2)]
    TOT, incS, Ein, Enin, Eex, Eend, Etot, tmpx, tmpy = [mk2("cE%d" % i, [128, 256]) for i in range(9)]
    at, rt, bt, kt, bh, kh = [mk2("cq%d" % i, [128, 256]) for i in range(6)]
    aT, rTb, bT, kT = [mk2("cT%d" % i, [128, 2, 128], BF16) for i in range(4)]
    rT = mk2("cTr", [128, 2, 128])
    bhc = [mk2("cbhc%d" % c, [128, 256], BF16) for c in range(2)]
    khc = [mk2("ckhc%d" % c, [128, 256], BF16) for c in range(2)]
    at_b = mk2("cat_b", [128, 256], BF16)
    v_b = mk2("cv_b", [128, 256], BF16)
    IM = A.sb("cIM", [128, 64])
    tt(P, "pool", IM, idn[:, 0:64], idn[:, 64:128], ALU.add)

    HB = []
    for dd in range(2):
        HB.append(alloc_chunk_heads(A, dd))
    MK4 = [A.sb("cMK4%d" % d, [128, 4, 128]) for d in range(2)]
    for d in range(2):
        ms_, mst_, mit_ = (0, 1, 2) if d == 0 else (3, 4, 5)
        for i_, mi_ in enumerate((ms_, mst_, mst_, mit_)):
            cp(P, "pool", MK4[d][:, i_, :], mk[:, mi_, :])
    yo = [A.sb("cyo%d" % i, [64, 2, 256]) for i in range(2)]
    STg = [[A.sb("gST%d%d" % (d, h), [32, 64]) for h in range(4)] for d in range(2)]
    for d in range(2):
        for h in range(4):
            P.op("dve", "memset", ap=STg[d][h], constant=0.0)
    gTOT, gincS, gEin, gEnin, gEend, gEtot, gtmp = [mk2("gE%d" % i, [128, 128]) for i in range(7)]
    gq, gk, gkh = [mk2("gq%d" % i, [128, 128]) for i in range(3)]
    gkhc = [mk2("gkhc%d" % c, [128, 128]) for c in range(2)]
    gqT, gkT, gPT = [[mk2("gT%d_%d" % (i, h), [32, 128]) for h in range(4)] for i in range(3)]
    gyo = [A.sb("gyo%d" % i, [64, 2, 256]) for i in range(2)]
    ps = C.ps
    border = [1, 0] + list(range(NT - 1, 1, -1))
    H4 = range(4)
    it = 0
    cut = C.dbg.get("chunk_cut", 99)

    def body(n, d):
        if True:
            (AM, XB, XX, AakT, ArbT, ArkT, TT, PM, Ap, M1, U0, RpT, DPC, Y0cc, Y0c, GH, GT, Hc, gA, gY0, gH) = HB[d]
            ps = C.ps[4 * d:4 * d + 4] + C.ps[4 - 4 * d:8 - 4 * d]
            t = n if d == 0 else border[n]
            q = d
            ch = chs[q]
            YO = yo[q]
            dma(P, "sp" if d == 0 else "pool", ch, V(C.CH_ap[t * 128:(t + 1) * 128, :], C.CH_bufs[t]))
            lw = ch[:, d * 256:(d + 1) * 256]
            ke = ch[:, 512 + d * 256:768 + d * 256]
            b_ = ch[:, 1024 + d * 256:1280 + d * 256]
            a_, r_, v_ = ch[:, 1536:1792], ch[:, 1792:2048], ch[:, 2048:2304]
            m_s, m_st, m_it = (0, 1, 2) if d == 0 else (3, 4, 5)
            pc = ps[4 + q]
            mm(P, pc[:, 0:256], mk[:, m_it, :], lw, True, True)
            mm(P, pc[:, 256:512], mk[:, 6, :], lw, True, True)
            cp(P, "dve", TOT[q], pc[:, 256:512])
            cp(P, "dve", incS[q], pc[:, 0:256])
            act(P, Ein[q], incS[q], AF.Exp)
            act(P, Enin[q], incS[q], AF.Exp, scale=-1.0)
            tt(P, "pool", tmpx[q], incS[q], lw, ALU.subtract)
            act(P, Eex[q], tmpx[q], AF.Exp)
            tt(P, "pool", tmpy[q], TOT[q], incS[q], ALU.subtract)
            act(P, Eend[q], tmpy[q], AF.Exp)
            act(P, Etot[q], TOT[q], AF.Exp)
            tt(P, "pool", at[q], a_, Eex[q], ALU.mult)
            cp(P, "act", at_b[q], at[q])
            cp(P, "act", v_b[q], v_)
            tt(P, "pool", rt[q], r_, Ein[q], ALU.mult)
            tt(P, "pool", bt[q], b_, Enin[q], ALU.mult)
            tt(P, "pool", kt[q], ke, Enin[q], ALU.mult)
            tt(P, "pool", bh[q], b_, Eend[q], ALU.mult)
            tt(P, "pool", kh[q], ke, Eend[q], ALU.mult)
            for c in range(2):
                ts(P, "pool", bhc[c][q], bh[q], mk[:, 6, c * 64:c * 64 + 1], ALU.mult)
                ts(P, "pool", khc[c][q], kh[q], mk[:, 6, c * 64:c * 64 + 1], ALU.mult)
            yield
            for qi, (src, dst) in enumerate(((at, aT), (rt, rT), (bt, bT), (kt, kT))):
                pb_ = ps[6 + qi % 2]
                for a2 in range(2):
                    tr(P, pb_[:, a2 * 128:(a2 + 1) * 128], src[q][:, a2 * 128:(a2 + 1) * 128], idn)
                cp(P, "dve", dst[q], pb_[:, 0:256].rr("p (a n) -> p a n", a=2))
                if qi == 1:
                    cp(P, "dve", rTb[q], pb_[:, 0:256].rr("p (a n) -> p a n", a=2))

            def hv(h):
                pair, hl = h // 2, h % 2
                return pair, slice(hl * 64, (hl + 1) * 64), slice(h * 64, (h + 1) * 64)
            if cut <= 2:
                return
            yield
            for h in H4:
                pair, hs, hc = hv(h)
                pA = ps[h]
                mm(P, pA[:, 0:128], aT[q][hs, pair, :], bT[q][hs, pair, :], True, True)
                mm(P, pA[:, 128:256], bT[q][hs, pair, :], aT[q][hs, pair, :], True, True)
                mm(P, pA[:, 256:384], kT[q][hs, pair, :], aT[q][hs, pair, :], True, True)
                mm(P, pA[:, 384:512], bT[q][hs, pair, :], rTb[q][hs, pair, :], True, True)
            for h in H4:
                pA = ps[h]
                tt(P, "dve", AM[h].rr("p a n -> p (a n)"), pA, MK4[d].rr("p a n -> p (a n)"), ALU.mult)
                tt(P, "pool", TT[h], AM[h][:, 1, :], idn, ALU.add)
            for h in H4:
                pair, hs, hc = hv(h)
                mm(P, ps[h][:, 0:128], kT[q][hs, pair, :], rTb[q][hs, pair, :], True, True)
            for h in H4:
                tt(P, "dve", ArkT[h], ps[h][:, 0:128], mk[:, m_it, :], ALU.mult)
            if cut <= 3:
                return
            yield
            for s in range(5):
                yield
                XXc, XXn = XX[s % 2], XX[(s + 1) % 2]
                for h in H4:
                    pX = ps[h]
                    mm(P, pX[:, 128:256], XXc[h][:, 1, :], XXc[h][:, 0, :], True, True)
                    if s < 4:
                        mm(P, pX[:, 256:384], XXc[h][:, 0, :], XXc[h][:, 1, :], True, True)
                for h in H4:
                    pX = ps[h]
                    if s < 4:
                        cp(P, "dve", XXn[h], pX[:, 128:384].rr("p (a n) -> p a n", a=2))
                    else:
                        cp(P, "dve", XXn[h][:, 0, :], pX[:, 128:256])
                for h in H4:
                    mm(P, ps[h][:, 384:512], XXn[h][:, 0, :], TT[h], True, True)
                for h in H4:
                    tt(P, "dve", TT[h], TT[h], ps[h][:, 384:512], ALU.add)
            if cut <= 4:
                return
            yield
            for h in H4:
                pair, hs, hc = hv(h)
                mm(P, ps[h][:, 0:64], TT[h], at_b[q][:, hc], True, True)
                mm(P, ps[h][:, 64:128], AakT[h], v_b[q][:, hc], True, True)
            for h in H4:
                cp(P, "dve", PM[h], ps[h][:, 0:128].rr("p (a n) -> p a n", a=2))
            for h in H4:
                mm(P, ps[h][:, 128:192], TT[h], M1[h], True, True)
            for h in H4:
                cp(P, "dve", U0[h], ps[h][:, 128:192])
            for h in H4:
                pair, hs, hc = hv(h)
                for c in range(2):
                    cs = slice(c * 64, (c + 1) * 64)
                    mm(P, ps[h][0:64, 192 + c * 64:256 + c * 64], ArbT[h][:, cs], U0[h], True, False)
                    mm(P, ps[h][0:64, 192 + c * 64:256 + c * 64], ArkT[h][:, cs], v_b[q][:, hc], False, True)
                mm(P, ps[h][0:64, 320:448], Ap[h], ArbT[h], True, True)
                tt(P, "pool", DPC[h], IM, Etot[q][:, hc], ALU.mult)
            for h in H4:
                pair, hs, hc = hv(h)
                cp(P, "dve", Y0cc[h], ps[h][0:64, 192:320].rr("p (a n) -> p a n", a=2))
                tt(P, "dve", RpT[h], ps[h][0:64, 320:448], rT[q][hs, pair, :], ALU.add)
            if cut <= 5:
                return
            for h in H4:
                pair, hs, hc = hv(h)
                pG = ps[h]
                for c in range(2):
                    cs = slice(c * 64, (c + 1) * 64)
                    o = c * 128
                    mm(P, pG[0:64, o:o + 64], Ap[h], bhc[c][q][:, hc], True, False)
                    mm(P, pG[0:64, o:o + 64], idn[:, cs], DPC[h], False, True)
                    mm(P, pG[0:64, o + 64:o + 128], bhc[c][q][:, hc], U0[h], True, False)
                    mm(P, pG[0:64, o + 64:o + 128], khc[c][q][:, hc], v_b[q][:, hc], False, True)
            for h in H4:
                pG = ps[h]
                cp(P, "dve", GH[h], pG[0:64, 0:256].rr("p (a n) -> p a n", a=4))
            if cut <= 6:
                return
            yield
            for c in ((0, 1) if d == 0 else (1, 0)):
                cs = slice(c * 64, (c + 1) * 64)
                for h in H4:
                    pG = ps[h]
                    S_ = ST[d][h]
                    mm(P, pG[0:64, 256:320], RpT[h][:, cs], S_, True, False)
                    mm(P, pG[0:64, 256:320], idn[0:64, 0:64], Y0c[c][h], False, True)
                    mm(P, pG[0:64, 320:384], GT[c][h], S_, True, False)
                    mm(P, pG[0:64, 320:384], idn[0:64, 0:64], Hc[c][h], False, True)
                for h in H4:
                    pair, hs, hc = hv(h)
                    pG = ps[h]
                    cp(P, "dve", YO[:, c, hc], pG[0:64, 256:320])
                    cp(P, "dve", ST[d][h], pG[0:64, 320:384])
            dma(P, "sp" if d == 0 else "pool",
                V(C.YT_ap[d][t * 128:(t + 1) * 128, :].rearrange("(c p) n -> p c n", p=64), C.YT_bufs[d][t]), YO)
            yield
            GYO = gyo[q]
            glw = ch[:, 2304 + d * 128:2432 + d * 128]
            gq_, gk_, gv_ = ch[:, 2560:2688], ch[:, 2688:2816], ch[:, 2816:3072]
            pcg = ps[4 + q]
            mm(P, pcg[:, 0:128], mk[:, m_it, :], glw, True, True)
            mm(P, pcg[:, 128:256], mk[:, 6, :], glw, True, True)
            cp(P, "dve", gincS[q], pcg[:, 0:128])
            cp(P, "dve", gTOT[q], pcg[:, 128:256])
            act(P, gEin[q], gincS[q], AF.Exp)
            act(P, gEnin[q], gincS[q], AF.Exp, scale=-1.0)
            tt(P, "pool", gtmp[q], gTOT[q], gincS[q], ALU.subtract)
            act(P, gEend[q], gtmp[q], AF.Exp)
            act(P, gEtot[q], gTOT[q], AF.Exp)
            tt(P, "pool", gq[q], gq_, gEin[q], ALU.mult)
            tt(P, "pool", gk[q], gk_, gEnin[q], ALU.mult)
            tt(P, "pool", gkh[q], gk_, gEend[q], ALU.mult)
            for c in range(2):
                ts(P, "pool", gkhc[c][q], gkh[q], mk[:, 6, c * 64:c * 64 + 1], ALU.mult)
            for h in H4:
                pT_ = ps[6 + h % 2]
                g32 = slice(h * 32, (h + 1) * 32)
                tr(P, pT_[0:32, 0:128], gq[q][:, g32], idn)
                tr(P, pT_[0:32, 128:256], gk[q][:, g32], idn)
                tr(P, pT_[0:32, 256:384], gEtot[q][:, g32], idn)
                cp(P, "dve", gqT[h][q], pT_[0:32, 0:128])
                cp(P, "dve", gkT[h][q], pT_[0:32, 128:256])
                cp(P, "dve", gPT[h][q], pT_[0:32, 256:384])
            for h in H4:
                mm(P, ps[h][:, 0:128], gkT[h][q], gqT[h][q], True, True)
            for h in H4:
                tt(P, "dve", gA[h], ps[h][:, 0:128], mk[:, m_it, :], ALU.mult)
            for h in H4:
                hc = slice(h * 64, (h + 1) * 64)
                g32 = slice(h * 32, (h + 1) * 32)
                for c in range(2):
                    cs = slice(c * 64, (c + 1) * 64)
                    mm(P, ps[h][0:64, 128 + c * 64:192 + c * 64], gA[h][:, cs], gv_[:, hc], True, True)
                    mm(P, ps[h][0:32, 256 + c * 64:320 + c * 64], gkhc[c][q][:, g32], gv_[:, hc], True, True)
            for h in H4:
                for c in range(2):
                    cp(P, "dve", gY0[c][h], ps[h][0:64, 128 + c * 64:192 + c * 64])
                    cp(P, "dve", gH[c][h], ps[h][0:32, 256 + c * 64:320 + c * 64])
            for c in ((0, 1) if d == 0 else (1, 0)):
                cs = slice(c * 64, (c + 1) * 64)
                for h in H4:
                    S_ = STg[d][h]
                    mm(P, ps[h][0:64, 384:448], gqT[h][q][:, cs], S_, True, False)
                    mm(P, ps[h][0:64, 384:448], idn[0:64, 0:64], gY0[c][h], False, True)
                for h in H4:
                    hc = slice(h * 64, (h + 1) * 64)
                    S_ = STg[d][h]
                    cp(P, "dve", GYO[:, c, hc], ps[h][0:64, 384:448])
                    stt(P, "dve", S_, S_, gPT[h][q][:, c * 64:c * 64 + 1], gH[c][h], ALU.mult, ALU.add)
            dma(P, "sp" if d == 1 else "pool",
                V(C.YTG_ap[d][t * 128:(t + 1) * 128, :].rearrange("(c p) n -> p c n", p=64), C.YTG_bufs[d][t]), GYO)
    for n in range(NT if cut > 50 else 1):
        gens = [body(n, 0), body(n, 1)]
        while gens:
            nxt = []
            for g in gens:
                try:
                    next(g)
                    nxt.append(g)
                except StopIteration:
                    pass
            gens = nxt
    A.close()


class LayerState:
    pass


def layer(C, l):
    P = C.P
    LA = Arena(P)
    L = LayerState()
    L.mod = LA.sb("mod", [128, 48, 2])
    L.sc1 = LA.sb("sc1", [128, 8, 2])
    L.sc2 = LA.sb("sc2", [128, 8, 2])
    L.grow = [[LA.sb("grow%d%d" % (ii, j), [128, 1024]) for j in range(2)] for ii in range(2)]
    phase_ada(C, l, L)
    if C.dbg.get("dump") and l == C.dbg.get("layer", 0):
        dma(P, "sp", C.dout("d_mod", [128, 48, 2]), L.mod)
        for ii in range(2):
            for j in range(2):
                dma(P, "sp", C.dout("d_grow%d%d" % (ii, j), [128, 1024]), L.grow[ii][j])
    HA = Arena(P)
    hfm = HA.sb("hfm", [128, 8, NTOK], BF16)
    phase_norm(C, l, L, 1, hfm)
    if C.dbg.get("dump") and l == C.dbg.get("layer", 0):
        dma(P, "sp", C.dout("d_hfm", [128, 8, NTOK], BF16), hfm)
    phase_win_tm(C, l, L, hfm)
    if not C.dbg.get("skip_gates"):
        phase_win_gates(C, l, L, hfm)
    HA.close()
    if C.dbg.get("stop_after") == "win":
        LA.close(); return
    if not C.dbg.get("skip_ab"):
        phase_prep(C, l, L)
        if C.dbg.get("stop_after") == "prep":
            LA.close(); return
        if C.dbg.get("old_gla") and not C.dbg.get("skip_scan"):
            phase_scan(C, l, C.dbg.get("nchunks"))
        if not C.dbg.get("old_rwkv"):
            phase_chunk(C, l, L)
        if C.dbg.get("stop_after") == "chunk":
            LA.close(); return
        if C.dbg.get("stop_after") == "scan":
            LA.close(); return
        phase_fin_ab(C, l, L)
    if C.dbg.get("stop_after") == "fin":
        LA.close(); return
    if not C.dbg.get("skip_attn"):
        phase_attn(C, l, L)
    if C.dbg.get("stop_after") == "attn":
        LA.close(); return
    if not C.dbg.get("skip_s5"):
        phase_s5(C, l, L)
    if C.dbg.get("stop_after") == "s5":
        LA.close(); return
    phase_merge(C, l, L)
    if C.dbg.get("stop_after") == "merge":
        LA.close(); return
    WA = Arena(P)
    L.WT = WA.sb("WT", [32, NTOK])
    HA = Arena(P)
    hfm2 = HA.sb("hfm2", [128, 8, NTOK], BF16)
    RA = Arena(P)
    phase_norm(C, l, L, 2, hfm2, per_tile=make_router(C, l, L, RA))
    RA.close()
    if C.dbg.get("dump") and l == C.dbg.get("layer", 0):
        dma(P, "sp", C.dout("d_hfm2", [128, 8, NTOK], BF16), hfm2)
        dma(P, "sp", C.dout("d_WT", [32, NTOK]), L.WT)
    H2 = V(C.H2_ap, C.H2_buf)
    dma(P, "sp", H2, hfm2)
    HA.close()
    if C.dbg.get("stop_after") == "norm2":
        WA.close(); LA.close(); return
    phase_moe(C, l, L, H2, l == DEPTH - 1)
    WA.close()
    LA.close()


def make_maskw():
    m = np.zeros((128, 384), np.float32)
    i = np.arange(128)[:, None]
    j = np.arange(128)[None, :]
    m[:, 0:128] = np.where(j >= i, 0.0, -1e30)
    m[:, 256:384] = np.where(j <= i, 0.0, -1e30)
    return m


def make_rope():
    rows = SEQ // 64
    row = np.repeat(np.arange(rows, dtype=np.float32), 64)
    col = np.tile(np.arange(64, dtype=np.float32), rows)
    inv = (10000.0 ** (-np.arange(16, dtype=np.float32) / 16)).astype(np.float32)
    ang = np.concatenate([row[:, None] * inv, col[:, None] * inv], axis=-1).astype(np.float32)
    return np.cos(ang).astype(np.float32), np.sin(ang).astype(np.float32)


ROPE = make_rope()


def s5_host(A):
    f = np.float32
    lre, lim, ldt = A("s5_lam_re"), A("s5_lam_im"), A("s5_log_dt")
    ldt_e = np.repeat(ldt[..., None], 64, axis=-1)
    rows = np.stack([lre.reshape(DEPTH, 2, 1024), lim.reshape(DEPTH, 2, 1024), ldt_e.reshape(DEPTH, 2, 1024)], axis=2)
    sm = rows.reshape(DEPTH, 2, 3, 8, 128).transpose(0, 4, 1, 2, 3)
    bre, bim = A("s5_b_re"), A("s5_b_im")
    bt = np.zeros((DEPTH, 2, 8, 128, 128), f)
    cre, cim = A("s5_c_re"), A("s5_c_im")
    ct = np.zeros((DEPTH, 2, 2, 8, 128, 128), f)
    for g in range(16):
        j, hh = g // 2, g % 2
        c0 = (g % 8) * 16
        for ri, b in enumerate((bre, bim)):
            bt[:, ri, j, c0:c0 + 16, hh * 64:(hh + 1) * 64] = b[:, g].transpose(0, 2, 1)
        for ri, c in enumerate((cre, cim)):
            ct[:, :, ri, j, hh * 64:(hh + 1) * 64, c0:c0 + 16] = c[:, :, g].transpose(0, 1, 3, 2)
    return {
        "s5_sm": np.ascontiguousarray(sm, f), "s5_rows": np.ascontiguousarray(rows, f),
        "s5_bt": bt, "s5_ct": ct,
        "pw2": (2.0 ** np.arange(16)).astype(f).reshape(1, 16),
        "s5_d_fm": np.ascontiguousarray(np.pad(A("s5_d").reshape(DEPTH, 2, 128).transpose(0, 2, 1), ((0, 0), (0, 0), (0, 14)))),
        "s5_bglu_fm": np.ascontiguousarray(np.pad(A("s5_b_glu").reshape(DEPTH, 2, 128).transpose(0, 2, 1), ((0, 0), (0, 0), (0, 14)))),
        "s5_w_glu": A("s5_w_glu"),
    }


def make_sele():
    s = np.zeros((32, 32, 128), np.float32)
    for e in range(32):
        s[e, e, :] = 1.0
    return s


def make_cmasks():
    r = np.arange(128)[:, None]
    c = np.arange(128)[None, :]
    same = (r // 64) == (c // 64)
    m = np.zeros((128, 7, 128), np.float32)
    m[:, 0] = same & (c < r)
    m[:, 1] = same & (r < c)
    m[:, 2] = same & (r <= c)
    m[:, 3] = same & (c > r)
    m[:, 4] = same & (r > c)
    m[:, 5] = same & (r >= c)
    m[:, 6] = same
    return m


def make_sel():
    s = np.zeros((128, 64, 128), np.float32)
    for j in range(64):
        for hh in range(2):
            s[2 * j + hh, j, hh * 64:(hh + 1) * 64] = 1.0
    return s


def blkdiag(mats):
    n = len(mats)
    L, r, c = mats[0].shape
    o = np.zeros((L, n * r, n * c), np.float32)
    for i, m in enumerate(mats):
        o[:, i * r:(i + 1) * r, i * c:(i + 1) * c] = m
    return o


def host_inputs(inputs, b):
    f = np.float32

    def A(k):
        return np.asarray(inputs[k], f)
    c = np.asarray(inputs["c"], f)[b]
    cctx = np.asarray(inputs["c_ctx"], f)
    cc = np.stack([c.reshape(8, 128).T, cctx.reshape(8, 128).T], axis=-1)
    m = {
        "xb": np.ascontiguousarray(np.asarray(inputs["x"], f)[b]),
        "ctxb": np.ascontiguousarray(np.asarray(inputs["ctx"], f)[b]),
        "cc": np.ascontiguousarray(cc),
        "w_ada": np.asarray(inputs["w_ada"], f),
        "b_ada": np.asarray(inputs["b_ada"], f),
        "b_ada_fm": np.ascontiguousarray(np.asarray(inputs["b_ada"], f).reshape(DEPTH, 48, 128).transpose(0, 2, 1)),
        "g1_fm": np.ascontiguousarray(np.asarray(inputs["norm1_g"], f).reshape(DEPTH, 8, 128).transpose(0, 2, 1)),
        "g2_fm": np.ascontiguousarray(np.asarray(inputs["norm2_g"], f).reshape(DEPTH, 8, 128).transpose(0, 2, 1)),
        "w_in": np.asarray(inputs["w_in"], f),
        "ident": np.eye(128, dtype=f),
        "sel": make_sel(),
        "maskw": make_maskw(),
        "ropec": ROPE[0],
        "ropes": ROPE[1],
        "attn_sink": np.ascontiguousarray(np.pad(A("attn_sink"), ((0, 0), (0, 12)))),
        **s5_host(A),
        "cmasks": make_cmasks(),
        "w_branch": A("w_branch"),
        "w_out": A("w_out"),
        "w_router": np.ascontiguousarray(np.concatenate([A("w_router_g"), A("w_router_e")], axis=2)),
        "b_router": np.ascontiguousarray(np.concatenate([A("b_router_g"), A("b_router_e")], axis=1)),
        "w_exp_gate": A("w_exp_gate"),
        "w_exp_up": A("w_exp_up"),
        "w_exp_down": A("w_exp_down"),
        "pv": np.ascontiguousarray(np.concatenate([
            A("rwkv_mu").reshape(DEPTH, -1), A("rwkv_kk"), A("rwkv_ka"), A("rwkv_rk").reshape(DEPTH, -1),
            A("rwkv_w0").reshape(DEPTH, -1), A("rwkv_a0").reshape(DEPTH, -1), A("rwkv_ln_g"),
            A("gla_ab").reshape(DEPTH, -1), A("gla_ln_g")], axis=1)),
        "w1cat": np.ascontiguousarray(np.concatenate([A("rwkv_w1")[:, 0], A("rwkv_w1")[:, 1],
                                                      A("rwkv_a1")[:, 0], A("rwkv_a1")[:, 1]], axis=2)),
        "w2blk": blkdiag([A("rwkv_w2")[:, 0], A("rwkv_w2")[:, 1], A("rwkv_a2")[:, 0], A("rwkv_a2")[:, 1]]),
        "g1": A("rwkv_g1"),
        "g2": A("rwkv_g2"),
        "a2blk": blkdiag([A("gla_a2")[:, 0], A("gla_a2")[:, 1]]),
        "final_g": np.asarray(inputs["final_norm_g"], f).reshape(1, D),
    }
    return m


def kernel(**inputs):
    nc = build_program()
    in_maps = [host_inputs(inputs, b) for b in range(8)]
    res = run_bass_kernel_spmd(nc, in_maps, core_ids=list(range(8)))
    return np.stack([r["out"] for r in res.results], axis=0)
```

```python
import math
from contextlib import ExitStack

import numpy as np
import concourse.bass as bass
import concourse.mybir as mybir
from concourse.bass_utils import run_bass_kernel_spmd

F32 = mybir.dt.float32
BF16 = mybir.dt.bfloat16
ALU = mybir.AluOpType
AF = mybir.ActivationFunctionType
AX = mybir.AxisListType

D = 1024
SEQ = 4096
CTX = 256
NT = (SEQ + CTX) // 128
NTOK = SEQ + CTX
DEPTH = 2
EPS = 1e-6
GN_EPS = 64e-5
O1, O2, O3, O4, PIN = 1024, 1824, 2336, 2592, 6688
PV_MU, PV_KK, PV_KA, PV_RK, PV_W0, PV_A0, PV_LNG, PV_GAB, PV_GLNG = 0, 1024, 1280, 1536, 1792, 2304, 2816, 3072, 3328
NPV = 3584

DEBUG = False
PENDING = "PENDING"
WKEYS = ("out", "accum_out", "ap")
SKEYS = ("scalar1", "scalar2", "scale", "bias", "scalar")


class Buf:
    __slots__ = ("name", "w", "rd", "ws")

    def __init__(self, name=""):
        self.name = name
        self.w = None
        self.rd = {}
        self.ws = False


class V:
    __slots__ = ("ap", "bufs")

    def __init__(self, ap, bufs):
        self.ap = ap
        self.bufs = bufs if isinstance(bufs, tuple) else (bufs,)

    def __getitem__(self, k):
        return V(self.ap[k], self.bufs)

    def rr(self, pat, **kw):
        return V(self.ap.rearrange(pat, **kw), self.bufs)

    def bc(self, shape):
        return V(self.ap.to_broadcast(list(shape)), self.bufs)

    def bitcast(self, dt):
        return V(self.ap.bitcast(dt), self.bufs)

    def wb(self, *bufs):
        return V(self.ap, tuple(bufs))

    @property
    def shape(self):
        return tuple(self.ap.shape)


class Prog:
    ENG = ("pe", "act", "dve", "pool", "sp")
    K = 6
    STRICT_ALL = False

    def __init__(self, nc):
        self.nc = nc
        self.eng = {"pe": nc.tensor, "act": nc.scalar, "dve": nc.vector, "pool": nc.gpsimd, "sp": nc.sync}
        self.sem = {e: nc.alloc_semaphore("s_" + e) for e in self.ENG}
        self.cnt = {e: 0 for e in self.ENG}
        self.dsem = {q: [nc.alloc_semaphore("d_%s%d" % (q, i)) for i in range(self.K)] for q in ("sp", "act", "pool")}
        self.dcnt = {q: 0 for q in self.dsem}
        self.known = {e: {} for e in self.ENG}
        self.pend_r = []
        self.pend_w = []
        self.uid = 0
        self.nins = 0

    def _need(self, e, tok, is_dma, strict=False):
        if tok is None:
            return
        if tok is PENDING:
            assert e == "pe" and not is_dma, "dependency on an unmarked PE op"
            return
        sem, val, owner = tok
        if owner == e and not is_dma and not (strict and e != "pe") and not self.STRICT_ALL:
            return
        k = self.known[e]
        if k.get(sem.num, 0) >= val:
            return
        self.eng[e].wait_ge(sem, val)
        self.nins += 1
        k[sem.num] = val

    def op(self, e, meth, mark=True, lax=False, **kw):
        reads, writes, args, sreads, awrites = [], [], {}, [], []
        for k, v in kw.items():
            if isinstance(v, V):
                (writes if k in WKEYS else reads).extend(v.bufs)
                if k in SKEYS:
                    sreads.extend(v.bufs)
                if k == "accum_out":
                    awrites.extend(v.bufs)
                args[k] = v.ap
            else:
                args[k] = v
        is_dma = meth == "dma_start"
        for b in reads:
            self._need(e, b.w, is_dma, strict=(not lax) or b.ws or e == "act" or b in sreads)
        for b in writes:
            self._need(e, b.w, is_dma)
            for t in b.rd.values():
                self._need(e, t, is_dma)
        if is_dma:
            n = self.dcnt[e]
            sem = self.dsem[e][n % self.K]
            r = n // self.K
            if r > 0:
                self._need(e, (sem, 16 * r, None), True)
            ins = getattr(self.eng[e], meth)(**args)
            ins.then_inc(sem, 16)
            self.dcnt[e] = n + 1
            tok = (sem, 16 * (r + 1), None)
            key = ("d", sem.num)
        else:
            ins = getattr(self.eng[e], meth)(**args)
            key = e
            if mark:
                self.cnt[e] += 1
                ins.then_inc(self.sem[e], 1)
                tok = (self.sem[e], self.cnt[e], e)
                if e == "pe" and (self.pend_r or self.pend_w):
                    for b in self.pend_r:
                        if b.rd.get("pe") is PENDING:
                            b.rd["pe"] = tok
                    for b in self.pend_w:
                        if b.w is PENDING:
                            b.w = tok
                    self.pend_r = []
                    self.pend_w = []
            else:
                assert e == "pe"
                tok = PENDING
                self.pend_r.extend(reads)
                self.pend_w.extend(writes)
        self.nins += 1
        for b in reads:
            b.rd[key] = tok
        for b in writes:
            b.w = tok
            b.rd = {}
            b.ws = (b in awrites) or e == "act"
        return ins

    def barrier(self):
        assert not self.pend_r and not self.pend_w
        toks = [(self.sem[e], self.cnt[e], e) for e in self.ENG if self.cnt[e] > 0]
        for q in self.dsem:
            n = self.dcnt[q]
            for i in range(self.K):
                c = (n - i + self.K - 1) // self.K if n > i else 0
                if c > 0:
                    toks.append((self.dsem[q][i], 16 * c, None))
        for e in self.ENG:
            for t in toks:
                self._need(e, t, False)

    def name(self, s):
        self.uid += 1
        return "%s_%d" % (s, self.uid)

    def dram(self, name, shape, dt, kind="Internal"):
        return self.nc.dram_tensor(name, list(shape), dt, kind=kind).ap()


class Arena:
    def __init__(self, P):
        self.P = P
        self.stack = ExitStack()

    def sb(self, name, shape, dt=F32):
        h = self.stack.enter_context(self.P.nc.sbuf_tensor(self.P.name(name), list(shape), dt))
        return V(h.ap(), Buf(name))

    def close(self):
        self.P.barrier()
        self.stack.close()


def dma(P, q, out, in_):
    return P.op(q, "dma_start", out=out, in_=in_)


def mm(P, out, lhsT, rhs, start, stop, mark=None):
    return P.op("pe", "matmul", mark=(stop if mark is None else mark), out=out, lhsT=lhsT, rhs=rhs,
                start=start, stop=stop)


def tr(P, out, in_, ident, mark=True):
    return P.op("pe", "transpose", mark=mark, out=out, in_=in_, identity=ident)


def tt(P, e, out, in0, in1, op):
    return P.op(e, "tensor_tensor", out=out, in0=in0, in1=in1, op=op)


def ts(P, e, out, in0, s1, op0, s2=None, op1=None, **kw):
    if op1 is None:
        return P.op(e, "tensor_scalar", out=out, in0=in0, scalar1=s1, scalar2=None, op0=op0, **kw)
    return P.op(e, "tensor_scalar", out=out, in0=in0, scalar1=s1, scalar2=s2, op0=op0, op1=op1, **kw)


def act(P, out, in_, func, **kw):
    return P.op("act", "activation", out=out, in_=in_, func=func, **kw)


def cp(P, e, out, in_):
    if e == "act":
        return act(P, out, in_, AF.Copy)
    return P.op(e, "tensor_copy", out=out, in_=in_)


class Ctx:
    pass


def build_program(dbg=None):
    dbg = dbg or {}
    nc = bass.Bass("TRN2", target_bir_lowering=False)
    P = Prog(nc)
    C = Ctx()
    C.P, C.nc, C.dbg = P, nc, dbg
    skind = "ExternalOutput" if dbg.get("expose") else "Internal"

    def din(name, shape, dt=F32):
        return V(nc.dram_tensor(name, list(shape), dt, kind="ExternalInput").ap(), Buf(name))

    I = {}
    I["xb"] = din("xb", [SEQ, D])
    I["ctxb"] = din("ctxb", [CTX, D])
    I["cc"] = din("cc", [128, 8, 2])
    I["w_ada"] = din("w_ada", [DEPTH, D, 6 * D])
    I["b_ada"] = din("b_ada", [DEPTH, 6 * D])
    I["b_ada_fm"] = din("b_ada_fm", [DEPTH, 128, 48])
    I["g1_fm"] = din("g1_fm", [DEPTH, 128, 8])
    I["g2_fm"] = din("g2_fm", [DEPTH, 128, 8])
    I["w_in"] = din("w_in", [DEPTH, D, PIN])
    I["ident"] = din("ident", [128, 128])
    I["sel"] = din("sel", [128, 64, 128])
    I["maskw"] = din("maskw", [128, 384])
    I["ropec"] = din("ropec", [SEQ, 32])
    I["ropes"] = din("ropes", [SEQ, 32])
    I["attn_sink"] = din("attn_sink", [DEPTH, 16])
    I["s5_sm"] = din("s5_sm", [DEPTH, 128, 2, 3, 8])
    I["s5_rows"] = din("s5_rows", [DEPTH, 2, 3, 1024])
    I["s5_bt"] = din("s5_bt", [DEPTH, 2, 8, 128, 128])
    I["s5_ct"] = din("s5_ct", [DEPTH, 2, 2, 8, 128, 128])
    I["pw2"] = din("pw2", [1, 16])
    I["cmasks"] = din("cmasks", [128, 7, 128])
    I["w_branch"] = din("w_branch", [DEPTH, 4, 256, D])
    I["w_out"] = din("w_out", [DEPTH, D, D])
    I["w_router"] = din("w_router", [DEPTH, D, 36])
    I["b_router"] = din("b_router", [DEPTH, 36])
    I["w_exp_gate"] = din("w_exp_gate", [DEPTH, 32, D, 512])
    I["w_exp_up"] = din("w_exp_up", [DEPTH, 32, D, 512])
    I["w_exp_down"] = din("w_exp_down", [DEPTH, 32, 512, D])
    I["s5_d_fm"] = din("s5_d_fm", [DEPTH, 128, 16])
    I["s5_bglu_fm"] = din("s5_bglu_fm", [DEPTH, 128, 16])
    I["s5_w_glu"] = din("s5_w_glu", [DEPTH, 256, 256])
    I["pv"] = din("pv", [DEPTH, NPV])
    I["w1cat"] = din("w1cat", [DEPTH, 256, 128])
    I["w2blk"] = din("w2blk", [DEPTH, 128, 1024])
    I["g1"] = din("g1", [DEPTH, 256, 64])
    I["g2"] = din("g2", [DEPTH, 64, 256])
    I["a2blk"] = din("a2blk", [DEPTH, 32, 256])
    I["final_g"] = din("final_g", [1, D])
    C.I = I

    out = V(nc.dram_tensor("out", [SEQ, D], F32, kind="ExternalOutput").ap(), Buf("out"))
    C.out = out

    def dout(name, shape, dt=F32):
        return V(nc.dram_tensor(name, list(shape), dt, kind="ExternalOutput").ap(), Buf(name))
    C.dout = dout

    xres_ap = P.dram("xres", [NTOK, D], F32, kind=skind)
    C.xres = [V(xres_ap[t * 128:(t + 1) * 128, :], Buf("xres%d" % t)) for t in range(NT)]
    C.U_ap = P.dram("U", [NTOK + 3, O4], F32, kind=skind)
    C.U_bufs = [Buf("U%d" % t) for t in range(NT)]
    C.U_pad = Buf("Upad")
    C.STR_ap = [P.dram("STR%d" % d, [NTOK, 2, 1024], BF16, kind=skind) for d in range(2)]
    C.STR_bufs = [[Buf("STR%d_%d" % (d, t)) for t in range(NT)] for d in range(2)]
    C.Vs_ap = P.dram("Vs", [128, NTOK, 6], F32, kind=skind)
    C.Vs_bufs = [Buf("Vs%d" % t) for t in range(NT)]
    C.Y_ap = [P.dram("Y%d" % d, [128, NTOK, 6], F32, kind=skind) for d in range(2)]
    C.Y_bufs = [[Buf("Y%d_%d" % (d, c)) for c in range(NTOK // 64)] for d in range(2)]
    C.FIN_ap = P.dram("FIN", [NTOK, 768], F32, kind=skind)
    C.FIN_bufs = [Buf("FIN%d" % t) for t in range(NT)]
    C.YB_ap = P.dram("YB", [4, 256, NTOK], BF16, kind=skind)
    C.YB_bufs = [[Buf("YB%d_%d" % (i, t)) for t in range(NT)] for i in range(4)]
    C.CH_ap = P.dram("CH", [NTOK, 3072], F32, kind=skind)
    C.CH_bufs = [Buf("CH%d" % t) for t in range(NT)]
    C.YT_ap = [P.dram("YT%d" % d, [NTOK, 256], F32, kind=skind) for d in range(2)]
    C.YT_bufs = [[Buf("YT%d_%d" % (d, t)) for t in range(NT)] for d in range(2)]
    C.YTG_ap = [P.dram("YTG%d" % d, [NTOK, 256], F32, kind=skind) for d in range(2)]
    C.YTG_bufs = [[Buf("YTG%d_%d" % (d, t)) for t in range(NT)] for d in range(2)]
    C.Gt_ap = P.dram("Gt", [4096, NTOK], BF16, kind=skind)
    C.Gt_bufs = [Buf("Gt%d" % b) for b in range(9)]
    C.H2_ap = P.dram("H2", [128, 8, NTOK], BF16, kind=skind)
    C.H2_buf = Buf("H2")

    G = Arena(P)
    C.G = G
    C.psall = nc.alloc_psum_tensor("psall", [128, 4096], F32).ap()
    C.psb = [Buf("ps%d" % i) for i in range(8)]
    C.ps = [V(C.psall[:, i * 512:(i + 1) * 512], C.psb[i]) for i in range(8)]
    C.ident = G.sb("ident", [128, 128])
    dma(P, "sp", C.ident, I["ident"])

    for l in range(DEPTH):
        layer(C, l)
        if dbg.get("stop_layer") == l:
            break
    if not dbg.get("stop"):
        final_norm(C)
    P.barrier()
    return nc


def urow(t):
    return 1 + t * 128 if t < 2 else 258 + (t - 2) * 128


def xsrc(C, l, t):
    if l == 0 and not C.__dict__.get("x_in_scratch"):
        if t < 2:
            return C.I["ctxb"][t * 128:(t + 1) * 128, :]
        return C.I["xb"][(t - 2) * 128:(t - 1) * 128, :]
    return C.xres[t]


def phase_ada(C, l, L):
    P, I = C.P, C.I
    A = Arena(P)
    cc = A.sb("cc", [128, 8, 2])
    sc = A.sb("sc", [128, 8, 2])
    screp = A.sb("screp", [128, 8, 2, 128])
    bfm = A.sb("bfm", [128, 48])
    g1 = A.sb("g1", [128, 8])
    g2 = A.sb("g2", [128, 8])
    wst = [A.sb("wst%d" % i, [128, 8, 512]) for i in range(2)]
    brow = [A.sb("brow%d" % i, [128, 1024]) for i in range(2)]
    dma(P, "sp", cc, I["cc"])
    dma(P, "sp", bfm, I["b_ada_fm"][l])
    dma(P, "sp", g1, I["g1_fm"][l])
    dma(P, "sp", g2, I["g2_fm"][l])
    for ii, i in enumerate((2, 5)):
        dma(P, "pool", brow[ii], I["b_ada"][l:l + 1, i * 1024:(i + 1) * 1024].bc([128, 1024]))
    act(P, sc, cc, AF.Silu)
    for k in range(8):
        for j in range(2):
            cp(P, "dve", screp[:, k, j, :], sc[:, k, j:j + 1].bc([128, 128]))
    wv = I["w_ada"][l].rr("(k p) n -> p k n", p=128)
    psA = C.ps[0]
    for c in range(12):
        w = wst[c % 2]
        dma(P, "sp" if c % 2 == 0 else "pool", w, wv[:, :, c * 512:(c + 1) * 512])
        for mi in range(4):
            m = c * 4 + mi
            for k in range(8):
                mm(P, psA[:, m * 2:(m + 1) * 2], w[:, k, mi * 128:(mi + 1) * 128], sc[:, k, :], k == 0, k == 7)
        if c in (4, 5, 10, 11):
            ii = 0 if c < 6 else 1
            half = c % 2
            for j in range(2):
                pr = C.ps[1 + j]
                for k in range(8):
                    mm(P, pr, screp[:, k, j, :], w[:, k, :], k == 0, k == 7)
                tt(P, "dve", L.grow[ii][j][:, half * 512:(half + 1) * 512], pr,
                   brow[ii][:, half * 512:(half + 1) * 512], ALU.add)
    tt(P, "dve", L.mod, psA[:, 0:96].rr("p (m j) -> p m j", j=2), bfm[:, :, None].bc([128, 48, 2]), ALU.add)
    ts(P, "dve", L.sc1, L.mod[:, 8:16, :], 1.0, ALU.add)
    tt(P, "dve", L.sc1, L.sc1, g1[:, :, None].bc([128, 8, 2]), ALU.mult)
    ts(P, "dve", L.sc2, L.mod[:, 32:40, :], 1.0, ALU.add)
    tt(P, "dve", L.sc2, L.sc2, g2[:, :, None].bc([128, 8, 2]), ALU.mult)
    A.close()


def phase_norm(C, l, L, which, hfm, per_tile=None):
    P = C.P
    A = Arena(P)
    sc = L.sc1 if which == 1 else L.sc2
    shb = 0 if which == 1 else 24
    xt = [A.sb("xt%d" % i, [128, D]) for i in range(2)]
    junk = A.sb("junk", [128, D])
    st = [A.sb("st%d" % i, [128, 2]) for i in range(2)]
    hf = [A.sb("hf%d" % i, [128, 8, 128]) for i in range(2)] if per_tile else None
    for t in range(NT):
        j = 1 if t < 2 else 0
        x = xt[t % 2]
        s = st[t % 2]
        dma(P, "sp" if t % 2 == 0 else "pool", x, xsrc(C, l, t))
        act(P, junk, x, AF.Square, accum_out=s[:, 0:1])
        ts(P, "dve", s[:, 1:2], s[:, 0:1], 1.0 / D, ALU.mult, EPS, ALU.add)
        act(P, s[:, 1:2], s[:, 1:2], AF.Sqrt)
        P.op("dve", "reciprocal", out=s[:, 1:2], in_=s[:, 1:2])
        ts(P, "dve", x, x, s[:, 1:2], ALU.mult)
        pa, pb = C.ps[2 + 2 * (t % 2)], C.ps[3 + 2 * (t % 2)]
        if C.dbg.get("dump_norm") and t == 2 and which == 1:
            dma(P, "sp", C.dout("d_xn", [128, D]), x)
            dma(P, "sp", C.dout("d_st", [128, 2]), s)
        for k in range(8):
            pp = pa if k < 4 else pb
            tr(P, pp[:, (k % 4) * 128:(k % 4 + 1) * 128], x[:, k * 128:(k + 1) * 128], C.ident)
        if C.dbg.get("dump_norm") and t == 2 and which == 1:
            cp(P, "dve", junk[:, 0:512], pa)
            dma(P, "sp", C.dout("d_pa", [128, 512]), junk[:, 0:512])
        for k in range(8):
            pp = pa if k < 4 else pb
            src = pp[:, (k % 4) * 128:(k % 4 + 1) * 128]
            if per_tile:
                dst = hf[t % 2][:, k, :]
            else:
                dst = hfm[:, k, t * 128:(t + 1) * 128]
            if k % 2 == 0:
                act(P, dst, src, AF.Identity, scale=sc[:, k, j:j + 1], bias=L.mod[:, shb + k, j:j + 1])
            else:
                ts(P, "dve", dst, src, sc[:, k, j:j + 1], ALU.mult, L.mod[:, shb + k, j:j + 1], ALU.add)
        if per_tile:
            for k in range(8):
                cp(P, "act" if k % 2 == 0 else "dve", hfm[:, k, t * 128:(t + 1) * 128], hf[t % 2][:, k, :])
            per_tile(t, hf[t % 2])
    A.close()


def phase_win_tm(C, l, L, hfm):
    P, I = C.P, C.I
    A = Arena(P)
    wA = A.sb("wA", [128, 8, O4], BF16)
    wst = [A.sb("wst%d" % i, [128, 8, 512]) for i in range(2)]
    ust = [A.sb("ust%d" % i, [128, O4]) for i in range(2)]
    zer = A.sb("zer", [1, O4])
    P.op("dve", "memset", ap=zer, constant=0.0)
    for r in (0, 257, NTOK + 2):
        dma(P, "sp", V(C.U_ap[r:r + 1, :], C.U_pad), zer)
    wv = I["w_in"][l].rr("(k p) n -> p k n", p=128)
    blocks = [(0, 512), (512, 1024), (1024, 1536), (1536, 1824), (1824, 2336), (2336, 2592)]
    for bi, (c0, c1) in enumerate(blocks):
        w = wst[bi % 2]
        dma(P, "sp" if bi % 2 == 0 else "pool", w[:, :, 0:c1 - c0], wv[:, :, c0:c1])
        cp(P, "act", wA[:, :, c0:c1], w[:, :, 0:c1 - c0])
    n = 0
    for t in range(NT):
        u = ust[t % 2]
        for bi, (c0, c1) in enumerate(blocks):
            ps = C.ps[n % 4]
            n += 1
            for k in range(8):
                mm(P, ps[:, 0:c1 - c0], hfm[:, k, t * 128:(t + 1) * 128], wA[:, k, c0:c1], k == 0, k == 7)
            cp(P, "act" if bi % 2 == 0 else "dve", u[:, c0:c1], ps[:, 0:c1 - c0])
        r0 = urow(t)
        dma(P, "sp" if t % 2 == 0 else "pool", V(C.U_ap[r0:r0 + 128, :], C.U_bufs[t]), u)
    A.close()


def stt(P, e, out, in0, scalar, in1, op0, op1, **kw):
    return P.op(e, "scalar_tensor_tensor", out=out, in0=in0, scalar=scalar, in1=in1, op0=op0, op1=op1, **kw)


def red(P, e, out, in_, **kw):
    return P.op(e, "tensor_reduce", out=out, in_=in_, axis=AX.X, op=ALU.add, **kw)


def load_bf16(P, A, name, shape, src, q="sp", ce="act"):
    st = A.sb(name + "_f", shape)
    wb = A.sb(name, shape, BF16)
    dma(P, q, st, src)
    cp(P, ce, wb, st)
    return wb


def cust(v, offset_elems, dims):
    ap = v.ap
    base = ap.ap[0]
    new = type(ap)(ap.tensor, ap.offset + offset_elems, [tuple(base)] + [tuple(d) for d in dims])
    return V(new, v.bufs)


def phase_prep(C, l, L):
    P, I = C.P, C.I
    A = Arena(P)
    pv = A.sb("pv", [128, NPV])
    dma(P, "sp", pv, I["pv"][l:l + 1, :].bc([128, NPV]))
    w1cat = load_bf16(P, A, "w1cat", [128, 2, 128], I["w1cat"][l].rr("(k p) n -> p k n", p=128))
    w2blk = load_bf16(P, A, "w2blk", [128, 1024], I["w2blk"][l])
    g1 = load_bf16(P, A, "g1w", [128, 2, 64], I["g1"][l].rr("(k p) n -> p k n", p=128))
    g2 = load_bf16(P, A, "g2w", [64, 256], I["g2"][l])
    a2blk = load_bf16(P, A, "a2blk", [32, 256], I["a2blk"][l])
    mu = pv[:, PV_MU:PV_MU + 1024]
    kkp = pv[:, PV_KK:PV_KK + 256]
    ka = pv[:, PV_KA:PV_KA + 256]
    rkp = pv[:, PV_RK:PV_RK + 256]
    w0 = pv[:, PV_W0:PV_W0 + 512]
    a0 = pv[:, PV_A0:PV_A0 + 512]
    gab = pv[:, PV_GAB:PV_GAB + 256]
    glng = pv[:, PV_GLNG:PV_GLNG + 256]

    uc = [A.sb("uc%d" % i, [128, O2]) for i in range(2)]
    up = [A.sb("up%d" % i, [128, 1024]) for i in range(2)]
    un = [A.sb("un%d" % i, [128, 1024]) for i in range(2)]
    rows = [[A.sb("rows%d%d" % (d, i), [128, 2, 1024], BF16) for i in range(2)] for d in range(2)]
    vt = [A.sb("vt%d" % i, [128, 128, 6]) for i in range(2)]
    fin = [A.sb("fin%d" % i, [128, 768]) for i in range(2)]
    cht = [A.sb("cht%d" % i, [128, 3072]) for i in range(2)]
    t0 = A.sb("t0", [128, 1024])
    mx = A.sb("mx", [128, 1024])
    xaT = A.sb("xaT", [128, 2, 128], BF16)
    z = A.sb("z", [128, 128], BF16)
    sg = A.sb("sg", [64, 128], BF16)
    wl = A.sb("wl", [128, 512])
    wdec = A.sb("wdec", [128, 512])
    il = A.sb("il", [128, 512])
    iclr = A.sb("iclr", [128, 512])
    kk0 = A.sb("kk0", [128, 256])
    sq = A.sb("sq", [128, 256])
    ss = A.sb("ss", [128, 8])
    kk = A.sb("kk", [128, 256])
    t1 = A.sb("t1", [128, 512])
    keff = A.sb("keff", [128, 512])
    bb = A.sb("bb", [128, 512])
    rkt = A.sb("rkt", [128, 256])
    alT = A.sb("alT", [32, 128], BF16)
    gl = A.sb("gl", [128, 256])
    gdec = A.sb("gdec", [128, 256])
    sr = A.sb("sr", [128, 256])

    def rv(R, c0, n):
        return R[:, :, c0:c0 + n]

    for t in range(NT):
        i = t % 2
        r0 = urow(t)
        nb = [C.U_bufs[t]]
        if t > 0:
            nb.append(C.U_bufs[t - 1])
        if t < NT - 1:
            nb.append(C.U_bufs[t + 1])
        nb.append(C.U_pad)
        dma(P, "sp", uc[i], V(C.U_ap[r0:r0 + 128, 0:O2], C.U_bufs[t]))
        dma(P, "pool", up[i], V(C.U_ap[r0 - 1:r0 + 127, 0:1024], tuple(nb)))
        dma(P, "sp", un[i], V(C.U_ap[r0 + 1:r0 + 129, 0:1024], tuple(nb)))
        u = uc[i]
        R0, R1 = rows[0][i], rows[1][i]
        F = fin[i]
        tt(P, "pool", t0, up[i], un[i], ALU.add)
        stt(P, "dve", t0, t0, 0.5, u[:, 0:1024], ALU.mult, ALU.subtract)
        tt(P, "pool", t0, t0, mu, ALU.mult)
        tt(P, "dve", mx, t0, u[:, 0:1024], ALU.add)
        r_, k_, v_, xa_ = mx[:, 0:256], mx[:, 256:512], mx[:, 512:768], mx[:, 768:1024]
        pT = C.ps[0]
        for kt in range(2):
            tr(P, pT[:, kt * 128:(kt + 1) * 128], xa_[:, kt * 128:(kt + 1) * 128], C.ident)
        cp(P, "act", xaT, pT[:, 0:256].rr("p (k n) -> p k n", k=2))
        pz = C.ps[1]
        for kt in range(2):
            mm(P, pz[:, 0:128], w1cat[:, kt, :], xaT[:, kt, :], kt == 0, kt == 1)
        for kt in range(2):
            mm(P, pz[0:64, 128:256], g1[:, kt, :], xaT[:, kt, :], kt == 0, kt == 1)
        act(P, z[0:64, :], pz[0:64, 0:128], AF.Tanh)
        cp(P, "dve", z[64:128, :], pz[64:128, 0:128])
        act(P, sg, pz[0:64, 128:256], AF.Sigmoid)
        pw, pa_, pg = C.ps[2], C.ps[3], C.ps[4]
        mm(P, pw, z, w2blk[:, 0:512], True, True)
        mm(P, pa_, z, w2blk[:, 512:1024], True, True)
        mm(P, pg[:, 0:256], sg, g2, True, True)
        tt(P, "dve", wl, pw, w0, ALU.add)
        act(P, wl, wl, AF.Sigmoid)
        act(P, wdec, wl, AF.Exp, scale=-0.6065306597126334)
        tt(P, "dve", il, pa_, a0, ALU.add)
        act(P, iclr, il, AF.Sigmoid)
        cp(P, "act", F[:, 0:256], pg[:, 0:256])
        tt(P, "pool", kk0, k_, kkp, ALU.mult)
        tt(P, "pool", sq, kk0, kk0, ALU.mult)
        red(P, "dve", ss[:, 0:4], sq.rr("p (h k) -> p h k", h=4))
        ts(P, "dve", ss[:, 0:4], ss[:, 0:4], EPS, ALU.add)
        act(P, ss[:, 0:4], ss[:, 0:4], AF.Sqrt)
        P.op("dve", "reciprocal", out=ss[:, 0:4], in_=ss[:, 0:4])
        tt(P, "dve", kk.rr("p (h k) -> p h k", h=4), kk0.rr("p (h k) -> p h k", h=4),
           ss[:, 0:4][:, :, None].bc([128, 4, 64]), ALU.mult)
        ic3 = iclr.rr("p (d c) -> p d c", d=2)
        stt(P, "dve", t1.rr("p (d c) -> p d c", d=2), ic3, -1.0, ka[:, None, :].bc([128, 2, 256]), ALU.add, ALU.mult)
        stt(P, "dve", keff.rr("p (d c) -> p d c", d=2), t1.rr("p (d c) -> p d c", d=2), 1.0,
            k_[:, None, :].bc([128, 2, 256]), ALU.add, ALU.mult)
        tt(P, "pool", bb.rr("p (d c) -> p d c", d=2), ic3, kk[:, None, :].bc([128, 2, 256]), ALU.mult)
        tt(P, "pool", rkt, r_, k_, ALU.mult)
        tt(P, "pool", rkt, rkt, rkp, ALU.mult)
        red(P, "dve", ss[:, 4:8], rkt.rr("p (h k) -> p h k", h=4))
        tt(P, "dve", F[:, 256:512].rr("p (h k) -> p h k", h=4), v_.rr("p (h k) -> p h k", h=4),
           ss[:, 4:8][:, :, None].bc([128, 4, 64]), ALU.mult)
        pT2 = C.ps[5]
        tr(P, pT2[0:32, 0:128], u[:, O1 + 768:O1 + 800], C.ident)
        cp(P, "act", alT, pT2[0:32, 0:128])
        mm(P, pT2[:, 128:384], alT, a2blk, True, True)
        tt(P, "dve", gl, pT2[:, 128:384], gab, ALU.add)
        act(P, gl, gl, AF.Sigmoid)
        act(P, gl, gl, AF.Ln)
        if C.dbg.get("old_gla"):
            act(P, gdec, gl, AF.Exp, scale=1.0 / 16.0)
        act(P, sr, u[:, O1 + 512:O1 + 768], AF.Silu)
        tt(P, "pool", F[:, 512:768], sr, glng, ALU.mult)
        CHt = cht[i]
        ts(P, "pool", CHt[:, 0:512], wl, -0.6065306597126334, ALU.mult)
        cp(P, "act", CHt[:, 512:1024], keff)
        cp(P, "pool", CHt[:, 1024:1536], bb)
        ts(P, "dve", CHt[:, 1536:1792], kk, -1.0, ALU.mult)
        cp(P, "act", CHt[:, 1792:2048], r_)
        cp(P, "pool", CHt[:, 2048:2304], v_)
        ts(P, "dve", CHt[:, 2304:2560], gl, 1.0 / 16.0, ALU.mult)
        ts(P, "pool", CHt[:, 2560:2688], u[:, O1:O1 + 128], 32.0 ** -0.5, ALU.mult)
        cp(P, "act", CHt[:, 2688:2816], u[:, O1 + 128:O1 + 256])
        cp(P, "pool", CHt[:, 2816:3072], u[:, O1 + 256:O1 + 512])
        dma(P, "sp", V(C.CH_ap[t * 128:(t + 1) * 128, :], C.CH_bufs[t]), CHt)
        if not C.dbg.get("old_gla"):
            dma(P, "pool", V(C.FIN_ap[t * 128:(t + 1) * 128, :], C.FIN_bufs[t]), F)
            continue
        for d, R in ((0, R0), (1, R1)):
            e1 = "dve" if d == 0 else "pool"
            e2 = "pool" if d == 0 else "dve"
            src = wdec[:, d * 256:(d + 1) * 256].rr("p (a h k) -> p a h k", a=2, h=2)
            hi = rv(R, 0, 128).rr("p h (a k) -> p a h k", a=2)
            lo = rv(R, 192, 128).rr("p h (a k) -> p a h k", a=2)
            cp(P, e1, hi, src)
            tt(P, e1, lo, src, hi, ALU.subtract)
            gsrc = gdec[:, d * 128:(d + 1) * 128].rr("p (a h k) -> p a h k", a=2, h=2)
            ghi = rv(R, 128, 64).rr("p h (a k) -> p a h k", a=2)
            glo = rv(R, 320, 64).rr("p h (a k) -> p a h k", a=2)
            cp(P, e2, ghi, gsrc)
            tt(P, e2, glo, gsrc, ghi, ALU.subtract)
            cp(P, e1, rv(R, 384, 128).rr("p h (a k) -> p a h k", a=2),
               keff[:, d * 256:(d + 1) * 256].rr("p (a h k) -> p a h k", a=2, h=2))
            cp(P, e2, rv(R, 512, 64).rr("p h (a k) -> p a h k", a=2),
               u[:, O1 + 128:O1 + 256].rr("p (a h k) -> p a h k", a=2, h=2))
            cp(P, e1, rv(R, 576, 128).rr("p h (a k) -> p a h k", a=2), r_.rr("p (a h k) -> p a h k", a=2, h=2))
            ts(P, e2, rv(R, 704, 64).rr("p h (a k) -> p a h k", a=2),
               u[:, O1:O1 + 128].rr("p (a h k) -> p a h k", a=2, h=2), 32.0 ** -0.5, ALU.mult)
            ts(P, e1, rv(R, 768, 128).rr("p h (a k) -> p a h k", a=2), kk.rr("p (a h k) -> p a h k", a=2, h=2),
               -1.0, ALU.mult)
            cp(P, e2, rv(R, 896, 128).rr("p h (a k) -> p a h k", a=2),
               bb[:, d * 256:(d + 1) * 256].rr("p (a h k) -> p a h k", a=2, h=2))
            dma(P, "sp" if d == 0 else "pool",
                V(C.STR_ap[d][t * 128:(t + 1) * 128].rearrange("t h n -> t (h n)"), C.STR_bufs[d][t]),
                R.rr("p h n -> p (h n)"))
        pv4 = C.ps[6]
        for a in range(2):
            tr(P, pv4[:, a * 128:(a + 1) * 128], v_[:, a * 128:(a + 1) * 128], C.ident)
        for a in range(2):
            tr(P, pv4[:, (2 + a) * 128:(3 + a) * 128], u[:, O1 + 256 + a * 128:O1 + 384 + a * 128], C.ident)
        VT = vt[i]
        cp(P, "act", VT[:, :, 0], pv4[:, 0:128])
        cp(P, "dve", VT[:, :, 1], pv4[:, 0:128])
        cp(P, "act", VT[:, :, 2], pv4[:, 128:256])
        cp(P, "dve", VT[:, :, 3], pv4[:, 128:256])
        cp(P, "act", VT[:, :, 4], pv4[:, 256:384])
        cp(P, "dve", VT[:, :, 5], pv4[:, 384:512])
        dma(P, "sp", V(C.Vs_ap[:, t * 128:(t + 1) * 128, :], C.Vs_bufs[t]), VT)
        dma(P, "pool", V(C.FIN_ap[t * 128:(t + 1) * 128, :], C.FIN_bufs[t]), F)
    A.close()


def phase_scan(C, l, nchunks=None):
    P, I = C.P, C.I
    A = Arena(P)
    S = A.sb("S", [128, 2, 64])
    T3 = A.sb("T3", [128, 2, 64])
    T4 = A.sb("T4", [128, 2, 64])
    self_f = A.sb("sel_f", [128, 64, 128])
    sel = A.sb("sel", [128, 64, 128], BF16)
    dma(P, "sp", self_f, I["sel"])
    cp(P, "pool", sel, self_f)
    rows = [[A.sb("srow%d%d" % (d, i), [128, 1024], BF16) for i in range(2)] for d in range(2)]
    vb = [A.sb("vb%d" % i, [128, 2, 64, 6]) for i in range(2)]
    yb = [A.sb("yb%d" % i, [128, 2, 64, 6]) for i in range(2)]
    P.op("dve", "memset", ap=S, constant=0.0)
    for i in range(2):
        P.op("pool", "memset", ap=yb[i], constant=0.0)
    NCH = NTOK // 64
    for c in range(NCH if nchunks is None else nchunks):
        zf = c * 64
        zb = (192 - 64 * c) if c < 4 else (4544 - 64 * c)
        i = c % 2
        dma(P, "sp", rows[0][i], V(C.STR_ap[0][zf:zf + 64].rearrange("t h n -> (t h) n"), C.STR_bufs[0][zf // 128]))
        dma(P, "pool", rows[1][i], V(C.STR_ap[1][zb:zb + 64].rearrange("t h n -> (t h) n"), C.STR_bufs[1][zb // 128]))
        dma(P, "sp", vb[i][:, 0], V(C.Vs_ap[:, zf:zf + 64, :], C.Vs_bufs[zf // 128]))
        dma(P, "pool", vb[i][:, 1], V(C.Vs_ap[:, zb:zb + 64, :], C.Vs_bufs[zb // 128]))
        YB = yb[i]
        for j in range(64):
            s = c * 64 + j
            pb = (s % 2) * 2
            for d in range(2):
                jj = j if d == 0 else 63 - j
                lt = sel[:, jj, :]
                R = rows[d][i]
                bx = C.ps[pb + d]
                mm(P, bx[:, 0:64], lt, R[:, 128:192], True, False, mark=False)
                mm(P, bx[:, 0:64], lt, R[:, 320:384], False, True, mark=False)
                mm(P, bx[:, 64:128], lt, R[:, 512:576], True, True, mark=False)
                mm(P, bx[:, 128:192], lt, R[:, 704:768], True, True, mark=(d == 1))
            R4 = V(C.psall[:, pb * 512:pb * 512 + 1024].rearrange("p (d x) -> p d x", d=2), tuple(C.psb[pb:pb + 2]))
            Dv, KKv, RQv = R4[:, :, 0:64], R4[:, :, 64:128], R4[:, :, 128:192]
            P.op("dve", "tensor_tensor", lax=True, out=S, in0=S, in1=Dv, op=ALU.mult)
            vv = cust(vb[i], j * 6 + 4, [((127 - 2 * j) * 6, 2), (1, 2), (0, 32)])
            P.op("dve", "tensor_tensor", lax=True, out=T3.rr("p d (g k) -> p d g k", g=2),
                 in0=KKv.rr("p d (g k) -> p d g k", g=2), in1=vv, op=ALU.mult)
            P.op("dve", "tensor_tensor", lax=True, out=S, in0=S, in1=T3, op=ALU.add)
            P.op("dve", "tensor_tensor", lax=True, out=T4, in0=S, in1=RQv, op=ALU.mult)
            yv = cust(YB, j * 6 + 4, [((127 - 2 * j) * 6, 2), (1, 2)])
            P.op("dve", "tensor_reduce", lax=True, out=yv, in_=T4.rr("p d (g k) -> p d g k", g=2), axis=AX.X, op=ALU.add)
        dma(P, "sp", V(C.Y_ap[0][:, zf:zf + 64, :], C.Y_bufs[0][zf // 64]), YB[:, 0])
        dma(P, "pool", V(C.Y_ap[1][:, zb:zb + 64, :], C.Y_bufs[1][zb // 64]), YB[:, 1])
    A.close()


def phase_fin_ab(C, l, L):
    P, I = C.P, C.I
    A = Arena(P)
    pv = A.sb("pv", [128, NPV])
    dma(P, "sp", pv, I["pv"][l:l + 1, :].bc([128, NPV]))
    lng = pv[:, PV_LNG:PV_LNG + 256]
    yt = [A.sb("yt%d" % i, [128, 2, 128, 6]) for i in range(2)]
    fin = [A.sb("finf%d" % i, [128, 768]) for i in range(2)]
    ya = A.sb("ya", [128, 256])
    ytm = [A.sb("ytm%d" % i, [128, 2, 256]) for i in range(2)]
    ytg = [A.sb("ytg%d" % i, [128, 2, 256]) for i in range(2)]
    yg = A.sb("yg", [128, 256])
    sq = A.sb("sqf", [128, 256])
    st = A.sb("stf", [128, 16])
    ob = [A.sb("ob%d" % i, [128, 4, 128], BF16) for i in range(2)]
    for t in range(NT):
        i = t % 2
        Y = yt[i]
        F = fin[i]
        if C.dbg.get("old_gla"):
            for d in range(2):
                dma(P, "sp" if d == 0 else "pool", Y[:, d],
                    V(C.Y_ap[d][:, t * 128:(t + 1) * 128, :], (C.Y_bufs[d][2 * t], C.Y_bufs[d][2 * t + 1])))
        dma(P, "sp", F, V(C.FIN_ap[t * 128:(t + 1) * 128, :], C.FIN_bufs[t]))
        pr, pg = C.ps[0], C.ps[1]
        if C.dbg.get("old_rwkv"):
            for a in range(2):
                n = 0
                for d in range(2):
                    for g in (2 * a, 2 * a + 1):
                        mm(P, pr[:, a * 128:(a + 1) * 128], Y[:, d, :, g], C.ident, n == 0, n == 3)
                        n += 1
        if C.dbg.get("old_gla"):
            for a in range(2):
                for d in range(2):
                    mm(P, pg[:, a * 128:(a + 1) * 128], Y[:, d, :, 4 + a], C.ident, d == 0, d == 1)
        if C.dbg.get("old_rwkv"):
            cp(P, "act", ya, pr[:, 0:256])
        else:
            for d in range(2):
                dma(P, "sp" if d == 0 else "pool", ytm[i][:, d, :], V(C.YT_ap[d][t * 128:(t + 1) * 128, :], C.YT_bufs[d][t]))
            tt(P, "pool", ya, ytm[i][:, 0, :], ytm[i][:, 1, :], ALU.add)
        ya4 = ya.rr("p (h k) -> p h k", h=4)
        red(P, "dve", st[:, 0:4], ya4)
        ts(P, "dve", st[:, 0:4], st[:, 0:4], 1.0 / 64.0, ALU.mult)
        tt(P, "dve", ya4, ya4, st[:, 0:4][:, :, None].bc([128, 4, 64]), ALU.subtract)
        tt(P, "pool", sq, ya, ya, ALU.mult)
        red(P, "dve", st[:, 4:8], sq.rr("p (h k) -> p h k", h=4))
        ts(P, "dve", st[:, 4:8], st[:, 4:8], 1.0 / 64.0, ALU.mult, GN_EPS, ALU.add)
        act(P, st[:, 4:8], st[:, 4:8], AF.Sqrt)
        P.op("dve", "reciprocal", out=st[:, 4:8], in_=st[:, 4:8])
        tt(P, "dve", ya4, ya4, st[:, 4:8][:, :, None].bc([128, 4, 64]), ALU.mult)
        tt(P, "pool", ya, ya, lng, ALU.mult)
        tt(P, "pool", ya, ya, F[:, 256:512], ALU.add)
        tt(P, "pool", ya, ya, F[:, 0:256], ALU.mult)
        if C.dbg.get("old_gla"):
            cp(P, "act", yg, pg[:, 0:256])
        else:
            for d in range(2):
                dma(P, "sp" if d == 0 else "pool", ytg[i][:, d, :], V(C.YTG_ap[d][t * 128:(t + 1) * 128, :], C.YTG_bufs[d][t]))
            tt(P, "pool", yg, ytg[i][:, 0, :], ytg[i][:, 1, :], ALU.add)
        yg4 = yg.rr("p (h k) -> p h k", h=4)
        tt(P, "pool", sq, yg, yg, ALU.mult)
        red(P, "dve", st[:, 8:12], sq.rr("p (h k) -> p h k", h=4))
        ts(P, "dve", st[:, 8:12], st[:, 8:12], 1.0 / 64.0, ALU.mult, EPS, ALU.add)
        act(P, st[:, 8:12], st[:, 8:12], AF.Sqrt)
        P.op("dve", "reciprocal", out=st[:, 8:12], in_=st[:, 8:12])
        tt(P, "dve", yg4, yg4, st[:, 8:12][:, :, None].bc([128, 4, 64]), ALU.mult)
        tt(P, "pool", yg, yg, F[:, 512:768], ALU.mult)
        po = C.ps[2]
        for a in range(2):
            tr(P, po[:, a * 128:(a + 1) * 128], ya[:, a * 128:(a + 1) * 128], C.ident)
            tr(P, po[:, (2 + a) * 128:(3 + a) * 128], yg[:, a * 128:(a + 1) * 128], C.ident)
        OB = ob[i]
        cp(P, "act", OB, po.rr("p (a n) -> p a n", a=4))
        for br in range(2):
            dma(P, "sp" if br == 0 else "pool",
                V(C.YB_ap[br][:, t * 128:(t + 1) * 128].rearrange("(a p) n -> p a n", p=128), C.YB_bufs[br][t]),
                OB[:, 2 * br:2 * br + 2, :])
    A.close()


def phase_attn(C, l, L):
    P, I = C.P, C.I
    A = Arena(P)
    qT = A.sb("qT", [128, 2, NTOK], BF16)
    kT = A.sb("kT", [128, 2, NTOK], BF16)
    vtm = A.sb("vtm", [128, NT, 128], BF16)
    maskw = A.sb("maskw", [128, 384])
    sink = A.sb("sink", [128, 16])
    identb = A.sb("identb", [128, 128], BF16)
    dma(P, "sp", maskw, I["maskw"])
    dma(P, "sp", sink, I["attn_sink"][l:l + 1, :].bc([128, 16]))
    cp(P, "pool", identb, C.ident)
    ua = [A.sb("ua%d" % i, [128, 512]) for i in range(2)]
    rc = [A.sb("rc%d" % i, [128, 32]) for i in range(2)]
    rs = [A.sb("rs%d" % i, [128, 32]) for i in range(2)]
    qk = A.sb("qk", [128, 6, 64])
    tmp = A.sb("tmpr", [128, 6, 32])
    kd = A.sb("kd", [128, 2, 2, 64])
    cut = C.dbg.get("attn_cut", 9)
    for t in range(NT if cut > 1 else 0):
        i = t % 2
        r0 = urow(t)
        u = ua[i]
        dma(P, "sp", u, V(C.U_ap[r0:r0 + 128, O2:O3], C.U_bufs[t]))
        u6 = u[:, 0:384].rr("p (h d) -> p h d", h=6)
        if t >= 2 and not C.dbg.get("norope"):
            dma(P, "pool", rc[i], I["ropec"][(t - 2) * 128:(t - 1) * 128, :])
            dma(P, "pool", rs[i], I["ropes"][(t - 2) * 128:(t - 1) * 128, :])
            cb = rc[i][:, None, :].bc([128, 6, 32])
            sb_ = rs[i][:, None, :].bc([128, 6, 32])
            z1, z2 = u6[:, :, 0:32], u6[:, :, 32:64]
            tt(P, "dve", qk[:, :, 0:32], z1, cb, ALU.mult)
            tt(P, "pool", tmp, z2, sb_, ALU.mult)
            tt(P, "dve", qk[:, :, 0:32], qk[:, :, 0:32], tmp, ALU.subtract)
            tt(P, "dve", qk[:, :, 32:64], z1, sb_, ALU.mult)
            tt(P, "pool", tmp, z2, cb, ALU.mult)
            tt(P, "dve", qk[:, :, 32:64], qk[:, :, 32:64], tmp, ALU.add)
        else:
            cp(P, "dve", qk, u6)
        if cut < 3:
            continue
        cp(P, "pool", kd[:, :, 0, :], qk[:, 4:6, :])
        cp(P, "pool", kd[:, :, 1, :], qk[:, 4:6, :])
        cp(P, "pool", vtm[:, t, :], u[:, 384:512])
        if cut < 4:
            continue
        pq = C.ps[t % 2]
        qf = qk.rr("p h d -> p (h d)")
        kf = kd.rr("p k r d -> p (k r d)")
        for a in range(2):
            tr(P, pq[:, a * 128:(a + 1) * 128], qf[:, a * 128:(a + 1) * 128], C.ident)
            tr(P, pq[:, (2 + a) * 128:(3 + a) * 128], kf[:, a * 128:(a + 1) * 128], C.ident)
        ts(P, "dve", qT[:, :, t * 128:(t + 1) * 128], pq[:, 0:256].rr("p (a n) -> p a n", a=2), 0.125, ALU.mult)
        cp(P, "dve", kT[:, :, t * 128:(t + 1) * 128], pq[:, 256:512].rr("p (a n) -> p a n", a=2))
    if C.dbg.get("attn_p1"):
        A.close()
        return
    sc = [A.sb("sc%d" % i, [128, 640]) for i in range(2)]
    pb = [A.sb("pb%d" % i, [128, 640], BF16) for i in range(2)]
    pTs = [A.sb("pTs%d" % i, [128, 5, 128], BF16) for i in range(2)]
    st = [A.sb("sta%d" % i, [128, 8]) for i in range(2)]
    yo = [A.sb("yo%d" % i, [128, 256]) for i in range(2)]
    oc = [A.sb("oc%d" % i, [128, 2, 128], BF16) for i in range(2)]
    psT = [V(C.psall[:, b * 512:(b + 1) * 512].bitcast(BF16), C.psb[b]) for b in (4, 5)]
    n = 0
    for t in range(NT):
        YO = yo[t % 2]
        if t >= 2:
            lo, hi = max(t - 1, 2), min(t + 1, NT - 1)
            nw = hi - lo + 1
            m0 = (lo - (t - 1)) * 128
        else:
            nw = 0
        nk = nw * 128 + 256
        nblk = nw + 2
        kblocks = ([lo + b for b in range(nw)] if nw else []) + [0, 1]
        po = C.ps[6 + (t % 2)]
        for h in range(4):
            kv, hl = h // 2, h % 2
            i = n % 2
            n += 1
            S_, Pb, PT, ST = sc[i], pb[i], pTs[i], st[i]
            qv = qT[hl * 64:(hl + 1) * 64, kv, t * 128:(t + 1) * 128]
            pa_, pc_ = C.ps[2 * i], C.ps[2 * i + 1]
            if nw:
                mm(P, pa_[:, 0:nw * 128], qv, kT[hl * 64:(hl + 1) * 64, kv, lo * 128:(hi + 1) * 128], True, True)
            mm(P, pc_[:, 0:256], qv, kT[hl * 64:(hl + 1) * 64, kv, 0:256], True, True)
            if nw:
                tt(P, "dve", S_[:, 0:nw * 128], pa_[:, 0:nw * 128], maskw[:, m0:m0 + nw * 128], ALU.add)
            cp(P, "act", S_[:, nw * 128:nk], pc_[:, 0:256])
            P.op("dve", "tensor_reduce", out=ST[:, 0:1], in_=S_[:, 0:nk], axis=AX.X, op=ALU.max)
            tt(P, "dve", ST[:, 0:1], ST[:, 0:1], sink[:, h:h + 1], ALU.max)
            ts(P, "dve", ST[:, 1:2], ST[:, 0:1], -1.0, ALU.mult)
            act(P, Pb[:, 0:nk], S_[:, 0:nk], AF.Exp, bias=ST[:, 1:2], accum_out=ST[:, 2:3])
            act(P, ST[:, 3:4], sink[:, h:h + 1], AF.Exp, bias=ST[:, 1:2])
            tt(P, "dve", ST[:, 4:5], ST[:, 2:3], ST[:, 3:4], ALU.add)
            P.op("dve", "reciprocal", out=ST[:, 5:6], in_=ST[:, 4:5])
            pt = psT[i]
            for b in range(nblk):
                tr(P, pt[:, b * 128:(b + 1) * 128], Pb[:, b * 128:(b + 1) * 128], identb)
            cp(P, "act" if h % 2 == 0 else "dve", PT[:, 0:nblk, :], pt[:, 0:nblk * 128].rr("p (b n) -> p b n", b=nblk))
            for b in range(nblk):
                mm(P, po[:, h * 64:(h + 1) * 64], PT[:, b, :], vtm[:, kblocks[b], kv * 64:(kv + 1) * 64],
                   b == 0, b == nblk - 1)
            ts(P, "dve", YO[:, h * 64:(h + 1) * 64], po[:, h * 64:(h + 1) * 64], ST[:, 5:6], ALU.mult)
        pf = C.ps[t % 2]
        for a in range(2):
            tr(P, pf[:, a * 128:(a + 1) * 128], YO[:, a * 128:(a + 1) * 128], C.ident)
        OC = oc[t % 2]
        cp(P, "act", OC, pf[:, 0:256].rr("p (a n) -> p a n", a=2))
        dma(P, "sp", V(C.YB_ap[2][:, t * 128:(t + 1) * 128].rearrange("(a p) n -> p a n", p=128), C.YB_bufs[2][t]), OC)
    A.close()


PI = math.pi
S5_BLOCKS = [(0, 256)] + [(256 + 512 * i, 512) for i in range(8)]


I32 = mybir.dt.int32
TWO_PI_HI = 6.28125
TWO_PI_LO = 2.0 * math.pi - 6.28125


def sincos(P, A, s_out, c_out, x, shape, tag):
    qi = A.sb("qi" + tag, shape, I32)
    kf = A.sb("kf" + tag, shape)
    r = A.sb("rr" + tag, shape)
    m = A.sb("mm" + tag, shape)
    for extra, out in ((0.0, s_out), (0.5 * PI, c_out)):
        ts(P, "dve", r, x, 16.0 * PI + extra, ALU.add)
        ts(P, "dve", qi, r, 1.0 / (2.0 * PI), ALU.mult)
        cp(P, "dve", kf, qi)
        stt(P, "dve", r, kf, -TWO_PI_HI, r, ALU.mult, ALU.add)
        stt(P, "dve", r, kf, -TWO_PI_LO, r, ALU.mult, ALU.add)
        ts(P, "dve", m, r, PI, ALU.is_gt)
        stt(P, "dve", r, m, -2.0 * PI, r, ALU.mult, ALU.add)
        ts(P, "dve", m, r, -PI, ALU.is_lt)
        stt(P, "dve", r, m, 2.0 * PI, r, ALU.mult, ALU.add)
        ts(P, "dve", r, r, PI, ALU.min, -PI, ALU.max)
        act(P, out, r, AF.Sin)


def phase_s5(C, l, L):
    P, I = C.P, C.I
    A = Arena(P)
    th_s = A.sb("th_s", [128, 2, 8])
    rho_s = A.sb("rho_s", [128, 2, 8])
    Ck = A.sb("Ck", [128, 2, 8, 13])
    Sk = A.sb("Sk", [128, 2, 8, 13])
    BbT = A.sb("BbT", [128, 2, 2, 8, 128], BF16)
    CT = A.sb("CT", [128, 2, 2, 8, 128], BF16)
    A0 = A
    A = Arena(P)
    tmpA = [A.sb("s5r%d" % i, [128, 1024]) for i in range(8)]
    lre, lim, ldt, t_s, t_c, t_a, t_b, t_d = tmpA
    pw2f = A.sb("pw2", [128, 16])
    dma(P, "sp", pw2f, I["pw2"][0:1, :].bc([128, 16]))
    pw2 = pw2f[:, 0:13]
    fR = [A.sb("fR%d" % d, [128, 1024]) for d in range(2)]
    fI = [A.sb("fI%d" % d, [128, 1024]) for d in range(2)]
    sm = A.sb("sm", [128, 2, 3, 8])
    dma(P, "sp", sm, I["s5_sm"][l])
    act(P, sm[:, :, 2, :], sm[:, :, 2, :], AF.Exp)
    tt(P, "dve", th_s, sm[:, :, 1, :], sm[:, :, 2, :], ALU.mult)
    tt(P, "dve", rho_s, sm[:, :, 0, :], sm[:, :, 2, :], ALU.mult)
    act(P, rho_s, rho_s, AF.Exp)
    ang13 = A.sb("ang13", [128, 16, 13])
    tt(P, "dve", ang13, th_s.rr("p d j -> p (d j)")[:, :, None].bc([128, 16, 13]), pw2[:, None, :].bc([128, 16, 13]), ALU.mult)
    sincos(P, A, Sk.rr("p d j k -> p (d j) k"), Ck.rr("p d j k -> p (d j) k"), ang13, [128, 16, 13], "k")
    for d in range(2):
        dma(P, "sp", lre, I["s5_rows"][l, d, 0:1, :].bc([128, 1024]))
        dma(P, "pool", lim, I["s5_rows"][l, d, 1:2, :].bc([128, 1024]))
        dma(P, "sp", ldt, I["s5_rows"][l, d, 2:3, :].bc([128, 1024]))
        act(P, ldt, ldt, AF.Exp)
        tt(P, "dve", t_a, lim, ldt, ALU.mult)
        sincos(P, A, t_s, t_c, t_a, [128, 1024], "r%d" % d)
        tt(P, "dve", t_a, lre, ldt, ALU.mult)
        act(P, t_a, t_a, AF.Exp)
        tt(P, "dve", t_c, t_c, t_a, ALU.mult)
        tt(P, "dve", t_s, t_s, t_a, ALU.mult)
        ts(P, "dve", t_c, t_c, -1.0, ALU.add)
        tt(P, "dve", t_a, lre, lre, ALU.mult)
        tt(P, "pool", t_b, lim, lim, ALU.mult)
        tt(P, "dve", t_a, t_a, t_b, ALU.add)
        P.op("dve", "reciprocal", out=t_a, in_=t_a)
        tt(P, "dve", t_b, t_c, lre, ALU.mult)
        tt(P, "pool", t_d, t_s, lim, ALU.mult)
        tt(P, "dve", t_b, t_b, t_d, ALU.add)
        tt(P, "dve", fR[d], t_b, t_a, ALU.mult)
        tt(P, "dve", t_b, t_s, lre, ALU.mult)
        tt(P, "pool", t_d, t_c, lim, ALU.mult)
        tt(P, "dve", t_b, t_b, t_d, ALU.subtract)
        tt(P, "dve", fI[d], t_b, t_a, ALU.mult)
    bt_f = A.sb("bt_f", [128, 2, 8, 128])
    dma(P, "sp", bt_f[:, 0], I["s5_bt"][l, 0].rr("j c s -> c j s"))
    dma(P, "pool", bt_f[:, 1], I["s5_bt"][l, 1].rr("j c s -> c j s"))
    for d in range(2):
        fr = fR[d].rr("p (j s) -> p j s", j=8)
        fi = fI[d].rr("p (j s) -> p j s", j=8)
        ta = t_a.rr("p (j s) -> p j s", j=8)
        tb = t_b.rr("p (j s) -> p j s", j=8)
        tt(P, "dve", ta, bt_f[:, 0], fr, ALU.mult)
        tt(P, "pool", tb, bt_f[:, 1], fi, ALU.mult)
        tt(P, "dve", BbT[:, d, 0], ta, tb, ALU.subtract)
        tt(P, "dve", ta, bt_f[:, 0], fi, ALU.mult)
        tt(P, "pool", tb, bt_f[:, 1], fr, ALU.mult)
        tt(P, "dve", BbT[:, d, 1], ta, tb, ALU.add)
        for ri in range(2):
            ctf = t_c if ri == 0 else t_d
            dma(P, "sp" if ri == 0 else "pool", ctf.rr("p (j c) -> p j c", j=8), I["s5_ct"][l, d, ri].rr("j s c -> s j c"))
            if ri == 0:
                cp(P, "pool", CT[:, d, 0], ctf.rr("p (j c) -> p j c", j=8))
            else:
                ts(P, "pool", CT[:, d, 1], ctf.rr("p (j c) -> p j c", j=8), -1.0, ALU.mult)
    A.close()
    A = A0
    cut = C.dbg.get("s5_cut", 99)
    if cut <= 1:
        A.close(); return
    if not C.dbg.get("s5_small"):
        ct, sn = A.sb("ct", [128, NTOK]), A.sb("sn", [128, NTOK])
        w_re, w_im = A.sb("w_re", [128, NTOK]), A.sb("w_im", [128, NTOK])
    uB = A.sb("uB", [128, 2, NTOK], BF16)
    yacc = A.sb("yacc", [128, 2, NTOK])
    x_re, x_im = A.sb("x_re", [128, NTOK], BF16), A.sb("x_im", [128, NTOK], BF16)
    dsk = A.sb("dsk", [128, 16])
    bgl = A.sb("bgl", [128, 16])
    dma(P, "sp", dsk, I["s5_d_fm"][l])
    dma(P, "sp", bgl, I["s5_bglu_fm"][l])
    wglu = load_bf16(P, A, "wglu", [128, 2, 256], I["s5_w_glu"][l].rr("(k p) n -> p k n", p=128))
    tmA = [[A.sb("tm%d_%d" % (b, i), [128, 512]) for i in range(4)] for b in range(2)]
    ut = [tmA[1][i][:, 0:256] for i in range(2)]
    var = C.dbg.get("s5_var", 9)
    for t in range(NT if var > 0 else 0):
        i = t % 2
        r0 = urow(t)
        dma(P, "sp" if i == 0 else "pool", ut[i], V(C.U_ap[r0:r0 + 128, O3:O4], C.U_bufs[t]))
        pp = C.ps[i]
        for a in range(2):
            tr(P, pp[:, a * 128:(a + 1) * 128], ut[i][:, a * 128:(a + 1) * 128], C.ident)
        if var < 2:
            continue
        for a in range(2):
            cp(P, "dve", uB[:, a, t * 128:(t + 1) * 128], pp[:, a * 128:(a + 1) * 128])
        for a in range(2):
            ts(P, "dve", yacc[:, a, t * 128:(t + 1) * 128], pp[:, a * 128:(a + 1) * 128], dsk[:, a:a + 1], ALU.mult)
    tm = tmA[0]
    if cut <= 2:
        A.close(); return
    nb = 0
    for d in range(2):
        for j in range(8):
            jt = j // 4
            th = th_s[:, d, j:j + 1]
            P.op("dve", "memset", ap=ct[:, 0:1], constant=1.0)
            P.op("dve", "memset", ap=sn[:, 0:1], constant=0.0)
            k = 0
            n = 1
            while n < NTOK:
                m = min(n, NTOK - n)
                ck, sk = Ck[:, d, j, k:k + 1], Sk[:, d, j, k:k + 1]
                e1, e2 = ("dve", "dve")
                ts(P, e1, ct[:, n:n + m], ct[:, 0:m], ck, ALU.mult)
                ts(P, e2, sn[:, n:n + m], sn[:, 0:m], ck, ALU.mult)
                ts(P, e2, tm[0][:, 0:min(m, 512)] if m <= 512 else w_re[:, 0:m], sn[:, 0:m], sk, ALU.mult)
                ts(P, e1, tm[1][:, 0:min(m, 512)] if m <= 512 else w_im[:, 0:m], ct[:, 0:m], sk, ALU.mult)
                ta_ = tm[0][:, 0:m] if m <= 512 else w_re[:, 0:m]
                tb_ = tm[1][:, 0:m] if m <= 512 else w_im[:, 0:m]
                tt(P, e1, ct[:, n:n + m], ct[:, n:n + m], ta_, ALU.subtract)
                tt(P, e2, sn[:, n:n + m], sn[:, n:n + m], tb_, ALU.add)
                n += m
                k += 1
            if cut <= 3:
                A.close(); return
            for bi, (t0, n) in enumerate(S5_BLOCKS):
                if d == 0:
                    rhs = uB[:, jt, t0:t0 + n]
                else:
                    last = (255 - t0) if t0 < 256 else (4607 - t0)
                    rhs = cust(uB, jt * NTOK + last, [(-1, n)])
                pr, pi_ = C.ps[(nb % 2) * 2], C.ps[(nb % 2) * 2 + 1]
                nb += 1
                mm(P, pr[:, 0:n], BbT[:, d, 0, j, :], rhs, True, True)
                mm(P, pi_[:, 0:n], BbT[:, d, 1, j, :], rhs, True, True)
                c_, s_ = ct[:, t0:t0 + n], sn[:, t0:t0 + n]
                tm = tmA[bi % 2]
                tt(P, "dve", tm[0][:, 0:n], pr[:, 0:n], c_, ALU.mult)
                tt(P, "dve", tm[1][:, 0:n], pi_[:, 0:n], s_, ALU.mult)
                tt(P, "dve", w_re[:, t0:t0 + n], tm[0][:, 0:n], tm[1][:, 0:n], ALU.add)
                tt(P, "dve", tm[2][:, 0:n], pi_[:, 0:n], c_, ALU.mult)
                tt(P, "dve", tm[3][:, 0:n], pr[:, 0:n], s_, ALU.mult)
                tt(P, "dve", w_im[:, t0:t0 + n], tm[2][:, 0:n], tm[3][:, 0:n], ALU.subtract)
            if cut <= 4:
                A.close(); return
            rb = rho_s[:, d, j:j + 1].bc([128, NTOK])
            P.op("dve", "tensor_tensor_scan", out=w_re, data0=rb, data1=w_re, initial=0.0, op0=ALU.mult, op1=ALU.add)
            P.op("dve", "tensor_tensor_scan", out=w_im, data0=rb, data1=w_im, initial=0.0, op0=ALU.mult, op1=ALU.add)
            if cut <= 5:
                A.close(); return
            for bi, (t0, n) in enumerate(S5_BLOCKS):
                c_, s_ = ct[:, t0:t0 + n], sn[:, t0:t0 + n]
                tm = tmA[bi % 2]
                tt(P, "dve", tm[0][:, 0:n], w_re[:, t0:t0 + n], c_, ALU.mult)
                tt(P, "dve", tm[1][:, 0:n], w_im[:, t0:t0 + n], s_, ALU.mult)
                tt(P, "dve", x_re[:, t0:t0 + n], tm[0][:, 0:n], tm[1][:, 0:n], ALU.subtract)
                tt(P, "dve", tm[2][:, 0:n], w_re[:, t0:t0 + n], s_, ALU.mult)
                tt(P, "dve", tm[3][:, 0:n], w_im[:, t0:t0 + n], c_, ALU.mult)
                tt(P, "dve", x_im[:, t0:t0 + n], tm[2][:, 0:n], tm[3][:, 0:n], ALU.add)
                py = C.ps[4 + (bi % 2)]
                if d == 0:
                    xr, xi = x_re[:, t0:t0 + n], x_im[:, t0:t0 + n]
                    k0 = t0
                else:
                    k0 = (256 - t0 - n) if t0 < 256 else (4608 - t0 - n)
                    s_last = t0 + n - 1
                    xr, xi = cust(x_re, s_last, [(-1, n)]), cust(x_im, s_last, [(-1, n)])
                mm(P, py[:, 0:n], CT[:, d, 0, j, :], xr, True, False)
                mm(P, py[:, 0:n], CT[:, d, 1, j, :], xi, False, True)
                tt(P, "dve", yacc[:, jt, k0:k0 + n], yacc[:, jt, k0:k0 + n], py[:, 0:n], ALU.add)
    if cut <= 7:
        A.close(); return
    glb = uB
    tm = tmA[0]
    ob = [x_re[:, 0:1024].rr("p (a n) -> p a n", a=2), x_im[:, 0:1024].rr("p (a n) -> p a n", a=2)]
    for bi, (t0, n) in enumerate(S5_BLOCKS):
        for a in range(2):
            y = yacc[:, a, t0:t0 + n]
            tt(P, "pool", tm[0][:, 0:n], y, y, ALU.mult)
            ts(P, "dve", tm[0][:, 0:n], tm[0][:, 0:n], 0.044715, ALU.mult, 1.0, ALU.add)
            tt(P, "pool", tm[0][:, 0:n], tm[0][:, 0:n], y, ALU.mult)
            act(P, tm[0][:, 0:n], tm[0][:, 0:n], AF.Sigmoid, scale=1.5957691216057308)
            tt(P, "dve", y, y, tm[0][:, 0:n], ALU.mult)
            cp(P, "pool", glb[:, a, t0:t0 + n], y)
        OB = ob[bi % 2]
        for a in range(2):
            pz = C.ps[6 + a]
            for kt in range(2):
                mm(P, pz[:, 0:n], wglu[:, kt, a * 128:(a + 1) * 128], glb[:, kt, t0:t0 + n], kt == 0, kt == 1)
            act(P, tm[1 + a][:, 0:n], pz[:, 0:n], AF.Sigmoid, bias=bgl[:, a:a + 1])
            tt(P, "dve", OB[:, a, 0:n], yacc[:, a, t0:t0 + n], tm[1 + a][:, 0:n], ALU.mult)
        tl = [t for t in range(NT) if t * 128 >= t0 and t * 128 < t0 + n]
        dma(P, "sp", V(C.YB_ap[3][:, t0:t0 + n].rearrange("(a p) n -> p a n", p=128), tuple(C.YB_bufs[3][t] for t in tl)),
            OB[:, :, 0:n])
    A.close()


TOKBLK = [(0, 256)] + [(256 + 512 * i, 512) for i in range(8)]


def phase_win_gates(C, l, L, hfm):
    P, I = C.P, C.I
    A = Arena(P)
    wst = [A.sb("wgs%d" % i, [128, 8, 512]) for i in range(2)]
    wb = [A.sb("wgb%d" % i, [128, 8, 512], BF16) for i in range(2)]
    gst = [A.sb("gst%d" % i, [128, 512], BF16) for i in range(4)]
    wv = I["w_in"][l].rr("(k p) n -> p k n", p=128)
    n = 0
    for cb in range(8):
        c0 = O4 + cb * 512
        dma(P, "sp" if cb % 2 == 0 else "pool", wst[cb % 2], wv[:, :, c0:c0 + 512])
        cp(P, "act" if cb % 2 == 0 else "dve", wb[cb % 2], wst[cb % 2])
        w = wb[cb % 2]
        for mi in range(4):
            row0 = cb * 512 + mi * 128
            for bi, (t0, nn) in enumerate(TOKBLK):
                ps = C.ps[n % 4]
                g = gst[n % 4]
                for k in range(8):
                    mm(P, ps[:, 0:nn], w[:, k, mi * 128:(mi + 1) * 128], hfm[:, k, t0:t0 + nn], k == 0, k == 7)
                cp(P, "act" if n % 2 == 0 else "dve", g[:, 0:nn], ps[:, 0:nn])
                dma(P, "sp" if n % 2 == 0 else "pool", V(C.Gt_ap[row0:row0 + 128, t0:t0 + nn], C.Gt_bufs[bi]), g[:, 0:nn])
                n += 1
    A.close()


def phase_merge(C, l, L):
    P, I = C.P, C.I
    A = Arena(P)
    wbr = A.sb("wbr", [128, 4, 2, 1024], BF16)
    wout = A.sb("wout", [128, 8, 1024], BF16)
    wst = A.sb("wmst", [128, 8, 1024])
    for i in range(4):
        dma(P, "sp", wst[:, 0:2, :], I["w_branch"][l, i].rr("(k p) n -> p k n", p=128))
        cp(P, "act", wbr[:, i], wst[:, 0:2, :])
    dma(P, "sp", wst, I["w_out"][l].rr("(k p) n -> p k n", p=128))
    cp(P, "act", wout, wst)
    yb = [A.sb("myb%d" % i, [128, 4, 2, 512], BF16) for i in range(2)]
    gt4 = [A.sb("mgt%d" % i, [128, 4, 512], BF16) for i in range(2)]
    sg4 = [A.sb("msg%d" % i, [128, 4, 512]) for i in range(2)]
    tmp = A.sb("mtmp", [128, 512])
    acc = A.sb("macc", [128, 512])
    mg = [A.sb("mmg%d" % i, [128, 8, 512], BF16) for i in range(2)]
    xt = [A.sb("mxt%d" % i, [128, 1024]) for i in range(2)]
    tm2 = A.sb("mtm2", [128, 1024])
    n = 0
    nx = 0
    for bi, (t0, nn) in enumerate(TOKBLK):
        YB = yb[bi % 2]
        tl = [t for t in range(NT) if t0 <= t * 128 < t0 + nn]
        for i in range(4):
            dma(P, "sp" if i % 2 == 0 else "pool", YB[:, i, :, 0:nn],
                V(C.YB_ap[i][:, t0:t0 + nn].rearrange("(a p) n -> p a n", p=128), tuple(C.YB_bufs[i][t] for t in tl)))
        MG = mg[bi % 2]
        for m in range(8):
            g4 = gt4[m % 2]
            s4 = sg4[m % 2]
            dma(P, "sp" if m % 2 == 0 else "pool", g4[:, :, 0:nn],
                V(C.Gt_ap.rearrange("(i r) n -> r i n", i=4)[m * 128:(m + 1) * 128, :, t0:t0 + nn], C.Gt_bufs[bi]))
            act(P, s4[:, :, 0:nn], g4[:, :, 0:nn], AF.Sigmoid)
            for i in range(4):
                s_ = s4[:, i, :]
                ps = C.ps[n % 4]
                n += 1
                for kt in range(2):
                    mm(P, ps[:, 0:nn], wbr[:, i, kt, m * 128:(m + 1) * 128], YB[:, i, kt, 0:nn], kt == 0, kt == 1)
                if i == 0:
                    tt(P, "dve", acc[:, 0:nn], ps[:, 0:nn], s_[:, 0:nn], ALU.mult)
                elif i < 3:
                    tt(P, "dve", tmp[:, 0:nn], ps[:, 0:nn], s_[:, 0:nn], ALU.mult)
                    tt(P, "pool", acc[:, 0:nn], acc[:, 0:nn], tmp[:, 0:nn], ALU.add)
                else:
                    tt(P, "dve", tmp[:, 0:nn], ps[:, 0:nn], s_[:, 0:nn], ALU.mult)
                    tt(P, "dve", MG[:, m, 0:nn], acc[:, 0:nn], tmp[:, 0:nn], ALU.add)
        for ti, t in enumerate(tl):
            j = 1 if t < 2 else 0
            x = xt[nx % 2]
            nx += 1
            dma(P, "sp", x, xsrc(C, l, t))
            for half in range(2):
                po = C.ps[4 + half + 2 * (nx % 2)]
                for k in range(8):
                    mm(P, po, MG[:, k, ti * 128:(ti + 1) * 128], wout[:, k, half * 512:(half + 1) * 512], k == 0, k == 7)
                tt(P, "dve", tm2[:, half * 512:(half + 1) * 512], po, L.grow[0][j][:, half * 512:(half + 1) * 512], ALU.mult)
            tt(P, "pool", x, x, tm2, ALU.add)
            dma(P, "pool", C.xres[t], x)
    A.close()
    C.x_in_scratch = True


def make_router(C, l, L, RA):
    P, I = C.P, C.I
    wr = RA.sb("wr", [128, 8, 36])
    brow = RA.sb("brow", [128, 36])
    dma(P, "sp", wr, I["w_router"][l].rr("(k p) n -> p k n", p=128))
    dma(P, "sp", brow, I["b_router"][l:l + 1, :].bc([128, 36]))
    lg = RA.sb("lg", [128, 36])
    st = RA.sb("rst", [128, 16])
    oh = RA.sb("roh", [128, 4])
    em = RA.sb("rem", [128, 32])
    em2 = RA.sb("rem2", [128, 32])
    oh1 = RA.sb("roh1", [128, 32])
    oh2 = RA.sb("roh2", [128, 32])
    wg = RA.sb("rwg", [128, 32])
    junk = RA.sb("rjunk", [128, 4])

    def per_tile(t, hf):
        pl = C.ps[6]
        for k in range(8):
            mm(P, pl[:, 0:36], hf[:, k, :], wr[:, k, :], k == 0, k == 7)
        tt(P, "dve", lg, pl[:, 0:36], brow, ALU.add)
        g, e = lg[:, 0:4], lg[:, 4:36]
        P.op("dve", "tensor_reduce", out=st[:, 0:1], in_=g, axis=AX.X, op=ALU.max)
        ts(P, "dve", oh, g, st[:, 0:1], ALU.is_equal)
        ts(P, "dve", st[:, 1:2], st[:, 0:1], -1.0, ALU.mult)
        act(P, junk, g, AF.Exp, bias=st[:, 1:2], accum_out=st[:, 2:3])
        P.op("dve", "reciprocal", out=st[:, 3:4], in_=st[:, 2:3])
        ts(P, "dve", oh, oh, 1e30, ALU.mult, -1e30, ALU.add)
        tt(P, "dve", em.rr("p (g k) -> p g k", g=4), e.rr("p (g k) -> p g k", g=4),
           oh[:, :, None].bc([128, 4, 8]), ALU.add)
        P.op("dve", "tensor_reduce", out=st[:, 4:5], in_=em, axis=AX.X, op=ALU.max)
        ts(P, "dve", oh1, em, st[:, 4:5], ALU.is_equal)
        stt(P, "dve", em2, oh1, -1e30, em, ALU.mult, ALU.add)
        P.op("dve", "tensor_reduce", out=st[:, 5:6], in_=em2, axis=AX.X, op=ALU.max)
        ts(P, "dve", oh2, em2, st[:, 5:6], ALU.is_equal)
        tt(P, "dve", st[:, 6:7], st[:, 5:6], st[:, 4:5], ALU.subtract)
        act(P, st[:, 7:8], st[:, 6:7], AF.Exp)
        ts(P, "dve", st[:, 8:9], st[:, 7:8], 1.0, ALU.add)
        P.op("dve", "reciprocal", out=st[:, 9:10], in_=st[:, 8:9])
        tt(P, "dve", st[:, 10:11], st[:, 7:8], st[:, 9:10], ALU.mult)
        ts(P, "dve", wg, oh1, st[:, 9:10], ALU.mult)
        stt(P, "dve", wg, oh2, st[:, 10:11], wg, ALU.mult, ALU.add)
        ts(P, "dve", wg, wg, st[:, 3:4], ALU.mult)
        pt = C.ps[7]
        tr(P, pt[0:32, 0:128], wg, C.ident)
        cp(P, "dve", L.WT[:, t * 128:(t + 1) * 128], pt[0:32, 0:128])
    return per_tile


MOE_GROUPS = [list(range(0, 12)), list(range(12, 24)), list(range(24, 34))]


def phase_moe(C, l, L, H2, last):
    P, I = C.P, C.I
    A = Arena(P)
    hg = A.sb("hg", [128, 8, 12 * 128], BF16)
    wst = [A.sb("ews%d" % i, [128, 4, 512]) for i in range(2)]
    wgu = [A.sb("wgu%d" % i, [128, 2, 8, 512], BF16) for i in range(2)]
    wd = [A.sb("wd%d" % i, [128, 4, 1024], BF16) for i in range(2)]
    yacc = A.sb("eyacc", [128, 12, 1024])
    hid = [A.sb("hid%d" % i, [128, 4, 512], BF16) for i in range(2)]
    sil = [A.sb("sil%d" % i, [128, 512], BF16) for i in range(2)]
    tu = [A.sb("etu%d" % i, [128, 512]) for i in range(2)]
    xt = [A.sb("ext%d" % i, [128, 1024]) for i in range(2)]
    tm2 = A.sb("etm2", [128, 1024])
    ne = 0
    nst = 0
    nb = 0
    for G in MOE_GROUPS:
        tiles = [t for t in G if not (last and t < 2)]
        if not tiles:
            continue
        blocks = [tiles[i:i + 4] for i in range(0, len(tiles), 4)]
        g0 = tiles[0] * 128
        gn = len(tiles) * 128
        dma(P, "sp", hg[:, :, 0:gn], H2[:, :, g0:g0 + gn])
        for e in range(32):
            WGU, WD = wgu[ne % 2], wd[ne % 2]
            ne += 1
            for gi, nm in enumerate(("w_exp_gate", "w_exp_up")):
                src = I[nm][l, e].rr("(k p) n -> p k n", p=128)
                for hf_ in range(2):
                    s_ = wst[nst % 2]
                    dma(P, "sp" if nst % 2 == 0 else "pool", s_, src[:, hf_ * 4:(hf_ + 1) * 4, :])
                    cp(P, "act", WGU[:, gi, hf_ * 4:(hf_ + 1) * 4, :], s_)
                    nst += 1
            srcd = I["w_exp_down"][l, e].rr("(k p) n -> p k n", p=128)
            for hf_ in range(2):
                s_ = wst[nst % 2]
                dma(P, "sp" if nst % 2 == 0 else "pool", s_.rr("p k n -> p (k n)").rr("p (k n) -> p k n", k=2), srcd[:, hf_ * 2:(hf_ + 1) * 2, :])
                cp(P, "dve", WD[:, hf_ * 2:(hf_ + 1) * 2, :], s_.rr("p k n -> p (k n)").rr("p (k n) -> p k n", k=2))
                nst += 1
            for blk in blocks:
                t0 = blk[0] * 128
                nn = len(blk) * 128
                HID = hid[nb % 2]
                psW = C.ps[0]
                mm(P, psW[:, 0:nn], C.ident[0:32, e:e + 1].bc([32, 128]), L.WT[:, t0:t0 + nn], True, True)
                for f in range(4):
                    i2 = (nb * 4 + f) % 2
                    psG, psU = C.ps[1 + 2 * i2], C.ps[2 + 2 * i2]
                    for k in range(8):
                        mm(P, psG[:, 0:nn], WGU[:, 0, k, f * 128:(f + 1) * 128], hg[:, k, t0 - g0:t0 - g0 + nn], k == 0, k == 7)
                    for k in range(8):
                        mm(P, psU[:, 0:nn], WGU[:, 1, k, f * 128:(f + 1) * 128], hg[:, k, t0 - g0:t0 - g0 + nn], k == 0, k == 7)
                    act(P, sil[i2][:, 0:nn], psG[:, 0:nn], AF.Silu)
                    tt(P, "dve", tu[i2][:, 0:nn], psU[:, 0:nn], sil[i2][:, 0:nn], ALU.mult)
                    tt(P, "dve", HID[:, f, 0:nn], tu[i2][:, 0:nn], psW[:, 0:nn], ALU.mult)
                for ti, t in enumerate(blk):
                    ya = yacc[:, t - tiles[0], :]
                    for half in range(2):
                        po = C.ps[5 + (nb * 8 + ti * 2 + half) % 3]
                        for f in range(4):
                            mm(P, po, HID[:, f, ti * 128:(ti + 1) * 128], WD[:, f, half * 512:(half + 1) * 512], f == 0, f == 3)
                        if e == 0:
                            cp(P, "dve", ya[:, half * 512:(half + 1) * 512], po)
                        else:
                            tt(P, "dve", ya[:, half * 512:(half + 1) * 512], ya[:, half * 512:(half + 1) * 512], po, ALU.add)
                nb += 1
        for t in tiles:
            j = 1 if t < 2 else 0
            x = xt[t % 2]
            dma(P, "sp", x, C.xres[t])
            tt(P, "dve", tm2, yacc[:, t - tiles[0], :], L.grow[1][j], ALU.mult)
            tt(P, "pool", x, x, tm2, ALU.add)
            dma(P, "pool", C.xres[t], x)
    A.close()


def final_norm(C):
    P, I = C.P, C.I
    A = Arena(P)
    g = A.sb("fg", [128, D])
    dma(P, "sp", g, I["final_g"][0:1, :].bc([128, D]))
    xt = [A.sb("fxt%d" % i, [128, D]) for i in range(2)]
    junk = A.sb("fjunk", [128, D])
    st = [A.sb("fst%d" % i, [128, 2]) for i in range(2)]
    for t in range(2, NT):
        x, s = xt[t % 2], st[t % 2]
        dma(P, "sp" if t % 2 == 0 else "pool", x, C.xres[t])
        act(P, junk, x, AF.Square, accum_out=s[:, 0:1])
        ts(P, "dve", s[:, 1:2], s[:, 0:1], 1.0 / D, ALU.mult, EPS, ALU.add)
        act(P, s[:, 1:2], s[:, 1:2], AF.Sqrt)
        P.op("dve", "reciprocal", out=s[:, 1:2], in_=s[:, 1:2])
        stt(P, "dve", x, x, s[:, 1:2], g, ALU.mult, ALU.mult)
        dma(P, "sp" if t % 2 == 1 else "pool", V(C.out.ap[(t - 2) * 128:(t - 1) * 128, :], Buf("o%d" % t)), x)
    A.close()


CH_COLS = 3072


def alloc_chunk_heads(A, dd):
    def mkh(name, shape, dt=F32):
        return [A.sb("%s_%d_%d" % (name, dd, h), shape, dt) for h in range(4)]
    AM = mkh("cAM", [128, 4, 128], BF16)
    XB = mkh("cXB", [128, 2, 128], BF16)
    XX = [[AM[h][:, 0:2, :] for h in range(4)], [XB[h] for h in range(4)]]
    AakT = [AM[h][:, 2, :] for h in range(4)]
    ArbT = [AM[h][:, 3, :] for h in range(4)]
    ArkT, TT = [mkh("cM%d" % i, [128, 128], BF16) for i in range(2)]
    PM = mkh("cPM", [128, 2, 64], BF16)
    Ap = [PM[h][:, 0, :] for h in range(4)]
    M1 = [PM[h][:, 1, :] for h in range(4)]
    U0 = mkh("cU0", [128, 64], BF16)
    RpT = mkh("cRpT", [64, 128])
    DPC = mkh("cDPC", [128, 64])
    Y0cc = mkh("cY0cc", [64, 2, 64])
    Y0c = [[Y0cc[h][:, c, :] for h in range(4)] for c in range(2)]
    GH = mkh("cGH", [64, 4, 64])
    GT = [[GH[h][:, 2 * c, :] for h in range(4)] for c in range(2)]
    Hc = [[GH[h][:, 2 * c + 1, :] for h in range(4)] for c in range(2)]
    gA = mkh("gA", [128, 128])
    gY0 = [mkh("gY0%d" % c, [64, 64]) for c in range(2)]
    gH = [mkh("gH%d" % c, [32, 64]) for c in range(2)]
    return (AM, XB, XX, AakT, ArbT, ArkT, TT, PM, Ap, M1, U0, RpT, DPC, Y0cc, Y0c, GH, GT, Hc, gA, gY0, gH)


def phase_chunk(C, l, L):
    P, I = C.P, C.I
    A = Arena(P)
    mk = A.sb("cmasks", [128, 7, 128])
    dma(P, "sp", mk, I["cmasks"])
    idn = C.ident
    chs = [A.sb("chs%d" % i, [128, CH_COLS]) for i in range(2)]
    ST = [[A.sb("cST%d%d" % (d, h), [64, 64]) for h in range(4)] for d in range(2)]
    for d in range(2):
        for h in range(4):
            P.op("dve", "memset", ap=ST[d][h], constant=0.0)

    def mk2(name, shape, dt=F32):
        return [A.sb("%s%d" % (name, i), shape, dt) for i in range(2)]
    TOT, incS, Ein, Enin, Eex, Eend, Etot, tmpx, tmpy = [mk2("cE%d" % i, [128, 256]) for i in range(9)]
    at, rt, bt, kt, bh, kh = [mk2("cq%d" % i, [128, 256]) for i in range(6)]
    aT, rTb, bT, kT = [mk2("cT%d" % i, [128, 2, 128], BF16) for i in range(4)]
    rT = mk2("cTr", [128, 2, 128])
    bhc = [mk2("cbhc%d" % c, [128, 256], BF16) for c in range(2)]
    khc = [mk2("ckhc%d" % c, [128, 256], BF16) for c in range(2)]
    at_b = mk2("cat_b", [128, 256], BF16)
    v_b = mk2("cv_b", [128, 256], BF16)
    IM = A.sb("cIM", [128, 64])
    tt(P, "pool", IM, idn[:, 0:64], idn[:, 64:128], ALU.add)

    HB = []
    for dd in range(2):
        HB.append(alloc_chunk_heads(A, dd))
    MK4 = [A.sb("cMK4%d" % d, [128, 4, 128]) for d in range(2)]
    for d in range(2):
        ms_, mst_, mit_ = (0, 1, 2) if d == 0 else (3, 4, 5)
        for i_, mi_ in enumerate((ms_, mst_, mst_, mit_)):
            cp(P, "pool", MK4[d][:, i_, :], mk[:, mi_, :])
    yo = [A.sb("cyo%d" % i, [64, 2, 256]) for i in range(2)]
    STg = [[A.sb("gST%d%d" % (d, h), [32, 64]) for h in range(4)] for d in range(2)]
    for d in range(2):
        for h in range(4):
            P.op("dve", "memset", ap=STg[d][h], constant=0.0)
    gTOT, gincS, gEin, gEnin, gEend, gEtot, gtmp = [mk2("gE%d" % i, [128, 128]) for i in range(7)]
    gq, gk, gkh = [mk2("gq%d" % i, [128, 128]) for i in range(3)]
    gkhc = [mk2("gkhc%d" % c, [128, 128]) for c in range(2)]
    gqT, gkT, gPT = [[mk2("gT%d_%d" % (i, h), [32, 128]) for h in range(4)] for i in range(3)]
    gyo = [A.sb("gyo%d" % i, [64, 2, 256]) for i in range(2)]
    ps = C.ps
    border = [1, 0] + list(range(NT - 1, 1, -1))
    H4 = range(4)
    it = 0
    cut = C.dbg.get("chunk_cut", 99)

    def body(n, d):
        if True:
            (AM, XB, XX, AakT, ArbT, ArkT, TT, PM, Ap, M1, U0, RpT, DPC, Y0cc, Y0c, GH, GT, Hc, gA, gY0, gH) = HB[d]
            ps = C.ps[4 * d:4 * d + 4] + C.ps[4 - 4 * d:8 - 4 * d]
            t = n if d == 0 else border[n]
            q = d
            ch = chs[q]
            YO = yo[q]
            dma(P, "sp" if d == 0 else "pool", ch, V(C.CH_ap[t * 128:(t + 1) * 128, :], C.CH_bufs[t]))
            lw = ch[:, d * 256:(d + 1) * 256]
            ke = ch[:, 512 + d * 256:768 + d * 256]
            b_ = ch[:, 1024 + d * 256:1280 + d * 256]
            a_, r_, v_ = ch[:, 1536:1792], ch[:, 1792:2048], ch[:, 2048:2304]
            m_s, m_st, m_it = (0, 1, 2) if d == 0 else (3, 4, 5)
            pc = ps[4 + q]
            mm(P, pc[:, 0:256], mk[:, m_it, :], lw, True, True)
            mm(P, pc[:, 256:512], mk[:, 6, :], lw, True, True)
            cp(P, "dve", TOT[q], pc[:, 256:512])
            cp(P, "dve", incS[q], pc[:, 0:256])
            act(P, Ein[q], incS[q], AF.Exp)
            act(P, Enin[q], incS[q], AF.Exp, scale=-1.0)
            tt(P, "pool", tmpx[q], incS[q], lw, ALU.subtract)
            act(P, Eex[q], tmpx[q], AF.Exp)
            tt(P, "pool", tmpy[q], TOT[q], incS[q], ALU.subtract)
            act(P, Eend[q], tmpy[q], AF.Exp)
            act(P, Etot[q], TOT[q], AF.Exp)
            tt(P, "pool", at[q], a_, Eex[q], ALU.mult)
            cp(P, "act", at_b[q], at[q])
            cp(P, "act", v_b[q], v_)
            tt(P, "pool", rt[q], r_, Ein[q], ALU.mult)
            tt(P, "pool", bt[q], b_, Enin[q], ALU.mult)
            tt(P, "pool", kt[q], ke, Enin[q], ALU.mult)
            tt(P, "pool", bh[q], b_, Eend[q], ALU.mult)
            tt(P, "pool", kh[q], ke, Eend[q], ALU.mult)
            for c in range(2):
                ts(P, "pool", bhc[c][q], bh[q], mk[:, 6, c * 64:c * 64 + 1], ALU.mult)
                ts(P, "pool", khc[c][q], kh[q], mk[:, 6, c * 64:c * 64 + 1], ALU.mult)
            yield
            for qi, (src, dst) in enumerate(((at, aT), (rt, rT), (bt, bT), (kt, kT))):
                pb_ = ps[6 + qi % 2]
                for a2 in range(2):
                    tr(P, pb_[:, a2 * 128:(a2 + 1) * 128], src[q][:, a2 * 128:(a2 + 1) * 128], idn)
                cp(P, "dve", dst[q], pb_[:, 0:256].rr("p (a n) -> p a n", a=2))
                if qi == 1:
                    cp(P, "dve", rTb[q], pb_[:, 0:256].rr("p (a n) -> p a n", a=2))

            def hv(h):
                pair, hl = h // 2, h % 2
                return pair, slice(hl * 64, (hl + 1) * 64), slice(h * 64, (h + 1) * 64)
            if cut <= 2:
                return
            yield
            for h in H4:
                pair, hs, hc = hv(h)
                pA = ps[h]
                mm(P, pA[:, 0:128], aT[q][hs, pair, :], bT[q][hs, pair, :], True, True)
                mm(P, pA[:, 128:256], bT[q][hs, pair, :], aT[q][hs, pair, :], True, True)
                mm(P, pA[:, 256:384], kT[q][hs, pair, :], aT[q][hs, pair, :], True, True)
                mm(P, pA[:, 384:512], bT[q][hs, pair, :], rTb[q][hs, pair, :], True, True)
            for h in H4:
                pA = ps[h]
                tt(P, "dve", AM[h].rr("p a n -> p (a n)"), pA, MK4[d].rr("p a n -> p (a n)"), ALU.mult)
                tt(P, "pool", TT[h], AM[h][:, 1, :], idn, ALU.add)
            for h in H4:
                pair, hs, hc = hv(h)
                mm(P, ps[h][:, 0:128], kT[q][hs, pair, :], rTb[q][hs, pair, :], True, True)
            for h in H4:
                tt(P, "dve", ArkT[h], ps[h][:, 0:128], mk[:, m_it, :], ALU.mult)
            if cut <= 3:
                return
            yield
            for s in range(5):
                yield
                XXc, XXn = XX[s % 2], XX[(s + 1) % 2]
                for h in H4:
                    pX = ps[h]
                    mm(P, pX[:, 128:256], XXc[h][:, 1, :], XXc[h][:, 0, :], True, True)
                    if s < 4:
                        mm(P, pX[:, 256:384], XXc[h][:, 0, :], XXc[h][:, 1, :], True, True)
                for h in H4:
                    pX = ps[h]
                    if s < 4:
                        cp(P, "dve", XXn[h], pX[:, 128:384].rr("p (a n) -> p a n", a=2))
                    else:
                        cp(P, "dve", XXn[h][:, 0, :], pX[:, 128:256])
                for h in H4:
                    mm(P, ps[h][:, 384:512], XXn[h][:, 0, :], TT[h], True, True)
                for h in H4:
                    tt(P, "dve", TT[h], TT[h], ps[h][:, 384:512], ALU.add)
            if cut <= 4:
                return
            yield
            for h in H4:
                pair, hs, hc = hv(h)
                mm(P, ps[h][:, 0:64], TT[h], at_b[q][:, hc], True, True)
                mm(P, ps[h][:, 64:128], AakT[h], v_b[q][:, hc], True, True)
            for h in H4:
                cp(P, "dve", PM[h], ps[h][:, 0:128].rr("p (a n) -> p a n", a=2))
            for h in H4:
                mm(P, ps[h][:, 128:192], TT[h], M1[h], True, True)
            for h in H4:
                cp(P, "dve", U0[h], ps[h][:, 128:192])
            for h in H4:
                pair, hs, hc = hv(h)
                for c in range(2):
                    cs = slice(c * 64, (c + 1) * 64)
                    mm(P, ps[h][0:64, 192 + c * 64:256 + c * 64], ArbT[h][:, cs], U0[h], True, False)
                    mm(P, ps[h][0:64, 192 + c * 64:256 + c * 64], ArkT[h][:, cs], v_b[q][:, hc], False, True)
                mm(P, ps[h][0:64, 320:448], Ap[h], ArbT[h], True, True)
                tt(P, "pool", DPC[h], IM, Etot[q][:, hc], ALU.mult)
            for h in H4:
                pair, hs, hc = hv(h)
                cp(P, "dve", Y0cc[h], ps[h][0:64, 192:320].rr("p (a n) -> p a n", a=2))
                tt(P, "dve", RpT[h], ps[h][0:64, 320:448], rT[q][hs, pair, :], ALU.add)
            if cut <= 5:
                return
            for h in H4:
                pair, hs, hc = hv(h)
                pG = ps[h]
                for c in range(2):
                    cs = slice(c * 64, (c + 1) * 64)
                    o = c * 128
                    mm(P, pG[0:64, o:o + 64], Ap[h], bhc[c][q][:, hc], True, False)
                    mm(P, pG[0:64, o:o + 64], idn[:, cs], DPC[h], False, True)
                    mm(P, pG[0:64, o + 64:o + 128], bhc[c][q][:, hc], U0[h], True, False)
                    mm(P, pG[0:64, o + 64:o + 128], khc[c][q][:, hc], v_b[q][:, hc], False, True)
            for h in H4:
                pG = ps[h]
                cp(P, "dve", GH[h], pG[0:64, 0:256].rr("p (a n) -> p a n", a=4))
            if cut <= 6:
                return
            yield
            for c in ((0, 1) if d == 0 else (1, 0)):
                cs = slice(c * 64, (c + 1) * 64)
                for h in H4:
                    pG = ps[h]
                    S_ = ST[d][h]
                    mm(P, pG[0:64, 256:320], RpT[h][:, cs], S_, True, False)
                    mm(P, pG[0:64, 256:320], idn[0:64, 0:64], Y0c[c][h], False, True)
                    mm(P, pG[0:64, 320:384], GT[c][h], S_, True, False)
                    mm(P, pG[0:64, 320:384], idn[0:64, 0:64], Hc[c][h], False, True)
                for h in H4:
                    pair, hs, hc = hv(h)
                    pG = ps[h]
                    cp(P, "dve", YO[:, c, hc], pG[0:64, 256:320])
                    cp(P, "dve", ST[d][h], pG[0:64, 320:384])
            dma(P, "sp" if d == 0 else "pool",
                V(C.YT_ap[d][t * 128:(t + 1) * 128, :].rearrange("(c p) n -> p c n", p=64), C.YT_bufs[d][t]), YO)
            yield
            GYO = gyo[q]
            glw = ch[:, 2304 + d * 128:2432 + d * 128]
            gq_, gk_, gv_ = ch[:, 2560:2688], ch[:, 2688:2816], ch[:, 2816:3072]
            pcg = ps[4 + q]
            mm(P, pcg[:, 0:128], mk[:, m_it, :], glw, True, True)
            mm(P, pcg[:, 128:256], mk[:, 6, :], glw, True, True)
            cp(P, "dve", gincS[q], pcg[:, 0:128])
            cp(P, "dve", gTOT[q], pcg[:, 128:256])
            act(P, gEin[q], gincS[q], AF.Exp)
            act(P, gEnin[q], gincS[q], AF.Exp, scale=-1.0)
            tt(P, "pool", gtmp[q], gTOT[q], gincS[q], ALU.subtract)
            act(P, gEend[q], gtmp[q], AF.Exp)
            act(P, gEtot[q], gTOT[q], AF.Exp)
            tt(P, "pool", gq[q], gq_, gEin[q], ALU.mult)
            tt(P, "pool", gk[q], gk_, gEnin[q], ALU.mult)
            tt(P, "pool", gkh[q], gk_, gEend[q], ALU.mult)
            for c in range(2):
                ts(P, "pool", gkhc[c][q], gkh[q], mk[:, 6, c * 64:c * 64 + 1], ALU.mult)
            for h in H4:
                pT_ = ps[6 + h % 2]
                g32 = slice(h * 32, (h + 1) * 32)
                tr(P, pT_[0:32, 0:128], gq[q][:, g32], idn)
                tr(P, pT_[0:32, 128:256], gk[q][:, g32], idn)
                tr(P, pT_[0:32, 256:384], gEtot[q][:, g32], idn)
                cp(P, "dve", gqT[h][q], pT_[0:32, 0:128])
                cp(P, "dve", gkT[h][q], pT_[0:32, 128:256])
                cp(P, "dve", gPT[h][q], pT_[0:32, 256:384])
            for h in H4:
                mm(P, ps[h][:, 0:128], gkT[h][q], gqT[h][q], True, True)
            for h in H4:
                tt(P, "dve", gA[h], ps[h][:, 0:128], mk[:, m_it, :], ALU.mult)
            for h in H4:
                hc = slice(h * 64, (h + 1) * 64)
                g32 = slice(h * 32, (h + 1) * 32)
                for c in range(2):
                    cs = slice(c * 64, (c + 1) * 64)
                    mm(P, ps[h][0:64, 128 + c * 64:192 + c * 64], gA[h][:, cs], gv_[:, hc], True, True)
                    mm(P, ps[h][0:32, 256 + c * 64:320 + c * 64], gkhc[c][q][:, g32], gv_[:, hc], True, True)
            for h in H4:
                for c in range(2):
                    cp(P, "dve", gY0[c][h], ps[h][0:64, 128 + c * 64:192 + c * 64])
                    cp(P, "dve", gH[c][h], ps[h][0:32, 256 + c * 64:320 + c * 64])
            for c in ((0, 1) if d == 0 else (1, 0)):
                cs = slice(c * 64, (c + 1) * 64)
                for h in H4:
                    S_ = STg[d][h]
                    mm(P, ps[h][0:64, 384:448], gqT[h][q][:, cs], S_, True, False)
                    mm(P, ps[h][0:64, 384:448], idn[0:64, 0:64], gY0[c][h], False, True)
                for h in H4:
                    hc = slice(h * 64, (h + 1) * 64)
                    S_ = STg[d][h]
                    cp(P, "dve", GYO[:, c, hc], ps[h][0:64, 384:448])
                    stt(P, "dve", S_, S_, gPT[h][q][:, c * 64:c * 64 + 1], gH[c][h], ALU.mult, ALU.add)
            dma(P, "sp" if d == 1 else "pool",
                V(C.YTG_ap[d][t * 128:(t + 1) * 128, :].rearrange("(c p) n -> p c n", p=64), C.YTG_bufs[d][t]), GYO)
    for n in range(NT if cut > 50 else 1):
        gens = [body(n, 0), body(n, 1)]
        while gens:
            nxt = []
            for g in gens:
                try:
                    next(g)
                    nxt.append(g)
                except StopIteration:
                    pass
            gens = nxt
    A.close()


class LayerState:
    pass


def layer(C, l):
    P = C.P
    LA = Arena(P)
    L = LayerState()
    L.mod = LA.sb("mod", [128, 48, 2])
    L.sc1 = LA.sb("sc1", [128, 8, 2])
    L.sc2 = LA.sb("sc2", [128, 8, 2])
    L.grow = [[LA.sb("grow%d%d" % (ii, j), [128, 1024]) for j in range(2)] for ii in range(2)]
    phase_ada(C, l, L)
    if C.dbg.get("dump") and l == C.dbg.get("layer", 0):
        dma(P, "sp", C.dout("d_mod", [128, 48, 2]), L.mod)
        for ii in range(2):
            for j in range(2):
                dma(P, "sp", C.dout("d_grow%d%d" % (ii, j), [128, 1024]), L.grow[ii][j])
    HA = Arena(P)
    hfm = HA.sb("hfm", [128, 8, NTOK], BF16)
    phase_norm(C, l, L, 1, hfm)
    if C.dbg.get("dump") and l == C.dbg.get("layer", 0):
        dma(P, "sp", C.dout("d_hfm", [128, 8, NTOK], BF16), hfm)
    phase_win_tm(C, l, L, hfm)
    if not C.dbg.get("skip_gates"):
        phase_win_gates(C, l, L, hfm)
    HA.close()
    if C.dbg.get("stop_after") == "win":
        LA.close(); return
    if not C.dbg.get("skip_ab"):
        phase_prep(C, l, L)
        if C.dbg.get("stop_after") == "prep":
            LA.close(); return
        if C.dbg.get("old_gla") and not C.dbg.get("skip_scan"):
            phase_scan(C, l, C.dbg.get("nchunks"))
        if not C.dbg.get("old_rwkv"):
            phase_chunk(C, l, L)
        if C.dbg.get("stop_after") == "chunk":
            LA.close(); return
        if C.dbg.get("stop_after") == "scan":
            LA.close(); return
        phase_fin_ab(C, l, L)
    if C.dbg.get("stop_after") == "fin":
        LA.close(); return
    if not C.dbg.get("skip_attn"):
        phase_attn(C, l, L)
    if C.dbg.get("stop_after") == "attn":
        LA.close(); return
    if not C.dbg.get("skip_s5"):
        phase_s5(C, l, L)
    if C.dbg.get("stop_after") == "s5":
        LA.close(); return
    phase_merge(C, l, L)
    if C.dbg.get("stop_after") == "merge":
        LA.close(); return
    WA = Arena(P)
    L.WT = WA.sb("WT", [32, NTOK])
    HA = Arena(P)
    hfm2 = HA.sb("hfm2", [128, 8, NTOK], BF16)
    RA = Arena(P)
    phase_norm(C, l, L, 2, hfm2, per_tile=make_router(C, l, L, RA))
    RA.close()
    if C.dbg.get("dump") and l == C.dbg.get("layer", 0):
        dma(P, "sp", C.dout("d_hfm2", [128, 8, NTOK], BF16), hfm2)
        dma(P, "sp", C.dout("d_WT", [32, NTOK]), L.WT)
    H2 = V(C.H2_ap, C.H2_buf)
    dma(P, "sp", H2, hfm2)
    HA.close()
    if C.dbg.get("stop_after") == "norm2":
        WA.close(); LA.close(); return
    phase_moe(C, l, L, H2, l == DEPTH - 1)
    WA.close()
    LA.close()


def make_maskw():
    m = np.zeros((128, 384), np.float32)
    i = np.arange(128)[:, None]
    j = np.arange(128)[None, :]
    m[:, 0:128] = np.where(j >= i, 0.0, -1e30)
    m[:, 256:384] = np.where(j <= i, 0.0, -1e30)
    return m


def make_rope():
    rows = SEQ // 64
    row = np.repeat(np.arange(rows, dtype=np.float32), 64)
    col = np.tile(np.arange(64, dtype=np.float32), rows)
    inv = (10000.0 ** (-np.arange(16, dtype=np.float32) / 16)).astype(np.float32)
    ang = np.concatenate([row[:, None] * inv, col[:, None] * inv], axis=-1).astype(np.float32)
    return np.cos(ang).astype(np.float32), np.sin(ang).astype(np.float32)


ROPE = make_rope()


def s5_host(A):
    f = np.float32
    lre, lim, ldt = A("s5_lam_re"), A("s5_lam_im"), A("s5_log_dt")
    ldt_e = np.repeat(ldt[..., None], 64, axis=-1)
    rows = np.stack([lre.reshape(DEPTH, 2, 1024), lim.reshape(DEPTH, 2, 1024), ldt_e.reshape(DEPTH, 2, 1024)], axis=2)
    sm = rows.reshape(DEPTH, 2, 3, 8, 128).transpose(0, 4, 1, 2, 3)
    bre, bim = A("s5_b_re"), A("s5_b_im")
    bt = np.zeros((DEPTH, 2, 8, 128, 128), f)
    cre, cim = A("s5_c_re"), A("s5_c_im")
    ct = np.zeros((DEPTH, 2, 2, 8, 128, 128), f)
    for g in range(16):
        j, hh = g // 2, g % 2
        c0 = (g % 8) * 16
        for ri, b in enumerate((bre, bim)):
            bt[:, ri, j, c0:c0 + 16, hh * 64:(hh + 1) * 64] = b[:, g].transpose(0, 2, 1)
        for ri, c in enumerate((cre, cim)):
            ct[:, :, ri, j, hh * 64:(hh + 1) * 64, c0:c0 + 16] = c[:, :, g].transpose(0, 1, 3, 2)
    return {
        "s5_sm": np.ascontiguousarray(sm, f), "s5_rows": np.ascontiguousarray(rows, f),
        "s5_bt": bt, "s5_ct": ct,
        "pw2": (2.0 ** np.arange(16)).astype(f).reshape(1, 16),
        "s5_d_fm": np.ascontiguousarray(np.pad(A("s5_d").reshape(DEPTH, 2, 128).transpose(0, 2, 1), ((0, 0), (0, 0), (0, 14)))),
        "s5_bglu_fm": np.ascontiguousarray(np.pad(A("s5_b_glu").reshape(DEPTH, 2, 128).transpose(0, 2, 1), ((0, 0), (0, 0), (0, 14)))),
        "s5_w_glu": A("s5_w_glu"),
    }


def make_sele():
    s = np.zeros((32, 32, 128), np.float32)
    for e in range(32):
        s[e, e, :] = 1.0
    return s


def make_cmasks():
    r = np.arange(128)[:, None]
    c = np.arange(128)[None, :]
    same = (r // 64) == (c // 64)
    m = np.zeros((128, 7, 128), np.float32)
    m[:, 0] = same & (c < r)
    m[:, 1] = same & (r < c)
    m[:, 2] = same & (r <= c)
    m[:, 3] = same & (c > r)
    m[:, 4] = same & (r > c)
    m[:, 5] = same & (r >= c)
    m[:, 6] = same
    return m


def make_sel():
    s = np.zeros((128, 64, 128), np.float32)
    for j in range(64):
        for hh in range(2):
            s[2 * j + hh, j, hh * 64:(hh + 1) * 64] = 1.0
    return s


def blkdiag(mats):
    n = len(mats)
    L, r, c = mats[0].shape
    o = np.zeros((L, n * r, n * c), np.float32)
    for i, m in enumerate(mats):
        o[:, i * r:(i + 1) * r, i * c:(i + 1) * c] = m
    return o


def host_inputs(inputs, b):
    f = np.float32

    def A(k):
        return np.asarray(inputs[k], f)
    c = np.asarray(inputs["c"], f)[b]
    cctx = np.asarray(inputs["c_ctx"], f)
    cc = np.stack([c.reshape(8, 128).T, cctx.reshape(8, 128).T], axis=-1)
    m = {
        "xb": np.ascontiguousarray(np.asarray(inputs["x"], f)[b]),
        "ctxb": np.ascontiguousarray(np.asarray(inputs["ctx"], f)[b]),
        "cc": np.ascontiguousarray(cc),
        "w_ada": np.asarray(inputs["w_ada"], f),
        "b_ada": np.asarray(inputs["b_ada"], f),
        "b_ada_fm": np.ascontiguousarray(np.asarray(inputs["b_ada"], f).reshape(DEPTH, 48, 128).transpose(0, 2, 1)),
        "g1_fm": np.ascontiguousarray(np.asarray(inputs["norm1_g"], f).reshape(DEPTH, 8, 128).transpose(0, 2, 1)),
        "g2_fm": np.ascontiguousarray(np.asarray(inputs["norm2_g"], f).reshape(DEPTH, 8, 128).transpose(0, 2, 1)),
        "w_in": np.asarray(inputs["w_in"], f),
        "ident": np.eye(128, dtype=f),
        "sel": make_sel(),
        "maskw": make_maskw(),
        "ropec": ROPE[0],
        "ropes": ROPE[1],
        "attn_sink": np.ascontiguousarray(np.pad(A("attn_sink"), ((0, 0), (0, 12)))),
        **s5_host(A),
        "cmasks": make_cmasks(),
        "w_branch": A("w_branch"),
        "w_out": A("w_out"),
        "w_router": np.ascontiguousarray(np.concatenate([A("w_router_g"), A("w_router_e")], axis=2)),
        "b_router": np.ascontiguousarray(np.concatenate([A("b_router_g"), A("b_router_e")], axis=1)),
        "w_exp_gate": A("w_exp_gate"),
        "w_exp_up": A("w_exp_up"),
        "w_exp_down": A("w_exp_down"),
        "pv": np.ascontiguousarray(np.concatenate([
            A("rwkv_mu").reshape(DEPTH, -1), A("rwkv_kk"), A("rwkv_ka"), A("rwkv_rk").reshape(DEPTH, -1),
            A("rwkv_w0").reshape(DEPTH, -1), A("rwkv_a0").reshape(DEPTH, -1), A("rwkv_ln_g"),
            A("gla_ab").reshape(DEPTH, -1), A("gla_ln_g")], axis=1)),
        "w1cat": np.ascontiguousarray(np.concatenate([A("rwkv_w1")[:, 0], A("rwkv_w1")[:, 1],
                                                      A("rwkv_a1")[:, 0], A("rwkv_a1")[:, 1]], axis=2)),
        "w2blk": blkdiag([A("rwkv_w2")[:, 0], A("rwkv_w2")[:, 1], A("rwkv_a2")[:, 0], A("rwkv_a2")[:, 1]]),
        "g1": A("rwkv_g1"),
        "g2": A("rwkv_g2"),
        "a2blk": blkdiag([A("gla_a2")[:, 0], A("gla_a2")[:, 1]]),
        "final_g": np.asarray(inputs["final_norm_g"], f).reshape(1, D),
    }
    return m


def kernel(**inputs):
    nc = build_program()
    in_maps = [host_inputs(inputs, b) for b in range(8)]
    res = run_bass_kernel_spmd(nc, in_maps, core_ids=list(range(8)))
    return np.stack([r["out"] for r in res.results], axis=0)
```

```python
import math
from contextlib import ExitStack

import numpy as np
import concourse.bass as bass
import concourse.mybir as mybir
from concourse.bass_utils import run_bass_kernel_spmd

F32 = mybir.dt.float32
BF16 = mybir.dt.bfloat16
ALU = mybir.AluOpType
AF = mybir.ActivationFunctionType
AX = mybir.AxisListType

D = 1024
SEQ = 4096
CTX = 256
NT = (SEQ + CTX) // 128
NTOK = SEQ + CTX
DEPTH = 2
EPS = 1e-6
GN_EPS = 64e-5
O1, O2, O3, O4, PIN = 1024, 1824, 2336, 2592, 6688
PV_MU, PV_KK, PV_KA, PV_RK, PV_W0, PV_A0, PV_LNG, PV_GAB, PV_GLNG = 0, 1024, 1280, 1536, 1792, 2304, 2816, 3072, 3328
NPV = 3584

DEBUG = False
PENDING = "PENDING"
WKEYS = ("out", "accum_out", "ap")
SKEYS = ("scalar1", "scalar2", "scale", "bias", "scalar")


class Buf:
    __slots__ = ("name", "w", "rd", "ws")

    def __init__(self, name=""):
        self.name = name
        self.w = None
        self.rd = {}
        self.ws = False


class V:
    __slots__ = ("ap", "bufs")

    def __init__(self, ap, bufs):
        self.ap = ap
        self.bufs = bufs if isinstance(bufs, tuple) else (bufs,)

    def __getitem__(self, k):
        return V(self.ap[k], self.bufs)

    def rr(self, pat, **kw):
        return V(self.ap.rearrange(pat, **kw), self.bufs)

    def bc(self, shape):
        return V(self.ap.to_broadcast(list(shape)), self.bufs)

    def bitcast(self, dt):
        return V(self.ap.bitcast(dt), self.bufs)

    def wb(self, *bufs):
        return V(self.ap, tuple(bufs))

    @property
    def shape(self):
        return tuple(self.ap.shape)


class Prog:
    ENG = ("pe", "act", "dve", "pool", "sp")
    K = 6
    STRICT_ALL = False

    def __init__(self, nc):
        self.nc = nc
        self.eng = {"pe": nc.tensor, "act": nc.scalar, "dve": nc.vector, "pool": nc.gpsimd, "sp": nc.sync}
        self.sem = {e: nc.alloc_semaphore("s_" + e) for e in self.ENG}
        self.cnt = {e: 0 for e in self.ENG}
        self.dsem = {q: [nc.alloc_semaphore("d_%s%d" % (q, i)) for i in range(self.K)] for q in ("sp", "act", "pool")}
        self.dcnt = {q: 0 for q in self.dsem}
        self.known = {e: {} for e in self.ENG}
        self.pend_r = []
        self.pend_w = []
        self.uid = 0
        self.nins = 0

    def _need(self, e, tok, is_dma, strict=False):
        if tok is None:
            return
        if tok is PENDING:
            assert e == "pe" and not is_dma, "dependency on an unmarked PE op"
            return
        sem, val, owner = tok
        if owner == e and not is_dma and not (strict and e != "pe") and not self.STRICT_ALL:
            return
        k = self.known[e]
        if k.get(sem.num, 0) >= val:
            return
        self.eng[e].wait_ge(sem, val)
        self.nins += 1
        k[sem.num] = val

    def op(self, e, meth, mark=True, lax=False, **kw):
        reads, writes, args, sreads, awrites = [], [], {}, [], []
        for k, v in kw.items():
            if isinstance(v, V):
                (writes if k in WKEYS else reads).extend(v.bufs)
                if k in SKEYS:
                    sreads.extend(v.bufs)
                if k == "accum_out":
                    awrites.extend(v.bufs)
                args[k] = v.ap
            else:
                args[k] = v
        is_dma = meth == "dma_start"
        for b in reads:
            self._need(e, b.w, is_dma, strict=(not lax) or b.ws or e == "act" or b in sreads)
        for b in writes:
            self._need(e, b.w, is_dma)
            for t in b.rd.values():
                self._need(e, t, is_dma)
        if is_dma:
            n = self.dcnt[e]
            sem = self.dsem[e][n % self.K]
            r = n // self.K
            if r > 0:
                self._need(e, (sem, 16 * r, None), True)
            ins = getattr(self.eng[e], meth)(**args)
            ins.then_inc(sem, 16)
            self.dcnt[e] = n + 1
            tok = (sem, 16 * (r + 1), None)
            key = ("d", sem.num)
        else:
            ins = getattr(self.eng[e], meth)(**args)
            key = e
            if mark:
                self.cnt[e] += 1
                ins.then_inc(self.sem[e], 1)
                tok = (self.sem[e], self.cnt[e], e)
                if e == "pe" and (self.pend_r or self.pend_w):
                    for b in self.pend_r:
                        if b.rd.get("pe") is PENDING:
                            b.rd["pe"] = tok
                    for b in self.pend_w:
                        if b.w is PENDING:
                            b.w = tok
                    self.pend_r = []
                    self.pend_w = []
            else:
                assert e == "pe"
                tok = PENDING
                self.pend_r.extend(reads)
                self.pend_w.extend(writes)
        self.nins += 1
        for b in reads:
            b.rd[key] = tok
        for b in writes:
            b.w = tok
            b.rd = {}
            b.ws = (b in awrites) or e == "act"
        return ins

    def barrier(self):
        assert not self.pend_r and not self.pend_w
        toks = [(self.sem[e], self.cnt[e], e) for e in self.ENG if self.cnt[e] > 0]
        for q in self.dsem:
            n = self.dcnt[q]
            for i in range(self.K):
                c = (n - i + self.K - 1) // self.K if n > i else 0
                if c > 0:
                    toks.append((self.dsem[q][i], 16 * c, None))
        for e in self.ENG:
            for t in toks:
                self._need(e, t, False)

    def name(self, s):
        self.uid += 1
        return "%s_%d" % (s, self.uid)

    def dram(self, name, shape, dt, kind="Internal"):
        return self.nc.dram_tensor(name, list(shape), dt, kind=kind).ap()


class Arena:
    def __init__(self, P):
        self.P = P
        self.stack = ExitStack()

    def sb(self, name, shape, dt=F32):
        h = self.stack.enter_context(self.P.nc.sbuf_tensor(self.P.name(name), list(shape), dt))
        return V(h.ap(), Buf(name))

    def close(self):
        self.P.barrier()
        self.stack.close()


def dma(P, q, out, in_):
    return P.op(q, "dma_start", out=out, in_=in_)


def mm(P, out, lhsT, rhs, start, stop, mark=None):
    return P.op("pe", "matmul", mark=(stop if mark is None else mark), out=out, lhsT=lhsT, rhs=rhs,
                start=start, stop=stop)


def tr(P, out, in_, ident, mark=True):
    return P.op("pe", "transpose", mark=mark, out=out, in_=in_, identity=ident)


def tt(P, e, out, in0, in1, op):
    return P.op(e, "tensor_tensor", out=out, in0=in0, in1=in1, op=op)


def ts(P, e, out, in0, s1, op0, s2=None, op1=None, **kw):
    if op1 is None:
        return P.op(e, "tensor_scalar", out=out, in0=in0, scalar1=s1, scalar2=None, op0=op0, **kw)
    return P.op(e, "tensor_scalar", out=out, in0=in0, scalar1=s1, scalar2=s2, op0=op0, op1=op1, **kw)


def act(P, out, in_, func, **kw):
    return P.op("act", "activation", out=out, in_=in_, func=func, **kw)


def cp(P, e, out, in_):
    if e == "act":
        return act(P, out, in_, AF.Copy)
    return P.op(e, "tensor_copy", out=out, in_=in_)


class Ctx:
    pass


def build_program(dbg=None):
    dbg = dbg or {}
    nc = bass.Bass("TRN2", target_bir_lowering=False)
    P = Prog(nc)
    C = Ctx()
    C.P, C.nc, C.dbg = P, nc, dbg
    skind = "ExternalOutput" if dbg.get("expose") else "Internal"

    def din(name, shape, dt=F32):
        return V(nc.dram_tensor(name, list(shape), dt, kind="ExternalInput").ap(), Buf(name))

    I = {}
    I["xb"] = din("xb", [SEQ, D])
    I["ctxb"] = din("ctxb", [CTX, D])
    I["cc"] = din("cc", [128, 8, 2])
    I["w_ada"] = din("w_ada", [DEPTH, D, 6 * D])
    I["b_ada"] = din("b_ada", [DEPTH, 6 * D])
    I["b_ada_fm"] = din("b_ada_fm", [DEPTH, 128, 48])
    I["g1_fm"] = din("g1_fm", [DEPTH, 128, 8])
    I["g2_fm"] = din("g2_fm", [DEPTH, 128, 8])
    I["w_in"] = din("w_in", [DEPTH, D, PIN])
    I["ident"] = din("ident", [128, 128])
    I["sel"] = din("sel", [128, 64, 128])
    I["maskw"] = din("maskw", [128, 384])
    I["ropec"] = din("ropec", [SEQ, 32])
    I["ropes"] = din("ropes", [SEQ, 32])
    I["attn_sink"] = din("attn_sink", [DEPTH, 16])
    I["s5_sm"] = din("s5_sm", [DEPTH, 128, 2, 3, 8])
    I["s5_rows"] = din("s5_rows", [DEPTH, 2, 3, 1024])
    I["s5_bt"] = din("s5_bt", [DEPTH, 2, 8, 128, 128])
    I["s5_ct"] = din("s5_ct", [DEPTH, 2, 2, 8, 128, 128])
    I["pw2"] = din("pw2", [1, 16])
    I["cmasks"] = din("cmasks", [128, 7, 128])
    I["w_branch"] = din("w_branch", [DEPTH, 4, 256, D])
    I["w_out"] = din("w_out", [DEPTH, D, D])
    I["w_router"] = din("w_router", [DEPTH, D, 36])
    I["b_router"] = din("b_router", [DEPTH, 36])
    I["w_exp_gate"] = din("w_exp_gate", [DEPTH, 32, D, 512])
    I["w_exp_up"] = din("w_exp_up", [DEPTH, 32, D, 512])
    I["w_exp_down"] = din("w_exp_down", [DEPTH, 32, 512, D])
    I["s5_d_fm"] = din("s5_d_fm", [DEPTH, 128, 16])
    I["s5_bglu_fm"] = din("s5_bglu_fm", [DEPTH, 128, 16])
    I["s5_w_glu"] = din("s5_w_glu", [DEPTH, 256, 256])
    I["pv"] = din("pv", [DEPTH, NPV])
    I["w1cat"] = din("w1cat", [DEPTH, 256, 128])
    I["w2blk"] = din("w2blk", [DEPTH, 128, 1024])
    I["g1"] = din("g1", [DEPTH, 256, 64])
    I["g2"] = din("g2", [DEPTH, 64, 256])
    I["a2blk"] = din("a2blk", [DEPTH, 32, 256])
    I["final_g"] = din("final_g", [1, D])
    C.I = I

    out = V(nc.dram_tensor("out", [SEQ, D], F32, kind="ExternalOutput").ap(), Buf("out"))
    C.out = out

    def dout(name, shape, dt=F32):
        return V(nc.dram_tensor(name, list(shape), dt, kind="ExternalOutput").ap(), Buf(name))
    C.dout = dout

    xres_ap = P.dram("xres", [NTOK, D], F32, kind=skind)
    C.xres = [V(xres_ap[t * 128:(t + 1) * 128, :], Buf("xres%d" % t)) for t in range(NT)]
    C.U_ap = P.dram("U", [NTOK + 3, O4], F32, kind=skind)
    C.U_bufs = [Buf("U%d" % t) for t in range(NT)]
    C.U_pad = Buf("Upad")
    C.STR_ap = [P.dram("STR%d" % d, [NTOK, 2, 1024], BF16, kind=skind) for d in range(2)]
    C.STR_bufs = [[Buf("STR%d_%d" % (d, t)) for t in range(NT)] for d in range(2)]
    C.Vs_ap = P.dram("Vs", [128, NTOK, 6], F32, kind=skind)
    C.Vs_bufs = [Buf("Vs%d" % t) for t in range(NT)]
    C.Y_ap = [P.dram("Y%d" % d, [128, NTOK, 6], F32, kind=skind) for d in range(2)]
    C.Y_bufs = [[Buf("Y%d_%d" % (d, c)) for c in range(NTOK // 64)] for d in range(2)]
    C.FIN_ap = P.dram("FIN", [NTOK, 768], F32, kind=skind)
    C.FIN_bufs = [Buf("FIN%d" % t) for t in range(NT)]
    C.YB_ap = P.dram("YB", [4, 256, NTOK], BF16, kind=skind)
    C.YB_bufs = [[Buf("YB%d_%d" % (i, t)) for t in range(NT)] for i in range(4)]
    C.CH_ap = P.dram("CH", [NTOK, 3072], F32, kind=skind)
    C.CH_bufs = [Buf("CH%d" % t) for t in range(NT)]
    C.YT_ap = [P.dram("YT%d" % d, [NTOK, 256], F32, kind=skind) for d in range(2)]
    C.YT_bufs = [[Buf("YT%d_%d" % (d, t)) for t in range(NT)] for d in range(2)]
    C.YTG_ap = [P.dram("YTG%d" % d, [NTOK, 256], F32, kind=skind) for d in range(2)]
    C.YTG_bufs = [[Buf("YTG%d_%d" % (d, t)) for t in range(NT)] for d in range(2)]
    C.Gt_ap = P.dram("Gt", [4096, NTOK], BF16, kind=skind)
    C.Gt_bufs = [Buf("Gt%d" % b) for b in range(9)]
    C.H2_ap = P.dram("H2", [128, 8, NTOK], BF16, kind=skind)
    C.H2_buf = Buf("H2")

    G = Arena(P)
    C.G = G
    C.psall = nc.alloc_psum_tensor("psall", [128, 4096], F32).ap()
    C.psb = [Buf("ps%d" % i) for i in range(8)]
    C.ps = [V(C.psall[:, i * 512:(i + 1) * 512], C.psb[i]) for i in range(8)]
    C.ident = G.sb("ident", [128, 128])
    dma(P, "sp", C.ident, I["ident"])

    for l in range(DEPTH):
        layer(C, l)
        if dbg.get("stop_layer") == l:
            break
    if not dbg.get("stop"):
        final_norm(C)
    P.barrier()
    return nc


def urow(t):
    return 1 + t * 128 if t < 2 else 258 + (t - 2) * 128


def xsrc(C, l, t):
    if l == 0 and not C.__dict__.get("x_in_scratch"):
        if t < 2:
            return C.I["ctxb"][t * 128:(t + 1) * 128, :]
        return C.I["xb"][(t - 2) * 128:(t - 1) * 128, :]
    return C.xres[t]


def phase_ada(C, l, L):
    P, I = C.P, C.I
    A = Arena(P)
    cc = A.sb("cc", [128, 8, 2])
    sc = A.sb("sc", [128, 8, 2])
    screp = A.sb("screp", [128, 8, 2, 128])
    bfm = A.sb("bfm", [128, 48])
    g1 = A.sb("g1", [128, 8])
    g2 = A.sb("g2", [128, 8])
    wst = [A.sb("wst%d" % i, [128, 8, 512]) for i in range(2)]
    brow = [A.sb("brow%d" % i, [128, 1024]) for i in range(2)]
    dma(P, "sp", cc, I["cc"])
    dma(P, "sp", bfm, I["b_ada_fm"][l])
    dma(P, "sp", g1, I["g1_fm"][l])
    dma(P, "sp", g2, I["g2_fm"][l])
    for ii, i in enumerate((2, 5)):
        dma(P, "pool", brow[ii], I["b_ada"][l:l + 1, i * 1024:(i + 1) * 1024].bc([128, 1024]))
    act(P, sc, cc, AF.Silu)
    for k in range(8):
        for j in range(2):
            cp(P, "dve", screp[:, k, j, :], sc[:, k, j:j + 1].bc([128, 128]))
    wv = I["w_ada"][l].rr("(k p) n -> p k n", p=128)
    psA = C.ps[0]
    for c in range(12):
        w = wst[c % 2]
        dma(P, "sp" if c % 2 == 0 else "pool", w, wv[:, :, c * 512:(c + 1) * 512])
        for mi in range(4):
            m = c * 4 + mi
            for k in range(8):
                mm(P, psA[:, m * 2:(m + 1) * 2], w[:, k, mi * 128:(mi + 1) * 128], sc[:, k, :], k == 0, k == 7)
        if c in (4, 5, 10, 11):
            ii = 0 if c < 6 else 1
            half = c % 2
            for j in range(2):
                pr = C.ps[1 + j]
                for k in range(8):
                    mm(P, pr, screp[:, k, j, :], w[:, k, :], k == 0, k == 7)
                tt(P, "dve", L.grow[ii][j][:, half * 512:(half + 1) * 512], pr,
                   brow[ii][:, half * 512:(half + 1) * 512], ALU.add)
    tt(P, "dve", L.mod, psA[:, 0:96].rr("p (m j) -> p m j", j=2), bfm[:, :, None].bc([128, 48, 2]), ALU.add)
    ts(P, "dve", L.sc1, L.mod[:, 8:16, :], 1.0, ALU.add)
    tt(P, "dve", L.sc1, L.sc1, g1[:, :, None].bc([128, 8, 2]), ALU.mult)
    ts(P, "dve", L.sc2, L.mod[:, 32:40, :], 1.0, ALU.add)
    tt(P, "dve", L.sc2, L.sc2, g2[:, :, None].bc([128, 8, 2]), ALU.mult)
    A.close()


def phase_norm(C, l, L, which, hfm, per_tile=None):
    P = C.P
    A = Arena(P)
    sc = L.sc1 if which == 1 else L.sc2
    shb = 0 if which == 1 else 24
    xt = [A.sb("xt%d" % i, [128, D]) for i in range(2)]
    junk = A.sb("junk", [128, D])
    st = [A.sb("st%d" % i, [128, 2]) for i in range(2)]
    hf = [A.sb("hf%d" % i, [128, 8, 128]) for i in range(2)] if per_tile else None
    for t in range(NT):
        j = 1 if t < 2 else 0
        x = xt[t % 2]
        s = st[t % 2]
        dma(P, "sp" if t % 2 == 0 else "pool", x, xsrc(C, l, t))
        act(P, junk, x, AF.Square, accum_out=s[:, 0:1])
        ts(P, "dve", s[:, 1:2], s[:, 0:1], 1.0 / D, ALU.mult, EPS, ALU.add)
        act(P, s[:, 1:2], s[:, 1:2], AF.Sqrt)
        P.op("dve", "reciprocal", out=s[:, 1:2], in_=s[:, 1:2])
        ts(P, "dve", x, x, s[:, 1:2], ALU.mult)
        pa, pb = C.ps[2 + 2 * (t % 2)], C.ps[3 + 2 * (t % 2)]
        if C.dbg.get("dump_norm") and t == 2 and which == 1:
            dma(P, "sp", C.dout("d_xn", [128, D]), x)
            dma(P, "sp", C.dout("d_st", [128, 2]), s)
        for k in range(8):
            pp = pa if k < 4 else pb
            tr(P, pp[:, (k % 4) * 128:(k % 4 + 1) * 128], x[:, k * 128:(k + 1) * 128], C.ident)
        if C.dbg.get("dump_norm") and t == 2 and which == 1:
            cp(P, "dve", junk[:, 0:512], pa)
            dma(P, "sp", C.dout("d_pa", [128, 512]), junk[:, 0:512])
        for k in range(8):
            pp = pa if k < 4 else pb
            src = pp[:, (k % 4) * 128:(k % 4 + 1) * 128]
            if per_tile:
                dst = hf[t % 2][:, k, :]
            else:
                dst = hfm[:, k, t * 128:(t + 1) * 128]
            if k % 2 == 0:
                act(P, dst, src, AF.Identity, scale=sc[:, k, j:j + 1], bias=L.mod[:, shb + k, j:j + 1])
            else:
                ts(P, "dve", dst, src, sc[:, k, j:j + 1], ALU.mult, L.mod[:, shb + k, j:j + 1], ALU.add)
        if per_tile:
            for k in range(8):
                cp(P, "act" if k % 2 == 0 else "dve", hfm[:, k, t * 128:(t + 1) * 128], hf[t % 2][:, k, :])
            per_tile(t, hf[t % 2])
    A.close()


def phase_win_tm(C, l, L, hfm):
    P, I = C.P, C.I
    A = Arena(P)
    wA = A.sb("wA", [128, 8, O4], BF16)
    wst = [A.sb("wst%d" % i, [128, 8, 512]) for i in range(2)]
    ust = [A.sb("ust%d" % i, [128, O4]) for i in range(2)]
    zer = A.sb("zer", [1, O4])
    P.op("dve", "memset", ap=zer, constant=0.0)
    for r in (0, 257, NTOK + 2):
        dma(P, "sp", V(C.U_ap[r:r + 1, :], C.U_pad), zer)
    wv = I["w_in"][l].rr("(k p) n -> p k n", p=128)
    blocks = [(0, 512), (512, 1024), (1024, 1536), (1536, 1824), (1824, 2336), (2336, 2592)]
    for bi, (c0, c1) in enumerate(blocks):
        w = wst[bi % 2]
        dma(P, "sp" if bi % 2 == 0 else "pool", w[:, :, 0:c1 - c0], wv[:, :, c0:c1])
        cp(P, "act", wA[:, :, c0:c1], w[:, :, 0:c1 - c0])
    n = 0
    for t in range(NT):
        u = ust[t % 2]
        for bi, (c0, c1) in enumerate(blocks):
            ps = C.ps[n % 4]
            n += 1
            for k in range(8):
                mm(P, ps[:, 0:c1 - c0], hfm[:, k, t * 128:(t + 1) * 128], wA[:, k, c0:c1], k == 0, k == 7)
            cp(P, "act" if bi % 2 == 0 else "dve", u[:, c0:c1], ps[:, 0:c1 - c0])
        r0 = urow(t)
        dma(P, "sp" if t % 2 == 0 else "pool", V(C.U_ap[r0:r0 + 128, :], C.U_bufs[t]), u)
    A.close()


def stt(P, e, out, in0, scalar, in1, op0, op1, **kw):
    return P.op(e, "scalar_tensor_tensor", out=out, in0=in0, scalar=scalar, in1=in1, op0=op0, op1=op1, **kw)


def red(P, e, out, in_, **kw):
    return P.op(e, "tensor_reduce", out=out, in_=in_, axis=AX.X, op=ALU.add, **kw)


def load_bf16(P, A, name, shape, src, q="sp", ce="act"):
    st = A.sb(name + "_f", shape)
    wb = A.sb(name, shape, BF16)
    dma(P, q, st, src)
    cp(P, ce, wb, st)
    return wb


def cust(v, offset_elems, dims):
    ap = v.ap
    base = ap.ap[0]
    new = type(ap)(ap.tensor, ap.offset + offset_elems, [tuple(base)] + [tuple(d) for d in dims])
    return V(new, v.bufs)


def phase_prep(C, l, L):
    P, I = C.P, C.I
    A = Arena(P)
    pv = A.sb("pv", [128, NPV])
    dma(P, "sp", pv, I["pv"][l:l + 1, :].bc([128, NPV]))
    w1cat = load_bf16(P, A, "w1cat", [128, 2, 128], I["w1cat"][l].rr("(k p) n -> p k n", p=128))
    w2blk = load_bf16(P, A, "w2blk", [128, 1024], I["w2blk"][l])
    g1 = load_bf16(P, A, "g1w", [128, 2, 64], I["g1"][l].rr("(k p) n -> p k n", p=128))
    g2 = load_bf16(P, A, "g2w", [64, 256], I["g2"][l])
    a2blk = load_bf16(P, A, "a2blk", [32, 256], I["a2blk"][l])
    mu = pv[:, PV_MU:PV_MU + 1024]
    kkp = pv[:, PV_KK:PV_KK + 256]
    ka = pv[:, PV_KA:PV_KA + 256]
    rkp = pv[:, PV_RK:PV_RK + 256]
    w0 = pv[:, PV_W0:PV_W0 + 512]
    a0 = pv[:, PV_A0:PV_A0 + 512]
    gab = pv[:, PV_GAB:PV_GAB + 256]
    glng = pv[:, PV_GLNG:PV_GLNG + 256]

    uc = [A.sb("uc%d" % i, [128, O2]) for i in range(2)]
    up = [A.sb("up%d" % i, [128, 1024]) for i in range(2)]
    un = [A.sb("un%d" % i, [128, 1024]) for i in range(2)]
    rows = [[A.sb("rows%d%d" % (d, i), [128, 2, 1024], BF16) for i in range(2)] for d in range(2)]
    vt = [A.sb("vt%d" % i, [128, 128, 6]) for i in range(2)]
    fin = [A.sb("fin%d" % i, [128, 768]) for i in range(2)]
    cht = [A.sb("cht%d" % i, [128, 3072]) for i in range(2)]
    t0 = A.sb("t0", [128, 1024])
    mx = A.sb("mx", [128, 1024])
    xaT = A.sb("xaT", [128, 2, 128], BF16)
    z = A.sb("z", [128, 128], BF16)
    sg = A.sb("sg", [64, 128], BF16)
    wl = A.sb("wl", [128, 512])
    wdec = A.sb("wdec", [128, 512])
    il = A.sb("il", [128, 512])
    iclr = A.sb("iclr", [128, 512])
    kk0 = A.sb("kk0", [128, 256])
    sq = A.sb("sq", [128, 256])
    ss = A.sb("ss", [128, 8])
    kk = A.sb("kk", [128, 256])
    t1 = A.sb("t1", [128, 512])
    keff = A.sb("keff", [128, 512])
    bb = A.sb("bb", [128, 512])
    rkt = A.sb("rkt", [128, 256])
    alT = A.sb("alT", [32, 128], BF16)
    gl = A.sb("gl", [128, 256])
    gdec = A.sb("gdec", [128, 256])
    sr = A.sb("sr", [128, 256])

    def rv(R, c0, n):
        return R[:, :, c0:c0 + n]

    for t in range(NT):
        i = t % 2
        r0 = urow(t)
        nb = [C.U_bufs[t]]
        if t > 0:
            nb.append(C.U_bufs[t - 1])
        if t < NT - 1:
            nb.append(C.U_bufs[t + 1])
        nb.append(C.U_pad)
        dma(P, "sp", uc[i], V(C.U_ap[r0:r0 + 128, 0:O2], C.U_bufs[t]))
        dma(P, "pool", up[i], V(C.U_ap[r0 - 1:r0 + 127, 0:1024], tuple(nb)))
        dma(P, "sp", un[i], V(C.U_ap[r0 + 1:r0 + 129, 0:1024], tuple(nb)))
        u = uc[i]
        R0, R1 = rows[0][i], rows[1][i]
        F = fin[i]
        tt(P, "dve", t0, up[i], un[i], ALU.add)
        stt(P, "dve", t0, t0, 0.5, u[:, 0:1024], ALU.mult, ALU.subtract)
        tt(P, "dve", t0, t0, mu, ALU.mult)
        tt(P, "dve", mx, t0, u[:, 0:1024], ALU.add)
        r_, k_, v_, xa_ = mx[:, 0:256], mx[:, 256:512], mx[:, 512:768], mx[:, 768:1024]
        pT = C.ps[0]
        for kt in range(2):
            tr(P, pT[:, kt * 128:(kt + 1) * 128], xa_[:, kt * 128:(kt + 1) * 128], C.ident)
        cp(P, "act", xaT, pT[:, 0:256].rr("p (k n) -> p k n", k=2))
        pz = C.ps[1]
        for kt in range(2):
            mm(P, pz[:, 0:128], w1cat[:, kt, :], xaT[:, kt, :], kt == 0, kt == 1)
        for kt in range(2):
            mm(P, pz[0:64, 128:256], g1[:, kt, :], xaT[:, kt, :], kt == 0, kt == 1)
        act(P, z[0:64, :], pz[0:64, 0:128], AF.Tanh)
        cp(P, "dve", z[64:128, :], pz[64:128, 0:128])
        act(P, sg, pz[0:64, 128:256], AF.Sigmoid)
        pw, pa_, pg = C.ps[2], C.ps[3], C.ps[4]
        mm(P, pw, z, w2blk[:, 0:512], True, True)
        mm(P, pa_, z, w2blk[:, 512:1024], True, True)
        mm(P, pg[:, 0:256], sg, g2, True, True)
        tt(P, "dve", wl, pw, w0, ALU.add)
        act(P, wl, wl, AF.Sigmoid)
        act(P, wdec, wl, AF.Exp, scale=-0.6065306597126334)
        tt(P, "dve", il, pa_, a0, ALU.add)
        act(P, iclr, il, AF.Sigmoid)
        cp(P, "act", F[:, 0:256], pg[:, 0:256])
        tt(P, "dve", kk0, k_, kkp, ALU.mult)
        tt(P, "dve", sq, kk0, kk0, ALU.mult)
        red(P, "dve", ss[:, 0:4], sq.rr("p (h k) -> p h k", h=4))
        ts(P, "dve", ss[:, 0:4], ss[:, 0:4], EPS, ALU.add)
        act(P, ss[:, 0:4], ss[:, 0:4], AF.Sqrt)
        P.op("dve", "reciprocal", out=ss[:, 0:4], in_=ss[:, 0:4])
        tt(P, "dve", kk.rr("p (h k) -> p h k", h=4), kk0.rr("p (h k) -> p h k", h=4),
           ss[:, 0:4][:, :, None].bc([128, 4, 64]), ALU.mult)
        ic3 = iclr.rr("p (d c) -> p d c", d=2)
        stt(P, "dve", t1.rr("p (d c) -> p d c", d=2), ic3, -1.0, ka[:, None, :].bc([128, 2, 256]), ALU.add, ALU.mult)
        stt(P, "dve", keff.rr("p (d c) -> p d c", d=2), t1.rr("p (d c) -> p d c", d=2), 1.0,
            k_[:, None, :].bc([128, 2, 256]), ALU.add, ALU.mult)
        tt(P, "dve", bb.rr("p (d c) -> p d c", d=2), ic3, kk[:, None, :].bc([128, 2, 256]), ALU.mult)
        tt(P, "dve", rkt, r_, k_, ALU.mult)
        tt(P, "dve", rkt, rkt, rkp, ALU.mult)
        red(P, "dve", ss[:, 4:8], rkt.rr("p (h k) -> p h k", h=4))
        tt(P, "dve", F[:, 256:512].rr("p (h k) -> p h k", h=4), v_.rr("p (h k) -> p h k", h=4),
           ss[:, 4:8][:, :, None].bc([128, 4, 64]), ALU.mult)
        pT2 = C.ps[5]
        tr(P, pT2[0:32, 0:128], u[:, O1 + 768:O1 + 800], C.ident)
        cp(P, "act", alT, pT2[0:32, 0:128])
        mm(P, pT2[:, 128:384], alT, a2blk, True, True)
        tt(P, "dve", gl, pT2[:, 128:384], gab, ALU.add)
        act(P, gl, gl, AF.Sigmoid)
        act(P, gl, gl, AF.Ln)
        if C.dbg.get("old_gla"):
            act(P, gdec, gl, AF.Exp, scale=1.0 / 16.0)
        act(P, sr, u[:, O1 + 512:O1 + 768], AF.Silu)
        tt(P, "dve", F[:, 512:768], sr, glng, ALU.mult)
        CHt = cht[i]
        ts(P, "dve", CHt[:, 0:512], wl, -0.6065306597126334, ALU.mult)
        cp(P, "act", CHt[:, 512:1024], keff)
        cp(P, "dve", CHt[:, 1024:1536], bb)
        ts(P, "dve", CHt[:, 1536:1792], kk, -1.0, ALU.mult)
        cp(P, "act", CHt[:, 1792:2048], r_)
        cp(P, "dve", CHt[:, 2048:2304], v_)
        ts(P, "dve", CHt[:, 2304:2560], gl, 1.0 / 16.0, ALU.mult)
        ts(P, "dve", CHt[:, 2560:2688], u[:, O1:O1 + 128], 32.0 ** -0.5, ALU.mult)
        cp(P, "act", CHt[:, 2688:2816], u[:, O1 + 128:O1 + 256])
        cp(P, "dve", CHt[:, 2816:3072], u[:, O1 + 256:O1 + 512])
        dma(P, "sp", V(C.CH_ap[t * 128:(t + 1) * 128, :], C.CH_bufs[t]), CHt)
        if not C.dbg.get("old_gla"):
            dma(P, "pool", V(C.FIN_ap[t * 128:(t + 1) * 128, :], C.FIN_bufs[t]), F)
            continue
        for d, R in ((0, R0), (1, R1)):
            e1 = "dve" if d == 0 else "pool"
            e2 = "pool" if d == 0 else "dve"
            src = wdec[:, d * 256:(d + 1) * 256].rr("p (a h k) -> p a h k", a=2, h=2)
            hi = rv(R, 0, 128).rr("p h (a k) -> p a h k", a=2)
            lo = rv(R, 192, 128).rr("p h (a k) -> p a h k", a=2)
            cp(P, e1, hi, src)
            tt(P, e1, lo, src, hi, ALU.subtract)
            gsrc = gdec[:, d * 128:(d + 1) * 128].rr("p (a h k) -> p a h k", a=2, h=2)
            ghi = rv(R, 128, 64).rr("p h (a k) -> p a h k", a=2)
            glo = rv(R, 320, 64).rr("p h (a k) -> p a h k", a=2)
            cp(P, e2, ghi, gsrc)
            tt(P, e2, glo, gsrc, ghi, ALU.subtract)
            cp(P, e1, rv(R, 384, 128).rr("p h (a k) -> p a h k", a=2),
               keff[:, d * 256:(d + 1) * 256].rr("p (a h k) -> p a h k", a=2, h=2))
            cp(P, e2, rv(R, 512, 64).rr("p h (a k) -> p a h k", a=2),
               u[:, O1 + 128:O1 + 256].rr("p (a h k) -> p a h k", a=2, h=2))
            cp(P, e1, rv(R, 576, 128).rr("p h (a k) -> p a h k", a=2), r_.rr("p (a h k) -> p a h k", a=2, h=2))
            ts(P, e2, rv(R, 704, 64).rr("p h (a k) -> p a h k", a=2),
               u[:, O1:O1 + 128].rr("p (a h k) -> p a h k", a=2, h=2), 32.0 ** -0.5, ALU.mult)
            ts(P, e1, rv(R, 768, 128).rr("p h (a k) -> p a h k", a=2), kk.rr("p (a h k) -> p a h k", a=2, h=2),
               -1.0, ALU.mult)
            cp(P, e2, rv(R, 896, 128).rr("p h (a k) -> p a h k", a=2),
               bb[:, d * 256:(d + 1) * 256].rr("p (a h k) -> p a h k", a=2, h=2))
            dma(P, "sp" if d == 0 else "pool",
                V(C.STR_ap[d][t * 128:(t + 1) * 128].rearrange("t h n -> t (h n)"), C.STR_bufs[d][t]),
                R.rr("p h n -> p (h n)"))
        pv4 = C.ps[6]
        for a in range(2):
            tr(P, pv4[:, a * 128:(a + 1) * 128], v_[:, a * 128:(a + 1) * 128], C.ident)
        for a in range(2):
            tr(P, pv4[:, (2 + a) * 128:(3 + a) * 128], u[:, O1 + 256 + a * 128:O1 + 384 + a * 128], C.ident)
        VT = vt[i]
        cp(P, "act", VT[:, :, 0], pv4[:, 0:128])
        cp(P, "dve", VT[:, :, 1], pv4[:, 0:128])
        cp(P, "act", VT[:, :, 2], pv4[:, 128:256])
        cp(P, "dve", VT[:, :, 3], pv4[:, 128:256])
        cp(P, "act", VT[:, :, 4], pv4[:, 256:384])
        cp(P, "dve", VT[:, :, 5], pv4[:, 384:512])
        dma(P, "sp", V(C.Vs_ap[:, t * 128:(t + 1) * 128, :], C.Vs_bufs[t]), VT)
        dma(P, "pool", V(C.FIN_ap[t * 128:(t + 1) * 128, :], C.FIN_bufs[t]), F)
    A.close()


def phase_scan(C, l, nchunks=None):
    P, I = C.P, C.I
    A = Arena(P)
    S = A.sb("S", [128, 2, 64])
    T3 = A.sb("T3", [128, 2, 64])
    T4 = A.sb("T4", [128, 2, 64])
    self_f = A.sb("sel_f", [128, 64, 128])
    sel = A.sb("sel", [128, 64, 128], BF16)
    dma(P, "sp", self_f, I["sel"])
    cp(P, "dve", sel, self_f)
    rows = [[A.sb("srow%d%d" % (d, i), [128, 1024], BF16) for i in range(2)] for d in range(2)]
    vb = [A.sb("vb%d" % i, [128, 2, 64, 6]) for i in range(2)]
    yb = [A.sb("yb%d" % i, [128, 2, 64, 6]) for i in range(2)]
    P.op("dve", "memset", ap=S, constant=0.0)
    for i in range(2):
        P.op("pool", "memset", ap=yb[i], constant=0.0)
    NCH = NTOK // 64
    for c in range(NCH if nchunks is None else nchunks):
        zf = c * 64
        zb = (192 - 64 * c) if c < 4 else (4544 - 64 * c)
        i = c % 2
        dma(P, "sp", rows[0][i], V(C.STR_ap[0][zf:zf + 64].rearrange("t h n -> (t h) n"), C.STR_bufs[0][zf // 128]))
        dma(P, "pool", rows[1][i], V(C.STR_ap[1][zb:zb + 64].rearrange("t h n -> (t h) n"), C.STR_bufs[1][zb // 128]))
        dma(P, "sp", vb[i][:, 0], V(C.Vs_ap[:, zf:zf + 64, :], C.Vs_bufs[zf // 128]))
        dma(P, "pool", vb[i][:, 1], V(C.Vs_ap[:, zb:zb + 64, :], C.Vs_bufs[zb // 128]))
        YB = yb[i]
        for j in range(64):
            s = c * 64 + j
            pb = (s % 2) * 2
            for d in range(2):
                jj = j if d == 0 else 63 - j
                lt = sel[:, jj, :]
                R = rows[d][i]
                bx = C.ps[pb + d]
                mm(P, bx[:, 0:64], lt, R[:, 128:192], True, False, mark=False)
                mm(P, bx[:, 0:64], lt, R[:, 320:384], False, True, mark=False)
                mm(P, bx[:, 64:128], lt, R[:, 512:576], True, True, mark=False)
                mm(P, bx[:, 128:192], lt, R[:, 704:768], True, True, mark=(d == 1))
            R4 = V(C.psall[:, pb * 512:pb * 512 + 1024].rearrange("p (d x) -> p d x", d=2), tuple(C.psb[pb:pb + 2]))
            Dv, KKv, RQv = R4[:, :, 0:64], R4[:, :, 64:128], R4[:, :, 128:192]
            P.op("dve", "tensor_tensor", lax=True, out=S, in0=S, in1=Dv, op=ALU.mult)
            vv = cust(vb[i], j * 6 + 4, [((127 - 2 * j) * 6, 2), (1, 2), (0, 32)])
            P.op("dve", "tensor_tensor", lax=True, out=T3.rr("p d (g k) -> p d g k", g=2),
                 in0=KKv.rr("p d (g k) -> p d g k", g=2), in1=vv, op=ALU.mult)
            P.op("dve", "tensor_tensor", lax=True, out=S, in0=S, in1=T3, op=ALU.add)
            P.op("dve", "tensor_tensor", lax=True, out=T4, in0=S, in1=RQv, op=ALU.mult)
            yv = cust(YB, j * 6 + 4, [((127 - 2 * j) * 6, 2), (1, 2)])
            P.op("dve", "tensor_reduce", lax=True, out=yv, in_=T4.rr("p d (g k) -> p d g k", g=2), axis=AX.X, op=ALU.add)
        dma(P, "sp", V(C.Y_ap[0][:, zf:zf + 64, :], C.Y_bufs[0][zf // 64]), YB[:, 0])
        dma(P, "pool", V(C.Y_ap[1][:, zb:zb + 64, :], C.Y_bufs[1][zb // 64]), YB[:, 1])
    A.close()


def phase_fin_ab(C, l, L):
    P, I = C.P, C.I
    A = Arena(P)
    pv = A.sb("pv", [128, NPV])
    dma(P, "sp", pv, I["pv"][l:l + 1, :].bc([128, NPV]))
    lng = pv[:, PV_LNG:PV_LNG + 256]
    yt = [A.sb("yt%d" % i, [128, 2, 128, 6]) for i in range(2)]
    fin = [A.sb("finf%d" % i, [128, 768]) for i in range(2)]
    ya = A.sb("ya", [128, 256])
    ytm = [A.sb("ytm%d" % i, [128, 2, 256]) for i in range(2)]
    ytg = [A.sb("ytg%d" % i, [128, 2, 256]) for i in range(2)]
    yg = A.sb("yg", [128, 256])
    sq = A.sb("sqf", [128, 256])
    st = A.sb("stf", [128, 16])
    ob = [A.sb("ob%d" % i, [128, 4, 128], BF16) for i in range(2)]
    for t in range(NT):
        i = t % 2
        Y = yt[i]
        F = fin[i]
        if C.dbg.get("old_gla"):
            for d in range(2):
                dma(P, "sp" if d == 0 else "pool", Y[:, d],
                    V(C.Y_ap[d][:, t * 128:(t + 1) * 128, :], (C.Y_bufs[d][2 * t], C.Y_bufs[d][2 * t + 1])))
        dma(P, "sp", F, V(C.FIN_ap[t * 128:(t + 1) * 128, :], C.FIN_bufs[t]))
        pr, pg = C.ps[0], C.ps[1]
        if C.dbg.get("old_rwkv"):
            for a in range(2):
                n = 0
                for d in range(2):
                    for g in (2 * a, 2 * a + 1):
                        mm(P, pr[:, a * 128:(a + 1) * 128], Y[:, d, :, g], C.ident, n == 0, n == 3)
                        n += 1
        if C.dbg.get("old_gla"):
            for a in range(2):
                for d in range(2):
                    mm(P, pg[:, a * 128:(a + 1) * 128], Y[:, d, :, 4 + a], C.ident, d == 0, d == 1)
        if C.dbg.get("old_rwkv"):
            cp(P, "act", ya, pr[:, 0:256])
        else:
            for d in range(2):
                dma(P, "sp" if d == 0 else "pool", ytm[i][:, d, :], V(C.YT_ap[d][t * 128:(t + 1) * 128, :], C.YT_bufs[d][t]))
            tt(P, "dve", ya, ytm[i][:, 0, :], ytm[i][:, 1, :], ALU.add)
        ya4 = ya.rr("p (h k) -> p h k", h=4)
        red(P, "dve", st[:, 0:4], ya4)
        ts(P, "dve", st[:, 0:4], st[:, 0:4], 1.0 / 64.0, ALU.mult)
        tt(P, "dve", ya4, ya4, st[:, 0:4][:, :, None].bc([128, 4, 64]), ALU.subtract)
        tt(P, "dve", sq, ya, ya, ALU.mult)
        red(P, "dve", st[:, 4:8], sq.rr("p (h k) -> p h k", h=4))
        ts(P, "dve", st[:, 4:8], st[:, 4:8], 1.0 / 64.0, ALU.mult, GN_EPS, ALU.add)
        act(P, st[:, 4:8], st[:, 4:8], AF.Sqrt)
        P.op("dve", "reciprocal", out=st[:, 4:8], in_=st[:, 4:8])
        tt(P, "dve", ya4, ya4, st[:, 4:8][:, :, None].bc([128, 4, 64]), ALU.mult)
        tt(P, "dve", ya, ya, lng, ALU.mult)
        tt(P, "dve", ya, ya, F[:, 256:512], ALU.add)
        tt(P, "dve", ya, ya, F[:, 0:256], ALU.mult)
        if C.dbg.get("old_gla"):
            cp(P, "act", yg, pg[:, 0:256])
        else:
            for d in range(2):
                dma(P, "sp" if d == 0 else "pool", ytg[i][:, d, :], V(C.YTG_ap[d][t * 128:(t + 1) * 128, :], C.YTG_bufs[d][t]))
            tt(P, "dve", yg, ytg[i][:, 0, :], ytg[i][:, 1, :], ALU.add)
        yg4 = yg.rr("p (h k) -> p h k", h=4)
        tt(P, "dve", sq, yg, yg, ALU.mult)
        red(P, "dve", st[:, 8:12], sq.rr("p (h k) -> p h k", h=4))
        ts(P, "dve", st[:, 8:12], st[:, 8:12], 1.0 / 64.0, ALU.mult, EPS, ALU.add)
        act(P, st[:, 8:12], st[:, 8:12], AF.Sqrt)
        P.op("dve", "reciprocal", out=st[:, 8:12], in_=st[:, 8:12])
        tt(P, "dve", yg4, yg4, st[:, 8:12][:, :, None].bc([128, 4, 64]), ALU.mult)
        tt(P, "dve", yg, yg, F[:, 512:768], ALU.mult)
        po = C.ps[2]
        for a in range(2):
            tr(P, po[:, a * 128:(a + 1) * 128], ya[:, a * 128:(a + 1) * 128], C.ident)
            tr(P, po[:, (2 + a) * 128:(3 + a) * 128], yg[:, a * 128:(a + 1) * 128], C.ident)
        OB = ob[i]
        cp(P, "act", OB, po.rr("p (a n) -> p a n", a=4))
        for br in range(2):
            dma(P, "sp" if br == 0 else "pool",
                V(C.YB_ap[br][:, t * 128:(t + 1) * 128].rearrange("(a p) n -> p a n", p=128), C.YB_bufs[br][t]),
                OB[:, 2 * br:2 * br + 2, :])
    A.close()


def phase_attn(C, l, L):
    P, I = C.P, C.I
    A = Arena(P)
    qT = A.sb("qT", [128, 2, NTOK], BF16)
    kT = A.sb("kT", [128, 2, NTOK], BF16)
    vtm = A.sb("vtm", [128, NT, 128], BF16)
    maskw = A.sb("maskw", [128, 384])
    sink = A.sb("sink", [128, 16])
    identb = A.sb("identb", [128, 128], BF16)
    dma(P, "sp", maskw, I["maskw"])
    dma(P, "sp", sink, I["attn_sink"][l:l + 1, :].bc([128, 16]))
    cp(P, "dve", identb, C.ident)
    ua = [A.sb("ua%d" % i, [128, 512]) for i in range(2)]
    rc = [A.sb("rc%d" % i, [128, 32]) for i in range(2)]
    rs = [A.sb("rs%d" % i, [128, 32]) for i in range(2)]
    qk = A.sb("qk", [128, 6, 64])
    tmp = A.sb("tmpr", [128, 6, 32])
    kd = A.sb("kd", [128, 2, 2, 64])
    cut = C.dbg.get("attn_cut", 9)
    for t in range(NT if cut > 1 else 0):
        i = t % 2
        r0 = urow(t)
        u = ua[i]
        dma(P, "sp", u, V(C.U_ap[r0:r0 + 128, O2:O3], C.U_bufs[t]))
        u6 = u[:, 0:384].rr("p (h d) -> p h d", h=6)
        if t >= 2 and not C.dbg.get("norope"):
            dma(P, "pool", rc[i], I["ropec"][(t - 2) * 128:(t - 1) * 128, :])
            dma(P, "pool", rs[i], I["ropes"][(t - 2) * 128:(t - 1) * 128, :])
            cb = rc[i][:, None, :].bc([128, 6, 32])
            sb_ = rs[i][:, None, :].bc([128, 6, 32])
            z1, z2 = u6[:, :, 0:32], u6[:, :, 32:64]
            tt(P, "dve", qk[:, :, 0:32], z1, cb, ALU.mult)
            tt(P, "dve", tmp, z2, sb_, ALU.mult)
            tt(P, "dve", qk[:, :, 0:32], qk[:, :, 0:32], tmp, ALU.subtract)
            tt(P, "dve", qk[:, :, 32:64], z1, sb_, ALU.mult)
            tt(P, "dve", tmp, z2, cb, ALU.mult)
            tt(P, "dve", qk[:, :, 32:64], qk[:, :, 32:64], tmp, ALU.add)
        else:
            cp(P, "dve", qk, u6)
        if cut < 3:
            continue
        cp(P, "dve", kd[:, :, 0, :], qk[:, 4:6, :])
        cp(P, "dve", kd[:, :, 1, :], qk[:, 4:6, :])
        cp(P, "dve", vtm[:, t, :], u[:, 384:512])
        if cut < 4:
            continue
        pq = C.ps[t % 2]
        qf = qk.rr("p h d -> p (h d)")
        kf = kd.rr("p k r d -> p (k r d)")
        for a in range(2):
            tr(P, pq[:, a * 128:(a + 1) * 128], qf[:, a * 128:(a + 1) * 128], C.ident)
            tr(P, pq[:, (2 + a) * 128:(3 + a) * 128], kf[:, a * 128:(a + 1) * 128], C.ident)
        ts(P, "dve", qT[:, :, t * 128:(t + 1) * 128], pq[:, 0:256].rr("p (a n) -> p a n", a=2), 0.125, ALU.mult)
        cp(P, "dve", kT[:, :, t * 128:(t + 1) * 128], pq[:, 256:512].rr("p (a n) -> p a n", a=2))
    if C.dbg.get("attn_p1"):
        A.close()
        return
    sc = [A.sb("sc%d" % i, [128, 640]) for i in range(2)]
    pb = [A.sb("pb%d" % i, [128, 640], BF16) for i in range(2)]
    pTs = [A.sb("pTs%d" % i, [128, 5, 128], BF16) for i in range(2)]
    st = [A.sb("sta%d" % i, [128, 8]) for i in range(2)]
    yo = [A.sb("yo%d" % i, [128, 256]) for i in range(2)]
    oc = [A.sb("oc%d" % i, [128, 2, 128], BF16) for i in range(2)]
    psT = [V(C.psall[:, b * 512:(b + 1) * 512].bitcast(BF16), C.psb[b]) for b in (4, 5)]
    n = 0
    for t in range(NT):
        YO = yo[t % 2]
        if t >= 2:
            lo, hi = max(t - 1, 2), min(t + 1, NT - 1)
            nw = hi - lo + 1
            m0 = (lo - (t - 1)) * 128
        else:
            nw = 0
        nk = nw * 128 + 256
        nblk = nw + 2
        kblocks = ([lo + b for b in range(nw)] if nw else []) + [0, 1]
        po = C.ps[6 + (t % 2)]
        for h in range(4):
            kv, hl = h // 2, h % 2
            i = n % 2
            n += 1
            S_, Pb, PT, ST = sc[i], pb[i], pTs[i], st[i]
            qv = qT[hl * 64:(hl + 1) * 64, kv, t * 128:(t + 1) * 128]
            pa_, pc_ = C.ps[2 * i], C.ps[2 * i + 1]
            if nw:
                mm(P, pa_[:, 0:nw * 128], qv, kT[hl * 64:(hl + 1) * 64, kv, lo * 128:(hi + 1) * 128], True, True)
            mm(P, pc_[:, 0:256], qv, kT[hl * 64:(hl + 1) * 64, kv, 0:256], True, True)
            if nw:
                tt(P, "dve", S_[:, 0:nw * 128], pa_[:, 0:nw * 128], maskw[:, m0:m0 + nw * 128], ALU.add)
            cp(P, "act", S_[:, nw * 128:nk], pc_[:, 0:256])
            P.op("dve", "tensor_reduce", out=ST[:, 0:1], in_=S_[:, 0:nk], axis=AX.X, op=ALU.max)
            tt(P, "dve", ST[:, 0:1], ST[:, 0:1], sink[:, h:h + 1], ALU.max)
            ts(P, "dve", ST[:, 1:2], ST[:, 0:1], -1.0, ALU.mult)
            act(P, Pb[:, 0:nk], S_[:, 0:nk], AF.Exp, bias=ST[:, 1:2], accum_out=ST[:, 2:3])
            act(P, ST[:, 3:4], sink[:, h:h + 1], AF.Exp, bias=ST[:, 1:2])
            tt(P, "dve", ST[:, 4:5], ST[:, 2:3], ST[:, 3:4], ALU.add)
            P.op("dve", "reciprocal", out=ST[:, 5:6], in_=ST[:, 4:5])
            pt = psT[i]
            for b in range(nblk):
                tr(P, pt[:, b * 128:(b + 1) * 128], Pb[:, b * 128:(b + 1) * 128], identb)
            cp(P, "act" if h % 2 == 0 else "dve", PT[:, 0:nblk, :], pt[:, 0:nblk * 128].rr("p (b n) -> p b n", b=nblk))
            for b in range(nblk):
                mm(P, po[:, h * 64:(h + 1) * 64], PT[:, b, :], vtm[:, kblocks[b], kv * 64:(kv + 1) * 64],
                   b == 0, b == nblk - 1)
            ts(P, "dve", YO[:, h * 64:(h + 1) * 64], po[:, h * 64:(h + 1) * 64], ST[:, 5:6], ALU.mult)
        pf = C.ps[t % 2]
        for a in range(2):
            tr(P, pf[:, a * 128:(a + 1) * 128], YO[:, a * 128:(a + 1) * 128], C.ident)
        OC = oc[t % 2]
        cp(P, "act", OC, pf[:, 0:256].rr("p (a n) -> p a n", a=2))
        dma(P, "sp", V(C.YB_ap[2][:, t * 128:(t + 1) * 128].rearrange("(a p) n -> p a n", p=128), C.YB_bufs[2][t]), OC)
    A.close()


PI = math.pi
S5_BLOCKS = [(0, 256)] + [(256 + 512 * i, 512) for i in range(8)]


I32 = mybir.dt.int32
TWO_PI_HI = 6.28125
TWO_PI_LO = 2.0 * math.pi - 6.28125


def sincos(P, A, s_out, c_out, x, shape, tag):
    qi = A.sb("qi" + tag, shape, I32)
    kf = A.sb("kf" + tag, shape)
    r = A.sb("rr" + tag, shape)
    m = A.sb("mm" + tag, shape)
    for extra, out in ((0.0, s_out), (0.5 * PI, c_out)):
        ts(P, "dve", r, x, 16.0 * PI + extra, ALU.add)
        ts(P, "dve", qi, r, 1.0 / (2.0 * PI), ALU.mult)
        cp(P, "dve", kf, qi)
        stt(P, "dve", r, kf, -TWO_PI_HI, r, ALU.mult, ALU.add)
        stt(P, "dve", r, kf, -TWO_PI_LO, r, ALU.mult, ALU.add)
        ts(P, "dve", m, r, PI, ALU.is_gt)
        stt(P, "dve", r, m, -2.0 * PI, r, ALU.mult, ALU.add)
        ts(P, "dve", m, r, -PI, ALU.is_lt)
        stt(P, "dve", r, m, 2.0 * PI, r, ALU.mult, ALU.add)
        ts(P, "dve", r, r, PI, ALU.min, -PI, ALU.max)
        act(P, out, r, AF.Sin)


def phase_s5(C, l, L):
    P, I = C.P, C.I
    A = Arena(P)
    th_s = A.sb("th_s", [128, 2, 8])
    rho_s = A.sb("rho_s", [128, 2, 8])
    Ck = A.sb("Ck", [128, 2, 8, 13])
    Sk = A.sb("Sk", [128, 2, 8, 13])
    BbT = A.sb("BbT", [128, 2, 2, 8, 128], BF16)
    CT = A.sb("CT", [128, 2, 2, 8, 128], BF16)
    A0 = A
    A = Arena(P)
    tmpA = [A.sb("s5r%d" % i, [128, 1024]) for i in range(8)]
    lre, lim, ldt, t_s, t_c, t_a, t_b, t_d = tmpA
    pw2f = A.sb("pw2", [128, 16])
    dma(P, "sp", pw2f, I["pw2"][0:1, :].bc([128, 16]))
    pw2 = pw2f[:, 0:13]
    fR = [A.sb("fR%d" % d, [128, 1024]) for d in range(2)]
    fI = [A.sb("fI%d" % d, [128, 1024]) for d in range(2)]
    sm = A.sb("sm", [128, 2, 3, 8])
    dma(P, "sp", sm, I["s5_sm"][l])
    act(P, sm[:, :, 2, :], sm[:, :, 2, :], AF.Exp)
    tt(P, "dve", th_s, sm[:, :, 1, :], sm[:, :, 2, :], ALU.mult)
    tt(P, "dve", rho_s, sm[:, :, 0, :], sm[:, :, 2, :], ALU.mult)
    act(P, rho_s, rho_s, AF.Exp)
    ang13 = A.sb("ang13", [128, 16, 13])
    tt(P, "dve", ang13, th_s.rr("p d j -> p (d j)")[:, :, None].bc([128, 16, 13]), pw2[:, None, :].bc([128, 16, 13]), ALU.mult)
    sincos(P, A, Sk.rr("p d j k -> p (d j) k"), Ck.rr("p d j k -> p (d j) k"), ang13, [128, 16, 13], "k")
    for d in range(2):
        dma(P, "sp", lre, I["s5_rows"][l, d, 0:1, :].bc([128, 1024]))
        dma(P, "pool", lim, I["s5_rows"][l, d, 1:2, :].bc([128, 1024]))
        dma(P, "sp", ldt, I["s5_rows"][l, d, 2:3, :].bc([128, 1024]))
        act(P, ldt, ldt, AF.Exp)
        tt(P, "dve", t_a, lim, ldt, ALU.mult)
        sincos(P, A, t_s, t_c, t_a, [128, 1024], "r%d" % d)
        tt(P, "dve", t_a, lre, ldt, ALU.mult)
        act(P, t_a, t_a, AF.Exp)
        tt(P, "dve", t_c, t_c, t_a, ALU.mult)
        tt(P, "dve", t_s, t_s, t_a, ALU.mult)
        ts(P, "dve", t_c, t_c, -1.0, ALU.add)
        tt(P, "dve", t_a, lre, lre, ALU.mult)
        tt(P, "dve", t_b, lim, lim, ALU.mult)
        tt(P, "dve", t_a, t_a, t_b, ALU.add)
        P.op("dve", "reciprocal", out=t_a, in_=t_a)
        tt(P, "dve", t_b, t_c, lre, ALU.mult)
        tt(P, "dve", t_d, t_s, lim, ALU.mult)
        tt(P, "dve", t_b, t_b, t_d, ALU.add)
        tt(P, "dve", fR[d], t_b, t_a, ALU.mult)
        tt(P, "dve", t_b, t_s, lre, ALU.mult)
        tt(P, "dve", t_d, t_c, lim, ALU.mult)
        tt(P, "dve", t_b, t_b, t_d, ALU.subtract)
        tt(P, "dve", fI[d], t_b, t_a, ALU.mult)
    bt_f = A.sb("bt_f", [128, 2, 8, 128])
    dma(P, "sp", bt_f[:, 0], I["s5_bt"][l, 0].rr("j c s -> c j s"))
    dma(P, "pool", bt_f[:, 1], I["s5_bt"][l, 1].rr("j c s -> c j s"))
    for d in range(2):
        fr = fR[d].rr("p (j s) -> p j s", j=8)
        fi = fI[d].rr("p (j s) -> p j s", j=8)
        ta = t_a.rr("p (j s) -> p j s", j=8)
        tb = t_b.rr("p (j s) -> p j s", j=8)
        tt(P, "dve", ta, bt_f[:, 0], fr, ALU.mult)
        tt(P, "dve", tb, bt_f[:, 1], fi, ALU.mult)
        tt(P, "dve", BbT[:, d, 0], ta, tb, ALU.subtract)
        tt(P, "dve", ta, bt_f[:, 0], fi, ALU.mult)
        tt(P, "dve", tb, bt_f[:, 1], fr, ALU.mult)
        tt(P, "dve", BbT[:, d, 1], ta, tb, ALU.add)
        for ri in range(2):
            ctf = t_c if ri == 0 else t_d
            dma(P, "sp" if ri == 0 else "pool", ctf.rr("p (j c) -> p j c", j=8), I["s5_ct"][l, d, ri].rr("j s c -> s j c"))
            if ri == 0:
                cp(P, "dve", CT[:, d, 0], ctf.rr("p (j c) -> p j c", j=8))
            else:
                ts(P, "dve", CT[:, d, 1], ctf.rr("p (j c) -> p j c", j=8), -1.0, ALU.mult)
    A.close()
    A = A0
    cut = C.dbg.get("s5_cut", 99)
    if cut <= 1:
        A.close(); return
    if not C.dbg.get("s5_small"):
        ct, sn = A.sb("ct", [128, NTOK]), A.sb("sn", [128, NTOK])
        w_re, w_im = A.sb("w_re", [128, NTOK]), A.sb("w_im", [128, NTOK])
    uB = A.sb("uB", [128, 2, NTOK], BF16)
    yacc = A.sb("yacc", [128, 2, NTOK])
    x_re, x_im = A.sb("x_re", [128, NTOK], BF16), A.sb("x_im", [128, NTOK], BF16)
    dsk = A.sb("dsk", [128, 16])
    bgl = A.sb("bgl", [128, 16])
    dma(P, "sp", dsk, I["s5_d_fm"][l])
    dma(P, "sp", bgl, I["s5_bglu_fm"][l])
    wglu = load_bf16(P, A, "wglu", [128, 2, 256], I["s5_w_glu"][l].rr("(k p) n -> p k n", p=128))
    tmA = [[A.sb("tm%d_%d" % (b, i), [128, 512]) for i in range(4)] for b in range(2)]
    ut = [tmA[1][i][:, 0:256] for i in range(2)]
    var = C.dbg.get("s5_var", 9)
    for t in range(NT if var > 0 else 0):
        i = t % 2
        r0 = urow(t)
        dma(P, "sp" if i == 0 else "pool", ut[i], V(C.U_ap[r0:r0 + 128, O3:O4], C.U_bufs[t]))
        pp = C.ps[i]
        for a in range(2):
            tr(P, pp[:, a * 128:(a + 1) * 128], ut[i][:, a * 128:(a + 1) * 128], C.ident)
        if var < 2:
            continue
        for a in range(2):
            cp(P, "dve", uB[:, a, t * 128:(t + 1) * 128], pp[:, a * 128:(a + 1) * 128])
        for a in range(2):
            ts(P, "dve", yacc[:, a, t * 128:(t + 1) * 128], pp[:, a * 128:(a + 1) * 128], dsk[:, a:a + 1], ALU.mult)
    tm = tmA[0]
    if cut <= 2:
        A.close(); return
    nb = 0
    for d in range(2):
        for j in range(8):
            jt = j // 4
            th = th_s[:, d, j:j + 1]
            P.op("dve", "memset", ap=ct[:, 0:1], constant=1.0)
            P.op("dve", "memset", ap=sn[:, 0:1], constant=0.0)
            k = 0
            n = 1
            while n < NTOK:
                m = min(n, NTOK - n)
                ck, sk = Ck[:, d, j, k:k + 1], Sk[:, d, j, k:k + 1]
                e1, e2 = ("dve", "dve")
                ts(P, e1, ct[:, n:n + m], ct[:, 0:m], ck, ALU.mult)
                ts(P, e2, sn[:, n:n + m], sn[:, 0:m], ck, ALU.mult)
                ts(P, e2, tm[0][:, 0:min(m, 512)] if m <= 512 else w_re[:, 0:m], sn[:, 0:m], sk, ALU.mult)
                ts(P, e1, tm[1][:, 0:min(m, 512)] if m <= 512 else w_im[:, 0:m], ct[:, 0:m], sk, ALU.mult)
                ta_ = tm[0][:, 0:m] if m <= 512 else w_re[:, 0:m]
                tb_ = tm[1][:, 0:m] if m <= 512 else w_im[:, 0:m]
                tt(P, e1, ct[:, n:n + m], ct[:, n:n + m], ta_, ALU.subtract)
                tt(P, e2, sn[:, n:n + m], sn[:, n:n + m], tb_, ALU.add)
                n += m
                k += 1
            if cut <= 3:
                A.close(); return
            for bi, (t0, n) in enumerate(S5_BLOCKS):
                if d == 0:
                    rhs = uB[:, jt, t0:t0 + n]
                else:
                    last = (255 - t0) if t0 < 256 else (4607 - t0)
                    rhs = cust(uB, jt * NTOK + last, [(-1, n)])
                pr, pi_ = C.ps[(nb % 2) * 2], C.ps[(nb % 2) * 2 + 1]
                nb += 1
                mm(P, pr[:, 0:n], BbT[:, d, 0, j, :], rhs, True, True)
                mm(P, pi_[:, 0:n], BbT[:, d, 1, j, :], rhs, True, True)
                c_, s_ = ct[:, t0:t0 + n], sn[:, t0:t0 + n]
                tm = tmA[bi % 2]
                tt(P, "dve", tm[0][:, 0:n], pr[:, 0:n], c_, ALU.mult)
                tt(P, "dve", tm[1][:, 0:n], pi_[:, 0:n], s_, ALU.mult)
                tt(P, "dve", w_re[:, t0:t0 + n], tm[0][:, 0:n], tm[1][:, 0:n], ALU.add)
                tt(P, "dve", tm[2][:, 0:n], pi_[:, 0:n], c_, ALU.mult)
                tt(P, "dve", tm[3][:, 0:n], pr[:, 0:n], s_, ALU.mult)
                tt(P, "dve", w_im[:, t0:t0 + n], tm[2][:, 0:n], tm[3][:, 0:n], ALU.subtract)
            if cut <= 4:
                A.close(); return
            rb = rho_s[:, d, j:j + 1].bc([128, NTOK])
            P.op("dve", "tensor_tensor_scan", out=w_re, data0=rb, data1=w_re, initial=0.0, op0=ALU.mult, op1=ALU.add)
            P.op("dve", "tensor_tensor_scan", out=w_im, data0=rb, data1=w_im, initial=0.0, op0=ALU.mult, op1=ALU.add)
            if cut <= 5:
                A.close(); return
            for bi, (t0, n) in enumerate(S5_BLOCKS):
                c_, s_ = ct[:, t0:t0 + n], sn[:, t0:t0 + n]
                tm = tmA[bi % 2]
                tt(P, "dve", tm[0][:, 0:n], w_re[:, t0:t0 + n], c_, ALU.mult)
                tt(P, "dve", tm[1][:, 0:n], w_im[:, t0:t0 + n], s_, ALU.mult)
                tt(P, "dve", x_re[:, t0:t0 + n], tm[0][:, 0:n], tm[1][:, 0:n], ALU.subtract)
                tt(P, "dve", tm[2][:, 0:n], w_re[:, t0:t0 + n], s_, ALU.mult)
                tt(P, "dve", tm[3][:, 0:n], w_im[:, t0:t0 + n], c_, ALU.mult)
                tt(P, "dve", x_im[:, t0:t0 + n], tm[2][:, 0:n], tm[3][:, 0:n], ALU.add)
                py = C.ps[4 + (bi % 2)]
                if d == 0:
                    xr, xi = x_re[:, t0:t0 + n], x_im[:, t0:t0 + n]
                    k0 = t0
                else:
                    k0 = (256 - t0 - n) if t0 < 256 else (4608 - t0 - n)
                    s_last = t0 + n - 1
                    xr, xi = cust(x_re, s_last, [(-1, n)]), cust(x_im, s_last, [(-1, n)])
                mm(P, py[:, 0:n], CT[:, d, 0, j, :], xr, True, False)
                mm(P, py[:, 0:n], CT[:, d, 1, j, :], xi, False, True)
                tt(P, "dve", yacc[:, jt, k0:k0 + n], yacc[:, jt, k0:k0 + n], py[:, 0:n], ALU.add)
    if cut <= 7:
        A.close(); return
    glb = uB
    tm = tmA[0]
    ob = [x_re[:, 0:1024].rr("p (a n) -> p a n", a=2), x_im[:, 0:1024].rr("p (a n) -> p a n", a=2)]
    for bi, (t0, n) in enumerate(S5_BLOCKS):
        for a in range(2):
            y = yacc[:, a, t0:t0 + n]
            tt(P, "dve", tm[0][:, 0:n], y, y, ALU.mult)
            ts(P, "dve", tm[0][:, 0:n], tm[0][:, 0:n], 0.044715, ALU.mult, 1.0, ALU.add)
            tt(P, "dve", tm[0][:, 0:n], tm[0][:, 0:n], y, ALU.mult)
            act(P, tm[0][:, 0:n], tm[0][:, 0:n], AF.Sigmoid, scale=1.5957691216057308)
            tt(P, "dve", y, y, tm[0][:, 0:n], ALU.mult)
            cp(P, "dve", glb[:, a, t0:t0 + n], y)
        OB = ob[bi % 2]
        for a in range(2):
            pz = C.ps[6 + a]
            for kt in range(2):
                mm(P, pz[:, 0:n], wglu[:, kt, a * 128:(a + 1) * 128], glb[:, kt, t0:t0 + n], kt == 0, kt == 1)
            act(P, tm[1 + a][:, 0:n], pz[:, 0:n], AF.Sigmoid, bias=bgl[:, a:a + 1])
            tt(P, "dve", OB[:, a, 0:n], yacc[:, a, t0:t0 + n], tm[1 + a][:, 0:n], ALU.mult)
        tl = [t for t in range(NT) if t * 128 >= t0 and t * 128 < t0 + n]
        dma(P, "sp", V(C.YB_ap[3][:, t0:t0 + n].rearrange("(a p) n -> p a n", p=128), tuple(C.YB_bufs[3][t] for t in tl)),
            OB[:, :, 0:n])
    A.close()


TOKBLK = [(0, 256)] + [(256 + 512 * i, 512) for i in range(8)]


def phase_win_gates(C, l, L, hfm):
    P, I = C.P, C.I
    A = Arena(P)
    wst = [A.sb("wgs%d" % i, [128, 8, 512]) for i in range(2)]
    wb = [A.sb("wgb%d" % i, [128, 8, 512], BF16) for i in range(2)]
    gst = [A.sb("gst%d" % i, [128, 512], BF16) for i in range(4)]
    wv = I["w_in"][l].rr("(k p) n -> p k n", p=128)
    n = 0
    for cb in range(8):
        c0 = O4 + cb * 512
        dma(P, "sp" if cb % 2 == 0 else "pool", wst[cb % 2], wv[:, :, c0:c0 + 512])
        cp(P, "act" if cb % 2 == 0 else "dve", wb[cb % 2], wst[cb % 2])
        w = wb[cb % 2]
        for mi in range(4):
            row0 = cb * 512 + mi * 128
            for bi, (t0, nn) in enumerate(TOKBLK):
                ps = C.ps[n % 4]
                g = gst[n % 4]
                for k in range(8):
                    mm(P, ps[:, 0:nn], w[:, k, mi * 128:(mi + 1) * 128], hfm[:, k, t0:t0 + nn], k == 0, k == 7)
                cp(P, "act" if n % 2 == 0 else "dve", g[:, 0:nn], ps[:, 0:nn])
                dma(P, "sp" if n % 2 == 0 else "pool", V(C.Gt_ap[row0:row0 + 128, t0:t0 + nn], C.Gt_bufs[bi]), g[:, 0:nn])
                n += 1
    A.close()


def phase_merge(C, l, L):
    P, I = C.P, C.I
    A = Arena(P)
    wbr = A.sb("wbr", [128, 4, 2, 1024], BF16)
    wout = A.sb("wout", [128, 8, 1024], BF16)
    wst = A.sb("wmst", [128, 8, 1024])
    for i in range(4):
        dma(P, "sp", wst[:, 0:2, :], I["w_branch"][l, i].rr("(k p) n -> p k n", p=128))
        cp(P, "act", wbr[:, i], wst[:, 0:2, :])
    dma(P, "sp", wst, I["w_out"][l].rr("(k p) n -> p k n", p=128))
    cp(P, "act", wout, wst)
    yb = [A.sb("myb%d" % i, [128, 4, 2, 512], BF16) for i in range(2)]
    gt4 = [A.sb("mgt%d" % i, [128, 4, 512], BF16) for i in range(2)]
    sg4 = [A.sb("msg%d" % i, [128, 4, 512]) for i in range(2)]
    tmp = A.sb("mtmp", [128, 512])
    acc = A.sb("macc", [128, 512])
    mg = [A.sb("mmg%d" % i, [128, 8, 512], BF16) for i in range(2)]
    xt = [A.sb("mxt%d" % i, [128, 1024]) for i in range(2)]
    tm2 = A.sb("mtm2", [128, 1024])
    n = 0
    nx = 0
    for bi, (t0, nn) in enumerate(TOKBLK):
        YB = yb[bi % 2]
        tl = [t for t in range(NT) if t0 <= t * 128 < t0 + nn]
        for i in range(4):
            dma(P, "sp" if i % 2 == 0 else "pool", YB[:, i, :, 0:nn],
                V(C.YB_ap[i][:, t0:t0 + nn].rearrange("(a p) n -> p a n", p=128), tuple(C.YB_bufs[i][t] for t in tl)))
        MG = mg[bi % 2]
        for m in range(8):
            g4 = gt4[m % 2]
            s4 = sg4[m % 2]
            dma(P, "sp" if m % 2 == 0 else "pool", g4[:, :, 0:nn],
                V(C.Gt_ap.rearrange("(i r) n -> r i n", i=4)[m * 128:(m + 1) * 128, :, t0:t0 + nn], C.Gt_bufs[bi]))
            act(P, s4[:, :, 0:nn], g4[:, :, 0:nn], AF.Sigmoid)
            for i in range(4):
                s_ = s4[:, i, :]
                ps = C.ps[n % 4]
                n += 1
                for kt in range(2):
                    mm(P, ps[:, 0:nn], wbr[:, i, kt, m * 128:(m + 1) * 128], YB[:, i, kt, 0:nn], kt == 0, kt == 1)
                if i == 0:
                    tt(P, "dve", acc[:, 0:nn], ps[:, 0:nn], s_[:, 0:nn], ALU.mult)
                elif i < 3:
                    tt(P, "dve", tmp[:, 0:nn], ps[:, 0:nn], s_[:, 0:nn], ALU.mult)
                    tt(P, "dve", acc[:, 0:nn], acc[:, 0:nn], tmp[:, 0:nn], ALU.add)
                else:
                    tt(P, "dve", tmp[:, 0:nn], ps[:, 0:nn], s_[:, 0:nn], ALU.mult)
                    tt(P, "dve", MG[:, m, 0:nn], acc[:, 0:nn], tmp[:, 0:nn], ALU.add)
        for ti, t in enumerate(tl):
            j = 1 if t < 2 else 0
            x = xt[nx % 2]
            nx += 1
            dma(P, "sp", x, xsrc(C, l, t))
            for half in range(2):
                po = C.ps[4 + half + 2 * (nx % 2)]
                for k in range(8):
                    mm(P, po, MG[:, k, ti * 128:(ti + 1) * 128], wout[:, k, half * 512:(half + 1) * 512], k == 0, k == 7)
                tt(P, "dve", tm2[:, half * 512:(half + 1) * 512], po, L.grow[0][j][:, half * 512:(half + 1) * 512], ALU.mult)
            tt(P, "dve", x, x, tm2, ALU.add)
            dma(P, "pool", C.xres[t], x)
    A.close()
    C.x_in_scratch = True


def make_router(C, l, L, RA):
    P, I = C.P, C.I
    wr = RA.sb("wr", [128, 8, 36])
    brow = RA.sb("brow", [128, 36])
    dma(P, "sp", wr, I["w_router"][l].rr("(k p) n -> p k n", p=128))
    dma(P, "sp", brow, I["b_router"][l:l + 1, :].bc([128, 36]))
    lg = RA.sb("lg", [128, 36])
    st = RA.sb("rst", [128, 16])
    oh = RA.sb("roh", [128, 4])
    em = RA.sb("rem", [128, 32])
    em2 = RA.sb("rem2", [128, 32])
    oh1 = RA.sb("roh1", [128, 32])
    oh2 = RA.sb("roh2", [128, 32])
    wg = RA.sb("rwg", [128, 32])
    junk = RA.sb("rjunk", [128, 4])

    def per_tile(t, hf):
        pl = C.ps[6]
        for k in range(8):
            mm(P, pl[:, 0:36], hf[:, k, :], wr[:, k, :], k == 0, k == 7)
        tt(P, "dve", lg, pl[:, 0:36], brow, ALU.add)
        g, e = lg[:, 0:4], lg[:, 4:36]
        P.op("dve", "tensor_reduce", out=st[:, 0:1], in_=g, axis=AX.X, op=ALU.max)
        ts(P, "dve", oh, g, st[:, 0:1], ALU.is_equal)
        ts(P, "dve", st[:, 1:2], st[:, 0:1], -1.0, ALU.mult)
        act(P, junk, g, AF.Exp, bias=st[:, 1:2], accum_out=st[:, 2:3])
        P.op("dve", "reciprocal", out=st[:, 3:4], in_=st[:, 2:3])
        ts(P, "dve", oh, oh, 1e30, ALU.mult, -1e30, ALU.add)
        tt(P, "dve", em.rr("p (g k) -> p g k", g=4), e.rr("p (g k) -> p g k", g=4),
           oh[:, :, None].bc([128, 4, 8]), ALU.add)
        P.op("dve", "tensor_reduce", out=st[:, 4:5], in_=em, axis=AX.X, op=ALU.max)
        ts(P, "dve", oh1, em, st[:, 4:5], ALU.is_equal)
        stt(P, "dve", em2, oh1, -1e30, em, ALU.mult, ALU.add)
        P.op("dve", "tensor_reduce", out=st[:, 5:6], in_=em2, axis=AX.X, op=ALU.max)
        ts(P, "dve", oh2, em2, st[:, 5:6], ALU.is_equal)
        tt(P, "dve", st[:, 6:7], st[:, 5:6], st[:, 4:5], ALU.subtract)
        act(P, st[:, 7:8], st[:, 6:7], AF.Exp)
        ts(P, "dve", st[:, 8:9], st[:, 7:8], 1.0, ALU.add)
        P.op("dve", "reciprocal", out=st[:, 9:10], in_=st[:, 8:9])
        tt(P, "dve", st[:, 10:11], st[:, 7:8], st[:, 9:10], ALU.mult)
        ts(P, "dve", wg, oh1, st[:, 9:10], ALU.mult)
        stt(P, "dve", wg, oh2, st[:, 10:11], wg, ALU.mult, ALU.add)
        ts(P, "dve", wg, wg, st[:, 3:4], ALU.mult)
        pt = C.ps[7]
        tr(P, pt[0:32, 0:128], wg, C.ident)
        cp(P, "dve", L.WT[:, t * 128:(t + 1) * 128], pt[0:32, 0:128])
    return per_tile


MOE_GROUPS = [list(range(0, 12)), list(range(12, 24)), list(range(24, 34))]


def phase_moe(C, l, L, H2, last):
    P, I = C.P, C.I
    A = Arena(P)
    hg = A.sb("hg", [128, 8, 12 * 128], BF16)
    wst = [A.sb("ews%d" % i, [128, 4, 512]) for i in range(2)]
    wgu = [A.sb("wgu%d" % i, [128, 2, 8, 512], BF16) for i in range(2)]
    wd = [A.sb("wd%d" % i, [128, 4, 1024], BF16) for i in range(2)]
    yacc = A.sb("eyacc", [128, 12, 1024])
    hid = [A.sb("hid%d" % i, [128, 4, 512], BF16) for i in range(2)]
    sil = [A.sb("sil%d" % i, [128, 512], BF16) for i in range(2)]
    tu = [A.sb("etu%d" % i, [128, 512]) for i in range(2)]
    xt = [A.sb("ext%d" % i, [128, 1024]) for i in range(2)]
    tm2 = A.sb("etm2", [128, 1024])
    ne = 0
    nst = 0
    nb = 0
    for G in MOE_GROUPS:
        tiles = [t for t in G if not (last and t < 2)]
        if not tiles:
            continue
        blocks = [tiles[i:i + 4] for i in range(0, len(tiles), 4)]
        g0 = tiles[0] * 128
        gn = len(tiles) * 128
        dma(P, "sp", hg[:, :, 0:gn], H2[:, :, g0:g0 + gn])
        for e in range(32):
            WGU, WD = wgu[ne % 2], wd[ne % 2]
            ne += 1
            for gi, nm in enumerate(("w_exp_gate", "w_exp_up")):
                src = I[nm][l, e].rr("(k p) n -> p k n", p=128)
                for hf_ in range(2):
                    s_ = wst[nst % 2]
                    dma(P, "sp" if nst % 2 == 0 else "pool", s_, src[:, hf_ * 4:(hf_ + 1) * 4, :])
                    cp(P, "act", WGU[:, gi, hf_ * 4:(hf_ + 1) * 4, :], s_)
                    nst += 1
            srcd = I["w_exp_down"][l, e].rr("(k p) n -> p k n", p=128)
            for hf_ in range(2):
                s_ = wst[nst % 2]
                dma(P, "sp" if nst % 2 == 0 else "pool", s_.rr("p k n -> p (k n)").rr("p (k n) -> p k n", k=2), srcd[:, hf_ * 2:(hf_ + 1) * 2, :])
                cp(P, "dve", WD[:, hf_ * 2:(hf_ + 1) * 2, :], s_.rr("p k n -> p (k n)").rr("p (k n) -> p k n", k=2))
                nst += 1
            for blk in blocks:
                t0 = blk[0] * 128
                nn = len(blk) * 128
                HID = hid[nb % 2]
                psW = C.ps[0]
                mm(P, psW[:, 0:nn], C.ident[0:32, e:e + 1].bc([32, 128]), L.WT[:, t0:t0 + nn], True, True)
                for f in range(4):
                    i2 = (nb * 4 + f) % 2
                    psG, psU = C.ps[1 + 2 * i2], C.ps[2 + 2 * i2]
                    for k in range(8):
                        mm(P, psG[:, 0:nn], WGU[:, 0, k, f * 128:(f + 1) * 128], hg[:, k, t0 - g0:t0 - g0 + nn], k == 0, k == 7)
                    for k in range(8):
                        mm(P, psU[:, 0:nn], WGU[:, 1, k, f * 128:(f + 1) * 128], hg[:, k, t0 - g0:t0 - g0 + nn], k == 0, k == 7)
                    act(P, sil[i2][:, 0:nn], psG[:, 0:nn], AF.Silu)
                    tt(P, "dve", tu[i2][:, 0:nn], psU[:, 0:nn], sil[i2][:, 0:nn], ALU.mult)
                    tt(P, "dve", HID[:, f, 0:nn], tu[i2][:, 0:nn], psW[:, 0:nn], ALU.mult)
                for ti, t in enumerate(blk):
                    ya = yacc[:, t - tiles[0], :]
                    for half in range(2):
                        po = C.ps[5 + (nb * 8 + ti * 2 + half) % 3]
                        for f in range(4):
                            mm(P, po, HID[:, f, ti * 128:(ti + 1) * 128], WD[:, f, half * 512:(half + 1) * 512], f == 0, f == 3)
                        if e == 0:
                            cp(P, "dve", ya[:, half * 512:(half + 1) * 512], po)
                        else:
                            tt(P, "dve", ya[:, half * 512:(half + 1) * 512], ya[:, half * 512:(half + 1) * 512], po, ALU.add)
                nb += 1
        for t in tiles:
            j = 1 if t < 2 else 0
            x = xt[t % 2]
            dma(P, "sp", x, C.xres[t])
            tt(P, "dve", tm2, yacc[:, t - tiles[0], :], L.grow[1][j], ALU.mult)
            tt(P, "dve", x, x, tm2, ALU.add)
            dma(P, "pool", C.xres[t], x)
    A.close()


def final_norm(C):
    P, I = C.P, C.I
    A = Arena(P)
    g = A.sb("fg", [128, D])
    dma(P, "sp", g, I["final_g"][0:1, :].bc([128, D]))
    xt = [A.sb("fxt%d" % i, [128, D]) for i in range(2)]
    junk = A.sb("fjunk", [128, D])
    st = [A.sb("fst%d" % i, [128, 2]) for i in range(2)]
    for t in range(2, NT):
        x, s = xt[t % 2], st[t % 2]
        dma(P, "sp" if t % 2 == 0 else "pool", x, C.xres[t])
        act(P, junk, x, AF.Square, accum_out=s[:, 0:1])
        ts(P, "dve", s[:, 1:2], s[:, 0:1], 1.0 / D, ALU.mult, EPS, ALU.add)
        act(P, s[:, 1:2], s[:, 1:2], AF.Sqrt)
        P.op("dve", "reciprocal", out=s[:, 1:2], in_=s[:, 1:2])
        stt(P, "dve", x, x, s[:, 1:2], g, ALU.mult, ALU.mult)
        dma(P, "sp" if t % 2 == 1 else "pool", V(C.out.ap[(t - 2) * 128:(t - 1) * 128, :], Buf("o%d" % t)), x)
    A.close()


CH_COLS = 3072


def alloc_chunk_heads(A, dd):
    def mkh(name, shape, dt=F32):
        return [A.sb("%s_%d_%d" % (name, dd, h), shape, dt) for h in range(4)]
    AM = mkh("cAM", [128, 4, 128], BF16)
    XB = mkh("cXB", [128, 2, 128], BF16)
    XX = [[AM[h][:, 0:2, :] for h in range(4)], [XB[h] for h in range(4)]]
    AakT = [AM[h][:, 2, :] for h in range(4)]
    ArbT = [AM[h][:, 3, :] for h in range(4)]
    ArkT, TT = [mkh("cM%d" % i, [128, 128], BF16) for i in range(2)]
    PM = mkh("cPM", [128, 2, 64], BF16)
    Ap = [PM[h][:, 0, :] for h in range(4)]
    M1 = [PM[h][:, 1, :] for h in range(4)]
    U0 = mkh("cU0", [128, 64], BF16)
    RpT = mkh("cRpT", [64, 128])
    DPC = mkh("cDPC", [128, 64])
    Y0cc = mkh("cY0cc", [64, 2, 64])
    Y0c = [[Y0cc[h][:, c, :] for h in range(4)] for c in range(2)]
    GH = mkh("cGH", [64, 4, 64])
    GT = [[GH[h][:, 2 * c, :] for h in range(4)] for c in range(2)]
    Hc = [[GH[h][:, 2 * c + 1, :] for h in range(4)] for c in range(2)]
    gA = mkh("gA", [128, 128])
    gY0 = [mkh("gY0%d" % c, [64, 64]) for c in range(2)]
    gH = [mkh("gH%d" % c, [32, 64]) for c in range(2)]
    return (AM, XB, XX, AakT, ArbT, ArkT, TT, PM, Ap, M1, U0, RpT, DPC, Y0cc, Y0c, GH, GT, Hc, gA, gY0, gH)


def phase_chunk(C, l, L):
    P, I = C.P, C.I
    A = Arena(P)
    mk = A.sb("cmasks", [128, 7, 128])
    dma(P, "sp", mk, I["cmasks"])
    idn = C.ident
    chs = [A.sb("chs%d" % i, [128, CH_COLS]) for i in range(2)]
    ST = [[A.sb("cST%d%d" % (d, h), [64, 64]) for h in range(4)] for d in range(2)]
    for d in range(2):
        for h in range(4):
            P.op("dve", "memset", ap=ST[d][h], constant=0.0)

    def mk2(name, shape, dt=F32):
        return [A.sb("%s%d" % (name, i), shape, dt) for i in range(2)]
    TOT, incS, Ein, Enin, Eex, Eend, Etot, tmpx, tmpy = [mk2("cE%d" % i, [128, 256]) for i in range(9)]
    at, rt, bt, kt, bh, kh = [mk2("cq%d" % i, [128, 256]) for i in range(6)]
    aT, rTb, bT, kT = [mk2("cT%d" % i, [128, 2, 128], BF16) for i in range(4)]
    rT = mk2("cTr", [128, 2, 128])
    bhc = [mk2("cbhc%d" % c, [128, 256], BF16) for c in range(2)]
    khc = [mk2("ckhc%d" % c, [128, 256], BF16) for c in range(2)]
    at_b = mk2("cat_b", [128, 256], BF16)
    v_b = mk2("cv_b", [128, 256], BF16)
    IM = A.sb("cIM", [128, 64])
    tt(P, "dve", IM, idn[:, 0:64], idn[:, 64:128], ALU.add)

    HB = []
    for dd in range(2):
        HB.append(alloc_chunk_heads(A, dd))
    MK4 = [A.sb("cMK4%d" % d, [128, 4, 128]) for d in range(2)]
    for d in range(2):
        ms_, mst_, mit_ = (0, 1, 2) if d == 0 else (3, 4, 5)
        for i_, mi_ in enumerate((ms_, mst_, mst_, mit_)):
            cp(P, "dve", MK4[d][:, i_, :], mk[:, mi_, :])
    yo = [A.sb("cyo%d" % i, [64, 2, 256]) for i in range(2)]
    STg = [[A.sb("gST%d%d" % (d, h), [32, 64]) for h in range(4)] for d in range(2)]
    for d in range(2):
        for h in range(4):
            P.op("dve", "memset", ap=STg[d][h], constant=0.0)
    gTOT, gincS, gEin, gEnin, gEend, gEtot, gtmp = [mk2("gE%d" % i, [128, 128]) for i in range(7)]
    gq, gk, gkh = [mk2("gq%d" % i, [128, 128]) for i in range(3)]
    gkhc = [mk2("gkhc%d" % c, [128, 128]) for c in range(2)]
    gqT, gkT, gPT = [[mk2("gT%d_%d" % (i, h), [32, 128]) for h in range(4)] for i in range(3)]
    gyo = [A.sb("gyo%d" % i, [64, 2, 256]) for i in range(2)]
    ps = C.ps
    border = [1, 0] + list(range(NT - 1, 1, -1))
    H4 = range(4)
    it = 0
    cut = C.dbg.get("chunk_cut", 99)

    def body(n, d):
        if True:
            (AM, XB, XX, AakT, ArbT, ArkT, TT, PM, Ap, M1, U0, RpT, DPC, Y0cc, Y0c, GH, GT, Hc, gA, gY0, gH) = HB[d]
            ps = C.ps[4 * d:4 * d + 4] + C.ps[4 - 4 * d:8 - 4 * d]
            t = n if d == 0 else border[n]
            q = d
            ch = chs[q]
            YO = yo[q]
            dma(P, "sp" if d == 0 else "pool", ch, V(C.CH_ap[t * 128:(t + 1) * 128, :], C.CH_bufs[t]))
            lw = ch[:, d * 256:(d + 1) * 256]
            ke = ch[:, 512 + d * 256:768 + d * 256]
            b_ = ch[:, 1024 + d * 256:1280 + d * 256]
            a_, r_, v_ = ch[:, 1536:1792], ch[:, 1792:2048], ch[:, 2048:2304]
            m_s, m_st, m_it = (0, 1, 2) if d == 0 else (3, 4, 5)
            pc = ps[4 + q]
            mm(P, pc[:, 0:256], mk[:, m_it, :], lw, True, True)
            mm(P, pc[:, 256:512], mk[:, 6, :], lw, True, True)
            cp(P, "dve", TOT[q], pc[:, 256:512])
            cp(P, "dve", incS[q], pc[:, 0:256])
            act(P, Ein[q], incS[q], AF.Exp)
            act(P, Enin[q], incS[q], AF.Exp, scale=-1.0)
            tt(P, "dve", tmpx[q], incS[q], lw, ALU.subtract)
            act(P, Eex[q], tmpx[q], AF.Exp)
            tt(P, "dve", tmpy[q], TOT[q], incS[q], ALU.subtract)
            act(P, Eend[q], tmpy[q], AF.Exp)
            act(P, Etot[q], TOT[q], AF.Exp)
            tt(P, "dve", at[q], a_, Eex[q], ALU.mult)
            cp(P, "act", at_b[q], at[q])
            cp(P, "act", v_b[q], v_)
            tt(P, "dve", rt[q], r_, Ein[q], ALU.mult)
            tt(P, "dve", bt[q], b_, Enin[q], ALU.mult)
            tt(P, "dve", kt[q], ke, Enin[q], ALU.mult)
            tt(P, "dve", bh[q], b_, Eend[q], ALU.mult)
            tt(P, "dve", kh[q], ke, Eend[q], ALU.mult)
            for c in range(2):
                ts(P, "dve", bhc[c][q], bh[q], mk[:, 6, c * 64:c * 64 + 1], ALU.mult)
                ts(P, "dve", khc[c][q], kh[q], mk[:, 6, c * 64:c * 64 + 1], ALU.mult)
            yield
            for qi, (src, dst) in enumerate(((at, aT), (rt, rT), (bt, bT), (kt, kT))):
                pb_ = ps[6 + qi % 2]
                for a2 in range(2):
                    tr(P, pb_[:, a2 * 128:(a2 + 1) * 128], src[q][:, a2 * 128:(a2 + 1) * 128], idn)
                cp(P, "dve", dst[q], pb_[:, 0:256].rr("p (a n) -> p a n", a=2))
                if qi == 1:
                    cp(P, "dve", rTb[q], pb_[:, 0:256].rr("p (a n) -> p a n", a=2))

            def hv(h):
                pair, hl = h // 2, h % 2
                return pair, slice(hl * 64, (hl + 1) * 64), slice(h * 64, (h + 1) * 64)
            if cut <= 2:
                return
            yield
            for h in H4:
                pair, hs, hc = hv(h)
                pA = ps[h]
                mm(P, pA[:, 0:128], aT[q][hs, pair, :], bT[q][hs, pair, :], True, True)
                mm(P, pA[:, 128:256], bT[q][hs, pair, :], aT[q][hs, pair, :], True, True)
                mm(P, pA[:, 256:384], kT[q][hs, pair, :], aT[q][hs, pair, :], True, True)
                mm(P, pA[:, 384:512], bT[q][hs, pair, :], rTb[q][hs, pair, :], True, True)
            for h in H4:
                pA = ps[h]
                tt(P, "dve", AM[h].rr("p a n -> p (a n)"), pA, MK4[d].rr("p a n -> p (a n)"), ALU.mult)
                tt(P, "dve", TT[h], AM[h][:, 1, :], idn, ALU.add)
            for h in H4:
                pair, hs, hc = hv(h)
                mm(P, ps[h][:, 0:128], kT[q][hs, pair, :], rTb[q][hs, pair, :], True, True)
            for h in H4:
                tt(P, "dve", ArkT[h], ps[h][:, 0:128], mk[:, m_it, :], ALU.mult)
            if cut <= 3:
                return
            yield
            for s in range(5):
                yield
                XXc, XXn = XX[s % 2], XX[(s + 1) % 2]
                for h in H4:
                    pX = ps[h]
                    mm(P, pX[:, 128:256], XXc[h][:, 1, :], XXc[h][:, 0, :], True, True)
                    if s < 4:
                        mm(P, pX[:, 256:384], XXc[h][:, 0, :], XXc[h][:, 1, :], True, True)
                for h in H4:
                    pX = ps[h]
                    if s < 4:
                        cp(P, "dve", XXn[h], pX[:, 128:384].rr("p (a n) -> p a n", a=2))
                    else:
                        cp(P, "dve", XXn[h][:, 0, :], pX[:, 128:256])
                for h in H4:
                    mm(P, ps[h][:, 384:512], XXn[h][:, 0, :], TT[h], True, True)
                for h in H4:
                    tt(P, "dve", TT[h], TT[h], ps[h][:, 384:512], ALU.add)
            if cut <= 4:
                return
            yield
            for h in H4:
                pair, hs, hc = hv(h)
                mm(P, ps[h][:, 0:64], TT[h], at_b[q][:, hc], True, True)
                mm(P, ps[h][:, 64:128], AakT[h], v_b[q][:, hc], True, True)
            for h in H4:
                cp(P, "dve", PM[h], ps[h][:, 0:128].rr("p (a n) -> p a n", a=2))
            for h in H4:
                mm(P, ps[h][:, 128:192], TT[h], M1[h], True, True)
            for h in H4:
                cp(P, "dve", U0[h], ps[h][:, 128:192])
            for h in H4:
                pair, hs, hc = hv(h)
                for c in range(2):
                    cs = slice(c * 64, (c + 1) * 64)
                    mm(P, ps[h][0:64, 192 + c * 64:256 + c * 64], ArbT[h][:, cs], U0[h], True, False)
                    mm(P, ps[h][0:64, 192 + c * 64:256 + c * 64], ArkT[h][:, cs], v_b[q][:, hc], False, True)
                mm(P, ps[h][0:64, 320:448], Ap[h], ArbT[h], True, True)
                tt(P, "dve", DPC[h], IM, Etot[q][:, hc], ALU.mult)
            for h in H4:
                pair, hs, hc = hv(h)
                cp(P, "dve", Y0cc[h], ps[h][0:64, 192:320].rr("p (a n) -> p a n", a=2))
                tt(P, "dve", RpT[h], ps[h][0:64, 320:448], rT[q][hs, pair, :], ALU.add)
            if cut <= 5:
                return
            for h in H4:
                pair, hs, hc = hv(h)
                pG = ps[h]
                for c in range(2):
                    cs = slice(c * 64, (c + 1) * 64)
                    o = c * 128
                    mm(P, pG[0:64, o:o + 64], Ap[h], bhc[c][q][:, hc], True, False)
                    mm(P, pG[0:64, o:o + 64], idn[:, cs], DPC[h], False, True)
                    mm(P, pG[0:64, o + 64:o + 128], bhc[c][q][:, hc], U0[h], True, False)
                    mm(P, pG[0:64, o + 64:o + 128], khc[c][q][:, hc], v_b[q][:, hc], False, True)
            for h in H4:
                pG = ps[h]
                cp(P, "dve", GH[h], pG[0:64, 0:256].rr("p (a n) -> p a n", a=4))
            if cut <= 6:
                return
            yield
            for c in ((0, 1) if d == 0 else (1, 0)):
                cs = slice(c * 64, (c + 1) * 64)
                for h in H4:
                    pG = ps[h]
                    S_ = ST[d][h]
                    mm(P, pG[0:64, 256:320], RpT[h][:, cs], S_, True, False)
                    mm(P, pG[0:64, 256:320], idn[0:64, 0:64], Y0c[c][h], False, True)
                    mm(P, pG[0:64, 320:384], GT[c][h], S_, True, False)
                    mm(P, pG[0:64, 320:384], idn[0:64, 0:64], Hc[c][h], False, True)
                for h in H4:
                    pair, hs, hc = hv(h)
                    pG = ps[h]
                    cp(P, "dve", YO[:, c, hc], pG[0:64, 256:320])
                    cp(P, "dve", ST[d][h], pG[0:64, 320:384])
            dma(P, "sp" if d == 0 else "pool",
                V(C.YT_ap[d][t * 128:(t + 1) * 128, :].rearrange("(c p) n -> p c n", p=64), C.YT_bufs[d][t]), YO)
            yield
            GYO = gyo[q]
            glw = ch[:, 2304 + d * 128:2432 + d * 128]
            gq_, gk_, gv_ = ch[:, 2560:2688], ch[:, 2688:2816], ch[:, 2816:3072]
            pcg = ps[4 + q]
            mm(P, pcg[:, 0:128], mk[:, m_it, :], glw, True, True)
            mm(P, pcg[:, 128:256], mk[:, 6, :], glw, True, True)
            cp(P, "dve", gincS[q], pcg[:, 0:128])
            cp(P, "dve", gTOT[q], pcg[:, 128:256])
            act(P, gEin[q], gincS[q], AF.Exp)
            act(P, gEnin[q], gincS[q], AF.Exp, scale=-1.0)
            tt(P, "dve", gtmp[q], gTOT[q], gincS[q], ALU.subtract)
            act(P, gEend[q], gtmp[q], AF.Exp)
            act(P, gEtot[q], gTOT[q], AF.Exp)
            tt(P, "dve", gq[q], gq_, gEin[q], ALU.mult)
            tt(P, "dve", gk[q], gk_, gEnin[q], ALU.mult)
            tt(P, "dve", gkh[q], gk_, gEend[q], ALU.mult)
            for c in range(2):
                ts(P, "dve", gkhc[c][q], gkh[q], mk[:, 6, c * 64:c * 64 + 1], ALU.mult)
            for h in H4:
                pT_ = ps[6 + h % 2]
                g32 = slice(h * 32, (h + 1) * 32)
                tr(P, pT_[0:32, 0:128], gq[q][:, g32], idn)
                tr(P, pT_[0:32, 128:256], gk[q][:, g32], idn)
                tr(P, pT_[0:32, 256:384], gEtot[q][:, g32], idn)
                cp(P, "dve", gqT[h][q], pT_[0:32, 0:128])
                cp(P, "dve", gkT[h][q], pT_[0:32, 128:256])
                cp(P, "dve", gPT[h][q], pT_[0:32, 256:384])
            for h in H4:
                mm(P, ps[h][:, 0:128], gkT[h][q], gqT[h][q], True, True)
            for h in H4:
                tt(P, "dve", gA[h], ps[h][:, 0:128], mk[:, m_it, :], ALU.mult)
            for h in H4:
                hc = slice(h * 64, (h + 1) * 64)
                g32 = slice(h * 32, (h + 1) * 32)
                for c in range(2):
                    cs = slice(c * 64, (c + 1) * 64)
                    mm(P, ps[h][0:64, 128 + c * 64:192 + c * 64], gA[h][:, cs], gv_[:, hc], True, True)
                    mm(P, ps[h][0:32, 256 + c * 64:320 + c * 64], gkhc[c][q][:, g32], gv_[:, hc], True, True)
            for h in H4:
                for c in range(2):
                    cp(P, "dve", gY0[c][h], ps[h][0:64, 128 + c * 64:192 + c * 64])
                    cp(P, "dve", gH[c][h], ps[h][0:32, 256 + c * 64:320 + c * 64])
            for c in ((0, 1) if d == 0 else (1, 0)):
                cs = slice(c * 64, (c + 1) * 64)
                for h in H4:
                    S_ = STg[d][h]
                    mm(P, ps[h][0:64, 384:448], gqT[h][q][:, cs], S_, True, False)
                    mm(P, ps[h][0:64, 384:448], idn[0:64, 0:64], gY0[c][h], False, True)
                for h in H4:
                    hc = slice(h * 64, (h + 1) * 64)
                    S_ = STg[d][h]
                    cp(P, "dve", GYO[:, c, hc], ps[h][0:64, 384:448])
                    stt(P, "dve", S_, S_, gPT[h][q][:, c * 64:c * 64 + 1], gH[c][h], ALU.mult, ALU.add)
            dma(P, "sp" if d == 1 else "pool",
                V(C.YTG_ap[d][t * 128:(t + 1) * 128, :].rearrange("(c p) n -> p c n", p=64), C.YTG_bufs[d][t]), GYO)
    for n in range(NT if cut > 50 else 1):
        gens = [body(n, 0), body(n, 1)]
        while gens:
            nxt = []
            for g in gens:
                try:
                    next(g)
                    nxt.append(g)
                except StopIteration:
                    pass
            gens = nxt
    A.close()


class LayerState:
    pass


def layer(C, l):
    P = C.P
    LA = Arena(P)
    L = LayerState()
    L.mod = LA.sb("mod", [128, 48, 2])
    L.sc1 = LA.sb("sc1", [128, 8, 2])
    L.sc2 = LA.sb("sc2", [128, 8, 2])
    L.grow = [[LA.sb("grow%d%d" % (ii, j), [128, 1024]) for j in range(2)] for ii in range(2)]
    phase_ada(C, l, L)
    if C.dbg.get("dump") and l == C.dbg.get("layer", 0):
        dma(P, "sp", C.dout("d_mod", [128, 48, 2]), L.mod)
        for ii in range(2):
            for j in range(2):
                dma(P, "sp", C.dout("d_grow%d%d" % (ii, j), [128, 1024]), L.grow[ii][j])
    HA = Arena(P)
    hfm = HA.sb("hfm", [128, 8, NTOK], BF16)
    phase_norm(C, l, L, 1, hfm)
    if C.dbg.get("dump") and l == C.dbg.get("layer", 0):
        dma(P, "sp", C.dout("d_hfm", [128, 8, NTOK], BF16), hfm)
    phase_win_tm(C, l, L, hfm)
    if not C.dbg.get("skip_gates"):
        phase_win_gates(C, l, L, hfm)
    HA.close()
    if C.dbg.get("stop_after") == "win":
        LA.close(); return
    if not C.dbg.get("skip_ab"):
        phase_prep(C, l, L)
        if C.dbg.get("stop_after") == "prep":
            LA.close(); return
        if C.dbg.get("old_gla") and not C.dbg.get("skip_scan"):
            phase_scan(C, l, C.dbg.get("nchunks"))
        if not C.dbg.get("old_rwkv"):
            phase_chunk(C, l, L)
        if C.dbg.get("stop_after") == "chunk":
            LA.close(); return
        if C.dbg.get("stop_after") == "scan":
            LA.close(); return
        phase_fin_ab(C, l, L)
    if C.dbg.get("stop_after") == "fin":
        LA.close(); return
    if not C.dbg.get("skip_attn"):
        phase_attn(C, l, L)
    if C.dbg.get("stop_after") == "attn":
        LA.close(); return
    if not C.dbg.get("skip_s5"):
        phase_s5(C, l, L)
    if C.dbg.get("stop_after") == "s5":
        LA.close(); return
    phase_merge(C, l, L)
    if C.dbg.get("stop_after") == "merge":
        LA.close(); return
    WA = Arena(P)
    L.WT = WA.sb("WT", [32, NTOK])
    HA = Arena(P)
    hfm2 = HA.sb("hfm2", [128, 8, NTOK], BF16)
    RA = Arena(P)
    phase_norm(C, l, L, 2, hfm2, per_tile=make_router(C, l, L, RA))
    RA.close()
    if C.dbg.get("dump") and l == C.dbg.get("layer", 0):
        dma(P, "sp", C.dout("d_hfm2", [128, 8, NTOK], BF16), hfm2)
        dma(P, "sp", C.dout("d_WT", [32, NTOK]), L.WT)
    H2 = V(C.H2_ap, C.H2_buf)
    dma(P, "sp", H2, hfm2)
    HA.close()
    if C.dbg.get("stop_after") == "norm2":
        WA.close(); LA.close(); return
    phase_moe(C, l, L, H2, l == DEPTH - 1)
    WA.close()
    LA.close()


def make_maskw():
    m = np.zeros((128, 384), np.float32)
    i = np.arange(128)[:, None]
    j = np.arange(128)[None, :]
    m[:, 0:128] = np.where(j >= i, 0.0, -1e30)
    m[:, 256:384] = np.where(j <= i, 0.0, -1e30)
    return m


def make_rope():
    rows = SEQ // 64
    row = np.repeat(np.arange(rows, dtype=np.float32), 64)
    col = np.tile(np.arange(64, dtype=np.float32), rows)
    inv = (10000.0 ** (-np.arange(16, dtype=np.float32) / 16)).astype(np.float32)
    ang = np.concatenate([row[:, None] * inv, col[:, None] * inv], axis=-1).astype(np.float32)
    return np.cos(ang).astype(np.float32), np.sin(ang).astype(np.float32)


ROPE = make_rope()


def s5_host(A):
    f = np.float32
    lre, lim, ldt = A("s5_lam_re"), A("s5_lam_im"), A("s5_log_dt")
    ldt_e = np.repeat(ldt[..., None], 64, axis=-1)
    rows = np.stack([lre.reshape(DEPTH, 2, 1024), lim.reshape(DEPTH, 2, 1024), ldt_e.reshape(DEPTH, 2, 1024)], axis=2)
    sm = rows.reshape(DEPTH, 2, 3, 8, 128).transpose(0, 4, 1, 2, 3)
    bre, bim = A("s5_b_re"), A("s5_b_im")
    bt = np.zeros((DEPTH, 2, 8, 128, 128), f)
    cre, cim = A("s5_c_re"), A("s5_c_im")
    ct = np.zeros((DEPTH, 2, 2, 8, 128, 128), f)
    for g in range(16):
        j, hh = g // 2, g % 2
        c0 = (g % 8) * 16
        for ri, b in enumerate((bre, bim)):
            bt[:, ri, j, c0:c0 + 16, hh * 64:(hh + 1) * 64] = b[:, g].transpose(0, 2, 1)
        for ri, c in enumerate((cre, cim)):
            ct[:, :, ri, j, hh * 64:(hh + 1) * 64, c0:c0 + 16] = c[:, :, g].transpose(0, 1, 3, 2)
    return {
        "s5_sm": np.ascontiguousarray(sm, f), "s5_rows": np.ascontiguousarray(rows, f),
        "s5_bt": bt, "s5_ct": ct,
        "pw2": (2.0 ** np.arange(16)).astype(f).reshape(1, 16),
        "s5_d_fm": np.ascontiguousarray(np.pad(A("s5_d").reshape(DEPTH, 2, 128).transpose(0, 2, 1), ((0, 0), (0, 0), (0, 14)))),
        "s5_bglu_fm": np.ascontiguousarray(np.pad(A("s5_b_glu").reshape(DEPTH, 2, 128).transpose(0, 2, 1), ((0, 0), (0, 0), (0, 14)))),
        "s5_w_glu": A("s5_w_glu"),
    }


def make_sele():
    s = np.zeros((32, 32, 128), np.float32)
    for e in range(32):
        s[e, e, :] = 1.0
    return s


def make_cmasks():
    r = np.arange(128)[:, None]
    c = np.arange(128)[None, :]
    same = (r // 64) == (c // 64)
    m = np.zeros((128, 7, 128), np.float32)
    m[:, 0] = same & (c < r)
    m[:, 1] = same & (r < c)
    m[:, 2] = same & (r <= c)
    m[:, 3] = same & (c > r)
    m[:, 4] = same & (r > c)
    m[:, 5] = same & (r >= c)
    m[:, 6] = same
    return m


def make_sel():
    s = np.zeros((128, 64, 128), np.float32)
    for j in range(64):
        for hh in range(2):
            s[2 * j + hh, j, hh * 64:(hh + 1) * 64] = 1.0
    return s


def blkdiag(mats):
    n = len(mats)
    L, r, c = mats[0].shape
    o = np.zeros((L, n * r, n * c), np.float32)
    for i, m in enumerate(mats):
        o[:, i * r:(i + 1) * r, i * c:(i + 1) * c] = m
    return o


def host_inputs(inputs, b):
    f = np.float32

    def A(k):
        return np.asarray(inputs[k], f)
    c = np.asarray(inputs["c"], f)[b]
    cctx = np.asarray(inputs["c_ctx"], f)
    cc = np.stack([c.reshape(8, 128).T, cctx.reshape(8, 128).T], axis=-1)
    m = {
        "xb": np.ascontiguousarray(np.asarray(inputs["x"], f)[b]),
        "ctxb": np.ascontiguousarray(np.asarray(inputs["ctx"], f)[b]),
        "cc": np.ascontiguousarray(cc),
        "w_ada": np.asarray(inputs["w_ada"], f),
        "b_ada": np.asarray(inputs["b_ada"], f),
        "b_ada_fm": np.ascontiguousarray(np.asarray(inputs["b_ada"], f).reshape(DEPTH, 48, 128).transpose(0, 2, 1)),
        "g1_fm": np.ascontiguousarray(np.asarray(inputs["norm1_g"], f).reshape(DEPTH, 8, 128).transpose(0, 2, 1)),
        "g2_fm": np.ascontiguousarray(np.asarray(inputs["norm2_g"], f).reshape(DEPTH, 8, 128).transpose(0, 2, 1)),
        "w_in": np.asarray(inputs["w_in"], f),
        "ident": np.eye(128, dtype=f),
        "sel": make_sel(),
        "maskw": make_maskw(),
        "ropec": ROPE[0],
        "ropes": ROPE[1],
        "attn_sink": np.ascontiguousarray(np.pad(A("attn_sink"), ((0, 0), (0, 12)))),
        **s5_host(A),
        "cmasks": make_cmasks(),
        "w_branch": A("w_branch"),
        "w_out": A("w_out"),
        "w_router": np.ascontiguousarray(np.concatenate([A("w_router_g"), A("w_router_e")], axis=2)),
        "b_router": np.ascontiguousarray(np.concatenate([A("b_router_g"), A("b_router_e")], axis=1)),
        "w_exp_gate": A("w_exp_gate"),
        "w_exp_up": A("w_exp_up"),
        "w_exp_down": A("w_exp_down"),
        "pv": np.ascontiguousarray(np.concatenate([
            A("rwkv_mu").reshape(DEPTH, -1), A("rwkv_kk"), A("rwkv_ka"), A("rwkv_rk").reshape(DEPTH, -1),
            A("rwkv_w0").reshape(DEPTH, -1), A("rwkv_a0").reshape(DEPTH, -1), A("rwkv_ln_g"),
            A("gla_ab").reshape(DEPTH, -1), A("gla_ln_g")], axis=1)),
        "w1cat": np.ascontiguousarray(np.concatenate([A("rwkv_w1")[:, 0], A("rwkv_w1")[:, 1],
                                                      A("rwkv_a1")[:, 0], A("rwkv_a1")[:, 1]], axis=2)),
        "w2blk": blkdiag([A("rwkv_w2")[:, 0], A("rwkv_w2")[:, 1], A("rwkv_a2")[:, 0], A("rwkv_a2")[:, 1]]),
        "g1": A("rwkv_g1"),
        "g2": A("rwkv_g2"),
        "a2blk": blkdiag([A("gla_a2")[:, 0], A("gla_a2")[:, 1]]),
        "final_g": np.asarray(inputs["final_norm_g"], f).reshape(1, D),
    }
    return m


def kernel(**inputs):
    nc = build_program()
    in_maps = [host_inputs(inputs, b) for b in range(8)]
    res = run_bass_kernel_spmd(nc, in_maps, core_ids=list(range(8)))
    return np.stack([r["out"] for r in res.results], axis=0)
```

```python
import math
from contextlib import ExitStack

import numpy as np
import concourse.bass as bass
import concourse.mybir as mybir
from concourse.bass_utils import run_bass_kernel_spmd

F32 = mybir.dt.float32
BF16 = mybir.dt.bfloat16
ALU = mybir.AluOpType
AF = mybir.ActivationFunctionType
AX = mybir.AxisListType

D = 1024
SEQ = 4096
CTX = 256
NT = (SEQ + CTX) // 128
NTOK = SEQ + CTX
DEPTH = 2
EPS = 1e-6
GN_EPS = 64e-5
O1, O2, O3, O4, PIN = 1024, 1824, 2336, 2592, 6688
PV_MU, PV_KK, PV_KA, PV_RK, PV_W0, PV_A0, PV_LNG, PV_GAB, PV_GLNG = 0, 1024, 1280, 1536, 1792, 2304, 2816, 3072, 3328
NPV = 3584

DEBUG = False
PENDING = "PENDING"
WKEYS = ("out", "accum_out", "ap")
SKEYS = ("scalar1", "scalar2", "scale", "bias", "scalar")


class Buf:
    __slots__ = ("name", "w", "rd", "ws", "wok")

    def __init__(self, name=""):
        self.name = name
        self.w = None
        self.rd = {}
        self.ws = False
        self.wok = False


class V:
    __slots__ = ("ap", "bufs")

    def __init__(self, ap, bufs):
        self.ap = ap
        self.bufs = bufs if isinstance(bufs, tuple) else (bufs,)

    def __getitem__(self, k):
        return V(self.ap[k], self.bufs)

    def rr(self, pat, **kw):
        return V(self.ap.rearrange(pat, **kw), self.bufs)

    def bc(self, shape):
        return V(self.ap.to_broadcast(list(shape)), self.bufs)

    def bitcast(self, dt):
        return V(self.ap.bitcast(dt), self.bufs)

    def wb(self, *bufs):
        return V(self.ap, tuple(bufs))

    @property
    def shape(self):
        return tuple(self.ap.shape)


class Prog:
    ENG = ("pe", "act", "dve", "pool", "sp")
    K = 6
    STRICT_ALL = False

    def __init__(self, nc):
        self.nc = nc
        self.eng = {"pe": nc.tensor, "act": nc.scalar, "dve": nc.vector, "pool": nc.gpsimd, "sp": nc.sync}
        self.sem = {e: nc.alloc_semaphore("s_" + e) for e in self.ENG}
        self.cnt = {e: 0 for e in self.ENG}
        self.dsem = {q: [nc.alloc_semaphore("d_%s%d" % (q, i)) for i in range(self.K)] for q in ("sp", "act", "pool")}
        self.dcnt = {q: 0 for q in self.dsem}
        self.known = {e: {} for e in self.ENG}
        self.pend_r = []
        self.pend_w = []
        self.uid = 0
        self.nins = 0

    def _need(self, e, tok, is_dma, strict=False):
        if tok is None:
            return
        if tok is PENDING:
            assert e == "pe" and not is_dma, "dependency on an unmarked PE op"
            return
        sem, val, owner = tok
        if owner == e and not is_dma and not (strict and e != "pe") and not self.STRICT_ALL:
            return
        k = self.known[e]
        if k.get(sem.num, 0) >= val:
            return
        self.eng[e].wait_ge(sem, val)
        self.nins += 1
        k[sem.num] = val

    def op(self, e, meth, mark=True, lax=False, **kw):
        reads, writes, args, sreads, awrites = [], [], {}, [], []
        big_w, small_r, psum_in = True, set(), False
        for k, v in kw.items():
            if isinstance(v, V):
                fsz = 1
                for d_ in v.ap.shape[1:]:
                    fsz *= d_
                if k in WKEYS:
                    if fsz < 512 or 0 in [st_ for st_, _ in v.ap.ap[1:]]:
                        big_w = False
                else:
                    if v.ap.name == "psall":
                        psum_in = True
                    if fsz < 512:
                        small_r.update(v.bufs)
                (writes if k in WKEYS else reads).extend(v.bufs)
                if k in SKEYS:
                    sreads.extend(v.bufs)
                if k == "accum_out":
                    awrites.extend(v.bufs)
                args[k] = v.ap
            else:
                args[k] = v
        is_dma = meth == "dma_start"
        for b in reads:
            relaxed = lax or (e == "dve" and b.wok and b not in small_r)
            self._need(e, b.w, is_dma, strict=(not relaxed) or b.ws or e == "act" or b in sreads)
        for b in writes:
            self._need(e, b.w, is_dma)
            for t in b.rd.values():
                self._need(e, t, is_dma)
        if is_dma:
            n = self.dcnt[e]
            sem = self.dsem[e][n % self.K]
            r = n // self.K
            if r > 0:
                self._need(e, (sem, 16 * r, None), True)
            ins = getattr(self.eng[e], meth)(**args)
            ins.then_inc(sem, 16)
            self.dcnt[e] = n + 1
            tok = (sem, 16 * (r + 1), None)
            key = ("d", sem.num)
        else:
            ins = getattr(self.eng[e], meth)(**args)
            key = e
            if mark:
                self.cnt[e] += 1
                ins.then_inc(self.sem[e], 1)
                tok = (self.sem[e], self.cnt[e], e)
                if e == "pe" and (self.pend_r or self.pend_w):
                    for b in self.pend_r:
                        if b.rd.get("pe") is PENDING:
                            b.rd["pe"] = tok
                    for b in self.pend_w:
                        if b.w is PENDING:
                            b.w = tok
                    self.pend_r = []
                    self.pend_w = []
            else:
                assert e == "pe"
                tok = PENDING
                self.pend_r.extend(reads)
                self.pend_w.extend(writes)
        self.nins += 1
        for b in reads:
            b.rd[key] = tok
        for b in writes:
            b.w = tok
            b.rd = {}
            b.ws = (b in awrites) or e == "act"
            b.wok = (e == "dve") and big_w and (not psum_in) and (b not in awrites) and not is_dma
        return ins

    def barrier(self):
        assert not self.pend_r and not self.pend_w
        toks = [(self.sem[e], self.cnt[e], e) for e in self.ENG if self.cnt[e] > 0]
        for q in self.dsem:
            n = self.dcnt[q]
            for i in range(self.K):
                c = (n - i + self.K - 1) // self.K if n > i else 0
                if c > 0:
                    toks.append((self.dsem[q][i], 16 * c, None))
        for e in self.ENG:
            for t in toks:
                self._need(e, t, False)

    def name(self, s):
        self.uid += 1
        return "%s_%d" % (s, self.uid)

    def dram(self, name, shape, dt, kind="Internal"):
        return self.nc.dram_tensor(name, list(shape), dt, kind=kind).ap()


class Arena:
    def __init__(self, P):
        self.P = P
        self.stack = ExitStack()

    def sb(self, name, shape, dt=F32):
        h = self.stack.enter_context(self.P.nc.sbuf_tensor(self.P.name(name), list(shape), dt))
        return V(h.ap(), Buf(name))

    def close(self):
        self.P.barrier()
        self.stack.close()


def dma(P, q, out, in_):
    return P.op(q, "dma_start", out=out, in_=in_)


def mm(P, out, lhsT, rhs, start, stop, mark=None):
    return P.op("pe", "matmul", mark=(stop if mark is None else mark), out=out, lhsT=lhsT, rhs=rhs,
                start=start, stop=stop)


def tr(P, out, in_, ident, mark=True):
    return P.op("pe", "transpose", mark=mark, out=out, in_=in_, identity=ident)


def tt(P, e, out, in0, in1, op):
    return P.op(e, "tensor_tensor", out=out, in0=in0, in1=in1, op=op)


def ts(P, e, out, in0, s1, op0, s2=None, op1=None, **kw):
    if op1 is None:
        return P.op(e, "tensor_scalar", out=out, in0=in0, scalar1=s1, scalar2=None, op0=op0, **kw)
    return P.op(e, "tensor_scalar", out=out, in0=in0, scalar1=s1, scalar2=s2, op0=op0, op1=op1, **kw)


def act(P, out, in_, func, **kw):
    return P.op("act", "activation", out=out, in_=in_, func=func, **kw)


def cp(P, e, out, in_):
    if e == "act":
        return act(P, out, in_, AF.Copy)
    return P.op(e, "tensor_copy", out=out, in_=in_)


class Ctx:
    pass


def build_program(dbg=None):
    dbg = dbg or {}
    nc = bass.Bass("TRN2", target_bir_lowering=False)
    P = Prog(nc)
    C = Ctx()
    C.P, C.nc, C.dbg = P, nc, dbg
    skind = "ExternalOutput" if dbg.get("expose") else "Internal"

    def din(name, shape, dt=F32):
        return V(nc.dram_tensor(name, list(shape), dt, kind="ExternalInput").ap(), Buf(name))

    I = {}
    I["xb"] = din("xb", [SEQ, D])
    I["ctxb"] = din("ctxb", [CTX, D])
    I["cc"] = din("cc", [128, 8, 2])
    I["w_ada"] = din("w_ada", [DEPTH, D, 6 * D])
    I["b_ada"] = din("b_ada", [DEPTH, 6 * D])
    I["b_ada_fm"] = din("b_ada_fm", [DEPTH, 128, 48])
    I["g1_fm"] = din("g1_fm", [DEPTH, 128, 8])
    I["g2_fm"] = din("g2_fm", [DEPTH, 128, 8])
    I["w_in"] = din("w_in", [DEPTH, D, PIN])
    I["ident"] = din("ident", [128, 128])
    I["sel"] = din("sel", [128, 64, 128])
    I["maskw"] = din("maskw", [128, 384])
    I["ropec"] = din("ropec", [SEQ, 32])
    I["ropes"] = din("ropes", [SEQ, 32])
    I["attn_sink"] = din("attn_sink", [DEPTH, 16])
    I["s5_sm"] = din("s5_sm", [DEPTH, 128, 2, 3, 8])
    I["s5_rows"] = din("s5_rows", [DEPTH, 2, 3, 1024])
    I["s5_bt"] = din("s5_bt", [DEPTH, 2, 8, 128, 128])
    I["s5_ct"] = din("s5_ct", [DEPTH, 2, 2, 8, 128, 128])
    I["pw2"] = din("pw2", [1, 16])
    I["cmasks"] = din("cmasks", [128, 7, 128])
    I["w_branch"] = din("w_branch", [DEPTH, 4, 256, D])
    I["w_out"] = din("w_out", [DEPTH, D, D])
    I["w_router"] = din("w_router", [DEPTH, D, 36])
    I["b_router"] = din("b_router", [DEPTH, 36])
    I["w_exp_gate"] = din("w_exp_gate", [DEPTH, 32, D, 512])
    I["w_exp_up"] = din("w_exp_up", [DEPTH, 32, D, 512])
    I["w_exp_down"] = din("w_exp_down", [DEPTH, 32, 512, D])
    I["s5_d_fm"] = din("s5_d_fm", [DEPTH, 128, 16])
    I["s5_bglu_fm"] = din("s5_bglu_fm", [DEPTH, 128, 16])
    I["s5_w_glu"] = din("s5_w_glu", [DEPTH, 256, 256])
    I["pv"] = din("pv", [DEPTH, NPV])
    I["w1cat"] = din("w1cat", [DEPTH, 256, 128])
    I["w2blk"] = din("w2blk", [DEPTH, 128, 1024])
    I["g1"] = din("g1", [DEPTH, 256, 64])
    I["g2"] = din("g2", [DEPTH, 64, 256])
    I["a2blk"] = din("a2blk", [DEPTH, 32, 256])
    I["final_g"] = din("final_g", [1, D])
    C.I = I

    out = V(nc.dram_tensor("out", [SEQ, D], F32, kind="ExternalOutput").ap(), Buf("out"))
    C.out = out

    def dout(name, shape, dt=F32):
        return V(nc.dram_tensor(name, list(shape), dt, kind="ExternalOutput").ap(), Buf(name))
    C.dout = dout

    xres_ap = P.dram("xres", [NTOK, D], F32, kind=skind)
    C.xres = [V(xres_ap[t * 128:(t + 1) * 128, :], Buf("xres%d" % t)) for t in range(NT)]
    C.U_ap = P.dram("U", [NTOK + 3, O4], F32, kind=skind)
    C.U_bufs = [Buf("U%d" % t) for t in range(NT)]
    C.U_pad = Buf("Upad")
    C.STR_ap = [P.dram("STR%d" % d, [NTOK, 2, 1024], BF16, kind=skind) for d in range(2)]
    C.STR_bufs = [[Buf("STR%d_%d" % (d, t)) for t in range(NT)] for d in range(2)]
    C.Vs_ap = P.dram("Vs", [128, NTOK, 6], F32, kind=skind)
    C.Vs_bufs = [Buf("Vs%d" % t) for t in range(NT)]
    C.Y_ap = [P.dram("Y%d" % d, [128, NTOK, 6], F32, kind=skind) for d in range(2)]
    C.Y_bufs = [[Buf("Y%d_%d" % (d, c)) for c in range(NTOK // 64)] for d in range(2)]
    C.FIN_ap = P.dram("FIN", [NTOK, 768], F32, kind=skind)
    C.FIN_bufs = [Buf("FIN%d" % t) for t in range(NT)]
    C.YB_ap = P.dram("YB", [4, 256, NTOK], BF16, kind=skind)
    C.YB_bufs = [[Buf("YB%d_%d" % (i, t)) for t in range(NT)] for i in range(4)]
    C.CH_ap = P.dram("CH", [NTOK, 3072], F32, kind=skind)
    C.CH_bufs = [Buf("CH%d" % t) for t in range(NT)]
    C.YT_ap = [P.dram("YT%d" % d, [NTOK, 256], F32, kind=skind) for d in range(2)]
    C.YT_bufs = [[Buf("YT%d_%d" % (d, t)) for t in range(NT)] for d in range(2)]
    C.YTG_ap = [P.dram("YTG%d" % d, [NTOK, 256], F32, kind=skind) for d in range(2)]
    C.YTG_bufs = [[Buf("YTG%d_%d" % (d, t)) for t in range(NT)] for d in range(2)]
    C.Gt_ap = P.dram("Gt", [4096, NTOK], BF16, kind=skind)
    C.Gt_bufs = [Buf("Gt%d" % b) for b in range(9)]
    C.H2_ap = P.dram("H2", [128, 8, NTOK], BF16, kind=skind)
    C.H2_buf = Buf("H2")

    G = Arena(P)
    C.G = G
    C.psall = nc.alloc_psum_tensor("psall", [128, 4096], F32).ap()
    C.psb = [Buf("ps%d" % i) for i in range(8)]
    C.ps = [V(C.psall[:, i * 512:(i + 1) * 512], C.psb[i]) for i in range(8)]
    C.ident = G.sb("ident", [128, 128])
    dma(P, "sp", C.ident, I["ident"])

    for l in range(DEPTH):
        layer(C, l)
        if dbg.get("stop_layer") == l:
            break
    if not dbg.get("stop"):
        final_norm(C)
    P.barrier()
    return nc


def urow(t):
    return 1 + t * 128 if t < 2 else 258 + (t - 2) * 128


def xsrc(C, l, t):
    if l == 0 and not C.__dict__.get("x_in_scratch"):
        if t < 2:
            return C.I["ctxb"][t * 128:(t + 1) * 128, :]
        return C.I["xb"][(t - 2) * 128:(t - 1) * 128, :]
    return C.xres[t]


def phase_ada(C, l, L):
    P, I = C.P, C.I
    A = Arena(P)
    cc = A.sb("cc", [128, 8, 2])
    sc = A.sb("sc", [128, 8, 2])
    screp = A.sb("screp", [128, 8, 2, 128])
    bfm = A.sb("bfm", [128, 48])
    g1 = A.sb("g1", [128, 8])
    g2 = A.sb("g2", [128, 8])
    wst = [A.sb("wst%d" % i, [128, 8, 512]) for i in range(2)]
    brow = [A.sb("brow%d" % i, [128, 1024]) for i in range(2)]
    dma(P, "sp", cc, I["cc"])
    dma(P, "sp", bfm, I["b_ada_fm"][l])
    dma(P, "sp", g1, I["g1_fm"][l])
    dma(P, "sp", g2, I["g2_fm"][l])
    for ii, i in enumerate((2, 5)):
        dma(P, "pool", brow[ii], I["b_ada"][l:l + 1, i * 1024:(i + 1) * 1024].bc([128, 1024]))
    act(P, sc, cc, AF.Silu)
    for k in range(8):
        for j in range(2):
            cp(P, "dve", screp[:, k, j, :], sc[:, k, j:j + 1].bc([128, 128]))
    wv = I["w_ada"][l].rr("(k p) n -> p k n", p=128)
    psA = C.ps[0]
    for c in range(12):
        w = wst[c % 2]
        dma(P, "sp" if c % 2 == 0 else "pool", w, wv[:, :, c * 512:(c + 1) * 512])
        for mi in range(4):
            m = c * 4 + mi
            for k in range(8):
                mm(P, psA[:, m * 2:(m + 1) * 2], w[:, k, mi * 128:(mi + 1) * 128], sc[:, k, :], k == 0, k == 7)
        if c in (4, 5, 10, 11):
            ii = 0 if c < 6 else 1
            half = c % 2
            for j in range(2):
                pr = C.ps[1 + j]
                for k in range(8):
                    mm(P, pr, screp[:, k, j, :], w[:, k, :], k == 0, k == 7)
                tt(P, "dve", L.grow[ii][j][:, half * 512:(half + 1) * 512], pr,
                   brow[ii][:, half * 512:(half + 1) * 512], ALU.add)
    tt(P, "dve", L.mod, psA[:, 0:96].rr("p (m j) -> p m j", j=2), bfm[:, :, None].bc([128, 48, 2]), ALU.add)
    ts(P, "dve", L.sc1, L.mod[:, 8:16, :], 1.0, ALU.add)
    tt(P, "dve", L.sc1, L.sc1, g1[:, :, None].bc([128, 8, 2]), ALU.mult)
    ts(P, "dve", L.sc2, L.mod[:, 32:40, :], 1.0, ALU.add)
    tt(P, "dve", L.sc2, L.sc2, g2[:, :, None].bc([128, 8, 2]), ALU.mult)
    A.close()


def phase_norm(C, l, L, which, hfm, per_tile=None):
    P = C.P
    A = Arena(P)
    sc = L.sc1 if which == 1 else L.sc2
    shb = 0 if which == 1 else 24
    xt = [A.sb("xt%d" % i, [128, D]) for i in range(2)]
    junk = A.sb("junk", [128, D])
    st = [A.sb("st%d" % i, [128, 2]) for i in range(2)]
    hf = [A.sb("hf%d" % i, [128, 8, 128]) for i in range(2)] if per_tile else None
    for t in range(NT):
        j = 1 if t < 2 else 0
        x = xt[t % 2]
        s = st[t % 2]
        dma(P, "sp" if t % 2 == 0 else "pool", x, xsrc(C, l, t))
        act(P, junk, x, AF.Square, accum_out=s[:, 0:1])
        ts(P, "dve", s[:, 1:2], s[:, 0:1], 1.0 / D, ALU.mult, EPS, ALU.add)
        act(P, s[:, 1:2], s[:, 1:2], AF.Sqrt)
        P.op("dve", "reciprocal", out=s[:, 1:2], in_=s[:, 1:2])
        ts(P, "dve", x, x, s[:, 1:2], ALU.mult)
        pa, pb = C.ps[2 + 2 * (t % 2)], C.ps[3 + 2 * (t % 2)]
        if C.dbg.get("dump_norm") and t == 2 and which == 1:
            dma(P, "sp", C.dout("d_xn", [128, D]), x)
            dma(P, "sp", C.dout("d_st", [128, 2]), s)
        for k in range(8):
            pp = pa if k < 4 else pb
            tr(P, pp[:, (k % 4) * 128:(k % 4 + 1) * 128], x[:, k * 128:(k + 1) * 128], C.ident)
        if C.dbg.get("dump_norm") and t == 2 and which == 1:
            cp(P, "dve", junk[:, 0:512], pa)
            dma(P, "sp", C.dout("d_pa", [128, 512]), junk[:, 0:512])
        for k in range(8):
            pp = pa if k < 4 else pb
            src = pp[:, (k % 4) * 128:(k % 4 + 1) * 128]
            if per_tile:
                dst = hf[t % 2][:, k, :]
            else:
                dst = hfm[:, k, t * 128:(t + 1) * 128]
            if k % 2 == 0:
                act(P, dst, src, AF.Identity, scale=sc[:, k, j:j + 1], bias=L.mod[:, shb + k, j:j + 1])
            else:
                ts(P, "dve", dst, src, sc[:, k, j:j + 1], ALU.mult, L.mod[:, shb + k, j:j + 1], ALU.add)
        if per_tile:
            for k in range(8):
                cp(P, "act" if k % 2 == 0 else "dve", hfm[:, k, t * 128:(t + 1) * 128], hf[t % 2][:, k, :])
            per_tile(t, hf[t % 2])
    A.close()


def phase_win_tm(C, l, L, hfm):
    P, I = C.P, C.I
    A = Arena(P)
    wA = A.sb("wA", [128, 8, O4], BF16)
    wst = [A.sb("wst%d" % i, [128, 8, 512]) for i in range(2)]
    ust = [A.sb("ust%d" % i, [128, O4]) for i in range(2)]
    zer = A.sb("zer", [1, O4])
    P.op("dve", "memset", ap=zer, constant=0.0)
    for r in (0, 257, NTOK + 2):
        dma(P, "sp", V(C.U_ap[r:r + 1, :], C.U_pad), zer)
    wv = I["w_in"][l].rr("(k p) n -> p k n", p=128)
    blocks = [(0, 512), (512, 1024), (1024, 1536), (1536, 1824), (1824, 2336), (2336, 2592)]
    for bi, (c0, c1) in enumerate(blocks):
        w = wst[bi % 2]
        dma(P, "sp" if bi % 2 == 0 else "pool", w[:, :, 0:c1 - c0], wv[:, :, c0:c1])
        cp(P, "act", wA[:, :, c0:c1], w[:, :, 0:c1 - c0])
    n = 0
    for t in range(NT):
        u = ust[t % 2]
        for bi, (c0, c1) in enumerate(blocks):
            ps = C.ps[n % 4]
            n += 1
            for k in range(8):
                mm(P, ps[:, 0:c1 - c0], hfm[:, k, t * 128:(t + 1) * 128], wA[:, k, c0:c1], k == 0, k == 7)
            cp(P, "act" if bi % 2 == 0 else "dve", u[:, c0:c1], ps[:, 0:c1 - c0])
        r0 = urow(t)
        dma(P, "sp" if t % 2 == 0 else "pool", V(C.U_ap[r0:r0 + 128, :], C.U_bufs[t]), u)
    A.close()


def stt(P, e, out, in0, scalar, in1, op0, op1, **kw):
    return P.op(e, "scalar_tensor_tensor", out=out, in0=in0, scalar=scalar, in1=in1, op0=op0, op1=op1, **kw)


def red(P, e, out, in_, **kw):
    return P.op(e, "tensor_reduce", out=out, in_=in_, axis=AX.X, op=ALU.add, **kw)


def load_bf16(P, A, name, shape, src, q="sp", ce="act"):
    st = A.sb(name + "_f", shape)
    wb = A.sb(name, shape, BF16)
    dma(P, q, st, src)
    cp(P, ce, wb, st)
    return wb


def cust(v, offset_elems, dims):
    ap = v.ap
    base = ap.ap[0]
    new = type(ap)(ap.tensor, ap.offset + offset_elems, [tuple(base)] + [tuple(d) for d in dims])
    return V(new, v.bufs)


def phase_prep(C, l, L):
    P, I = C.P, C.I
    A = Arena(P)
    pv = A.sb("pv", [128, NPV])
    dma(P, "sp", pv, I["pv"][l:l + 1, :].bc([128, NPV]))
    w1cat = load_bf16(P, A, "w1cat", [128, 2, 128], I["w1cat"][l].rr("(k p) n -> p k n", p=128))
    w2blk = load_bf16(P, A, "w2blk", [128, 1024], I["w2blk"][l])
    g1 = load_bf16(P, A, "g1w", [128, 2, 64], I["g1"][l].rr("(k p) n -> p k n", p=128))
    g2 = load_bf16(P, A, "g2w", [64, 256], I["g2"][l])
    a2blk = load_bf16(P, A, "a2blk", [32, 256], I["a2blk"][l])
    mu = pv[:, PV_MU:PV_MU + 1024]
    kkp = pv[:, PV_KK:PV_KK + 256]
    ka = pv[:, PV_KA:PV_KA + 256]
    rkp = pv[:, PV_RK:PV_RK + 256]
    w0 = pv[:, PV_W0:PV_W0 + 512]
    a0 = pv[:, PV_A0:PV_A0 + 512]
    gab = pv[:, PV_GAB:PV_GAB + 256]
    glng = pv[:, PV_GLNG:PV_GLNG + 256]

    uc = [A.sb("uc%d" % i, [128, O2]) for i in range(2)]
    up = [A.sb("up%d" % i, [128, 1024]) for i in range(2)]
    un = [A.sb("un%d" % i, [128, 1024]) for i in range(2)]
    rows = [[A.sb("rows%d%d" % (d, i), [128, 2, 1024], BF16) for i in range(2)] for d in range(2)]
    vt = [A.sb("vt%d" % i, [128, 128, 6]) for i in range(2)]
    fin = [A.sb("fin%d" % i, [128, 768]) for i in range(2)]
    cht = [A.sb("cht%d" % i, [128, 3072]) for i in range(2)]
    t0 = A.sb("t0", [128, 1024])
    mx = A.sb("mx", [128, 1024])
    xaT = A.sb("xaT", [128, 2, 128], BF16)
    z = A.sb("z", [128, 128], BF16)
    sg = A.sb("sg", [64, 128], BF16)
    wl = A.sb("wl", [128, 512])
    wdec = A.sb("wdec", [128, 512])
    il = A.sb("il", [128, 512])
    iclr = A.sb("iclr", [128, 512])
    kk0 = A.sb("kk0", [128, 256])
    sq = A.sb("sq", [128, 256])
    ss = A.sb("ss", [128, 8])
    kk = A.sb("kk", [128, 256])
    t1 = A.sb("t1", [128, 512])
    keff = A.sb("keff", [128, 512])
    bb = A.sb("bb", [128, 512])
    rkt = A.sb("rkt", [128, 256])
    alT = A.sb("alT", [32, 128], BF16)
    gl = A.sb("gl", [128, 256])
    gdec = A.sb("gdec", [128, 256])
    sr = A.sb("sr", [128, 256])

    def rv(R, c0, n):
        return R[:, :, c0:c0 + n]

    for t in range(NT):
        i = t % 2
        r0 = urow(t)
        nb = [C.U_bufs[t]]
        if t > 0:
            nb.append(C.U_bufs[t - 1])
        if t < NT - 1:
            nb.append(C.U_bufs[t + 1])
        nb.append(C.U_pad)
        dma(P, "sp", uc[i], V(C.U_ap[r0:r0 + 128, 0:O2], C.U_bufs[t]))
        dma(P, "pool", up[i], V(C.U_ap[r0 - 1:r0 + 127, 0:1024], tuple(nb)))
        dma(P, "sp", un[i], V(C.U_ap[r0 + 1:r0 + 129, 0:1024], tuple(nb)))
        u = uc[i]
        R0, R1 = rows[0][i], rows[1][i]
        F = fin[i]
        tt(P, "dve", t0, up[i], un[i], ALU.add)
        stt(P, "dve", t0, t0, 0.5, u[:, 0:1024], ALU.mult, ALU.subtract)
        tt(P, "dve", t0, t0, mu, ALU.mult)
        tt(P, "dve", mx, t0, u[:, 0:1024], ALU.add)
        r_, k_, v_, xa_ = mx[:, 0:256], mx[:, 256:512], mx[:, 512:768], mx[:, 768:1024]
        pT = C.ps[0]
        for kt in range(2):
            tr(P, pT[:, kt * 128:(kt + 1) * 128], xa_[:, kt * 128:(kt + 1) * 128], C.ident)
        cp(P, "act", xaT, pT[:, 0:256].rr("p (k n) -> p k n", k=2))
        pz = C.ps[1]
        for kt in range(2):
            mm(P, pz[:, 0:128], w1cat[:, kt, :], xaT[:, kt, :], kt == 0, kt == 1)
        for kt in range(2):
            mm(P, pz[0:64, 128:256], g1[:, kt, :], xaT[:, kt, :], kt == 0, kt == 1)
        act(P, z[0:64, :], pz[0:64, 0:128], AF.Tanh)
        cp(P, "dve", z[64:128, :], pz[64:128, 0:128])
        act(P, sg, pz[0:64, 128:256], AF.Sigmoid)
        pw, pa_, pg = C.ps[2], C.ps[3], C.ps[4]
        mm(P, pw, z, w2blk[:, 0:512], True, True)
        mm(P, pa_, z, w2blk[:, 512:1024], True, True)
        mm(P, pg[:, 0:256], sg, g2, True, True)
        tt(P, "dve", wl, pw, w0, ALU.add)
        act(P, wl, wl, AF.Sigmoid)
        act(P, wdec, wl, AF.Exp, scale=-0.6065306597126334)
        tt(P, "dve", il, pa_, a0, ALU.add)
        act(P, iclr, il, AF.Sigmoid)
        cp(P, "act", F[:, 0:256], pg[:, 0:256])
        tt(P, "dve", kk0, k_, kkp, ALU.mult)
        tt(P, "dve", sq, kk0, kk0, ALU.mult)
        red(P, "dve", ss[:, 0:4], sq.rr("p (h k) -> p h k", h=4))
        ts(P, "dve", ss[:, 0:4], ss[:, 0:4], EPS, ALU.add)
        act(P, ss[:, 0:4], ss[:, 0:4], AF.Sqrt)
        P.op("dve", "reciprocal", out=ss[:, 0:4], in_=ss[:, 0:4])
        tt(P, "dve", kk.rr("p (h k) -> p h k", h=4), kk0.rr("p (h k) -> p h k", h=4),
           ss[:, 0:4][:, :, None].bc([128, 4, 64]), ALU.mult)
        ic3 = iclr.rr("p (d c) -> p d c", d=2)
        stt(P, "dve", t1.rr("p (d c) -> p d c", d=2), ic3, -1.0, ka[:, None, :].bc([128, 2, 256]), ALU.add, ALU.mult)
        stt(P, "dve", keff.rr("p (d c) -> p d c", d=2), t1.rr("p (d c) -> p d c", d=2), 1.0,
            k_[:, None, :].bc([128, 2, 256]), ALU.add, ALU.mult)
        tt(P, "dve", bb.rr("p (d c) -> p d c", d=2), ic3, kk[:, None, :].bc([128, 2, 256]), ALU.mult)
        tt(P, "dve", rkt, r_, k_, ALU.mult)
        tt(P, "dve", rkt, rkt, rkp, ALU.mult)
        red(P, "dve", ss[:, 4:8], rkt.rr("p (h k) -> p h k", h=4))
        tt(P, "dve", F[:, 256:512].rr("p (h k) -> p h k", h=4), v_.rr("p (h k) -> p h k", h=4),
           ss[:, 4:8][:, :, None].bc([128, 4, 64]), ALU.mult)
        pT2 = C.ps[5]
        tr(P, pT2[0:32, 0:128], u[:, O1 + 768:O1 + 800], C.ident)
        cp(P, "act", alT, pT2[0:32, 0:128])
        mm(P, pT2[:, 128:384], alT, a2blk, True, True)
        tt(P, "dve", gl, pT2[:, 128:384], gab, ALU.add)
        act(P, gl, gl, AF.Sigmoid)
        act(P, gl, gl, AF.Ln)
        if C.dbg.get("old_gla"):
            act(P, gdec, gl, AF.Exp, scale=1.0 / 16.0)
        act(P, sr, u[:, O1 + 512:O1 + 768], AF.Silu)
        tt(P, "dve", F[:, 512:768], sr, glng, ALU.mult)
        CHt = cht[i]
        ts(P, "dve", CHt[:, 0:512], wl, -0.6065306597126334, ALU.mult)
        cp(P, "act", CHt[:, 512:1024], keff)
        cp(P, "dve", CHt[:, 1024:1536], bb)
        ts(P, "dve", CHt[:, 1536:1792], kk, -1.0, ALU.mult)
        cp(P, "act", CHt[:, 1792:2048], r_)
        cp(P, "dve", CHt[:, 2048:2304], v_)
        ts(P, "dve", CHt[:, 2304:2560], gl, 1.0 / 16.0, ALU.mult)
        ts(P, "dve", CHt[:, 2560:2688], u[:, O1:O1 + 128], 32.0 ** -0.5, ALU.mult)
        cp(P, "act", CHt[:, 2688:2816], u[:, O1 + 128:O1 + 256])
        cp(P, "dve", CHt[:, 2816:3072], u[:, O1 + 256:O1 + 512])
        dma(P, "sp", V(C.CH_ap[t * 128:(t + 1) * 128, :], C.CH_bufs[t]), CHt)
        if not C.dbg.get("old_gla"):
            dma(P, "pool", V(C.FIN_ap[t * 128:(t + 1) * 128, :], C.FIN_bufs[t]), F)
            continue
        for d, R in ((0, R0), (1, R1)):
            e1 = "dve" if d == 0 else "pool"
            e2 = "pool" if d == 0 else "dve"
            src = wdec[:, d * 256:(d + 1) * 256].rr("p (a h k) -> p a h k", a=2, h=2)
            hi = rv(R, 0, 128).rr("p h (a k) -> p a h k", a=2)
            lo = rv(R, 192, 128).rr("p h (a k) -> p a h k", a=2)
            cp(P, e1, hi, src)
            tt(P, e1, lo, src, hi, ALU.subtract)
            gsrc = gdec[:, d * 128:(d + 1) * 128].rr("p (a h k) -> p a h k", a=2, h=2)
            ghi = rv(R, 128, 64).rr("p h (a k) -> p a h k", a=2)
            glo = rv(R, 320, 64).rr("p h (a k) -> p a h k", a=2)
            cp(P, e2, ghi, gsrc)
            tt(P, e2, glo, gsrc, ghi, ALU.subtract)
            cp(P, e1, rv(R, 384, 128).rr("p h (a k) -> p a h k", a=2),
               keff[:, d * 256:(d + 1) * 256].rr("p (a h k) -> p a h k", a=2, h=2))
            cp(P, e2, rv(R, 512, 64).rr("p h (a k) -> p a h k", a=2),
               u[:, O1 + 128:O1 + 256].rr("p (a h k) -> p a h k", a=2, h=2))
            cp(P, e1, rv(R, 576, 128).rr("p h (a k) -> p a h k", a=2), r_.rr("p (a h k) -> p a h k", a=2, h=2))
            ts(P, e2, rv(R, 704, 64).rr("p h (a k) -> p a h k", a=2),
               u[:, O1:O1 + 128].rr("p (a h k) -> p a h k", a=2, h=2), 32.0 ** -0.5, ALU.mult)
            ts(P, e1, rv(R, 768, 128).rr("p h (a k) -> p a h k", a=2), kk.rr("p (a h k) -> p a h k", a=2, h=2),
               -1.0, ALU.mult)
            cp(P, e2, rv(R, 896, 128).rr("p h (a k) -> p a h k", a=2),
               bb[:, d * 256:(d + 1) * 256].rr("p (a h k) -> p a h k", a=2, h=2))
            dma(P, "sp" if d == 0 else "pool",
                V(C.STR_ap[d][t * 128:(t + 1) * 128].rearrange("t h n -> t (h n)"), C.STR_bufs[d][t]),
                R.rr("p h n -> p (h n)"))
        pv4 = C.ps[6]
        for a in range(2):
            tr(P, pv4[:, a * 128:(a + 1) * 128], v_[:, a * 128:(a + 1) * 128], C.ident)
        for a in range(2):
            tr(P, pv4[:, (2 + a) * 128:(3 + a) * 128], u[:, O1 + 256 + a * 128:O1 + 384 + a * 128], C.ident)
        VT = vt[i]
        cp(P, "act", VT[:, :, 0], pv4[:, 0:128])
        cp(P, "dve", VT[:, :, 1], pv4[:, 0:128])
        cp(P, "act", VT[:, :, 2], pv4[:, 128:256])
        cp(P, "dve", VT[:, :, 3], pv4[:, 128:256])
        cp(P, "act", VT[:, :, 4], pv4[:, 256:384])
        cp(P, "dve", VT[:, :, 5], pv4[:, 384:512])
        dma(P, "sp", V(C.Vs_ap[:, t * 128:(t + 1) * 128, :], C.Vs_bufs[t]), VT)
        dma(P, "pool", V(C.FIN_ap[t * 128:(t + 1) * 128, :], C.FIN_bufs[t]), F)
    A.close()


def phase_scan(C, l, nchunks=None):
    P, I = C.P, C.I
    A = Arena(P)
    S = A.sb("S", [128, 2, 64])
    T3 = A.sb("T3", [128, 2, 64])
    T4 = A.sb("T4", [128, 2, 64])
    self_f = A.sb("sel_f", [128, 64, 128])
    sel = A.sb("sel", [128, 64, 128], BF16)
    dma(P, "sp", self_f, I["sel"])
    cp(P, "dve", sel, self_f)
    rows = [[A.sb("srow%d%d" % (d, i), [128, 1024], BF16) for i in range(2)] for d in range(2)]
    vb = [A.sb("vb%d" % i, [128, 2, 64, 6]) for i in range(2)]
    yb = [A.sb("yb%d" % i, [128, 2, 64, 6]) for i in range(2)]
    P.op("dve", "memset", ap=S, constant=0.0)
    for i in range(2):
        P.op("pool", "memset", ap=yb[i], constant=0.0)
    NCH = NTOK // 64
    for c in range(NCH if nchunks is None else nchunks):
        zf = c * 64
        zb = (192 - 64 * c) if c < 4 else (4544 - 64 * c)
        i = c % 2
        dma(P, "sp", rows[0][i], V(C.STR_ap[0][zf:zf + 64].rearrange("t h n -> (t h) n"), C.STR_bufs[0][zf // 128]))
        dma(P, "pool", rows[1][i], V(C.STR_ap[1][zb:zb + 64].rearrange("t h n -> (t h) n"), C.STR_bufs[1][zb // 128]))
        dma(P, "sp", vb[i][:, 0], V(C.Vs_ap[:, zf:zf + 64, :], C.Vs_bufs[zf // 128]))
        dma(P, "pool", vb[i][:, 1], V(C.Vs_ap[:, zb:zb + 64, :], C.Vs_bufs[zb // 128]))
        YB = yb[i]
        for j in range(64):
            s = c * 64 + j
            pb = (s % 2) * 2
            for d in range(2):
                jj = j if d == 0 else 63 - j
                lt = sel[:, jj, :]
                R = rows[d][i]
                bx = C.ps[pb + d]
                mm(P, bx[:, 0:64], lt, R[:, 128:192], True, False, mark=False)
                mm(P, bx[:, 0:64], lt, R[:, 320:384], False, True, mark=False)
                mm(P, bx[:, 64:128], lt, R[:, 512:576], True, True, mark=False)
                mm(P, bx[:, 128:192], lt, R[:, 704:768], True, True, mark=(d == 1))
            R4 = V(C.psall[:, pb * 512:pb * 512 + 1024].rearrange("p (d x) -> p d x", d=2), tuple(C.psb[pb:pb + 2]))
            Dv, KKv, RQv = R4[:, :, 0:64], R4[:, :, 64:128], R4[:, :, 128:192]
            P.op("dve", "tensor_tensor", lax=True, out=S, in0=S, in1=Dv, op=ALU.mult)
            vv = cust(vb[i], j * 6 + 4, [((127 - 2 * j) * 6, 2), (1, 2), (0, 32)])
            P.op("dve", "tensor_tensor", lax=True, out=T3.rr("p d (g k) -> p d g k", g=2),
                 in0=KKv.rr("p d (g k) -> p d g k", g=2), in1=vv, op=ALU.mult)
            P.op("dve", "tensor_tensor", lax=True, out=S, in0=S, in1=T3, op=ALU.add)
            P.op("dve", "tensor_tensor", lax=True, out=T4, in0=S, in1=RQv, op=ALU.mult)
            yv = cust(YB, j * 6 + 4, [((127 - 2 * j) * 6, 2), (1, 2)])
            P.op("dve", "tensor_reduce", lax=True, out=yv, in_=T4.rr("p d (g k) -> p d g k", g=2), axis=AX.X, op=ALU.add)
        dma(P, "sp", V(C.Y_ap[0][:, zf:zf + 64, :], C.Y_bufs[0][zf // 64]), YB[:, 0])
        dma(P, "pool", V(C.Y_ap[1][:, zb:zb + 64, :], C.Y_bufs[1][zb // 64]), YB[:, 1])
    A.close()


def phase_fin_ab(C, l, L):
    P, I = C.P, C.I
    A = Arena(P)
    pv = A.sb("pv", [128, NPV])
    dma(P, "sp", pv, I["pv"][l:l + 1, :].bc([128, NPV]))
    lng = pv[:, PV_LNG:PV_LNG + 256]
    yt = [A.sb("yt%d" % i, [128, 2, 128, 6]) for i in range(2)]
    fin = [A.sb("finf%d" % i, [128, 768]) for i in range(2)]
    ya = A.sb("ya", [128, 256])
    ytm = [A.sb("ytm%d" % i, [128, 2, 256]) for i in range(2)]
    ytg = [A.sb("ytg%d" % i, [128, 2, 256]) for i in range(2)]
    yg = A.sb("yg", [128, 256])
    sq = A.sb("sqf", [128, 256])
    st = A.sb("stf", [128, 16])
    ob = [A.sb("ob%d" % i, [128, 4, 128], BF16) for i in range(2)]
    for t in range(NT):
        i = t % 2
        Y = yt[i]
        F = fin[i]
        if C.dbg.get("old_gla"):
            for d in range(2):
                dma(P, "sp" if d == 0 else "pool", Y[:, d],
                    V(C.Y_ap[d][:, t * 128:(t + 1) * 128, :], (C.Y_bufs[d][2 * t], C.Y_bufs[d][2 * t + 1])))
        dma(P, "sp", F, V(C.FIN_ap[t * 128:(t + 1) * 128, :], C.FIN_bufs[t]))
        pr, pg = C.ps[0], C.ps[1]
        if C.dbg.get("old_rwkv"):
            for a in range(2):
                n = 0
                for d in range(2):
                    for g in (2 * a, 2 * a + 1):
                        mm(P, pr[:, a * 128:(a + 1) * 128], Y[:, d, :, g], C.ident, n == 0, n == 3)
                        n += 1
        if C.dbg.get("old_gla"):
            for a in range(2):
                for d in range(2):
                    mm(P, pg[:, a * 128:(a + 1) * 128], Y[:, d, :, 4 + a], C.ident, d == 0, d == 1)
        if C.dbg.get("old_rwkv"):
            cp(P, "act", ya, pr[:, 0:256])
        else:
            for d in range(2):
                dma(P, "sp" if d == 0 else "pool", ytm[i][:, d, :], V(C.YT_ap[d][t * 128:(t + 1) * 128, :], C.YT_bufs[d][t]))
            tt(P, "dve", ya, ytm[i][:, 0, :], ytm[i][:, 1, :], ALU.add)
        ya4 = ya.rr("p (h k) -> p h k", h=4)
        red(P, "dve", st[:, 0:4], ya4)
        ts(P, "dve", st[:, 0:4], st[:, 0:4], 1.0 / 64.0, ALU.mult)
        tt(P, "dve", ya4, ya4, st[:, 0:4][:, :, None].bc([128, 4, 64]), ALU.subtract)
        tt(P, "dve", sq, ya, ya, ALU.mult)
        red(P, "dve", st[:, 4:8], sq.rr("p (h k) -> p h k", h=4))
        ts(P, "dve", st[:, 4:8], st[:, 4:8], 1.0 / 64.0, ALU.mult, GN_EPS, ALU.add)
        act(P, st[:, 4:8], st[:, 4:8], AF.Sqrt)
        P.op("dve", "reciprocal", out=st[:, 4:8], in_=st[:, 4:8])
        tt(P, "dve", ya4, ya4, st[:, 4:8][:, :, None].bc([128, 4, 64]), ALU.mult)
        tt(P, "dve", ya, ya, lng, ALU.mult)
        tt(P, "dve", ya, ya, F[:, 256:512], ALU.add)
        tt(P, "dve", ya, ya, F[:, 0:256], ALU.mult)
        if C.dbg.get("old_gla"):
            cp(P, "act", yg, pg[:, 0:256])
        else:
            for d in range(2):
                dma(P, "sp" if d == 0 else "pool", ytg[i][:, d, :], V(C.YTG_ap[d][t * 128:(t + 1) * 128, :], C.YTG_bufs[d][t]))
            tt(P, "dve", yg, ytg[i][:, 0, :], ytg[i][:, 1, :], ALU.add)
        yg4 = yg.rr("p (h k) -> p h k", h=4)
        tt(P, "dve", sq, yg, yg, ALU.mult)
        red(P, "dve", st[:, 8:12], sq.rr("p (h k) -> p h k", h=4))
        ts(P, "dve", st[:, 8:12], st[:, 8:12], 1.0 / 64.0, ALU.mult, EPS, ALU.add)
        act(P, st[:, 8:12], st[:, 8:12], AF.Sqrt)
        P.op("dve", "reciprocal", out=st[:, 8:12], in_=st[:, 8:12])
        tt(P, "dve", yg4, yg4, st[:, 8:12][:, :, None].bc([128, 4, 64]), ALU.mult)
        tt(P, "dve", yg, yg, F[:, 512:768], ALU.mult)
        po = C.ps[2]
        for a in range(2):
            tr(P, po[:, a * 128:(a + 1) * 128], ya[:, a * 128:(a + 1) * 128], C.ident)
            tr(P, po[:, (2 + a) * 128:(3 + a) * 128], yg[:, a * 128:(a + 1) * 128], C.ident)
        OB = ob[i]
        cp(P, "act", OB, po.rr("p (a n) -> p a n", a=4))
        for br in range(2):
            dma(P, "sp" if br == 0 else "pool",
                V(C.YB_ap[br][:, t * 128:(t + 1) * 128].rearrange("(a p) n -> p a n", p=128), C.YB_bufs[br][t]),
                OB[:, 2 * br:2 * br + 2, :])
    A.close()


def phase_attn(C, l, L):
    P, I = C.P, C.I
    A = Arena(P)
    qT = A.sb("qT", [128, 2, NTOK], BF16)
    kT = A.sb("kT", [128, 2, NTOK], BF16)
    vtm = A.sb("vtm", [128, NT, 128], BF16)
    maskw = A.sb("maskw", [128, 384])
    sink = A.sb("sink", [128, 16])
    identb = A.sb("identb", [128, 128], BF16)
    dma(P, "sp", maskw, I["maskw"])
    dma(P, "sp", sink, I["attn_sink"][l:l + 1, :].bc([128, 16]))
    cp(P, "dve", identb, C.ident)
    ua = [A.sb("ua%d" % i, [128, 512]) for i in range(2)]
    rc = [A.sb("rc%d" % i, [128, 32]) for i in range(2)]
    rs = [A.sb("rs%d" % i, [128, 32]) for i in range(2)]
    qk = A.sb("qk", [128, 6, 64])
    tmp = A.sb("tmpr", [128, 6, 32])
    kd = A.sb("kd", [128, 2, 2, 64])
    cut = C.dbg.get("attn_cut", 9)
    for t in range(NT if cut > 1 else 0):
        i = t % 2
        r0 = urow(t)
        u = ua[i]
        dma(P, "sp", u, V(C.U_ap[r0:r0 + 128, O2:O3], C.U_bufs[t]))
        u6 = u[:, 0:384].rr("p (h d) -> p h d", h=6)
        if t >= 2 and not C.dbg.get("norope"):
            dma(P, "pool", rc[i], I["ropec"][(t - 2) * 128:(t - 1) * 128, :])
            dma(P, "pool", rs[i], I["ropes"][(t - 2) * 128:(t - 1) * 128, :])
            cb = rc[i][:, None, :].bc([128, 6, 32])
            sb_ = rs[i][:, None, :].bc([128, 6, 32])
            z1, z2 = u6[:, :, 0:32], u6[:, :, 32:64]
            tt(P, "dve", qk[:, :, 0:32], z1, cb, ALU.mult)
            tt(P, "dve", tmp, z2, sb_, ALU.mult)
            tt(P, "dve", qk[:, :, 0:32], qk[:, :, 0:32], tmp, ALU.subtract)
            tt(P, "dve", qk[:, :, 32:64], z1, sb_, ALU.mult)
            tt(P, "dve", tmp, z2, cb, ALU.mult)
            tt(P, "dve", qk[:, :, 32:64], qk[:, :, 32:64], tmp, ALU.add)
        else:
            cp(P, "dve", qk, u6)
        if cut < 3:
            continue
        cp(P, "dve", kd[:, :, 0, :], qk[:, 4:6, :])
        cp(P, "dve", kd[:, :, 1, :], qk[:, 4:6, :])
        cp(P, "dve", vtm[:, t, :], u[:, 384:512])
        if cut < 4:
            continue
        pq = C.ps[t % 2]
        qf = qk.rr("p h d -> p (h d)")
        kf = kd.rr("p k r d -> p (k r d)")
        for a in range(2):
            tr(P, pq[:, a * 128:(a + 1) * 128], qf[:, a * 128:(a + 1) * 128], C.ident)
            tr(P, pq[:, (2 + a) * 128:(3 + a) * 128], kf[:, a * 128:(a + 1) * 128], C.ident)
        ts(P, "dve", qT[:, :, t * 128:(t + 1) * 128], pq[:, 0:256].rr("p (a n) -> p a n", a=2), 0.125, ALU.mult)
        cp(P, "dve", kT[:, :, t * 128:(t + 1) * 128], pq[:, 256:512].rr("p (a n) -> p a n", a=2))
    if C.dbg.get("attn_p1"):
        A.close()
        return
    sc = [A.sb("sc%d" % i, [128, 640]) for i in range(2)]
    pb = [A.sb("pb%d" % i, [128, 640], BF16) for i in range(2)]
    pTs = [A.sb("pTs%d" % i, [128, 5, 128], BF16) for i in range(2)]
    st = [A.sb("sta%d" % i, [128, 8]) for i in range(2)]
    yo = [A.sb("yo%d" % i, [128, 256]) for i in range(2)]
    oc = [A.sb("oc%d" % i, [128, 2, 128], BF16) for i in range(2)]
    psT = [V(C.psall[:, b * 512:(b + 1) * 512].bitcast(BF16), C.psb[b]) for b in (4, 5)]
    n = 0
    for t in range(NT):
        YO = yo[t % 2]
        if t >= 2:
            lo, hi = max(t - 1, 2), min(t + 1, NT - 1)
            nw = hi - lo + 1
            m0 = (lo - (t - 1)) * 128
        else:
            nw = 0
        nk = nw * 128 + 256
        nblk = nw + 2
        kblocks = ([lo + b for b in range(nw)] if nw else []) + [0, 1]
        po = C.ps[6 + (t % 2)]
        for h in range(4):
            kv, hl = h // 2, h % 2
            i = n % 2
            n += 1
            S_, Pb, PT, ST = sc[i], pb[i], pTs[i], st[i]
            qv = qT[hl * 64:(hl + 1) * 64, kv, t * 128:(t + 1) * 128]
            pa_, pc_ = C.ps[2 * i], C.ps[2 * i + 1]
            if nw:
                mm(P, pa_[:, 0:nw * 128], qv, kT[hl * 64:(hl + 1) * 64, kv, lo * 128:(hi + 1) * 128], True, True)
            mm(P, pc_[:, 0:256], qv, kT[hl * 64:(hl + 1) * 64, kv, 0:256], True, True)
            if nw:
                tt(P, "dve", S_[:, 0:nw * 128], pa_[:, 0:nw * 128], maskw[:, m0:m0 + nw * 128], ALU.add)
            cp(P, "act", S_[:, nw * 128:nk], pc_[:, 0:256])
            P.op("dve", "tensor_reduce", out=ST[:, 0:1], in_=S_[:, 0:nk], axis=AX.X, op=ALU.max)
            tt(P, "dve", ST[:, 0:1], ST[:, 0:1], sink[:, h:h + 1], ALU.max)
            ts(P, "dve", ST[:, 1:2], ST[:, 0:1], -1.0, ALU.mult)
            act(P, Pb[:, 0:nk], S_[:, 0:nk], AF.Exp, bias=ST[:, 1:2], accum_out=ST[:, 2:3])
            act(P, ST[:, 3:4], sink[:, h:h + 1], AF.Exp, bias=ST[:, 1:2])
            tt(P, "dve", ST[:, 4:5], ST[:, 2:3], ST[:, 3:4], ALU.add)
            P.op("dve", "reciprocal", out=ST[:, 5:6], in_=ST[:, 4:5])
            pt = psT[i]
            for b in range(nblk):
                tr(P, pt[:, b * 128:(b + 1) * 128], Pb[:, b * 128:(b + 1) * 128], identb)
            cp(P, "act" if h % 2 == 0 else "dve", PT[:, 0:nblk, :], pt[:, 0:nblk * 128].rr("p (b n) -> p b n", b=nblk))
            for b in range(nblk):
                mm(P, po[:, h * 64:(h + 1) * 64], PT[:, b, :], vtm[:, kblocks[b], kv * 64:(kv + 1) * 64],
                   b == 0, b == nblk - 1)
            ts(P, "dve", YO[:, h * 64:(h + 1) * 64], po[:, h * 64:(h + 1) * 64], ST[:, 5:6], ALU.mult)
        pf = C.ps[t % 2]
        for a in range(2):
            tr(P, pf[:, a * 128:(a + 1) * 128], YO[:, a * 128:(a + 1) * 128], C.ident)
        OC = oc[t % 2]
        cp(P, "act", OC, pf[:, 0:256].rr("p (a n) -> p a n", a=2))
        dma(P, "sp", V(C.YB_ap[2][:, t * 128:(t + 1) * 128].rearrange("(a p) n -> p a n", p=128), C.YB_bufs[2][t]), OC)
    A.close()


PI = math.pi
S5_BLOCKS = [(0, 256)] + [(256 + 512 * i, 512) for i in range(8)]


I32 = mybir.dt.int32
TWO_PI_HI = 6.28125
TWO_PI_LO = 2.0 * math.pi - 6.28125


def sincos(P, A, s_out, c_out, x, shape, tag):
    qi = A.sb("qi" + tag, shape, I32)
    kf = A.sb("kf" + tag, shape)
    r = A.sb("rr" + tag, shape)
    m = A.sb("mm" + tag, shape)
    for extra, out in ((0.0, s_out), (0.5 * PI, c_out)):
        ts(P, "dve", r, x, 16.0 * PI + extra, ALU.add)
        ts(P, "dve", qi, r, 1.0 / (2.0 * PI), ALU.mult)
        cp(P, "dve", kf, qi)
        stt(P, "dve", r, kf, -TWO_PI_HI, r, ALU.mult, ALU.add)
        stt(P, "dve", r, kf, -TWO_PI_LO, r, ALU.mult, ALU.add)
        ts(P, "dve", m, r, PI, ALU.is_gt)
        stt(P, "dve", r, m, -2.0 * PI, r, ALU.mult, ALU.add)
        ts(P, "dve", m, r, -PI, ALU.is_lt)
        stt(P, "dve", r, m, 2.0 * PI, r, ALU.mult, ALU.add)
        ts(P, "dve", r, r, PI, ALU.min, -PI, ALU.max)
        act(P, out, r, AF.Sin)


def phase_s5(C, l, L):
    P, I = C.P, C.I
    A = Arena(P)
    th_s = A.sb("th_s", [128, 2, 8])
    rho_s = A.sb("rho_s", [128, 2, 8])
    Ck = A.sb("Ck", [128, 2, 8, 13])
    Sk = A.sb("Sk", [128, 2, 8, 13])
    BbT = A.sb("BbT", [128, 2, 2, 8, 128], BF16)
    CT = A.sb("CT", [128, 2, 2, 8, 128], BF16)
    A0 = A
    A = Arena(P)
    tmpA = [A.sb("s5r%d" % i, [128, 1024]) for i in range(8)]
    lre, lim, ldt, t_s, t_c, t_a, t_b, t_d = tmpA
    pw2f = A.sb("pw2", [128, 16])
    dma(P, "sp", pw2f, I["pw2"][0:1, :].bc([128, 16]))
    pw2 = pw2f[:, 0:13]
    fR = [A.sb("fR%d" % d, [128, 1024]) for d in range(2)]
    fI = [A.sb("fI%d" % d, [128, 1024]) for d in range(2)]
    sm = A.sb("sm", [128, 2, 3, 8])
    dma(P, "sp", sm, I["s5_sm"][l])
    act(P, sm[:, :, 2, :], sm[:, :, 2, :], AF.Exp)
    tt(P, "dve", th_s, sm[:, :, 1, :], sm[:, :, 2, :], ALU.mult)
    tt(P, "dve", rho_s, sm[:, :, 0, :], sm[:, :, 2, :], ALU.mult)
    act(P, rho_s, rho_s, AF.Exp)
    ang13 = A.sb("ang13", [128, 16, 13])
    tt(P, "dve", ang13, th_s.rr("p d j -> p (d j)")[:, :, None].bc([128, 16, 13]), pw2[:, None, :].bc([128, 16, 13]), ALU.mult)
    sincos(P, A, Sk.rr("p d j k -> p (d j) k"), Ck.rr("p d j k -> p (d j) k"), ang13, [128, 16, 13], "k")
    for d in range(2):
        dma(P, "sp", lre, I["s5_rows"][l, d, 0:1, :].bc([128, 1024]))
        dma(P, "pool", lim, I["s5_rows"][l, d, 1:2, :].bc([128, 1024]))
        dma(P, "sp", ldt, I["s5_rows"][l, d, 2:3, :].bc([128, 1024]))
        act(P, ldt, ldt, AF.Exp)
        tt(P, "dve", t_a, lim, ldt, ALU.mult)
        sincos(P, A, t_s, t_c, t_a, [128, 1024], "r%d" % d)
        tt(P, "dve", t_a, lre, ldt, ALU.mult)
        act(P, t_a, t_a, AF.Exp)
        tt(P, "dve", t_c, t_c, t_a, ALU.mult)
        tt(P, "dve", t_s, t_s, t_a, ALU.mult)
        ts(P, "dve", t_c, t_c, -1.0, ALU.add)
        tt(P, "dve", t_a, lre, lre, ALU.mult)
        tt(P, "dve", t_b, lim, lim, ALU.mult)
        tt(P, "dve", t_a, t_a, t_b, ALU.add)
        P.op("dve", "reciprocal", out=t_a, in_=t_a)
        tt(P, "dve", t_b, t_c, lre, ALU.mult)
        tt(P, "dve", t_d, t_s, lim, ALU.mult)
        tt(P, "dve", t_b, t_b, t_d, ALU.add)
        tt(P, "dve", fR[d], t_b, t_a, ALU.mult)
        tt(P, "dve", t_b, t_s, lre, ALU.mult)
        tt(P, "dve", t_d, t_c, lim, ALU.mult)
        tt(P, "dve", t_b, t_b, t_d, ALU.subtract)
        tt(P, "dve", fI[d], t_b, t_a, ALU.mult)
    bt_f = A.sb("bt_f", [128, 2, 8, 128])
    dma(P, "sp", bt_f[:, 0], I["s5_bt"][l, 0].rr("j c s -> c j s"))
    dma(P, "pool", bt_f[:, 1], I["s5_bt"][l, 1].rr("j c s -> c j s"))
    for d in range(2):
        fr = fR[d].rr("p (j s) -> p j s", j=8)
        fi = fI[d].rr("p (j s) -> p j s", j=8)
        ta = t_a.rr("p (j s) -> p j s", j=8)
        tb = t_b.rr("p (j s) -> p j s", j=8)
        tt(P, "dve", ta, bt_f[:, 0], fr, ALU.mult)
        tt(P, "dve", tb, bt_f[:, 1], fi, ALU.mult)
        tt(P, "dve", BbT[:, d, 0], ta, tb, ALU.subtract)
        tt(P, "dve", ta, bt_f[:, 0], fi, ALU.mult)
        tt(P, "dve", tb, bt_f[:, 1], fr, ALU.mult)
        tt(P, "dve", BbT[:, d, 1], ta, tb, ALU.add)
        for ri in range(2):
            ctf = t_c if ri == 0 else t_d
            dma(P, "sp" if ri == 0 else "pool", ctf.rr("p (j c) -> p j c", j=8), I["s5_ct"][l, d, ri].rr("j s c -> s j c"))
            if ri == 0:
                cp(P, "dve", CT[:, d, 0], ctf.rr("p (j c) -> p j c", j=8))
            else:
                ts(P, "dve", CT[:, d, 1], ctf.rr("p (j c) -> p j c", j=8), -1.0, ALU.mult)
    A.close()
    A = A0
    cut = C.dbg.get("s5_cut", 99)
    if cut <= 1:
        A.close(); return
    if not C.dbg.get("s5_small"):
        ct, sn = A.sb("ct", [128, NTOK]), A.sb("sn", [128, NTOK])
        w_re, w_im = A.sb("w_re", [128, NTOK]), A.sb("w_im", [128, NTOK])
    uB = A.sb("uB", [128, 2, NTOK], BF16)
    yacc = A.sb("yacc", [128, 2, NTOK])
    x_re, x_im = A.sb("x_re", [128, NTOK], BF16), A.sb("x_im", [128, NTOK], BF16)
    dsk = A.sb("dsk", [128, 16])
    bgl = A.sb("bgl", [128, 16])
    dma(P, "sp", dsk, I["s5_d_fm"][l])
    dma(P, "sp", bgl, I["s5_bglu_fm"][l])
    wglu = load_bf16(P, A, "wglu", [128, 2, 256], I["s5_w_glu"][l].rr("(k p) n -> p k n", p=128))
    tmA = [[A.sb("tm%d_%d" % (b, i), [128, 512]) for i in range(4)] for b in range(2)]
    ut = [tmA[1][i][:, 0:256] for i in range(2)]
    var = C.dbg.get("s5_var", 9)
    for t in range(NT if var > 0 else 0):
        i = t % 2
        r0 = urow(t)
        dma(P, "sp" if i == 0 else "pool", ut[i], V(C.U_ap[r0:r0 + 128, O3:O4], C.U_bufs[t]))
        pp = C.ps[i]
        for a in range(2):
            tr(P, pp[:, a * 128:(a + 1) * 128], ut[i][:, a * 128:(a + 1) * 128], C.ident)
        if var < 2:
            continue
        for a in range(2):
            cp(P, "dve", uB[:, a, t * 128:(t + 1) * 128], pp[:, a * 128:(a + 1) * 128])
        for a in range(2):
            ts(P, "dve", yacc[:, a, t * 128:(t + 1) * 128], pp[:, a * 128:(a + 1) * 128], dsk[:, a:a + 1], ALU.mult)
    tm = tmA[0]
    if cut <= 2:
        A.close(); return
    nb = 0
    for d in range(2):
        for j in range(8):
            jt = j // 4
            th = th_s[:, d, j:j + 1]
            P.op("dve", "memset", ap=ct[:, 0:1], constant=1.0)
            P.op("dve", "memset", ap=sn[:, 0:1], constant=0.0)
            k = 0
            n = 1
            while n < NTOK:
                m = min(n, NTOK - n)
                ck, sk = Ck[:, d, j, k:k + 1], Sk[:, d, j, k:k + 1]
                e1, e2 = ("dve", "dve")
                ts(P, e1, ct[:, n:n + m], ct[:, 0:m], ck, ALU.mult)
                ts(P, e2, sn[:, n:n + m], sn[:, 0:m], ck, ALU.mult)
                ts(P, e2, tm[0][:, 0:min(m, 512)] if m <= 512 else w_re[:, 0:m], sn[:, 0:m], sk, ALU.mult)
                ts(P, e1, tm[1][:, 0:min(m, 512)] if m <= 512 else w_im[:, 0:m], ct[:, 0:m], sk, ALU.mult)
                ta_ = tm[0][:, 0:m] if m <= 512 else w_re[:, 0:m]
                tb_ = tm[1][:, 0:m] if m <= 512 else w_im[:, 0:m]
                tt(P, e1, ct[:, n:n + m], ct[:, n:n + m], ta_, ALU.subtract)
                tt(P, e2, sn[:, n:n + m], sn[:, n:n + m], tb_, ALU.add)
                n += m
                k += 1
            if cut <= 3:
                A.close(); return
            for bi, (t0, n) in enumerate(S5_BLOCKS):
                if d == 0:
                    rhs = uB[:, jt, t0:t0 + n]
                else:
                    last = (255 - t0) if t0 < 256 else (4607 - t0)
                    rhs = cust(uB, jt * NTOK + last, [(-1, n)])
                pr, pi_ = C.ps[(nb % 2) * 2], C.ps[(nb % 2) * 2 + 1]
                nb += 1
                mm(P, pr[:, 0:n], BbT[:, d, 0, j, :], rhs, True, True)
                mm(P, pi_[:, 0:n], BbT[:, d, 1, j, :], rhs, True, True)
                c_, s_ = ct[:, t0:t0 + n], sn[:, t0:t0 + n]
                tm = tmA[bi % 2]
                tt(P, "dve", tm[0][:, 0:n], pr[:, 0:n], c_, ALU.mult)
                tt(P, "dve", tm[1][:, 0:n], pi_[:, 0:n], s_, ALU.mult)
                tt(P, "dve", w_re[:, t0:t0 + n], tm[0][:, 0:n], tm[1][:, 0:n], ALU.add)
                tt(P, "dve", tm[2][:, 0:n], pi_[:, 0:n], c_, ALU.mult)
                tt(P, "dve", tm[3][:, 0:n], pr[:, 0:n], s_, ALU.mult)
                tt(P, "dve", w_im[:, t0:t0 + n], tm[2][:, 0:n], tm[3][:, 0:n], ALU.subtract)
            if cut <= 4:
                A.close(); return
            rb = rho_s[:, d, j:j + 1].bc([128, NTOK])
            P.op("dve", "tensor_tensor_scan", out=w_re, data0=rb, data1=w_re, initial=0.0, op0=ALU.mult, op1=ALU.add)
            P.op("dve", "tensor_tensor_scan", out=w_im, data0=rb, data1=w_im, initial=0.0, op0=ALU.mult, op1=ALU.add)
            if cut <= 5:
                A.close(); return
            for bi, (t0, n) in enumerate(S5_BLOCKS):
                c_, s_ = ct[:, t0:t0 + n], sn[:, t0:t0 + n]
                tm = tmA[bi % 2]
                tt(P, "dve", tm[0][:, 0:n], w_re[:, t0:t0 + n], c_, ALU.mult)
                tt(P, "dve", tm[1][:, 0:n], w_im[:, t0:t0 + n], s_, ALU.mult)
                tt(P, "dve", x_re[:, t0:t0 + n], tm[0][:, 0:n], tm[1][:, 0:n], ALU.subtract)
                tt(P, "dve", tm[2][:, 0:n], w_re[:, t0:t0 + n], s_, ALU.mult)
                tt(P, "dve", tm[3][:, 0:n], w_im[:, t0:t0 + n], c_, ALU.mult)
                tt(P, "dve", x_im[:, t0:t0 + n], tm[2][:, 0:n], tm[3][:, 0:n], ALU.add)
                py = C.ps[4 + (bi % 2)]
                if d == 0:
                    xr, xi = x_re[:, t0:t0 + n], x_im[:, t0:t0 + n]
                    k0 = t0
                else:
                    k0 = (256 - t0 - n) if t0 < 256 else (4608 - t0 - n)
                    s_last = t0 + n - 1
                    xr, xi = cust(x_re, s_last, [(-1, n)]), cust(x_im, s_last, [(-1, n)])
                mm(P, py[:, 0:n], CT[:, d, 0, j, :], xr, True, False)
                mm(P, py[:, 0:n], CT[:, d, 1, j, :], xi, False, True)
                tt(P, "dve", yacc[:, jt, k0:k0 + n], yacc[:, jt, k0:k0 + n], py[:, 0:n], ALU.add)
    if cut <= 7:
        A.close(); return
    glb = uB
    tm = tmA[0]
    ob = [x_re[:, 0:1024].rr("p (a n) -> p a n", a=2), x_im[:, 0:1024].rr("p (a n) -> p a n", a=2)]
    for bi, (t0, n) in enumerate(S5_BLOCKS):
        for a in range(2):
            y = yacc[:, a, t0:t0 + n]
            tt(P, "dve", tm[0][:, 0:n], y, y, ALU.mult)
            ts(P, "dve", tm[0][:, 0:n], tm[0][:, 0:n], 0.044715, ALU.mult, 1.0, ALU.add)
            tt(P, "dve", tm[0][:, 0:n], tm[0][:, 0:n], y, ALU.mult)
            act(P, tm[0][:, 0:n], tm[0][:, 0:n], AF.Sigmoid, scale=1.5957691216057308)
            tt(P, "dve", y, y, tm[0][:, 0:n], ALU.mult)
            cp(P, "dve", glb[:, a, t0:t0 + n], y)
        OB = ob[bi % 2]
        for a in range(2):
            pz = C.ps[6 + a]
            for kt in range(2):
                mm(P, pz[:, 0:n], wglu[:, kt, a * 128:(a + 1) * 128], glb[:, kt, t0:t0 + n], kt == 0, kt == 1)
            act(P, tm[1 + a][:, 0:n], pz[:, 0:n], AF.Sigmoid, bias=bgl[:, a:a + 1])
            tt(P, "dve", OB[:, a, 0:n], yacc[:, a, t0:t0 + n], tm[1 + a][:, 0:n], ALU.mult)
        tl = [t for t in range(NT) if t * 128 >= t0 and t * 128 < t0 + n]
        dma(P, "sp", V(C.YB_ap[3][:, t0:t0 + n].rearrange("(a p) n -> p a n", p=128), tuple(C.YB_bufs[3][t] for t in tl)),
            OB[:, :, 0:n])
    A.close()


TOKBLK = [(0, 256)] + [(256 + 512 * i, 512) for i in range(8)]


def phase_win_gates(C, l, L, hfm):
    P, I = C.P, C.I
    A = Arena(P)
    wst = [A.sb("wgs%d" % i, [128, 8, 512]) for i in range(2)]
    wb = [A.sb("wgb%d" % i, [128, 8, 512], BF16) for i in range(2)]
    gst = [A.sb("gst%d" % i, [128, 512], BF16) for i in range(4)]
    wv = I["w_in"][l].rr("(k p) n -> p k n", p=128)
    n = 0
    for cb in range(8):
        c0 = O4 + cb * 512
        dma(P, "sp" if cb % 2 == 0 else "pool", wst[cb % 2], wv[:, :, c0:c0 + 512])
        cp(P, "act" if cb % 2 == 0 else "dve", wb[cb % 2], wst[cb % 2])
        w = wb[cb % 2]
        for mi in range(4):
            row0 = cb * 512 + mi * 128
            for bi, (t0, nn) in enumerate(TOKBLK):
                ps = C.ps[n % 4]
                g = gst[n % 4]
                for k in range(8):
                    mm(P, ps[:, 0:nn], w[:, k, mi * 128:(mi + 1) * 128], hfm[:, k, t0:t0 + nn], k == 0, k == 7)
                cp(P, "act" if n % 2 == 0 else "dve", g[:, 0:nn], ps[:, 0:nn])
                dma(P, "sp" if n % 2 == 0 else "pool", V(C.Gt_ap[row0:row0 + 128, t0:t0 + nn], C.Gt_bufs[bi]), g[:, 0:nn])
                n += 1
    A.close()


def phase_merge(C, l, L):
    P, I = C.P, C.I
    A = Arena(P)
    wbr = A.sb("wbr", [128, 4, 2, 1024], BF16)
    wout = A.sb("wout", [128, 8, 1024], BF16)
    wst = A.sb("wmst", [128, 8, 1024])
    for i in range(4):
        dma(P, "sp", wst[:, 0:2, :], I["w_branch"][l, i].rr("(k p) n -> p k n", p=128))
        cp(P, "act", wbr[:, i], wst[:, 0:2, :])
    dma(P, "sp", wst, I["w_out"][l].rr("(k p) n -> p k n", p=128))
    cp(P, "act", wout, wst)
    yb = [A.sb("myb%d" % i, [128, 4, 2, 512], BF16) for i in range(2)]
    gt4 = [A.sb("mgt%d" % i, [128, 4, 512], BF16) for i in range(2)]
    sg4 = [A.sb("msg%d" % i, [128, 4, 512]) for i in range(2)]
    tmp = A.sb("mtmp", [128, 512])
    acc = A.sb("macc", [128, 512])
    mg = [A.sb("mmg%d" % i, [128, 8, 512], BF16) for i in range(2)]
    xt = [A.sb("mxt%d" % i, [128, 1024]) for i in range(2)]
    tm2 = A.sb("mtm2", [128, 1024])
    n = 0
    nx = 0
    for bi, (t0, nn) in enumerate(TOKBLK):
        YB = yb[bi % 2]
        tl = [t for t in range(NT) if t0 <= t * 128 < t0 + nn]
        for i in range(4):
            dma(P, "sp" if i % 2 == 0 else "pool", YB[:, i, :, 0:nn],
                V(C.YB_ap[i][:, t0:t0 + nn].rearrange("(a p) n -> p a n", p=128), tuple(C.YB_bufs[i][t] for t in tl)))
        MG = mg[bi % 2]
        for m in range(8):
            g4 = gt4[m % 2]
            s4 = sg4[m % 2]
            dma(P, "sp" if m % 2 == 0 else "pool", g4[:, :, 0:nn],
                V(C.Gt_ap.rearrange("(i r) n -> r i n", i=4)[m * 128:(m + 1) * 128, :, t0:t0 + nn], C.Gt_bufs[bi]))
            act(P, s4[:, :, 0:nn], g4[:, :, 0:nn], AF.Sigmoid)
            for i in range(4):
                s_ = s4[:, i, :]
                ps = C.ps[n % 4]
                n += 1
                for kt in range(2):
                    mm(P, ps[:, 0:nn], wbr[:, i, kt, m * 128:(m + 1) * 128], YB[:, i, kt, 0:nn], kt == 0, kt == 1)
                if i == 0:
                    tt(P, "dve", acc[:, 0:nn], ps[:, 0:nn], s_[:, 0:nn], ALU.mult)
                elif i < 3:
                    tt(P, "dve", tmp[:, 0:nn], ps[:, 0:nn], s_[:, 0:nn], ALU.mult)
                    tt(P, "dve", acc[:, 0:nn], acc[:, 0:nn], tmp[:, 0:nn], ALU.add)
                else:
                    tt(P, "dve", tmp[:, 0:nn], ps[:, 0:nn], s_[:, 0:nn], ALU.mult)
                    tt(P, "dve", MG[:, m, 0:nn], acc[:, 0:nn], tmp[:, 0:nn], ALU.add)
        for ti, t in enumerate(tl):
            j = 1 if t < 2 else 0
            x = xt[nx % 2]
            nx += 1
            dma(P, "sp", x, xsrc(C, l, t))
            for half in range(2):
                po = C.ps[4 + half + 2 * (nx % 2)]
                for k in range(8):
                    mm(P, po, MG[:, k, ti * 128:(ti + 1) * 128], wout[:, k, half * 512:(half + 1) * 512], k == 0, k == 7)
                tt(P, "dve", tm2[:, half * 512:(half + 1) * 512], po, L.grow[0][j][:, half * 512:(half + 1) * 512], ALU.mult)
            tt(P, "dve", x, x, tm2, ALU.add)
            dma(P, "pool", C.xres[t], x)
    A.close()
    C.x_in_scratch = True


def make_router(C, l, L, RA):
    P, I = C.P, C.I
    wr = RA.sb("wr", [128, 8, 36])
    brow = RA.sb("brow", [128, 36])
    dma(P, "sp", wr, I["w_router"][l].rr("(k p) n -> p k n", p=128))
    dma(P, "sp", brow, I["b_router"][l:l + 1, :].bc([128, 36]))
    lg = RA.sb("lg", [128, 36])
    st = RA.sb("rst", [128, 16])
    oh = RA.sb("roh", [128, 4])
    em = RA.sb("rem", [128, 32])
    em2 = RA.sb("rem2", [128, 32])
    oh1 = RA.sb("roh1", [128, 32])
    oh2 = RA.sb("roh2", [128, 32])
    wg = RA.sb("rwg", [128, 32])
    junk = RA.sb("rjunk", [128, 4])

    def per_tile(t, hf):
        pl = C.ps[6]
        for k in range(8):
            mm(P, pl[:, 0:36], hf[:, k, :], wr[:, k, :], k == 0, k == 7)
        tt(P, "dve", lg, pl[:, 0:36], brow, ALU.add)
        g, e = lg[:, 0:4], lg[:, 4:36]
        P.op("dve", "tensor_reduce", out=st[:, 0:1], in_=g, axis=AX.X, op=ALU.max)
        ts(P, "dve", oh, g, st[:, 0:1], ALU.is_equal)
        ts(P, "dve", st[:, 1:2], st[:, 0:1], -1.0, ALU.mult)
        act(P, junk, g, AF.Exp, bias=st[:, 1:2], accum_out=st[:, 2:3])
        P.op("dve", "reciprocal", out=st[:, 3:4], in_=st[:, 2:3])
        ts(P, "dve", oh, oh, 1e30, ALU.mult, -1e30, ALU.add)
        tt(P, "dve", em.rr("p (g k) -> p g k", g=4), e.rr("p (g k) -> p g k", g=4),
           oh[:, :, None].bc([128, 4, 8]), ALU.add)
        P.op("dve", "tensor_reduce", out=st[:, 4:5], in_=em, axis=AX.X, op=ALU.max)
        ts(P, "dve", oh1, em, st[:, 4:5], ALU.is_equal)
        stt(P, "dve", em2, oh1, -1e30, em, ALU.mult, ALU.add)
        P.op("dve", "tensor_reduce", out=st[:, 5:6], in_=em2, axis=AX.X, op=ALU.max)
        ts(P, "dve", oh2, em2, st[:, 5:6], ALU.is_equal)
        tt(P, "dve", st[:, 6:7], st[:, 5:6], st[:, 4:5], ALU.subtract)
        act(P, st[:, 7:8], st[:, 6:7], AF.Exp)
        ts(P, "dve", st[:, 8:9], st[:, 7:8], 1.0, ALU.add)
        P.op("dve", "reciprocal", out=st[:, 9:10], in_=st[:, 8:9])
        tt(P, "dve", st[:, 10:11], st[:, 7:8], st[:, 9:10], ALU.mult)
        ts(P, "dve", wg, oh1, st[:, 9:10], ALU.mult)
        stt(P, "dve", wg, oh2, st[:, 10:11], wg, ALU.mult, ALU.add)
        ts(P, "dve", wg, wg, st[:, 3:4], ALU.mult)
        pt = C.ps[7]
        tr(P, pt[0:32, 0:128], wg, C.ident)
        cp(P, "dve", L.WT[:, t * 128:(t + 1) * 128], pt[0:32, 0:128])
    return per_tile


MOE_GROUPS = [list(range(0, 12)), list(range(12, 24)), list(range(24, 34))]


def phase_moe(C, l, L, H2, last):
    P, I = C.P, C.I
    A = Arena(P)
    hg = A.sb("hg", [128, 8, 12 * 128], BF16)
    wst = [A.sb("ews%d" % i, [128, 4, 512]) for i in range(2)]
    wgu = [A.sb("wgu%d" % i, [128, 2, 8, 512], BF16) for i in range(2)]
    wd = [A.sb("wd%d" % i, [128, 4, 1024], BF16) for i in range(2)]
    yacc = A.sb("eyacc", [128, 12, 1024])
    hid = [A.sb("hid%d" % i, [128, 4, 512], BF16) for i in range(2)]
    sil = [A.sb("sil%d" % i, [128, 512], BF16) for i in range(2)]
    tu = [A.sb("etu%d" % i, [128, 512]) for i in range(2)]
    xt = [A.sb("ext%d" % i, [128, 1024]) for i in range(2)]
    tm2 = A.sb("etm2", [128, 1024])
    ne = 0
    nst = 0
    nb = 0
    for G in MOE_GROUPS:
        tiles = [t for t in G if not (last and t < 2)]
        if not tiles:
            continue
        blocks = [tiles[i:i + 4] for i in range(0, len(tiles), 4)]
        g0 = tiles[0] * 128
        gn = len(tiles) * 128
        dma(P, "sp", hg[:, :, 0:gn], H2[:, :, g0:g0 + gn])
        for e in range(32):
            WGU, WD = wgu[ne % 2], wd[ne % 2]
            ne += 1
            for gi, nm in enumerate(("w_exp_gate", "w_exp_up")):
                src = I[nm][l, e].rr("(k p) n -> p k n", p=128)
                for hf_ in range(2):
                    s_ = wst[nst % 2]
                    dma(P, "sp" if nst % 2 == 0 else "pool", s_, src[:, hf_ * 4:(hf_ + 1) * 4, :])
                    cp(P, "act", WGU[:, gi, hf_ * 4:(hf_ + 1) * 4, :], s_)
                    nst += 1
            srcd = I["w_exp_down"][l, e].rr("(k p) n -> p k n", p=128)
            for hf_ in range(2):
                s_ = wst[nst % 2]
                dma(P, "sp" if nst % 2 == 0 else "pool", s_.rr("p k n -> p (k n)").rr("p (k n) -> p k n", k=2), srcd[:, hf_ * 2:(hf_ + 1) * 2, :])
                cp(P, "dve", WD[:, hf_ * 2:(hf_ + 1) * 2, :], s_.rr("p k n -> p (k n)").rr("p (k n) -> p k n", k=2))
                nst += 1
            for blk in blocks:
                t0 = blk[0] * 128
                nn = len(blk) * 128
                HID = hid[nb % 2]
                psW = C.ps[0]
                mm(P, psW[:, 0:nn], C.ident[0:32, e:e + 1].bc([32, 128]), L.WT[:, t0:t0 + nn], True, True)
                for f in range(4):
                    i2 = (nb * 4 + f) % 2
                    psG, psU = C.ps[1 + 2 * i2], C.ps[2 + 2 * i2]
                    for k in range(8):
                        mm(P, psG[:, 0:nn], WGU[:, 0, k, f * 128:(f + 1) * 128], hg[:, k, t0 - g0:t0 - g0 + nn], k == 0, k == 7)
                    for k in range(8):
                        mm(P, psU[:, 0:nn], WGU[:, 1, k, f * 128:(f + 1) * 128], hg[:, k, t0 - g0:t0 - g0 + nn], k == 0, k == 7)
                    act(P, sil[i2][:, 0:nn], psG[:, 0:nn], AF.Silu)
                    tt(P, "dve", tu[i2][:, 0:nn], psU[:, 0:nn], sil[i2][:, 0:nn], ALU.mult)
                    tt(P, "dve", HID[:, f, 0:nn], tu[i2][:, 0:nn], psW[:, 0:nn], ALU.mult)
                for ti, t in enumerate(blk):
                    ya = yacc[:, t - tiles[0], :]
                    for half in range(2):
                        po = C.ps[5 + (nb * 8 + ti * 2 + half) % 3]
                        for f in range(4):
                            mm(P, po, HID[:, f, ti * 128:(ti + 1) * 128], WD[:, f, half * 512:(half + 1) * 512], f == 0, f == 3)
                        if e == 0:
                            cp(P, "dve", ya[:, half * 512:(half + 1) * 512], po)
                        else:
                            tt(P, "dve", ya[:, half * 512:(half + 1) * 512], ya[:, half * 512:(half + 1) * 512], po, ALU.add)
                nb += 1
        for t in tiles:
            j = 1 if t < 2 else 0
            x = xt[t % 2]
            dma(P, "sp", x, C.xres[t])
            tt(P, "dve", tm2, yacc[:, t - tiles[0], :], L.grow[1][j], ALU.mult)
            tt(P, "dve", x, x, tm2, ALU.add)
            dma(P, "pool", C.xres[t], x)
    A.close()


def final_norm(C):
    P, I = C.P, C.I
    A = Arena(P)
    g = A.sb("fg", [128, D])
    dma(P, "sp", g, I["final_g"][0:1, :].bc([128, D]))
    xt = [A.sb("fxt%d" % i, [128, D]) for i in range(2)]
    junk = A.sb("fjunk", [128, D])
    st = [A.sb("fst%d" % i, [128, 2]) for i in range(2)]
    for t in range(2, NT):
        x, s = xt[t % 2], st[t % 2]
        dma(P, "sp" if t % 2 == 0 else "pool", x, C.xres[t])
        act(P, junk, x, AF.Square, accum_out=s[:, 0:1])
        ts(P, "dve", s[:, 1:2], s[:, 0:1], 1.0 / D, ALU.mult, EPS, ALU.add)
        act(P, s[:, 1:2], s[:, 1:2], AF.Sqrt)
        P.op("dve", "reciprocal", out=s[:, 1:2], in_=s[:, 1:2])
        stt(P, "dve", x, x, s[:, 1:2], g, ALU.mult, ALU.mult)
        dma(P, "sp" if t % 2 == 1 else "pool", V(C.out.ap[(t - 2) * 128:(t - 1) * 128, :], Buf("o%d" % t)), x)
    A.close()


CH_COLS = 3072


def alloc_chunk_heads(A, dd):
    def mkh(name, shape, dt=F32):
        return [A.sb("%s_%d_%d" % (name, dd, h), shape, dt) for h in range(4)]
    AM = mkh("cAM", [128, 4, 128], BF16)
    XB = mkh("cXB", [128, 2, 128], BF16)
    XX = [[AM[h][:, 0:2, :] for h in range(4)], [XB[h] for h in range(4)]]
    AakT = [AM[h][:, 2, :] for h in range(4)]
    ArbT = [AM[h][:, 3, :] for h in range(4)]
    ArkT, TT = [mkh("cM%d" % i, [128, 128], BF16) for i in range(2)]
    PM = mkh("cPM", [128, 2, 64], BF16)
    Ap = [PM[h][:, 0, :] for h in range(4)]
    M1 = [PM[h][:, 1, :] for h in range(4)]
    U0 = mkh("cU0", [128, 64], BF16)
    RpT = mkh("cRpT", [64, 128])
    DPC = mkh("cDPC", [128, 64])
    Y0cc = mkh("cY0cc", [64, 2, 64])
    Y0c = [[Y0cc[h][:, c, :] for h in range(4)] for c in range(2)]
    GH = mkh("cGH", [64, 4, 64])
    GT = [[GH[h][:, 2 * c, :] for h in range(4)] for c in range(2)]
    Hc = [[GH[h][:, 2 * c + 1, :] for h in range(4)] for c in range(2)]
    gA = mkh("gA", [128, 128])
    gY0 = [mkh("gY0%d" % c, [64, 64]) for c in range(2)]
    gH = [mkh("gH%d" % c, [32, 64]) for c in range(2)]
    return (AM, XB, XX, AakT, ArbT, ArkT, TT, PM, Ap, M1, U0, RpT, DPC, Y0cc, Y0c, GH, GT, Hc, gA, gY0, gH)


def phase_chunk(C, l, L):
    P, I = C.P, C.I
    A = Arena(P)
    mk = A.sb("cmasks", [128, 7, 128])
    dma(P, "sp", mk, I["cmasks"])
    idn = C.ident
    chs = [A.sb("chs%d" % i, [128, CH_COLS]) for i in range(2)]
    ST = [[A.sb("cST%d%d" % (d, h), [64, 64]) for h in range(4)] for d in range(2)]
    for d in range(2):
        for h in range(4):
            P.op("dve", "memset", ap=ST[d][h], constant=0.0)

    def mk2(name, shape, dt=F32):
        return [A.sb("%s%d" % (name, i), shape, dt) for i in range(2)]
    TOT, incS, Ein, Enin, Eex, Eend, Etot, tmpx, tmpy = [mk2("cE%d" % i, [128, 256]) for i in range(9)]
    at, rt, bt, kt, bh, kh = [mk2("cq%d" % i, [128, 256]) for i in range(6)]
    aT, rTb, bT, kT = [mk2("cT%d" % i, [128, 2, 128], BF16) for i in range(4)]
    rT = mk2("cTr", [128, 2, 128])
    bhc = [mk2("cbhc%d" % c, [128, 256], BF16) for c in range(2)]
    khc = [mk2("ckhc%d" % c, [128, 256], BF16) for c in range(2)]
    at_b = mk2("cat_b", [128, 256], BF16)
    v_b = mk2("cv_b", [128, 256], BF16)
    IM = A.sb("cIM", [128, 64])
    tt(P, "dve", IM, idn[:, 0:64], idn[:, 64:128], ALU.add)

    HB = []
    for dd in range(2):
        HB.append(alloc_chunk_heads(A, dd))
    MK4 = [A.sb("cMK4%d" % d, [128, 4, 128]) for d in range(2)]
    for d in range(2):
        ms_, mst_, mit_ = (0, 1, 2) if d == 0 else (3, 4, 5)
        for i_, mi_ in enumerate((ms_, mst_, mst_, mit_)):
            cp(P, "dve", MK4[d][:, i_, :], mk[:, mi_, :])
    yo = [A.sb("cyo%d" % i, [64, 2, 256]) for i in range(2)]
    STg = [[A.sb("gST%d%d" % (d, h), [32, 64]) for h in range(4)] for d in range(2)]
    for d in range(2):
        for h in range(4):
            P.op("dve", "memset", ap=STg[d][h], constant=0.0)
    gTOT, gincS, gEin, gEnin, gEend, gEtot, gtmp = [mk2("gE%d" % i, [128, 128]) for i in range(7)]
    gq, gk, gkh = [mk2("gq%d" % i, [128, 128]) for i in range(3)]
    gkhc = [mk2("gkhc%d" % c, [128, 128]) for c in range(2)]
    gqT, gkT, gPT = [[mk2("gT%d_%d" % (i, h), [32, 128]) for h in range(4)] for i in range(3)]
    gyo = [A.sb("gyo%d" % i, [64, 2, 256]) for i in range(2)]
    ps = C.ps
    border = [1, 0] + list(range(NT - 1, 1, -1))
    H4 = range(4)
    it = 0
    cut = C.dbg.get("chunk_cut", 99)

    def body(n, d):
        if True:
            (AM, XB, XX, AakT, ArbT, ArkT, TT, PM, Ap, M1, U0, RpT, DPC, Y0cc, Y0c, GH, GT, Hc, gA, gY0, gH) = HB[d]
            ps = C.ps[4 * d:4 * d + 4] + C.ps[4 - 4 * d:8 - 4 * d]
            t = n if d == 0 else border[n]
            q = d
            ch = chs[q]
            YO = yo[q]
            dma(P, "sp" if d == 0 else "pool", ch, V(C.CH_ap[t * 128:(t + 1) * 128, :], C.CH_bufs[t]))
            lw = ch[:, d * 256:(d + 1) * 256]
            ke = ch[:, 512 + d * 256:768 + d * 256]
            b_ = ch[:, 1024 + d * 256:1280 + d * 256]
            a_, r_, v_ = ch[:, 1536:1792], ch[:, 1792:2048], ch[:, 2048:2304]
            m_s, m_st, m_it = (0, 1, 2) if d == 0 else (3, 4, 5)
            pc = ps[4 + q]
            mm(P, pc[:, 0:256], mk[:, m_it, :], lw, True, True)
            mm(P, pc[:, 256:512], mk[:, 6, :], lw, True, True)
            cp(P, "dve", TOT[q], pc[:, 256:512])
            cp(P, "dve", incS[q], pc[:, 0:256])
            act(P, Ein[q], incS[q], AF.Exp)
            act(P, Enin[q], incS[q], AF.Exp, scale=-1.0)
            tt(P, "dve", tmpx[q], incS[q], lw, ALU.subtract)
            act(P, Eex[q], tmpx[q], AF.Exp)
            tt(P, "dve", tmpy[q], TOT[q], incS[q], ALU.subtract)
            act(P, Eend[q], tmpy[q], AF.Exp)
            act(P, Etot[q], TOT[q], AF.Exp)
            tt(P, "dve", at[q], a_, Eex[q], ALU.mult)
            cp(P, "act", at_b[q], at[q])
            cp(P, "act", v_b[q], v_)
            tt(P, "dve", rt[q], r_, Ein[q], ALU.mult)
            tt(P, "dve", bt[q], b_, Enin[q], ALU.mult)
            tt(P, "dve", kt[q], ke, Enin[q], ALU.mult)
            tt(P, "dve", bh[q], b_, Eend[q], ALU.mult)
            tt(P, "dve", kh[q], ke, Eend[q], ALU.mult)
            for c in range(2):
                ts(P, "dve", bhc[c][q], bh[q], mk[:, 6, c * 64:c * 64 + 1], ALU.mult)
                ts(P, "dve", khc[c][q], kh[q], mk[:, 6, c * 64:c * 64 + 1], ALU.mult)
            yield
            for qi, (src, dst) in enumerate(((at, aT), (rt, rT), (bt, bT), (kt, kT))):
                pb_ = ps[6 + qi % 2]
                for a2 in range(2):
                    tr(P, pb_[:, a2 * 128:(a2 + 1) * 128], src[q][:, a2 * 128:(a2 + 1) * 128], idn)
                cp(P, "dve", dst[q], pb_[:, 0:256].rr("p (a n) -> p a n", a=2))
                if qi == 1:
                    cp(P, "dve", rTb[q], pb_[:, 0:256].rr("p (a n) -> p a n", a=2))

            def hv(h):
                pair, hl = h // 2, h % 2
                return pair, slice(hl * 64, (hl + 1) * 64), slice(h * 64, (h + 1) * 64)
            if cut <= 2:
                return
            yield
            for h in H4:
                pair, hs, hc = hv(h)
                pA = ps[h]
                mm(P, pA[:, 0:128], aT[q][hs, pair, :], bT[q][hs, pair, :], True, True)
                mm(P, pA[:, 128:256], bT[q][hs, pair, :], aT[q][hs, pair, :], True, True)
                mm(P, pA[:, 256:384], kT[q][hs, pair, :], aT[q][hs, pair, :], True, True)
                mm(P, pA[:, 384:512], bT[q][hs, pair, :], rTb[q][hs, pair, :], True, True)
            for h in H4:
                pA = ps[h]
                tt(P, "dve", AM[h].rr("p a n -> p (a n)"), pA, MK4[d].rr("p a n -> p (a n)"), ALU.mult)
                tt(P, "dve", TT[h], AM[h][:, 1, :], idn, ALU.add)
            for h in H4:
                pair, hs, hc = hv(h)
                mm(P, ps[h][:, 0:128], kT[q][hs, pair, :], rTb[q][hs, pair, :], True, True)
            for h in H4:
                tt(P, "dve", ArkT[h], ps[h][:, 0:128], mk[:, m_it, :], ALU.mult)
            if cut <= 3:
                return
            yield
            for s in range(5):
                yield
                XXc, XXn = XX[s % 2], XX[(s + 1) % 2]
                for h in H4:
                    pX = ps[h]
                    mm(P, pX[:, 128:256], XXc[h][:, 1, :], XXc[h][:, 0, :], True, True)
                    if s < 4:
                        mm(P, pX[:, 256:384], XXc[h][:, 0, :], XXc[h][:, 1, :], True, True)
                for h in H4:
                    pX = ps[h]
                    if s < 4:
                        cp(P, "dve", XXn[h], pX[:, 128:384].rr("p (a n) -> p a n", a=2))
                    else:
                        cp(P, "dve", XXn[h][:, 0, :], pX[:, 128:256])
                for h in H4:
                    mm(P, ps[h][:, 384:512], XXn[h][:, 0, :], TT[h], True, True)
                for h in H4:
                    tt(P, "dve", TT[h], TT[h], ps[h][:, 384:512], ALU.add)
            if cut <= 4:
                return
            yield
            for h in H4:
                pair, hs, hc = hv(h)
                mm(P, ps[h][:, 0:64], TT[h], at_b[q][:, hc], True, True)
                mm(P, ps[h][:, 64:128], AakT[h], v_b[q][:, hc], True, True)
            for h in H4:
                cp(P, "dve", PM[h], ps[h][:, 0:128].rr("p (a n) -> p a n", a=2))
            for h in H4:
                mm(P, ps[h][:, 128:192], TT[h], M1[h], True, True)
            for h in H4:
                cp(P, "dve", U0[h], ps[h][:, 128:192])
            for h in H4:
                pair, hs, hc = hv(h)
                for c in range(2):
                    cs = slice(c * 64, (c + 1) * 64)
                    mm(P, ps[h][0:64, 192 + c * 64:256 + c * 64], ArbT[h][:, cs], U0[h], True, False)
                    mm(P, ps[h][0:64, 192 + c * 64:256 + c * 64], ArkT[h][:, cs], v_b[q][:, hc], False, True)
                mm(P, ps[h][0:64, 320:448], Ap[h], ArbT[h], True, True)
                tt(P, "dve", DPC[h], IM, Etot[q][:, hc], ALU.mult)
            for h in H4:
                pair, hs, hc = hv(h)
                cp(P, "dve", Y0cc[h], ps[h][0:64, 192:320].rr("p (a n) -> p a n", a=2))
                tt(P, "dve", RpT[h], ps[h][0:64, 320:448], rT[q][hs, pair, :], ALU.add)
            if cut <= 5:
                return
            for h in H4:
                pair, hs, hc = hv(h)
                pG = ps[h]
                for c in range(2):
                    cs = slice(c * 64, (c + 1) * 64)
                    o = c * 128
                    mm(P, pG[0:64, o:o + 64], Ap[h], bhc[c][q][:, hc], True, False)
                    mm(P, pG[0:64, o:o + 64], idn[:, cs], DPC[h], False, True)
                    mm(P, pG[0:64, o + 64:o + 128], bhc[c][q][:, hc], U0[h], True, False)
                    mm(P, pG[0:64, o + 64:o + 128], khc[c][q][:, hc], v_b[q][:, hc], False, True)
            for h in H4:
                pG = ps[h]
                cp(P, "dve", GH[h], pG[0:64, 0:256].rr("p (a n) -> p a n", a=4))
            if cut <= 6:
                return
            yield
            for c in ((0, 1) if d == 0 else (1, 0)):
                cs = slice(c * 64, (c + 1) * 64)
                for h in H4:
                    pG = ps[h]
                    S_ = ST[d][h]
                    mm(P, pG[0:64, 256:320], RpT[h][:, cs], S_, True, False)
                    mm(P, pG[0:64, 256:320], idn[0:64, 0:64], Y0c[c][h], False, True)
                    mm(P, pG[0:64, 320:384], GT[c][h], S_, True, False)
                    mm(P, pG[0:64, 320:384], idn[0:64, 0:64], Hc[c][h], False, True)
                for h in H4:
                    pair, hs, hc = hv(h)
                    pG = ps[h]
                    cp(P, "dve", YO[:, c, hc], pG[0:64, 256:320])
                    cp(P, "dve", ST[d][h], pG[0:64, 320:384])
            dma(P, "sp" if d == 0 else "pool",
                V(C.YT_ap[d][t * 128:(t + 1) * 128, :].rearrange("(c p) n -> p c n", p=64), C.YT_bufs[d][t]), YO)
            yield
            GYO = gyo[q]
            glw = ch[:, 2304 + d * 128:2432 + d * 128]
            gq_, gk_, gv_ = ch[:, 2560:2688], ch[:, 2688:2816], ch[:, 2816:3072]
            pcg = ps[4 + q]
            mm(P, pcg[:, 0:128], mk[:, m_it, :], glw, True, True)
            mm(P, pcg[:, 128:256], mk[:, 6, :], glw, True, True)
            cp(P, "dve", gincS[q], pcg[:, 0:128])
            cp(P, "dve", gTOT[q], pcg[:, 128:256])
            act(P, gEin[q], gincS[q], AF.Exp)
            act(P, gEnin[q], gincS[q], AF.Exp, scale=-1.0)
            tt(P, "dve", gtmp[q], gTOT[q], gincS[q], ALU.subtract)
            act(P, gEend[q], gtmp[q], AF.Exp)
            act(P, gEtot[q], gTOT[q], AF.Exp)
            tt(P, "dve", gq[q], gq_, gEin[q], ALU.mult)
            tt(P, "dve", gk[q], gk_, gEnin[q], ALU.mult)
            tt(P, "dve", gkh[q], gk_, gEend[q], ALU.mult)
            for c in range(2):
                ts(P, "dve", gkhc[c][q], gkh[q], mk[:, 6, c * 64:c * 64 + 1], ALU.mult)
            for h in H4:
                pT_ = ps[6 + h % 2]
                g32 = slice(h * 32, (h + 1) * 32)
                tr(P, pT_[0:32, 0:128], gq[q][:, g32], idn)
                tr(P, pT_[0:32, 128:256], gk[q][:, g32], idn)
                tr(P, pT_[0:32, 256:384], gEtot[q][:, g32], idn)
                cp(P, "dve", gqT[h][q], pT_[0:32, 0:128])
                cp(P, "dve", gkT[h][q], pT_[0:32, 128:256])
                cp(P, "dve", gPT[h][q], pT_[0:32, 256:384])
            for h in H4:
                mm(P, ps[h][:, 0:128], gkT[h][q], gqT[h][q], True, True)
            for h in H4:
                tt(P, "dve", gA[h], ps[h][:, 0:128], mk[:, m_it, :], ALU.mult)
            for h in H4:
                hc = slice(h * 64, (h + 1) * 64)
                g32 = slice(h * 32, (h + 1) * 32)
                for c in range(2):
                    cs = slice(c * 64, (c + 1) * 64)
                    mm(P, ps[h][0:64, 128 + c * 64:192 + c * 64], gA[h][:, cs], gv_[:, hc], True, True)
                    mm(P, ps[h][0:32, 256 + c * 64:320 + c * 64], gkhc[c][q][:, g32], gv_[:, hc], True, True)
            for h in H4:
                for c in range(2):
                    cp(P, "dve", gY0[c][h], ps[h][0:64, 128 + c * 64:192 + c * 64])
                    cp(P, "dve", gH[c][h], ps[h][0:32, 256 + c * 64:320 + c * 64])
            for c in ((0, 1) if d == 0 else (1, 0)):
                cs = slice(c * 64, (c + 1) * 64)
                for h in H4:
                    S_ = STg[d][h]
                    mm(P, ps[h][0:64, 384:448], gqT[h][q][:, cs], S_, True, False)
                    mm(P, ps[h][0:64, 384:448], idn[0:64, 0:64], gY0[c][h], False, True)
                for h in H4:
                    hc = slice(h * 64, (h + 1) * 64)
                    S_ = STg[d][h]
                    cp(P, "dve", GYO[:, c, hc], ps[h][0:64, 384:448])
                    stt(P, "dve", S_, S_, gPT[h][q][:, c * 64:c * 64 + 1], gH[c][h], ALU.mult, ALU.add)
            dma(P, "sp" if d == 1 else "pool",
                V(C.YTG_ap[d][t * 128:(t + 1) * 128, :].rearrange("(c p) n -> p c n", p=64), C.YTG_bufs[d][t]), GYO)
    for n in range(NT if cut > 50 else 1):
        gens = [body(n, 0), body(n, 1)]
        while gens:
            nxt = []
            for g in gens:
                try:
                    next(g)
                    nxt.append(g)
                except StopIteration:
                    pass
            gens = nxt
    A.close()


class LayerState:
    pass


def layer(C, l):
    P = C.P
    LA = Arena(P)
    L = LayerState()
    L.mod = LA.sb("mod", [128, 48, 2])
    L.sc1 = LA.sb("sc1", [128, 8, 2])
    L.sc2 = LA.sb("sc2", [128, 8, 2])
    L.grow = [[LA.sb("grow%d%d" % (ii, j), [128, 1024]) for j in range(2)] for ii in range(2)]
    phase_ada(C, l, L)
    if C.dbg.get("dump") and l == C.dbg.get("layer", 0):
        dma(P, "sp", C.dout("d_mod", [128, 48, 2]), L.mod)
        for ii in range(2):
            for j in range(2):
                dma(P, "sp", C.dout("d_grow%d%d" % (ii, j), [128, 1024]), L.grow[ii][j])
    HA = Arena(P)
    hfm = HA.sb("hfm", [128, 8, NTOK], BF16)
    phase_norm(C, l, L, 1, hfm)
    if C.dbg.get("dump") and l == C.dbg.get("layer", 0):
        dma(P, "sp", C.dout("d_hfm", [128, 8, NTOK], BF16), hfm)
    phase_win_tm(C, l, L, hfm)
    if not C.dbg.get("skip_gates"):
        phase_win_gates(C, l, L, hfm)
    HA.close()
    if C.dbg.get("stop_after") == "win":
        LA.close(); return
    if not C.dbg.get("skip_ab"):
        phase_prep(C, l, L)
        if C.dbg.get("stop_after") == "prep":
            LA.close(); return
        if C.dbg.get("old_gla") and not C.dbg.get("skip_scan"):
            phase_scan(C, l, C.dbg.get("nchunks"))
        if not C.dbg.get("old_rwkv"):
            phase_chunk(C, l, L)
        if C.dbg.get("stop_after") == "chunk":
            LA.close(); return
        if C.dbg.get("stop_after") == "scan":
            LA.close(); return
        phase_fin_ab(C, l, L)
    if C.dbg.get("stop_after") == "fin":
        LA.close(); return
    if not C.dbg.get("skip_attn"):
        phase_attn(C, l, L)
    if C.dbg.get("stop_after") == "attn":
        LA.close(); return
    if not C.dbg.get("skip_s5"):
        phase_s5(C, l, L)
    if C.dbg.get("stop_after") == "s5":
        LA.close(); return
    phase_merge(C, l, L)
    if C.dbg.get("stop_after") == "merge":
        LA.close(); return
    WA = Arena(P)
    L.WT = WA.sb("WT", [32, NTOK])
    HA = Arena(P)
    hfm2 = HA.sb("hfm2", [128, 8, NTOK], BF16)
    RA = Arena(P)
    phase_norm(C, l, L, 2, hfm2, per_tile=make_router(C, l, L, RA))
    RA.close()
    if C.dbg.get("dump") and l == C.dbg.get("layer", 0):
        dma(P, "sp", C.dout("d_hfm2", [128, 8, NTOK], BF16), hfm2)
        dma(P, "sp", C.dout("d_WT", [32, NTOK]), L.WT)
    H2 = V(C.H2_ap, C.H2_buf)
    dma(P, "sp", H2, hfm2)
    HA.close()
    if C.dbg.get("stop_after") == "norm2":
        WA.close(); LA.close(); return
    phase_moe(C, l, L, H2, l == DEPTH - 1)
    WA.close()
    LA.close()


def make_maskw():
    m = np.zeros((128, 384), np.float32)
    i = np.arange(128)[:, None]
    j = np.arange(128)[None, :]
    m[:, 0:128] = np.where(j >= i, 0.0, -1e30)
    m[:, 256:384] = np.where(j <= i, 0.0, -1e30)
    return m


def make_rope():
    rows = SEQ // 64
    row = np.repeat(np.arange(rows, dtype=np.float32), 64)
    col = np.tile(np.arange(64, dtype=np.float32), rows)
    inv = (10000.0 ** (-np.arange(16, dtype=np.float32) / 16)).astype(np.float32)
    ang = np.concatenate([row[:, None] * inv, col[:, None] * inv], axis=-1).astype(np.float32)
    return np.cos(ang).astype(np.float32), np.sin(ang).astype(np.float32)


ROPE = make_rope()


def s5_host(A):
    f = np.float32
    lre, lim, ldt = A("s5_lam_re"), A("s5_lam_im"), A("s5_log_dt")
    ldt_e = np.repeat(ldt[..., None], 64, axis=-1)
    rows = np.stack([lre.reshape(DEPTH, 2, 1024), lim.reshape(DEPTH, 2, 1024), ldt_e.reshape(DEPTH, 2, 1024)], axis=2)
    sm = rows.reshape(DEPTH, 2, 3, 8, 128).transpose(0, 4, 1, 2, 3)
    bre, bim = A("s5_b_re"), A("s5_b_im")
    bt = np.zeros((DEPTH, 2, 8, 128, 128), f)
    cre, cim = A("s5_c_re"), A("s5_c_im")
    ct = np.zeros((DEPTH, 2, 2, 8, 128, 128), f)
    for g in range(16):
        j, hh = g // 2, g % 2
        c0 = (g % 8) * 16
        for ri, b in enumerate((bre, bim)):
            bt[:, ri, j, c0:c0 + 16, hh * 64:(hh + 1) * 64] = b[:, g].transpose(0, 2, 1)
        for ri, c in enumerate((cre, cim)):
            ct[:, :, ri, j, hh * 64:(hh + 1) * 64, c0:c0 + 16] = c[:, :, g].transpose(0, 1, 3, 2)
    return {
        "s5_sm": np.ascontiguousarray(sm, f), "s5_rows": np.ascontiguousarray(rows, f),
        "s5_bt": bt, "s5_ct": ct,
        "pw2": (2.0 ** np.arange(16)).astype(f).reshape(1, 16),
        "s5_d_fm": np.ascontiguousarray(np.pad(A("s5_d").reshape(DEPTH, 2, 128).transpose(0, 2, 1), ((0, 0), (0, 0), (0, 14)))),
        "s5_bglu_fm": np.ascontiguousarray(np.pad(A("s5_b_glu").reshape(DEPTH, 2, 128).transpose(0, 2, 1), ((0, 0), (0, 0), (0, 14)))),
        "s5_w_glu": A("s5_w_glu"),
    }


def make_sele():
    s = np.zeros((32, 32, 128), np.float32)
    for e in range(32):
        s[e, e, :] = 1.0
    return s


def make_cmasks():
    r = np.arange(128)[:, None]
    c = np.arange(128)[None, :]
    same = (r // 64) == (c // 64)
    m = np.zeros((128, 7, 128), np.float32)
    m[:, 0] = same & (c < r)
    m[:, 1] = same & (r < c)
    m[:, 2] = same & (r <= c)
    m[:, 3] = same & (c > r)
    m[:, 4] = same & (r > c)
    m[:, 5] = same & (r >= c)
    m[:, 6] = same
    return m


def make_sel():
    s = np.zeros((128, 64, 128), np.float32)
    for j in range(64):
        for hh in range(2):
            s[2 * j + hh, j, hh * 64:(hh + 1) * 64] = 1.0
    return s


def blkdiag(mats):
    n = len(mats)
    L, r, c = mats[0].shape
    o = np.zeros((L, n * r, n * c), np.float32)
    for i, m in enumerate(mats):
        o[:, i * r:(i + 1) * r, i * c:(i + 1) * c] = m
    return o


def host_inputs(inputs, b):
    f = np.float32

    def A(k):
        return np.asarray(inputs[k], f)
    c = np.asarray(inputs["c"], f)[b]
    cctx = np.asarray(inputs["c_ctx"], f)
    cc = np.stack([c.reshape(8, 128).T, cctx.reshape(8, 128).T], axis=-1)
    m = {
        "xb": np.ascontiguousarray(np.asarray(inputs["x"], f)[b]),
        "ctxb": np.ascontiguousarray(np.asarray(inputs["ctx"], f)[b]),
        "cc": np.ascontiguousarray(cc),
        "w_ada": np.asarray(inputs["w_ada"], f),
        "b_ada": np.asarray(inputs["b_ada"], f),
        "b_ada_fm": np.ascontiguousarray(np.asarray(inputs["b_ada"], f).reshape(DEPTH, 48, 128).transpose(0, 2, 1)),
        "g1_fm": np.ascontiguousarray(np.asarray(inputs["norm1_g"], f).reshape(DEPTH, 8, 128).transpose(0, 2, 1)),
        "g2_fm": np.ascontiguousarray(np.asarray(inputs["norm2_g"], f).reshape(DEPTH, 8, 128).transpose(0, 2, 1)),
        "w_in": np.asarray(inputs["w_in"], f),
        "ident": np.eye(128, dtype=f),
        "sel": make_sel(),
        "maskw": make_maskw(),
        "ropec": ROPE[0],
        "ropes": ROPE[1],
        "attn_sink": np.ascontiguousarray(np.pad(A("attn_sink"), ((0, 0), (0, 12)))),
        **s5_host(A),
        "cmasks": make_cmasks(),
        "w_branch": A("w_branch"),
        "w_out": A("w_out"),
        "w_router": np.ascontiguousarray(np.concatenate([A("w_router_g"), A("w_router_e")], axis=2)),
        "b_router": np.ascontiguousarray(np.concatenate([A("b_router_g"), A("b_router_e")], axis=1)),
        "w_exp_gate": A("w_exp_gate"),
        "w_exp_up": A("w_exp_up"),
        "w_exp_down": A("w_exp_down"),
        "pv": np.ascontiguousarray(np.concatenate([
            A("rwkv_mu").reshape(DEPTH, -1), A("rwkv_kk"), A("rwkv_ka"), A("rwkv_rk").reshape(DEPTH, -1),
            A("rwkv_w0").reshape(DEPTH, -1), A("rwkv_a0").reshape(DEPTH, -1), A("rwkv_ln_g"),
            A("gla_ab").reshape(DEPTH, -1), A("gla_ln_g")], axis=1)),
        "w1cat": np.ascontiguousarray(np.concatenate([A("rwkv_w1")[:, 0], A("rwkv_w1")[:, 1],
                                                      A("rwkv_a1")[:, 0], A("rwkv_a1")[:, 1]], axis=2)),
        "w2blk": blkdiag([A("rwkv_w2")[:, 0], A("rwkv_w2")[:, 1], A("rwkv_a2")[:, 0], A("rwkv_a2")[:, 1]]),
        "g1": A("rwkv_g1"),
        "g2": A("rwkv_g2"),
        "a2blk": blkdiag([A("gla_a2")[:, 0], A("gla_a2")[:, 1]]),
        "final_g": np.asarray(inputs["final_norm_g"], f).reshape(1, D),
    }
    return m


def kernel(**inputs):
    nc = build_program()
    in_maps = [host_inputs(inputs, b) for b in range(8)]
    res = run_bass_kernel_spmd(nc, in_maps, core_ids=list(range(8)))
    return np.stack([r["out"] for r in res.results], axis=0)
```

```python
import math
from contextlib import ExitStack

import numpy as np
import concourse.bass as bass
import concourse.mybir as mybir
from concourse.bass_utils import run_bass_kernel_spmd

F32 = mybir.dt.float32
BF16 = mybir.dt.bfloat16
ALU = mybir.AluOpType
AF = mybir.ActivationFunctionType
AX = mybir.AxisListType

D = 1024
SEQ = 4096
CTX = 256
NT = (SEQ + CTX) // 128
NTOK = SEQ + CTX
DEPTH = 2
EPS = 1e-6
GN_EPS = 64e-5
O1, O2, O3, O4, PIN = 1024, 1824, 2336, 2592, 6688
PV_MU, PV_KK, PV_KA, PV_RK, PV_W0, PV_A0, PV_LNG, PV_GAB, PV_GLNG = 0, 1024, 1280, 1536, 1792, 2304, 2816, 3072, 3328
NPV = 3584

DEBUG = False
PENDING = "PENDING"
WKEYS = ("out", "accum_out", "ap")
SKEYS = ("scalar1", "scalar2", "scale", "bias", "scalar")


class Buf:
    __slots__ = ("name", "w", "rd", "ws", "wok")

    def __init__(self, name=""):
        self.name = name
        self.w = None
        self.rd = {}
        self.ws = False
        self.wok = False


class V:
    __slots__ = ("ap", "bufs")

    def __init__(self, ap, bufs):
        self.ap = ap
        self.bufs = bufs if isinstance(bufs, tuple) else (bufs,)

    def __getitem__(self, k):
        return V(self.ap[k], self.bufs)

    def rr(self, pat, **kw):
        return V(self.ap.rearrange(pat, **kw), self.bufs)

    def bc(self, shape):
        return V(self.ap.to_broadcast(list(shape)), self.bufs)

    def bitcast(self, dt):
        return V(self.ap.bitcast(dt), self.bufs)

    def wb(self, *bufs):
        return V(self.ap, tuple(bufs))

    @property
    def shape(self):
        return tuple(self.ap.shape)


class Prog:
    ENG = ("pe", "act", "dve", "pool", "sp")
    K = 6
    STRICT_ALL = False

    def __init__(self, nc):
        self.nc = nc
        self.eng = {"pe": nc.tensor, "act": nc.scalar, "dve": nc.vector, "pool": nc.gpsimd, "sp": nc.sync}
        self.sem = {e: nc.alloc_semaphore("s_" + e) for e in self.ENG}
        self.cnt = {e: 0 for e in self.ENG}
        self.dsem = {q: [nc.alloc_semaphore("d_%s%d" % (q, i)) for i in range(self.K)] for q in ("sp", "act", "pool")}
        self.dcnt = {q: 0 for q in self.dsem}
        self.known = {e: {} for e in self.ENG}
        self.pend_r = []
        self.pend_w = []
        self.uid = 0
        self.nins = 0

    def _need(self, e, tok, is_dma, strict=False):
        if tok is None:
            return
        if tok is PENDING:
            assert e == "pe" and not is_dma, "dependency on an unmarked PE op"
            return
        sem, val, owner = tok
        if owner == e and not is_dma and not (strict and e != "pe") and not self.STRICT_ALL:
            return
        k = self.known[e]
        if k.get(sem.num, 0) >= val:
            return
        self.eng[e].wait_ge(sem, val)
        self.nins += 1
        k[sem.num] = val

    def op(self, e, meth, mark=True, lax=False, **kw):
        reads, writes, args, sreads, awrites = [], [], {}, [], []
        big_w, small_r, psum_in = True, set(), False
        for k, v in kw.items():
            if isinstance(v, V):
                fsz = 1
                for d_ in v.ap.shape[1:]:
                    fsz *= d_
                if k in WKEYS:
                    if fsz < 128 or 0 in [st_ for st_, _ in v.ap.ap[1:]]:
                        big_w = False
                else:
                    if v.ap.name == "psall":
                        psum_in = True
                    if fsz < 128:
                        small_r.update(v.bufs)
                (writes if k in WKEYS else reads).extend(v.bufs)
                if k in SKEYS:
                    sreads.extend(v.bufs)
                if k == "accum_out":
                    awrites.extend(v.bufs)
                args[k] = v.ap
            else:
                args[k] = v
        is_dma = meth == "dma_start"
        for b in reads:
            relaxed = lax or (e == "dve" and b.wok and b not in small_r)
            self._need(e, b.w, is_dma, strict=(not relaxed) or b.ws or e == "act" or b in sreads)
        for b in writes:
            self._need(e, b.w, is_dma)
            for t in b.rd.values():
                self._need(e, t, is_dma)
        if is_dma:
            n = self.dcnt[e]
            sem = self.dsem[e][n % self.K]
            r = n // self.K
            if r > 0:
                self._need(e, (sem, 16 * r, None), True)
            ins = getattr(self.eng[e], meth)(**args)
            ins.then_inc(sem, 16)
            self.dcnt[e] = n + 1
            tok = (sem, 16 * (r + 1), None)
            key = ("d", sem.num)
        else:
            ins = getattr(self.eng[e], meth)(**args)
            key = e
            if mark:
                self.cnt[e] += 1
                ins.then_inc(self.sem[e], 1)
                tok = (self.sem[e], self.cnt[e], e)
                if e == "pe" and (self.pend_r or self.pend_w):
                    for b in self.pend_r:
                        if b.rd.get("pe") is PENDING:
                            b.rd["pe"] = tok
                    for b in self.pend_w:
                        if b.w is PENDING:
                            b.w = tok
                    self.pend_r = []
                    self.pend_w = []
            else:
                assert e == "pe"
                tok = PENDING
                self.pend_r.extend(reads)
                self.pend_w.extend(writes)
        self.nins += 1
        for b in reads:
            b.rd[key] = tok
        for b in writes:
            b.w = tok
            b.rd = {}
            b.ws = (b in awrites) or e == "act"
            b.wok = (e == "dve") and big_w and (not psum_in) and (b not in awrites) and not is_dma
        return ins

    def barrier(self):
        assert not self.pend_r and not self.pend_w
        toks = [(self.sem[e], self.cnt[e], e) for e in self.ENG if self.cnt[e] > 0]
        for q in self.dsem:
            n = self.dcnt[q]
            for i in range(self.K):
                c = (n - i + self.K - 1) // self.K if n > i else 0
                if c > 0:
                    toks.append((self.dsem[q][i], 16 * c, None))
        for e in self.ENG:
            for t in toks:
                self._need(e, t, False)

    def name(self, s):
        self.uid += 1
        return "%s_%d" % (s, self.uid)

    def dram(self, name, shape, dt, kind="Internal"):
        return self.nc.dram_tensor(name, list(shape), dt, kind=kind).ap()


class Arena:
    def __init__(self, P):
        self.P = P
        self.stack = ExitStack()

    def sb(self, name, shape, dt=F32):
        h = self.stack.enter_context(self.P.nc.sbuf_tensor(self.P.name(name), list(shape), dt))
        return V(h.ap(), Buf(name))

    def close(self):
        self.P.barrier()
        self.stack.close()


def dma(P, q, out, in_):
    return P.op(q, "dma_start", out=out, in_=in_)


def mm(P, out, lhsT, rhs, start, stop, mark=None):
    return P.op("pe", "matmul", mark=(stop if mark is None else mark), out=out, lhsT=lhsT, rhs=rhs,
                start=start, stop=stop)


def tr(P, out, in_, ident, mark=True):
    return P.op("pe", "transpose", mark=mark, out=out, in_=in_, identity=ident)


def tt(P, e, out, in0, in1, op):
    return P.op(e, "tensor_tensor", out=out, in0=in0, in1=in1, op=op)


def ts(P, e, out, in0, s1, op0, s2=None, op1=None, **kw):
    if op1 is None:
        return P.op(e, "tensor_scalar", out=out, in0=in0, scalar1=s1, scalar2=None, op0=op0, **kw)
    return P.op(e, "tensor_scalar", out=out, in0=in0, scalar1=s1, scalar2=s2, op0=op0, op1=op1, **kw)


def act(P, out, in_, func, **kw):
    return P.op("act", "activation", out=out, in_=in_, func=func, **kw)


def cp(P, e, out, in_):
    if e == "act":
        return act(P, out, in_, AF.Copy)
    return P.op(e, "tensor_copy", out=out, in_=in_)


class Ctx:
    pass


def build_program(dbg=None):
    dbg = dbg or {}
    nc = bass.Bass("TRN2", target_bir_lowering=False)
    P = Prog(nc)
    C = Ctx()
    C.P, C.nc, C.dbg = P, nc, dbg
    skind = "ExternalOutput" if dbg.get("expose") else "Internal"

    def din(name, shape, dt=F32):
        return V(nc.dram_tensor(name, list(shape), dt, kind="ExternalInput").ap(), Buf(name))

    I = {}
    I["xb"] = din("xb", [SEQ, D])
    I["ctxb"] = din("ctxb", [CTX, D])
    I["cc"] = din("cc", [128, 8, 2])
    I["w_ada"] = din("w_ada", [DEPTH, D, 6 * D])
    I["b_ada"] = din("b_ada", [DEPTH, 6 * D])
    I["b_ada_fm"] = din("b_ada_fm", [DEPTH, 128, 48])
    I["g1_fm"] = din("g1_fm", [DEPTH, 128, 8])
    I["g2_fm"] = din("g2_fm", [DEPTH, 128, 8])
    I["w_in"] = din("w_in", [DEPTH, D, PIN])
    I["ident"] = din("ident", [128, 128])
    I["sel"] = din("sel", [128, 64, 128])
    I["maskw"] = din("maskw", [128, 384])
    I["ropec"] = din("ropec", [SEQ, 32])
    I["ropes"] = din("ropes", [SEQ, 32])
    I["attn_sink"] = din("attn_sink", [DEPTH, 16])
    I["s5_sm"] = din("s5_sm", [DEPTH, 128, 2, 3, 8])
    I["s5_rows"] = din("s5_rows", [DEPTH, 2, 3, 1024])
    I["s5_bt"] = din("s5_bt", [DEPTH, 2, 8, 128, 128])
    I["s5_ct"] = din("s5_ct", [DEPTH, 2, 2, 8, 128, 128])
    I["pw2"] = din("pw2", [1, 16])
    I["cmasks"] = din("cmasks", [128, 7, 128])
    I["w_branch"] = din("w_branch", [DEPTH, 4, 256, D])
    I["w_out"] = din("w_out", [DEPTH, D, D])
    I["w_router"] = din("w_router", [DEPTH, D, 36])
    I["b_router"] = din("b_router", [DEPTH, 36])
    I["w_exp_gate"] = din("w_exp_gate", [DEPTH, 32, D, 512])
    I["w_exp_up"] = din("w_exp_up", [DEPTH, 32, D, 512])
    I["w_exp_down"] = din("w_exp_down", [DEPTH, 32, 512, D])
    I["s5_d_fm"] = din("s5_d_fm", [DEPTH, 128, 16])
    I["s5_bglu_fm"] = din("s5_bglu_fm", [DEPTH, 128, 16])
    I["s5_w_glu"] = din("s5_w_glu", [DEPTH, 256, 256])
    I["pv"] = din("pv", [DEPTH, NPV])
    I["w1cat"] = din("w1cat", [DEPTH, 256, 128])
    I["w2blk"] = din("w2blk", [DEPTH, 128, 1024])
    I["g1"] = din("g1", [DEPTH, 256, 64])
    I["g2"] = din("g2", [DEPTH, 64, 256])
    I["a2blk"] = din("a2blk", [DEPTH, 32, 256])
    I["final_g"] = din("final_g", [1, D])
    C.I = I

    out = V(nc.dram_tensor("out", [SEQ, D], F32, kind="ExternalOutput").ap(), Buf("out"))
    C.out = out

    def dout(name, shape, dt=F32):
        return V(nc.dram_tensor(name, list(shape), dt, kind="ExternalOutput").ap(), Buf(name))
    C.dout = dout

    xres_ap = P.dram("xres", [NTOK, D], F32, kind=skind)
    C.xres = [V(xres_ap[t * 128:(t + 1) * 128, :], Buf("xres%d" % t)) for t in range(NT)]
    C.U_ap = P.dram("U", [NTOK + 3, O4], F32, kind=skind)
    C.U_bufs = [Buf("U%d" % t) for t in range(NT)]
    C.U_pad = Buf("Upad")
    C.STR_ap = [P.dram("STR%d" % d, [NTOK, 2, 1024], BF16, kind=skind) for d in range(2)]
    C.STR_bufs = [[Buf("STR%d_%d" % (d, t)) for t in range(NT)] for d in range(2)]
    C.Vs_ap = P.dram("Vs", [128, NTOK, 6], F32, kind=skind)
    C.Vs_bufs = [Buf("Vs%d" % t) for t in range(NT)]
    C.Y_ap = [P.dram("Y%d" % d, [128, NTOK, 6], F32, kind=skind) for d in range(2)]
    C.Y_bufs = [[Buf("Y%d_%d" % (d, c)) for c in range(NTOK // 64)] for d in range(2)]
    C.FIN_ap = P.dram("FIN", [NTOK, 768], F32, kind=skind)
    C.FIN_bufs = [Buf("FIN%d" % t) for t in range(NT)]
    C.YB_ap = P.dram("YB", [4, 256, NTOK], BF16, kind=skind)
    C.YB_bufs = [[Buf("YB%d_%d" % (i, t)) for t in range(NT)] for i in range(4)]
    C.CH_ap = P.dram("CH", [NTOK, 3072], F32, kind=skind)
    C.CH_bufs = [Buf("CH%d" % t) for t in range(NT)]
    C.YT_ap = [P.dram("YT%d" % d, [NTOK, 256], F32, kind=skind) for d in range(2)]
    C.YT_bufs = [[Buf("YT%d_%d" % (d, t)) for t in range(NT)] for d in range(2)]
    C.YTG_ap = [P.dram("YTG%d" % d, [NTOK, 256], F32, kind=skind) for d in range(2)]
    C.YTG_bufs = [[Buf("YTG%d_%d" % (d, t)) for t in range(NT)] for d in range(2)]
    C.Gt_ap = P.dram("Gt", [4096, NTOK], BF16, kind=skind)
    C.Gt_bufs = [Buf("Gt%d" % b) for b in range(9)]
    C.H2_ap = P.dram("H2", [128, 8, NTOK], BF16, kind=skind)
    C.H2_buf = Buf("H2")

    G = Arena(P)
    C.G = G
    C.psall = nc.alloc_psum_tensor("psall", [128, 4096], F32).ap()
    C.psb = [Buf("ps%d" % i) for i in range(8)]
    C.ps = [V(C.psall[:, i * 512:(i + 1) * 512], C.psb[i]) for i in range(8)]
    C.ident = G.sb("ident", [128, 128])
    dma(P, "sp", C.ident, I["ident"])

    for l in range(DEPTH):
        layer(C, l)
        if dbg.get("stop_layer") == l:
            break
    if not dbg.get("stop"):
        final_norm(C)
    P.barrier()
    return nc


def urow(t):
    return 1 + t * 128 if t < 2 else 258 + (t - 2) * 128


def xsrc(C, l, t):
    if l == 0 and not C.__dict__.get("x_in_scratch"):
        if t < 2:
            return C.I["ctxb"][t * 128:(t + 1) * 128, :]
        return C.I["xb"][(t - 2) * 128:(t - 1) * 128, :]
    return C.xres[t]


def phase_ada(C, l, L):
    P, I = C.P, C.I
    A = Arena(P)
    cc = A.sb("cc", [128, 8, 2])
    sc = A.sb("sc", [128, 8, 2])
    screp = A.sb("screp", [128, 8, 2, 128])
    bfm = A.sb("bfm", [128, 48])
    g1 = A.sb("g1", [128, 8])
    g2 = A.sb("g2", [128, 8])
    wst = [A.sb("wst%d" % i, [128, 8, 512]) for i in range(2)]
    brow = [A.sb("brow%d" % i, [128, 1024]) for i in range(2)]
    dma(P, "sp", cc, I["cc"])
    dma(P, "sp", bfm, I["b_ada_fm"][l])
    dma(P, "sp", g1, I["g1_fm"][l])
    dma(P, "sp", g2, I["g2_fm"][l])
    for ii, i in enumerate((2, 5)):
        dma(P, "pool", brow[ii], I["b_ada"][l:l + 1, i * 1024:(i + 1) * 1024].bc([128, 1024]))
    act(P, sc, cc, AF.Silu)
    for k in range(8):
        for j in range(2):
            cp(P, "dve", screp[:, k, j, :], sc[:, k, j:j + 1].bc([128, 128]))
    wv = I["w_ada"][l].rr("(k p) n -> p k n", p=128)
    psA = C.ps[0]
    for c in range(12):
        w = wst[c % 2]
        dma(P, "sp" if c % 2 == 0 else "pool", w, wv[:, :, c * 512:(c + 1) * 512])
        for mi in range(4):
            m = c * 4 + mi
            for k in range(8):
                mm(P, psA[:, m * 2:(m + 1) * 2], w[:, k, mi * 128:(mi + 1) * 128], sc[:, k, :], k == 0, k == 7)
        if c in (4, 5, 10, 11):
            ii = 0 if c < 6 else 1
            half = c % 2
            for j in range(2):
                pr = C.ps[1 + j]
                for k in range(8):
                    mm(P, pr, screp[:, k, j, :], w[:, k, :], k == 0, k == 7)
                tt(P, "dve", L.grow[ii][j][:, half * 512:(half + 1) * 512], pr,
                   brow[ii][:, half * 512:(half + 1) * 512], ALU.add)
    tt(P, "dve", L.mod, psA[:, 0:96].rr("p (m j) -> p m j", j=2), bfm[:, :, None].bc([128, 48, 2]), ALU.add)
    ts(P, "dve", L.sc1, L.mod[:, 8:16, :], 1.0, ALU.add)
    tt(P, "dve", L.sc1, L.sc1, g1[:, :, None].bc([128, 8, 2]), ALU.mult)
    ts(P, "dve", L.sc2, L.mod[:, 32:40, :], 1.0, ALU.add)
    tt(P, "dve", L.sc2, L.sc2, g2[:, :, None].bc([128, 8, 2]), ALU.mult)
    A.close()


def phase_norm(C, l, L, which, hfm, per_tile=None):
    P = C.P
    A = Arena(P)
    sc = L.sc1 if which == 1 else L.sc2
    shb = 0 if which == 1 else 24
    xt = [A.sb("xt%d" % i, [128, D]) for i in range(2)]
    junk = A.sb("junk", [128, D])
    st = [A.sb("st%d" % i, [128, 2]) for i in range(2)]
    hf = [A.sb("hf%d" % i, [128, 8, 128]) for i in range(2)] if per_tile else None
    for t in range(NT):
        j = 1 if t < 2 else 0
        x = xt[t % 2]
        s = st[t % 2]
        dma(P, "sp" if t % 2 == 0 else "pool", x, xsrc(C, l, t))
        act(P, junk, x, AF.Square, accum_out=s[:, 0:1])
        ts(P, "dve", s[:, 1:2], s[:, 0:1], 1.0 / D, ALU.mult, EPS, ALU.add)
        act(P, s[:, 1:2], s[:, 1:2], AF.Sqrt)
        P.op("dve", "reciprocal", out=s[:, 1:2], in_=s[:, 1:2])
        ts(P, "dve", x, x, s[:, 1:2], ALU.mult)
        pa, pb = C.ps[2 + 2 * (t % 2)], C.ps[3 + 2 * (t % 2)]
        if C.dbg.get("dump_norm") and t == 2 and which == 1:
            dma(P, "sp", C.dout("d_xn", [128, D]), x)
            dma(P, "sp", C.dout("d_st", [128, 2]), s)
        for k in range(8):
            pp = pa if k < 4 else pb
            tr(P, pp[:, (k % 4) * 128:(k % 4 + 1) * 128], x[:, k * 128:(k + 1) * 128], C.ident)
        if C.dbg.get("dump_norm") and t == 2 and which == 1:
            cp(P, "dve", junk[:, 0:512], pa)
            dma(P, "sp", C.dout("d_pa", [128, 512]), junk[:, 0:512])
        for k in range(8):
            pp = pa if k < 4 else pb
            src = pp[:, (k % 4) * 128:(k % 4 + 1) * 128]
            if per_tile:
                dst = hf[t % 2][:, k, :]
            else:
                dst = hfm[:, k, t * 128:(t + 1) * 128]
            if k % 2 == 0:
                act(P, dst, src, AF.Identity, scale=sc[:, k, j:j + 1], bias=L.mod[:, shb + k, j:j + 1])
            else:
                ts(P, "dve", dst, src, sc[:, k, j:j + 1], ALU.mult, L.mod[:, shb + k, j:j + 1], ALU.add)
        if per_tile:
            for k in range(8):
                cp(P, "act" if k % 2 == 0 else "dve", hfm[:, k, t * 128:(t + 1) * 128], hf[t % 2][:, k, :])
            per_tile(t, hf[t % 2])
    A.close()


def phase_win_tm(C, l, L, hfm):
    P, I = C.P, C.I
    A = Arena(P)
    wA = A.sb("wA", [128, 8, O4], BF16)
    wst = [A.sb("wst%d" % i, [128, 8, 512]) for i in range(2)]
    ust = [A.sb("ust%d" % i, [128, O4]) for i in range(2)]
    zer = A.sb("zer", [1, O4])
    P.op("dve", "memset", ap=zer, constant=0.0)
    for r in (0, 257, NTOK + 2):
        dma(P, "sp", V(C.U_ap[r:r + 1, :], C.U_pad), zer)
    wv = I["w_in"][l].rr("(k p) n -> p k n", p=128)
    blocks = [(0, 512), (512, 1024), (1024, 1536), (1536, 1824), (1824, 2336), (2336, 2592)]
    for bi, (c0, c1) in enumerate(blocks):
        w = wst[bi % 2]
        dma(P, "sp" if bi % 2 == 0 else "pool", w[:, :, 0:c1 - c0], wv[:, :, c0:c1])
        cp(P, "act", wA[:, :, c0:c1], w[:, :, 0:c1 - c0])
    n = 0
    for t in range(NT):
        u = ust[t % 2]
        for bi, (c0, c1) in enumerate(blocks):
            ps = C.ps[n % 4]
            n += 1
            for k in range(8):
                mm(P, ps[:, 0:c1 - c0], hfm[:, k, t * 128:(t + 1) * 128], wA[:, k, c0:c1], k == 0, k == 7)
            cp(P, "act" if bi % 2 == 0 else "dve", u[:, c0:c1], ps[:, 0:c1 - c0])
        r0 = urow(t)
        dma(P, "sp" if t % 2 == 0 else "pool", V(C.U_ap[r0:r0 + 128, :], C.U_bufs[t]), u)
    A.close()


def stt(P, e, out, in0, scalar, in1, op0, op1, **kw):
    return P.op(e, "scalar_tensor_tensor", out=out, in0=in0, scalar=scalar, in1=in1, op0=op0, op1=op1, **kw)


def red(P, e, out, in_, **kw):
    return P.op(e, "tensor_reduce", out=out, in_=in_, axis=AX.X, op=ALU.add, **kw)


def load_bf16(P, A, name, shape, src, q="sp", ce="act"):
    st = A.sb(name + "_f", shape)
    wb = A.sb(name, shape, BF16)
    dma(P, q, st, src)
    cp(P, ce, wb, st)
    return wb


def cust(v, offset_elems, dims):
    ap = v.ap
    base = ap.ap[0]
    new = type(ap)(ap.tensor, ap.offset + offset_elems, [tuple(base)] + [tuple(d) for d in dims])
    return V(new, v.bufs)


def phase_prep(C, l, L):
    P, I = C.P, C.I
    A = Arena(P)
    pv = A.sb("pv", [128, NPV])
    dma(P, "sp", pv, I["pv"][l:l + 1, :].bc([128, NPV]))
    w1cat = load_bf16(P, A, "w1cat", [128, 2, 128], I["w1cat"][l].rr("(k p) n -> p k n", p=128))
    w2blk = load_bf16(P, A, "w2blk", [128, 1024], I["w2blk"][l])
    g1 = load_bf16(P, A, "g1w", [128, 2, 64], I["g1"][l].rr("(k p) n -> p k n", p=128))
    g2 = load_bf16(P, A, "g2w", [64, 256], I["g2"][l])
    a2blk = load_bf16(P, A, "a2blk", [32, 256], I["a2blk"][l])
    mu = pv[:, PV_MU:PV_MU + 1024]
    kkp = pv[:, PV_KK:PV_KK + 256]
    ka = pv[:, PV_KA:PV_KA + 256]
    rkp = pv[:, PV_RK:PV_RK + 256]
    w0 = pv[:, PV_W0:PV_W0 + 512]
    a0 = pv[:, PV_A0:PV_A0 + 512]
    gab = pv[:, PV_GAB:PV_GAB + 256]
    glng = pv[:, PV_GLNG:PV_GLNG + 256]

    uc = [A.sb("uc%d" % i, [128, O2]) for i in range(2)]
    up = [A.sb("up%d" % i, [128, 1024]) for i in range(2)]
    un = [A.sb("un%d" % i, [128, 1024]) for i in range(2)]
    rows = [[A.sb("rows%d%d" % (d, i), [128, 2, 1024], BF16) for i in range(2)] for d in range(2)]
    vt = [A.sb("vt%d" % i, [128, 128, 6]) for i in range(2)]
    fin = [A.sb("fin%d" % i, [128, 768]) for i in range(2)]
    cht = [A.sb("cht%d" % i, [128, 3072]) for i in range(2)]
    t0 = A.sb("t0", [128, 1024])
    mx = A.sb("mx", [128, 1024])
    xaT = A.sb("xaT", [128, 2, 128], BF16)
    z = A.sb("z", [128, 128], BF16)
    sg = A.sb("sg", [64, 128], BF16)
    wl = A.sb("wl", [128, 512])
    wdec = A.sb("wdec", [128, 512])
    il = A.sb("il", [128, 512])
    iclr = A.sb("iclr", [128, 512])
    kk0 = A.sb("kk0", [128, 256])
    sq = A.sb("sq", [128, 256])
    ss = A.sb("ss", [128, 8])
    kk = A.sb("kk", [128, 256])
    t1 = A.sb("t1", [128, 512])
    keff = A.sb("keff", [128, 512])
    bb = A.sb("bb", [128, 512])
    rkt = A.sb("rkt", [128, 256])
    alT = A.sb("alT", [32, 128], BF16)
    gl = A.sb("gl", [128, 256])
    gdec = A.sb("gdec", [128, 256])
    sr = A.sb("sr", [128, 256])

    def rv(R, c0, n):
        return R[:, :, c0:c0 + n]

    for t in range(NT):
        i = t % 2
        r0 = urow(t)
        nb = [C.U_bufs[t]]
        if t > 0:
            nb.append(C.U_bufs[t - 1])
        if t < NT - 1:
            nb.append(C.U_bufs[t + 1])
        nb.append(C.U_pad)
        dma(P, "sp", uc[i], V(C.U_ap[r0:r0 + 128, 0:O2], C.U_bufs[t]))
        dma(P, "pool", up[i], V(C.U_ap[r0 - 1:r0 + 127, 0:1024], tuple(nb)))
        dma(P, "sp", un[i], V(C.U_ap[r0 + 1:r0 + 129, 0:1024], tuple(nb)))
        u = uc[i]
        R0, R1 = rows[0][i], rows[1][i]
        F = fin[i]
        tt(P, "dve", t0, up[i], un[i], ALU.add)
        stt(P, "dve", t0, t0, 0.5, u[:, 0:1024], ALU.mult, ALU.subtract)
        tt(P, "dve", t0, t0, mu, ALU.mult)
        tt(P, "dve", mx, t0, u[:, 0:1024], ALU.add)
        r_, k_, v_, xa_ = mx[:, 0:256], mx[:, 256:512], mx[:, 512:768], mx[:, 768:1024]
        pT = C.ps[0]
        for kt in range(2):
            tr(P, pT[:, kt * 128:(kt + 1) * 128], xa_[:, kt * 128:(kt + 1) * 128], C.ident)
        cp(P, "act", xaT, pT[:, 0:256].rr("p (k n) -> p k n", k=2))
        pz = C.ps[1]
        for kt in range(2):
            mm(P, pz[:, 0:128], w1cat[:, kt, :], xaT[:, kt, :], kt == 0, kt == 1)
        for kt in range(2):
            mm(P, pz[0:64, 128:256], g1[:, kt, :], xaT[:, kt, :], kt == 0, kt == 1)
        act(P, z[0:64, :], pz[0:64, 0:128], AF.Tanh)
        cp(P, "dve", z[64:128, :], pz[64:128, 0:128])
        act(P, sg, pz[0:64, 128:256], AF.Sigmoid)
        pw, pa_, pg = C.ps[2], C.ps[3], C.ps[4]
        mm(P, pw, z, w2blk[:, 0:512], True, True)
        mm(P, pa_, z, w2blk[:, 512:1024], True, True)
        mm(P, pg[:, 0:256], sg, g2, True, True)
        tt(P, "dve", wl, pw, w0, ALU.add)
        act(P, wl, wl, AF.Sigmoid)
        act(P, wdec, wl, AF.Exp, scale=-0.6065306597126334)
        tt(P, "dve", il, pa_, a0, ALU.add)
        act(P, iclr, il, AF.Sigmoid)
        cp(P, "act", F[:, 0:256], pg[:, 0:256])
        tt(P, "dve", kk0, k_, kkp, ALU.mult)
        tt(P, "dve", sq, kk0, kk0, ALU.mult)
        red(P, "dve", ss[:, 0:4], sq.rr("p (h k) -> p h k", h=4))
        ts(P, "dve", ss[:, 0:4], ss[:, 0:4], EPS, ALU.add)
        act(P, ss[:, 0:4], ss[:, 0:4], AF.Sqrt)
        P.op("dve", "reciprocal", out=ss[:, 0:4], in_=ss[:, 0:4])
        tt(P, "dve", kk.rr("p (h k) -> p h k", h=4), kk0.rr("p (h k) -> p h k", h=4),
           ss[:, 0:4][:, :, None].bc([128, 4, 64]), ALU.mult)
        ic3 = iclr.rr("p (d c) -> p d c", d=2)
        stt(P, "dve", t1.rr("p (d c) -> p d c", d=2), ic3, -1.0, ka[:, None, :].bc([128, 2, 256]), ALU.add, ALU.mult)
        stt(P, "dve", keff.rr("p (d c) -> p d c", d=2), t1.rr("p (d c) -> p d c", d=2), 1.0,
            k_[:, None, :].bc([128, 2, 256]), ALU.add, ALU.mult)
        tt(P, "dve", bb.rr("p (d c) -> p d c", d=2), ic3, kk[:, None, :].bc([128, 2, 256]), ALU.mult)
        tt(P, "dve", rkt, r_, k_, ALU.mult)
        tt(P, "dve", rkt, rkt, rkp, ALU.mult)
        red(P, "dve", ss[:, 4:8], rkt.rr("p (h k) -> p h k", h=4))
        tt(P, "dve", F[:, 256:512].rr("p (h k) -> p h k", h=4), v_.rr("p (h k) -> p h k", h=4),
           ss[:, 4:8][:, :, None].bc([128, 4, 64]), ALU.mult)
        pT2 = C.ps[5]
        tr(P, pT2[0:32, 0:128], u[:, O1 + 768:O1 + 800], C.ident)
        cp(P, "act", alT, pT2[0:32, 0:128])
        mm(P, pT2[:, 128:384], alT, a2blk, True, True)
        tt(P, "dve", gl, pT2[:, 128:384], gab, ALU.add)
        act(P, gl, gl, AF.Sigmoid)
        act(P, gl, gl, AF.Ln)
        if C.dbg.get("old_gla"):
            act(P, gdec, gl, AF.Exp, scale=1.0 / 16.0)
        act(P, sr, u[:, O1 + 512:O1 + 768], AF.Silu)
        tt(P, "dve", F[:, 512:768], sr, glng, ALU.mult)
        CHt = cht[i]
        ts(P, "dve", CHt[:, 0:512], wl, -0.6065306597126334, ALU.mult)
        cp(P, "act", CHt[:, 512:1024], keff)
        cp(P, "dve", CHt[:, 1024:1536], bb)
        ts(P, "dve", CHt[:, 1536:1792], kk, -1.0, ALU.mult)
        cp(P, "act", CHt[:, 1792:2048], r_)
        cp(P, "dve", CHt[:, 2048:2304], v_)
        ts(P, "dve", CHt[:, 2304:2560], gl, 1.0 / 16.0, ALU.mult)
        ts(P, "dve", CHt[:, 2560:2688], u[:, O1:O1 + 128], 32.0 ** -0.5, ALU.mult)
        cp(P, "act", CHt[:, 2688:2816], u[:, O1 + 128:O1 + 256])
        cp(P, "dve", CHt[:, 2816:3072], u[:, O1 + 256:O1 + 512])
        dma(P, "sp", V(C.CH_ap[t * 128:(t + 1) * 128, :], C.CH_bufs[t]), CHt)
        if not C.dbg.get("old_gla"):
            dma(P, "pool", V(C.FIN_ap[t * 128:(t + 1) * 128, :], C.FIN_bufs[t]), F)
            continue
        for d, R in ((0, R0), (1, R1)):
            e1 = "dve" if d == 0 else "pool"
            e2 = "pool" if d == 0 else "dve"
            src = wdec[:, d * 256:(d + 1) * 256].rr("p (a h k) -> p a h k", a=2, h=2)
            hi = rv(R, 0, 128).rr("p h (a k) -> p a h k", a=2)
            lo = rv(R, 192, 128).rr("p h (a k) -> p a h k", a=2)
            cp(P, e1, hi, src)
            tt(P, e1, lo, src, hi, ALU.subtract)
            gsrc = gdec[:, d * 128:(d + 1) * 128].rr("p (a h k) -> p a h k", a=2, h=2)
            ghi = rv(R, 128, 64).rr("p h (a k) -> p a h k", a=2)
            glo = rv(R, 320, 64).rr("p h (a k) -> p a h k", a=2)
            cp(P, e2, ghi, gsrc)
            tt(P, e2, glo, gsrc, ghi, ALU.subtract)
            cp(P, e1, rv(R, 384, 128).rr("p h (a k) -> p a h k", a=2),
               keff[:, d * 256:(d + 1) * 256].rr("p (a h k) -> p a h k", a=2, h=2))
            cp(P, e2, rv(R, 512, 64).rr("p h (a k) -> p a h k", a=2),
               u[:, O1 + 128:O1 + 256].rr("p (a h k) -> p a h k", a=2, h=2))
            cp(P, e1, rv(R, 576, 128).rr("p h (a k) -> p a h k", a=2), r_.rr("p (a h k) -> p a h k", a=2, h=2))
            ts(P, e2, rv(R, 704, 64).rr("p h (a k) -> p a h k", a=2),
               u[:, O1:O1 + 128].rr("p (a h k) -> p a h k", a=2, h=2), 32.0 ** -0.5, ALU.mult)
            ts(P, e1, rv(R, 768, 128).rr("p h (a k) -> p a h k", a=2), kk.rr("p (a h k) -> p a h k", a=2, h=2),
               -1.0, ALU.mult)
            cp(P, e2, rv(R, 896, 128).rr("p h (a k) -> p a h k", a=2),
               bb[:, d * 256:(d + 1) * 256].rr("p (a h k) -> p a h k", a=2, h=2))
            dma(P, "sp" if d == 0 else "pool",
                V(C.STR_ap[d][t * 128:(t + 1) * 128].rearrange("t h n -> t (h n)"), C.STR_bufs[d][t]),
                R.rr("p h n -> p (h n)"))
        pv4 = C.ps[6]
        for a in range(2):
            tr(P, pv4[:, a * 128:(a + 1) * 128], v_[:, a * 128:(a + 1) * 128], C.ident)
        for a in range(2):
            tr(P, pv4[:, (2 + a) * 128:(3 + a) * 128], u[:, O1 + 256 + a * 128:O1 + 384 + a * 128], C.ident)
        VT = vt[i]
        cp(P, "act", VT[:, :, 0], pv4[:, 0:128])
        cp(P, "dve", VT[:, :, 1], pv4[:, 0:128])
        cp(P, "act", VT[:, :, 2], pv4[:, 128:256])
        cp(P, "dve", VT[:, :, 3], pv4[:, 128:256])
        cp(P, "act", VT[:, :, 4], pv4[:, 256:384])
        cp(P, "dve", VT[:, :, 5], pv4[:, 384:512])
        dma(P, "sp", V(C.Vs_ap[:, t * 128:(t + 1) * 128, :], C.Vs_bufs[t]), VT)
        dma(P, "pool", V(C.FIN_ap[t * 128:(t + 1) * 128, :], C.FIN_bufs[t]), F)
    A.close()


def phase_scan(C, l, nchunks=None):
    P, I = C.P, C.I
    A = Arena(P)
    S = A.sb("S", [128, 2, 64])
    T3 = A.sb("T3", [128, 2, 64])
    T4 = A.sb("T4", [128, 2, 64])
    self_f = A.sb("sel_f", [128, 64, 128])
    sel = A.sb("sel", [128, 64, 128], BF16)
    dma(P, "sp", self_f, I["sel"])
    cp(P, "dve", sel, self_f)
    rows = [[A.sb("srow%d%d" % (d, i), [128, 1024], BF16) for i in range(2)] for d in range(2)]
    vb = [A.sb("vb%d" % i, [128, 2, 64, 6]) for i in range(2)]
    yb = [A.sb("yb%d" % i, [128, 2, 64, 6]) for i in range(2)]
    P.op("dve", "memset", ap=S, constant=0.0)
    for i in range(2):
        P.op("pool", "memset", ap=yb[i], constant=0.0)
    NCH = NTOK // 64
    for c in range(NCH if nchunks is None else nchunks):
        zf = c * 64
        zb = (192 - 64 * c) if c < 4 else (4544 - 64 * c)
        i = c % 2
        dma(P, "sp", rows[0][i], V(C.STR_ap[0][zf:zf + 64].rearrange("t h n -> (t h) n"), C.STR_bufs[0][zf // 128]))
        dma(P, "pool", rows[1][i], V(C.STR_ap[1][zb:zb + 64].rearrange("t h n -> (t h) n"), C.STR_bufs[1][zb // 128]))
        dma(P, "sp", vb[i][:, 0], V(C.Vs_ap[:, zf:zf + 64, :], C.Vs_bufs[zf // 128]))
        dma(P, "pool", vb[i][:, 1], V(C.Vs_ap[:, zb:zb + 64, :], C.Vs_bufs[zb // 128]))
        YB = yb[i]
        for j in range(64):
            s = c * 64 + j
            pb = (s % 2) * 2
            for d in range(2):
                jj = j if d == 0 else 63 - j
                lt = sel[:, jj, :]
                R = rows[d][i]
                bx = C.ps[pb + d]
                mm(P, bx[:, 0:64], lt, R[:, 128:192], True, False, mark=False)
                mm(P, bx[:, 0:64], lt, R[:, 320:384], False, True, mark=False)
                mm(P, bx[:, 64:128], lt, R[:, 512:576], True, True, mark=False)
                mm(P, bx[:, 128:192], lt, R[:, 704:768], True, True, mark=(d == 1))
            R4 = V(C.psall[:, pb * 512:pb * 512 + 1024].rearrange("p (d x) -> p d x", d=2), tuple(C.psb[pb:pb + 2]))
            Dv, KKv, RQv = R4[:, :, 0:64], R4[:, :, 64:128], R4[:, :, 128:192]
            P.op("dve", "tensor_tensor", lax=True, out=S, in0=S, in1=Dv, op=ALU.mult)
            vv = cust(vb[i], j * 6 + 4, [((127 - 2 * j) * 6, 2), (1, 2), (0, 32)])
            P.op("dve", "tensor_tensor", lax=True, out=T3.rr("p d (g k) -> p d g k", g=2),
                 in0=KKv.rr("p d (g k) -> p d g k", g=2), in1=vv, op=ALU.mult)
            P.op("dve", "tensor_tensor", lax=True, out=S, in0=S, in1=T3, op=ALU.add)
            P.op("dve", "tensor_tensor", lax=True, out=T4, in0=S, in1=RQv, op=ALU.mult)
            yv = cust(YB, j * 6 + 4, [((127 - 2 * j) * 6, 2), (1, 2)])
            P.op("dve", "tensor_reduce", lax=True, out=yv, in_=T4.rr("p d (g k) -> p d g k", g=2), axis=AX.X, op=ALU.add)
        dma(P, "sp", V(C.Y_ap[0][:, zf:zf + 64, :], C.Y_bufs[0][zf // 64]), YB[:, 0])
        dma(P, "pool", V(C.Y_ap[1][:, zb:zb + 64, :], C.Y_bufs[1][zb // 64]), YB[:, 1])
    A.close()


def phase_fin_ab(C, l, L):
    P, I = C.P, C.I
    A = Arena(P)
    pv = A.sb("pv", [128, NPV])
    dma(P, "sp", pv, I["pv"][l:l + 1, :].bc([128, NPV]))
    lng = pv[:, PV_LNG:PV_LNG + 256]
    yt = [A.sb("yt%d" % i, [128, 2, 128, 6]) for i in range(2)]
    fin = [A.sb("finf%d" % i, [128, 768]) for i in range(2)]
    ya = A.sb("ya", [128, 256])
    ytm = [A.sb("ytm%d" % i, [128, 2, 256]) for i in range(2)]
    ytg = [A.sb("ytg%d" % i, [128, 2, 256]) for i in range(2)]
    yg = A.sb("yg", [128, 256])
    sq = A.sb("sqf", [128, 256])
    st = A.sb("stf", [128, 16])
    ob = [A.sb("ob%d" % i, [128, 4, 128], BF16) for i in range(2)]
    for t in range(NT):
        i = t % 2
        Y = yt[i]
        F = fin[i]
        if C.dbg.get("old_gla"):
            for d in range(2):
                dma(P, "sp" if d == 0 else "pool", Y[:, d],
                    V(C.Y_ap[d][:, t * 128:(t + 1) * 128, :], (C.Y_bufs[d][2 * t], C.Y_bufs[d][2 * t + 1])))
        dma(P, "sp", F, V(C.FIN_ap[t * 128:(t + 1) * 128, :], C.FIN_bufs[t]))
        pr, pg = C.ps[0], C.ps[1]
        if C.dbg.get("old_rwkv"):
            for a in range(2):
                n = 0
                for d in range(2):
                    for g in (2 * a, 2 * a + 1):
                        mm(P, pr[:, a * 128:(a + 1) * 128], Y[:, d, :, g], C.ident, n == 0, n == 3)
                        n += 1
        if C.dbg.get("old_gla"):
            for a in range(2):
                for d in range(2):
                    mm(P, pg[:, a * 128:(a + 1) * 128], Y[:, d, :, 4 + a], C.ident, d == 0, d == 1)
        if C.dbg.get("old_rwkv"):
            cp(P, "act", ya, pr[:, 0:256])
        else:
            for d in range(2):
                dma(P, "sp" if d == 0 else "pool", ytm[i][:, d, :], V(C.YT_ap[d][t * 128:(t + 1) * 128, :], C.YT_bufs[d][t]))
            tt(P, "dve", ya, ytm[i][:, 0, :], ytm[i][:, 1, :], ALU.add)
        ya4 = ya.rr("p (h k) -> p h k", h=4)
        red(P, "dve", st[:, 0:4], ya4)
        ts(P, "dve", st[:, 0:4], st[:, 0:4], 1.0 / 64.0, ALU.mult)
        tt(P, "dve", ya4, ya4, st[:, 0:4][:, :, None].bc([128, 4, 64]), ALU.subtract)
        tt(P, "dve", sq, ya, ya, ALU.mult)
        red(P, "dve", st[:, 4:8], sq.rr("p (h k) -> p h k", h=4))
        ts(P, "dve", st[:, 4:8], st[:, 4:8], 1.0 / 64.0, ALU.mult, GN_EPS, ALU.add)
        act(P, st[:, 4:8], st[:, 4:8], AF.Sqrt)
        P.op("dve", "reciprocal", out=st[:, 4:8], in_=st[:, 4:8])
        tt(P, "dve", ya4, ya4, st[:, 4:8][:, :, None].bc([128, 4, 64]), ALU.mult)
        tt(P, "dve", ya, ya, lng, ALU.mult)
        tt(P, "dve", ya, ya, F[:, 256:512], ALU.add)
        tt(P, "dve", ya, ya, F[:, 0:256], ALU.mult)
        if C.dbg.get("old_gla"):
            cp(P, "act", yg, pg[:, 0:256])
        else:
            for d in range(2):
                dma(P, "sp" if d == 0 else "pool", ytg[i][:, d, :], V(C.YTG_ap[d][t * 128:(t + 1) * 128, :], C.YTG_bufs[d][t]))
            tt(P, "dve", yg, ytg[i][:, 0, :], ytg[i][:, 1, :], ALU.add)
        yg4 = yg.rr("p (h k) -> p h k", h=4)
        tt(P, "dve", sq, yg, yg, ALU.mult)
        red(P, "dve", st[:, 8:12], sq.rr("p (h k) -> p h k", h=4))
        ts(P, "dve", st[:, 8:12], st[:, 8:12], 1.0 / 64.0, ALU.mult, EPS, ALU.add)
        act(P, st[:, 8:12], st[:, 8:12], AF.Sqrt)
        P.op("dve", "reciprocal", out=st[:, 8:12], in_=st[:, 8:12])
        tt(P, "dve", yg4, yg4, st[:, 8:12][:, :, None].bc([128, 4, 64]), ALU.mult)
        tt(P, "dve", yg, yg, F[:, 512:768], ALU.mult)
        po = C.ps[2]
        for a in range(2):
            tr(P, po[:, a * 128:(a + 1) * 128], ya[:, a * 128:(a + 1) * 128], C.ident)
            tr(P, po[:, (2 + a) * 128:(3 + a) * 128], yg[:, a * 128:(a + 1) * 128], C.ident)
        OB = ob[i]
        cp(P, "act", OB, po.rr("p (a n) -> p a n", a=4))
        for br in range(2):
            dma(P, "sp" if br == 0 else "pool",
                V(C.YB_ap[br][:, t * 128:(t + 1) * 128].rearrange("(a p) n -> p a n", p=128), C.YB_bufs[br][t]),
                OB[:, 2 * br:2 * br + 2, :])
    A.close()


def phase_attn(C, l, L):
    P, I = C.P, C.I
    A = Arena(P)
    qT = A.sb("qT", [128, 2, NTOK], BF16)
    kT = A.sb("kT", [128, 2, NTOK], BF16)
    vtm = A.sb("vtm", [128, NT, 128], BF16)
    maskw = A.sb("maskw", [128, 384])
    sink = A.sb("sink", [128, 16])
    identb = A.sb("identb", [128, 128], BF16)
    dma(P, "sp", maskw, I["maskw"])
    dma(P, "sp", sink, I["attn_sink"][l:l + 1, :].bc([128, 16]))
    cp(P, "dve", identb, C.ident)
    ua = [A.sb("ua%d" % i, [128, 512]) for i in range(2)]
    rc = [A.sb("rc%d" % i, [128, 32]) for i in range(2)]
    rs = [A.sb("rs%d" % i, [128, 32]) for i in range(2)]
    qk = A.sb("qk", [128, 6, 64])
    tmp = A.sb("tmpr", [128, 6, 32])
    kd = A.sb("kd", [128, 2, 2, 64])
    cut = C.dbg.get("attn_cut", 9)
    for t in range(NT if cut > 1 else 0):
        i = t % 2
        r0 = urow(t)
        u = ua[i]
        dma(P, "sp", u, V(C.U_ap[r0:r0 + 128, O2:O3], C.U_bufs[t]))
        u6 = u[:, 0:384].rr("p (h d) -> p h d", h=6)
        if t >= 2 and not C.dbg.get("norope"):
            dma(P, "pool", rc[i], I["ropec"][(t - 2) * 128:(t - 1) * 128, :])
            dma(P, "pool", rs[i], I["ropes"][(t - 2) * 128:(t - 1) * 128, :])
            cb = rc[i][:, None, :].bc([128, 6, 32])
            sb_ = rs[i][:, None, :].bc([128, 6, 32])
            z1, z2 = u6[:, :, 0:32], u6[:, :, 32:64]
            tt(P, "dve", qk[:, :, 0:32], z1, cb, ALU.mult)
            tt(P, "dve", tmp, z2, sb_, ALU.mult)
            tt(P, "dve", qk[:, :, 0:32], qk[:, :, 0:32], tmp, ALU.subtract)
            tt(P, "dve", qk[:, :, 32:64], z1, sb_, ALU.mult)
            tt(P, "dve", tmp, z2, cb, ALU.mult)
            tt(P, "dve", qk[:, :, 32:64], qk[:, :, 32:64], tmp, ALU.add)
        else:
            cp(P, "dve", qk, u6)
        if cut < 3:
            continue
        cp(P, "dve", kd[:, :, 0, :], qk[:, 4:6, :])
        cp(P, "dve", kd[:, :, 1, :], qk[:, 4:6, :])
        cp(P, "dve", vtm[:, t, :], u[:, 384:512])
        if cut < 4:
            continue
        pq = C.ps[t % 2]
        qf = qk.rr("p h d -> p (h d)")
        kf = kd.rr("p k r d -> p (k r d)")
        for a in range(2):
            tr(P, pq[:, a * 128:(a + 1) * 128], qf[:, a * 128:(a + 1) * 128], C.ident)
            tr(P, pq[:, (2 + a) * 128:(3 + a) * 128], kf[:, a * 128:(a + 1) * 128], C.ident)
        ts(P, "dve", qT[:, :, t * 128:(t + 1) * 128], pq[:, 0:256].rr("p (a n) -> p a n", a=2), 0.125, ALU.mult)
        cp(P, "dve", kT[:, :, t * 128:(t + 1) * 128], pq[:, 256:512].rr("p (a n) -> p a n", a=2))
    if C.dbg.get("attn_p1"):
        A.close()
        return
    sc = [A.sb("sc%d" % i, [128, 640]) for i in range(2)]
    pb = [A.sb("pb%d" % i, [128, 640], BF16) for i in range(2)]
    pTs = [A.sb("pTs%d" % i, [128, 5, 128], BF16) for i in range(2)]
    st = [A.sb("sta%d" % i, [128, 8]) for i in range(2)]
    yo = [A.sb("yo%d" % i, [128, 256]) for i in range(2)]
    oc = [A.sb("oc%d" % i, [128, 2, 128], BF16) for i in range(2)]
    psT = [V(C.psall[:, b * 512:(b + 1) * 512].bitcast(BF16), C.psb[b]) for b in (4, 5)]
    n = 0
    for t in range(NT):
        YO = yo[t % 2]
        if t >= 2:
            lo, hi = max(t - 1, 2), min(t + 1, NT - 1)
            nw = hi - lo + 1
            m0 = (lo - (t - 1)) * 128
        else:
            nw = 0
        nk = nw * 128 + 256
        nblk = nw + 2
        kblocks = ([lo + b for b in range(nw)] if nw else []) + [0, 1]
        po = C.ps[6 + (t % 2)]
        for h in range(4):
            kv, hl = h // 2, h % 2
            i = n % 2
            n += 1
            S_, Pb, PT, ST = sc[i], pb[i], pTs[i], st[i]
            qv = qT[hl * 64:(hl + 1) * 64, kv, t * 128:(t + 1) * 128]
            pa_, pc_ = C.ps[2 * i], C.ps[2 * i + 1]
            if nw:
                mm(P, pa_[:, 0:nw * 128], qv, kT[hl * 64:(hl + 1) * 64, kv, lo * 128:(hi + 1) * 128], True, True)
            mm(P, pc_[:, 0:256], qv, kT[hl * 64:(hl + 1) * 64, kv, 0:256], True, True)
            if nw:
                tt(P, "dve", S_[:, 0:nw * 128], pa_[:, 0:nw * 128], maskw[:, m0:m0 + nw * 128], ALU.add)
            cp(P, "act", S_[:, nw * 128:nk], pc_[:, 0:256])
            P.op("dve", "tensor_reduce", out=ST[:, 0:1], in_=S_[:, 0:nk], axis=AX.X, op=ALU.max)
            tt(P, "dve", ST[:, 0:1], ST[:, 0:1], sink[:, h:h + 1], ALU.max)
            ts(P, "dve", ST[:, 1:2], ST[:, 0:1], -1.0, ALU.mult)
            act(P, Pb[:, 0:nk], S_[:, 0:nk], AF.Exp, bias=ST[:, 1:2], accum_out=ST[:, 2:3])
            act(P, ST[:, 3:4], sink[:, h:h + 1], AF.Exp, bias=ST[:, 1:2])
            tt(P, "dve", ST[:, 4:5], ST[:, 2:3], ST[:, 3:4], ALU.add)
            P.op("dve", "reciprocal", out=ST[:, 5:6], in_=ST[:, 4:5])
            pt = psT[i]
            for b in range(nblk):
                tr(P, pt[:, b * 128:(b + 1) * 128], Pb[:, b * 128:(b + 1) * 128], identb)
            cp(P, "act" if h % 2 == 0 else "dve", PT[:, 0:nblk, :], pt[:, 0:nblk * 128].rr("p (b n) -> p b n", b=nblk))
            for b in range(nblk):
                mm(P, po[:, h * 64:(h + 1) * 64], PT[:, b, :], vtm[:, kblocks[b], kv * 64:(kv + 1) * 64],
                   b == 0, b == nblk - 1)
            ts(P, "dve", YO[:, h * 64:(h + 1) * 64], po[:, h * 64:(h + 1) * 64], ST[:, 5:6], ALU.mult)
        pf = C.ps[t % 2]
        for a in range(2):
            tr(P, pf[:, a * 128:(a + 1) * 128], YO[:, a * 128:(a + 1) * 128], C.ident)
        OC = oc[t % 2]
        cp(P, "act", OC, pf[:, 0:256].rr("p (a n) -> p a n", a=2))
        dma(P, "sp", V(C.YB_ap[2][:, t * 128:(t + 1) * 128].rearrange("(a p) n -> p a n", p=128), C.YB_bufs[2][t]), OC)
    A.close()


PI = math.pi
S5_BLOCKS = [(0, 256)] + [(256 + 512 * i, 512) for i in range(8)]


I32 = mybir.dt.int32
TWO_PI_HI = 6.28125
TWO_PI_LO = 2.0 * math.pi - 6.28125


def sincos(P, A, s_out, c_out, x, shape, tag):
    qi = A.sb("qi" + tag, shape, I32)
    kf = A.sb("kf" + tag, shape)
    r = A.sb("rr" + tag, shape)
    m = A.sb("mm" + tag, shape)
    for extra, out in ((0.0, s_out), (0.5 * PI, c_out)):
        ts(P, "dve", r, x, 16.0 * PI + extra, ALU.add)
        ts(P, "dve", qi, r, 1.0 / (2.0 * PI), ALU.mult)
        cp(P, "dve", kf, qi)
        stt(P, "dve", r, kf, -TWO_PI_HI, r, ALU.mult, ALU.add)
        stt(P, "dve", r, kf, -TWO_PI_LO, r, ALU.mult, ALU.add)
        ts(P, "dve", m, r, PI, ALU.is_gt)
        stt(P, "dve", r, m, -2.0 * PI, r, ALU.mult, ALU.add)
        ts(P, "dve", m, r, -PI, ALU.is_lt)
        stt(P, "dve", r, m, 2.0 * PI, r, ALU.mult, ALU.add)
        ts(P, "dve", r, r, PI, ALU.min, -PI, ALU.max)
        act(P, out, r, AF.Sin)


def phase_s5(C, l, L):
    P, I = C.P, C.I
    A = Arena(P)
    th_s = A.sb("th_s", [128, 2, 8])
    rho_s = A.sb("rho_s", [128, 2, 8])
    Ck = A.sb("Ck", [128, 2, 8, 13])
    Sk = A.sb("Sk", [128, 2, 8, 13])
    BbT = A.sb("BbT", [128, 2, 2, 8, 128], BF16)
    CT = A.sb("CT", [128, 2, 2, 8, 128], BF16)
    A0 = A
    A = Arena(P)
    tmpA = [A.sb("s5r%d" % i, [128, 1024]) for i in range(8)]
    lre, lim, ldt, t_s, t_c, t_a, t_b, t_d = tmpA
    pw2f = A.sb("pw2", [128, 16])
    dma(P, "sp", pw2f, I["pw2"][0:1, :].bc([128, 16]))
    pw2 = pw2f[:, 0:13]
    fR = [A.sb("fR%d" % d, [128, 1024]) for d in range(2)]
    fI = [A.sb("fI%d" % d, [128, 1024]) for d in range(2)]
    sm = A.sb("sm", [128, 2, 3, 8])
    dma(P, "sp", sm, I["s5_sm"][l])
    act(P, sm[:, :, 2, :], sm[:, :, 2, :], AF.Exp)
    tt(P, "dve", th_s, sm[:, :, 1, :], sm[:, :, 2, :], ALU.mult)
    tt(P, "dve", rho_s, sm[:, :, 0, :], sm[:, :, 2, :], ALU.mult)
    act(P, rho_s, rho_s, AF.Exp)
    ang13 = A.sb("ang13", [128, 16, 13])
    tt(P, "dve", ang13, th_s.rr("p d j -> p (d j)")[:, :, None].bc([128, 16, 13]), pw2[:, None, :].bc([128, 16, 13]), ALU.mult)
    sincos(P, A, Sk.rr("p d j k -> p (d j) k"), Ck.rr("p d j k -> p (d j) k"), ang13, [128, 16, 13], "k")
    for d in range(2):
        dma(P, "sp", lre, I["s5_rows"][l, d, 0:1, :].bc([128, 1024]))
        dma(P, "pool", lim, I["s5_rows"][l, d, 1:2, :].bc([128, 1024]))
        dma(P, "sp", ldt, I["s5_rows"][l, d, 2:3, :].bc([128, 1024]))
        act(P, ldt, ldt, AF.Exp)
        tt(P, "dve", t_a, lim, ldt, ALU.mult)
        sincos(P, A, t_s, t_c, t_a, [128, 1024], "r%d" % d)
        tt(P, "dve", t_a, lre, ldt, ALU.mult)
        act(P, t_a, t_a, AF.Exp)
        tt(P, "dve", t_c, t_c, t_a, ALU.mult)
        tt(P, "dve", t_s, t_s, t_a, ALU.mult)
        ts(P, "dve", t_c, t_c, -1.0, ALU.add)
        tt(P, "dve", t_a, lre, lre, ALU.mult)
        tt(P, "dve", t_b, lim, lim, ALU.mult)
        tt(P, "dve", t_a, t_a, t_b, ALU.add)
        P.op("dve", "reciprocal", out=t_a, in_=t_a)
        tt(P, "dve", t_b, t_c, lre, ALU.mult)
        tt(P, "dve", t_d, t_s, lim, ALU.mult)
        tt(P, "dve", t_b, t_b, t_d, ALU.add)
        tt(P, "dve", fR[d], t_b, t_a, ALU.mult)
        tt(P, "dve", t_b, t_s, lre, ALU.mult)
        tt(P, "dve", t_d, t_c, lim, ALU.mult)
        tt(P, "dve", t_b, t_b, t_d, ALU.subtract)
        tt(P, "dve", fI[d], t_b, t_a, ALU.mult)
    bt_f = A.sb("bt_f", [128, 2, 8, 128])
    dma(P, "sp", bt_f[:, 0], I["s5_bt"][l, 0].rr("j c s -> c j s"))
    dma(P, "pool", bt_f[:, 1], I["s5_bt"][l, 1].rr("j c s -> c j s"))
    for d in range(2):
        fr = fR[d].rr("p (j s) -> p j s", j=8)
        fi = fI[d].rr("p (j s) -> p j s", j=8)
        ta = t_a.rr("p (j s) -> p j s", j=8)
        tb = t_b.rr("p (j s) -> p j s", j=8)
        tt(P, "dve", ta, bt_f[:, 0], fr, ALU.mult)
        tt(P, "dve", tb, bt_f[:, 1], fi, ALU.mult)
        tt(P, "dve", BbT[:, d, 0], ta, tb, ALU.subtract)
        tt(P, "dve", ta, bt_f[:, 0], fi, ALU.mult)
        tt(P, "dve", tb, bt_f[:, 1], fr, ALU.mult)
        tt(P, "dve", BbT[:, d, 1], ta, tb, ALU.add)
        for ri in range(2):
            ctf = t_c if ri == 0 else t_d
            dma(P, "sp" if ri == 0 else "pool", ctf.rr("p (j c) -> p j c", j=8), I["s5_ct"][l, d, ri].rr("j s c -> s j c"))
            if ri == 0:
                cp(P, "dve", CT[:, d, 0], ctf.rr("p (j c) -> p j c", j=8))
            else:
                ts(P, "dve", CT[:, d, 1], ctf.rr("p (j c) -> p j c", j=8), -1.0, ALU.mult)
    A.close()
    A = A0
    cut = C.dbg.get("s5_cut", 99)
    if cut <= 1:
        A.close(); return
    if not C.dbg.get("s5_small"):
        ct, sn = A.sb("ct", [128, NTOK]), A.sb("sn", [128, NTOK])
        w_re, w_im = A.sb("w_re", [128, NTOK]), A.sb("w_im", [128, NTOK])
    uB = A.sb("uB", [128, 2, NTOK], BF16)
    yacc = A.sb("yacc", [128, 2, NTOK])
    x_re, x_im = A.sb("x_re", [128, NTOK], BF16), A.sb("x_im", [128, NTOK], BF16)
    dsk = A.sb("dsk", [128, 16])
    bgl = A.sb("bgl", [128, 16])
    dma(P, "sp", dsk, I["s5_d_fm"][l])
    dma(P, "sp", bgl, I["s5_bglu_fm"][l])
    wglu = load_bf16(P, A, "wglu", [128, 2, 256], I["s5_w_glu"][l].rr("(k p) n -> p k n", p=128))
    tmA = [[A.sb("tm%d_%d" % (b, i), [128, 512]) for i in range(4)] for b in range(2)]
    ut = [tmA[1][i][:, 0:256] for i in range(2)]
    var = C.dbg.get("s5_var", 9)
    for t in range(NT if var > 0 else 0):
        i = t % 2
        r0 = urow(t)
        dma(P, "sp" if i == 0 else "pool", ut[i], V(C.U_ap[r0:r0 + 128, O3:O4], C.U_bufs[t]))
        pp = C.ps[i]
        for a in range(2):
            tr(P, pp[:, a * 128:(a + 1) * 128], ut[i][:, a * 128:(a + 1) * 128], C.ident)
        if var < 2:
            continue
        for a in range(2):
            cp(P, "dve", uB[:, a, t * 128:(t + 1) * 128], pp[:, a * 128:(a + 1) * 128])
        for a in range(2):
            ts(P, "dve", yacc[:, a, t * 128:(t + 1) * 128], pp[:, a * 128:(a + 1) * 128], dsk[:, a:a + 1], ALU.mult)
    tm = tmA[0]
    if cut <= 2:
        A.close(); return
    nb = 0
    for d in range(2):
        for j in range(8):
            jt = j // 4
            th = th_s[:, d, j:j + 1]
            P.op("dve", "memset", ap=ct[:, 0:1], constant=1.0)
            P.op("dve", "memset", ap=sn[:, 0:1], constant=0.0)
            k = 0
            n = 1
            while n < NTOK:
                m = min(n, NTOK - n)
                ck, sk = Ck[:, d, j, k:k + 1], Sk[:, d, j, k:k + 1]
                e1, e2 = ("dve", "dve")
                ts(P, e1, ct[:, n:n + m], ct[:, 0:m], ck, ALU.mult)
                ts(P, e2, sn[:, n:n + m], sn[:, 0:m], ck, ALU.mult)
                ts(P, e2, tm[0][:, 0:min(m, 512)] if m <= 512 else w_re[:, 0:m], sn[:, 0:m], sk, ALU.mult)
                ts(P, e1, tm[1][:, 0:min(m, 512)] if m <= 512 else w_im[:, 0:m], ct[:, 0:m], sk, ALU.mult)
                ta_ = tm[0][:, 0:m] if m <= 512 else w_re[:, 0:m]
                tb_ = tm[1][:, 0:m] if m <= 512 else w_im[:, 0:m]
                tt(P, e1, ct[:, n:n + m], ct[:, n:n + m], ta_, ALU.subtract)
                tt(P, e2, sn[:, n:n + m], sn[:, n:n + m], tb_, ALU.add)
                n += m
                k += 1
            if cut <= 3:
                A.close(); return
            for bi, (t0, n) in enumerate(S5_BLOCKS):
                if d == 0:
                    rhs = uB[:, jt, t0:t0 + n]
                else:
                    last = (255 - t0) if t0 < 256 else (4607 - t0)
                    rhs = cust(uB, jt * NTOK + last, [(-1, n)])
                pr, pi_ = C.ps[(nb % 2) * 2], C.ps[(nb % 2) * 2 + 1]
                nb += 1
                mm(P, pr[:, 0:n], BbT[:, d, 0, j, :], rhs, True, True)
                mm(P, pi_[:, 0:n], BbT[:, d, 1, j, :], rhs, True, True)
                c_, s_ = ct[:, t0:t0 + n], sn[:, t0:t0 + n]
                tm = tmA[bi % 2]
                tt(P, "dve", tm[0][:, 0:n], pr[:, 0:n], c_, ALU.mult)
                tt(P, "dve", tm[1][:, 0:n], pi_[:, 0:n], s_, ALU.mult)
                tt(P, "dve", w_re[:, t0:t0 + n], tm[0][:, 0:n], tm[1][:, 0:n], ALU.add)
                tt(P, "dve", tm[2][:, 0:n], pi_[:, 0:n], c_, ALU.mult)
                tt(P, "dve", tm[3][:, 0:n], pr[:, 0:n], s_, ALU.mult)
                tt(P, "dve", w_im[:, t0:t0 + n], tm[2][:, 0:n], tm[3][:, 0:n], ALU.subtract)
            if cut <= 4:
                A.close(); return
            rb = rho_s[:, d, j:j + 1].bc([128, NTOK])
            P.op("dve", "tensor_tensor_scan", out=w_re, data0=rb, data1=w_re, initial=0.0, op0=ALU.mult, op1=ALU.add)
            P.op("dve", "tensor_tensor_scan", out=w_im, data0=rb, data1=w_im, initial=0.0, op0=ALU.mult, op1=ALU.add)
            if cut <= 5:
                A.close(); return
            for bi, (t0, n) in enumerate(S5_BLOCKS):
                c_, s_ = ct[:, t0:t0 + n], sn[:, t0:t0 + n]
                tm = tmA[bi % 2]
                tt(P, "dve", tm[0][:, 0:n], w_re[:, t0:t0 + n], c_, ALU.mult)
                tt(P, "dve", tm[1][:, 0:n], w_im[:, t0:t0 + n], s_, ALU.mult)
                tt(P, "dve", x_re[:, t0:t0 + n], tm[0][:, 0:n], tm[1][:, 0:n], ALU.subtract)
                tt(P, "dve", tm[2][:, 0:n], w_re[:, t0:t0 + n], s_, ALU.mult)
                tt(P, "dve", tm[3][:, 0:n], w_im[:, t0:t0 + n], c_, ALU.mult)
                tt(P, "dve", x_im[:, t0:t0 + n], tm[2][:, 0:n], tm[3][:, 0:n], ALU.add)
                py = C.ps[4 + (bi % 2)]
                if d == 0:
                    xr, xi = x_re[:, t0:t0 + n], x_im[:, t0:t0 + n]
                    k0 = t0
                else:
                    k0 = (256 - t0 - n) if t0 < 256 else (4608 - t0 - n)
                    s_last = t0 + n - 1
                    xr, xi = cust(x_re, s_last, [(-1, n)]), cust(x_im, s_last, [(-1, n)])
                mm(P, py[:, 0:n], CT[:, d, 0, j, :], xr, True, False)
                mm(P, py[:, 0:n], CT[:, d, 1, j, :], xi, False, True)
                tt(P, "dve", yacc[:, jt, k0:k0 + n], yacc[:, jt, k0:k0 + n], py[:, 0:n], ALU.add)
    if cut <= 7:
        A.close(); return
    glb = uB
    tm = tmA[0]
    ob = [x_re[:, 0:1024].rr("p (a n) -> p a n", a=2), x_im[:, 0:1024].rr("p (a n) -> p a n", a=2)]
    for bi, (t0, n) in enumerate(S5_BLOCKS):
        for a in range(2):
            y = yacc[:, a, t0:t0 + n]
            tt(P, "dve", tm[0][:, 0:n], y, y, ALU.mult)
            ts(P, "dve", tm[0][:, 0:n], tm[0][:, 0:n], 0.044715, ALU.mult, 1.0, ALU.add)
            tt(P, "dve", tm[0][:, 0:n], tm[0][:, 0:n], y, ALU.mult)
            act(P, tm[0][:, 0:n], tm[0][:, 0:n], AF.Sigmoid, scale=1.5957691216057308)
            tt(P, "dve", y, y, tm[0][:, 0:n], ALU.mult)
            cp(P, "dve", glb[:, a, t0:t0 + n], y)
        OB = ob[bi % 2]
        for a in range(2):
            pz = C.ps[6 + a]
            for kt in range(2):
                mm(P, pz[:, 0:n], wglu[:, kt, a * 128:(a + 1) * 128], glb[:, kt, t0:t0 + n], kt == 0, kt == 1)
            act(P, tm[1 + a][:, 0:n], pz[:, 0:n], AF.Sigmoid, bias=bgl[:, a:a + 1])
            tt(P, "dve", OB[:, a, 0:n], yacc[:, a, t0:t0 + n], tm[1 + a][:, 0:n], ALU.mult)
        tl = [t for t in range(NT) if t * 128 >= t0 and t * 128 < t0 + n]
        dma(P, "sp", V(C.YB_ap[3][:, t0:t0 + n].rearrange("(a p) n -> p a n", p=128), tuple(C.YB_bufs[3][t] for t in tl)),
            OB[:, :, 0:n])
    A.close()


TOKBLK = [(0, 256)] + [(256 + 512 * i, 512) for i in range(8)]


def phase_win_gates(C, l, L, hfm):
    P, I = C.P, C.I
    A = Arena(P)
    wst = [A.sb("wgs%d" % i, [128, 8, 512]) for i in range(2)]
    wb = [A.sb("wgb%d" % i, [128, 8, 512], BF16) for i in range(2)]
    gst = [A.sb("gst%d" % i, [128, 512], BF16) for i in range(4)]
    wv = I["w_in"][l].rr("(k p) n -> p k n", p=128)
    n = 0
    for cb in range(8):
        c0 = O4 + cb * 512
        dma(P, "sp" if cb % 2 == 0 else "pool", wst[cb % 2], wv[:, :, c0:c0 + 512])
        cp(P, "act" if cb % 2 == 0 else "dve", wb[cb % 2], wst[cb % 2])
        w = wb[cb % 2]
        for mi in range(4):
            row0 = cb * 512 + mi * 128
            for bi, (t0, nn) in enumerate(TOKBLK):
                ps = C.ps[n % 4]
                g = gst[n % 4]
                for k in range(8):
                    mm(P, ps[:, 0:nn], w[:, k, mi * 128:(mi + 1) * 128], hfm[:, k, t0:t0 + nn], k == 0, k == 7)
                cp(P, "act" if n % 2 == 0 else "dve", g[:, 0:nn], ps[:, 0:nn])
                dma(P, "sp" if n % 2 == 0 else "pool", V(C.Gt_ap[row0:row0 + 128, t0:t0 + nn], C.Gt_bufs[bi]), g[:, 0:nn])
                n += 1
    A.close()


def phase_merge(C, l, L):
    P, I = C.P, C.I
    A = Arena(P)
    wbr = A.sb("wbr", [128, 4, 2, 1024], BF16)
    wout = A.sb("wout", [128, 8, 1024], BF16)
    wst = A.sb("wmst", [128, 8, 1024])
    for i in range(4):
        dma(P, "sp", wst[:, 0:2, :], I["w_branch"][l, i].rr("(k p) n -> p k n", p=128))
        cp(P, "act", wbr[:, i], wst[:, 0:2, :])
    dma(P, "sp", wst, I["w_out"][l].rr("(k p) n -> p k n", p=128))
    cp(P, "act", wout, wst)
    yb = [A.sb("myb%d" % i, [128, 4, 2, 512], BF16) for i in range(2)]
    gt4 = [A.sb("mgt%d" % i, [128, 4, 512], BF16) for i in range(2)]
    sg4 = [A.sb("msg%d" % i, [128, 4, 512]) for i in range(2)]
    tmp = A.sb("mtmp", [128, 512])
    acc = A.sb("macc", [128, 512])
    mg = [A.sb("mmg%d" % i, [128, 8, 512], BF16) for i in range(2)]
    xt = [A.sb("mxt%d" % i, [128, 1024]) for i in range(2)]
    tm2 = A.sb("mtm2", [128, 1024])
    n = 0
    nx = 0
    for bi, (t0, nn) in enumerate(TOKBLK):
        YB = yb[bi % 2]
        tl = [t for t in range(NT) if t0 <= t * 128 < t0 + nn]
        for i in range(4):
            dma(P, "sp" if i % 2 == 0 else "pool", YB[:, i, :, 0:nn],
                V(C.YB_ap[i][:, t0:t0 + nn].rearrange("(a p) n -> p a n", p=128), tuple(C.YB_bufs[i][t] for t in tl)))
        MG = mg[bi % 2]
        for m in range(8):
            g4 = gt4[m % 2]
            s4 = sg4[m % 2]
            dma(P, "sp" if m % 2 == 0 else "pool", g4[:, :, 0:nn],
                V(C.Gt_ap.rearrange("(i r) n -> r i n", i=4)[m * 128:(m + 1) * 128, :, t0:t0 + nn], C.Gt_bufs[bi]))
            act(P, s4[:, :, 0:nn], g4[:, :, 0:nn], AF.Sigmoid)
            for i in range(4):
                s_ = s4[:, i, :]
                ps = C.ps[n % 4]
                n += 1
                for kt in range(2):
                    mm(P, ps[:, 0:nn], wbr[:, i, kt, m * 128:(m + 1) * 128], YB[:, i, kt, 0:nn], kt == 0, kt == 1)
                if i == 0:
                    tt(P, "dve", acc[:, 0:nn], ps[:, 0:nn], s_[:, 0:nn], ALU.mult)
                elif i < 3:
                    tt(P, "dve", tmp[:, 0:nn], ps[:, 0:nn], s_[:, 0:nn], ALU.mult)
                    tt(P, "dve", acc[:, 0:nn], acc[:, 0:nn], tmp[:, 0:nn], ALU.add)
                else:
                    tt(P, "dve", tmp[:, 0:nn], ps[:, 0:nn], s_[:, 0:nn], ALU.mult)
                    tt(P, "dve", MG[:, m, 0:nn], acc[:, 0:nn], tmp[:, 0:nn], ALU.add)
        for ti, t in enumerate(tl):
            j = 1 if t < 2 else 0
            x = xt[nx % 2]
            nx += 1
            dma(P, "sp", x, xsrc(C, l, t))
            for half in range(2):
                po = C.ps[4 + half + 2 * (nx % 2)]
                for k in range(8):
                    mm(P, po, MG[:, k, ti * 128:(ti + 1) * 128], wout[:, k, half * 512:(half + 1) * 512], k == 0, k == 7)
                tt(P, "dve", tm2[:, half * 512:(half + 1) * 512], po, L.grow[0][j][:, half * 512:(half + 1) * 512], ALU.mult)
            tt(P, "dve", x, x, tm2, ALU.add)
            dma(P, "pool", C.xres[t], x)
    A.close()
    C.x_in_scratch = True


def make_router(C, l, L, RA):
    P, I = C.P, C.I
    wr = RA.sb("wr", [128, 8, 36])
    brow = RA.sb("brow", [128, 36])
    dma(P, "sp", wr, I["w_router"][l].rr("(k p) n -> p k n", p=128))
    dma(P, "sp", brow, I["b_router"][l:l + 1, :].bc([128, 36]))
    lg = RA.sb("lg", [128, 36])
    st = RA.sb("rst", [128, 16])
    oh = RA.sb("roh", [128, 4])
    em = RA.sb("rem", [128, 32])
    em2 = RA.sb("rem2", [128, 32])
    oh1 = RA.sb("roh1", [128, 32])
    oh2 = RA.sb("roh2", [128, 32])
    wg = RA.sb("rwg", [128, 32])
    junk = RA.sb("rjunk", [128, 4])

    def per_tile(t, hf):
        pl = C.ps[6]
        for k in range(8):
            mm(P, pl[:, 0:36], hf[:, k, :], wr[:, k, :], k == 0, k == 7)
        tt(P, "dve", lg, pl[:, 0:36], brow, ALU.add)
        g, e = lg[:, 0:4], lg[:, 4:36]
        P.op("dve", "tensor_reduce", out=st[:, 0:1], in_=g, axis=AX.X, op=ALU.max)
        ts(P, "dve", oh, g, st[:, 0:1], ALU.is_equal)
        ts(P, "dve", st[:, 1:2], st[:, 0:1], -1.0, ALU.mult)
        act(P, junk, g, AF.Exp, bias=st[:, 1:2], accum_out=st[:, 2:3])
        P.op("dve", "reciprocal", out=st[:, 3:4], in_=st[:, 2:3])
        ts(P, "dve", oh, oh, 1e30, ALU.mult, -1e30, ALU.add)
        tt(P, "dve", em.rr("p (g k) -> p g k", g=4), e.rr("p (g k) -> p g k", g=4),
           oh[:, :, None].bc([128, 4, 8]), ALU.add)
        P.op("dve", "tensor_reduce", out=st[:, 4:5], in_=em, axis=AX.X, op=ALU.max)
        ts(P, "dve", oh1, em, st[:, 4:5], ALU.is_equal)
        stt(P, "dve", em2, oh1, -1e30, em, ALU.mult, ALU.add)
        P.op("dve", "tensor_reduce", out=st[:, 5:6], in_=em2, axis=AX.X, op=ALU.max)
        ts(P, "dve", oh2, em2, st[:, 5:6], ALU.is_equal)
        tt(P, "dve", st[:, 6:7], st[:, 5:6], st[:, 4:5], ALU.subtract)
        act(P, st[:, 7:8], st[:, 6:7], AF.Exp)
        ts(P, "dve", st[:, 8:9], st[:, 7:8], 1.0, ALU.add)
        P.op("dve", "reciprocal", out=st[:, 9:10], in_=st[:, 8:9])
        tt(P, "dve", st[:, 10:11], st[:, 7:8], st[:, 9:10], ALU.mult)
        ts(P, "dve", wg, oh1, st[:, 9:10], ALU.mult)
        stt(P, "dve", wg, oh2, st[:, 10:11], wg, ALU.mult, ALU.add)
        ts(P, "dve", wg, wg, st[:, 3:4], ALU.mult)
        pt = C.ps[7]
        tr(P, pt[0:32, 0:128], wg, C.ident)
        cp(P, "dve", L.WT[:, t * 128:(t + 1) * 128], pt[0:32, 0:128])
    return per_tile


MOE_GROUPS = [list(range(0, 12)), list(range(12, 24)), list(range(24, 34))]


def phase_moe(C, l, L, H2, last):
    P, I = C.P, C.I
    A = Arena(P)
    hg = A.sb("hg", [128, 8, 12 * 128], BF16)
    wst = [A.sb("ews%d" % i, [128, 4, 512]) for i in range(2)]
    wgu = [A.sb("wgu%d" % i, [128, 2, 8, 512], BF16) for i in range(2)]
    wd = [A.sb("wd%d" % i, [128, 4, 1024], BF16) for i in range(2)]
    yacc = A.sb("eyacc", [128, 12, 1024])
    hid = [A.sb("hid%d" % i, [128, 4, 512], BF16) for i in range(2)]
    sil = [A.sb("sil%d" % i, [128, 512], BF16) for i in range(2)]
    tu = [A.sb("etu%d" % i, [128, 512]) for i in range(2)]
    xt = [A.sb("ext%d" % i, [128, 1024]) for i in range(2)]
    tm2 = A.sb("etm2", [128, 1024])
    ne = 0
    nst = 0
    nb = 0
    for G in MOE_GROUPS:
        tiles = [t for t in G if not (last and t < 2)]
        if not tiles:
            continue
        blocks = [tiles[i:i + 4] for i in range(0, len(tiles), 4)]
        g0 = tiles[0] * 128
        gn = len(tiles) * 128
        dma(P, "sp", hg[:, :, 0:gn], H2[:, :, g0:g0 + gn])
        for e in range(32):
            WGU, WD = wgu[ne % 2], wd[ne % 2]
            ne += 1
            for gi, nm in enumerate(("w_exp_gate", "w_exp_up")):
                src = I[nm][l, e].rr("(k p) n -> p k n", p=128)
                for hf_ in range(2):
                    s_ = wst[nst % 2]
                    dma(P, "sp" if nst % 2 == 0 else "pool", s_, src[:, hf_ * 4:(hf_ + 1) * 4, :])
                    cp(P, "act", WGU[:, gi, hf_ * 4:(hf_ + 1) * 4, :], s_)
                    nst += 1
            srcd = I["w_exp_down"][l, e].rr("(k p) n -> p k n", p=128)
            for hf_ in range(2):
                s_ = wst[nst % 2]
                dma(P, "sp" if nst % 2 == 0 else "pool", s_.rr("p k n -> p (k n)").rr("p (k n) -> p k n", k=2), srcd[:, hf_ * 2:(hf_ + 1) * 2, :])
                cp(P, "dve", WD[:, hf_ * 2:(hf_ + 1) * 2, :], s_.rr("p k n -> p (k n)").rr("p (k n) -> p k n", k=2))
                nst += 1
            for blk in blocks:
                t0 = blk[0] * 128
                nn = len(blk) * 128
                HID = hid[nb % 2]
                psW = C.ps[0]
                mm(P, psW[:, 0:nn], C.ident[0:32, e:e + 1].bc([32, 128]), L.WT[:, t0:t0 + nn], True, True)
                for f in range(4):
                    i2 = (nb * 4 + f) % 2
                    psG, psU = C.ps[1 + 2 * i2], C.ps[2 + 2 * i2]
                    for k in range(8):
                        mm(P, psG[:, 0:nn], WGU[:, 0, k, f * 128:(f + 1) * 128], hg[:, k, t0 - g0:t0 - g0 + nn], k == 0, k == 7)
                    for k in range(8):
                        mm(P, psU[:, 0:nn], WGU[:, 1, k, f * 128:(f + 1) * 128], hg[:, k, t0 - g0:t0 - g0 + nn], k == 0, k == 7)
                    act(P, sil[i2][:, 0:nn], psG[:, 0:nn], AF.Silu)
                    tt(P, "dve", tu[i2][:, 0:nn], psU[:, 0:nn], sil[i2][:, 0:nn], ALU.mult)
                    tt(P, "dve", HID[:, f, 0:nn], tu[i2][:, 0:nn], psW[:, 0:nn], ALU.mult)
                for ti, t in enumerate(blk):
                    ya = yacc[:, t - tiles[0], :]
                    for half in range(2):
                        po = C.ps[5 + (nb * 8 + ti * 2 + half) % 3]
                        for f in range(4):
                            mm(P, po, HID[:, f, ti * 128:(ti + 1) * 128], WD[:, f, half * 512:(half + 1) * 512], f == 0, f == 3)
                        if e == 0:
                            cp(P, "dve", ya[:, half * 512:(half + 1) * 512], po)
                        else:
                            tt(P, "dve", ya[:, half * 512:(half + 1) * 512], ya[:, half * 512:(half + 1) * 512], po, ALU.add)
                nb += 1
        for t in tiles:
            j = 1 if t < 2 else 0
            x = xt[t % 2]
            dma(P, "sp", x, C.xres[t])
            tt(P, "dve", tm2, yacc[:, t - tiles[0], :], L.grow[1][j], ALU.mult)
            tt(P, "dve", x, x, tm2, ALU.add)
            dma(P, "pool", C.xres[t], x)
    A.close()


def final_norm(C):
    P, I = C.P, C.I
    A = Arena(P)
    g = A.sb("fg", [128, D])
    dma(P, "sp", g, I["final_g"][0:1, :].bc([128, D]))
    xt = [A.sb("fxt%d" % i, [128, D]) for i in range(2)]
    junk = A.sb("fjunk", [128, D])
    st = [A.sb("fst%d" % i, [128, 2]) for i in range(2)]
    for t in range(2, NT):
        x, s = xt[t % 2], st[t % 2]
        dma(P, "sp" if t % 2 == 0 else "pool", x, C.xres[t])
        act(P, junk, x, AF.Square, accum_out=s[:, 0:1])
        ts(P, "dve", s[:, 1:2], s[:, 0:1], 1.0 / D, ALU.mult, EPS, ALU.add)
        act(P, s[:, 1:2], s[:, 1:2], AF.Sqrt)
        P.op("dve", "reciprocal", out=s[:, 1:2], in_=s[:, 1:2])
        stt(P, "dve", x, x, s[:, 1:2], g, ALU.mult, ALU.mult)
        dma(P, "sp" if t % 2 == 1 else "pool", V(C.out.ap[(t - 2) * 128:(t - 1) * 128, :], Buf("o%d" % t)), x)
    A.close()


CH_COLS = 3072


def alloc_chunk_heads(A, dd):
    def mkh(name, shape, dt=F32):
        return [A.sb("%s_%d_%d" % (name, dd, h), shape, dt) for h in range(4)]
    AM = mkh("cAM", [128, 4, 128], BF16)
    XB = mkh("cXB", [128, 2, 128], BF16)
    XX = [[AM[h][:, 0:2, :] for h in range(4)], [XB[h] for h in range(4)]]
    AakT = [AM[h][:, 2, :] for h in range(4)]
    ArbT = [AM[h][:, 3, :] for h in range(4)]
    ArkT, TT = [mkh("cM%d" % i, [128, 128], BF16) for i in range(2)]
    PM = mkh("cPM", [128, 2, 64], BF16)
    Ap = [PM[h][:, 0, :] for h in range(4)]
    M1 = [PM[h][:, 1, :] for h in range(4)]
    U0 = mkh("cU0", [128, 64], BF16)
    RpT = mkh("cRpT", [64, 128])
    DPC = mkh("cDPC", [128, 64])
    Y0cc = mkh("cY0cc", [64, 2, 64])
    Y0c = [[Y0cc[h][:, c, :] for h in range(4)] for c in range(2)]
    GH = mkh("cGH", [64, 4, 64])
    GT = [[GH[h][:, 2 * c, :] for h in range(4)] for c in range(2)]
    Hc = [[GH[h][:, 2 * c + 1, :] for h in range(4)] for c in range(2)]
    gA = mkh("gA", [128, 128])
    gY0 = [mkh("gY0%d" % c, [64, 64]) for c in range(2)]
    gH = [mkh("gH%d" % c, [32, 64]) for c in range(2)]
    return (AM, XB, XX, AakT, ArbT, ArkT, TT, PM, Ap, M1, U0, RpT, DPC, Y0cc, Y0c, GH, GT, Hc, gA, gY0, gH)


def phase_chunk(C, l, L):
    P, I = C.P, C.I
    A = Arena(P)
    mk = A.sb("cmasks", [128, 7, 128])
    dma(P, "sp", mk, I["cmasks"])
    idn = C.ident
    chs = [A.sb("chs%d" % i, [128, CH_COLS]) for i in range(2)]
    ST = [[A.sb("cST%d%d" % (d, h), [64, 64]) for h in range(4)] for d in range(2)]
    for d in range(2):
        for h in range(4):
            P.op("dve", "memset", ap=ST[d][h], constant=0.0)

    def mk2(name, shape, dt=F32):
        return [A.sb("%s%d" % (name, i), shape, dt) for i in range(2)]
    TOT, incS, Ein, Enin, Eex, Eend, Etot, tmpx, tmpy = [mk2("cE%d" % i, [128, 256]) for i in range(9)]
    at, rt, bt, kt, bh, kh = [mk2("cq%d" % i, [128, 256]) for i in range(6)]
    aT, rTb, bT, kT = [mk2("cT%d" % i, [128, 2, 128], BF16) for i in range(4)]
    rT = mk2("cTr", [128, 2, 128])
    bhc = [mk2("cbhc%d" % c, [128, 256], BF16) for c in range(2)]
    khc = [mk2("ckhc%d" % c, [128, 256], BF16) for c in range(2)]
    at_b = mk2("cat_b", [128, 256], BF16)
    v_b = mk2("cv_b", [128, 256], BF16)
    IM = A.sb("cIM", [128, 64])
    tt(P, "dve", IM, idn[:, 0:64], idn[:, 64:128], ALU.add)

    HB = []
    for dd in range(2):
        HB.append(alloc_chunk_heads(A, dd))
    MK4 = [A.sb("cMK4%d" % d, [128, 4, 128]) for d in range(2)]
    for d in range(2):
        ms_, mst_, mit_ = (0, 1, 2) if d == 0 else (3, 4, 5)
        for i_, mi_ in enumerate((ms_, mst_, mst_, mit_)):
            cp(P, "dve", MK4[d][:, i_, :], mk[:, mi_, :])
    yo = [A.sb("cyo%d" % i, [64, 2, 256]) for i in range(2)]
    STg = [[A.sb("gST%d%d" % (d, h), [32, 64]) for h in range(4)] for d in range(2)]
    for d in range(2):
        for h in range(4):
            P.op("dve", "memset", ap=STg[d][h], constant=0.0)
    gTOT, gincS, gEin, gEnin, gEend, gEtot, gtmp = [mk2("gE%d" % i, [128, 128]) for i in range(7)]
    gq, gk, gkh = [mk2("gq%d" % i, [128, 128]) for i in range(3)]
    gkhc = [mk2("gkhc%d" % c, [128, 128]) for c in range(2)]
    gqT, gkT, gPT = [[mk2("gT%d_%d" % (i, h), [32, 128]) for h in range(4)] for i in range(3)]
    gyo = [A.sb("gyo%d" % i, [64, 2, 256]) for i in range(2)]
    ps = C.ps
    border = [1, 0] + list(range(NT - 1, 1, -1))
    H4 = range(4)
    it = 0
    cut = C.dbg.get("chunk_cut", 99)

    def body(n, d):
        if True:
            (AM, XB, XX, AakT, ArbT, ArkT, TT, PM, Ap, M1, U0, RpT, DPC, Y0cc, Y0c, GH, GT, Hc, gA, gY0, gH) = HB[d]
            ps = C.ps[4 * d:4 * d + 4] + C.ps[4 - 4 * d:8 - 4 * d]
            t = n if d == 0 else border[n]
            q = d
            ch = chs[q]
            YO = yo[q]
            dma(P, "sp" if d == 0 else "pool", ch, V(C.CH_ap[t * 128:(t + 1) * 128, :], C.CH_bufs[t]))
            lw = ch[:, d * 256:(d + 1) * 256]
            ke = ch[:, 512 + d * 256:768 + d * 256]
            b_ = ch[:, 1024 + d * 256:1280 + d * 256]
            a_, r_, v_ = ch[:, 1536:1792], ch[:, 1792:2048], ch[:, 2048:2304]
            m_s, m_st, m_it = (0, 1, 2) if d == 0 else (3, 4, 5)
            pc = ps[4 + q]
            mm(P, pc[:, 0:256], mk[:, m_it, :], lw, True, True)
            mm(P, pc[:, 256:512], mk[:, 6, :], lw, True, True)
            cp(P, "dve", TOT[q], pc[:, 256:512])
            cp(P, "dve", incS[q], pc[:, 0:256])
            act(P, Ein[q], incS[q], AF.Exp)
            act(P, Enin[q], incS[q], AF.Exp, scale=-1.0)
            tt(P, "dve", tmpx[q], incS[q], lw, ALU.subtract)
            act(P, Eex[q], tmpx[q], AF.Exp)
            tt(P, "dve", tmpy[q], TOT[q], incS[q], ALU.subtract)
            act(P, Eend[q], tmpy[q], AF.Exp)
            act(P, Etot[q], TOT[q], AF.Exp)
            tt(P, "dve", at[q], a_, Eex[q], ALU.mult)
            cp(P, "act", at_b[q], at[q])
            cp(P, "act", v_b[q], v_)
            tt(P, "dve", rt[q], r_, Ein[q], ALU.mult)
            tt(P, "dve", bt[q], b_, Enin[q], ALU.mult)
            tt(P, "dve", kt[q], ke, Enin[q], ALU.mult)
            tt(P, "dve", bh[q], b_, Eend[q], ALU.mult)
            tt(P, "dve", kh[q], ke, Eend[q], ALU.mult)
            for c in range(2):
                ts(P, "dve", bhc[c][q], bh[q], mk[:, 6, c * 64:c * 64 + 1], ALU.mult)
                ts(P, "dve", khc[c][q], kh[q], mk[:, 6, c * 64:c * 64 + 1], ALU.mult)
            yield
            for qi, (src, dst) in enumerate(((at, aT), (rt, rT), (bt, bT), (kt, kT))):
                pb_ = ps[6 + qi % 2]
                for a2 in range(2):
                    tr(P, pb_[:, a2 * 128:(a2 + 1) * 128], src[q][:, a2 * 128:(a2 + 1) * 128], idn)
                cp(P, "dve", dst[q], pb_[:, 0:256].rr("p (a n) -> p a n", a=2))
                if qi == 1:
                    cp(P, "dve", rTb[q], pb_[:, 0:256].rr("p (a n) -> p a n", a=2))

            def hv(h):
                pair, hl = h // 2, h % 2
                return pair, slice(hl * 64, (hl + 1) * 64), slice(h * 64, (h + 1) * 64)
            if cut <= 2:
                return
            yield
            for h in H4:
                pair, hs, hc = hv(h)
                pA = ps[h]
                mm(P, pA[:, 0:128], aT[q][hs, pair, :], bT[q][hs, pair, :], True, True)
                mm(P, pA[:, 128:256], bT[q][hs, pair, :], aT[q][hs, pair, :], True, True)
                mm(P, pA[:, 256:384], kT[q][hs, pair, :], aT[q][hs, pair, :], True, True)
                mm(P, pA[:, 384:512], bT[q][hs, pair, :], rTb[q][hs, pair, :], True, True)
            for h in H4:
                pA = ps[h]
                tt(P, "dve", AM[h].rr("p a n -> p (a n)"), pA, MK4[d].rr("p a n -> p (a n)"), ALU.mult)
                tt(P, "dve", TT[h], AM[h][:, 1, :], idn, ALU.add)
            for h in H4:
                pair, hs, hc = hv(h)
                mm(P, ps[h][:, 0:128], kT[q][hs, pair, :], rTb[q][hs, pair, :], True, True)
            for h in H4:
                tt(P, "dve", ArkT[h], ps[h][:, 0:128], mk[:, m_it, :], ALU.mult)
            if cut <= 3:
                return
            yield
            for s in range(5):
                yield
                XXc, XXn = XX[s % 2], XX[(s + 1) % 2]
                for h in H4:
                    pX = ps[h]
                    mm(P, pX[:, 128:256], XXc[h][:, 1, :], XXc[h][:, 0, :], True, True)
                    if s < 4:
                        mm(P, pX[:, 256:384], XXc[h][:, 0, :], XXc[h][:, 1, :], True, True)
                for h in H4:
                    pX = ps[h]
                    if s < 4:
                        cp(P, "dve", XXn[h], pX[:, 128:384].rr("p (a n) -> p a n", a=2))
                    else:
                        cp(P, "dve", XXn[h][:, 0, :], pX[:, 128:256])
                for h in H4:
                    mm(P, ps[h][:, 384:512], XXn[h][:, 0, :], TT[h], True, True)
                for h in H4:
                    tt(P, "dve", TT[h], TT[h], ps[h][:, 384:512], ALU.add)
            if cut <= 4:
                return
            yield
            for h in H4:
                pair, hs, hc = hv(h)
                mm(P, ps[h][:, 0:64], TT[h], at_b[q][:, hc], True, True)
                mm(P, ps[h][:, 64:128], AakT[h], v_b[q][:, hc], True, True)
            for h in H4:
                cp(P, "dve", PM[h], ps[h][:, 0:128].rr("p (a n) -> p a n", a=2))
            for h in H4:
                mm(P, ps[h][:, 128:192], TT[h], M1[h], True, True)
            for h in H4:
                cp(P, "dve", U0[h], ps[h][:, 128:192])
            for h in H4:
                pair, hs, hc = hv(h)
                for c in range(2):
                    cs = slice(c * 64, (c + 1) * 64)
                    mm(P, ps[h][0:64, 192 + c * 64:256 + c * 64], ArbT[h][:, cs], U0[h], True, False)
                    mm(P, ps[h][0:64, 192 + c * 64:256 + c * 64], ArkT[h][:, cs], v_b[q][:, hc], False, True)
                mm(P, ps[h][0:64, 320:448], Ap[h], ArbT[h], True, True)
                tt(P, "dve", DPC[h], IM, Etot[q][:, hc], ALU.mult)
            for h in H4:
                pair, hs, hc = hv(h)
                cp(P, "dve", Y0cc[h], ps[h][0:64, 192:320].rr("p (a n) -> p a n", a=2))
                tt(P, "dve", RpT[h], ps[h][0:64, 320:448], rT[q][hs, pair, :], ALU.add)
            if cut <= 5:
                return
            for h in H4:
                pair, hs, hc = hv(h)
                pG = ps[h]
                for c in range(2):
                    cs = slice(c * 64, (c + 1) * 64)
                    o = c * 128
                    mm(P, pG[0:64, o:o + 64], Ap[h], bhc[c][q][:, hc], True, False)
                    mm(P, pG[0:64, o:o + 64], idn[:, cs], DPC[h], False, True)
                    mm(P, pG[0:64, o + 64:o + 128], bhc[c][q][:, hc], U0[h], True, False)
                    mm(P, pG[0:64, o + 64:o + 128], khc[c][q][:, hc], v_b[q][:, hc], False, True)
            for h in H4:
                pG = ps[h]
                cp(P, "dve", GH[h], pG[0:64, 0:256].rr("p (a n) -> p a n", a=4))
            if cut <= 6:
                return
            yield
            for c in ((0, 1) if d == 0 else (1, 0)):
                cs = slice(c * 64, (c + 1) * 64)
                for h in H4:
                    pG = ps[h]
                    S_ = ST[d][h]
                    mm(P, pG[0:64, 256:320], RpT[h][:, cs], S_, True, False)
                    mm(P, pG[0:64, 256:320], idn[0:64, 0:64], Y0c[c][h], False, True)
                    mm(P, pG[0:64, 320:384], GT[c][h], S_, True, False)
                    mm(P, pG[0:64, 320:384], idn[0:64, 0:64], Hc[c][h], False, True)
                for h in H4:
                    pair, hs, hc = hv(h)
                    pG = ps[h]
                    cp(P, "dve", YO[:, c, hc], pG[0:64, 256:320])
                    cp(P, "dve", ST[d][h], pG[0:64, 320:384])
            dma(P, "sp" if d == 0 else "pool",
                V(C.YT_ap[d][t * 128:(t + 1) * 128, :].rearrange("(c p) n -> p c n", p=64), C.YT_bufs[d][t]), YO)
            yield
            GYO = gyo[q]
            glw = ch[:, 2304 + d * 128:2432 + d * 128]
            gq_, gk_, gv_ = ch[:, 2560:2688], ch[:, 2688:2816], ch[:, 2816:3072]
            pcg = ps[4 + q]
            mm(P, pcg[:, 0:128], mk[:, m_it, :], glw, True, True)
            mm(P, pcg[:, 128:256], mk[:, 6, :], glw, True, True)
            cp(P, "dve", gincS[q], pcg[:, 0:128])
            cp(P, "dve", gTOT[q], pcg[:, 128:256])
            act(P, gEin[q], gincS[q], AF.Exp)
            act(P, gEnin[q], gincS[q], AF.Exp, scale=-1.0)
            tt(P, "dve", gtmp[q], gTOT[q], gincS[q], ALU.subtract)
            act(P, gEend[q], gtmp[q], AF.Exp)
            act(P, gEtot[q], gTOT[q], AF.Exp)
            tt(P, "dve", gq[q], gq_, gEin[q], ALU.mult)
            tt(P, "dve", gk[q], gk_, gEnin[q], ALU.mult)
            tt(P, "dve", gkh[q], gk_, gEend[q], ALU.mult)
            for c in range(2):
                ts(P, "dve", gkhc[c][q], gkh[q], mk[:, 6, c * 64:c * 64 + 1], ALU.mult)
            for h in H4:
                pT_ = ps[6 + h % 2]
                g32 = slice(h * 32, (h + 1) * 32)
                tr(P, pT_[0:32, 0:128], gq[q][:, g32], idn)
                tr(P, pT_[0:32, 128:256], gk[q][:, g32], idn)
                tr(P, pT_[0:32, 256:384], gEtot[q][:, g32], idn)
                cp(P, "dve", gqT[h][q], pT_[0:32, 0:128])
                cp(P, "dve", gkT[h][q], pT_[0:32, 128:256])
                cp(P, "dve", gPT[h][q], pT_[0:32, 256:384])
            for h in H4:
                mm(P, ps[h][:, 0:128], gkT[h][q], gqT[h][q], True, True)
            for h in H4:
                tt(P, "dve", gA[h], ps[h][:, 0:128], mk[:, m_it, :], ALU.mult)
            for h in H4:
                hc = slice(h * 64, (h + 1) * 64)
                g32 = slice(h * 32, (h + 1) * 32)
                for c in range(2):
                    cs = slice(c * 64, (c + 1) * 64)
                    mm(P, ps[h][0:64, 128 + c * 64:192 + c * 64], gA[h][:, cs], gv_[:, hc], True, True)
                    mm(P, ps[h][0:32, 256 + c * 64:320 + c * 64], gkhc[c][q][:, g32], gv_[:, hc], True, True)
            for h in H4:
                for c in range(2):
                    cp(P, "dve", gY0[c][h], ps[h][0:64, 128 + c * 64:192 + c * 64])
                    cp(P, "dve", gH[c][h], ps[h][0:32, 256 + c * 64:320 + c * 64])
            for c in ((0, 1) if d == 0 else (1, 0)):
                cs = slice(c * 64, (c + 1) * 64)
                for h in H4:
                    S_ = STg[d][h]
                    mm(P, ps[h][0:64, 384:448], gqT[h][q][:, cs], S_, True, False)
                    mm(P, ps[h][0:64, 384:448], idn[0:64, 0:64], gY0[c][h], False, True)
                for h in H4:
                    hc = slice(h * 64, (h + 1) * 64)
                    S_ = STg[d][h]
                    cp(P, "dve", GYO[:, c, hc], ps[h][0:64, 384:448])
                    stt(P, "dve", S_, S_, gPT[h][q][:, c * 64:c * 64 + 1], gH[c][h], ALU.mult, ALU.add)
            dma(P, "sp" if d == 1 else "pool",
                V(C.YTG_ap[d][t * 128:(t + 1) * 128, :].rearrange("(c p) n -> p c n", p=64), C.YTG_bufs[d][t]), GYO)
    for n in range(NT if cut > 50 else 1):
        gens = [body(n, 0), body(n, 1)]
        while gens:
            nxt = []
            for g in gens:
                try:
                    next(g)
                    nxt.append(g)
                except StopIteration:
                    pass
            gens = nxt
    A.close()


class LayerState:
    pass


def layer(C, l):
    P = C.P
    LA = Arena(P)
    L = LayerState()
    L.mod = LA.sb("mod", [128, 48, 2])
    L.sc1 = LA.sb("sc1", [128, 8, 2])
    L.sc2 = LA.sb("sc2", [128, 8, 2])
    L.grow = [[LA.sb("grow%d%d" % (ii, j), [128, 1024]) for j in range(2)] for ii in range(2)]
    phase_ada(C, l, L)
    if C.dbg.get("dump") and l == C.dbg.get("layer", 0):
        dma(P, "sp", C.dout("d_mod", [128, 48, 2]), L.mod)
        for ii in range(2):
            for j in range(2):
                dma(P, "sp", C.dout("d_grow%d%d" % (ii, j), [128, 1024]), L.grow[ii][j])
    HA = Arena(P)
    hfm = HA.sb("hfm", [128, 8, NTOK], BF16)
    phase_norm(C, l, L, 1, hfm)
    if C.dbg.get("dump") and l == C.dbg.get("layer", 0):
        dma(P, "sp", C.dout("d_hfm", [128, 8, NTOK], BF16), hfm)
    phase_win_tm(C, l, L, hfm)
    if not C.dbg.get("skip_gates"):
        phase_win_gates(C, l, L, hfm)
    HA.close()
    if C.dbg.get("stop_after") == "win":
        LA.close(); return
    if not C.dbg.get("skip_ab"):
        phase_prep(C, l, L)
        if C.dbg.get("stop_after") == "prep":
            LA.close(); return
        if C.dbg.get("old_gla") and not C.dbg.get("skip_scan"):
            phase_scan(C, l, C.dbg.get("nchunks"))
        if not C.dbg.get("old_rwkv"):
            phase_chunk(C, l, L)
        if C.dbg.get("stop_after") == "chunk":
            LA.close(); return
        if C.dbg.get("stop_after") == "scan":
            LA.close(); return
        phase_fin_ab(C, l, L)
    if C.dbg.get("stop_after") == "fin":
        LA.close(); return
    if not C.dbg.get("skip_attn"):
        phase_attn(C, l, L)
    if C.dbg.get("stop_after") == "attn":
        LA.close(); return
    if not C.dbg.get("skip_s5"):
        phase_s5(C, l, L)
    if C.dbg.get("stop_after") == "s5":
        LA.close(); return
    phase_merge(C, l, L)
    if C.dbg.get("stop_after") == "merge":
        LA.close(); return
    WA = Arena(P)
    L.WT = WA.sb("WT", [32, NTOK])
    HA = Arena(P)
    hfm2 = HA.sb("hfm2", [128, 8, NTOK], BF16)
    RA = Arena(P)
    phase_norm(C, l, L, 2, hfm2, per_tile=make_router(C, l, L, RA))
    RA.close()
    if C.dbg.get("dump") and l == C.dbg.get("layer", 0):
        dma(P, "sp", C.dout("d_hfm2", [128, 8, NTOK], BF16), hfm2)
        dma(P, "sp", C.dout("d_WT", [32, NTOK]), L.WT)
    H2 = V(C.H2_ap, C.H2_buf)
    dma(P, "sp", H2, hfm2)
    HA.close()
    if C.dbg.get("stop_after") == "norm2":
        WA.close(); LA.close(); return
    phase_moe(C, l, L, H2, l == DEPTH - 1)
    WA.close()
    LA.close()


def make_maskw():
    m = np.zeros((128, 384), np.float32)
    i = np.arange(128)[:, None]
    j = np.arange(128)[None, :]
    m[:, 0:128] = np.where(j >= i, 0.0, -1e30)
    m[:, 256:384] = np.where(j <= i, 0.0, -1e30)
    return m


def make_rope():
    rows = SEQ // 64
    row = np.repeat(np.arange(rows, dtype=np.float32), 64)
    col = np.tile(np.arange(64, dtype=np.float32), rows)
    inv = (10000.0 ** (-np.arange(16, dtype=np.float32) / 16)).astype(np.float32)
    ang = np.concatenate([row[:, None] * inv, col[:, None] * inv], axis=-1).astype(np.float32)
    return np.cos(ang).astype(np.float32), np.sin(ang).astype(np.float32)


ROPE = make_rope()


def s5_host(A):
    f = np.float32
    lre, lim, ldt = A("s5_lam_re"), A("s5_lam_im"), A("s5_log_dt")
    ldt_e = np.repeat(ldt[..., None], 64, axis=-1)
    rows = np.stack([lre.reshape(DEPTH, 2, 1024), lim.reshape(DEPTH, 2, 1024), ldt_e.reshape(DEPTH, 2, 1024)], axis=2)
    sm = rows.reshape(DEPTH, 2, 3, 8, 128).transpose(0, 4, 1, 2, 3)
    bre, bim = A("s5_b_re"), A("s5_b_im")
    bt = np.zeros((DEPTH, 2, 8, 128, 128), f)
    cre, cim = A("s5_c_re"), A("s5_c_im")
    ct = np.zeros((DEPTH, 2, 2, 8, 128, 128), f)
    for g in range(16):
        j, hh = g // 2, g % 2
        c0 = (g % 8) * 16
        for ri, b in enumerate((bre, bim)):
            bt[:, ri, j, c0:c0 + 16, hh * 64:(hh + 1) * 64] = b[:, g].transpose(0, 2, 1)
        for ri, c in enumerate((cre, cim)):
            ct[:, :, ri, j, hh * 64:(hh + 1) * 64, c0:c0 + 16] = c[:, :, g].transpose(0, 1, 3, 2)
    return {
        "s5_sm": np.ascontiguousarray(sm, f), "s5_rows": np.ascontiguousarray(rows, f),
        "s5_bt": bt, "s5_ct": ct,
        "pw2": (2.0 ** np.arange(16)).astype(f).reshape(1, 16),
        "s5_d_fm": np.ascontiguousarray(np.pad(A("s5_d").reshape(DEPTH, 2, 128).transpose(0, 2, 1), ((0, 0), (0, 0), (0, 14)))),
        "s5_bglu_fm": np.ascontiguousarray(np.pad(A("s5_b_glu").reshape(DEPTH, 2, 128).transpose(0, 2, 1), ((0, 0), (0, 0), (0, 14)))),
        "s5_w_glu": A("s5_w_glu"),
    }


def make_sele():
    s = np.zeros((32, 32, 128), np.float32)
    for e in range(32):
        s[e, e, :] = 1.0
    return s


def make_cmasks():
    r = np.arange(128)[:, None]
    c = np.arange(128)[None, :]
    same = (r // 64) == (c // 64)
    m = np.zeros((128, 7, 128), np.float32)
    m[:, 0] = same & (c < r)
    m[:, 1] = same & (r < c)
    m[:, 2] = same & (r <= c)
    m[:, 3] = same & (c > r)
    m[:, 4] = same & (r > c)
    m[:, 5] = same & (r >= c)
    m[:, 6] = same
    return m


def make_sel():
    s = np.zeros((128, 64, 128), np.float32)
    for j in range(64):
        for hh in range(2):
            s[2 * j + hh, j, hh * 64:(hh + 1) * 64] = 1.0
    return s


def blkdiag(mats):
    n = len(mats)
    L, r, c = mats[0].shape
    o = np.zeros((L, n * r, n * c), np.float32)
    for i, m in enumerate(mats):
        o[:, i * r:(i + 1) * r, i * c:(i + 1) * c] = m
    return o


def host_inputs(inputs, b):
    f = np.float32

    def A(k):
        return np.asarray(inputs[k], f)
    c = np.asarray(inputs["c"], f)[b]
    cctx = np.asarray(inputs["c_ctx"], f)
    cc = np.stack([c.reshape(8, 128).T, cctx.reshape(8, 128).T], axis=-1)
    m = {
        "xb": np.ascontiguousarray(np.asarray(inputs["x"], f)[b]),
        "ctxb": np.ascontiguousarray(np.asarray(inputs["ctx"], f)[b]),
        "cc": np.ascontiguousarray(cc),
        "w_ada": np.asarray(inputs["w_ada"], f),
        "b_ada": np.asarray(inputs["b_ada"], f),
        "b_ada_fm": np.ascontiguousarray(np.asarray(inputs["b_ada"], f).reshape(DEPTH, 48, 128).transpose(0, 2, 1)),
        "g1_fm": np.ascontiguousarray(np.asarray(inputs["norm1_g"], f).reshape(DEPTH, 8, 128).transpose(0, 2, 1)),
        "g2_fm": np.ascontiguousarray(np.asarray(inputs["norm2_g"], f).reshape(DEPTH, 8, 128).transpose(0, 2, 1)),
        "w_in": np.asarray(inputs["w_in"], f),
        "ident": np.eye(128, dtype=f),
        "sel": make_sel(),
        "maskw": make_maskw(),
        "ropec": ROPE[0],
        "ropes": ROPE[1],
        "attn_sink": np.ascontiguousarray(np.pad(A("attn_sink"), ((0, 0), (0, 12)))),
        **s5_host(A),
        "cmasks": make_cmasks(),
        "w_branch": A("w_branch"),
        "w_out": A("w_out"),
        "w_router": np.ascontiguousarray(np.concatenate([A("w_router_g"), A("w_router_e")], axis=2)),
        "b_router": np.ascontiguousarray(np.concatenate([A("b_router_g"), A("b_router_e")], axis=1)),
        "w_exp_gate": A("w_exp_gate"),
        "w_exp_up": A("w_exp_up"),
        "w_exp_down": A("w_exp_down"),
        "pv": np.ascontiguousarray(np.concatenate([
            A("rwkv_mu").reshape(DEPTH, -1), A("rwkv_kk"), A("rwkv_ka"), A("rwkv_rk").reshape(DEPTH, -1),
            A("rwkv_w0").reshape(DEPTH, -1), A("rwkv_a0").reshape(DEPTH, -1), A("rwkv_ln_g"),
            A("gla_ab").reshape(DEPTH, -1), A("gla_ln_g")], axis=1)),
        "w1cat": np.ascontiguousarray(np.concatenate([A("rwkv_w1")[:, 0], A("rwkv_w1")[:, 1],
                                                      A("rwkv_a1")[:, 0], A("rwkv_a1")[:, 1]], axis=2)),
        "w2blk": blkdiag([A("rwkv_w2")[:, 0], A("rwkv_w2")[:, 1], A("rwkv_a2")[:, 0], A("rwkv_a2")[:, 1]]),
        "g1": A("rwkv_g1"),
        "g2": A("rwkv_g2"),
        "a2blk": blkdiag([A("gla_a2")[:, 0], A("gla_a2")[:, 1]]),
        "final_g": np.asarray(inputs["final_norm_g"], f).reshape(1, D),
    }
    return m


def kernel(**inputs):
    nc = build_program()
    in_maps = [host_inputs(inputs, b) for b in range(8)]
    res = run_bass_kernel_spmd(nc, in_maps, core_ids=list(range(8)))
    return np.stack([r["out"] for r in res.results], axis=0)
```
